# Optimizing a Trainium2 kernel written in Bass

```python
import jax, jax.numpy as jnp
from jax import lax
import numpy as np

D_MODEL = 1024
BATCH = 8
SEQ = 2048
DEPTH = 1

GRID_W = 64
CTX_LEN = 256
CHUNK = 64
HG_HEADS = 4
HG_DK = 128
HG_DV = 128
HG_WIDTH = HG_HEADS * HG_DK
ML_HEADS = 4
ML_DH = 128
ML_WIDTH = ML_HEADS * ML_DH
D_MIX = HG_WIDTH + ML_WIDTH
CONV_K = 3
PEER_HEADS = 8
PEER_NKEYS = 128
PEER_EXPERTS = PEER_NKEYS * PEER_NKEYS
PEER_DQ = 256
PEER_TOPK = 16
PEER_BLOCK = 128
DEEPNORM_ALPHA = (2.0 * DEPTH) ** 0.25
DEEPNORM_BETA = (8.0 * DEPTH) ** -0.25
LN_EPS = 1e-6
IN_SPLITS = (HG_WIDTH, HG_WIDTH, HG_WIDTH, HG_WIDTH, HG_WIDTH,
             ML_WIDTH, ML_WIDTH, ML_WIDTH, ML_WIDTH, 4 * ML_HEADS)
D_IN = 5 * HG_WIDTH + 4 * ML_WIDTH + 4 * ML_HEADS

kernel_name = "hybrid_hgrn2_mlstm_peer_diffusion_layer"


def _layer_norm(x):
    xf = x.astype(jnp.float32)
    mu = jnp.mean(xf, axis=-1, keepdims=True)
    var = jnp.mean(jnp.square(xf - mu), axis=-1, keepdims=True)
    return ((xf - mu) * lax.rsqrt(var + LN_EPS)).astype(x.dtype)


def _affine_ln(x, g, b):
    return _layer_norm(x) * g + b


def _modulate(x, shift, scale):
    return _layer_norm(x) * (1 + scale) + shift


def _to_heads(t, n_heads):
    b, l, w = t.shape
    return t.reshape(b, l, n_heads, w // n_heads).transpose(0, 2, 1, 3)


def _from_heads(t):
    b, h, l, d = t.shape
    return t.transpose(0, 2, 1, 3).reshape(b, l, h * d)


def _head_rms_norm(o, g):
    of = o.astype(jnp.float32)
    y = of * lax.rsqrt(jnp.mean(of * of, axis=-1, keepdims=True) + LN_EPS)
    return y * g.reshape(o.shape[1], 1, o.shape[-1])


def _head_layer_norm(o, g):
    return _layer_norm(o.astype(jnp.float32)) * g.reshape(o.shape[1], 1, o.shape[-1])


def _split_projection(p):
    offsets = np.cumsum(IN_SPLITS)[:-1].tolist()
    return jnp.split(p, offsets, axis=-1)


def _conv_grid(t, w, b):
    bsz, l, ch = t.shape
    rows = l // GRID_W
    y = lax.conv_general_dilated(t.reshape(bsz, rows, GRID_W, ch), w[:, :, None, :], (1, 1), 'SAME',
                                 dimension_numbers=('NHWC', 'HWIO', 'NHWC'), feature_group_count=ch)
    return y.reshape(bsz, l, ch) + b


def _conv_seq(t, w_row, b):
    ch = t.shape[-1]
    y = lax.conv_general_dilated(t, w_row[:, None, :], (1,), 'SAME',
                                 dimension_numbers=('NWC', 'WIO', 'NWC'), feature_group_count=ch)
    return y + b


def _join(a_ctx, a_lat, reverse):
    if reverse:
        a_ctx, a_lat = jnp.flip(a_ctx, axis=2), jnp.flip(a_lat, axis=2)
    return jnp.concatenate([a_ctx, a_lat], axis=2)


def _unjoin(o, n_ctx, reverse):
    o_ctx, o_lat = o[:, :, :n_ctx], o[:, :, n_ctx:]
    if reverse:
        o_ctx, o_lat = jnp.flip(o_ctx, axis=2), jnp.flip(o_lat, axis=2)
    return o_ctx, o_lat


def _bidirectional(scan_fn, shared_ctx, shared_lat, dir_ctx, dir_lat):
    n_ctx = shared_ctx[0].shape[2]
    outs = []
    for d, rev in enumerate((False, True)):
        args = [_join(a_c, a_l, rev) for a_c, a_l in zip(shared_ctx + dir_ctx[d], shared_lat + dir_lat[d])]
        outs.append(_unjoin(scan_fn(*args), n_ctx, rev))
    return outs[0][0] + outs[1][0], outs[0][1] + outs[1][1]


def _gla_chunkwise(q, v, k, log_f):
    bsz, h, t, dk = q.shape
    dv = v.shape[-1]
    n = t // CHUNK
    q, k, log_f = (a.reshape(bsz, h, n, CHUNK, dk) for a in (q, k, log_f))
    v = v.reshape(bsz, h, n, CHUNK, dv)
    b = jnp.cumsum(log_f.astype(jnp.float32), axis=3)
    b_ref = b[:, :, :, CHUNK // 2:CHUNK // 2 + 1]
    lower = jnp.tril(jnp.ones((CHUNK, CHUNK), dtype=bool))
    scores = jnp.einsum('bhnck,bhnsk->bhncs', q * jnp.exp(b - b_ref), k * jnp.exp(b_ref - b))
    o = jnp.einsum('bhncs,bhnsv->bhncv', jnp.where(lower, scores, 0.0), v)
    b_last = b[:, :, :, -1:]
    upd = jnp.einsum('bhnsk,bhnsv->bhnkv', k * jnp.exp(b_last - b), v)

    def step(s, xs):
        decay, u = xs
        return decay[..., None] * s + u, s

    s0 = jnp.zeros((bsz, h, dk, dv), upd.dtype)
    _, s_prev = lax.scan(step, s0, (jnp.moveaxis(jnp.exp(b_last[:, :, :, 0]), 2, 0), jnp.moveaxis(upd, 2, 0)))
    s_prev = jnp.moveaxis(s_prev, 0, 2)
    o = o + jnp.einsum('bhnck,bhnkv->bhncv', q * jnp.exp(b), s_prev)
    return o.reshape(bsz, h, t, dv)


def _mlstm_chunkwise(q, k, v, log_i, log_f):
    bsz, h, t, d = q.shape
    n = t // CHUNK
    q, k, v = (a.reshape(bsz, h, n, CHUNK, d) for a in (q, k, v))
    log_i = log_i.astype(jnp.float32).reshape(bsz, h, n, CHUNK)
    log_f = log_f.astype(jnp.float32).reshape(bsz, h, n, CHUNK)
    a = jnp.cumsum(log_f, axis=-1)
    a_last = a[..., -1]
    lower = jnp.tril(jnp.ones((CHUNK, CHUNK), dtype=bool))
    log_d = jnp.where(lower, a[..., :, None] - a[..., None, :] + log_i[..., None, :], -jnp.inf)
    g = a_last[..., None] - a + log_i
    m_loc = jnp.max(g, axis=-1)
    w = jnp.exp(g - m_loc[..., None])
    upd_c = jnp.einsum('bhnc,bhnck,bhncv->bhnkv', w, k, v)
    upd_n = jnp.einsum('bhnc,bhnck->bhnk', w, k)

    def step(carry, xs):
        c_st, n_st, m = carry
        al, ml, uc, un = xs
        m_new = jnp.maximum(al + m, ml)
        s_old = jnp.exp(al + m - m_new)
        s_new = jnp.exp(ml - m_new)
        c_new = s_old[..., None, None] * c_st + s_new[..., None, None] * uc
        n_new = s_old[..., None] * n_st + s_new[..., None] * un
        return (c_new, n_new, m_new), (c_st, n_st, m)

    init = (jnp.zeros((bsz, h, d, d), upd_c.dtype), jnp.zeros((bsz, h, d), upd_n.dtype),
            jnp.zeros((bsz, h), jnp.float32))
    xs = tuple(jnp.moveaxis(a_, 2, 0) for a_ in (a_last, m_loc, upd_c, upd_n))
    _, (c_prev, n_prev, m_prev) = lax.scan(step, init, xs)
    c_prev, n_prev, m_prev = (jnp.moveaxis(a_, 0, 2) for a_ in (c_prev, n_prev, m_prev))
    m_inter = a + m_prev[..., None]
    m_t = jnp.maximum(jnp.max(log_d, axis=-1), m_inter)
    w_intra = jnp.einsum('bhnck,bhnsk->bhncs', q, k) * jnp.exp(log_d - m_t[..., None])
    w_inter = jnp.exp(m_inter - m_t)
    num = (jnp.einsum('bhncs,bhnsv->bhncv', w_intra, v)
           + w_inter[..., None] * jnp.einsum('bhnck,bhnkv->bhncv', q, c_prev))
    den = jnp.sum(w_intra, axis=-1) + w_inter * jnp.einsum('bhnck,bhnk->bhnc', q, n_prev)
    hout = num / jnp.maximum(jnp.abs(den), jnp.exp(-m_t))[..., None]
    return hout.reshape(bsz, h, t, d)


def _hgrn_gates(z, lb):
    zf = z.astype(jnp.float32)
    log_f = jnp.log(lb + (1.0 - lb) * jax.nn.sigmoid(zf))
    key = (1.0 - lb) * jax.nn.sigmoid(-zf)
    return key, log_f


def _mlstm_gates(gt, gate_b):
    g = jnp.moveaxis((gt + gate_b).astype(jnp.float32), -1, 1)
    hh = ML_HEADS
    return ((g[:, 0:hh], jax.nn.log_sigmoid(g[:, 2 * hh:3 * hh])),
            (g[:, hh:2 * hh], jax.nn.log_sigmoid(g[:, 3 * hh:4 * hh])))


def _readout(o_hg, h_ml, hg_gate, ml_gate, hg_norm_g, ml_norm_g):
    a = _from_heads(_head_rms_norm(o_hg, hg_norm_g)) * jax.nn.silu(hg_gate)
    b = _from_heads(_head_layer_norm(h_ml, ml_norm_g)) * jax.nn.sigmoid(ml_gate)
    return jnp.concatenate([a, b], axis=-1)


def _token_mixers(p_ctx, p_lat, lb, hg_norm_g, conv_w, conv_b, gate_b, ml_norm_g):
    hq_c, hi_c, hg_c, hf0_c, hf1_c, mq_c, mk_c, mv_c, mo_c, mgt_c = _split_projection(p_ctx)
    hq_l, hi_l, hg_l, hf0_l, hf1_l, mq_l, mk_l, mv_l, mo_l, mgt_l = _split_projection(p_lat)

    def hg_dirs(f0, f1):
        return tuple(tuple(_to_heads(a_, HG_HEADS) for a_ in _hgrn_gates(f, lb[d]))
                     for d, f in enumerate((f0, f1)))

    o_hg_c, o_hg_l = _bidirectional(
        _gla_chunkwise,
        (_to_heads(hq_c, HG_HEADS), _to_heads(hi_c, HG_HEADS)),
        (_to_heads(hq_l, HG_HEADS), _to_heads(hi_l, HG_HEADS)),
        hg_dirs(hf0_c, hf1_c), hg_dirs(hf0_l, hf1_l))

    qk_c = jax.nn.silu(_conv_seq(jnp.concatenate([mq_c, mk_c], axis=-1), conv_w[CONV_K // 2], conv_b))
    qk_l = jax.nn.silu(_conv_grid(jnp.concatenate([mq_l, mk_l], axis=-1), conv_w, conv_b))
    kscale = ML_DH ** -0.5

    def ml_shared(qk, v):
        q_, k_ = jnp.split(qk, 2, axis=-1)
        return (_to_heads(q_, ML_HEADS), _to_heads(k_ * kscale, ML_HEADS), _to_heads(v, ML_HEADS))

    h_ml_c, h_ml_l = _bidirectional(
        _mlstm_chunkwise, ml_shared(qk_c, mv_c), ml_shared(qk_l, mv_l),
        _mlstm_gates(mgt_c, gate_b), _mlstm_gates(mgt_l, gate_b))

    mix_c = _readout(o_hg_c, h_ml_c, hg_c, mo_c, hg_norm_g, ml_norm_g)
    mix_l = _readout(o_hg_l, h_ml_l, hg_l, mo_l, hg_norm_g, ml_norm_g)
    return mix_c, mix_l


def _peer(h, wq, keys, u_tab, v_tab):
    bsz, l, dm = h.shape
    q = (h @ wq).reshape(bsz, l, PEER_HEADS, 2, PEER_DQ // 2)
    s = jnp.einsum('blhpd,hpkd->blhpk', q, keys).astype(jnp.float32)
    top_s, top_i = lax.top_k(s, PEER_TOPK)
    cand = (top_s[..., 0, :, None] + top_s[..., 1, None, :]).reshape(bsz, l, PEER_HEADS, PEER_TOPK * PEER_TOPK)
    best_s, best_c = lax.top_k(cand, PEER_TOPK)
    idx = (jnp.take_along_axis(top_i[..., 0, :], best_c // PEER_TOPK, axis=-1) * PEER_NKEYS
           + jnp.take_along_axis(top_i[..., 1, :], best_c % PEER_TOPK, axis=-1))
    gate = jax.nn.softmax(best_s, axis=-1)
    n_blk = (bsz * l) // PEER_BLOCK
    hb = h.reshape(n_blk, PEER_BLOCK, dm)
    ib = idx.reshape(n_blk, PEER_BLOCK, PEER_HEADS * PEER_TOPK)
    gb = gate.reshape(n_blk, PEER_BLOCK, PEER_HEADS * PEER_TOPK).astype(h.dtype)

    def block(args):
        hx, ix, gx = args
        act = jax.nn.gelu(jnp.einsum('pd,ped->pe', hx, u_tab[ix]), approximate=False)
        return jnp.einsum('pe,ped->pd', gx * act, v_tab[ix])

    return lax.map(block, (hb, ib, gb)).reshape(bsz, l, dm)


def setup_inputs(seed: int = 0) -> dict:
    key = jax.random.key(seed)
    ks = jax.random.split(key, 26)
    f32 = jnp.float32

    def nrm(k, shape, scale):
        return jax.random.normal(k, shape, f32) * scale

    forget_bias = jnp.tile(jnp.linspace(3.0, 6.0, ML_HEADS, dtype=f32), (DEPTH, 2))
    ml_gate_b = jnp.concatenate([nrm(ks[11], (DEPTH, 2 * ML_HEADS), 0.1),
                                 forget_bias + nrm(ks[12], (DEPTH, 2 * ML_HEADS), 0.1)], axis=-1)
    return {
        "x": nrm(ks[0], (BATCH, SEQ, D_MODEL), 1.0),
        "c": nrm(ks[1], (BATCH, D_MODEL), 1.0),
        "ctx": nrm(ks[2], (BATCH, CTX_LEN, D_MODEL), 1.0),
        "c_ctx": nrm(ks[3], (D_MODEL,), 1.0),
        "w_mod": nrm(ks[4], (DEPTH, D_MODEL, 6 * D_MODEL), D_MODEL ** -0.5),
        "b_mod": nrm(ks[5], (DEPTH, 6 * D_MODEL), 0.02),
        "w_in": nrm(ks[6], (DEPTH, D_MODEL, D_IN), D_MODEL ** -0.5),
        "hg_lb_logits": nrm(ks[7], (2, DEPTH + 1, HG_WIDTH), 0.1),
        "hg_norm_g": 1.0 + nrm(ks[8], (DEPTH, HG_WIDTH), 0.02),
        "ml_conv_w": nrm(ks[9], (DEPTH, CONV_K, CONV_K, 2 * ML_WIDTH), 1.0 / CONV_K),
        "ml_conv_b": nrm(ks[10], (DEPTH, 2 * ML_WIDTH), 0.02),
        "ml_gate_b": ml_gate_b,
        "ml_norm_g": 1.0 + nrm(ks[13], (DEPTH, ML_WIDTH), 0.02),
        "w_out": nrm(ks[14], (DEPTH, D_MIX, D_MODEL), DEEPNORM_BETA * D_MIX ** -0.5),
        "ln1_g": 1.0 + nrm(ks[15], (DEPTH, D_MODEL), 0.02),
        "ln1_b": nrm(ks[16], (DEPTH, D_MODEL), 0.02),
        "peer_wq": nrm(ks[17], (DEPTH, D_MODEL, PEER_HEADS * PEER_DQ), D_MODEL ** -0.5),
        "peer_keys": nrm(ks[18], (DEPTH, PEER_HEADS, 2, PEER_NKEYS, PEER_DQ // 2), (PEER_DQ // 2) ** -0.5),
        "peer_u": nrm(ks[19], (DEPTH, PEER_EXPERTS, D_MODEL), D_MODEL ** -0.5),
        "peer_v": nrm(ks[20], (DEPTH, PEER_EXPERTS, D_MODEL), DEEPNORM_BETA),
        "ln2_g": 1.0 + nrm(ks[21], (DEPTH, D_MODEL), 0.02),
        "ln2_b": nrm(ks[22], (DEPTH, D_MODEL), 0.02),
    }


def reference(x, c, ctx, c_ctx, w_mod, b_mod, w_in, hg_lb_logits, hg_norm_g, ml_conv_w, ml_conv_b,
              ml_gate_b, ml_norm_g, w_out, ln1_g, ln1_b, peer_wq, peer_keys, peer_u, peer_v, ln2_g, ln2_b):
    lower_bounds = jnp.cumsum(jax.nn.softmax(hg_lb_logits.astype(jnp.float32), axis=1), axis=1)
    for l in range(DEPTH):
        mod_l = jax.nn.silu(c) @ w_mod[l] + b_mod[l]
        mod_c = jax.nn.silu(c_ctx) @ w_mod[l] + b_mod[l]
        sh1_l, sc1_l, g1_l, sh2_l, sc2_l, g2_l = jnp.split(mod_l[:, None, :], 6, axis=-1)
        sh1_c, sc1_c, g1_c, sh2_c, sc2_c, g2_c = jnp.split(mod_c[None, None, :], 6, axis=-1)

        p_c = _modulate(ctx, sh1_c, sc1_c) @ w_in[l]
        p_l = _modulate(x, sh1_l, sc1_l) @ w_in[l]
        mix_c, mix_l = _token_mixers(p_c, p_l, lower_bounds[:, l], hg_norm_g[l], ml_conv_w[l],
                                     ml_conv_b[l], ml_gate_b[l], ml_norm_g[l])
        x_new = _affine_ln(DEEPNORM_ALPHA * x + g1_l * (mix_l @ w_out[l]), ln1_g[l], ln1_b[l])
        if l < DEPTH - 1:
            ctx = _affine_ln(DEEPNORM_ALPHA * ctx + g1_c * (mix_c @ w_out[l]), ln1_g[l], ln1_b[l])
            ctx = _affine_ln(DEEPNORM_ALPHA * ctx
                             + g2_c * _peer(_modulate(ctx, sh2_c, sc2_c), peer_wq[l], peer_keys[l], peer_u[l], peer_v[l]),
                             ln2_g[l], ln2_b[l])
        x = x_new

        y = _peer(_modulate(x, sh2_l, sc2_l), peer_wq[l], peer_keys[l], peer_u[l], peer_v[l])
        x = _affine_ln(DEEPNORM_ALPHA * x + g2_l * y, ln2_g[l], ln2_b[l])
    return x
```

```python
import os
import numpy as np
from contextlib import ExitStack
import concourse.bass as bass
import concourse.mybir as mybir
from concourse.bass_utils import run_bass_kernel_spmd

F32 = mybir.dt.float32; BF16 = mybir.dt.bfloat16; I32 = mybir.dt.int32; U32 = mybir.dt.uint32
AF = mybir.ActivationFunctionType; ALU = mybir.AluOpType; AX = mybir.AxisListType

NTOK = 2304; NLAT = 2048; NCH = 36; NLCH = 32
ALPHA = 2.0 ** 0.25
EPS = 1e-6


class Sched:
    NDMA = 32

    def __init__(self, nc, es):
        self.nc = nc
        self.engs = {'pe': nc.tensor, 'act': nc.scalar, 'dve': nc.vector, 'pool': nc.gpsimd, 'sp': nc.sync}
        self.sem = {k: es.enter_context(nc.semaphore("sem_" + k)) for k in self.engs}
        self.cnt = {k: 0 for k in self.engs}
        self.dsem = [es.enter_context(nc.semaphore("dsem%d" % i)) for i in range(self.NDMA)]
        self.dcnt = [0] * self.NDMA
        self.dnext = 0
        self.seen = {k: {} for k in self.engs}
        self.bufs = {}

    def _deps(self, reads, writes):
        deps = []
        for r in reads:
            b = self.bufs.get(r)
            if b and b['w'] is not None:
                deps.append(b['w'])
        for w in writes:
            b = self.bufs.get(w)
            if b:
                if b['w'] is not None:
                    deps.append(b['w'])
                deps.extend(b['r'])
        return deps

    def _wait(self, eng, deps, skip_self=False):
        best = {}
        for (sid, sem, val, owner) in deps:
            if skip_self and owner == eng:
                continue
            if best.get(sid, (None, 0))[1] < val:
                best[sid] = (sem, val)
        for sid, (sem, val) in best.items():
            if self.seen[eng].get(sid, 0) >= val:
                continue
            self.engs[eng].wait_ge(sem, val)
            self.seen[eng][sid] = val

    def _record(self, dep, reads, writes):
        for r in reads:
            b = self.bufs.setdefault(r, {'w': None, 'r': []})
            b['r'] = [d for d in b['r'] if d[0] != dep[0]] + [dep]
        for w in writes:
            self.bufs[w] = {'w': dep, 'r': []}

    @staticmethod
    def _split(keys):
        norm, ps = [], []
        for k in keys:
            if k.startswith('pb') and len(k) > 2 and k[2].isdigit():
                ps.append(k[:3])
            else:
                norm.append(k)
        return norm, ps

    def op(self, eng, fn, reads=(), writes=(), skip_self=False):
        reads, pr = self._split(reads)
        writes, pw = self._split(writes)
        banks = sorted(set(pr + pw))
        deps = self._deps(reads, writes)
        deps += [d for d in self._deps((), banks) if d[3] != eng]
        self._wait(eng, deps, skip_self)
        ins = fn(self.engs[eng])
        self.cnt[eng] += 1
        ins.then_inc(self.sem[eng], 1)
        self._record(('e_' + eng, self.sem[eng], self.cnt[eng], eng), reads, list(writes) + banks)
        return ins

    def dma(self, q, fn, reads=(), writes=()):
        deps = self._deps(reads, writes)
        j = self.dnext
        self.dnext = (self.dnext + 1) % self.NDMA
        if self.dcnt[j] > 0:
            deps = deps + [('d%d' % j, self.dsem[j], self.dcnt[j], 'dma')]
        self._wait(q, deps)
        ins = fn(self.engs[q])
        self.dcnt[j] += 16
        ins.then_inc(self.dsem[j], 16)
        self._record(('d%d' % j, self.dsem[j], self.dcnt[j], 'dma'), reads, writes)

    def wait_all(self, eng, keys):
        deps = []
        for k in keys:
            b = self.bufs.get(k)
            if b:
                if b['w'] is not None:
                    deps.append(b['w'])
                deps.extend(b['r'])
        self._wait(eng, deps)

    def barrier(self):
        for e in self.engs:
            deps = [('e_' + f, self.sem[f], self.cnt[f], f) for f in self.engs if f != e and self.cnt[f] > 0]
            deps += [('d%d' % j, self.dsem[j], self.dcnt[j], 'dma') for j in range(self.NDMA) if self.dcnt[j] > 0]
            self._wait(e, deps)


def K(name, a, b=None):
    if b is None:
        return ["%s%d" % (name, a)]
    return ["%s%d" % (name, i) for i in range(a, b)]


def build(debug=None):
    nc = bass.Bass("TRN2", target_bir_lowering=False)
    D = {}

    def din(name, shape, dt=F32):
        D[name] = nc.dram_tensor(name, shape, dt, kind="ExternalInput").ap()
        return D[name]

    xs = din("xs", [NTOK, 1024]); cT_d = din("cT", [128, 8, 2]); wmod_d = din("w_mod", [128, 8, 6144])
    bmod_d = din("b_modT", [128, 48]); win_d = din("w_in", [128, 8, 4624]); lg_d = din("lgT", [128, 2, 2, 4])
    hgn_d = din("hgn", [64, 512]); mln_d = din("mln", [64, 512]); convw_d = din("convw", [128, 9, 8])
    convb_d = din("convb", [128, 8]); gateb_d = din("gateb", [8, 2]); wout_d = din("w_out", [128, 8, 1024])
    ln1g_d = din("ln1g", [128, 1024]); ln1b_d = din("ln1b", [128, 1024]); ln2g_d = din("ln2g", [128, 1024])
    ln2b_d = din("ln2b", [128, 1024]); wq_d = din("wq", [128, 8, 2048]); keysT_d = din("keysT", [128, 16, 128])
    NEXP = 128 if debug else 16384
    pu_d = din("pu", [NEXP, 1024]); pv_d = din("pv", [NEXP, 1024])
    ident_d = din("ident", [128, 128]); masks_d = din("masks", [64, 2, 64]); rmask_d = din("rmask", [128, 512])
    sel8_d = din("sel8", [8, 8]); dirm_d = din("dirm", [8, 2])
    out_d = nc.dram_tensor("out", [NLAT, 1024], F32, kind="ExternalOutput").ap()
    dbg_d = None
    if debug:
        dbg_d = nc.dram_tensor("dbg", [NLAT, 1024] if debug != 'mix' else [1024, NLAT], F32, kind="ExternalOutput").ap()

    with ExitStack() as es:
        S = Sched(nc, es)

        uid = [0]

        def sb(st, name, shape, dt=F32):
            uid[0] += 1
            return st.enter_context(nc.sbuf_tensor("s%d_%s" % (uid[0], name), shape, dt))

        PB = [es.enter_context(nc.psum_tensor("pb%d" % i, [128, 512], F32)) for i in range(8)]

        ident = sb(es, "ident", [128, 128]); identb = sb(es, "identb", [128, 128], BF16)
        masks = sb(es, "masks", [64, 2, 64]); rmask = sb(es, "rmask", [128, 512])
        ones = sb(es, "ones", [128, 128]); epsc = sb(es, "epsc", [128, 1])
        modv = sb(es, "modv", [128, 48, 2])
        S.dma('sp', lambda e: e.dma_start(out=ident[:], in_=ident_d[:, :]), writes=['ident'])
        S.dma('sp', lambda e: e.dma_start(out=masks[:], in_=masks_d[:, :, :]), writes=['masks'])
        S.dma('sp', lambda e: e.dma_start(out=rmask[:], in_=rmask_d[:, :]), writes=['rmask'])
        S.op('dve', lambda e: e.tensor_copy(out=identb[:], in_=ident[:]), reads=['ident'], writes=['identb'])
        S.op('dve', lambda e: e.memset(ones[:], 1.0), writes=['ones'])
        S.op('dve', lambda e: e.memset(epsc[:], EPS), writes=['epsc'])

        with ExitStack() as p0:
            cT = sb(p0, "cT", [128, 8, 2]); scT = sb(p0, "scT", [128, 8, 2]); bmodT = sb(p0, "bmodT", [128, 48])
            wm = [sb(p0, "wm%d" % i, [128, 6144]) for i in range(2)]
            S.dma('sp', lambda e: e.dma_start(out=cT[:], in_=cT_d[:, :, :]), writes=['cT'])
            S.dma('sp', lambda e: e.dma_start(out=bmodT[:], in_=bmod_d[:, :]), writes=['bmodT'])
            S.op('act', lambda e: e.activation(out=scT[:], in_=cT[:], func=AF.Silu), reads=['cT'], writes=['scT'])
            for kc in range(8):
                w = wm[kc % 2]; wk = 'wm%d' % (kc % 2)
                S.dma('sp' if kc % 2 == 0 else 'pool', lambda e, w=w, kc=kc: e.dma_start(out=w[:], in_=wmod_d[:, kc, :]), writes=[wk])
                for j in range(48):
                    S.op('pe', lambda e, w=w, kc=kc, j=j: e.matmul(PB[kc // 4][:, (kc % 4) * 96 + 2 * j:(kc % 4) * 96 + 2 * j + 2], lhsT=w[:, j * 128:(j + 1) * 128], rhs=scT[:, kc, :],
                                                                 start=True, stop=True),
                         reads=[wk, 'scT'], writes=['pb%d' % (kc // 4)], skip_self=True)
            mflat = modv[:].rearrange("p j n -> p (j n)")
            S.op('dve', lambda e: e.tensor_tensor(out=modv[:], in0=PB[0][:, 0:96].rearrange("p (j n) -> p j n", n=2), in1=bmodT[:].unsqueeze(2).to_broadcast([128, 48, 2]), op=ALU.add),
                 reads=['pb0', 'bmodT'], writes=['modv'])
            for kc in range(1, 8):
                S.op('dve', lambda e, kc=kc: e.tensor_tensor(out=mflat, in0=mflat, in1=PB[kc // 4][:, (kc % 4) * 96:(kc % 4) * 96 + 96], op=ALU.add),
                     reads=['pb%d' % (kc // 4), 'modv'], writes=['modv'])
            S.op('dve', lambda e: e.tensor_scalar_add(out=modv[:, 8:16, :], in0=modv[:, 8:16, :], scalar1=1.0), reads=['modv'], writes=['modv'])
            S.op('dve', lambda e: e.tensor_scalar_add(out=modv[:, 32:40, :], in0=modv[:, 32:40, :], scalar1=1.0), reads=['modv'], writes=['modv'])
            S.barrier()
        if debug == 'p0':
            S.dma('sp', lambda e: e.dma_start(out=dbg_d[0:128, 0:96], in_=modv[:].rearrange("p a b -> p (a b)")), reads=['modv'], writes=['dbg'])
            S.wait_all('sp', ['dbg']); S.barrier()
            return nc

        scA = ExitStack(); scB = ExitStack()
        mixT = sb(scA, "mixT", [128, 8, NLAT], BF16)
        hT = sb(scB, "hT", [128, 8, NTOK], BF16)
        wstg = sb(scB, "wstg", [128, 8, 128])

        def layer_norm_rows(st_ap, mv_ap, rstd_ap, src, dst, skey, dkey, tag):
            S.op('dve', lambda e: e.bn_stats(out=st_ap[:, 0, :], in_=src[:, 0:512]), reads=[skey], writes=[tag + 'st'])
            S.op('dve', lambda e: e.bn_stats(out=st_ap[:, 1, :], in_=src[:, 512:1024]), reads=[skey], writes=[tag + 'st'])
            S.op('dve', lambda e: e.bn_aggr(out=mv_ap, in_=st_ap[:].rearrange("p a b -> p (a b)")), reads=[tag + 'st'], writes=[tag + 'mv'])
            S.op('act', lambda e: e.activation(out=rstd_ap, in_=mv_ap[:, 1:2], func=AF.Sqrt, bias=epsc[:, 0:1], scale=1.0),
                 reads=[tag + 'mv', 'epsc'], writes=[tag + 'rs'])
            S.op('dve', lambda e: e.reciprocal(out=rstd_ap, in_=rstd_ap), reads=[tag + 'rs'], writes=[tag + 'rs'])
            S.op('dve', lambda e: e.tensor_scalar(out=dst, in0=src, scalar1=mv_ap[:, 0:1], scalar2=rstd_ap, op0=ALU.subtract, op1=ALU.mult),
                 reads=[skey, tag + 'mv', tag + 'rs'], writes=[dkey])

        with ExitStack() as p1:
            xt = [sb(p1, "xt%d" % i, [128, 1024]) for i in range(2)]
            xn = [sb(p1, "xn%d" % i, [128, 1024]) for i in range(2)]
            st = [sb(p1, "st%d" % i, [128, 2, 6]) for i in range(2)]
            mv = [sb(p1, "mv%d" % i, [128, 2]) for i in range(2)]
            rs = [sb(p1, "rs%d" % i, [128, 1]) for i in range(2)]
            PSAP = bool(os.environ.get("KPSAP"))
            for t in range(18):
                b = t % 2
                n = 1 if t < 2 else 0
                S.dma('sp' if b == 0 else 'pool', lambda e, t=t, b=b: e.dma_start(out=xt[b][:], in_=xs[t * 128:(t + 1) * 128, :]), writes=['xt%d' % b])
                layer_norm_rows(st[b], mv[b][:], rs[b][:], xt[b][:], xn[b][:], 'xt%d' % b, 'xn%d' % b, 'p1%d' % b)
                for half in range(2):
                    bank = 2 * b + half
                    pk = 'pb%d' % bank
                    for c4 in range(4):
                        ch = half * 4 + c4
                        S.op('pe', lambda e, b=b, ch=ch, c4=c4, bank=bank: e.transpose(PB[bank][:, c4 * 128:(c4 + 1) * 128], xn[b][:, ch * 128:(ch + 1) * 128], ident[:]),
                             reads=['xn%d' % b, 'ident'], writes=[pk], skip_self=True)
                    if not PSAP:
                        S.op('act', lambda e, b=b, half=half, bank=bank: e.copy(out=xt[b][:, half * 512:(half + 1) * 512], in_=PB[bank][:, :]), reads=[pk, 'xt%d' % b], writes=['xt%d' % b])
                    for c4 in range(4):
                        ch = half * 4 + c4
                        dst = hT[:, ch, t * 128:(t + 1) * 128]
                        src = PB[bank][:, c4 * 128:(c4 + 1) * 128] if PSAP else xt[b][:, ch * 128:(ch + 1) * 128]
                        S.op('dve', lambda e, dst=dst, src=src, ch=ch, n=n: e.tensor_scalar(out=dst, in0=src, scalar1=modv[:, 8 + ch, n:n + 1], scalar2=modv[:, ch, n:n + 1],
                                                                                      op0=ALU.mult, op1=ALU.add),
                             reads=([pk] if PSAP else ['xt%d' % b]) + ['modv'], writes=K('hT', t))
            S.barrier()

        if debug == 'p1':
            with ExitStack() as dd:
                hf = sb(dd, "hf", [128, 1024])
                for t in range(16):
                    S.op('dve', lambda e, t=t: e.tensor_copy(out=hf[:].rearrange("p (a b) -> p a b", b=128), in_=hT[:, :, 256 + t * 128:256 + (t + 1) * 128]), reads=K('hT', t + 2) + ['hf'], writes=['hf'])
                    S.dma('sp', lambda e, t=t: e.dma_start(out=dbg_d[t * 128:(t + 1) * 128, :], in_=hf[:]), reads=['hf'], writes=['dbg'])
                S.wait_all('sp', ['dbg']); S.barrier()
            scB.close(); scA.close()
            return nc
        def load_w(stk, name, col0, ncols, src=None):
            src = win_d if src is None else src
            wb = sb(stk, name, [128, 8, ncols], BF16)
            S.dma('sp', lambda e: e.dma_start(out=wstg[:, :, 0:ncols], in_=src[:, :, col0:col0 + ncols]), writes=['wstg'])
            S.op('pool', lambda e: e.tensor_copy(out=wb[:], in_=wstg[:, :, 0:ncols]), reads=['wstg'], writes=[name])
            return wb

        BLKS = [(0, 512), (512, 512), (1024, 512), (1536, 512), (2048, 256)]

        def proj_fm_block(wb, wname, ncols, bank, t0, nt):
            for kc in range(8):
                S.op('pe', lambda e, kc=kc: e.matmul(PB[bank][0:ncols, 0:nt], lhsT=wb[:, kc, :], rhs=hT[:, kc, t0:t0 + nt], start=(kc == 0), stop=(kc == 7)),
                     reads=[wname] + K('hT', t0 // 128, (t0 + nt) // 128), writes=['pb%d' % bank], skip_self=True)

        def proj_tm_group(wb, wname, ncols, bank, c0, ncks):
            for j in range(ncks):
                n = c0 + j
                for kc in range(8):
                    S.op('pe', lambda e, kc=kc, j=j, n=n: e.matmul(PB[bank][0:64, j * ncols:(j + 1) * ncols], lhsT=hT[:, kc, n * 64:(n + 1) * 64], rhs=wb[:, kc, :],
                                                                   start=(kc == 0), stop=(kc == 7)),
                         reads=[wname] + K('hT', n // 2), writes=['pb%d' % bank], skip_self=True)

        S32 = sb(scB, "S32", [128, 2, 132]); Sbf = sb(scB, "Sbf", [128, 2, 132], BF16)
        stm = sb(scB, "stm", [64, 2, 64], BF16)
        usb = sb(scB, "usb", [128, 2, 132])

        def chunk_loop(dirn, KsT, QsT, Vi, QoT, Ku, Vu, dec_ap, W, evac, rk, tagk):
            order = list(range(36)) if dirn == 0 else [3, 2, 1, 0] + list(range(35, 3, -1))
            S.op('dve', lambda e: e.memset(S32[:, 0, :], 0.0), writes=['S32_0'])
            S.op('dve', lambda e: e.memset(Sbf[:, 0, :], 0.0), writes=['Sbf_0'])
            cur = 0
            for idx, n in enumerate(order):
                sl = slice(n * 64, (n + 1) * 64)
                s2 = idx % 2
                if n >= 4:
                    pst = PB[2 + s2][0:64, 0:64]; pstk = 'pb%d' % (2 + s2)
                    po = PB[4 + s2][0:64, 0:W]; pok = 'pb%d' % (4 + s2)
                    S.op('pe', lambda e, pst=pst, sl=sl: e.matmul(pst, lhsT=KsT[:, sl], rhs=QsT[:, sl], start=True, stop=True),
                         reads=rk, writes=[pstk], skip_self=True)
                    S.op('dve', lambda e, pst=pst, s2=s2: e.tensor_tensor(out=stm[:, s2, :], in0=pst, in1=masks[:, dirn, :], op=ALU.mult),
                         reads=[pstk, 'masks'], writes=['stm%d' % s2])
                    S.op('pe', lambda e, po=po, s2=s2, n=n: e.matmul(po, lhsT=stm[:, s2, :], rhs=Vi[:, n, 0:W], start=True, stop=False),
                         reads=['stm%d' % s2] + rk, writes=[pok], skip_self=True)
                    S.op('pe', lambda e, po=po, sl=sl, cur=cur: e.matmul(po, lhsT=QoT[:, sl], rhs=Sbf[:, cur, 0:W], start=False, stop=True),
                         reads=['Sbf_%d' % cur] + rk, writes=[pok], skip_self=True)
                    evac(n - 4, n, po, pok)
                if idx < 35:
                    pu = PB[6 + s2][:, 0:W]; puk = 'pb%d' % (6 + s2)
                    S.op('pe', lambda e, pu=pu, n=n: e.matmul(pu, lhsT=Ku[:, n, :], rhs=Vu[:, n, 0:W], start=True, stop=True),
                         reads=rk, writes=[puk], skip_self=True)
                    nxt = 1 - cur
                    S.op('act', lambda e, pu=pu, s2=s2: e.copy(out=usb[:, s2, 0:W], in_=pu), reads=[puk], writes=['usb%d' % s2])
                    S.op('dve', lambda e, n=n, cur=cur, nxt=nxt, s2=s2: e.scalar_tensor_tensor(out=Sbf[:, nxt, 0:W], in0=S32[:, cur, 0:W], scalar=dec_ap(n), in1=usb[:, s2, 0:W],
                                                                                            op0=ALU.mult, op1=ALU.add),
                         reads=['S32_%d' % cur, 'usb%d' % s2, tagk], writes=['Sbf_%d' % nxt])
                    S.op('dve', lambda e, n=n, cur=cur, nxt=nxt, s2=s2: e.scalar_tensor_tensor(out=S32[:, nxt, 0:W], in0=S32[:, cur, 0:W], scalar=dec_ap(n), in1=usb[:, s2, 0:W],
                                                                                            op0=ALU.mult, op1=ALU.add),
                         reads=['S32_%d' % cur, 'usb%d' % s2, tagk], writes=['S32_%d' % nxt])
                    cur = nxt

        def transpose_chunks_to_mixT(src, skey, head):
            for g in range(4):
                bank = g % 2
                for j in range(8):
                    n = g * 8 + j
                    S.op('pe', lambda e, n=n, j=j, bank=bank: e.transpose(PB[bank][:, j * 64:(j + 1) * 64], src[:, n, :], ident[0:64, 0:64]),
                         reads=[skey, 'ident'], writes=['pb%d' % bank], skip_self=True)
                S.op('act', lambda e, g=g, bank=bank: e.copy(out=mixT[:, head, g * 512:(g + 1) * 512], in_=PB[bank][:, :]),
                     reads=['pb%d' % bank], writes=K('mixT', head))

        with ExitStack() as g0:
            lgT = sb(g0, "lgT", [128, 2, 2, 4]); lbT = sb(g0, "lbT", [128, 2, 4]); omlT = sb(g0, "omlT", [128, 2, 4]); nomlT = sb(g0, "nomlT", [128, 2, 4])
            hgn = sb(g0, "hgn", [64, 512])
            S.dma('sp', lambda e: e.dma_start(out=lgT[:], in_=lg_d[:, :, :, :]), writes=['lgT'])
            S.dma('sp', lambda e: e.dma_start(out=hgn[:], in_=hgn_d[:, :]), writes=['hgn'])
            S.op('dve', lambda e: e.tensor_tensor(out=lbT[:], in0=lgT[:, :, 0, :], in1=lgT[:, :, 1, :], op=ALU.subtract), reads=['lgT'], writes=['lbT'])
            S.op('act', lambda e: e.activation(out=lbT[:], in_=lbT[:], func=AF.Sigmoid), reads=['lbT'], writes=['lbT'])
            S.op('dve', lambda e: e.tensor_scalar(out=omlT[:], in0=lbT[:], scalar1=-1.0, scalar2=1.0, op0=ALU.mult, op1=ALU.add), reads=['lbT'], writes=['omlT'])
            S.op('dve', lambda e: e.tensor_scalar_mul(out=nomlT[:], in0=omlT[:], scalar1=-1.0), reads=['omlT'], writes=['nomlT'])
            QsT = [sb(g0, "gQsT%d" % d, [128, NTOK], BF16) for d in range(2)]
            KsT = [sb(g0, "gKsT%d" % d, [128, NTOK], BF16) for d in range(2)]
            QoT = [sb(g0, "gQoT%d" % d, [128, NTOK], BF16) for d in range(2)]
            Ku = [sb(g0, "gKu%d" % d, [64, NCH, 128], BF16) for d in range(2)]
            dec = sb(g0, "gdec", [128, 2, NCH])
            vtm = sb(g0, "gv", [64, NCH, 128], BF16); gs = sb(g0, "ggs", [64, NLCH, 128], BF16); oacc = sb(g0, "goacc", [64, NLCH, 128])
            qf = sb(g0, "gqf", [128, 512]); sg = sb(g0, "gsg", [128, 512]); lf = sb(g0, "glf", [128, 512]); key = sb(g0, "gkey", [128, 512])
            Bc = sb(g0, "gB", [128, 512]); T1 = sb(g0, "gT1", [128, 512]); E1 = sb(g0, "gE1", [128, 512]); khT = sb(g0, "gkhT", [128, 512], BF16)
            sq = sb(g0, "gsq", [64, NLCH, 128]); ss = sb(g0, "gss", [64, NLCH])
            for hd in range(4):
                with ExitStack() as hs:
                    wq_ = load_w(hs, "gwq", 0 + hd * 128, 128); wi_ = load_w(hs, "gwi", 512 + hd * 128, 128); wg_ = load_w(hs, "gwg", 1024 + hd * 128, 128)
                    wf = [load_w(hs, "gwf0", 1536 + hd * 128, 128), load_w(hs, "gwf1", 2048 + hd * 128, 128)]
                    for g in range(9):
                        bk = 3 + g % 2
                        proj_tm_group(wi_, "gwi", 128, bk, g * 4, 4)
                        S.op('act', lambda e, g=g, bk=bk: e.copy(out=vtm[:, g * 4:(g + 1) * 4, :], in_=PB[bk][0:64, :].rearrange("p (j c) -> p j c", c=128)),
                             reads=['pb%d' % bk], writes=['gv'])
                    for g in range(8):
                        bk = 3 + (g + 1) % 2
                        proj_tm_group(wg_, "gwg", 128, bk, 4 + g * 4, 4)
                        S.op('act', lambda e, g=g, bk=bk: e.activation(out=gs[:, g * 4:(g + 1) * 4, :], in_=PB[bk][0:64, :].rearrange("p (j c) -> p j c", c=128), func=AF.Silu),
                             reads=['pb%d' % bk], writes=['ggs'])
                    for (t0, nt) in BLKS:
                        nck = nt // 64; c0 = t0 // 64
                        proj_fm_block(wq_, "gwq", 128, 0, t0, nt)
                        S.op('act', lambda e, nt=nt: e.copy(out=qf[:, 0:nt], in_=PB[0][:, 0:nt]), reads=['pb0'], writes=['gqf'])
                        for d in range(2):
                            proj_fm_block(wf[d], "gwf%d" % d, 128, 1 + d, t0, nt)
                            col = d * 4 + hd
                            lbp = lbT[:, d, hd:hd + 1]; omp = omlT[:, d, hd:hd + 1]; nomp = nomlT[:, d, hd:hd + 1]
                            S.op('act', lambda e, d=d, nt=nt: e.activation(out=sg[:, 0:nt], in_=PB[1 + d][:, 0:nt], func=AF.Sigmoid), reads=['pb%d' % (1 + d)], writes=['gsg'])
                            S.op('act', lambda e, nt=nt, lbp=lbp, omp=omp: e.activation(out=lf[:, 0:nt], in_=sg[:, 0:nt], func=AF.Ln, bias=lbp, scale=omp),
                                 reads=['gsg', 'lbT', 'omlT'], writes=['glf'])
                            S.op('dve', lambda e, nt=nt, nomp=nomp, omp=omp: e.tensor_scalar(out=key[:, 0:nt], in0=sg[:, 0:nt], scalar1=nomp, scalar2=omp, op0=ALU.mult, op1=ALU.add),
                                 reads=['gsg', 'omlT', 'nomlT'], writes=['gkey'])
                            S.op('dve', lambda e, nt=nt: e.tensor_tensor_scan(out=Bc[:, 0:nt], data0=rmask[:, 0:nt], data1=lf[:, 0:nt], initial=0.0, op0=ALU.mult, op1=ALU.add),
                                 reads=['glf', 'rmask'], writes=['gB'])
                            B3 = Bc[:, 0:nt].rearrange("p (n c) -> p n c", c=64)
                            T3 = T1[:, 0:nt].rearrange("p (n c) -> p n c", c=64)
                            if d == 1:
                                S.op('dve', lambda e, B3=B3, T3=T3, nck=nck: e.tensor_tensor(out=T3, in0=B3[:, :, 63:64].to_broadcast([128, nck, 64]), in1=B3, op=ALU.subtract),
                                     reads=['gB'], writes=['gT1'])
                                S.op('dve', lambda e, nt=nt: e.tensor_tensor(out=Bc[:, 0:nt], in0=T1[:, 0:nt], in1=lf[:, 0:nt], op=ALU.add), reads=['gT1', 'glf'], writes=['gB'])
                            li = 63 if d == 0 else 0
                            S.op('act', lambda e, B3=B3, li=li, d=d, c0=c0, nck=nck: e.activation(out=dec[:, d, c0:c0 + nck], in_=B3[:, :, li], func=AF.Exp), reads=['gB'], writes=['gdec'])
                            S.op('dve', lambda e, B3=B3, T3=T3, nck=nck: e.tensor_tensor(out=T3, in0=B3, in1=B3[:, :, 32:33].to_broadcast([128, nck, 64]), op=ALU.subtract),
                                 reads=['gB'], writes=['gT1'])
                            S.op('act', lambda e, nt=nt: e.activation(out=E1[:, 0:nt], in_=T1[:, 0:nt], func=AF.Exp), reads=['gT1'], writes=['gE1'])
                            S.op('dve', lambda e, nt=nt, t0=t0, d=d: e.tensor_tensor(out=QsT[d][:, t0:t0 + nt], in0=qf[:, 0:nt], in1=E1[:, 0:nt], op=ALU.mult),
                                 reads=['gqf', 'gE1'], writes=['gQsT%d' % d])
                            S.op('act', lambda e, nt=nt: e.activation(out=E1[:, 0:nt], in_=T1[:, 0:nt], func=AF.Exp, scale=-1.0), reads=['gT1'], writes=['gE1'])
                            S.op('dve', lambda e, nt=nt, t0=t0, d=d: e.tensor_tensor(out=KsT[d][:, t0:t0 + nt], in0=key[:, 0:nt], in1=E1[:, 0:nt], op=ALU.mult),
                                 reads=['gkey', 'gE1'], writes=['gKsT%d' % d])
                            S.op('act', lambda e, nt=nt: e.activation(out=E1[:, 0:nt], in_=Bc[:, 0:nt], func=AF.Exp), reads=['gB'], writes=['gE1'])
                            S.op('dve', lambda e, nt=nt, t0=t0, d=d: e.tensor_tensor(out=QoT[d][:, t0:t0 + nt], in0=qf[:, 0:nt], in1=E1[:, 0:nt], op=ALU.mult),
                                 reads=['gqf', 'gE1'], writes=['gQoT%d' % d])
                            S.op('dve', lambda e, B3=B3, T3=T3, nck=nck, li=li: e.tensor_tensor(out=T3, in0=B3[:, :, li:li + 1].to_broadcast([128, nck, 64]), in1=B3, op=ALU.subtract),
                                 reads=['gB'], writes=['gT1'])
                            S.op('act', lambda e, nt=nt: e.activation(out=E1[:, 0:nt], in_=T1[:, 0:nt], func=AF.Exp), reads=['gT1'], writes=['gE1'])
                            S.op('dve', lambda e, nt=nt: e.tensor_tensor(out=khT[:, 0:nt], in0=key[:, 0:nt], in1=E1[:, 0:nt], op=ALU.mult), reads=['gkey', 'gE1'], writes=['gkhT'])
                            pbb = PB[3][:].bitcast(BF16)
                            for j in range(nck):
                                S.op('pe', lambda e, j=j: e.transpose(pbb[0:64, j * 128:(j + 1) * 128], khT[:, j * 64:(j + 1) * 64], identb[:]),
                                     reads=['gkhT', 'identb'], writes=['pb3'], skip_self=True)
                            S.op('act', lambda e, d=d, c0=c0, nck=nck: e.copy(out=Ku[d][:, c0:c0 + nck, :], in_=pbb[0:64, 0:nck * 128].rearrange("p (j c) -> p j c", c=128)),
                                 reads=['pb3'], writes=['gKu%d' % d])
                    for d in range(2):
                        def evac(nl, n, po, pok, d=d):
                            if d == 0:
                                S.op('act', lambda e: e.copy(out=oacc[:, nl, :], in_=po), reads=[pok], writes=K('goacc', nl))
                            else:
                                S.op('dve', lambda e: e.tensor_tensor(out=oacc[:, nl, :], in0=po, in1=oacc[:, nl, :], op=ALU.add), reads=[pok] + K('goacc', nl), writes=K('goacc', nl))
                        chunk_loop(d, KsT[d], QsT[d], vtm, QoT[d], Ku[d], vtm, lambda n, d=d: dec[:, d, n:n + 1], 128, evac,
                                   ['gQsT%d' % d, 'gKsT%d' % d, 'gQoT%d' % d, 'gKu%d' % d, 'gv'], 'gdec')
                    allo = K('goacc', 0, NLCH)
                    S.op('dve', lambda e: e.tensor_tensor(out=sq[:], in0=oacc[:], in1=oacc[:], op=ALU.mult), reads=allo, writes=['gsq'])
                    S.op('dve', lambda e: e.tensor_reduce(out=ss[:], in_=sq[:], axis=AX.X, op=ALU.add), reads=['gsq'], writes=['gss'])
                    S.op('act', lambda e: e.activation(out=ss[:], in_=ss[:], func=AF.Sqrt, bias=epsc[0:64, 0:1], scale=1.0 / 128.0), reads=['gss', 'epsc'], writes=['gss'])
                    S.op('dve', lambda e: e.reciprocal(out=ss[:], in_=ss[:]), reads=['gss'], writes=['gss'])
                    S.op('dve', lambda e: e.tensor_tensor(out=sq[:], in0=oacc[:], in1=ss[:].unsqueeze(2).to_broadcast([64, NLCH, 128]), op=ALU.mult),
                         reads=allo + ['gss'], writes=['gsq'])
                    S.op('dve', lambda e, hd=hd: e.tensor_tensor(out=sq[:], in0=sq[:], in1=hgn[:, hd * 128:(hd + 1) * 128].unsqueeze(1).to_broadcast([64, NLCH, 128]), op=ALU.mult),
                         reads=['gsq', 'hgn'], writes=['gsq'])
                    S.op('dve', lambda e: e.tensor_tensor(out=sq[:], in0=sq[:], in1=gs[:], op=ALU.mult), reads=['gsq', 'ggs'], writes=['gsq'])
                    transpose_chunks_to_mixT(sq, 'gsq', hd)
                    S.barrier()
            S.barrier()

        with ExitStack() as m0:
            mln = sb(m0, "mln", [64, 512]); convw = sb(m0, "convw", [128, 9, 8]); convb = sb(m0, "convb", [128, 8])
            gateb = sb(m0, "gateb", [8, 2]); sel8 = sb(m0, "sel8", [8, 8]); dirm = sb(m0, "dirm", [8, 2])
            S.dma('sp', lambda e: e.dma_start(out=mln[:], in_=mln_d[:, :]), writes=['mln'])
            S.dma('sp', lambda e: e.dma_start(out=convw[:], in_=convw_d[:, :, :]), writes=['convw'])
            S.dma('sp', lambda e: e.dma_start(out=convb[:], in_=convb_d[:, :]), writes=['convb'])
            S.dma('sp', lambda e: e.dma_start(out=gateb[:], in_=gateb_d[:, :]), writes=['gateb'])
            S.dma('sp', lambda e: e.dma_start(out=sel8[:], in_=sel8_d[:, :]), writes=['sel8'])
            S.dma('sp', lambda e: e.dma_start(out=dirm[:], in_=dirm_d[:, :]), writes=['dirm'])
            RUU = sb(m0, "RUU", [64, NCH, 24]); dchunk = sb(m0, "dchunk", [128, 8, NCH])
            with ExitStack() as gp:
                wgi = load_w(gp, "mwgi", 4608, 8); wgf = load_w(gp, "mwgf", 4616, 8)
                LI = sb(gp, "LI", [8, NTOK]); LF = sb(gp, "LF", [8, NTOK]); Af = sb(gp, "Af", [8, NTOK]); Ab = sb(gp, "Ab", [8, NTOK]); Aa = sb(gp, "Aa", [8, NTOK])
                R = [sb(gp, "Rr%d" % i, [8, NTOK]) for i in range(3)]
                bd = sb(gp, "bd", [8, 8, NCH])
                for (t0, nt) in BLKS:
                    proj_fm_block(wgi, "mwgi", 8, 0, t0, nt)
                    S.op('act', lambda e, t0=t0, nt=nt: e.copy(out=LI[:, t0:t0 + nt], in_=PB[0][0:8, 0:nt]), reads=['pb0'], writes=['LI'])
                    proj_fm_block(wgf, "mwgf", 8, 1, t0, nt)
                    S.op('act', lambda e, t0=t0, nt=nt: e.copy(out=LF[:, t0:t0 + nt], in_=PB[1][0:8, 0:nt]), reads=['pb1'], writes=['LF'])
                S.op('dve', lambda e: e.tensor_scalar_add(out=LI[:], in0=LI[:], scalar1=gateb[:, 0:1]), reads=['LI', 'gateb'], writes=['LI'])
                S.op('act', lambda e: e.activation(out=LF[:], in_=LF[:], func=AF.Sigmoid, bias=gateb[:, 1:2], scale=1.0), reads=['LF', 'gateb'], writes=['LF'])
                S.op('act', lambda e: e.activation(out=LF[:], in_=LF[:], func=AF.Ln), reads=['LF'], writes=['LF'])
                for (t0, nt) in BLKS:
                    S.op('dve', lambda e, t0=t0, nt=nt: e.tensor_tensor_scan(out=Af[:, t0:t0 + nt], data0=rmask[0:8, 0:nt], data1=LF[:, t0:t0 + nt], initial=0.0, op0=ALU.mult, op1=ALU.add),
                         reads=['LF', 'rmask'], writes=['Af'])
                A3 = Af[:].rearrange("p (n c) -> p n c", c=64)
                tot = A3[:, :, 63:64]
                S.op('dve', lambda e: e.tensor_tensor(out=Ab[:].rearrange("p (n c) -> p n c", c=64), in0=tot.to_broadcast([8, NCH, 64]), in1=A3, op=ALU.subtract), reads=['Af'], writes=['Ab'])
                S.op('dve', lambda e: e.tensor_tensor(out=Ab[:], in0=Ab[:], in1=LF[:], op=ALU.add), reads=['Ab', 'LF'], writes=['Ab'])
                S.op('dve', lambda e: e.tensor_scalar_mul(out=Aa[:], in0=Af[:], scalar1=dirm[:, 0:1]), reads=['Af', 'dirm'], writes=['Aa'])
                S.op('dve', lambda e: e.scalar_tensor_tensor(out=Aa[:], in0=Ab[:], scalar=dirm[:, 1:2], in1=Aa[:], op0=ALU.mult, op1=ALU.add), reads=['Ab', 'dirm', 'Aa'], writes=['Aa'])
                S.op('act', lambda e: e.activation(out=R[0][:], in_=Aa[:], func=AF.Exp), reads=['Aa'], writes=['Rr0'])
                S.op('dve', lambda e: e.tensor_tensor(out=Ab[:], in0=LI[:], in1=Aa[:], op=ALU.subtract), reads=['LI', 'Aa', 'Ab'], writes=['Ab'])
                S.op('act', lambda e: e.activation(out=R[1][:], in_=Ab[:], func=AF.Exp), reads=['Ab'], writes=['Rr1'])
                S.op('dve', lambda e: e.tensor_tensor(out=Aa[:].rearrange("p (n c) -> p n c", c=64), in0=Ab[:].rearrange("p (n c) -> p n c", c=64), in1=tot.to_broadcast([8, NCH, 64]), op=ALU.add),
                     reads=['Ab', 'Af', 'Aa', 'Rr0'], writes=['Aa'])
                S.op('act', lambda e: e.activation(out=R[2][:], in_=Aa[:], func=AF.Exp), reads=['Aa'], writes=['Rr2'])
                for half in range(2):
                    for j in range(18):
                        n = half * 18 + j
                        for q in range(3):
                            S.op('pe', lambda e, n=n, j=j, q=q: e.transpose(PB[2][0:64, j * 24 + q * 8:j * 24 + q * 8 + 8], R[q][:, n * 64:(n + 1) * 64], ident[0:8, 0:8]),
                                 reads=['Rr%d' % q, 'ident'], writes=['pb2'], skip_self=True)
                    S.op('act', lambda e, half=half: e.copy(out=RUU[:, half * 18:(half + 1) * 18, :], in_=PB[2][0:64, 0:432].rearrange("p (j c) -> p j c", c=24)),
                         reads=['pb2'], writes=['RUU'])
                S.op('dve', lambda e: e.tensor_tensor(out=bd[:], in0=tot.rearrange("p n c -> p c n").to_broadcast([8, 8, NCH]), in1=sel8[:].unsqueeze(2).to_broadcast([8, 8, NCH]), op=ALU.mult),
                     reads=['Af', 'sel8'], writes=['bd'])
                S.op('pe', lambda e: e.matmul(PB[3][:, 0:288], lhsT=ones[0:8, :], rhs=bd[:].rearrange("p a n -> p (a n)"), start=True, stop=True), reads=['ones', 'bd'], writes=['pb3'], skip_self=True)
                S.op('act', lambda e: e.activation(out=dchunk[:].rearrange("p a n -> p (a n)"), in_=PB[3][:, 0:288], func=AF.Exp), reads=['pb3'], writes=['dchunk'])
                S.barrier()
            qc = sb(m0, "mqc", [128, NTOK], BF16); kc_ = sb(m0, "mkc", [128, NTOK], BF16); kTM = sb(m0, "mkTM", [64, NCH, 128], BF16)
            raw = sb(m0, "mraw", [128, NTOK]); acc = sb(m0, "macc", [128, NTOK])
            vext = sb(m0, "mvext", [64, NCH, 132], BF16); vi = sb(m0, "mvi", [64, NCH, 132], BF16); vu = sb(m0, "mvu", [64, NCH, 132], BF16)
            og = sb(m0, "mog", [64, NLCH, 128], BF16); oext = sb(m0, "moext", [64, NLCH, 132]); obuf = sb(m0, "mobuf", [64, NLCH, 128])
            den = sb(m0, "mden", [64, NLCH]); mu = sb(m0, "mmu", [64, NLCH]); m2 = sb(m0, "mm2", [64, NLCH])
            S.op('dve', lambda e: e.memset(vext[:], 1.0), writes=['mvext'])
            for hd in range(4):
                with ExitStack() as hs:
                    wq_ = load_w(hs, "mwq", 2560 + hd * 128, 128); wk_ = load_w(hs, "mwk", 3072 + hd * 128, 128)
                    wv_ = load_w(hs, "mwv", 3584 + hd * 128, 128); wo_ = load_w(hs, "mwo", 4096 + hd * 128, 128)
                    for g in range(9):
                        bk = 3 + g % 2
                        proj_tm_group(wv_, "mwv", 128, bk, g * 4, 4)
                        S.op('act', lambda e, g=g, bk=bk: e.copy(out=vext[:, g * 4:(g + 1) * 4, 0:128], in_=PB[bk][0:64, :].rearrange("p (j c) -> p j c", c=128)),
                             reads=['pb%d' % bk], writes=['mvext'])
                    for g in range(8):
                        bk = 3 + (g + 1) % 2
                        proj_tm_group(wo_, "mwo", 128, bk, 4 + g * 4, 4)
                        S.op('act', lambda e, g=g, bk=bk: e.activation(out=og[:, g * 4:(g + 1) * 4, :], in_=PB[bk][0:64, :].rearrange("p (j c) -> p j c", c=128), func=AF.Sigmoid),
                             reads=['pb%d' % bk], writes=['mog'])
                    for qi, (wb, wn, dstb) in enumerate(((wq_, "mwq", qc), (wk_, "mwk", kc_))):
                        chn = qi * 4 + hd
                        for bi, (t0, nt) in enumerate(BLKS):
                            proj_fm_block(wb, wn, 128, bi % 2, t0, nt)
                            S.op('act', lambda e, t0=t0, nt=nt, bi=bi: e.copy(out=raw[:, t0:t0 + nt], in_=PB[bi % 2][:, 0:nt]), reads=['pb%d' % (bi % 2)], writes=['mraw'])
                        S.op('dve', lambda e, chn=chn: e.tensor_scalar(out=acc[:, 0:256], in0=raw[:, 0:256], scalar1=convw[:, 4, chn:chn + 1], scalar2=convb[:, chn:chn + 1], op0=ALU.mult, op1=ALU.add),
                             reads=['mraw', 'convw', 'convb'], writes=['macc'])
                        S.op('dve', lambda e, chn=chn: e.scalar_tensor_tensor(out=acc[:, 1:256], in0=raw[:, 0:255], scalar=convw[:, 3, chn:chn + 1], in1=acc[:, 1:256], op0=ALU.mult, op1=ALU.add),
                             reads=['mraw', 'convw', 'macc'], writes=['macc'])
                        S.op('dve', lambda e, chn=chn: e.scalar_tensor_tensor(out=acc[:, 0:255], in0=raw[:, 1:256], scalar=convw[:, 5, chn:chn + 1], in1=acc[:, 0:255], op0=ALU.mult, op1=ALU.add),
                             reads=['mraw', 'convw', 'macc'], writes=['macc'])
                        X = raw[:, 256:NTOK].rearrange("p (r c) -> p r c", c=64); Y = acc[:, 256:NTOK].rearrange("p (r c) -> p r c", c=64)
                        S.op('dve', lambda e, chn=chn: e.tensor_scalar(out=acc[:, 256:NTOK], in0=raw[:, 256:NTOK], scalar1=convw[:, 4, chn:chn + 1], scalar2=convb[:, chn:chn + 1], op0=ALU.mult, op1=ALU.add),
                             reads=['mraw', 'convw', 'convb', 'macc'], writes=['macc'])
                        for ky in range(3):
                            for kx in range(3):
                                if ky == 1 and kx == 1:
                                    continue
                                dy = ky - 1; dx = kx - 1
                                r0 = max(0, -dy); r1 = 32 - max(0, dy); c0 = max(0, -dx); c1 = 64 - max(0, dx)
                                S.op('dve', lambda e, chn=chn, ky=ky, kx=kx, r0=r0, r1=r1, c0=c0, c1=c1, dy=dy, dx=dx: e.scalar_tensor_tensor(
                                    out=Y[:, r0:r1, c0:c1], in0=X[:, r0 + dy:r1 + dy, c0 + dx:c1 + dx], scalar=convw[:, ky * 3 + kx, chn:chn + 1], in1=Y[:, r0:r1, c0:c1],
                                    op0=ALU.mult, op1=ALU.add), reads=['mraw', 'convw', 'macc'], writes=['macc'])
                        S.op('act', lambda e: e.activation(out=acc[:], in_=acc[:], func=AF.Silu), reads=['macc'], writes=['macc'])
                        if qi == 0:
                            S.op('dve', lambda e: e.tensor_copy(out=qc[:], in_=acc[:]), reads=['macc'], writes=['mqc'])
                        else:
                            S.op('dve', lambda e: e.tensor_scalar_mul(out=kc_[:], in0=acc[:], scalar1=128.0 ** -0.5), reads=['macc'], writes=['mkc'])
                    pbb = PB[3][:].bitcast(BF16)
                    for g in range(5):
                        nck = 8 if g < 4 else 4
                        for j in range(nck):
                            n = g * 8 + j
                            S.op('pe', lambda e, j=j, n=n: e.transpose(pbb[0:64, j * 128:(j + 1) * 128], kc_[:, n * 64:(n + 1) * 64], identb[:]),
                                 reads=['mkc', 'identb'], writes=['pb3'], skip_self=True)
                        S.op('act', lambda e, g=g, nck=nck: e.copy(out=kTM[:, g * 8:g * 8 + nck, :], in_=pbb[0:64, 0:nck * 128].rearrange("p (j c) -> p j c", c=128)),
                             reads=['pb3'], writes=['mkTM'])
                    for d in range(2):
                        row = d * 4 + hd
                        S.op('dve', lambda e, row=row: e.tensor_tensor(out=vi[:], in0=vext[:], in1=RUU[:, :, 8 + row:9 + row].to_broadcast([64, NCH, 132]), op=ALU.mult),
                             reads=['mvext', 'RUU'], writes=['mvi'])
                        S.op('dve', lambda e, row=row: e.tensor_tensor(out=vu[:], in0=vext[:], in1=RUU[:, :, 16 + row:17 + row].to_broadcast([64, NCH, 132]), op=ALU.mult),
                             reads=['mvext', 'RUU'], writes=['mvu'])

                        def evac(nl, n, po, pok, row=row):
                            S.op('act', lambda e: e.copy(out=oext[:, nl, 0:129], in_=po), reads=[pok], writes=K('moext', nl))
                        chunk_loop(d, kc_, qc, vi, qc, kTM, vu, lambda n, row=row: dchunk[:, row, n:n + 1], 129, evac,
                                   ['mqc', 'mkc', 'mvi', 'mvu', 'mkTM'], 'dchunk')
                        allx = K('moext', 0, NLCH)
                        S.op('dve', lambda e, row=row: e.tensor_tensor(out=oext[:, :, 0:129], in0=oext[:, :, 0:129], in1=RUU[:, 4:NCH, row:row + 1].to_broadcast([64, NLCH, 129]), op=ALU.mult),
                             reads=allx + ['RUU'], writes=allx)
                        S.op('act', lambda e: e.activation(out=den[:], in_=oext[:, :, 128], func=AF.Abs), reads=allx, writes=['mden'])
                        S.op('dve', lambda e: e.tensor_scalar_max(out=den[:], in0=den[:], scalar1=1.0), reads=['mden'], writes=['mden'])
                        S.op('dve', lambda e: e.reciprocal(out=den[:], in_=den[:]), reads=['mden'], writes=['mden'])
                        if d == 0:
                            S.op('dve', lambda e: e.tensor_tensor(out=obuf[:], in0=oext[:, :, 0:128], in1=den[:].unsqueeze(2).to_broadcast([64, NLCH, 128]), op=ALU.mult),
                                 reads=allx + ['mden'], writes=['mobuf'])
                        else:
                            S.op('dve', lambda e: e.tensor_tensor(out=oext[:, :, 0:128], in0=oext[:, :, 0:128], in1=den[:].unsqueeze(2).to_broadcast([64, NLCH, 128]), op=ALU.mult),
                                 reads=allx + ['mden'], writes=allx)
                            S.op('dve', lambda e: e.tensor_tensor(out=obuf[:], in0=obuf[:], in1=oext[:, :, 0:128], op=ALU.add), reads=allx + ['mobuf'], writes=['mobuf'])
                    allx = K('moext', 0, NLCH)
                    S.op('dve', lambda e: e.tensor_reduce(out=mu[:], in_=obuf[:], axis=AX.X, op=ALU.add), reads=['mobuf'], writes=['mmu'])
                    S.op('dve', lambda e: e.tensor_scalar_mul(out=mu[:], in0=mu[:], scalar1=1.0 / 128.0), reads=['mmu'], writes=['mmu'])
                    S.op('dve', lambda e: e.tensor_tensor(out=obuf[:], in0=obuf[:], in1=mu[:].unsqueeze(2).to_broadcast([64, NLCH, 128]), op=ALU.subtract), reads=['mobuf', 'mmu'], writes=['mobuf'])
                    S.op('dve', lambda e: e.tensor_tensor(out=oext[:, :, 0:128], in0=obuf[:], in1=obuf[:], op=ALU.mult), reads=['mobuf'] + allx, writes=allx)
                    S.op('dve', lambda e: e.tensor_reduce(out=m2[:], in_=oext[:, :, 0:128], axis=AX.X, op=ALU.add), reads=allx, writes=['mm2'])
                    S.op('act', lambda e: e.activation(out=m2[:], in_=m2[:], func=AF.Sqrt, bias=epsc[0:64, 0:1], scale=1.0 / 128.0), reads=['mm2', 'epsc'], writes=['mm2'])
                    S.op('dve', lambda e: e.reciprocal(out=m2[:], in_=m2[:]), reads=['mm2'], writes=['mm2'])
                    S.op('dve', lambda e: e.tensor_tensor(out=obuf[:], in0=obuf[:], in1=m2[:].unsqueeze(2).to_broadcast([64, NLCH, 128]), op=ALU.mult), reads=['mobuf', 'mm2'], writes=['mobuf'])
                    S.op('dve', lambda e, hd=hd: e.tensor_tensor(out=obuf[:], in0=obuf[:], in1=mln[:, hd * 128:(hd + 1) * 128].unsqueeze(1).to_broadcast([64, NLCH, 128]), op=ALU.mult),
                         reads=['mobuf', 'mln'], writes=['mobuf'])
                    S.op('dve', lambda e: e.tensor_tensor(out=obuf[:], in0=obuf[:], in1=og[:], op=ALU.mult), reads=['mobuf', 'mog'], writes=['mobuf'])
                    transpose_chunks_to_mixT(obuf, 'mobuf', 4 + hd)
                    S.barrier()
            S.barrier()

        if debug == 'mix':
            with ExitStack() as dd:
                mf = sb(dd, "mf", [128, NLAT])
                for h in range(8):
                    S.op('dve', lambda e, h=h: e.tensor_copy(out=mf[:], in_=mixT[:, h, :]), reads=K('mixT', h) + ['mf'], writes=['mf'])
                    S.dma('sp', lambda e, h=h: e.dma_start(out=dbg_d[h * 128:(h + 1) * 128, :], in_=mf[:]), reads=['mf'], writes=['dbg'])
                S.wait_all('sp', ['dbg']); S.barrier()
            scB.close(); scA.close()
            return nc
        scB.close()
        x1s = nc.dram_tensor("x1s", [NLAT, 1024], F32, kind="Internal").ap()

        def bcast_tile(dst, dkey, c0, dg):
            for ch in range(8):
                S.op('dve', lambda e, ch=ch: e.tensor_scalar_mul(out=dg[:], in0=ident[:], scalar1=modv[:, c0 + ch, 0:1]), reads=['ident', 'modv', 'dg'], writes=['dg'])
                bank = ch // 4
                S.op('pe', lambda e, ch=ch, bank=bank: e.matmul(PB[bank][:, (ch % 4) * 128:(ch % 4 + 1) * 128], lhsT=ones[:], rhs=dg[:], start=True, stop=True),
                     reads=['ones', 'dg'], writes=['pb%d' % bank], skip_self=True)
            S.op('act', lambda e: e.copy(out=dst[:, 0:512], in_=PB[0][:, :]), reads=['pb0'], writes=[dkey])
            S.op('act', lambda e: e.copy(out=dst[:, 512:1024], in_=PB[1][:, :]), reads=['pb1'], writes=[dkey])

        with ExitStack() as p3:
            bct = {}
            for nm in ("g1b", "ln1g", "ln1b"):
                bct[nm] = sb(p3, nm, [128, 1024])
            for nm, dd in (("ln1g", ln1g_d), ("ln1b", ln1b_d)):
                S.dma('sp', lambda e, nm=nm, dd=dd: e.dma_start(out=bct[nm][:], in_=dd[:, :]), writes=[nm])
            dg = sb(p3, "dg", [128, 128])
            bcast_tile(bct["g1b"], "g1b", 16, dg)
            wo_b = sb(p3, "wout", [128, 8, 1024], BF16); wos = sb(p3, "wos", [128, 1024])
            for kc in range(8):
                S.dma('sp', lambda e, kc=kc: e.dma_start(out=wos[:], in_=wout_d[:, kc, :]), writes=['wos'])
                S.op('pool', lambda e, kc=kc: e.tensor_copy(out=wo_b[:, kc, :], in_=wos[:]), reads=['wos'], writes=['wout'])
            xt = [sb(p3, "x3t%d" % i, [128, 1024]) for i in range(2)]
            t1 = [sb(p3, "t1_%d" % i, [128, 1024]) for i in range(2)]
            st = [sb(p3, "st3%d" % i, [128, 2, 6]) for i in range(2)]
            mv = [sb(p3, "mv3%d" % i, [128, 2]) for i in range(2)]
            rs = [sb(p3, "rs3%d" % i, [128, 1]) for i in range(2)]
            for t in range(16):
                b = t % 2
                S.dma('sp' if b == 0 else 'pool', lambda e, t=t, b=b: e.dma_start(out=xt[b][:], in_=xs[256 + t * 128:256 + (t + 1) * 128, :]), writes=['x3t%d' % b])
                for half in range(2):
                    bank = 2 * b + half
                    for kc in range(8):
                        S.op('pe', lambda e, kc=kc, t=t, half=half, bank=bank: e.matmul(PB[bank][:, :], lhsT=mixT[:, kc, t * 128:(t + 1) * 128], rhs=wo_b[:, kc, half * 512:(half + 1) * 512],
                                                                                      start=(kc == 0), stop=(kc == 7)),
                             reads=K('mixT', kc) + ['wout'], writes=['pb%d' % bank], skip_self=True)
                    S.op('dve', lambda e, b=b, half=half, bank=bank: e.tensor_tensor(out=t1[b][:, half * 512:(half + 1) * 512], in0=PB[bank][:, :], in1=bct["g1b"][:, half * 512:(half + 1) * 512], op=ALU.mult),
                         reads=['pb%d' % bank, 'g1b'], writes=['t1_%d' % b])
                S.op('dve', lambda e, b=b: e.scalar_tensor_tensor(out=t1[b][:], in0=xt[b][:], scalar=ALPHA, in1=t1[b][:], op0=ALU.mult, op1=ALU.add),
                     reads=['x3t%d' % b, 't1_%d' % b], writes=['t1_%d' % b])
                layer_norm_rows(st[b], mv[b][:], rs[b][:], t1[b][:], t1[b][:], 't1_%d' % b, 't1_%d' % b, 'p3%d' % b)
                S.op('dve', lambda e, b=b: e.tensor_tensor(out=t1[b][:], in0=t1[b][:], in1=bct["ln1g"][:], op=ALU.mult), reads=['t1_%d' % b, 'ln1g'], writes=['t1_%d' % b])
                S.op('dve', lambda e, b=b, t=t: e.tensor_tensor(out=t1[b][:], in0=t1[b][:], in1=bct["ln1b"][:], op=ALU.add), reads=['t1_%d' % b, 'ln1b'], writes=['t1_%d' % b])
                S.dma('sp', lambda e, t=t, b=b: e.dma_start(out=x1s[t * 128:(t + 1) * 128, :], in_=t1[b][:]), reads=['t1_%d' % b], writes=K('x1_', t))
                if debug == 'x1':
                    S.dma('sp', lambda e, t=t, b=b: e.dma_start(out=dbg_d[t * 128:(t + 1) * 128, :], in_=t1[b][:]), reads=['t1_%d' % b], writes=['dbg'])
            S.barrier()
        scA.close()

        with ExitStack() as p4:
            bct = {}
            for nm in ("g2b", "sc2b", "sh2b", "ln2g", "ln2b"):
                bct[nm] = sb(p4, nm, [128, 1024])
            for nm, dd in (("ln2g", ln2g_d), ("ln2b", ln2b_d)):
                S.dma('sp', lambda e, nm=nm, dd=dd: e.dma_start(out=bct[nm][:], in_=dd[:, :]), writes=[nm])
            dg = sb(p4, "dg4", [128, 128])
            bcast_tile(bct["g2b"], "g2b", 40, dg); bcast_tile(bct["sc2b"], "sc2b", 32, dg); bcast_tile(bct["sh2b"], "sh2b", 24, dg)
            x1t = [sb(p4, "x1t%d" % i, [128, 1024]) for i in range(2)]
            wqb = sb(p4, "wqb", [128, 8, 2048], BF16)
            keysT = sb(p4, "keysT", [128, 16, 128], BF16)
            with ExitStack() as tmp:
                stg = sb(tmp, "wq_stg", [128, 2048])
                for kc in range(8):
                    S.dma('sp', lambda e, kc=kc: e.dma_start(out=stg[:], in_=wq_d[:, kc, :]), writes=['wq_stg'])
                    S.op('pool', lambda e, kc=kc: e.tensor_copy(out=wqb[:, kc, :], in_=stg[:]), reads=['wq_stg'], writes=['wqb'])
                kst = sb(tmp, "kst", [128, 16, 128])
                S.dma('sp', lambda e: e.dma_start(out=kst[:], in_=keysT_d[:, :, :]), writes=['kst'])
                S.op('pool', lambda e: e.tensor_copy(out=keysT[:], in_=kst[:]), reads=['kst'], writes=['keysT'])
                S.barrier()
            NG = 3
            ub = [sb(p4, "ub%d" % i, [128, 1024]) for i in range(NG)]
            vb = [sb(p4, "vb%d" % i, [128, 1024]) for i in range(NG)]
            h2 = sb(p4, "h2", [128, 1024]); h2T = sb(p4, "h2T", [128, 8, 128], BF16); qT = sb(p4, "qT", [128, 16, 128], BF16)
            sc = sb(p4, "sc", [128, 16, 128]); scw = sb(p4, "scw", [128, 16, 128])
            top = sb(p4, "top", [128, 16, 16]); topi = sb(p4, "topi", [128, 16, 16], U32); topf = sb(p4, "topf", [128, 16, 16])
            cand = sb(p4, "cand", [128, 8, 256]); candw = sb(p4, "candw", [128, 8, 256]); cidx = sb(p4, "cidx", [128, 8, 256])
            best = sb(p4, "best", [128, 8, 16]); gate = sb(p4, "gate", [128, 128]); idxf = sb(p4, "idxf", [128, 128]); idxi = sb(p4, "idxi", [128, 128], I32)
            dots = sb(p4, "dots", [128, 128]); wgt = sb(p4, "wgt", [128, 128]); junk = sb(p4, "junk", [128, 1024]); junk2 = sb(p4, "junk2", [128, 256])
            yacc = [sb(p4, "yacc%d" % i, [128, 1024]) for i in range(2)]
            zs = sb(p4, "zs", [128, 8]); nmx = sb(p4, "nmx", [128, 8])
            st = sb(p4, "st4", [128, 2, 6]); mv = sb(p4, "mv4", [128, 2]); rs = sb(p4, "rs4", [128, 1])
            fin = sb(p4, "fin", [128, 1024])
            for t in range(16 if debug != 'x1' else 0):
                xb_ = x1t[t % 2]; x1k = ['x1t%d' % (t % 2)]
                S.dma('sp', lambda e, t=t, xb_=xb_: e.dma_start(out=xb_[:], in_=x1s[t * 128:(t + 1) * 128, :]), reads=K('x1_', t), writes=x1k)
                S.op('dve', lambda e: e.memset(idxf[:], 0.0), writes=['idxf'])
                S.op('dve', lambda e: e.memset(dots[:], 0.0), writes=['dots'])
                S.op('dve', lambda e: e.memset(zs[:], 0.0), writes=['zs'])
                layer_norm_rows(st, mv[:], rs[:], xb_[:], h2[:], x1k[0], 'h2', 'p4')
                S.op('dve', lambda e: e.tensor_tensor(out=h2[:], in0=h2[:], in1=bct["sc2b"][:], op=ALU.mult), reads=['h2', 'sc2b'], writes=['h2'])
                S.op('dve', lambda e: e.tensor_tensor(out=h2[:], in0=h2[:], in1=bct["sh2b"][:], op=ALU.add), reads=['h2', 'sh2b'], writes=['h2'])
                for ch in range(8):
                    bank = ch // 4
                    S.op('pe', lambda e, ch=ch, bank=bank: e.transpose(PB[bank][:, (ch % 4) * 128:(ch % 4 + 1) * 128], h2[:, ch * 128:(ch + 1) * 128], ident[:]),
                         reads=['h2', 'ident'], writes=['pb%d' % bank], skip_self=True)
                S.op('act', lambda e: e.copy(out=h2T[:, 0:4, :], in_=PB[0][:, :].rearrange("p (j c) -> p j c", c=128)), reads=['pb0'], writes=['h2T'])
                S.op('act', lambda e: e.copy(out=h2T[:, 4:8, :], in_=PB[1][:, :].rearrange("p (j c) -> p j c", c=128)), reads=['pb1'], writes=['h2T'])
                for c in range(16):
                    bank = 2 + c // 4
                    for kc in range(8):
                        S.op('pe', lambda e, c=c, kc=kc, bank=bank: e.matmul(PB[bank][:, (c % 4) * 128:(c % 4 + 1) * 128], lhsT=wqb[:, kc, c * 128:(c + 1) * 128], rhs=h2T[:, kc, :],
                                                                           start=(kc == 0), stop=(kc == 7)), reads=['wqb', 'h2T'], writes=['pb%d' % bank], skip_self=True)
                for g in range(4):
                    S.op('act' if g % 2 == 0 else 'dve', lambda e, g=g: (e.copy if g % 2 == 0 else e.tensor_copy)(out=qT[:, g * 4:(g + 1) * 4, :], in_=PB[2 + g][:, :].rearrange("p (j c) -> p j c", c=128)),
                         reads=['pb%d' % (2 + g)], writes=['qT'])
                for c in range(16):
                    bank = 2 + c // 4
                    S.op('pe', lambda e, c=c, bank=bank: e.matmul(PB[bank][:, (c % 4) * 128:(c % 4 + 1) * 128], lhsT=qT[:, c, :], rhs=keysT[:, c, :], start=True, stop=True),
                         reads=['qT', 'keysT'], writes=['pb%d' % bank], skip_self=True)
                for g in range(4):
                    S.op('act' if g % 2 == 0 else 'dve', lambda e, g=g: (e.copy if g % 2 == 0 else e.tensor_copy)(out=sc[:, g * 4:(g + 1) * 4, :], in_=PB[2 + g][:, :].rearrange("p (j c) -> p j c", c=128)),
                         reads=['pb%d' % (2 + g)], writes=['sc'])
                for c in range(16):
                    S.op('dve', lambda e, c=c: e.max(out=top[:, c, 0:8], in_=sc[:, c, :]), reads=['sc'], writes=['top'])
                    S.op('dve', lambda e, c=c: e.max_index(out=topi[:, c, 0:8], in_max=top[:, c, 0:8], in_values=sc[:, c, :]), reads=['sc', 'top'], writes=['topi'])
                    S.op('dve', lambda e, c=c: e.match_replace(out=scw[:, c, :], in_to_replace=top[:, c, 0:8], in_values=sc[:, c, :], imm_value=-1e30), reads=['sc', 'top'], writes=['scw'])
                    S.op('dve', lambda e, c=c: e.max(out=top[:, c, 8:16], in_=scw[:, c, :]), reads=['scw'], writes=['top'])
                    S.op('dve', lambda e, c=c: e.max_index(out=topi[:, c, 8:16], in_max=top[:, c, 8:16], in_values=scw[:, c, :]), reads=['scw', 'top'], writes=['topi'])
                S.op('dve', lambda e: e.tensor_copy(out=topf[:], in_=topi[:]), reads=['topi'], writes=['topf'])
                t4 = top[:].rearrange("p (h two) k -> p h two k", two=2); f4 = topf[:].rearrange("p (h two) k -> p h two k", two=2)
                c4 = cand[:].rearrange("p h (a b) -> p h a b", b=16); i4 = cidx[:].rearrange("p h (a b) -> p h a b", b=16)
                for h in range(8):
                    S.op('dve', lambda e, h=h: e.tensor_tensor(out=c4[:, h, :, :], in0=t4[:, h, 0, :].unsqueeze(2).to_broadcast([128, 16, 16]), in1=t4[:, h, 1, :].unsqueeze(1).to_broadcast([128, 16, 16]), op=ALU.add),
                         reads=['top'], writes=['cand'])
                    S.op('dve', lambda e, h=h: e.scalar_tensor_tensor(out=i4[:, h, :, :], in0=f4[:, h, 0, :].unsqueeze(2).to_broadcast([128, 16, 16]), scalar=128.0, in1=f4[:, h, 1, :].unsqueeze(1).to_broadcast([128, 16, 16]),
                                                                    op0=ALU.mult, op1=ALU.add), reads=['topf'], writes=['cidx'])
                for h in range(8):
                    S.op('dve', lambda e, h=h: e.max(out=best[:, h, 0:8], in_=cand[:, h, :]), reads=['cand'], writes=['best'])
                    S.op('dve', lambda e, h=h: e.match_replace(out=candw[:, h, :], in_to_replace=best[:, h, 0:8], in_values=cand[:, h, :], imm_value=-1e30), reads=['cand', 'best'], writes=['candw'])
                    S.op('dve', lambda e, h=h: e.max(out=best[:, h, 8:16], in_=candw[:, h, :]), reads=['candw'], writes=['best'])
                for h in range(8):
                    for k in range(16):
                        S.op('dve', lambda e, h=h, k=k: e.scalar_tensor_tensor(out=junk2[:], in0=cand[:, h, :], scalar=best[:, h, k:k + 1], in1=cidx[:, h, :], op0=ALU.is_equal, op1=ALU.mult,
                                                                             accum_out=idxf[:, h * 16 + k:h * 16 + k + 1]), reads=['cand', 'best', 'cidx', 'junk2'], writes=['junk2', 'idxf'])
                S.op('dve', lambda e: e.tensor_scalar_min(out=idxf[:], in0=idxf[:], scalar1=16383.0), reads=['idxf'], writes=['idxf'])
                S.op('dve', lambda e: e.tensor_copy(out=idxi[:], in_=idxf[:]), reads=['idxf'], writes=['idxi'])
                S.op('dve', lambda e: e.tensor_scalar_mul(out=nmx[:], in0=best[:, :, 0], scalar1=-1.0), reads=['best'], writes=['nmx'])
                g3 = gate[:].rearrange("p (h k) -> p h k", k=16)
                for h in range(8):
                    S.op('act', lambda e, h=h: e.activation(out=g3[:, h, :], in_=best[:, h, :], func=AF.Exp, bias=nmx[:, h:h + 1], scale=1.0, accum_out=zs[:, h:h + 1]),
                         reads=['best', 'nmx'], writes=['gate', 'zs'])
                S.op('dve', lambda e: e.reciprocal(out=zs[:], in_=zs[:]), reads=['zs'], writes=['zs'])
                S.op('dve', lambda e: e.tensor_tensor(out=g3, in0=g3, in1=zs[:].unsqueeze(2).to_broadcast([128, 8, 16]), op=ALU.mult), reads=['gate', 'zs'], writes=['gate'])
                for k in range(128):
                    s = k % NG
                    S.dma('pool', lambda e, k=k, s=s: e.indirect_dma_start(out=ub[s][:], out_offset=None, in_=pu_d[:, :], in_offset=bass.IndirectOffsetOnAxis(ap=idxi[:, k:k + 1], axis=0)), reads=['idxi'], writes=['ub%d' % s])
                    S.op('dve', lambda e, k=k, s=s: e.scalar_tensor_tensor(out=junk[:], in0=ub[s][:], scalar=1.0, in1=h2[:], op0=ALU.mult, op1=ALU.mult, accum_out=dots[:, k:k + 1]),
                         reads=['ub%d' % s, 'h2', 'junk'], writes=['junk', 'dots'])
                S.op('act', lambda e: e.activation(out=wgt[:], in_=dots[:], func=AF.Gelu), reads=['dots'], writes=['wgt'])
                S.op('dve', lambda e: e.tensor_tensor(out=wgt[:], in0=wgt[:], in1=gate[:], op=ALU.mult), reads=['wgt', 'gate'], writes=['wgt'])
                for k in range(128):
                    s = k % NG; a = k % 2
                    S.dma('pool', lambda e, k=k, s=s: e.indirect_dma_start(out=vb[s][:], out_offset=None, in_=pv_d[:, :], in_offset=bass.IndirectOffsetOnAxis(ap=idxi[:, k:k + 1], axis=0)), reads=['idxi'], writes=['vb%d' % s])
                    if k < 2:
                        S.op('dve', lambda e, k=k, s=s, a=a: e.tensor_scalar_mul(out=yacc[a][:], in0=vb[s][:], scalar1=wgt[:, k:k + 1]), reads=['vb%d' % s, 'wgt'], writes=['yacc%d' % a])
                    else:
                        S.op('dve', lambda e, k=k, s=s, a=a: e.scalar_tensor_tensor(out=yacc[a][:], in0=vb[s][:], scalar=wgt[:, k:k + 1], in1=yacc[a][:], op0=ALU.mult, op1=ALU.add),
                             reads=['vb%d' % s, 'wgt', 'yacc%d' % a], writes=['yacc%d' % a])
                S.op('dve', lambda e: e.tensor_tensor(out=yacc[0][:], in0=yacc[0][:], in1=yacc[1][:], op=ALU.add), reads=['yacc0', 'yacc1'], writes=['yacc0'])
                S.op('dve', lambda e: e.tensor_tensor(out=yacc[0][:], in0=yacc[0][:], in1=bct["g2b"][:], op=ALU.mult), reads=['yacc0', 'g2b'], writes=['yacc0'])
                S.op('dve', lambda e, t=t, xb_=xb_: e.scalar_tensor_tensor(out=fin[:], in0=xb_[:], scalar=ALPHA, in1=yacc[0][:], op0=ALU.mult, op1=ALU.add), reads=x1k + ['yacc0', 'fin'], writes=['fin'])
                layer_norm_rows(st, mv[:], rs[:], fin[:], fin[:], 'fin', 'fin', 'p4')
                S.op('dve', lambda e: e.tensor_tensor(out=fin[:], in0=fin[:], in1=bct["ln2g"][:], op=ALU.mult), reads=['fin', 'ln2g'], writes=['fin'])
                S.op('dve', lambda e: e.tensor_tensor(out=fin[:], in0=fin[:], in1=bct["ln2b"][:], op=ALU.add), reads=['fin', 'ln2b'], writes=['fin'])
                S.dma('sp', lambda e, t=t: e.dma_start(out=out_d[t * 128:(t + 1) * 128, :], in_=fin[:]), reads=['fin'], writes=['out'])
            S.wait_all('sp', ['out', 'dbg'])
            S.barrier()
    return nc


def _prep_shared(inp):
    f = np.float32
    sh = {}
    sh["w_mod"] = np.ascontiguousarray(inp["w_mod"][0].reshape(8, 128, 6144).transpose(1, 0, 2))
    sh["b_modT"] = np.ascontiguousarray(inp["b_mod"][0].reshape(48, 128).T)
    sh["w_in"] = np.ascontiguousarray(inp["w_in"][0].reshape(8, 128, 4624).transpose(1, 0, 2))
    sh["lgT"] = np.ascontiguousarray(inp["hg_lb_logits"].reshape(2, 2, 4, 128).transpose(3, 0, 1, 2))
    sh["hgn"] = np.ascontiguousarray(np.broadcast_to(inp["hg_norm_g"][0][None, :], (64, 512)))
    sh["mln"] = np.ascontiguousarray(np.broadcast_to(inp["ml_norm_g"][0][None, :], (64, 512)))
    sh["convw"] = np.ascontiguousarray(inp["ml_conv_w"][0].reshape(9, 8, 128).transpose(2, 0, 1))
    sh["convb"] = np.ascontiguousarray(inp["ml_conv_b"][0].reshape(8, 128).T)
    sh["gateb"] = np.ascontiguousarray(inp["ml_gate_b"][0].reshape(2, 8).T)
    sh["w_out"] = np.ascontiguousarray(inp["w_out"][0].reshape(8, 128, 1024).transpose(1, 0, 2))
    for nm, key in (("ln1g", "ln1_g"), ("ln1b", "ln1_b"), ("ln2g", "ln2_g"), ("ln2b", "ln2_b")):
        sh[nm] = np.ascontiguousarray(np.broadcast_to(inp[key][0][None, :], (128, 1024)))
    sh["wq"] = np.ascontiguousarray(inp["peer_wq"][0].reshape(8, 128, 2048).transpose(1, 0, 2))
    sh["keysT"] = np.ascontiguousarray(inp["peer_keys"][0].reshape(16, 128, 128).transpose(2, 0, 1))
    nexp = 128 if os.environ.get("KDEBUG") else 16384
    sh["pu"] = np.ascontiguousarray(inp["peer_u"][0][:nexp])
    sh["pv"] = np.ascontiguousarray(inp["peer_v"][0][:nexp])
    sh["ident"] = np.eye(128, dtype=f)
    m = np.zeros((64, 2, 64), f)
    s = np.arange(64)[:, None]; c = np.arange(64)[None, :]
    m[:, 0, :] = (s <= c); m[:, 1, :] = (s >= c)
    sh["masks"] = m
    rm = np.ones((128, 512), f); rm[:, ::64] = 0.0
    sh["rmask"] = rm
    sh["sel8"] = np.eye(8, dtype=f)
    dm = np.zeros((8, 2), f); dm[0:4, 0] = 1.0; dm[4:8, 1] = 1.0
    sh["dirm"] = dm
    return {k: np.asarray(v, dtype=f) for k, v in sh.items()}


def kernel(**inputs):
    inp = {k: np.asarray(v) for k, v in inputs.items()}
    debug = os.environ.get("KDEBUG") or None
    nc = build(debug)
    sh = _prep_shared(inp)
    in_maps = []
    for b in range(8):
        m = dict(sh)
        m["xs"] = np.ascontiguousarray(np.concatenate([inp["ctx"][b], inp["x"][b]], axis=0).astype(np.float32))
        m["cT"] = np.ascontiguousarray(np.stack([inp["c"][b], inp["c_ctx"]], axis=-1).reshape(8, 128, 2).transpose(1, 0, 2).astype(np.float32))
        in_maps.append(m)
    res = run_bass_kernel_spmd(nc, in_maps, core_ids=list(range(8)))
    key = "dbg" if debug else "out"
    return np.stack([np.asarray(r[key]) for r in res.results], axis=0).astype(np.float32)
```

```python
import os
import numpy as np
from contextlib import ExitStack
import concourse.bass as bass
import concourse.mybir as mybir
from concourse.bass_utils import run_bass_kernel_spmd

F32 = mybir.dt.float32; BF16 = mybir.dt.bfloat16; I32 = mybir.dt.int32; U32 = mybir.dt.uint32
AF = mybir.ActivationFunctionType; ALU = mybir.AluOpType; AX = mybir.AxisListType

NTOK = 2304; NLAT = 2048; NCH = 36; NLCH = 32
ALPHA = 2.0 ** 0.25
EPS = 1e-6


class Sched:
    NDMA = 32

    def __init__(self, nc, es):
        self.nc = nc
        self.engs = {'pe': nc.tensor, 'act': nc.scalar, 'dve': nc.vector, 'pool': nc.gpsimd, 'sp': nc.sync}
        self.sem = {k: es.enter_context(nc.semaphore("sem_" + k)) for k in self.engs}
        self.cnt = {k: 0 for k in self.engs}
        self.dsem = [es.enter_context(nc.semaphore("dsem%d" % i)) for i in range(self.NDMA)]
        self.dcnt = [0] * self.NDMA
        self.dnext = 0
        self.seen = {k: {} for k in self.engs}
        self.bufs = {}

    def _deps(self, reads, writes):
        deps = []
        for r in reads:
            b = self.bufs.get(r)
            if b and b['w'] is not None:
                deps.append(b['w'])
        for w in writes:
            b = self.bufs.get(w)
            if b:
                if b['w'] is not None:
                    deps.append(b['w'])
                deps.extend(b['r'])
        return deps

    def _wait(self, eng, deps, skip_self=False):
        best = {}
        for (sid, sem, val, owner) in deps:
            if skip_self and owner == eng:
                continue
            if best.get(sid, (None, 0))[1] < val:
                best[sid] = (sem, val)
        for sid, (sem, val) in best.items():
            if self.seen[eng].get(sid, 0) >= val:
                continue
            self.engs[eng].wait_ge(sem, val)
            self.seen[eng][sid] = val

    def _record(self, dep, reads, writes):
        for r in reads:
            b = self.bufs.setdefault(r, {'w': None, 'r': []})
            b['r'] = [d for d in b['r'] if d[0] != dep[0]] + [dep]
        for w in writes:
            self.bufs[w] = {'w': dep, 'r': []}

    @staticmethod
    def _split(keys):
        norm, ps = [], []
        for k in keys:
            if k.startswith('pb') and len(k) > 2 and k[2].isdigit():
                ps.append(k[:3])
            else:
                norm.append(k)
        return norm, ps

    def op(self, eng, fn, reads=(), writes=(), skip_self=False):
        reads, pr = self._split(reads)
        writes, pw = self._split(writes)
        banks = sorted(set(pr + pw))
        deps = self._deps(reads, writes)
        deps += [d for d in self._deps((), banks) if d[3] != eng]
        self._wait(eng, deps, skip_self)
        ins = fn(self.engs[eng])
        self.cnt[eng] += 1
        ins.then_inc(self.sem[eng], 1)
        self._record(('e_' + eng, self.sem[eng], self.cnt[eng], eng), reads, list(writes) + banks)
        return ins

    def dma(self, q, fn, reads=(), writes=()):
        deps = self._deps(reads, writes)
        j = self.dnext
        self.dnext = (self.dnext + 1) % self.NDMA
        if self.dcnt[j] > 0:
            deps = deps + [('d%d' % j, self.dsem[j], self.dcnt[j], 'dma')]
        self._wait(q, deps)
        ins = fn(self.engs[q])
        self.dcnt[j] += 16
        ins.then_inc(self.dsem[j], 16)
        self._record(('d%d' % j, self.dsem[j], self.dcnt[j], 'dma'), reads, writes)

    def wait_all(self, eng, keys):
        deps = []
        for k in keys:
            b = self.bufs.get(k)
            if b:
                if b['w'] is not None:
                    deps.append(b['w'])
                deps.extend(b['r'])
        self._wait(eng, deps)

    def barrier(self):
        for e in self.engs:
            deps = [('e_' + f, self.sem[f], self.cnt[f], f) for f in self.engs if f != e and self.cnt[f] > 0]
            deps += [('d%d' % j, self.dsem[j], self.dcnt[j], 'dma') for j in range(self.NDMA) if self.dcnt[j] > 0]
            self._wait(e, deps)


def K(name, a, b=None):
    if b is None:
        return ["%s%d" % (name, a)]
    return ["%s%d" % (name, i) for i in range(a, b)]


def build(debug=None):
    nc = bass.Bass("TRN2", target_bir_lowering=False)
    D = {}

    def din(name, shape, dt=F32):
        D[name] = nc.dram_tensor(name, shape, dt, kind="ExternalInput").ap()
        return D[name]

    xs = din("xs", [NTOK, 1024]); cT_d = din("cT", [128, 8, 2]); wmod_d = din("w_mod", [128, 8, 6144])
    bmod_d = din("b_modT", [128, 48]); win_d = din("w_in", [128, 8, 4624]); lg_d = din("lgT", [128, 2, 2, 4])
    hgn_d = din("hgn", [64, 512]); mln_d = din("mln", [64, 512]); convw_d = din("convw", [128, 9, 8])
    convb_d = din("convb", [128, 8]); gateb_d = din("gateb", [8, 2]); wout_d = din("w_out", [128, 8, 1024])
    ln1g_d = din("ln1g", [128, 1024]); ln1b_d = din("ln1b", [128, 1024]); ln2g_d = din("ln2g", [128, 1024])
    ln2b_d = din("ln2b", [128, 1024]); wq_d = din("wq", [128, 8, 2048]); keysT_d = din("keysT", [128, 16, 128])
    NEXP = 128 if debug else 16384
    pu_d = din("pu", [NEXP, 1024]); pv_d = din("pv", [NEXP, 1024])
    ident_d = din("ident", [128, 128]); masks_d = din("masks", [64, 2, 64]); rmask_d = din("rmask", [128, 512])
    sel8_d = din("sel8", [8, 8]); dirm_d = din("dirm", [8, 2])
    out_d = nc.dram_tensor("out", [NLAT, 1024], F32, kind="ExternalOutput").ap()
    dbg_d = None
    if debug:
        dbg_d = nc.dram_tensor("dbg", [NLAT, 1024] if debug != 'mix' else [1024, NLAT], F32, kind="ExternalOutput").ap()

    with ExitStack() as es:
        S = Sched(nc, es)

        uid = [0]

        def sb(st, name, shape, dt=F32):
            uid[0] += 1
            return st.enter_context(nc.sbuf_tensor("s%d_%s" % (uid[0], name), shape, dt))

        PB = [es.enter_context(nc.psum_tensor("pb%d" % i, [128, 512], F32)) for i in range(8)]

        ident = sb(es, "ident", [128, 128]); identb = sb(es, "identb", [128, 128], BF16)
        masks = sb(es, "masks", [64, 2, 64]); rmask = sb(es, "rmask", [128, 512])
        ones = sb(es, "ones", [128, 128]); epsc = sb(es, "epsc", [128, 1])
        modv = sb(es, "modv", [128, 48, 2])
        S.dma('sp', lambda e: e.dma_start(out=ident[:], in_=ident_d[:, :]), writes=['ident'])
        S.dma('sp', lambda e: e.dma_start(out=masks[:], in_=masks_d[:, :, :]), writes=['masks'])
        S.dma('sp', lambda e: e.dma_start(out=rmask[:], in_=rmask_d[:, :]), writes=['rmask'])
        S.op('dve', lambda e: e.tensor_copy(out=identb[:], in_=ident[:]), reads=['ident'], writes=['identb'])
        S.op('dve', lambda e: e.memset(ones[:], 1.0), writes=['ones'])
        S.op('dve', lambda e: e.memset(epsc[:], EPS), writes=['epsc'])

        with ExitStack() as p0:
            cT = sb(p0, "cT", [128, 8, 2]); scT = sb(p0, "scT", [128, 8, 2]); bmodT = sb(p0, "bmodT", [128, 48])
            wm = [sb(p0, "wm%d" % i, [128, 6144]) for i in range(2)]
            S.dma('sp', lambda e: e.dma_start(out=cT[:], in_=cT_d[:, :, :]), writes=['cT'])
            S.dma('sp', lambda e: e.dma_start(out=bmodT[:], in_=bmod_d[:, :]), writes=['bmodT'])
            S.op('act', lambda e: e.activation(out=scT[:], in_=cT[:], func=AF.Silu), reads=['cT'], writes=['scT'])
            for kc in range(8):
                w = wm[kc % 2]; wk = 'wm%d' % (kc % 2)
                S.dma('sp' if kc % 2 == 0 else 'pool', lambda e, w=w, kc=kc: e.dma_start(out=w[:], in_=wmod_d[:, kc, :]), writes=[wk])
                for j in range(48):
                    S.op('pe', lambda e, w=w, kc=kc, j=j: e.matmul(PB[kc // 4][:, (kc % 4) * 96 + 2 * j:(kc % 4) * 96 + 2 * j + 2], lhsT=w[:, j * 128:(j + 1) * 128], rhs=scT[:, kc, :],
                                                                 start=True, stop=True),
                         reads=[wk, 'scT'], writes=['pb%d' % (kc // 4)], skip_self=True)
            mflat = modv[:].rearrange("p j n -> p (j n)")
            S.op('dve', lambda e: e.tensor_tensor(out=modv[:], in0=PB[0][:, 0:96].rearrange("p (j n) -> p j n", n=2), in1=bmodT[:].unsqueeze(2).to_broadcast([128, 48, 2]), op=ALU.add),
                 reads=['pb0', 'bmodT'], writes=['modv'])
            for kc in range(1, 8):
                S.op('dve', lambda e, kc=kc: e.tensor_tensor(out=mflat, in0=mflat, in1=PB[kc // 4][:, (kc % 4) * 96:(kc % 4) * 96 + 96], op=ALU.add),
                     reads=['pb%d' % (kc // 4), 'modv'], writes=['modv'])
            S.op('dve', lambda e: e.tensor_scalar_add(out=modv[:, 8:16, :], in0=modv[:, 8:16, :], scalar1=1.0), reads=['modv'], writes=['modv'])
            S.op('dve', lambda e: e.tensor_scalar_add(out=modv[:, 32:40, :], in0=modv[:, 32:40, :], scalar1=1.0), reads=['modv'], writes=['modv'])
            S.barrier()
        if debug == 'p0':
            S.dma('sp', lambda e: e.dma_start(out=dbg_d[0:128, 0:96], in_=modv[:].rearrange("p a b -> p (a b)")), reads=['modv'], writes=['dbg'])
            S.wait_all('sp', ['dbg']); S.barrier()
            return nc

        scA = ExitStack(); scB = ExitStack()
        mixT = sb(scA, "mixT", [128, 8, NLAT], BF16)
        hT = sb(scB, "hT", [128, 8, NTOK], BF16)
        wstg = sb(scB, "wstg", [128, 8, 128])
        tstg = [sb(scB, "tstg%d" % i, [128, 512]) for i in range(2)]
        tbf = [sb(scB, "tbf%d" % i, [128, 512], BF16) for i in range(2)]
        ubf_d = nc.dram_tensor("ubf", [NEXP, 1024], BF16, kind="Internal").ap()
        vbf_d = nc.dram_tensor("vbf", [NEXP, 1024], BF16, kind="Internal").ap()
        prep_pos = [0]

        def prep_tables(npieces):
            if debug:
                return
            for _ in range(npieces):
                i = prep_pos[0]
                if i >= 512:
                    return
                prep_pos[0] += 1
                src, dst = (pu_d, ubf_d) if i < 256 else (pv_d, vbf_d)
                r = (i % 256) // 2; c = (i % 2) * 512; bb = i % 2
                S.dma('sp', lambda e, src=src, r=r, c=c, bb=bb: e.dma_start(out=tstg[bb][:], in_=src[r * 128:(r + 1) * 128, c:c + 512]), writes=['tstg%d' % bb])
                S.op('pool', lambda e, bb=bb: e.tensor_copy(out=tbf[bb][:], in_=tstg[bb][:]), reads=['tstg%d' % bb], writes=['tbf%d' % bb])
                S.dma('pool', lambda e, dst=dst, r=r, c=c, bb=bb: e.dma_start(out=dst[r * 128:(r + 1) * 128, c:c + 512], in_=tbf[bb][:]), reads=['tbf%d' % bb], writes=['tabs'])

        def layer_norm_rows(st_ap, mv_ap, rstd_ap, src, dst, skey, dkey, tag):
            S.op('dve', lambda e: e.bn_stats(out=st_ap[:, 0, :], in_=src[:, 0:512]), reads=[skey], writes=[tag + 'st'])
            S.op('dve', lambda e: e.bn_stats(out=st_ap[:, 1, :], in_=src[:, 512:1024]), reads=[skey], writes=[tag + 'st'])
            S.op('dve', lambda e: e.bn_aggr(out=mv_ap, in_=st_ap[:].rearrange("p a b -> p (a b)")), reads=[tag + 'st'], writes=[tag + 'mv'])
            S.op('act', lambda e: e.activation(out=rstd_ap, in_=mv_ap[:, 1:2], func=AF.Sqrt, bias=epsc[:, 0:1], scale=1.0),
                 reads=[tag + 'mv', 'epsc'], writes=[tag + 'rs'])
            S.op('dve', lambda e: e.reciprocal(out=rstd_ap, in_=rstd_ap), reads=[tag + 'rs'], writes=[tag + 'rs'])
            S.op('dve', lambda e: e.tensor_scalar(out=dst, in0=src, scalar1=mv_ap[:, 0:1], scalar2=rstd_ap, op0=ALU.subtract, op1=ALU.mult),
                 reads=[skey, tag + 'mv', tag + 'rs'], writes=[dkey])

        with ExitStack() as p1:
            xt = [sb(p1, "xt%d" % i, [128, 1024]) for i in range(2)]
            xn = [sb(p1, "xn%d" % i, [128, 1024]) for i in range(2)]
            st = [sb(p1, "st%d" % i, [128, 2, 6]) for i in range(2)]
            mv = [sb(p1, "mv%d" % i, [128, 2]) for i in range(2)]
            rs = [sb(p1, "rs%d" % i, [128, 1]) for i in range(2)]
            PSAP = bool(os.environ.get("KPSAP"))
            for t in range(18):
                b = t % 2
                n = 1 if t < 2 else 0
                S.dma('sp' if b == 0 else 'pool', lambda e, t=t, b=b: e.dma_start(out=xt[b][:], in_=xs[t * 128:(t + 1) * 128, :]), writes=['xt%d' % b])
                layer_norm_rows(st[b], mv[b][:], rs[b][:], xt[b][:], xn[b][:], 'xt%d' % b, 'xn%d' % b, 'p1%d' % b)
                for half in range(2):
                    bank = 2 * b + half
                    pk = 'pb%d' % bank
                    for c4 in range(4):
                        ch = half * 4 + c4
                        S.op('pe', lambda e, b=b, ch=ch, c4=c4, bank=bank: e.transpose(PB[bank][:, c4 * 128:(c4 + 1) * 128], xn[b][:, ch * 128:(ch + 1) * 128], ident[:]),
                             reads=['xn%d' % b, 'ident'], writes=[pk], skip_self=True)
                    if not PSAP:
                        S.op('act', lambda e, b=b, half=half, bank=bank: e.copy(out=xt[b][:, half * 512:(half + 1) * 512], in_=PB[bank][:, :]), reads=[pk, 'xt%d' % b], writes=['xt%d' % b])
                    for c4 in range(4):
                        ch = half * 4 + c4
                        dst = hT[:, ch, t * 128:(t + 1) * 128]
                        src = PB[bank][:, c4 * 128:(c4 + 1) * 128] if PSAP else xt[b][:, ch * 128:(ch + 1) * 128]
                        S.op('dve', lambda e, dst=dst, src=src, ch=ch, n=n: e.tensor_scalar(out=dst, in0=src, scalar1=modv[:, 8 + ch, n:n + 1], scalar2=modv[:, ch, n:n + 1],
                                                                                      op0=ALU.mult, op1=ALU.add),
                             reads=([pk] if PSAP else ['xt%d' % b]) + ['modv'], writes=K('hT', t))
            S.barrier()

        if debug == 'p1':
            with ExitStack() as dd:
                hf = sb(dd, "hf", [128, 1024])
                for t in range(16):
                    S.op('dve', lambda e, t=t: e.tensor_copy(out=hf[:].rearrange("p (a b) -> p a b", b=128), in_=hT[:, :, 256 + t * 128:256 + (t + 1) * 128]), reads=K('hT', t + 2) + ['hf'], writes=['hf'])
                    S.dma('sp', lambda e, t=t: e.dma_start(out=dbg_d[t * 128:(t + 1) * 128, :], in_=hf[:]), reads=['hf'], writes=['dbg'])
                S.wait_all('sp', ['dbg']); S.barrier()
            scB.close(); scA.close()
            return nc
        def load_w(stk, name, col0, ncols, src=None):
            src = win_d if src is None else src
            wb = sb(stk, name, [128, 8, ncols], BF16)
            S.dma('sp', lambda e: e.dma_start(out=wstg[:, :, 0:ncols], in_=src[:, :, col0:col0 + ncols]), writes=['wstg'])
            S.op('pool', lambda e: e.tensor_copy(out=wb[:], in_=wstg[:, :, 0:ncols]), reads=['wstg'], writes=[name])
            return wb

        BLKS = [(0, 512), (512, 512), (1024, 512), (1536, 512), (2048, 256)]

        def proj_fm_block(wb, wname, ncols, bank, t0, nt):
            for kc in range(8):
                S.op('pe', lambda e, kc=kc: e.matmul(PB[bank][0:ncols, 0:nt], lhsT=wb[:, kc, :], rhs=hT[:, kc, t0:t0 + nt], start=(kc == 0), stop=(kc == 7)),
                     reads=[wname] + K('hT', t0 // 128, (t0 + nt) // 128), writes=['pb%d' % bank], skip_self=True)

        def proj_tm_group(wb, wname, ncols, bank, c0, ncks):
            for j in range(ncks):
                n = c0 + j
                for kc in range(8):
                    S.op('pe', lambda e, kc=kc, j=j, n=n: e.matmul(PB[bank][0:64, j * ncols:(j + 1) * ncols], lhsT=hT[:, kc, n * 64:(n + 1) * 64], rhs=wb[:, kc, :],
                                                                   start=(kc == 0), stop=(kc == 7)),
                         reads=[wname] + K('hT', n // 2), writes=['pb%d' % bank], skip_self=True)

        S32 = sb(scB, "S32", [128, 2, 132]); Sbf = sb(scB, "Sbf", [128, 2, 132], BF16)
        stm = sb(scB, "stm", [64, 2, 64], BF16)
        usb = sb(scB, "usb", [128, 2, 132])

        def chunk_loop(dirn, KsT, QsT, Vi, QoT, Ku, Vu, dec_ap, W, evac, rk, tagk):
            order = list(range(36)) if dirn == 0 else [3, 2, 1, 0] + list(range(35, 3, -1))
            S.op('dve', lambda e: e.memset(S32[:, 0, :], 0.0), writes=['S32_0'])
            S.op('dve', lambda e: e.memset(Sbf[:, 0, :], 0.0), writes=['Sbf_0'])
            cur = 0
            for idx, n in enumerate(order):
                sl = slice(n * 64, (n + 1) * 64)
                s2 = idx % 2
                if n >= 4:
                    pst = PB[2 + s2][0:64, 0:64]; pstk = 'pb%d' % (2 + s2)
                    po = PB[4 + s2][0:64, 0:W]; pok = 'pb%d' % (4 + s2)
                    S.op('pe', lambda e, pst=pst, sl=sl: e.matmul(pst, lhsT=KsT[:, sl], rhs=QsT[:, sl], start=True, stop=True),
                         reads=rk, writes=[pstk], skip_self=True)
                    S.op('dve', lambda e, pst=pst, s2=s2: e.tensor_tensor(out=stm[:, s2, :], in0=pst, in1=masks[:, dirn, :], op=ALU.mult),
                         reads=[pstk, 'masks'], writes=['stm%d' % s2])
                    S.op('pe', lambda e, po=po, s2=s2, n=n: e.matmul(po, lhsT=stm[:, s2, :], rhs=Vi[:, n, 0:W], start=True, stop=False),
                         reads=['stm%d' % s2] + rk, writes=[pok], skip_self=True)
                    S.op('pe', lambda e, po=po, sl=sl, cur=cur: e.matmul(po, lhsT=QoT[:, sl], rhs=Sbf[:, cur, 0:W], start=False, stop=True),
                         reads=['Sbf_%d' % cur] + rk, writes=[pok], skip_self=True)
                    evac(n - 4, n, po, pok)
                if idx < 35:
                    pu = PB[6 + s2][:, 0:W]; puk = 'pb%d' % (6 + s2)
                    S.op('pe', lambda e, pu=pu, n=n: e.matmul(pu, lhsT=Ku[:, n, :], rhs=Vu[:, n, 0:W], start=True, stop=True),
                         reads=rk, writes=[puk], skip_self=True)
                    nxt = 1 - cur
                    S.op('act', lambda e, pu=pu, s2=s2: e.copy(out=usb[:, s2, 0:W], in_=pu), reads=[puk], writes=['usb%d' % s2])
                    S.op('dve', lambda e, n=n, cur=cur, nxt=nxt, s2=s2: e.scalar_tensor_tensor(out=Sbf[:, nxt, 0:W], in0=S32[:, cur, 0:W], scalar=dec_ap(n), in1=usb[:, s2, 0:W],
                                                                                            op0=ALU.mult, op1=ALU.add),
                         reads=['S32_%d' % cur, 'usb%d' % s2, tagk], writes=['Sbf_%d' % nxt])
                    S.op('dve', lambda e, n=n, cur=cur, nxt=nxt, s2=s2: e.scalar_tensor_tensor(out=S32[:, nxt, 0:W], in0=S32[:, cur, 0:W], scalar=dec_ap(n), in1=usb[:, s2, 0:W],
                                                                                            op0=ALU.mult, op1=ALU.add),
                         reads=['S32_%d' % cur, 'usb%d' % s2, tagk], writes=['S32_%d' % nxt])
                    cur = nxt

        def transpose_chunks_to_mixT(src, skey, head):
            for g in range(4):
                bank = g % 2
                for j in range(8):
                    n = g * 8 + j
                    S.op('pe', lambda e, n=n, j=j, bank=bank: e.transpose(PB[bank][:, j * 64:(j + 1) * 64], src[:, n, :], ident[0:64, 0:64]),
                         reads=list(skey) + ['ident'], writes=['pb%d' % bank], skip_self=True)
                S.op('act', lambda e, g=g, bank=bank: e.copy(out=mixT[:, head, g * 512:(g + 1) * 512], in_=PB[bank][:, :]),
                     reads=['pb%d' % bank], writes=K('mixT', head))

        with ExitStack() as g0:
            lgT = sb(g0, "lgT", [128, 2, 2, 4]); lbT = sb(g0, "lbT", [128, 2, 4]); omlT = sb(g0, "omlT", [128, 2, 4]); nomlT = sb(g0, "nomlT", [128, 2, 4])
            hgn = sb(g0, "hgn", [64, 512])
            S.dma('sp', lambda e: e.dma_start(out=lgT[:], in_=lg_d[:, :, :, :]), writes=['lgT'])
            S.dma('sp', lambda e: e.dma_start(out=hgn[:], in_=hgn_d[:, :]), writes=['hgn'])
            S.op('dve', lambda e: e.tensor_tensor(out=lbT[:], in0=lgT[:, :, 0, :], in1=lgT[:, :, 1, :], op=ALU.subtract), reads=['lgT'], writes=['lbT'])
            S.op('act', lambda e: e.activation(out=lbT[:], in_=lbT[:], func=AF.Sigmoid), reads=['lbT'], writes=['lbT'])
            S.op('dve', lambda e: e.tensor_scalar(out=omlT[:], in0=lbT[:], scalar1=-1.0, scalar2=1.0, op0=ALU.mult, op1=ALU.add), reads=['lbT'], writes=['omlT'])
            S.op('dve', lambda e: e.tensor_scalar_mul(out=nomlT[:], in0=omlT[:], scalar1=-1.0), reads=['omlT'], writes=['nomlT'])
            QsT = [sb(g0, "gQsT%d" % d, [128, NTOK], BF16) for d in range(2)]
            KsT = [sb(g0, "gKsT%d" % d, [128, NTOK], BF16) for d in range(2)]
            QoT = [sb(g0, "gQoT%d" % d, [128, NTOK], BF16) for d in range(2)]
            Ku = [sb(g0, "gKu%d" % d, [64, NCH, 128], BF16) for d in range(2)]
            dec = sb(g0, "gdec", [128, 2, NCH])
            vtm = sb(g0, "gv", [64, NCH, 128], BF16); gs = sb(g0, "ggs", [64, NLCH, 128], BF16); oacc = sb(g0, "goacc", [64, NLCH, 128])
            qf = sb(g0, "gqf", [128, 512]); sg = sb(g0, "gsg", [128, 512]); lf = sb(g0, "glf", [128, 512]); key = sb(g0, "gkey", [128, 512])
            Bc = sb(g0, "gB", [128, 512]); T1 = sb(g0, "gT1", [128, 512]); E1 = sb(g0, "gE1", [128, 512]); khT = sb(g0, "gkhT", [128, 512], BF16)
            sq = sb(g0, "gsq", [64, NLCH, 128], BF16); ss = sb(g0, "gss", [64, NLCH])
            for hd in range(4):
                with ExitStack() as hs:
                    wq_ = load_w(hs, "gwq", 0 + hd * 128, 128); wi_ = load_w(hs, "gwi", 512 + hd * 128, 128); wg_ = load_w(hs, "gwg", 1024 + hd * 128, 128)
                    wf = [load_w(hs, "gwf0", 1536 + hd * 128, 128), load_w(hs, "gwf1", 2048 + hd * 128, 128)]
                    prep_tables(64)
                    for g in range(9):
                        bk = 3 + g % 2
                        proj_tm_group(wi_, "gwi", 128, bk, g * 4, 4)
                        S.op('act', lambda e, g=g, bk=bk: e.copy(out=vtm[:, g * 4:(g + 1) * 4, :], in_=PB[bk][0:64, :].rearrange("p (j c) -> p j c", c=128)),
                             reads=['pb%d' % bk], writes=['gv'])
                    for g in range(8):
                        bk = 3 + (g + 1) % 2
                        proj_tm_group(wg_, "gwg", 128, bk, 4 + g * 4, 4)
                        S.op('act', lambda e, g=g, bk=bk: e.activation(out=gs[:, g * 4:(g + 1) * 4, :], in_=PB[bk][0:64, :].rearrange("p (j c) -> p j c", c=128), func=AF.Silu),
                             reads=['pb%d' % bk], writes=['ggs'])
                    for (t0, nt) in BLKS:
                        nck = nt // 64; c0 = t0 // 64
                        proj_fm_block(wq_, "gwq", 128, 0, t0, nt)
                        S.op('act', lambda e, nt=nt: e.copy(out=qf[:, 0:nt], in_=PB[0][:, 0:nt]), reads=['pb0'], writes=['gqf'])
                        for d in range(2):
                            proj_fm_block(wf[d], "gwf%d" % d, 128, 1 + d, t0, nt)
                            col = d * 4 + hd
                            lbp = lbT[:, d, hd:hd + 1]; omp = omlT[:, d, hd:hd + 1]; nomp = nomlT[:, d, hd:hd + 1]
                            S.op('act', lambda e, d=d, nt=nt: e.activation(out=sg[:, 0:nt], in_=PB[1 + d][:, 0:nt], func=AF.Sigmoid), reads=['pb%d' % (1 + d)], writes=['gsg'])
                            S.op('act', lambda e, nt=nt, lbp=lbp, omp=omp: e.activation(out=lf[:, 0:nt], in_=sg[:, 0:nt], func=AF.Ln, bias=lbp, scale=omp),
                                 reads=['gsg', 'lbT', 'omlT'], writes=['glf'])
                            S.op('dve', lambda e, nt=nt, nomp=nomp, omp=omp: e.tensor_scalar(out=key[:, 0:nt], in0=sg[:, 0:nt], scalar1=nomp, scalar2=omp, op0=ALU.mult, op1=ALU.add),
                                 reads=['gsg', 'omlT', 'nomlT'], writes=['gkey'])
                            S.op('dve', lambda e, nt=nt: e.tensor_tensor_scan(out=Bc[:, 0:nt], data0=rmask[:, 0:nt], data1=lf[:, 0:nt], initial=0.0, op0=ALU.mult, op1=ALU.add),
                                 reads=['glf', 'rmask'], writes=['gB'])
                            B3 = Bc[:, 0:nt].rearrange("p (n c) -> p n c", c=64)
                            T3 = T1[:, 0:nt].rearrange("p (n c) -> p n c", c=64)
                            if d == 1:
                                S.op('dve', lambda e, B3=B3, T3=T3, nck=nck: e.tensor_tensor(out=T3, in0=B3[:, :, 63:64].to_broadcast([128, nck, 64]), in1=B3, op=ALU.subtract),
                                     reads=['gB'], writes=['gT1'])
                                S.op('dve', lambda e, nt=nt: e.tensor_tensor(out=Bc[:, 0:nt], in0=T1[:, 0:nt], in1=lf[:, 0:nt], op=ALU.add), reads=['gT1', 'glf'], writes=['gB'])
                            li = 63 if d == 0 else 0
                            S.op('act', lambda e, B3=B3, li=li, d=d, c0=c0, nck=nck: e.activation(out=dec[:, d, c0:c0 + nck], in_=B3[:, :, li], func=AF.Exp), reads=['gB'], writes=['gdec'])
                            S.op('dve', lambda e, B3=B3, T3=T3, nck=nck: e.tensor_tensor(out=T3, in0=B3, in1=B3[:, :, 32:33].to_broadcast([128, nck, 64]), op=ALU.subtract),
                                 reads=['gB'], writes=['gT1'])
                            S.op('act', lambda e, nt=nt: e.activation(out=E1[:, 0:nt], in_=T1[:, 0:nt], func=AF.Exp), reads=['gT1'], writes=['gE1'])
                            S.op('dve', lambda e, nt=nt, t0=t0, d=d: e.tensor_tensor(out=QsT[d][:, t0:t0 + nt], in0=qf[:, 0:nt], in1=E1[:, 0:nt], op=ALU.mult),
                                 reads=['gqf', 'gE1'], writes=['gQsT%d' % d])
                            S.op('act', lambda e, nt=nt: e.activation(out=E1[:, 0:nt], in_=T1[:, 0:nt], func=AF.Exp, scale=-1.0), reads=['gT1'], writes=['gE1'])
                            S.op('dve', lambda e, nt=nt, t0=t0, d=d: e.tensor_tensor(out=KsT[d][:, t0:t0 + nt], in0=key[:, 0:nt], in1=E1[:, 0:nt], op=ALU.mult),
                                 reads=['gkey', 'gE1'], writes=['gKsT%d' % d])
                            S.op('act', lambda e, nt=nt: e.activation(out=E1[:, 0:nt], in_=Bc[:, 0:nt], func=AF.Exp), reads=['gB'], writes=['gE1'])
                            S.op('dve', lambda e, nt=nt, t0=t0, d=d: e.tensor_tensor(out=QoT[d][:, t0:t0 + nt], in0=qf[:, 0:nt], in1=E1[:, 0:nt], op=ALU.mult),
                                 reads=['gqf', 'gE1'], writes=['gQoT%d' % d])
                            S.op('dve', lambda e, B3=B3, T3=T3, nck=nck, li=li: e.tensor_tensor(out=T3, in0=B3[:, :, li:li + 1].to_broadcast([128, nck, 64]), in1=B3, op=ALU.subtract),
                                 reads=['gB'], writes=['gT1'])
                            S.op('act', lambda e, nt=nt: e.activation(out=E1[:, 0:nt], in_=T1[:, 0:nt], func=AF.Exp), reads=['gT1'], writes=['gE1'])
                            S.op('dve', lambda e, nt=nt: e.tensor_tensor(out=khT[:, 0:nt], in0=key[:, 0:nt], in1=E1[:, 0:nt], op=ALU.mult), reads=['gkey', 'gE1'], writes=['gkhT'])
                            pbb = PB[3][:].bitcast(BF16)
                            for j in range(nck):
                                S.op('pe', lambda e, j=j: e.transpose(pbb[0:64, j * 128:(j + 1) * 128], khT[:, j * 64:(j + 1) * 64], identb[:]),
                                     reads=['gkhT', 'identb'], writes=['pb3'], skip_self=True)
                            S.op('act', lambda e, d=d, c0=c0, nck=nck: e.copy(out=Ku[d][:, c0:c0 + nck, :], in_=pbb[0:64, 0:nck * 128].rearrange("p (j c) -> p j c", c=128)),
                                 reads=['pb3'], writes=['gKu%d' % d])
                    for d in range(2):
                        def evac(nl, n, po, pok, d=d):
                            if d == 0:
                                S.op('act', lambda e: e.copy(out=oacc[:, nl, :], in_=po), reads=[pok], writes=K('goacc', nl))
                            else:
                                S.op('dve', lambda e: e.tensor_tensor(out=oacc[:, nl, :], in0=po, in1=oacc[:, nl, :], op=ALU.add), reads=[pok] + K('goacc', nl), writes=K('goacc', nl))
                        chunk_loop(d, KsT[d], QsT[d], vtm, QoT[d], Ku[d], vtm, lambda n, d=d: dec[:, d, n:n + 1], 128, evac,
                                   ['gQsT%d' % d, 'gKsT%d' % d, 'gQoT%d' % d, 'gKu%d' % d, 'gv'], 'gdec')
                    allo = K('goacc', 0, NLCH)
                    S.op('dve', lambda e: e.tensor_tensor(out=sq[:], in0=oacc[:], in1=oacc[:], op=ALU.mult), reads=allo, writes=['gsq'])
                    S.op('dve', lambda e: e.tensor_reduce(out=ss[:], in_=sq[:], axis=AX.X, op=ALU.add), reads=['gsq'], writes=['gss'])
                    S.op('act', lambda e: e.activation(out=ss[:], in_=ss[:], func=AF.Sqrt, bias=epsc[0:64, 0:1], scale=1.0 / 128.0), reads=['gss', 'epsc'], writes=['gss'])
                    S.op('dve', lambda e: e.reciprocal(out=ss[:], in_=ss[:]), reads=['gss'], writes=['gss'])
                    S.op('dve', lambda e: e.tensor_tensor(out=oacc[:], in0=oacc[:], in1=ss[:].unsqueeze(2).to_broadcast([64, NLCH, 128]), op=ALU.mult),
                         reads=allo + ['gss'], writes=allo)
                    S.op('dve', lambda e, hd=hd: e.tensor_tensor(out=oacc[:], in0=oacc[:], in1=hgn[:, hd * 128:(hd + 1) * 128].unsqueeze(1).to_broadcast([64, NLCH, 128]), op=ALU.mult),
                         reads=allo + ['hgn'], writes=allo)
                    S.op('dve', lambda e: e.tensor_tensor(out=oacc[:], in0=oacc[:], in1=gs[:], op=ALU.mult), reads=allo + ['ggs'], writes=allo)
                    transpose_chunks_to_mixT(oacc, allo, hd)
                    S.barrier()
            S.barrier()

        with ExitStack() as m0:
            mln = sb(m0, "mln", [64, 512]); convw = sb(m0, "convw", [128, 9, 8]); convb = sb(m0, "convb", [128, 8])
            gateb = sb(m0, "gateb", [8, 2]); sel8 = sb(m0, "sel8", [8, 8]); dirm = sb(m0, "dirm", [8, 2])
            S.dma('sp', lambda e: e.dma_start(out=mln[:], in_=mln_d[:, :]), writes=['mln'])
            S.dma('sp', lambda e: e.dma_start(out=convw[:], in_=convw_d[:, :, :]), writes=['convw'])
            S.dma('sp', lambda e: e.dma_start(out=convb[:], in_=convb_d[:, :]), writes=['convb'])
            S.dma('sp', lambda e: e.dma_start(out=gateb[:], in_=gateb_d[:, :]), writes=['gateb'])
            S.dma('sp', lambda e: e.dma_start(out=sel8[:], in_=sel8_d[:, :]), writes=['sel8'])
            S.dma('sp', lambda e: e.dma_start(out=dirm[:], in_=dirm_d[:, :]), writes=['dirm'])
            RUU = sb(m0, "RUU", [64, NCH, 24]); dchunk = sb(m0, "dchunk", [128, 8, NCH])
            with ExitStack() as gp:
                wgi = load_w(gp, "mwgi", 4608, 8); wgf = load_w(gp, "mwgf", 4616, 8)
                LI = sb(gp, "LI", [8, NTOK]); LF = sb(gp, "LF", [8, NTOK]); Af = sb(gp, "Af", [8, NTOK]); Ab = sb(gp, "Ab", [8, NTOK]); Aa = sb(gp, "Aa", [8, NTOK])
                R = [sb(gp, "Rr%d" % i, [8, NTOK]) for i in range(3)]
                bd = sb(gp, "bd", [8, 8, NCH])
                for (t0, nt) in BLKS:
                    proj_fm_block(wgi, "mwgi", 8, 0, t0, nt)
                    S.op('act', lambda e, t0=t0, nt=nt: e.copy(out=LI[:, t0:t0 + nt], in_=PB[0][0:8, 0:nt]), reads=['pb0'], writes=['LI'])
                    proj_fm_block(wgf, "mwgf", 8, 1, t0, nt)
                    S.op('act', lambda e, t0=t0, nt=nt: e.copy(out=LF[:, t0:t0 + nt], in_=PB[1][0:8, 0:nt]), reads=['pb1'], writes=['LF'])
                S.op('dve', lambda e: e.tensor_scalar_add(out=LI[:], in0=LI[:], scalar1=gateb[:, 0:1]), reads=['LI', 'gateb'], writes=['LI'])
                S.op('act', lambda e: e.activation(out=LF[:], in_=LF[:], func=AF.Sigmoid, bias=gateb[:, 1:2], scale=1.0), reads=['LF', 'gateb'], writes=['LF'])
                S.op('act', lambda e: e.activation(out=LF[:], in_=LF[:], func=AF.Ln), reads=['LF'], writes=['LF'])
                for (t0, nt) in BLKS:
                    S.op('dve', lambda e, t0=t0, nt=nt: e.tensor_tensor_scan(out=Af[:, t0:t0 + nt], data0=rmask[0:8, 0:nt], data1=LF[:, t0:t0 + nt], initial=0.0, op0=ALU.mult, op1=ALU.add),
                         reads=['LF', 'rmask'], writes=['Af'])
                A3 = Af[:].rearrange("p (n c) -> p n c", c=64)
                tot = A3[:, :, 63:64]
                S.op('dve', lambda e: e.tensor_tensor(out=Ab[:].rearrange("p (n c) -> p n c", c=64), in0=tot.to_broadcast([8, NCH, 64]), in1=A3, op=ALU.subtract), reads=['Af'], writes=['Ab'])
                S.op('dve', lambda e: e.tensor_tensor(out=Ab[:], in0=Ab[:], in1=LF[:], op=ALU.add), reads=['Ab', 'LF'], writes=['Ab'])
                S.op('dve', lambda e: e.tensor_scalar_mul(out=Aa[:], in0=Af[:], scalar1=dirm[:, 0:1]), reads=['Af', 'dirm'], writes=['Aa'])
                S.op('dve', lambda e: e.scalar_tensor_tensor(out=Aa[:], in0=Ab[:], scalar=dirm[:, 1:2], in1=Aa[:], op0=ALU.mult, op1=ALU.add), reads=['Ab', 'dirm', 'Aa'], writes=['Aa'])
                S.op('act', lambda e: e.activation(out=R[0][:], in_=Aa[:], func=AF.Exp), reads=['Aa'], writes=['Rr0'])
                S.op('dve', lambda e: e.tensor_tensor(out=Ab[:], in0=LI[:], in1=Aa[:], op=ALU.subtract), reads=['LI', 'Aa', 'Ab'], writes=['Ab'])
                S.op('act', lambda e: e.activation(out=R[1][:], in_=Ab[:], func=AF.Exp), reads=['Ab'], writes=['Rr1'])
                S.op('dve', lambda e: e.tensor_tensor(out=Aa[:].rearrange("p (n c) -> p n c", c=64), in0=Ab[:].rearrange("p (n c) -> p n c", c=64), in1=tot.to_broadcast([8, NCH, 64]), op=ALU.add),
                     reads=['Ab', 'Af', 'Aa', 'Rr0'], writes=['Aa'])
                S.op('act', lambda e: e.activation(out=R[2][:], in_=Aa[:], func=AF.Exp), reads=['Aa'], writes=['Rr2'])
                for half in range(2):
                    for j in range(18):
                        n = half * 18 + j
                        for q in range(3):
                            S.op('pe', lambda e, n=n, j=j, q=q: e.transpose(PB[2][0:64, j * 24 + q * 8:j * 24 + q * 8 + 8], R[q][:, n * 64:(n + 1) * 64], ident[0:8, 0:8]),
                                 reads=['Rr%d' % q, 'ident'], writes=['pb2'], skip_self=True)
                    S.op('act', lambda e, half=half: e.copy(out=RUU[:, half * 18:(half + 1) * 18, :], in_=PB[2][0:64, 0:432].rearrange("p (j c) -> p j c", c=24)),
                         reads=['pb2'], writes=['RUU'])
                S.op('dve', lambda e: e.tensor_tensor(out=bd[:], in0=tot.rearrange("p n c -> p c n").to_broadcast([8, 8, NCH]), in1=sel8[:].unsqueeze(2).to_broadcast([8, 8, NCH]), op=ALU.mult),
                     reads=['Af', 'sel8'], writes=['bd'])
                S.op('pe', lambda e: e.matmul(PB[3][:, 0:288], lhsT=ones[0:8, :], rhs=bd[:].rearrange("p a n -> p (a n)"), start=True, stop=True), reads=['ones', 'bd'], writes=['pb3'], skip_self=True)
                S.op('act', lambda e: e.activation(out=dchunk[:].rearrange("p a n -> p (a n)"), in_=PB[3][:, 0:288], func=AF.Exp), reads=['pb3'], writes=['dchunk'])
                S.barrier()
            qc = sb(m0, "mqc", [128, NTOK], BF16); kc_ = sb(m0, "mkc", [128, NTOK], BF16); kTM = sb(m0, "mkTM", [64, NCH, 128], BF16)
            raw = sb(m0, "mraw", [128, NTOK]); acc = sb(m0, "macc", [128, NTOK])
            vext = sb(m0, "mvext", [64, NCH, 132], BF16); vi = sb(m0, "mvi", [64, NCH, 132], BF16); vu = sb(m0, "mvu", [64, NCH, 132], BF16)
            og = sb(m0, "mog", [64, NLCH, 128], BF16); oext = sb(m0, "moext", [64, NLCH, 132]); obuf = sb(m0, "mobuf", [64, NLCH, 128])
            den = sb(m0, "mden", [64, NLCH]); mu = sb(m0, "mmu", [64, NLCH]); m2 = sb(m0, "mm2", [64, NLCH])
            S.op('dve', lambda e: e.memset(vext[:], 1.0), writes=['mvext'])
            for hd in range(4):
                with ExitStack() as hs:
                    wq_ = load_w(hs, "mwq", 2560 + hd * 128, 128); wk_ = load_w(hs, "mwk", 3072 + hd * 128, 128)
                    wv_ = load_w(hs, "mwv", 3584 + hd * 128, 128); wo_ = load_w(hs, "mwo", 4096 + hd * 128, 128)
                    prep_tables(64)
                    for g in range(9):
                        bk = 3 + g % 2
                        proj_tm_group(wv_, "mwv", 128, bk, g * 4, 4)
                        S.op('act', lambda e, g=g, bk=bk: e.copy(out=vext[:, g * 4:(g + 1) * 4, 0:128], in_=PB[bk][0:64, :].rearrange("p (j c) -> p j c", c=128)),
                             reads=['pb%d' % bk], writes=['mvext'])
                    for g in range(8):
                        bk = 3 + (g + 1) % 2
                        proj_tm_group(wo_, "mwo", 128, bk, 4 + g * 4, 4)
                        S.op('act', lambda e, g=g, bk=bk: e.activation(out=og[:, g * 4:(g + 1) * 4, :], in_=PB[bk][0:64, :].rearrange("p (j c) -> p j c", c=128), func=AF.Sigmoid),
                             reads=['pb%d' % bk], writes=['mog'])
                    for qi, (wb, wn, dstb) in enumerate(((wq_, "mwq", qc), (wk_, "mwk", kc_))):
                        chn = qi * 4 + hd
                        for bi, (t0, nt) in enumerate(BLKS):
                            proj_fm_block(wb, wn, 128, bi % 2, t0, nt)
                            S.op('act', lambda e, t0=t0, nt=nt, bi=bi: e.copy(out=raw[:, t0:t0 + nt], in_=PB[bi % 2][:, 0:nt]), reads=['pb%d' % (bi % 2)], writes=['mraw'])
                        S.op('dve', lambda e, chn=chn: e.tensor_scalar(out=acc[:, 0:256], in0=raw[:, 0:256], scalar1=convw[:, 4, chn:chn + 1], scalar2=convb[:, chn:chn + 1], op0=ALU.mult, op1=ALU.add),
                             reads=['mraw', 'convw', 'convb'], writes=['macc'])
                        S.op('dve', lambda e, chn=chn: e.scalar_tensor_tensor(out=acc[:, 1:256], in0=raw[:, 0:255], scalar=convw[:, 3, chn:chn + 1], in1=acc[:, 1:256], op0=ALU.mult, op1=ALU.add),
                             reads=['mraw', 'convw', 'macc'], writes=['macc'])
                        S.op('dve', lambda e, chn=chn: e.scalar_tensor_tensor(out=acc[:, 0:255], in0=raw[:, 1:256], scalar=convw[:, 5, chn:chn + 1], in1=acc[:, 0:255], op0=ALU.mult, op1=ALU.add),
                             reads=['mraw', 'convw', 'macc'], writes=['macc'])
                        X = raw[:, 256:NTOK].rearrange("p (r c) -> p r c", c=64); Y = acc[:, 256:NTOK].rearrange("p (r c) -> p r c", c=64)
                        S.op('dve', lambda e, chn=chn: e.tensor_scalar(out=acc[:, 256:NTOK], in0=raw[:, 256:NTOK], scalar1=convw[:, 4, chn:chn + 1], scalar2=convb[:, chn:chn + 1], op0=ALU.mult, op1=ALU.add),
                             reads=['mraw', 'convw', 'convb', 'macc'], writes=['macc'])
                        for ky in range(3):
                            for kx in range(3):
                                if ky == 1 and kx == 1:
                                    continue
                                dy = ky - 1; dx = kx - 1
                                r0 = max(0, -dy); r1 = 32 - max(0, dy); c0 = max(0, -dx); c1 = 64 - max(0, dx)
                                S.op('dve', lambda e, chn=chn, ky=ky, kx=kx, r0=r0, r1=r1, c0=c0, c1=c1, dy=dy, dx=dx: e.scalar_tensor_tensor(
                                    out=Y[:, r0:r1, c0:c1], in0=X[:, r0 + dy:r1 + dy, c0 + dx:c1 + dx], scalar=convw[:, ky * 3 + kx, chn:chn + 1], in1=Y[:, r0:r1, c0:c1],
                                    op0=ALU.mult, op1=ALU.add), reads=['mraw', 'convw', 'macc'], writes=['macc'])
                        S.op('act', lambda e: e.activation(out=acc[:], in_=acc[:], func=AF.Silu), reads=['macc'], writes=['macc'])
                        if qi == 0:
                            S.op('dve', lambda e: e.tensor_copy(out=qc[:], in_=acc[:]), reads=['macc'], writes=['mqc'])
                        else:
                            S.op('dve', lambda e: e.tensor_scalar_mul(out=kc_[:], in0=acc[:], scalar1=128.0 ** -0.5), reads=['macc'], writes=['mkc'])
                    pbb = PB[3][:].bitcast(BF16)
                    for g in range(5):
                        nck = 8 if g < 4 else 4
                        for j in range(nck):
                            n = g * 8 + j
                            S.op('pe', lambda e, j=j, n=n: e.transpose(pbb[0:64, j * 128:(j + 1) * 128], kc_[:, n * 64:(n + 1) * 64], identb[:]),
                                 reads=['mkc', 'identb'], writes=['pb3'], skip_self=True)
                        S.op('act', lambda e, g=g, nck=nck: e.copy(out=kTM[:, g * 8:g * 8 + nck, :], in_=pbb[0:64, 0:nck * 128].rearrange("p (j c) -> p j c", c=128)),
                             reads=['pb3'], writes=['mkTM'])
                    for d in range(2):
                        row = d * 4 + hd
                        S.op('dve', lambda e, row=row: e.tensor_tensor(out=vi[:], in0=vext[:], in1=RUU[:, :, 8 + row:9 + row].to_broadcast([64, NCH, 132]), op=ALU.mult),
                             reads=['mvext', 'RUU'], writes=['mvi'])
                        S.op('dve', lambda e, row=row: e.tensor_tensor(out=vu[:], in0=vext[:], in1=RUU[:, :, 16 + row:17 + row].to_broadcast([64, NCH, 132]), op=ALU.mult),
                             reads=['mvext', 'RUU'], writes=['mvu'])

                        def evac(nl, n, po, pok, row=row):
                            S.op('act', lambda e: e.copy(out=oext[:, nl, 0:129], in_=po), reads=[pok], writes=K('moext', nl))
                        chunk_loop(d, kc_, qc, vi, qc, kTM, vu, lambda n, row=row: dchunk[:, row, n:n + 1], 129, evac,
                                   ['mqc', 'mkc', 'mvi', 'mvu', 'mkTM'], 'dchunk')
                        allx = K('moext', 0, NLCH)
                        S.op('dve', lambda e, row=row: e.tensor_tensor(out=oext[:, :, 0:129], in0=oext[:, :, 0:129], in1=RUU[:, 4:NCH, row:row + 1].to_broadcast([64, NLCH, 129]), op=ALU.mult),
                             reads=allx + ['RUU'], writes=allx)
                        S.op('act', lambda e: e.activation(out=den[:], in_=oext[:, :, 128], func=AF.Abs), reads=allx, writes=['mden'])
                        S.op('dve', lambda e: e.tensor_scalar_max(out=den[:], in0=den[:], scalar1=1.0), reads=['mden'], writes=['mden'])
                        S.op('dve', lambda e: e.reciprocal(out=den[:], in_=den[:]), reads=['mden'], writes=['mden'])
                        if d == 0:
                            S.op('dve', lambda e: e.tensor_tensor(out=obuf[:], in0=oext[:, :, 0:128], in1=den[:].unsqueeze(2).to_broadcast([64, NLCH, 128]), op=ALU.mult),
                                 reads=allx + ['mden'], writes=['mobuf'])
                        else:
                            S.op('dve', lambda e: e.tensor_tensor(out=oext[:, :, 0:128], in0=oext[:, :, 0:128], in1=den[:].unsqueeze(2).to_broadcast([64, NLCH, 128]), op=ALU.mult),
                                 reads=allx + ['mden'], writes=allx)
                            S.op('dve', lambda e: e.tensor_tensor(out=obuf[:], in0=obuf[:], in1=oext[:, :, 0:128], op=ALU.add), reads=allx + ['mobuf'], writes=['mobuf'])
                    allx = K('moext', 0, NLCH)
                    S.op('dve', lambda e: e.tensor_reduce(out=mu[:], in_=obuf[:], axis=AX.X, op=ALU.add), reads=['mobuf'], writes=['mmu'])
                    S.op('dve', lambda e: e.tensor_scalar_mul(out=mu[:], in0=mu[:], scalar1=1.0 / 128.0), reads=['mmu'], writes=['mmu'])
                    S.op('dve', lambda e: e.tensor_tensor(out=obuf[:], in0=obuf[:], in1=mu[:].unsqueeze(2).to_broadcast([64, NLCH, 128]), op=ALU.subtract), reads=['mobuf', 'mmu'], writes=['mobuf'])
                    S.op('dve', lambda e: e.tensor_tensor(out=oext[:, :, 0:128], in0=obuf[:], in1=obuf[:], op=ALU.mult), reads=['mobuf'] + allx, writes=allx)
                    S.op('dve', lambda e: e.tensor_reduce(out=m2[:], in_=oext[:, :, 0:128], axis=AX.X, op=ALU.add), reads=allx, writes=['mm2'])
                    S.op('act', lambda e: e.activation(out=m2[:], in_=m2[:], func=AF.Sqrt, bias=epsc[0:64, 0:1], scale=1.0 / 128.0), reads=['mm2', 'epsc'], writes=['mm2'])
                    S.op('dve', lambda e: e.reciprocal(out=m2[:], in_=m2[:]), reads=['mm2'], writes=['mm2'])
                    S.op('dve', lambda e: e.tensor_tensor(out=obuf[:], in0=obuf[:], in1=m2[:].unsqueeze(2).to_broadcast([64, NLCH, 128]), op=ALU.mult), reads=['mobuf', 'mm2'], writes=['mobuf'])
                    S.op('dve', lambda e, hd=hd: e.tensor_tensor(out=obuf[:], in0=obuf[:], in1=mln[:, hd * 128:(hd + 1) * 128].unsqueeze(1).to_broadcast([64, NLCH, 128]), op=ALU.mult),
                         reads=['mobuf', 'mln'], writes=['mobuf'])
                    S.op('dve', lambda e: e.tensor_tensor(out=obuf[:], in0=obuf[:], in1=og[:], op=ALU.mult), reads=['mobuf', 'mog'], writes=['mobuf'])
                    transpose_chunks_to_mixT(obuf, ['mobuf'], 4 + hd)
                    S.barrier()
            S.barrier()

        if debug == 'mix':
            with ExitStack() as dd:
                mf = sb(dd, "mf", [128, NLAT])
                for h in range(8):
                    S.op('dve', lambda e, h=h: e.tensor_copy(out=mf[:], in_=mixT[:, h, :]), reads=K('mixT', h) + ['mf'], writes=['mf'])
                    S.dma('sp', lambda e, h=h: e.dma_start(out=dbg_d[h * 128:(h + 1) * 128, :], in_=mf[:]), reads=['mf'], writes=['dbg'])
                S.wait_all('sp', ['dbg']); S.barrier()
            scB.close(); scA.close()
            return nc
        prep_tables(512)
        S.barrier()
        scB.close()
        x1s = nc.dram_tensor("x1s", [NLAT, 1024], F32, kind="Internal").ap()

        def bcast_tile(dst, dkey, c0, dg):
            for ch in range(8):
                S.op('dve', lambda e, ch=ch: e.tensor_scalar_mul(out=dg[:], in0=ident[:], scalar1=modv[:, c0 + ch, 0:1]), reads=['ident', 'modv', 'dg'], writes=['dg'])
                bank = ch // 4
                S.op('pe', lambda e, ch=ch, bank=bank: e.matmul(PB[bank][:, (ch % 4) * 128:(ch % 4 + 1) * 128], lhsT=ones[:], rhs=dg[:], start=True, stop=True),
                     reads=['ones', 'dg'], writes=['pb%d' % bank], skip_self=True)
            S.op('act', lambda e: e.copy(out=dst[:, 0:512], in_=PB[0][:, :]), reads=['pb0'], writes=[dkey])
            S.op('act', lambda e: e.copy(out=dst[:, 512:1024], in_=PB[1][:, :]), reads=['pb1'], writes=[dkey])

        with ExitStack() as p3:
            bct = {}
            for nm in ("g1b", "ln1g", "ln1b"):
                bct[nm] = sb(p3, nm, [128, 1024])
            for nm, dd in (("ln1g", ln1g_d), ("ln1b", ln1b_d)):
                S.dma('sp', lambda e, nm=nm, dd=dd: e.dma_start(out=bct[nm][:], in_=dd[:, :]), writes=[nm])
            dg = sb(p3, "dg", [128, 128])
            bcast_tile(bct["g1b"], "g1b", 16, dg)
            wo_b = sb(p3, "wout", [128, 8, 1024], BF16); wos = sb(p3, "wos", [128, 1024])
            for kc in range(8):
                S.dma('sp', lambda e, kc=kc: e.dma_start(out=wos[:], in_=wout_d[:, kc, :]), writes=['wos'])
                S.op('pool', lambda e, kc=kc: e.tensor_copy(out=wo_b[:, kc, :], in_=wos[:]), reads=['wos'], writes=['wout'])
            xt = [sb(p3, "x3t%d" % i, [128, 1024]) for i in range(2)]
            t1 = [sb(p3, "t1_%d" % i, [128, 1024]) for i in range(2)]
            st = [sb(p3, "st3%d" % i, [128, 2, 6]) for i in range(2)]
            mv = [sb(p3, "mv3%d" % i, [128, 2]) for i in range(2)]
            rs = [sb(p3, "rs3%d" % i, [128, 1]) for i in range(2)]
            for t in range(16):
                b = t % 2
                S.dma('sp' if b == 0 else 'pool', lambda e, t=t, b=b: e.dma_start(out=xt[b][:], in_=xs[256 + t * 128:256 + (t + 1) * 128, :]), writes=['x3t%d' % b])
                for half in range(2):
                    bank = 2 * b + half
                    for kc in range(8):
                        S.op('pe', lambda e, kc=kc, t=t, half=half, bank=bank: e.matmul(PB[bank][:, :], lhsT=mixT[:, kc, t * 128:(t + 1) * 128], rhs=wo_b[:, kc, half * 512:(half + 1) * 512],
                                                                                      start=(kc == 0), stop=(kc == 7)),
                             reads=K('mixT', kc) + ['wout'], writes=['pb%d' % bank], skip_self=True)
                    S.op('dve', lambda e, b=b, half=half, bank=bank: e.tensor_tensor(out=t1[b][:, half * 512:(half + 1) * 512], in0=PB[bank][:, :], in1=bct["g1b"][:, half * 512:(half + 1) * 512], op=ALU.mult),
                         reads=['pb%d' % bank, 'g1b'], writes=['t1_%d' % b])
                S.op('dve', lambda e, b=b: e.scalar_tensor_tensor(out=t1[b][:], in0=xt[b][:], scalar=ALPHA, in1=t1[b][:], op0=ALU.mult, op1=ALU.add),
                     reads=['x3t%d' % b, 't1_%d' % b], writes=['t1_%d' % b])
                layer_norm_rows(st[b], mv[b][:], rs[b][:], t1[b][:], t1[b][:], 't1_%d' % b, 't1_%d' % b, 'p3%d' % b)
                S.op('dve', lambda e, b=b: e.tensor_tensor(out=t1[b][:], in0=t1[b][:], in1=bct["ln1g"][:], op=ALU.mult), reads=['t1_%d' % b, 'ln1g'], writes=['t1_%d' % b])
                S.op('dve', lambda e, b=b, t=t: e.tensor_tensor(out=t1[b][:], in0=t1[b][:], in1=bct["ln1b"][:], op=ALU.add), reads=['t1_%d' % b, 'ln1b'], writes=['t1_%d' % b])
                S.dma('sp', lambda e, t=t, b=b: e.dma_start(out=x1s[t * 128:(t + 1) * 128, :], in_=t1[b][:]), reads=['t1_%d' % b], writes=K('x1_', t))
                if debug == 'x1':
                    S.dma('sp', lambda e, t=t, b=b: e.dma_start(out=dbg_d[t * 128:(t + 1) * 128, :], in_=t1[b][:]), reads=['t1_%d' % b], writes=['dbg'])
            S.barrier()
        scA.close()

        with ExitStack() as p4:
            bct = {}
            for nm in ("g2b", "sc2b", "sh2b", "ln2g", "ln2b"):
                bct[nm] = sb(p4, nm, [128, 1024])
            for nm, dd in (("ln2g", ln2g_d), ("ln2b", ln2b_d)):
                S.dma('sp', lambda e, nm=nm, dd=dd: e.dma_start(out=bct[nm][:], in_=dd[:, :]), writes=[nm])
            dg = sb(p4, "dg4", [128, 128])
            bcast_tile(bct["g2b"], "g2b", 40, dg); bcast_tile(bct["sc2b"], "sc2b", 32, dg); bcast_tile(bct["sh2b"], "sh2b", 24, dg)
            x1t = [sb(p4, "x1t%d" % i, [128, 1024]) for i in range(2)]
            wqb = sb(p4, "wqb", [128, 8, 2048], BF16)
            keysT = sb(p4, "keysT", [128, 16, 128], BF16)
            with ExitStack() as tmp:
                stg = sb(tmp, "wq_stg", [128, 2048])
                for kc in range(8):
                    S.dma('sp', lambda e, kc=kc: e.dma_start(out=stg[:], in_=wq_d[:, kc, :]), writes=['wq_stg'])
                    S.op('pool', lambda e, kc=kc: e.tensor_copy(out=wqb[:, kc, :], in_=stg[:]), reads=['wq_stg'], writes=['wqb'])
                kst = sb(tmp, "kst", [128, 16, 128])
                S.dma('sp', lambda e: e.dma_start(out=kst[:], in_=keysT_d[:, :, :]), writes=['kst'])
                S.op('pool', lambda e: e.tensor_copy(out=keysT[:], in_=kst[:]), reads=['kst'], writes=['keysT'])
                S.barrier()
            NG = 8
            ub = [sb(p4, "ub%d" % i, [128, 1024], BF16) for i in range(NG)]
            vb = [sb(p4, "vb%d" % i, [128, 1024], BF16) for i in range(NG)]
            dgk = [sb(p4, "dgk%d" % i, [128, 128], BF16) for i in range(4)]
            h2 = sb(p4, "h2", [128, 1024]); h2b = [sb(p4, "h2b%d" % i, [128, 1024], BF16) for i in range(2)]
            h2T = sb(p4, "h2T", [128, 8, 128], BF16); qT = sb(p4, "qT", [128, 16, 128], BF16)
            sc = sb(p4, "sc", [128, 16, 128]); scw = sb(p4, "scw", [128, 16, 128])
            top = sb(p4, "top", [128, 16, 16]); topi = sb(p4, "topi", [128, 16, 16], U32); topf = sb(p4, "topf", [128, 16, 16])
            cand = sb(p4, "cand", [128, 8, 256]); candw = sb(p4, "candw", [128, 8, 256]); cidx = sb(p4, "cidx", [128, 8, 256])
            best = sb(p4, "best", [128, 8, 16]); idxf = sb(p4, "idxf", [128, 128])
            gate = [sb(p4, "gate%d" % i, [128, 128]) for i in range(2)]
            idxi = [sb(p4, "idxi%d" % i, [128, 128], I32) for i in range(2)]
            wgt = [sb(p4, "wgt%d" % i, [128, 128]) for i in range(2)]
            dots = sb(p4, "dots", [128, 128]); junk = sb(p4, "junk", [128, 1024], BF16); junk2 = sb(p4, "junk2", [128, 256])
            zs = sb(p4, "zs", [128, 8]); nmx = sb(p4, "nmx", [128, 8])
            st = sb(p4, "st4", [128, 2, 6]); mv = sb(p4, "mv4", [128, 2]); rs = sb(p4, "rs4", [128, 1])
            fin = sb(p4, "fin", [128, 1024]); yb = sb(p4, "yb", [128, 1024])
            NT4 = 16 if debug != 'x1' else 0

            def prologue(t):
                p = t % 2
                xb_ = x1t[p]; x1k = 'x1t%d' % p
                S.dma('sp', lambda e: e.dma_start(out=xb_[:], in_=x1s[t * 128:(t + 1) * 128, :]), reads=K('x1_', t), writes=[x1k])
                S.op('dve', lambda e: e.memset(idxf[:], 0.0), writes=['idxf'])
                S.op('dve', lambda e: e.memset(zs[:], 0.0), writes=['zs'])
                layer_norm_rows(st, mv[:], rs[:], xb_[:], h2[:], x1k, 'h2', 'p4')
                S.op('dve', lambda e: e.tensor_tensor(out=h2[:], in0=h2[:], in1=bct["sc2b"][:], op=ALU.mult), reads=['h2', 'sc2b'], writes=['h2'])
                S.op('dve', lambda e: e.tensor_tensor(out=h2[:], in0=h2[:], in1=bct["sh2b"][:], op=ALU.add), reads=['h2', 'sh2b'], writes=['h2'])
                S.op('act', lambda e: e.copy(out=h2b[p][:], in_=h2[:]), reads=['h2'], writes=['h2b%d' % p])
                for half in range(2):
                    for c4 in range(4):
                        ch = half * 4 + c4
                        S.op('pe', lambda e, ch=ch, c4=c4, half=half: e.transpose(PB[half][:, c4 * 128:(c4 + 1) * 128], h2[:, ch * 128:(ch + 1) * 128], ident[:]),
                             reads=['h2', 'ident'], writes=['pb%d' % half], skip_self=True)
                    S.op('act', lambda e, half=half: e.copy(out=h2T[:, half * 4:(half + 1) * 4, :], in_=PB[half][:, :].rearrange("p (j c) -> p j c", c=128)), reads=['pb%d' % half], writes=['h2T'])
                for g in range(4):
                    for c4 in range(4):
                        c = g * 4 + c4
                        for kc in range(8):
                            S.op('pe', lambda e, c=c, c4=c4, kc=kc, g=g: e.matmul(PB[2 + g][:, c4 * 128:(c4 + 1) * 128], lhsT=wqb[:, kc, c * 128:(c + 1) * 128], rhs=h2T[:, kc, :],
                                                                                start=(kc == 0), stop=(kc == 7)), reads=['wqb', 'h2T'], writes=['pb%d' % (2 + g)], skip_self=True)
                    S.op('act' if g % 2 == 0 else 'dve', lambda e, g=g: (e.copy if g % 2 == 0 else e.tensor_copy)(out=qT[:, g * 4:(g + 1) * 4, :], in_=PB[2 + g][:, :].rearrange("p (j c) -> p j c", c=128)),
                         reads=['pb%d' % (2 + g)], writes=['qT'])
                for g in range(4):
                    for c4 in range(4):
                        c = g * 4 + c4
                        S.op('pe', lambda e, c=c, c4=c4, g=g: e.matmul(PB[2 + g][:, c4 * 128:(c4 + 1) * 128], lhsT=qT[:, c, :], rhs=keysT[:, c, :], start=True, stop=True),
                             reads=['qT', 'keysT'], writes=['pb%d' % (2 + g)], skip_self=True)
                    S.op('act' if g % 2 == 0 else 'dve', lambda e, g=g: (e.copy if g % 2 == 0 else e.tensor_copy)(out=sc[:, g * 4:(g + 1) * 4, :], in_=PB[2 + g][:, :].rearrange("p (j c) -> p j c", c=128)),
                         reads=['pb%d' % (2 + g)], writes=['sc'])
                for c in range(16):
                    S.op('dve', lambda e, c=c: e.max(out=top[:, c, 0:8], in_=sc[:, c, :]), reads=['sc'], writes=['top'])
                    S.op('dve', lambda e, c=c: e.max_index(out=topi[:, c, 0:8], in_max=top[:, c, 0:8], in_values=sc[:, c, :]), reads=['sc', 'top'], writes=['topi'])
                    S.op('dve', lambda e, c=c: e.match_replace(out=scw[:, c, :], in_to_replace=top[:, c, 0:8], in_values=sc[:, c, :], imm_value=-1e30), reads=['sc', 'top'], writes=['scw'])
                    S.op('dve', lambda e, c=c: e.max(out=top[:, c, 8:16], in_=scw[:, c, :]), reads=['scw'], writes=['top'])
                    S.op('dve', lambda e, c=c: e.max_index(out=topi[:, c, 8:16], in_max=top[:, c, 8:16], in_values=scw[:, c, :]), reads=['scw', 'top'], writes=['topi'])
                S.op('dve', lambda e: e.tensor_copy(out=topf[:], in_=topi[:]), reads=['topi'], writes=['topf'])
                t4 = top[:].rearrange("p (h two) k -> p h two k", two=2); f4 = topf[:].rearrange("p (h two) k -> p h two k", two=2)
                c4v = cand[:].rearrange("p h (a b) -> p h a b", b=16); i4 = cidx[:].rearrange("p h (a b) -> p h a b", b=16)
                for h in range(8):
                    S.op('dve', lambda e, h=h: e.tensor_tensor(out=c4v[:, h, :, :], in0=t4[:, h, 0, :].unsqueeze(2).to_broadcast([128, 16, 16]), in1=t4[:, h, 1, :].unsqueeze(1).to_broadcast([128, 16, 16]), op=ALU.add),
                         reads=['top'], writes=['cand'])
                    S.op('dve', lambda e, h=h: e.scalar_tensor_tensor(out=i4[:, h, :, :], in0=f4[:, h, 0, :].unsqueeze(2).to_broadcast([128, 16, 16]), scalar=128.0, in1=f4[:, h, 1, :].unsqueeze(1).to_broadcast([128, 16, 16]),
                                                                    op0=ALU.mult, op1=ALU.add), reads=['topf'], writes=['cidx'])
                for h in range(8):
                    S.op('dve', lambda e, h=h: e.max(out=best[:, h, 0:8], in_=cand[:, h, :]), reads=['cand'], writes=['best'])
                    S.op('dve', lambda e, h=h: e.match_replace(out=candw[:, h, :], in_to_replace=best[:, h, 0:8], in_values=cand[:, h, :], imm_value=-1e30), reads=['cand', 'best'], writes=['candw'])
                    S.op('dve', lambda e, h=h: e.max(out=best[:, h, 8:16], in_=candw[:, h, :]), reads=['candw'], writes=['best'])
                for h in range(8):
                    for k in range(16):
                        S.op('dve', lambda e, h=h, k=k: e.scalar_tensor_tensor(out=junk2[:], in0=cand[:, h, :], scalar=best[:, h, k:k + 1], in1=cidx[:, h, :], op0=ALU.is_equal, op1=ALU.mult,
                                                                             accum_out=idxf[:, h * 16 + k:h * 16 + k + 1]), reads=['cand', 'best', 'cidx', 'junk2'], writes=['junk2', 'idxf'])
                S.op('dve', lambda e: e.tensor_scalar_min(out=idxf[:], in0=idxf[:], scalar1=16383.0), reads=['idxf'], writes=['idxf'])
                S.op('dve', lambda e: e.tensor_copy(out=idxi[p][:], in_=idxf[:]), reads=['idxf'], writes=['idxi%d' % p])
                S.op('dve', lambda e: e.tensor_scalar_mul(out=nmx[:], in0=best[:, :, 0], scalar1=-1.0), reads=['best'], writes=['nmx'])
                g3 = gate[p][:].rearrange("p (h k) -> p h k", k=16)
                for h in range(8):
                    S.op('act', lambda e, h=h: e.activation(out=g3[:, h, :], in_=best[:, h, :], func=AF.Exp, bias=nmx[:, h:h + 1], scale=1.0, accum_out=zs[:, h:h + 1]),
                         reads=['best', 'nmx'], writes=['gate%d' % p, 'zs'])
                S.op('dve', lambda e: e.reciprocal(out=zs[:], in_=zs[:]), reads=['zs'], writes=['zs'])
                S.op('dve', lambda e: e.tensor_tensor(out=g3, in0=g3, in1=zs[:].unsqueeze(2).to_broadcast([128, 8, 16]), op=ALU.mult), reads=['gate%d' % p, 'zs'], writes=['gate%d' % p])

            def u_side(t):
                p = t % 2
                S.op('dve', lambda e: e.memset(dots[:], 0.0), writes=['dots'])
                for k in range(128):
                    s_ = k % NG
                    S.dma('pool', lambda e, k=k, s_=s_: e.indirect_dma_start(out=ub[s_][:], out_offset=None, in_=ubf_d[:, :], in_offset=bass.IndirectOffsetOnAxis(ap=idxi[p][:, k:k + 1], axis=0)),
                          reads=['idxi%d' % p, 'tabs'], writes=['ub%d' % s_])
                    S.op('dve', lambda e, k=k, s_=s_: e.scalar_tensor_tensor(out=junk[:], in0=ub[s_][:], scalar=1.0, in1=h2b[p][:], op0=ALU.mult, op1=ALU.mult, accum_out=dots[:, k:k + 1]),
                         reads=['ub%d' % s_, 'h2b%d' % p, 'junk'], writes=['junk', 'dots'])
                S.op('act', lambda e: e.activation(out=wgt[p][:], in_=dots[:], func=AF.Gelu), reads=['dots'], writes=['wgt%d' % p])
                S.op('dve', lambda e: e.tensor_tensor(out=wgt[p][:], in0=wgt[p][:], in1=gate[p][:], op=ALU.mult), reads=['wgt%d' % p, 'gate%d' % p], writes=['wgt%d' % p])

            def v_side(t):
                p = t % 2
                for k in range(128):
                    s_ = k % NG; d4 = k % 4
                    S.dma('pool', lambda e, k=k, s_=s_: e.indirect_dma_start(out=vb[s_][:], out_offset=None, in_=vbf_d[:, :], in_offset=bass.IndirectOffsetOnAxis(ap=idxi[p][:, k:k + 1], axis=0)),
                          reads=['idxi%d' % p, 'tabs'], writes=['vb%d' % s_])
                    S.op('act', lambda e, k=k, d4=d4: e.activation(out=dgk[d4][:], in_=identb[:], func=AF.Identity, bias=0.0, scale=wgt[p][:, k:k + 1]),
                         reads=['identb', 'wgt%d' % p], writes=['dgk%d' % d4])
                    for half in range(2):
                        S.op('pe', lambda e, k=k, s_=s_, d4=d4, half=half: e.matmul(PB[6 + half][:, :], lhsT=dgk[d4][:], rhs=vb[s_][:, half * 512:(half + 1) * 512], start=(k == 0), stop=(k == 127)),
                             reads=['dgk%d' % d4, 'vb%d' % s_], writes=['pb%d' % (6 + half)], skip_self=True)
                xb_ = x1t[p]; x1k = 'x1t%d' % p
                for half in range(2):
                    S.op('dve', lambda e, half=half: e.tensor_tensor(out=yb[:, half * 512:(half + 1) * 512], in0=PB[6 + half][:, :], in1=bct["g2b"][:, half * 512:(half + 1) * 512], op=ALU.mult),
                         reads=['pb%d' % (6 + half), 'g2b', 'yb'], writes=['yb'])
                S.op('dve', lambda e: e.scalar_tensor_tensor(out=fin[:], in0=xb_[:], scalar=ALPHA, in1=yb[:], op0=ALU.mult, op1=ALU.add), reads=[x1k, 'yb', 'fin'], writes=['fin'])
                layer_norm_rows(st, mv[:], rs[:], fin[:], fin[:], 'fin', 'fin', 'p4')
                S.op('dve', lambda e: e.tensor_tensor(out=fin[:], in0=fin[:], in1=bct["ln2g"][:], op=ALU.mult), reads=['fin', 'ln2g'], writes=['fin'])
                S.op('dve', lambda e: e.tensor_tensor(out=fin[:], in0=fin[:], in1=bct["ln2b"][:], op=ALU.add), reads=['fin', 'ln2b'], writes=['fin'])
                S.dma('sp', lambda e: e.dma_start(out=out_d[t * 128:(t + 1) * 128, :], in_=fin[:]), reads=['fin'], writes=['out'])

            if NT4:
                prologue(0)
            for t in range(NT4):
                u_side(t)
                if t + 1 < NT4:
                    prologue(t + 1)
                v_side(t)
            S.wait_all('sp', ['out', 'dbg'])
            S.barrier()
    return nc


def _prep_shared(inp):
    f = np.float32
    sh = {}
    sh["w_mod"] = np.ascontiguousarray(inp["w_mod"][0].reshape(8, 128, 6144).transpose(1, 0, 2))
    sh["b_modT"] = np.ascontiguousarray(inp["b_mod"][0].reshape(48, 128).T)
    sh["w_in"] = np.ascontiguousarray(inp["w_in"][0].reshape(8, 128, 4624).transpose(1, 0, 2))
    sh["lgT"] = np.ascontiguousarray(inp["hg_lb_logits"].reshape(2, 2, 4, 128).transpose(3, 0, 1, 2))
    sh["hgn"] = np.ascontiguousarray(np.broadcast_to(inp["hg_norm_g"][0][None, :], (64, 512)))
    sh["mln"] = np.ascontiguousarray(np.broadcast_to(inp["ml_norm_g"][0][None, :], (64, 512)))
    sh["convw"] = np.ascontiguousarray(inp["ml_conv_w"][0].reshape(9, 8, 128).transpose(2, 0, 1))
    sh["convb"] = np.ascontiguousarray(inp["ml_conv_b"][0].reshape(8, 128).T)
    sh["gateb"] = np.ascontiguousarray(inp["ml_gate_b"][0].reshape(2, 8).T)
    sh["w_out"] = np.ascontiguousarray(inp["w_out"][0].reshape(8, 128, 1024).transpose(1, 0, 2))
    for nm, key in (("ln1g", "ln1_g"), ("ln1b", "ln1_b"), ("ln2g", "ln2_g"), ("ln2b", "ln2_b")):
        sh[nm] = np.ascontiguousarray(np.broadcast_to(inp[key][0][None, :], (128, 1024)))
    sh["wq"] = np.ascontiguousarray(inp["peer_wq"][0].reshape(8, 128, 2048).transpose(1, 0, 2))
    sh["keysT"] = np.ascontiguousarray(inp["peer_keys"][0].reshape(16, 128, 128).transpose(2, 0, 1))
    nexp = 128 if os.environ.get("KDEBUG") else 16384
    sh["pu"] = np.ascontiguousarray(inp["peer_u"][0][:nexp])
    sh["pv"] = np.ascontiguousarray(inp["peer_v"][0][:nexp])
    sh["ident"] = np.eye(128, dtype=f)
    m = np.zeros((64, 2, 64), f)
    s = np.arange(64)[:, None]; c = np.arange(64)[None, :]
    m[:, 0, :] = (s <= c); m[:, 1, :] = (s >= c)
    sh["masks"] = m
    rm = np.ones((128, 512), f); rm[:, ::64] = 0.0
    sh["rmask"] = rm
    sh["sel8"] = np.eye(8, dtype=f)
    dm = np.zeros((8, 2), f); dm[0:4, 0] = 1.0; dm[4:8, 1] = 1.0
    sh["dirm"] = dm
    return {k: np.asarray(v, dtype=f) for k, v in sh.items()}


def kernel(**inputs):
    inp = {k: np.asarray(v) for k, v in inputs.items()}
    debug = os.environ.get("KDEBUG") or None
    nc = build(debug)
    sh = _prep_shared(inp)
    in_maps = []
    for b in range(8):
        m = dict(sh)
        m["xs"] = np.ascontiguousarray(np.concatenate([inp["ctx"][b], inp["x"][b]], axis=0).astype(np.float32))
        m["cT"] = np.ascontiguousarray(np.stack([inp["c"][b], inp["c_ctx"]], axis=-1).reshape(8, 128, 2).transpose(1, 0, 2).astype(np.float32))
        in_maps.append(m)
    res = run_bass_kernel_spmd(nc, in_maps, core_ids=list(range(8)))
    key = "dbg" if debug else "out"
    return np.stack([np.asarray(r[key]) for r in res.results], axis=0).astype(np.float32)
```

```python
import os
import numpy as np
from contextlib import ExitStack
import concourse.bass as bass
import concourse.mybir as mybir
from concourse.bass_utils import run_bass_kernel_spmd

F32 = mybir.dt.float32; BF16 = mybir.dt.bfloat16; I32 = mybir.dt.int32; U32 = mybir.dt.uint32
AF = mybir.ActivationFunctionType; ALU = mybir.AluOpType; AX = mybir.AxisListType

NTOK = 2304; NLAT = 2048; NCH = 36; NLCH = 32
ALPHA = 2.0 ** 0.25
EPS = 1e-6


class Sched:
    NDMA = 32

    def __init__(self, nc, es):
        self.nc = nc
        self.engs = {'pe': nc.tensor, 'act': nc.scalar, 'dve': nc.vector, 'pool': nc.gpsimd, 'sp': nc.sync}
        self.sem = {k: es.enter_context(nc.semaphore("sem_" + k)) for k in self.engs}
        self.cnt = {k: 0 for k in self.engs}
        self.dsem = [es.enter_context(nc.semaphore("dsem%d" % i)) for i in range(self.NDMA)]
        self.dcnt = [0] * self.NDMA
        self.dnext = 0
        self.seen = {k: {} for k in self.engs}
        self.bufs = {}

    def _deps(self, reads, writes):
        deps = []
        for r in reads:
            b = self.bufs.get(r)
            if b and b['w'] is not None:
                deps.append(b['w'])
        for w in writes:
            b = self.bufs.get(w)
            if b:
                if b['w'] is not None:
                    deps.append(b['w'])
                deps.extend(b['r'])
        return deps

    def _wait(self, eng, deps, skip_self=False):
        best = {}
        for (sid, sem, val, owner) in deps:
            if skip_self and owner == eng:
                continue
            if best.get(sid, (None, 0))[1] < val:
                best[sid] = (sem, val)
        for sid, (sem, val) in best.items():
            if self.seen[eng].get(sid, 0) >= val:
                continue
            self.engs[eng].wait_ge(sem, val)
            self.seen[eng][sid] = val

    def _record(self, dep, reads, writes):
        for r in reads:
            b = self.bufs.setdefault(r, {'w': None, 'r': []})
            b['r'] = [d for d in b['r'] if d[0] != dep[0]] + [dep]
        for w in writes:
            self.bufs[w] = {'w': dep, 'r': []}

    @staticmethod
    def _split(keys):
        norm, ps = [], []
        for k in keys:
            if k.startswith('pb') and len(k) > 2 and k[2].isdigit():
                ps.append(k[:3])
            else:
                norm.append(k)
        return norm, ps

    def op(self, eng, fn, reads=(), writes=(), skip_self=False):
        reads, pr = self._split(reads)
        writes, pw = self._split(writes)
        banks = sorted(set(pr + pw))
        deps = self._deps(reads, writes)
        deps += [d for d in self._deps((), banks) if d[3] != eng]
        self._wait(eng, deps, skip_self)
        ins = fn(self.engs[eng])
        self.cnt[eng] += 1
        ins.then_inc(self.sem[eng], 1)
        self._record(('e_' + eng, self.sem[eng], self.cnt[eng], eng), reads, list(writes) + banks)
        return ins

    def dma(self, q, fn, reads=(), writes=()):
        deps = self._deps(reads, writes)
        j = self.dnext
        self.dnext = (self.dnext + 1) % self.NDMA
        if self.dcnt[j] > 0:
            deps = deps + [('d%d' % j, self.dsem[j], self.dcnt[j], 'dma')]
        self._wait(q, deps)
        ins = fn(self.engs[q])
        self.dcnt[j] += 16
        ins.then_inc(self.dsem[j], 16)
        self._record(('d%d' % j, self.dsem[j], self.dcnt[j], 'dma'), reads, writes)

    def wait_all(self, eng, keys):
        deps = []
        for k in keys:
            b = self.bufs.get(k)
            if b:
                if b['w'] is not None:
                    deps.append(b['w'])
                deps.extend(b['r'])
        self._wait(eng, deps)

    def barrier(self):
        for e in self.engs:
            deps = [('e_' + f, self.sem[f], self.cnt[f], f) for f in self.engs if f != e and self.cnt[f] > 0]
            deps += [('d%d' % j, self.dsem[j], self.dcnt[j], 'dma') for j in range(self.NDMA) if self.dcnt[j] > 0]
            self._wait(e, deps)


def K(name, a, b=None):
    if b is None:
        return ["%s%d" % (name, a)]
    return ["%s%d" % (name, i) for i in range(a, b)]


def build(debug=None):
    nc = bass.Bass("TRN2", target_bir_lowering=False)
    D = {}

    def din(name, shape, dt=F32):
        D[name] = nc.dram_tensor(name, shape, dt, kind="ExternalInput").ap()
        return D[name]

    xs = din("xs", [NTOK, 1024]); cT_d = din("cT", [128, 8, 2]); wmod_d = din("w_mod", [128, 8, 6144])
    bmod_d = din("b_modT", [128, 48]); win_d = din("w_in", [128, 8, 4624]); lg_d = din("lgT", [128, 2, 2, 4])
    hgn_d = din("hgn", [64, 512]); mln_d = din("mln", [64, 512]); convw_d = din("convw", [128, 9, 8])
    convb_d = din("convb", [128, 8]); gateb_d = din("gateb", [8, 2]); wout_d = din("w_out", [128, 8, 1024])
    ln1g_d = din("ln1g", [128, 1024]); ln1b_d = din("ln1b", [128, 1024]); ln2g_d = din("ln2g", [128, 1024])
    ln2b_d = din("ln2b", [128, 1024]); wq_d = din("wq", [128, 8, 2048]); keysT_d = din("keysT", [128, 16, 128])
    NEXP = 128 if debug else 16384
    pu_d = din("pu", [NEXP, 1024]); pv_d = din("pv", [NEXP, 1024])
    ident_d = din("ident", [128, 128]); masks_d = din("masks", [64, 2, 64]); rmask_d = din("rmask", [128, 512])
    sel8_d = din("sel8", [8, 8]); dirm_d = din("dirm", [8, 2])
    out_d = nc.dram_tensor("out", [NLAT, 1024], F32, kind="ExternalOutput").ap()
    dbg_d = None
    if debug:
        dbg_d = nc.dram_tensor("dbg", [NLAT, 1024] if debug != 'mix' else [1024, NLAT], F32, kind="ExternalOutput").ap()

    with ExitStack() as es:
        S = Sched(nc, es)

        uid = [0]

        def sb(st, name, shape, dt=F32):
            uid[0] += 1
            return st.enter_context(nc.sbuf_tensor("s%d_%s" % (uid[0], name), shape, dt))

        PB = [es.enter_context(nc.psum_tensor("pb%d" % i, [128, 512], F32)) for i in range(8)]

        ident = sb(es, "ident", [128, 128]); identb = sb(es, "identb", [128, 128], BF16)
        masks = sb(es, "masks", [64, 2, 64]); rmask = sb(es, "rmask", [128, 512])
        ones = sb(es, "ones", [128, 128]); epsc = sb(es, "epsc", [128, 1])
        modv = sb(es, "modv", [128, 48, 2])
        S.dma('sp', lambda e: e.dma_start(out=ident[:], in_=ident_d[:, :]), writes=['ident'])
        S.dma('sp', lambda e: e.dma_start(out=masks[:], in_=masks_d[:, :, :]), writes=['masks'])
        S.dma('sp', lambda e: e.dma_start(out=rmask[:], in_=rmask_d[:, :]), writes=['rmask'])
        S.op('dve', lambda e: e.tensor_copy(out=identb[:], in_=ident[:]), reads=['ident'], writes=['identb'])
        S.op('dve', lambda e: e.memset(ones[:], 1.0), writes=['ones'])
        S.op('dve', lambda e: e.memset(epsc[:], EPS), writes=['epsc'])

        with ExitStack() as p0:
            cT = sb(p0, "cT", [128, 8, 2]); scT = sb(p0, "scT", [128, 8, 2]); bmodT = sb(p0, "bmodT", [128, 48])
            wm = [sb(p0, "wm%d" % i, [128, 6144]) for i in range(2)]
            S.dma('sp', lambda e: e.dma_start(out=cT[:], in_=cT_d[:, :, :]), writes=['cT'])
            S.dma('sp', lambda e: e.dma_start(out=bmodT[:], in_=bmod_d[:, :]), writes=['bmodT'])
            S.op('act', lambda e: e.activation(out=scT[:], in_=cT[:], func=AF.Silu), reads=['cT'], writes=['scT'])
            for kc in range(8):
                w = wm[kc % 2]; wk = 'wm%d' % (kc % 2)
                S.dma('sp' if kc % 2 == 0 else 'pool', lambda e, w=w, kc=kc: e.dma_start(out=w[:], in_=wmod_d[:, kc, :]), writes=[wk])
                for j in range(48):
                    S.op('pe', lambda e, w=w, kc=kc, j=j: e.matmul(PB[kc // 4][:, (kc % 4) * 96 + 2 * j:(kc % 4) * 96 + 2 * j + 2], lhsT=w[:, j * 128:(j + 1) * 128], rhs=scT[:, kc, :],
                                                                 start=True, stop=True),
                         reads=[wk, 'scT'], writes=['pb%d' % (kc // 4)], skip_self=True)
            mflat = modv[:].rearrange("p j n -> p (j n)")
            S.op('dve', lambda e: e.tensor_tensor(out=modv[:], in0=PB[0][:, 0:96].rearrange("p (j n) -> p j n", n=2), in1=bmodT[:].unsqueeze(2).to_broadcast([128, 48, 2]), op=ALU.add),
                 reads=['pb0', 'bmodT'], writes=['modv'])
            for kc in range(1, 8):
                S.op('dve', lambda e, kc=kc: e.tensor_tensor(out=mflat, in0=mflat, in1=PB[kc // 4][:, (kc % 4) * 96:(kc % 4) * 96 + 96], op=ALU.add),
                     reads=['pb%d' % (kc // 4), 'modv'], writes=['modv'])
            S.op('dve', lambda e: e.tensor_scalar_add(out=modv[:, 8:16, :], in0=modv[:, 8:16, :], scalar1=1.0), reads=['modv'], writes=['modv'])
            S.op('dve', lambda e: e.tensor_scalar_add(out=modv[:, 32:40, :], in0=modv[:, 32:40, :], scalar1=1.0), reads=['modv'], writes=['modv'])
            S.barrier()
        if debug == 'p0':
            S.dma('sp', lambda e: e.dma_start(out=dbg_d[0:128, 0:96], in_=modv[:].rearrange("p a b -> p (a b)")), reads=['modv'], writes=['dbg'])
            S.wait_all('sp', ['dbg']); S.barrier()
            return nc

        scA = ExitStack(); scB = ExitStack()
        mixT = sb(scA, "mixT", [128, 8, NLAT], BF16)
        hT = sb(scB, "hT", [128, 8, NTOK], BF16)
        wstg = sb(scB, "wstg", [128, 8, 128])
        tstg = [sb(scB, "tstg%d" % i, [128, 512]) for i in range(2)]
        tbf = [sb(scB, "tbf%d" % i, [128, 512], BF16) for i in range(2)]
        ubf_d = nc.dram_tensor("ubf", [NEXP, 1024], BF16, kind="Internal").ap()
        vbf_d = nc.dram_tensor("vbf", [NEXP, 1024], BF16, kind="Internal").ap()
        prep_pos = [0]

        def prep_tables(npieces):
            if debug:
                return
            for _ in range(npieces):
                i = prep_pos[0]
                if i >= 512:
                    return
                prep_pos[0] += 1
                src, dst = (pu_d, ubf_d) if i < 256 else (pv_d, vbf_d)
                r = (i % 256) // 2; c = (i % 2) * 512; bb = i % 2
                S.dma('sp', lambda e, src=src, r=r, c=c, bb=bb: e.dma_start(out=tstg[bb][:], in_=src[r * 128:(r + 1) * 128, c:c + 512]), writes=['tstg%d' % bb])
                S.op('pool', lambda e, bb=bb: e.tensor_copy(out=tbf[bb][:], in_=tstg[bb][:]), reads=['tstg%d' % bb], writes=['tbf%d' % bb])
                S.dma('pool', lambda e, dst=dst, r=r, c=c, bb=bb: e.dma_start(out=dst[r * 128:(r + 1) * 128, c:c + 512], in_=tbf[bb][:]), reads=['tbf%d' % bb], writes=['tabs'])

        def layer_norm_rows(st_ap, mv_ap, rstd_ap, src, dst, skey, dkey, tag):
            S.op('dve', lambda e: e.bn_stats(out=st_ap[:, 0, :], in_=src[:, 0:512]), reads=[skey], writes=[tag + 'st'])
            S.op('dve', lambda e: e.bn_stats(out=st_ap[:, 1, :], in_=src[:, 512:1024]), reads=[skey], writes=[tag + 'st'])
            S.op('dve', lambda e: e.bn_aggr(out=mv_ap, in_=st_ap[:].rearrange("p a b -> p (a b)")), reads=[tag + 'st'], writes=[tag + 'mv'])
            S.op('act', lambda e: e.activation(out=rstd_ap, in_=mv_ap[:, 1:2], func=AF.Sqrt, bias=epsc[:, 0:1], scale=1.0),
                 reads=[tag + 'mv', 'epsc'], writes=[tag + 'rs'])
            S.op('dve', lambda e: e.reciprocal(out=rstd_ap, in_=rstd_ap), reads=[tag + 'rs'], writes=[tag + 'rs'])
            S.op('dve', lambda e: e.tensor_scalar(out=dst, in0=src, scalar1=mv_ap[:, 0:1], scalar2=rstd_ap, op0=ALU.subtract, op1=ALU.mult),
                 reads=[skey, tag + 'mv', tag + 'rs'], writes=[dkey])

        with ExitStack() as p1:
            xt = [sb(p1, "xt%d" % i, [128, 1024]) for i in range(2)]
            xn = [sb(p1, "xn%d" % i, [128, 1024]) for i in range(2)]
            st = [sb(p1, "st%d" % i, [128, 2, 6]) for i in range(2)]
            mv = [sb(p1, "mv%d" % i, [128, 2]) for i in range(2)]
            rs = [sb(p1, "rs%d" % i, [128, 1]) for i in range(2)]
            PSAP = bool(os.environ.get("KPSAP"))
            for t in range(18):
                b = t % 2
                n = 1 if t < 2 else 0
                S.dma('sp' if b == 0 else 'pool', lambda e, t=t, b=b: e.dma_start(out=xt[b][:], in_=xs[t * 128:(t + 1) * 128, :]), writes=['xt%d' % b])
                layer_norm_rows(st[b], mv[b][:], rs[b][:], xt[b][:], xn[b][:], 'xt%d' % b, 'xn%d' % b, 'p1%d' % b)
                for half in range(2):
                    bank = 2 * b + half
                    pk = 'pb%d' % bank
                    for c4 in range(4):
                        ch = half * 4 + c4
                        S.op('pe', lambda e, b=b, ch=ch, c4=c4, bank=bank: e.transpose(PB[bank][:, c4 * 128:(c4 + 1) * 128], xn[b][:, ch * 128:(ch + 1) * 128], ident[:]),
                             reads=['xn%d' % b, 'ident'], writes=[pk], skip_self=True)
                    if not PSAP:
                        S.op('act', lambda e, b=b, half=half, bank=bank: e.copy(out=xt[b][:, half * 512:(half + 1) * 512], in_=PB[bank][:, :]), reads=[pk, 'xt%d' % b], writes=['xt%d' % b])
                    for c4 in range(4):
                        ch = half * 4 + c4
                        dst = hT[:, ch, t * 128:(t + 1) * 128]
                        src = PB[bank][:, c4 * 128:(c4 + 1) * 128] if PSAP else xt[b][:, ch * 128:(ch + 1) * 128]
                        S.op('dve', lambda e, dst=dst, src=src, ch=ch, n=n: e.tensor_scalar(out=dst, in0=src, scalar1=modv[:, 8 + ch, n:n + 1], scalar2=modv[:, ch, n:n + 1],
                                                                                      op0=ALU.mult, op1=ALU.add),
                             reads=([pk] if PSAP else ['xt%d' % b]) + ['modv'], writes=K('hT', t))
            S.barrier()

        if debug == 'p1':
            with ExitStack() as dd:
                hf = sb(dd, "hf", [128, 1024])
                for t in range(16):
                    S.op('dve', lambda e, t=t: e.tensor_copy(out=hf[:].rearrange("p (a b) -> p a b", b=128), in_=hT[:, :, 256 + t * 128:256 + (t + 1) * 128]), reads=K('hT', t + 2) + ['hf'], writes=['hf'])
                    S.dma('sp', lambda e, t=t: e.dma_start(out=dbg_d[t * 128:(t + 1) * 128, :], in_=hf[:]), reads=['hf'], writes=['dbg'])
                S.wait_all('sp', ['dbg']); S.barrier()
            scB.close(); scA.close()
            return nc
        def load_w(stk, name, col0, ncols, src=None):
            src = win_d if src is None else src
            wb = sb(stk, name, [128, 8, ncols], BF16)
            S.dma('sp', lambda e: e.dma_start(out=wstg[:, :, 0:ncols], in_=src[:, :, col0:col0 + ncols]), writes=['wstg'])
            S.op('pool', lambda e: e.tensor_copy(out=wb[:], in_=wstg[:, :, 0:ncols]), reads=['wstg'], writes=[name])
            return wb

        BLKS = [(0, 512), (512, 512), (1024, 512), (1536, 512), (2048, 256)]

        def proj_fm_block(wb, wname, ncols, bank, t0, nt):
            for kc in range(8):
                S.op('pe', lambda e, kc=kc: e.matmul(PB[bank][0:ncols, 0:nt], lhsT=wb[:, kc, :], rhs=hT[:, kc, t0:t0 + nt], start=(kc == 0), stop=(kc == 7)),
                     reads=[wname] + K('hT', t0 // 128, (t0 + nt) // 128), writes=['pb%d' % bank], skip_self=True)

        def proj_tm_group(wb, wname, ncols, bank, c0, ncks):
            for j in range(ncks):
                n = c0 + j
                for kc in range(8):
                    S.op('pe', lambda e, kc=kc, j=j, n=n: e.matmul(PB[bank][0:64, j * ncols:(j + 1) * ncols], lhsT=hT[:, kc, n * 64:(n + 1) * 64], rhs=wb[:, kc, :],
                                                                   start=(kc == 0), stop=(kc == 7)),
                         reads=[wname] + K('hT', n // 2), writes=['pb%d' % bank], skip_self=True)

        S32 = sb(scB, "S32", [128, 2, 132]); Sbf = sb(scB, "Sbf", [128, 2, 132], BF16)
        stm = sb(scB, "stm", [64, 2, 64], BF16)
        usb = sb(scB, "usb", [128, 2, 132])

        def chunk_loop(dirn, KsT, QsT, Vi, QoT, Ku, Vu, dec_ap, W, evac, rk, tagk):
            order = list(range(36)) if dirn == 0 else [3, 2, 1, 0] + list(range(35, 3, -1))
            S.op('dve', lambda e: e.memset(S32[:, 0, :], 0.0), writes=['S32_0'])
            S.op('dve', lambda e: e.memset(Sbf[:, 0, :], 0.0), writes=['Sbf_0'])
            cur = 0
            for idx, n in enumerate(order):
                sl = slice(n * 64, (n + 1) * 64)
                s2 = idx % 2
                if n >= 4:
                    pst = PB[2 + s2][0:64, 0:64]; pstk = 'pb%d' % (2 + s2)
                    po = PB[4 + s2][0:64, 0:W]; pok = 'pb%d' % (4 + s2)
                    S.op('pe', lambda e, pst=pst, sl=sl: e.matmul(pst, lhsT=KsT[:, sl], rhs=QsT[:, sl], start=True, stop=True),
                         reads=rk, writes=[pstk], skip_self=True)
                    S.op('dve', lambda e, pst=pst, s2=s2: e.tensor_tensor(out=stm[:, s2, :], in0=pst, in1=masks[:, dirn, :], op=ALU.mult),
                         reads=[pstk, 'masks'], writes=['stm%d' % s2])
                    S.op('pe', lambda e, po=po, s2=s2, n=n: e.matmul(po, lhsT=stm[:, s2, :], rhs=Vi[:, n, 0:W], start=True, stop=False),
                         reads=['stm%d' % s2] + rk, writes=[pok], skip_self=True)
                    S.op('pe', lambda e, po=po, sl=sl, cur=cur: e.matmul(po, lhsT=QoT[:, sl], rhs=Sbf[:, cur, 0:W], start=False, stop=True),
                         reads=['Sbf_%d' % cur] + rk, writes=[pok], skip_self=True)
                    evac(n - 4, n, po, pok)
                if idx < 35:
                    pu = PB[6 + s2][:, 0:W]; puk = 'pb%d' % (6 + s2)
                    S.op('pe', lambda e, pu=pu, n=n: e.matmul(pu, lhsT=Ku[:, n, :], rhs=Vu[:, n, 0:W], start=True, stop=True),
                         reads=rk, writes=[puk], skip_self=True)
                    nxt = 1 - cur
                    S.op('act', lambda e, pu=pu, s2=s2: e.copy(out=usb[:, s2, 0:W], in_=pu), reads=[puk], writes=['usb%d' % s2])
                    S.op('dve', lambda e, n=n, cur=cur, nxt=nxt, s2=s2: e.scalar_tensor_tensor(out=Sbf[:, nxt, 0:W], in0=S32[:, cur, 0:W], scalar=dec_ap(n), in1=usb[:, s2, 0:W],
                                                                                            op0=ALU.mult, op1=ALU.add),
                         reads=['S32_%d' % cur, 'usb%d' % s2, tagk], writes=['Sbf_%d' % nxt])
                    S.op('dve', lambda e, n=n, cur=cur, nxt=nxt, s2=s2: e.scalar_tensor_tensor(out=S32[:, nxt, 0:W], in0=S32[:, cur, 0:W], scalar=dec_ap(n), in1=usb[:, s2, 0:W],
                                                                                            op0=ALU.mult, op1=ALU.add),
                         reads=['S32_%d' % cur, 'usb%d' % s2, tagk], writes=['S32_%d' % nxt])
                    cur = nxt

        def transpose_chunks_to_mixT(src, skey, head):
            for g in range(4):
                bank = g % 2
                for j in range(8):
                    n = g * 8 + j
                    S.op('pe', lambda e, n=n, j=j, bank=bank: e.transpose(PB[bank][:, j * 64:(j + 1) * 64], src[:, n, :], ident[0:64, 0:64]),
                         reads=list(skey) + ['ident'], writes=['pb%d' % bank], skip_self=True)
                S.op('act', lambda e, g=g, bank=bank: e.copy(out=mixT[:, head, g * 512:(g + 1) * 512], in_=PB[bank][:, :]),
                     reads=['pb%d' % bank], writes=K('mixT', head))

        with ExitStack() as g0:
            lgT = sb(g0, "lgT", [128, 2, 2, 4]); lbT = sb(g0, "lbT", [128, 2, 4]); omlT = sb(g0, "omlT", [128, 2, 4]); nomlT = sb(g0, "nomlT", [128, 2, 4])
            hgn = sb(g0, "hgn", [64, 512])
            S.dma('sp', lambda e: e.dma_start(out=lgT[:], in_=lg_d[:, :, :, :]), writes=['lgT'])
            S.dma('sp', lambda e: e.dma_start(out=hgn[:], in_=hgn_d[:, :]), writes=['hgn'])
            S.op('dve', lambda e: e.tensor_tensor(out=lbT[:], in0=lgT[:, :, 0, :], in1=lgT[:, :, 1, :], op=ALU.subtract), reads=['lgT'], writes=['lbT'])
            S.op('act', lambda e: e.activation(out=lbT[:], in_=lbT[:], func=AF.Sigmoid), reads=['lbT'], writes=['lbT'])
            S.op('dve', lambda e: e.tensor_scalar(out=omlT[:], in0=lbT[:], scalar1=-1.0, scalar2=1.0, op0=ALU.mult, op1=ALU.add), reads=['lbT'], writes=['omlT'])
            S.op('dve', lambda e: e.tensor_scalar_mul(out=nomlT[:], in0=omlT[:], scalar1=-1.0), reads=['omlT'], writes=['nomlT'])
            QsT = [sb(g0, "gQsT%d" % d, [128, NTOK], BF16) for d in range(2)]
            KsT = [sb(g0, "gKsT%d" % d, [128, NTOK], BF16) for d in range(2)]
            QoT = [sb(g0, "gQoT%d" % d, [128, NTOK], BF16) for d in range(2)]
            Ku = [sb(g0, "gKu%d" % d, [64, NCH, 128], BF16) for d in range(2)]
            dec = sb(g0, "gdec", [128, 2, NCH])
            vtm = sb(g0, "gv", [64, NCH, 128], BF16); gs = sb(g0, "ggs", [64, NLCH, 128], BF16); oacc = sb(g0, "goacc", [64, NLCH, 128])
            qf = sb(g0, "gqf", [128, 512]); sg = sb(g0, "gsg", [128, 512]); lf = sb(g0, "glf", [128, 512]); key = sb(g0, "gkey", [128, 512])
            Bc = sb(g0, "gB", [128, 512]); T1 = sb(g0, "gT1", [128, 512]); E1 = sb(g0, "gE1", [128, 512]); khT = sb(g0, "gkhT", [128, 512], BF16)
            sq = sb(g0, "gsq", [64, NLCH, 128], BF16); ss = sb(g0, "gss", [64, NLCH])
            for hd in range(4):
                with ExitStack() as hs:
                    wq_ = load_w(hs, "gwq", 0 + hd * 128, 128); wi_ = load_w(hs, "gwi", 512 + hd * 128, 128); wg_ = load_w(hs, "gwg", 1024 + hd * 128, 128)
                    wf = [load_w(hs, "gwf0", 1536 + hd * 128, 128), load_w(hs, "gwf1", 2048 + hd * 128, 128)]
                    prep_tables(64)
                    for g in range(9):
                        bk = 3 + g % 2
                        proj_tm_group(wi_, "gwi", 128, bk, g * 4, 4)
                        S.op('act', lambda e, g=g, bk=bk: e.copy(out=vtm[:, g * 4:(g + 1) * 4, :], in_=PB[bk][0:64, :].rearrange("p (j c) -> p j c", c=128)),
                             reads=['pb%d' % bk], writes=['gv'])
                    for g in range(8):
                        bk = 3 + (g + 1) % 2
                        proj_tm_group(wg_, "gwg", 128, bk, 4 + g * 4, 4)
                        S.op('act', lambda e, g=g, bk=bk: e.activation(out=gs[:, g * 4:(g + 1) * 4, :], in_=PB[bk][0:64, :].rearrange("p (j c) -> p j c", c=128), func=AF.Silu),
                             reads=['pb%d' % bk], writes=['ggs'])
                    for (t0, nt) in BLKS:
                        nck = nt // 64; c0 = t0 // 64
                        proj_fm_block(wq_, "gwq", 128, 0, t0, nt)
                        S.op('act', lambda e, nt=nt: e.copy(out=qf[:, 0:nt], in_=PB[0][:, 0:nt]), reads=['pb0'], writes=['gqf'])
                        for d in range(2):
                            proj_fm_block(wf[d], "gwf%d" % d, 128, 1 + d, t0, nt)
                            col = d * 4 + hd
                            lbp = lbT[:, d, hd:hd + 1]; omp = omlT[:, d, hd:hd + 1]; nomp = nomlT[:, d, hd:hd + 1]
                            S.op('act', lambda e, d=d, nt=nt: e.activation(out=sg[:, 0:nt], in_=PB[1 + d][:, 0:nt], func=AF.Sigmoid), reads=['pb%d' % (1 + d)], writes=['gsg'])
                            S.op('act', lambda e, nt=nt, lbp=lbp, omp=omp: e.activation(out=lf[:, 0:nt], in_=sg[:, 0:nt], func=AF.Ln, bias=lbp, scale=omp),
                                 reads=['gsg', 'lbT', 'omlT'], writes=['glf'])
                            S.op('dve', lambda e, nt=nt, nomp=nomp, omp=omp: e.tensor_scalar(out=key[:, 0:nt], in0=sg[:, 0:nt], scalar1=nomp, scalar2=omp, op0=ALU.mult, op1=ALU.add),
                                 reads=['gsg', 'omlT', 'nomlT'], writes=['gkey'])
                            S.op('dve', lambda e, nt=nt: e.tensor_tensor_scan(out=Bc[:, 0:nt], data0=rmask[:, 0:nt], data1=lf[:, 0:nt], initial=0.0, op0=ALU.mult, op1=ALU.add),
                                 reads=['glf', 'rmask'], writes=['gB'])
                            B3 = Bc[:, 0:nt].rearrange("p (n c) -> p n c", c=64)
                            T3 = T1[:, 0:nt].rearrange("p (n c) -> p n c", c=64)
                            if d == 1:
                                S.op('dve', lambda e, B3=B3, T3=T3, nck=nck: e.tensor_tensor(out=T3, in0=B3[:, :, 63:64].to_broadcast([128, nck, 64]), in1=B3, op=ALU.subtract),
                                     reads=['gB'], writes=['gT1'])
                                S.op('dve', lambda e, nt=nt: e.tensor_tensor(out=Bc[:, 0:nt], in0=T1[:, 0:nt], in1=lf[:, 0:nt], op=ALU.add), reads=['gT1', 'glf'], writes=['gB'])
                            li = 63 if d == 0 else 0
                            S.op('act', lambda e, B3=B3, li=li, d=d, c0=c0, nck=nck: e.activation(out=dec[:, d, c0:c0 + nck], in_=B3[:, :, li], func=AF.Exp), reads=['gB'], writes=['gdec'])
                            S.op('dve', lambda e, B3=B3, T3=T3, nck=nck: e.tensor_tensor(out=T3, in0=B3, in1=B3[:, :, 32:33].to_broadcast([128, nck, 64]), op=ALU.subtract),
                                 reads=['gB'], writes=['gT1'])
                            S.op('act', lambda e, nt=nt: e.activation(out=E1[:, 0:nt], in_=T1[:, 0:nt], func=AF.Exp), reads=['gT1'], writes=['gE1'])
                            S.op('dve', lambda e, nt=nt, t0=t0, d=d: e.tensor_tensor(out=QsT[d][:, t0:t0 + nt], in0=qf[:, 0:nt], in1=E1[:, 0:nt], op=ALU.mult),
                                 reads=['gqf', 'gE1'], writes=['gQsT%d' % d])
                            S.op('act', lambda e, nt=nt: e.activation(out=E1[:, 0:nt], in_=T1[:, 0:nt], func=AF.Exp, scale=-1.0), reads=['gT1'], writes=['gE1'])
                            S.op('dve', lambda e, nt=nt, t0=t0, d=d: e.tensor_tensor(out=KsT[d][:, t0:t0 + nt], in0=key[:, 0:nt], in1=E1[:, 0:nt], op=ALU.mult),
                                 reads=['gkey', 'gE1'], writes=['gKsT%d' % d])
                            S.op('act', lambda e, nt=nt: e.activation(out=E1[:, 0:nt], in_=Bc[:, 0:nt], func=AF.Exp), reads=['gB'], writes=['gE1'])
                            S.op('dve', lambda e, nt=nt, t0=t0, d=d: e.tensor_tensor(out=QoT[d][:, t0:t0 + nt], in0=qf[:, 0:nt], in1=E1[:, 0:nt], op=ALU.mult),
                                 reads=['gqf', 'gE1'], writes=['gQoT%d' % d])
                            S.op('dve', lambda e, B3=B3, T3=T3, nck=nck, li=li: e.tensor_tensor(out=T3, in0=B3[:, :, li:li + 1].to_broadcast([128, nck, 64]), in1=B3, op=ALU.subtract),
                                 reads=['gB'], writes=['gT1'])
                            S.op('act', lambda e, nt=nt: e.activation(out=E1[:, 0:nt], in_=T1[:, 0:nt], func=AF.Exp), reads=['gT1'], writes=['gE1'])
                            S.op('dve', lambda e, nt=nt: e.tensor_tensor(out=khT[:, 0:nt], in0=key[:, 0:nt], in1=E1[:, 0:nt], op=ALU.mult), reads=['gkey', 'gE1'], writes=['gkhT'])
                            pbb = PB[3][:].bitcast(BF16)
                            for j in range(nck):
                                S.op('pe', lambda e, j=j: e.transpose(pbb[0:64, j * 128:(j + 1) * 128], khT[:, j * 64:(j + 1) * 64], identb[:]),
                                     reads=['gkhT', 'identb'], writes=['pb3'], skip_self=True)
                            S.op('act', lambda e, d=d, c0=c0, nck=nck: e.copy(out=Ku[d][:, c0:c0 + nck, :], in_=pbb[0:64, 0:nck * 128].rearrange("p (j c) -> p j c", c=128)),
                                 reads=['pb3'], writes=['gKu%d' % d])
                    for d in range(2):
                        def evac(nl, n, po, pok, d=d):
                            if d == 0:
                                S.op('act', lambda e: e.copy(out=oacc[:, nl, :], in_=po), reads=[pok], writes=K('goacc', nl))
                            else:
                                S.op('dve', lambda e: e.tensor_tensor(out=oacc[:, nl, :], in0=po, in1=oacc[:, nl, :], op=ALU.add), reads=[pok] + K('goacc', nl), writes=K('goacc', nl))
                        chunk_loop(d, KsT[d], QsT[d], vtm, QoT[d], Ku[d], vtm, lambda n, d=d: dec[:, d, n:n + 1], 128, evac,
                                   ['gQsT%d' % d, 'gKsT%d' % d, 'gQoT%d' % d, 'gKu%d' % d, 'gv'], 'gdec')
                    allo = K('goacc', 0, NLCH)
                    S.op('dve', lambda e: e.tensor_tensor(out=sq[:], in0=oacc[:], in1=oacc[:], op=ALU.mult), reads=allo, writes=['gsq'])
                    S.op('dve', lambda e: e.tensor_reduce(out=ss[:], in_=sq[:], axis=AX.X, op=ALU.add), reads=['gsq'], writes=['gss'])
                    S.op('act', lambda e: e.activation(out=ss[:], in_=ss[:], func=AF.Sqrt, bias=epsc[0:64, 0:1], scale=1.0 / 128.0), reads=['gss', 'epsc'], writes=['gss'])
                    S.op('dve', lambda e: e.reciprocal(out=ss[:], in_=ss[:]), reads=['gss'], writes=['gss'])
                    S.op('dve', lambda e: e.tensor_tensor(out=oacc[:], in0=oacc[:], in1=ss[:].unsqueeze(2).to_broadcast([64, NLCH, 128]), op=ALU.mult),
                         reads=allo + ['gss'], writes=allo)
                    S.op('dve', lambda e, hd=hd: e.tensor_tensor(out=oacc[:], in0=oacc[:], in1=hgn[:, hd * 128:(hd + 1) * 128].unsqueeze(1).to_broadcast([64, NLCH, 128]), op=ALU.mult),
                         reads=allo + ['hgn'], writes=allo)
                    S.op('dve', lambda e: e.tensor_tensor(out=oacc[:], in0=oacc[:], in1=gs[:], op=ALU.mult), reads=allo + ['ggs'], writes=allo)
                    transpose_chunks_to_mixT(oacc, allo, hd)
                    S.barrier()
            S.barrier()

        with ExitStack() as m0:
            mln = sb(m0, "mln", [64, 512]); convw = sb(m0, "convw", [128, 9, 8]); convb = sb(m0, "convb", [128, 8])
            gateb = sb(m0, "gateb", [8, 2]); sel8 = sb(m0, "sel8", [8, 8]); dirm = sb(m0, "dirm", [8, 2])
            S.dma('sp', lambda e: e.dma_start(out=mln[:], in_=mln_d[:, :]), writes=['mln'])
            S.dma('sp', lambda e: e.dma_start(out=convw[:], in_=convw_d[:, :, :]), writes=['convw'])
            S.dma('sp', lambda e: e.dma_start(out=convb[:], in_=convb_d[:, :]), writes=['convb'])
            S.dma('sp', lambda e: e.dma_start(out=gateb[:], in_=gateb_d[:, :]), writes=['gateb'])
            S.dma('sp', lambda e: e.dma_start(out=sel8[:], in_=sel8_d[:, :]), writes=['sel8'])
            S.dma('sp', lambda e: e.dma_start(out=dirm[:], in_=dirm_d[:, :]), writes=['dirm'])
            RUU = sb(m0, "RUU", [64, NCH, 24]); dchunk = sb(m0, "dchunk", [128, 8, NCH])
            with ExitStack() as gp:
                wgi = load_w(gp, "mwgi", 4608, 8); wgf = load_w(gp, "mwgf", 4616, 8)
                LI = sb(gp, "LI", [8, NTOK]); LF = sb(gp, "LF", [8, NTOK]); Af = sb(gp, "Af", [8, NTOK]); Ab = sb(gp, "Ab", [8, NTOK]); Aa = sb(gp, "Aa", [8, NTOK])
                R = [sb(gp, "Rr%d" % i, [8, NTOK]) for i in range(3)]
                bd = sb(gp, "bd", [8, 8, NCH])
                for (t0, nt) in BLKS:
                    proj_fm_block(wgi, "mwgi", 8, 0, t0, nt)
                    S.op('act', lambda e, t0=t0, nt=nt: e.copy(out=LI[:, t0:t0 + nt], in_=PB[0][0:8, 0:nt]), reads=['pb0'], writes=['LI'])
                    proj_fm_block(wgf, "mwgf", 8, 1, t0, nt)
                    S.op('act', lambda e, t0=t0, nt=nt: e.copy(out=LF[:, t0:t0 + nt], in_=PB[1][0:8, 0:nt]), reads=['pb1'], writes=['LF'])
                S.op('dve', lambda e: e.tensor_scalar_add(out=LI[:], in0=LI[:], scalar1=gateb[:, 0:1]), reads=['LI', 'gateb'], writes=['LI'])
                S.op('act', lambda e: e.activation(out=LF[:], in_=LF[:], func=AF.Sigmoid, bias=gateb[:, 1:2], scale=1.0), reads=['LF', 'gateb'], writes=['LF'])
                S.op('act', lambda e: e.activation(out=LF[:], in_=LF[:], func=AF.Ln), reads=['LF'], writes=['LF'])
                for (t0, nt) in BLKS:
                    S.op('dve', lambda e, t0=t0, nt=nt: e.tensor_tensor_scan(out=Af[:, t0:t0 + nt], data0=rmask[0:8, 0:nt], data1=LF[:, t0:t0 + nt], initial=0.0, op0=ALU.mult, op1=ALU.add),
                         reads=['LF', 'rmask'], writes=['Af'])
                A3 = Af[:].rearrange("p (n c) -> p n c", c=64)
                tot = A3[:, :, 63:64]
                S.op('dve', lambda e: e.tensor_tensor(out=Ab[:].rearrange("p (n c) -> p n c", c=64), in0=tot.to_broadcast([8, NCH, 64]), in1=A3, op=ALU.subtract), reads=['Af'], writes=['Ab'])
                S.op('dve', lambda e: e.tensor_tensor(out=Ab[:], in0=Ab[:], in1=LF[:], op=ALU.add), reads=['Ab', 'LF'], writes=['Ab'])
                S.op('dve', lambda e: e.tensor_scalar_mul(out=Aa[:], in0=Af[:], scalar1=dirm[:, 0:1]), reads=['Af', 'dirm'], writes=['Aa'])
                S.op('dve', lambda e: e.scalar_tensor_tensor(out=Aa[:], in0=Ab[:], scalar=dirm[:, 1:2], in1=Aa[:], op0=ALU.mult, op1=ALU.add), reads=['Ab', 'dirm', 'Aa'], writes=['Aa'])
                S.op('act', lambda e: e.activation(out=R[0][:], in_=Aa[:], func=AF.Exp), reads=['Aa'], writes=['Rr0'])
                S.op('dve', lambda e: e.tensor_tensor(out=Ab[:], in0=LI[:], in1=Aa[:], op=ALU.subtract), reads=['LI', 'Aa', 'Ab'], writes=['Ab'])
                S.op('act', lambda e: e.activation(out=R[1][:], in_=Ab[:], func=AF.Exp), reads=['Ab'], writes=['Rr1'])
                S.op('dve', lambda e: e.tensor_tensor(out=Aa[:].rearrange("p (n c) -> p n c", c=64), in0=Ab[:].rearrange("p (n c) -> p n c", c=64), in1=tot.to_broadcast([8, NCH, 64]), op=ALU.add),
                     reads=['Ab', 'Af', 'Aa', 'Rr0'], writes=['Aa'])
                S.op('act', lambda e: e.activation(out=R[2][:], in_=Aa[:], func=AF.Exp), reads=['Aa'], writes=['Rr2'])
                for half in range(2):
                    for j in range(18):
                        n = half * 18 + j
                        for q in range(3):
                            S.op('pe', lambda e, n=n, j=j, q=q: e.transpose(PB[2][0:64, j * 24 + q * 8:j * 24 + q * 8 + 8], R[q][:, n * 64:(n + 1) * 64], ident[0:8, 0:8]),
                                 reads=['Rr%d' % q, 'ident'], writes=['pb2'], skip_self=True)
                    S.op('act', lambda e, half=half: e.copy(out=RUU[:, half * 18:(half + 1) * 18, :], in_=PB[2][0:64, 0:432].rearrange("p (j c) -> p j c", c=24)),
                         reads=['pb2'], writes=['RUU'])
                S.op('dve', lambda e: e.tensor_tensor(out=bd[:], in0=tot.rearrange("p n c -> p c n").to_broadcast([8, 8, NCH]), in1=sel8[:].unsqueeze(2).to_broadcast([8, 8, NCH]), op=ALU.mult),
                     reads=['Af', 'sel8'], writes=['bd'])
                S.op('pe', lambda e: e.matmul(PB[3][:, 0:288], lhsT=ones[0:8, :], rhs=bd[:].rearrange("p a n -> p (a n)"), start=True, stop=True), reads=['ones', 'bd'], writes=['pb3'], skip_self=True)
                S.op('act', lambda e: e.activation(out=dchunk[:].rearrange("p a n -> p (a n)"), in_=PB[3][:, 0:288], func=AF.Exp), reads=['pb3'], writes=['dchunk'])
                S.barrier()
            qc = sb(m0, "mqc", [128, NTOK], BF16); kc_ = sb(m0, "mkc", [128, NTOK], BF16); kTM = sb(m0, "mkTM", [64, NCH, 128], BF16)
            raw = sb(m0, "mraw", [128, NTOK]); acc = sb(m0, "macc", [128, NTOK])
            vext = sb(m0, "mvext", [64, NCH, 132], BF16); vi = sb(m0, "mvi", [64, NCH, 132], BF16); vu = sb(m0, "mvu", [64, NCH, 132], BF16)
            og = sb(m0, "mog", [64, NLCH, 128], BF16); oext = sb(m0, "moext", [64, NLCH, 132]); obuf = sb(m0, "mobuf", [64, NLCH, 128])
            den = sb(m0, "mden", [64, NLCH]); mu = sb(m0, "mmu", [64, NLCH]); m2 = sb(m0, "mm2", [64, NLCH])
            S.op('dve', lambda e: e.memset(vext[:], 1.0), writes=['mvext'])
            for hd in range(4):
                with ExitStack() as hs:
                    wq_ = load_w(hs, "mwq", 2560 + hd * 128, 128); wk_ = load_w(hs, "mwk", 3072 + hd * 128, 128)
                    wv_ = load_w(hs, "mwv", 3584 + hd * 128, 128); wo_ = load_w(hs, "mwo", 4096 + hd * 128, 128)
                    prep_tables(64)
                    for g in range(9):
                        bk = 3 + g % 2
                        proj_tm_group(wv_, "mwv", 128, bk, g * 4, 4)
                        S.op('act', lambda e, g=g, bk=bk: e.copy(out=vext[:, g * 4:(g + 1) * 4, 0:128], in_=PB[bk][0:64, :].rearrange("p (j c) -> p j c", c=128)),
                             reads=['pb%d' % bk], writes=['mvext'])
                    for g in range(8):
                        bk = 3 + (g + 1) % 2
                        proj_tm_group(wo_, "mwo", 128, bk, 4 + g * 4, 4)
                        S.op('act', lambda e, g=g, bk=bk: e.activation(out=og[:, g * 4:(g + 1) * 4, :], in_=PB[bk][0:64, :].rearrange("p (j c) -> p j c", c=128), func=AF.Sigmoid),
                             reads=['pb%d' % bk], writes=['mog'])
                    for qi, (wb, wn, dstb) in enumerate(((wq_, "mwq", qc), (wk_, "mwk", kc_))):
                        chn = qi * 4 + hd
                        for bi, (t0, nt) in enumerate(BLKS):
                            proj_fm_block(wb, wn, 128, bi % 2, t0, nt)
                            S.op('act', lambda e, t0=t0, nt=nt, bi=bi: e.copy(out=raw[:, t0:t0 + nt], in_=PB[bi % 2][:, 0:nt]), reads=['pb%d' % (bi % 2)], writes=['mraw'])
                        S.op('dve', lambda e, chn=chn: e.tensor_scalar(out=acc[:, 0:256], in0=raw[:, 0:256], scalar1=convw[:, 4, chn:chn + 1], scalar2=convb[:, chn:chn + 1], op0=ALU.mult, op1=ALU.add),
                             reads=['mraw', 'convw', 'convb'], writes=['macc'])
                        S.op('dve', lambda e, chn=chn: e.scalar_tensor_tensor(out=acc[:, 1:256], in0=raw[:, 0:255], scalar=convw[:, 3, chn:chn + 1], in1=acc[:, 1:256], op0=ALU.mult, op1=ALU.add),
                             reads=['mraw', 'convw', 'macc'], writes=['macc'])
                        S.op('dve', lambda e, chn=chn: e.scalar_tensor_tensor(out=acc[:, 0:255], in0=raw[:, 1:256], scalar=convw[:, 5, chn:chn + 1], in1=acc[:, 0:255], op0=ALU.mult, op1=ALU.add),
                             reads=['mraw', 'convw', 'macc'], writes=['macc'])
                        X = raw[:, 256:NTOK].rearrange("p (r c) -> p r c", c=64); Y = acc[:, 256:NTOK].rearrange("p (r c) -> p r c", c=64)
                        S.op('dve', lambda e, chn=chn: e.tensor_scalar(out=acc[:, 256:NTOK], in0=raw[:, 256:NTOK], scalar1=convw[:, 4, chn:chn + 1], scalar2=convb[:, chn:chn + 1], op0=ALU.mult, op1=ALU.add),
                             reads=['mraw', 'convw', 'convb', 'macc'], writes=['macc'])
                        for ky in range(3):
                            for kx in range(3):
                                if ky == 1 and kx == 1:
                                    continue
                                dy = ky - 1; dx = kx - 1
                                r0 = max(0, -dy); r1 = 32 - max(0, dy); c0 = max(0, -dx); c1 = 64 - max(0, dx)
                                S.op('dve', lambda e, chn=chn, ky=ky, kx=kx, r0=r0, r1=r1, c0=c0, c1=c1, dy=dy, dx=dx: e.scalar_tensor_tensor(
                                    out=Y[:, r0:r1, c0:c1], in0=X[:, r0 + dy:r1 + dy, c0 + dx:c1 + dx], scalar=convw[:, ky * 3 + kx, chn:chn + 1], in1=Y[:, r0:r1, c0:c1],
                                    op0=ALU.mult, op1=ALU.add), reads=['mraw', 'convw', 'macc'], writes=['macc'])
                        S.op('act', lambda e: e.activation(out=acc[:], in_=acc[:], func=AF.Silu), reads=['macc'], writes=['macc'])
                        if qi == 0:
                            S.op('dve', lambda e: e.tensor_copy(out=qc[:], in_=acc[:]), reads=['macc'], writes=['mqc'])
                        else:
                            S.op('dve', lambda e: e.tensor_scalar_mul(out=kc_[:], in0=acc[:], scalar1=128.0 ** -0.5), reads=['macc'], writes=['mkc'])
                    pbb = PB[3][:].bitcast(BF16)
                    for g in range(5):
                        nck = 8 if g < 4 else 4
                        for j in range(nck):
                            n = g * 8 + j
                            S.op('pe', lambda e, j=j, n=n: e.transpose(pbb[0:64, j * 128:(j + 1) * 128], kc_[:, n * 64:(n + 1) * 64], identb[:]),
                                 reads=['mkc', 'identb'], writes=['pb3'], skip_self=True)
                        S.op('act', lambda e, g=g, nck=nck: e.copy(out=kTM[:, g * 8:g * 8 + nck, :], in_=pbb[0:64, 0:nck * 128].rearrange("p (j c) -> p j c", c=128)),
                             reads=['pb3'], writes=['mkTM'])
                    for d in range(2):
                        row = d * 4 + hd
                        S.op('dve', lambda e, row=row: e.tensor_tensor(out=vi[:], in0=vext[:], in1=RUU[:, :, 8 + row:9 + row].to_broadcast([64, NCH, 132]), op=ALU.mult),
                             reads=['mvext', 'RUU'], writes=['mvi'])
                        S.op('dve', lambda e, row=row: e.tensor_tensor(out=vu[:], in0=vext[:], in1=RUU[:, :, 16 + row:17 + row].to_broadcast([64, NCH, 132]), op=ALU.mult),
                             reads=['mvext', 'RUU'], writes=['mvu'])

                        def evac(nl, n, po, pok, row=row):
                            S.op('act', lambda e: e.copy(out=oext[:, nl, 0:129], in_=po), reads=[pok], writes=K('moext', nl))
                        chunk_loop(d, kc_, qc, vi, qc, kTM, vu, lambda n, row=row: dchunk[:, row, n:n + 1], 129, evac,
                                   ['mqc', 'mkc', 'mvi', 'mvu', 'mkTM'], 'dchunk')
                        allx = K('moext', 0, NLCH)
                        S.op('dve', lambda e, row=row: e.tensor_tensor(out=oext[:, :, 0:129], in0=oext[:, :, 0:129], in1=RUU[:, 4:NCH, row:row + 1].to_broadcast([64, NLCH, 129]), op=ALU.mult),
                             reads=allx + ['RUU'], writes=allx)
                        S.op('act', lambda e: e.activation(out=den[:], in_=oext[:, :, 128], func=AF.Abs), reads=allx, writes=['mden'])
                        S.op('dve', lambda e: e.tensor_scalar_max(out=den[:], in0=den[:], scalar1=1.0), reads=['mden'], writes=['mden'])
                        S.op('dve', lambda e: e.reciprocal(out=den[:], in_=den[:]), reads=['mden'], writes=['mden'])
                        if d == 0:
                            S.op('dve', lambda e: e.tensor_tensor(out=obuf[:], in0=oext[:, :, 0:128], in1=den[:].unsqueeze(2).to_broadcast([64, NLCH, 128]), op=ALU.mult),
                                 reads=allx + ['mden'], writes=['mobuf'])
                        else:
                            S.op('dve', lambda e: e.tensor_tensor(out=oext[:, :, 0:128], in0=oext[:, :, 0:128], in1=den[:].unsqueeze(2).to_broadcast([64, NLCH, 128]), op=ALU.mult),
                                 reads=allx + ['mden'], writes=allx)
                            S.op('dve', lambda e: e.tensor_tensor(out=obuf[:], in0=obuf[:], in1=oext[:, :, 0:128], op=ALU.add), reads=allx + ['mobuf'], writes=['mobuf'])
                    allx = K('moext', 0, NLCH)
                    S.op('dve', lambda e: e.tensor_reduce(out=mu[:], in_=obuf[:], axis=AX.X, op=ALU.add), reads=['mobuf'], writes=['mmu'])
                    S.op('dve', lambda e: e.tensor_scalar_mul(out=mu[:], in0=mu[:], scalar1=1.0 / 128.0), reads=['mmu'], writes=['mmu'])
                    S.op('dve', lambda e: e.tensor_tensor(out=obuf[:], in0=obuf[:], in1=mu[:].unsqueeze(2).to_broadcast([64, NLCH, 128]), op=ALU.subtract), reads=['mobuf', 'mmu'], writes=['mobuf'])
                    S.op('dve', lambda e: e.tensor_tensor(out=oext[:, :, 0:128], in0=obuf[:], in1=obuf[:], op=ALU.mult), reads=['mobuf'] + allx, writes=allx)
                    S.op('dve', lambda e: e.tensor_reduce(out=m2[:], in_=oext[:, :, 0:128], axis=AX.X, op=ALU.add), reads=allx, writes=['mm2'])
                    S.op('act', lambda e: e.activation(out=m2[:], in_=m2[:], func=AF.Sqrt, bias=epsc[0:64, 0:1], scale=1.0 / 128.0), reads=['mm2', 'epsc'], writes=['mm2'])
                    S.op('dve', lambda e: e.reciprocal(out=m2[:], in_=m2[:]), reads=['mm2'], writes=['mm2'])
                    S.op('dve', lambda e: e.tensor_tensor(out=obuf[:], in0=obuf[:], in1=m2[:].unsqueeze(2).to_broadcast([64, NLCH, 128]), op=ALU.mult), reads=['mobuf', 'mm2'], writes=['mobuf'])
                    S.op('dve', lambda e, hd=hd: e.tensor_tensor(out=obuf[:], in0=obuf[:], in1=mln[:, hd * 128:(hd + 1) * 128].unsqueeze(1).to_broadcast([64, NLCH, 128]), op=ALU.mult),
                         reads=['mobuf', 'mln'], writes=['mobuf'])
                    S.op('dve', lambda e: e.tensor_tensor(out=obuf[:], in0=obuf[:], in1=og[:], op=ALU.mult), reads=['mobuf', 'mog'], writes=['mobuf'])
                    transpose_chunks_to_mixT(obuf, ['mobuf'], 4 + hd)
                    S.barrier()
            S.barrier()

        if debug == 'mix':
            with ExitStack() as dd:
                mf = sb(dd, "mf", [128, NLAT])
                for h in range(8):
                    S.op('dve', lambda e, h=h: e.tensor_copy(out=mf[:], in_=mixT[:, h, :]), reads=K('mixT', h) + ['mf'], writes=['mf'])
                    S.dma('sp', lambda e, h=h: e.dma_start(out=dbg_d[h * 128:(h + 1) * 128, :], in_=mf[:]), reads=['mf'], writes=['dbg'])
                S.wait_all('sp', ['dbg']); S.barrier()
            scB.close(); scA.close()
            return nc
        prep_tables(512)
        S.barrier()
        scB.close()
        x1s = nc.dram_tensor("x1s", [NLAT, 1024], F32, kind="Internal").ap()

        def bcast_tile(dst, dkey, c0, dg):
            for ch in range(8):
                S.op('dve', lambda e, ch=ch: e.tensor_scalar_mul(out=dg[:], in0=ident[:], scalar1=modv[:, c0 + ch, 0:1]), reads=['ident', 'modv', 'dg'], writes=['dg'])
                bank = ch // 4
                S.op('pe', lambda e, ch=ch, bank=bank: e.matmul(PB[bank][:, (ch % 4) * 128:(ch % 4 + 1) * 128], lhsT=ones[:], rhs=dg[:], start=True, stop=True),
                     reads=['ones', 'dg'], writes=['pb%d' % bank], skip_self=True)
            S.op('act', lambda e: e.copy(out=dst[:, 0:512], in_=PB[0][:, :]), reads=['pb0'], writes=[dkey])
            S.op('act', lambda e: e.copy(out=dst[:, 512:1024], in_=PB[1][:, :]), reads=['pb1'], writes=[dkey])

        with ExitStack() as p3:
            bct = {}
            for nm in ("g1b", "ln1g", "ln1b"):
                bct[nm] = sb(p3, nm, [128, 1024])
            for nm, dd in (("ln1g", ln1g_d), ("ln1b", ln1b_d)):
                S.dma('sp', lambda e, nm=nm, dd=dd: e.dma_start(out=bct[nm][:], in_=dd[:, :]), writes=[nm])
            dg = sb(p3, "dg", [128, 128])
            bcast_tile(bct["g1b"], "g1b", 16, dg)
            wo_b = sb(p3, "wout", [128, 8, 1024], BF16); wos = sb(p3, "wos", [128, 1024])
            for kc in range(8):
                S.dma('sp', lambda e, kc=kc: e.dma_start(out=wos[:], in_=wout_d[:, kc, :]), writes=['wos'])
                S.op('pool', lambda e, kc=kc: e.tensor_copy(out=wo_b[:, kc, :], in_=wos[:]), reads=['wos'], writes=['wout'])
            xt = [sb(p3, "x3t%d" % i, [128, 1024]) for i in range(2)]
            t1 = [sb(p3, "t1_%d" % i, [128, 1024]) for i in range(2)]
            st = [sb(p3, "st3%d" % i, [128, 2, 6]) for i in range(2)]
            mv = [sb(p3, "mv3%d" % i, [128, 2]) for i in range(2)]
            rs = [sb(p3, "rs3%d" % i, [128, 1]) for i in range(2)]
            for t in range(16):
                b = t % 2
                S.dma('sp' if b == 0 else 'pool', lambda e, t=t, b=b: e.dma_start(out=xt[b][:], in_=xs[256 + t * 128:256 + (t + 1) * 128, :]), writes=['x3t%d' % b])
                for half in range(2):
                    bank = 2 * b + half
                    for kc in range(8):
                        S.op('pe', lambda e, kc=kc, t=t, half=half, bank=bank: e.matmul(PB[bank][:, :], lhsT=mixT[:, kc, t * 128:(t + 1) * 128], rhs=wo_b[:, kc, half * 512:(half + 1) * 512],
                                                                                      start=(kc == 0), stop=(kc == 7)),
                             reads=K('mixT', kc) + ['wout'], writes=['pb%d' % bank], skip_self=True)
                    S.op('dve', lambda e, b=b, half=half, bank=bank: e.tensor_tensor(out=t1[b][:, half * 512:(half + 1) * 512], in0=PB[bank][:, :], in1=bct["g1b"][:, half * 512:(half + 1) * 512], op=ALU.mult),
                         reads=['pb%d' % bank, 'g1b'], writes=['t1_%d' % b])
                S.op('dve', lambda e, b=b: e.scalar_tensor_tensor(out=t1[b][:], in0=xt[b][:], scalar=ALPHA, in1=t1[b][:], op0=ALU.mult, op1=ALU.add),
                     reads=['x3t%d' % b, 't1_%d' % b], writes=['t1_%d' % b])
                layer_norm_rows(st[b], mv[b][:], rs[b][:], t1[b][:], t1[b][:], 't1_%d' % b, 't1_%d' % b, 'p3%d' % b)
                S.op('dve', lambda e, b=b: e.tensor_tensor(out=t1[b][:], in0=t1[b][:], in1=bct["ln1g"][:], op=ALU.mult), reads=['t1_%d' % b, 'ln1g'], writes=['t1_%d' % b])
                S.op('dve', lambda e, b=b, t=t: e.tensor_tensor(out=t1[b][:], in0=t1[b][:], in1=bct["ln1b"][:], op=ALU.add), reads=['t1_%d' % b, 'ln1b'], writes=['t1_%d' % b])
                S.dma('sp', lambda e, t=t, b=b: e.dma_start(out=x1s[t * 128:(t + 1) * 128, :], in_=t1[b][:]), reads=['t1_%d' % b], writes=K('x1_', t))
                if debug == 'x1':
                    S.dma('sp', lambda e, t=t, b=b: e.dma_start(out=dbg_d[t * 128:(t + 1) * 128, :], in_=t1[b][:]), reads=['t1_%d' % b], writes=['dbg'])
            S.barrier()
        scA.close()

        with ExitStack() as p4:
            bct = {}
            for nm in ("g2b", "sc2b", "sh2b", "ln2g", "ln2b"):
                bct[nm] = sb(p4, nm, [128, 1024])
            for nm, dd in (("ln2g", ln2g_d), ("ln2b", ln2b_d)):
                S.dma('sp', lambda e, nm=nm, dd=dd: e.dma_start(out=bct[nm][:], in_=dd[:, :]), writes=[nm])
            dg = sb(p4, "dg4", [128, 128])
            bcast_tile(bct["g2b"], "g2b", 40, dg); bcast_tile(bct["sc2b"], "sc2b", 32, dg); bcast_tile(bct["sh2b"], "sh2b", 24, dg)
            x1t = [sb(p4, "x1t%d" % i, [128, 1024]) for i in range(2)]
            wqb = sb(p4, "wqb", [128, 8, 2048], BF16)
            keysT = sb(p4, "keysT", [128, 16, 128], BF16)
            with ExitStack() as tmp:
                stg = sb(tmp, "wq_stg", [128, 2048])
                for kc in range(8):
                    S.dma('sp', lambda e, kc=kc: e.dma_start(out=stg[:], in_=wq_d[:, kc, :]), writes=['wq_stg'])
                    S.op('pool', lambda e, kc=kc: e.tensor_copy(out=wqb[:, kc, :], in_=stg[:]), reads=['wq_stg'], writes=['wqb'])
                kst = sb(tmp, "kst", [128, 16, 128])
                S.dma('sp', lambda e: e.dma_start(out=kst[:], in_=keysT_d[:, :, :]), writes=['kst'])
                S.op('pool', lambda e: e.tensor_copy(out=keysT[:], in_=kst[:]), reads=['kst'], writes=['keysT'])
                S.barrier()
            NG = 8
            ub = [sb(p4, "ub%d" % i, [128, 1024], BF16) for i in range(NG)]
            vb = [sb(p4, "vb%d" % i, [128, 1024], BF16) for i in range(NG)]
            dgk = [sb(p4, "dgk%d" % i, [128, 128], BF16) for i in range(4)]
            h2 = sb(p4, "h2", [128, 1024]); h2b = [sb(p4, "h2b%d" % i, [128, 1024], BF16) for i in range(2)]
            h2T = sb(p4, "h2T", [128, 8, 128], BF16); qT = sb(p4, "qT", [128, 16, 128], BF16)
            sc = sb(p4, "sc", [128, 16, 128]); scw = sb(p4, "scw", [128, 16, 128])
            top = sb(p4, "top", [128, 16, 16]); topi = sb(p4, "topi", [128, 16, 16], U32); topf = sb(p4, "topf", [128, 16, 16])
            cand = sb(p4, "cand", [128, 8, 256]); candw = sb(p4, "candw", [128, 8, 256]); cidx = sb(p4, "cidx", [128, 8, 256])
            best = sb(p4, "best", [128, 8, 16]); idxf = sb(p4, "idxf", [128, 128])
            gate = [sb(p4, "gate%d" % i, [128, 128]) for i in range(2)]
            idxi = [sb(p4, "idxi%d" % i, [128, 128], I32) for i in range(2)]
            wgt = [sb(p4, "wgt%d" % i, [128, 128]) for i in range(2)]
            dots = sb(p4, "dots", [128, 128]); junk = sb(p4, "junk", [128, 1024], BF16); junk2 = sb(p4, "junk2", [128, 256])
            zs = sb(p4, "zs", [128, 8]); nmx = sb(p4, "nmx", [128, 8])
            st = sb(p4, "st4", [128, 2, 6]); mv = sb(p4, "mv4", [128, 2]); rs = sb(p4, "rs4", [128, 1])
            fin = sb(p4, "fin", [128, 1024]); yb = sb(p4, "yb", [128, 1024])
            NT4 = 16 if debug != 'x1' else 0

            def prologue(t):
                p = t % 2
                xb_ = x1t[p]; x1k = 'x1t%d' % p
                S.dma('sp', lambda e: e.dma_start(out=xb_[:], in_=x1s[t * 128:(t + 1) * 128, :]), reads=K('x1_', t), writes=[x1k])
                S.op('dve', lambda e: e.memset(idxf[:], 0.0), writes=['idxf'])
                S.op('dve', lambda e: e.memset(zs[:], 0.0), writes=['zs'])
                layer_norm_rows(st, mv[:], rs[:], xb_[:], h2[:], x1k, 'h2', 'p4')
                S.op('dve', lambda e: e.tensor_tensor(out=h2[:], in0=h2[:], in1=bct["sc2b"][:], op=ALU.mult), reads=['h2', 'sc2b'], writes=['h2'])
                S.op('dve', lambda e: e.tensor_tensor(out=h2[:], in0=h2[:], in1=bct["sh2b"][:], op=ALU.add), reads=['h2', 'sh2b'], writes=['h2'])
                S.op('act', lambda e: e.copy(out=h2b[p][:], in_=h2[:]), reads=['h2'], writes=['h2b%d' % p])
                yield
                for half in range(2):
                    for c4 in range(4):
                        ch = half * 4 + c4
                        S.op('pe', lambda e, ch=ch, c4=c4, half=half: e.transpose(PB[half][:, c4 * 128:(c4 + 1) * 128], h2[:, ch * 128:(ch + 1) * 128], ident[:]),
                             reads=['h2', 'ident'], writes=['pb%d' % half], skip_self=True)
                    yield
                    S.op('act', lambda e, half=half: e.copy(out=h2T[:, half * 4:(half + 1) * 4, :], in_=PB[half][:, :].rearrange("p (j c) -> p j c", c=128)), reads=['pb%d' % half], writes=['h2T'])
                for g in range(4):
                    for c4 in range(4):
                        c = g * 4 + c4
                        for kc in range(8):
                            S.op('pe', lambda e, c=c, c4=c4, kc=kc, g=g: e.matmul(PB[2 + g][:, c4 * 128:(c4 + 1) * 128], lhsT=wqb[:, kc, c * 128:(c + 1) * 128], rhs=h2T[:, kc, :],
                                                                                start=(kc == 0), stop=(kc == 7)), reads=['wqb', 'h2T'], writes=['pb%d' % (2 + g)], skip_self=True)
                    yield
                    S.op('act' if g % 2 == 0 else 'dve', lambda e, g=g: (e.copy if g % 2 == 0 else e.tensor_copy)(out=qT[:, g * 4:(g + 1) * 4, :], in_=PB[2 + g][:, :].rearrange("p (j c) -> p j c", c=128)),
                         reads=['pb%d' % (2 + g)], writes=['qT'])
                for g in range(4):
                    for c4 in range(4):
                        c = g * 4 + c4
                        S.op('pe', lambda e, c=c, c4=c4, g=g: e.matmul(PB[2 + g][:, c4 * 128:(c4 + 1) * 128], lhsT=qT[:, c, :], rhs=keysT[:, c, :], start=True, stop=True),
                             reads=['qT', 'keysT'], writes=['pb%d' % (2 + g)], skip_self=True)
                    yield
                    S.op('act' if g % 2 == 0 else 'dve', lambda e, g=g: (e.copy if g % 2 == 0 else e.tensor_copy)(out=sc[:, g * 4:(g + 1) * 4, :], in_=PB[2 + g][:, :].rearrange("p (j c) -> p j c", c=128)),
                         reads=['pb%d' % (2 + g)], writes=['sc'])
                for c in range(16):
                    S.op('dve', lambda e, c=c: e.max(out=top[:, c, 0:8], in_=sc[:, c, :]), reads=['sc'], writes=['top'])
                    S.op('dve', lambda e, c=c: e.max_index(out=topi[:, c, 0:8], in_max=top[:, c, 0:8], in_values=sc[:, c, :]), reads=['sc', 'top'], writes=['topi'])
                    S.op('dve', lambda e, c=c: e.match_replace(out=scw[:, c, :], in_to_replace=top[:, c, 0:8], in_values=sc[:, c, :], imm_value=-1e30), reads=['sc', 'top'], writes=['scw'])
                    S.op('dve', lambda e, c=c: e.max(out=top[:, c, 8:16], in_=scw[:, c, :]), reads=['scw'], writes=['top'])
                    S.op('dve', lambda e, c=c: e.max_index(out=topi[:, c, 8:16], in_max=top[:, c, 8:16], in_values=scw[:, c, :]), reads=['scw', 'top'], writes=['topi'])
                    yield
                S.op('dve', lambda e: e.tensor_copy(out=topf[:], in_=topi[:]), reads=['topi'], writes=['topf'])
                t4 = top[:].rearrange("p (h two) k -> p h two k", two=2); f4 = topf[:].rearrange("p (h two) k -> p h two k", two=2)
                c4v = cand[:].rearrange("p h (a b) -> p h a b", b=16); i4 = cidx[:].rearrange("p h (a b) -> p h a b", b=16)
                for h in range(8):
                    S.op('dve', lambda e, h=h: e.tensor_tensor(out=c4v[:, h, :, :], in0=t4[:, h, 0, :].unsqueeze(2).to_broadcast([128, 16, 16]), in1=t4[:, h, 1, :].unsqueeze(1).to_broadcast([128, 16, 16]), op=ALU.add),
                         reads=['top'], writes=['cand'])
                    S.op('dve', lambda e, h=h: e.scalar_tensor_tensor(out=i4[:, h, :, :], in0=f4[:, h, 0, :].unsqueeze(2).to_broadcast([128, 16, 16]), scalar=128.0, in1=f4[:, h, 1, :].unsqueeze(1).to_broadcast([128, 16, 16]),
                                                                    op0=ALU.mult, op1=ALU.add), reads=['topf'], writes=['cidx'])
                    yield
                for h in range(8):
                    S.op('dve', lambda e, h=h: e.max(out=best[:, h, 0:8], in_=cand[:, h, :]), reads=['cand'], writes=['best'])
                    S.op('dve', lambda e, h=h: e.match_replace(out=candw[:, h, :], in_to_replace=best[:, h, 0:8], in_values=cand[:, h, :], imm_value=-1e30), reads=['cand', 'best'], writes=['candw'])
                    S.op('dve', lambda e, h=h: e.max(out=best[:, h, 8:16], in_=candw[:, h, :]), reads=['candw'], writes=['best'])
                    yield
                for h in range(8):
                    yield
                    for k in range(16):
                        S.op('dve', lambda e, h=h, k=k: e.scalar_tensor_tensor(out=junk2[:], in0=cand[:, h, :], scalar=best[:, h, k:k + 1], in1=cidx[:, h, :], op0=ALU.is_equal, op1=ALU.mult,
                                                                             accum_out=idxf[:, h * 16 + k:h * 16 + k + 1]), reads=['cand', 'best', 'cidx', 'junk2'], writes=['junk2', 'idxf'])
                S.op('dve', lambda e: e.tensor_scalar_min(out=idxf[:], in0=idxf[:], scalar1=16383.0), reads=['idxf'], writes=['idxf'])
                S.op('dve', lambda e: e.tensor_copy(out=idxi[p][:], in_=idxf[:]), reads=['idxf'], writes=['idxi%d' % p])
                S.op('dve', lambda e: e.tensor_scalar_mul(out=nmx[:], in0=best[:, :, 0], scalar1=-1.0), reads=['best'], writes=['nmx'])
                g3 = gate[p][:].rearrange("p (h k) -> p h k", k=16)
                for h in range(8):
                    S.op('act', lambda e, h=h: e.activation(out=g3[:, h, :], in_=best[:, h, :], func=AF.Exp, bias=nmx[:, h:h + 1], scale=1.0, accum_out=zs[:, h:h + 1]),
                         reads=['best', 'nmx'], writes=['gate%d' % p, 'zs'])
                S.op('dve', lambda e: e.reciprocal(out=zs[:], in_=zs[:]), reads=['zs'], writes=['zs'])
                S.op('dve', lambda e: e.tensor_tensor(out=g3, in0=g3, in1=zs[:].unsqueeze(2).to_broadcast([128, 8, 16]), op=ALU.mult), reads=['gate%d' % p, 'zs'], writes=['gate%d' % p])

            def u_side(t):
                p = t % 2
                S.op('dve', lambda e: e.memset(dots[:], 0.0), writes=['dots'])
                for k in range(128):
                    s_ = k % NG
                    S.dma('pool', lambda e, k=k, s_=s_: e.indirect_dma_start(out=ub[s_][:], out_offset=None, in_=ubf_d[:, :], in_offset=bass.IndirectOffsetOnAxis(ap=idxi[p][:, k:k + 1], axis=0)),
                          reads=['idxi%d' % p, 'tabs'], writes=['ub%d' % s_])
                    S.op('dve', lambda e, k=k, s_=s_: e.scalar_tensor_tensor(out=junk[:], in0=ub[s_][:], scalar=1.0, in1=h2b[p][:], op0=ALU.mult, op1=ALU.mult, accum_out=dots[:, k:k + 1]),
                         reads=['ub%d' % s_, 'h2b%d' % p, 'junk'], writes=['junk', 'dots'])
                S.op('act', lambda e: e.activation(out=wgt[p][:], in_=dots[:], func=AF.Gelu), reads=['dots'], writes=['wgt%d' % p])
                S.op('dve', lambda e: e.tensor_tensor(out=wgt[p][:], in0=wgt[p][:], in1=gate[p][:], op=ALU.mult), reads=['wgt%d' % p, 'gate%d' % p], writes=['wgt%d' % p])

            def v_side(t, gen=None):
                p = t % 2
                for k in range(128):
                    if gen is not None and k % 2 == 1:
                        next(gen, None)
                    s_ = k % NG; d4 = k % 4
                    S.dma('pool', lambda e, k=k, s_=s_: e.indirect_dma_start(out=vb[s_][:], out_offset=None, in_=vbf_d[:, :], in_offset=bass.IndirectOffsetOnAxis(ap=idxi[p][:, k:k + 1], axis=0)),
                          reads=['idxi%d' % p, 'tabs'], writes=['vb%d' % s_])
                    S.op('act', lambda e, k=k, d4=d4: e.activation(out=dgk[d4][:], in_=identb[:], func=AF.Identity, bias=0.0, scale=wgt[p][:, k:k + 1]),
                         reads=['identb', 'wgt%d' % p], writes=['dgk%d' % d4])
                    for half in range(2):
                        S.op('pe', lambda e, k=k, s_=s_, d4=d4, half=half: e.matmul(PB[6 + half][:, :], lhsT=dgk[d4][:], rhs=vb[s_][:, half * 512:(half + 1) * 512], start=(k == 0), stop=(k == 127)),
                             reads=['dgk%d' % d4, 'vb%d' % s_], writes=['pb%d' % (6 + half)], skip_self=True)
                if gen is not None:
                    for _ in gen:
                        pass
                xb_ = x1t[p]; x1k = 'x1t%d' % p
                for half in range(2):
                    S.op('dve', lambda e, half=half: e.tensor_tensor(out=yb[:, half * 512:(half + 1) * 512], in0=PB[6 + half][:, :], in1=bct["g2b"][:, half * 512:(half + 1) * 512], op=ALU.mult),
                         reads=['pb%d' % (6 + half), 'g2b', 'yb'], writes=['yb'])
                S.op('dve', lambda e: e.scalar_tensor_tensor(out=fin[:], in0=xb_[:], scalar=ALPHA, in1=yb[:], op0=ALU.mult, op1=ALU.add), reads=[x1k, 'yb', 'fin'], writes=['fin'])
                layer_norm_rows(st, mv[:], rs[:], fin[:], fin[:], 'fin', 'fin', 'p4')
                S.op('dve', lambda e: e.tensor_tensor(out=fin[:], in0=fin[:], in1=bct["ln2g"][:], op=ALU.mult), reads=['fin', 'ln2g'], writes=['fin'])
                S.op('dve', lambda e: e.tensor_tensor(out=fin[:], in0=fin[:], in1=bct["ln2b"][:], op=ALU.add), reads=['fin', 'ln2b'], writes=['fin'])
                S.dma('sp', lambda e: e.dma_start(out=out_d[t * 128:(t + 1) * 128, :], in_=fin[:]), reads=['fin'], writes=['out'])

            if NT4:
                for _ in prologue(0):
                    pass
            for t in range(NT4):
                u_side(t)
                v_side(t, prologue(t + 1) if t + 1 < NT4 else None)
            S.wait_all('sp', ['out', 'dbg'])
            S.barrier()
    return nc


def _prep_shared(inp):
    f = np.float32
    sh = {}
    sh["w_mod"] = np.ascontiguousarray(inp["w_mod"][0].reshape(8, 128, 6144).transpose(1, 0, 2))
    sh["b_modT"] = np.ascontiguousarray(inp["b_mod"][0].reshape(48, 128).T)
    sh["w_in"] = np.ascontiguousarray(inp["w_in"][0].reshape(8, 128, 4624).transpose(1, 0, 2))
    sh["lgT"] = np.ascontiguousarray(inp["hg_lb_logits"].reshape(2, 2, 4, 128).transpose(3, 0, 1, 2))
    sh["hgn"] = np.ascontiguousarray(np.broadcast_to(inp["hg_norm_g"][0][None, :], (64, 512)))
    sh["mln"] = np.ascontiguousarray(np.broadcast_to(inp["ml_norm_g"][0][None, :], (64, 512)))
    sh["convw"] = np.ascontiguousarray(inp["ml_conv_w"][0].reshape(9, 8, 128).transpose(2, 0, 1))
    sh["convb"] = np.ascontiguousarray(inp["ml_conv_b"][0].reshape(8, 128).T)
    sh["gateb"] = np.ascontiguousarray(inp["ml_gate_b"][0].reshape(2, 8).T)
    sh["w_out"] = np.ascontiguousarray(inp["w_out"][0].reshape(8, 128, 1024).transpose(1, 0, 2))
    for nm, key in (("ln1g", "ln1_g"), ("ln1b", "ln1_b"), ("ln2g", "ln2_g"), ("ln2b", "ln2_b")):
        sh[nm] = np.ascontiguousarray(np.broadcast_to(inp[key][0][None, :], (128, 1024)))
    sh["wq"] = np.ascontiguousarray(inp["peer_wq"][0].reshape(8, 128, 2048).transpose(1, 0, 2))
    sh["keysT"] = np.ascontiguousarray(inp["peer_keys"][0].reshape(16, 128, 128).transpose(2, 0, 1))
    nexp = 128 if os.environ.get("KDEBUG") else 16384
    sh["pu"] = np.ascontiguousarray(inp["peer_u"][0][:nexp])
    sh["pv"] = np.ascontiguousarray(inp["peer_v"][0][:nexp])
    sh["ident"] = np.eye(128, dtype=f)
    m = np.zeros((64, 2, 64), f)
    s = np.arange(64)[:, None]; c = np.arange(64)[None, :]
    m[:, 0, :] = (s <= c); m[:, 1, :] = (s >= c)
    sh["masks"] = m
    rm = np.ones((128, 512), f); rm[:, ::64] = 0.0
    sh["rmask"] = rm
    sh["sel8"] = np.eye(8, dtype=f)
    dm = np.zeros((8, 2), f); dm[0:4, 0] = 1.0; dm[4:8, 1] = 1.0
    sh["dirm"] = dm
    return {k: np.asarray(v, dtype=f) for k, v in sh.items()}


def kernel(**inputs):
    inp = {k: np.asarray(v) for k, v in inputs.items()}
    debug = os.environ.get("KDEBUG") or None
    nc = build(debug)
    sh = _prep_shared(inp)
    in_maps = []
    for b in range(8):
        m = dict(sh)
        m["xs"] = np.ascontiguousarray(np.concatenate([inp["ctx"][b], inp["x"][b]], axis=0).astype(np.float32))
        m["cT"] = np.ascontiguousarray(np.stack([inp["c"][b], inp["c_ctx"]], axis=-1).reshape(8, 128, 2).transpose(1, 0, 2).astype(np.float32))
        in_maps.append(m)
    res = run_bass_kernel_spmd(nc, in_maps, core_ids=list(range(8)))
    key = "dbg" if debug else "out"
    return np.stack([np.asarray(r[key]) for r in res.results], axis=0).astype(np.float32)
```

```python
import os
import numpy as np
from contextlib import ExitStack
import concourse.bass as bass
import concourse.mybir as mybir
from concourse.bass_utils import run_bass_kernel_spmd

F32 = mybir.dt.float32; BF16 = mybir.dt.bfloat16; I32 = mybir.dt.int32; U32 = mybir.dt.uint32
AF = mybir.ActivationFunctionType; ALU = mybir.AluOpType; AX = mybir.AxisListType

NTOK = 2304; NLAT = 2048; NCH = 36; NLCH = 32
ALPHA = 2.0 ** 0.25
EPS = 1e-6


class Sched:
    NDMA = 32

    def __init__(self, nc, es):
        self.nc = nc
        self.engs = {'pe': nc.tensor, 'act': nc.scalar, 'dve': nc.vector, 'pool': nc.gpsimd, 'sp': nc.sync}
        self.sem = {k: es.enter_context(nc.semaphore("sem_" + k)) for k in self.engs}
        self.cnt = {k: 0 for k in self.engs}
        self.dsem = [es.enter_context(nc.semaphore("dsem%d" % i)) for i in range(self.NDMA)]
        self.dcnt = [0] * self.NDMA
        self.dnext = 0
        self.seen = {k: {} for k in self.engs}
        self.bufs = {}

    def _deps(self, reads, writes):
        deps = []
        for r in reads:
            b = self.bufs.get(r)
            if b and b['w'] is not None:
                deps.append(b['w'])
        for w in writes:
            b = self.bufs.get(w)
            if b:
                if b['w'] is not None:
                    deps.append(b['w'])
                deps.extend(b['r'])
        return deps

    def _wait(self, eng, deps, skip_self=False):
        best = {}
        for (sid, sem, val, owner) in deps:
            if skip_self and owner == eng:
                continue
            if best.get(sid, (None, 0))[1] < val:
                best[sid] = (sem, val)
        for sid, (sem, val) in best.items():
            if self.seen[eng].get(sid, 0) >= val:
                continue
            self.engs[eng].wait_ge(sem, val)
            self.seen[eng][sid] = val

    def _record(self, dep, reads, writes):
        for r in reads:
            b = self.bufs.setdefault(r, {'w': None, 'r': []})
            b['r'] = [d for d in b['r'] if d[0] != dep[0]] + [dep]
        for w in writes:
            self.bufs[w] = {'w': dep, 'r': []}

    @staticmethod
    def _split(keys):
        norm, ps = [], []
        for k in keys:
            if k.startswith('pb') and len(k) > 2 and k[2].isdigit():
                ps.append(k[:3])
            else:
                norm.append(k)
        return norm, ps

    def op(self, eng, fn, reads=(), writes=(), skip_self=False):
        reads, pr = self._split(reads)
        writes, pw = self._split(writes)
        banks = sorted(set(pr + pw))
        deps = self._deps(reads, writes)
        deps += [d for d in self._deps((), banks) if d[3] != eng]
        self._wait(eng, deps, skip_self)
        ins = fn(self.engs[eng])
        self.cnt[eng] += 1
        ins.then_inc(self.sem[eng], 1)
        self._record(('e_' + eng, self.sem[eng], self.cnt[eng], eng), reads, list(writes) + banks)
        return ins

    def dma(self, q, fn, reads=(), writes=()):
        deps = self._deps(reads, writes)
        j = self.dnext
        self.dnext = (self.dnext + 1) % self.NDMA
        if self.dcnt[j] > 0:
            deps = deps + [('d%d' % j, self.dsem[j], self.dcnt[j], 'dma')]
        self._wait(q, deps)
        ins = fn(self.engs[q])
        self.dcnt[j] += 16
        ins.then_inc(self.dsem[j], 16)
        self._record(('d%d' % j, self.dsem[j], self.dcnt[j], 'dma'), reads, writes)

    def wait_all(self, eng, keys):
        deps = []
        for k in keys:
            b = self.bufs.get(k)
            if b:
                if b['w'] is not None:
                    deps.append(b['w'])
                deps.extend(b['r'])
        self._wait(eng, deps)

    def barrier(self):
        for e in self.engs:
            deps = [('e_' + f, self.sem[f], self.cnt[f], f) for f in self.engs if f != e and self.cnt[f] > 0]
            deps += [('d%d' % j, self.dsem[j], self.dcnt[j], 'dma') for j in range(self.NDMA) if self.dcnt[j] > 0]
            self._wait(e, deps)


def K(name, a, b=None):
    if b is None:
        return ["%s%d" % (name, a)]
    return ["%s%d" % (name, i) for i in range(a, b)]


def build(debug=None):
    nc = bass.Bass("TRN2", target_bir_lowering=False)
    D = {}

    def din(name, shape, dt=F32):
        D[name] = nc.dram_tensor(name, shape, dt, kind="ExternalInput").ap()
        return D[name]

    xs = din("xs", [NTOK, 1024]); cT_d = din("cT", [128, 8, 2]); wmod_d = din("w_mod", [128, 8, 6144])
    bmod_d = din("b_modT", [128, 48]); win_d = din("w_in", [128, 8, 4624]); lg_d = din("lgT", [128, 2, 2, 4])
    hgn_d = din("hgn", [64, 512]); mln_d = din("mln", [64, 512]); convw_d = din("convw", [128, 9, 8])
    convb_d = din("convb", [128, 8]); gateb_d = din("gateb", [8, 2]); wout_d = din("w_out", [128, 8, 1024])
    ln1g_d = din("ln1g", [128, 1024]); ln1b_d = din("ln1b", [128, 1024]); ln2g_d = din("ln2g", [128, 1024])
    ln2b_d = din("ln2b", [128, 1024]); wq_d = din("wq", [128, 8, 2048]); keysT_d = din("keysT", [128, 16, 128])
    NEXP = 128 if debug else 16384
    pu_d = din("pu", [NEXP, 1024]); pv_d = din("pv", [NEXP, 1024])
    ident_d = din("ident", [128, 128]); masks_d = din("masks", [64, 2, 64]); rmask_d = din("rmask", [128, 512])
    sel8_d = din("sel8", [8, 8]); dirm_d = din("dirm", [8, 2])
    out_d = nc.dram_tensor("out", [NLAT, 1024], F32, kind="ExternalOutput").ap()
    dbg_d = None
    if debug:
        dbg_d = nc.dram_tensor("dbg", [NLAT, 1024] if debug != 'mix' else [1024, NLAT], F32, kind="ExternalOutput").ap()

    with ExitStack() as es:
        S = Sched(nc, es)

        uid = [0]

        def sb(st, name, shape, dt=F32):
            uid[0] += 1
            return st.enter_context(nc.sbuf_tensor("s%d_%s" % (uid[0], name), shape, dt))

        PB = [es.enter_context(nc.psum_tensor("pb%d" % i, [128, 512], F32)) for i in range(8)]

        ident = sb(es, "ident", [128, 128]); identb = sb(es, "identb", [128, 128], BF16)
        masks = sb(es, "masks", [64, 2, 64]); rmask = sb(es, "rmask", [128, 512])
        ones = sb(es, "ones", [128, 128]); epsc = sb(es, "epsc", [128, 1])
        modv = sb(es, "modv", [128, 48, 2])
        S.dma('sp', lambda e: e.dma_start(out=ident[:], in_=ident_d[:, :]), writes=['ident'])
        S.dma('sp', lambda e: e.dma_start(out=masks[:], in_=masks_d[:, :, :]), writes=['masks'])
        S.dma('sp', lambda e: e.dma_start(out=rmask[:], in_=rmask_d[:, :]), writes=['rmask'])
        S.op('dve', lambda e: e.tensor_copy(out=identb[:], in_=ident[:]), reads=['ident'], writes=['identb'])
        S.op('dve', lambda e: e.memset(ones[:], 1.0), writes=['ones'])
        S.op('dve', lambda e: e.memset(epsc[:], EPS), writes=['epsc'])

        with ExitStack() as p0:
            cT = sb(p0, "cT", [128, 8, 2]); scT = sb(p0, "scT", [128, 8, 2]); bmodT = sb(p0, "bmodT", [128, 48])
            wm = [sb(p0, "wm%d" % i, [128, 6144]) for i in range(2)]
            S.dma('sp', lambda e: e.dma_start(out=cT[:], in_=cT_d[:, :, :]), writes=['cT'])
            S.dma('sp', lambda e: e.dma_start(out=bmodT[:], in_=bmod_d[:, :]), writes=['bmodT'])
            S.op('act', lambda e: e.activation(out=scT[:], in_=cT[:], func=AF.Silu), reads=['cT'], writes=['scT'])
            for kc in range(8):
                w = wm[kc % 2]; wk = 'wm%d' % (kc % 2)
                S.dma('sp' if kc % 2 == 0 else 'pool', lambda e, w=w, kc=kc: e.dma_start(out=w[:], in_=wmod_d[:, kc, :]), writes=[wk])
                for j in range(48):
                    S.op('pe', lambda e, w=w, kc=kc, j=j: e.matmul(PB[kc // 4][:, (kc % 4) * 96 + 2 * j:(kc % 4) * 96 + 2 * j + 2], lhsT=w[:, j * 128:(j + 1) * 128], rhs=scT[:, kc, :],
                                                                 start=True, stop=True),
                         reads=[wk, 'scT'], writes=['pb%d' % (kc // 4)], skip_self=True)
            mflat = modv[:].rearrange("p j n -> p (j n)")
            S.op('dve', lambda e: e.tensor_tensor(out=modv[:], in0=PB[0][:, 0:96].rearrange("p (j n) -> p j n", n=2), in1=bmodT[:].unsqueeze(2).to_broadcast([128, 48, 2]), op=ALU.add),
                 reads=['pb0', 'bmodT'], writes=['modv'])
            for kc in range(1, 8):
                S.op('dve', lambda e, kc=kc: e.tensor_tensor(out=mflat, in0=mflat, in1=PB[kc // 4][:, (kc % 4) * 96:(kc % 4) * 96 + 96], op=ALU.add),
                     reads=['pb%d' % (kc // 4), 'modv'], writes=['modv'])
            S.op('dve', lambda e: e.tensor_scalar_add(out=modv[:, 8:16, :], in0=modv[:, 8:16, :], scalar1=1.0), reads=['modv'], writes=['modv'])
            S.op('dve', lambda e: e.tensor_scalar_add(out=modv[:, 32:40, :], in0=modv[:, 32:40, :], scalar1=1.0), reads=['modv'], writes=['modv'])
            S.barrier()
        if debug == 'p0':
            S.dma('sp', lambda e: e.dma_start(out=dbg_d[0:128, 0:96], in_=modv[:].rearrange("p a b -> p (a b)")), reads=['modv'], writes=['dbg'])
            S.wait_all('sp', ['dbg']); S.barrier()
            return nc

        scA = ExitStack(); scB = ExitStack()
        mixT = sb(scA, "mixT", [128, 8, NLAT], BF16)
        hT = sb(scB, "hT", [128, 8, NTOK], BF16)
        wstg = sb(scB, "wstg", [128, 8, 128])
        tstg = [sb(scB, "tstg%d" % i, [128, 512]) for i in range(2)]
        tbf = [sb(scB, "tbf%d" % i, [128, 512], BF16) for i in range(2)]
        uv_d = nc.dram_tensor("uvbf", [NEXP, 2048], BF16, kind="Internal").ap()
        prep_pos = [0]

        def prep_tables(npieces):
            if debug:
                return
            for _ in range(npieces):
                i = prep_pos[0]
                if i >= 512:
                    return
                prep_pos[0] += 1
                src, coff = (pu_d, 0) if i < 256 else (pv_d, 1024)
                r = (i % 256) // 2; c = (i % 2) * 512; bb = i % 2
                S.dma('sp', lambda e, src=src, r=r, c=c, bb=bb: e.dma_start(out=tstg[bb][:], in_=src[r * 128:(r + 1) * 128, c:c + 512]), writes=['tstg%d' % bb])
                S.op('pool', lambda e, bb=bb: e.tensor_copy(out=tbf[bb][:], in_=tstg[bb][:]), reads=['tstg%d' % bb], writes=['tbf%d' % bb])
                S.dma('pool', lambda e, coff=coff, r=r, c=c, bb=bb: e.dma_start(out=uv_d[r * 128:(r + 1) * 128, coff + c:coff + c + 512], in_=tbf[bb][:]), reads=['tbf%d' % bb], writes=['tabs'])

        def layer_norm_rows(st_ap, mv_ap, rstd_ap, src, dst, skey, dkey, tag):
            S.op('dve', lambda e: e.bn_stats(out=st_ap[:, 0, :], in_=src[:, 0:512]), reads=[skey], writes=[tag + 'st'])
            S.op('dve', lambda e: e.bn_stats(out=st_ap[:, 1, :], in_=src[:, 512:1024]), reads=[skey], writes=[tag + 'st'])
            S.op('dve', lambda e: e.bn_aggr(out=mv_ap, in_=st_ap[:].rearrange("p a b -> p (a b)")), reads=[tag + 'st'], writes=[tag + 'mv'])
            S.op('act', lambda e: e.activation(out=rstd_ap, in_=mv_ap[:, 1:2], func=AF.Sqrt, bias=epsc[:, 0:1], scale=1.0),
                 reads=[tag + 'mv', 'epsc'], writes=[tag + 'rs'])
            S.op('dve', lambda e: e.reciprocal(out=rstd_ap, in_=rstd_ap), reads=[tag + 'rs'], writes=[tag + 'rs'])
            S.op('dve', lambda e: e.tensor_scalar(out=dst, in0=src, scalar1=mv_ap[:, 0:1], scalar2=rstd_ap, op0=ALU.subtract, op1=ALU.mult),
                 reads=[skey, tag + 'mv', tag + 'rs'], writes=[dkey])

        with ExitStack() as p1:
            xt = [sb(p1, "xt%d" % i, [128, 1024]) for i in range(2)]
            xn = [sb(p1, "xn%d" % i, [128, 1024]) for i in range(2)]
            st = [sb(p1, "st%d" % i, [128, 2, 6]) for i in range(2)]
            mv = [sb(p1, "mv%d" % i, [128, 2]) for i in range(2)]
            rs = [sb(p1, "rs%d" % i, [128, 1]) for i in range(2)]
            PSAP = bool(os.environ.get("KPSAP"))
            for t in range(18):
                b = t % 2
                n = 1 if t < 2 else 0
                S.dma('sp' if b == 0 else 'pool', lambda e, t=t, b=b: e.dma_start(out=xt[b][:], in_=xs[t * 128:(t + 1) * 128, :]), writes=['xt%d' % b])
                layer_norm_rows(st[b], mv[b][:], rs[b][:], xt[b][:], xn[b][:], 'xt%d' % b, 'xn%d' % b, 'p1%d' % b)
                for half in range(2):
                    bank = 2 * b + half
                    pk = 'pb%d' % bank
                    for c4 in range(4):
                        ch = half * 4 + c4
                        S.op('pe', lambda e, b=b, ch=ch, c4=c4, bank=bank: e.transpose(PB[bank][:, c4 * 128:(c4 + 1) * 128], xn[b][:, ch * 128:(ch + 1) * 128], ident[:]),
                             reads=['xn%d' % b, 'ident'], writes=[pk], skip_self=True)
                    if not PSAP:
                        S.op('act', lambda e, b=b, half=half, bank=bank: e.copy(out=xt[b][:, half * 512:(half + 1) * 512], in_=PB[bank][:, :]), reads=[pk, 'xt%d' % b], writes=['xt%d' % b])
                    for c4 in range(4):
                        ch = half * 4 + c4
                        dst = hT[:, ch, t * 128:(t + 1) * 128]
                        src = PB[bank][:, c4 * 128:(c4 + 1) * 128] if PSAP else xt[b][:, ch * 128:(ch + 1) * 128]
                        S.op('dve', lambda e, dst=dst, src=src, ch=ch, n=n: e.tensor_scalar(out=dst, in0=src, scalar1=modv[:, 8 + ch, n:n + 1], scalar2=modv[:, ch, n:n + 1],
                                                                                      op0=ALU.mult, op1=ALU.add),
                             reads=([pk] if PSAP else ['xt%d' % b]) + ['modv'], writes=K('hT', t))
            S.barrier()

        if debug == 'p1':
            with ExitStack() as dd:
                hf = sb(dd, "hf", [128, 1024])
                for t in range(16):
                    S.op('dve', lambda e, t=t: e.tensor_copy(out=hf[:].rearrange("p (a b) -> p a b", b=128), in_=hT[:, :, 256 + t * 128:256 + (t + 1) * 128]), reads=K('hT', t + 2) + ['hf'], writes=['hf'])
                    S.dma('sp', lambda e, t=t: e.dma_start(out=dbg_d[t * 128:(t + 1) * 128, :], in_=hf[:]), reads=['hf'], writes=['dbg'])
                S.wait_all('sp', ['dbg']); S.barrier()
            scB.close(); scA.close()
            return nc
        def load_w(stk, name, col0, ncols, src=None):
            src = win_d if src is None else src
            wb = sb(stk, name, [128, 8, ncols], BF16)
            S.dma('sp', lambda e: e.dma_start(out=wstg[:, :, 0:ncols], in_=src[:, :, col0:col0 + ncols]), writes=['wstg'])
            S.op('pool', lambda e: e.tensor_copy(out=wb[:], in_=wstg[:, :, 0:ncols]), reads=['wstg'], writes=[name])
            return wb

        BLKS = [(0, 512), (512, 512), (1024, 512), (1536, 512), (2048, 256)]

        def proj_fm_block(wb, wname, ncols, bank, t0, nt):
            for kc in range(8):
                S.op('pe', lambda e, kc=kc: e.matmul(PB[bank][0:ncols, 0:nt], lhsT=wb[:, kc, :], rhs=hT[:, kc, t0:t0 + nt], start=(kc == 0), stop=(kc == 7)),
                     reads=[wname] + K('hT', t0 // 128, (t0 + nt) // 128), writes=['pb%d' % bank], skip_self=True)

        def proj_tm_group(wb, wname, ncols, bank, c0, ncks):
            for j in range(ncks):
                n = c0 + j
                for kc in range(8):
                    S.op('pe', lambda e, kc=kc, j=j, n=n: e.matmul(PB[bank][0:64, j * ncols:(j + 1) * ncols], lhsT=hT[:, kc, n * 64:(n + 1) * 64], rhs=wb[:, kc, :],
                                                                   start=(kc == 0), stop=(kc == 7)),
                         reads=[wname] + K('hT', n // 2), writes=['pb%d' % bank], skip_self=True)

        S32 = sb(scB, "S32", [128, 2, 132]); Sbf = sb(scB, "Sbf", [128, 2, 132], BF16)
        stm = sb(scB, "stm", [64, 2, 64], BF16)
        usb = sb(scB, "usb", [128, 2, 132])

        def chunk_loop(dirn, KsT, QsT, Vi, QoT, Ku, Vu, dec_ap, W, evac, rk, tagk):
            order = list(range(36)) if dirn == 0 else [3, 2, 1, 0] + list(range(35, 3, -1))
            S.op('dve', lambda e: e.memset(S32[:, 0, :], 0.0), writes=['S32_0'])
            S.op('dve', lambda e: e.memset(Sbf[:, 0, :], 0.0), writes=['Sbf_0'])
            cur = 0
            for idx, n in enumerate(order):
                sl = slice(n * 64, (n + 1) * 64)
                s2 = idx % 2
                if n >= 4:
                    pst = PB[2 + s2][0:64, 0:64]; pstk = 'pb%d' % (2 + s2)
                    po = PB[4 + s2][0:64, 0:W]; pok = 'pb%d' % (4 + s2)
                    S.op('pe', lambda e, pst=pst, sl=sl: e.matmul(pst, lhsT=KsT[:, sl], rhs=QsT[:, sl], start=True, stop=True),
                         reads=rk, writes=[pstk], skip_self=True)
                    S.op('dve', lambda e, pst=pst, s2=s2: e.tensor_tensor(out=stm[:, s2, :], in0=pst, in1=masks[:, dirn, :], op=ALU.mult),
                         reads=[pstk, 'masks'], writes=['stm%d' % s2])
                    S.op('pe', lambda e, po=po, s2=s2, n=n: e.matmul(po, lhsT=stm[:, s2, :], rhs=Vi[:, n, 0:W], start=True, stop=False),
                         reads=['stm%d' % s2] + rk, writes=[pok], skip_self=True)
                    S.op('pe', lambda e, po=po, sl=sl, cur=cur: e.matmul(po, lhsT=QoT[:, sl], rhs=Sbf[:, cur, 0:W], start=False, stop=True),
                         reads=['Sbf_%d' % cur] + rk, writes=[pok], skip_self=True)
                    evac(n - 4, n, po, pok)
                if idx < 35:
                    pu = PB[6 + s2][:, 0:W]; puk = 'pb%d' % (6 + s2)
                    S.op('pe', lambda e, pu=pu, n=n: e.matmul(pu, lhsT=Ku[:, n, :], rhs=Vu[:, n, 0:W], start=True, stop=True),
                         reads=rk, writes=[puk], skip_self=True)
                    nxt = 1 - cur
                    S.op('act', lambda e, pu=pu, s2=s2: e.copy(out=usb[:, s2, 0:W], in_=pu), reads=[puk], writes=['usb%d' % s2])
                    S.op('dve', lambda e, n=n, cur=cur, nxt=nxt, s2=s2: e.scalar_tensor_tensor(out=Sbf[:, nxt, 0:W], in0=S32[:, cur, 0:W], scalar=dec_ap(n), in1=usb[:, s2, 0:W],
                                                                                            op0=ALU.mult, op1=ALU.add),
                         reads=['S32_%d' % cur, 'usb%d' % s2, tagk], writes=['Sbf_%d' % nxt])
                    S.op('dve', lambda e, n=n, cur=cur, nxt=nxt, s2=s2: e.scalar_tensor_tensor(out=S32[:, nxt, 0:W], in0=S32[:, cur, 0:W], scalar=dec_ap(n), in1=usb[:, s2, 0:W],
                                                                                            op0=ALU.mult, op1=ALU.add),
                         reads=['S32_%d' % cur, 'usb%d' % s2, tagk], writes=['S32_%d' % nxt])
                    cur = nxt

        def transpose_chunks_to_mixT(src, skey, head):
            for g in range(4):
                bank = g % 2
                for j in range(8):
                    n = g * 8 + j
                    S.op('pe', lambda e, n=n, j=j, bank=bank: e.transpose(PB[bank][:, j * 64:(j + 1) * 64], src[:, n, :], ident[0:64, 0:64]),
                         reads=list(skey) + ['ident'], writes=['pb%d' % bank], skip_self=True)
                S.op('act', lambda e, g=g, bank=bank: e.copy(out=mixT[:, head, g * 512:(g + 1) * 512], in_=PB[bank][:, :]),
                     reads=['pb%d' % bank], writes=K('mixT', head))

        with ExitStack() as g0:
            lgT = sb(g0, "lgT", [128, 2, 2, 4]); lbT = sb(g0, "lbT", [128, 2, 4]); omlT = sb(g0, "omlT", [128, 2, 4]); nomlT = sb(g0, "nomlT", [128, 2, 4])
            hgn = sb(g0, "hgn", [64, 512])
            S.dma('sp', lambda e: e.dma_start(out=lgT[:], in_=lg_d[:, :, :, :]), writes=['lgT'])
            S.dma('sp', lambda e: e.dma_start(out=hgn[:], in_=hgn_d[:, :]), writes=['hgn'])
            S.op('dve', lambda e: e.tensor_tensor(out=lbT[:], in0=lgT[:, :, 0, :], in1=lgT[:, :, 1, :], op=ALU.subtract), reads=['lgT'], writes=['lbT'])
            S.op('act', lambda e: e.activation(out=lbT[:], in_=lbT[:], func=AF.Sigmoid), reads=['lbT'], writes=['lbT'])
            S.op('dve', lambda e: e.tensor_scalar(out=omlT[:], in0=lbT[:], scalar1=-1.0, scalar2=1.0, op0=ALU.mult, op1=ALU.add), reads=['lbT'], writes=['omlT'])
            S.op('dve', lambda e: e.tensor_scalar_mul(out=nomlT[:], in0=omlT[:], scalar1=-1.0), reads=['omlT'], writes=['nomlT'])
            QsT = [sb(g0, "gQsT%d" % d, [128, NTOK], BF16) for d in range(2)]
            KsT = [sb(g0, "gKsT%d" % d, [128, NTOK], BF16) for d in range(2)]
            QoT = [sb(g0, "gQoT%d" % d, [128, NTOK], BF16) for d in range(2)]
            Ku = [sb(g0, "gKu%d" % d, [64, NCH, 128], BF16) for d in range(2)]
            dec = sb(g0, "gdec", [128, 2, NCH])
            vtm = sb(g0, "gv", [64, NCH, 128], BF16); gs = sb(g0, "ggs", [64, NLCH, 128], BF16); oacc = sb(g0, "goacc", [64, NLCH, 128])
            qf = sb(g0, "gqf", [128, 512]); sg = sb(g0, "gsg", [128, 512]); lf = sb(g0, "glf", [128, 512]); key = sb(g0, "gkey", [128, 512])
            Bc = sb(g0, "gB", [128, 512]); T1 = sb(g0, "gT1", [128, 512]); E1 = sb(g0, "gE1", [128, 512]); khT = sb(g0, "gkhT", [128, 512], BF16)
            sq = sb(g0, "gsq", [64, NLCH, 128], BF16); ss = sb(g0, "gss", [64, NLCH])
            for hd in range(4):
                with ExitStack() as hs:
                    wq_ = load_w(hs, "gwq", 0 + hd * 128, 128); wi_ = load_w(hs, "gwi", 512 + hd * 128, 128); wg_ = load_w(hs, "gwg", 1024 + hd * 128, 128)
                    wf = [load_w(hs, "gwf0", 1536 + hd * 128, 128), load_w(hs, "gwf1", 2048 + hd * 128, 128)]
                    prep_tables(64)
                    for g in range(9):
                        bk = 3 + g % 2
                        proj_tm_group(wi_, "gwi", 128, bk, g * 4, 4)
                        S.op('act', lambda e, g=g, bk=bk: e.copy(out=vtm[:, g * 4:(g + 1) * 4, :], in_=PB[bk][0:64, :].rearrange("p (j c) -> p j c", c=128)),
                             reads=['pb%d' % bk], writes=['gv'])
                    for g in range(8):
                        bk = 3 + (g + 1) % 2
                        proj_tm_group(wg_, "gwg", 128, bk, 4 + g * 4, 4)
                        S.op('act', lambda e, g=g, bk=bk: e.activation(out=gs[:, g * 4:(g + 1) * 4, :], in_=PB[bk][0:64, :].rearrange("p (j c) -> p j c", c=128), func=AF.Silu),
                             reads=['pb%d' % bk], writes=['ggs'])
                    for (t0, nt) in BLKS:
                        nck = nt // 64; c0 = t0 // 64
                        proj_fm_block(wq_, "gwq", 128, 0, t0, nt)
                        S.op('act', lambda e, nt=nt: e.copy(out=qf[:, 0:nt], in_=PB[0][:, 0:nt]), reads=['pb0'], writes=['gqf'])
                        for d in range(2):
                            proj_fm_block(wf[d], "gwf%d" % d, 128, 1 + d, t0, nt)
                            col = d * 4 + hd
                            lbp = lbT[:, d, hd:hd + 1]; omp = omlT[:, d, hd:hd + 1]; nomp = nomlT[:, d, hd:hd + 1]
                            S.op('act', lambda e, d=d, nt=nt: e.activation(out=sg[:, 0:nt], in_=PB[1 + d][:, 0:nt], func=AF.Sigmoid), reads=['pb%d' % (1 + d)], writes=['gsg'])
                            S.op('act', lambda e, nt=nt, lbp=lbp, omp=omp: e.activation(out=lf[:, 0:nt], in_=sg[:, 0:nt], func=AF.Ln, bias=lbp, scale=omp),
                                 reads=['gsg', 'lbT', 'omlT'], writes=['glf'])
                            S.op('dve', lambda e, nt=nt, nomp=nomp, omp=omp: e.tensor_scalar(out=key[:, 0:nt], in0=sg[:, 0:nt], scalar1=nomp, scalar2=omp, op0=ALU.mult, op1=ALU.add),
                                 reads=['gsg', 'omlT', 'nomlT'], writes=['gkey'])
                            S.op('dve', lambda e, nt=nt: e.tensor_tensor_scan(out=Bc[:, 0:nt], data0=rmask[:, 0:nt], data1=lf[:, 0:nt], initial=0.0, op0=ALU.mult, op1=ALU.add),
                                 reads=['glf', 'rmask'], writes=['gB'])
                            B3 = Bc[:, 0:nt].rearrange("p (n c) -> p n c", c=64)
                            T3 = T1[:, 0:nt].rearrange("p (n c) -> p n c", c=64)
                            if d == 1:
                                S.op('dve', lambda e, B3=B3, T3=T3, nck=nck: e.tensor_tensor(out=T3, in0=B3[:, :, 63:64].to_broadcast([128, nck, 64]), in1=B3, op=ALU.subtract),
                                     reads=['gB'], writes=['gT1'])
                                S.op('dve', lambda e, nt=nt: e.tensor_tensor(out=Bc[:, 0:nt], in0=T1[:, 0:nt], in1=lf[:, 0:nt], op=ALU.add), reads=['gT1', 'glf'], writes=['gB'])
                            li = 63 if d == 0 else 0
                            S.op('act', lambda e, B3=B3, li=li, d=d, c0=c0, nck=nck: e.activation(out=dec[:, d, c0:c0 + nck], in_=B3[:, :, li], func=AF.Exp), reads=['gB'], writes=['gdec'])
                            S.op('dve', lambda e, B3=B3, T3=T3, nck=nck: e.tensor_tensor(out=T3, in0=B3, in1=B3[:, :, 32:33].to_broadcast([128, nck, 64]), op=ALU.subtract),
                                 reads=['gB'], writes=['gT1'])
                            S.op('act', lambda e, nt=nt: e.activation(out=E1[:, 0:nt], in_=T1[:, 0:nt], func=AF.Exp), reads=['gT1'], writes=['gE1'])
                            S.op('dve', lambda e, nt=nt, t0=t0, d=d: e.tensor_tensor(out=QsT[d][:, t0:t0 + nt], in0=qf[:, 0:nt], in1=E1[:, 0:nt], op=ALU.mult),
                                 reads=['gqf', 'gE1'], writes=['gQsT%d' % d])
                            S.op('act', lambda e, nt=nt: e.activation(out=E1[:, 0:nt], in_=T1[:, 0:nt], func=AF.Exp, scale=-1.0), reads=['gT1'], writes=['gE1'])
                            S.op('dve', lambda e, nt=nt, t0=t0, d=d: e.tensor_tensor(out=KsT[d][:, t0:t0 + nt], in0=key[:, 0:nt], in1=E1[:, 0:nt], op=ALU.mult),
                                 reads=['gkey', 'gE1'], writes=['gKsT%d' % d])
                            S.op('act', lambda e, nt=nt: e.activation(out=E1[:, 0:nt], in_=Bc[:, 0:nt], func=AF.Exp), reads=['gB'], writes=['gE1'])
                            S.op('dve', lambda e, nt=nt, t0=t0, d=d: e.tensor_tensor(out=QoT[d][:, t0:t0 + nt], in0=qf[:, 0:nt], in1=E1[:, 0:nt], op=ALU.mult),
                                 reads=['gqf', 'gE1'], writes=['gQoT%d' % d])
                            S.op('dve', lambda e, B3=B3, T3=T3, nck=nck, li=li: e.tensor_tensor(out=T3, in0=B3[:, :, li:li + 1].to_broadcast([128, nck, 64]), in1=B3, op=ALU.subtract),
                                 reads=['gB'], writes=['gT1'])
                            S.op('act', lambda e, nt=nt: e.activation(out=E1[:, 0:nt], in_=T1[:, 0:nt], func=AF.Exp), reads=['gT1'], writes=['gE1'])
                            S.op('dve', lambda e, nt=nt: e.tensor_tensor(out=khT[:, 0:nt], in0=key[:, 0:nt], in1=E1[:, 0:nt], op=ALU.mult), reads=['gkey', 'gE1'], writes=['gkhT'])
                            pbb = PB[3][:].bitcast(BF16)
                            for j in range(nck):
                                S.op('pe', lambda e, j=j: e.transpose(pbb[0:64, j * 128:(j + 1) * 128], khT[:, j * 64:(j + 1) * 64], identb[:]),
                                     reads=['gkhT', 'identb'], writes=['pb3'], skip_self=True)
                            S.op('act', lambda e, d=d, c0=c0, nck=nck: e.copy(out=Ku[d][:, c0:c0 + nck, :], in_=pbb[0:64, 0:nck * 128].rearrange("p (j c) -> p j c", c=128)),
                                 reads=['pb3'], writes=['gKu%d' % d])
                    for d in range(2):
                        def evac(nl, n, po, pok, d=d):
                            if d == 0:
                                S.op('act', lambda e: e.copy(out=oacc[:, nl, :], in_=po), reads=[pok], writes=K('goacc', nl))
                            else:
                                S.op('dve', lambda e: e.tensor_tensor(out=oacc[:, nl, :], in0=po, in1=oacc[:, nl, :], op=ALU.add), reads=[pok] + K('goacc', nl), writes=K('goacc', nl))
                        chunk_loop(d, KsT[d], QsT[d], vtm, QoT[d], Ku[d], vtm, lambda n, d=d: dec[:, d, n:n + 1], 128, evac,
                                   ['gQsT%d' % d, 'gKsT%d' % d, 'gQoT%d' % d, 'gKu%d' % d, 'gv'], 'gdec')
                    allo = K('goacc', 0, NLCH)
                    S.op('dve', lambda e: e.tensor_tensor(out=sq[:], in0=oacc[:], in1=oacc[:], op=ALU.mult), reads=allo, writes=['gsq'])
                    S.op('dve', lambda e: e.tensor_reduce(out=ss[:], in_=sq[:], axis=AX.X, op=ALU.add), reads=['gsq'], writes=['gss'])
                    S.op('act', lambda e: e.activation(out=ss[:], in_=ss[:], func=AF.Sqrt, bias=epsc[0:64, 0:1], scale=1.0 / 128.0), reads=['gss', 'epsc'], writes=['gss'])
                    S.op('dve', lambda e: e.reciprocal(out=ss[:], in_=ss[:]), reads=['gss'], writes=['gss'])
                    S.op('dve', lambda e: e.tensor_tensor(out=oacc[:], in0=oacc[:], in1=ss[:].unsqueeze(2).to_broadcast([64, NLCH, 128]), op=ALU.mult),
                         reads=allo + ['gss'], writes=allo)
                    S.op('dve', lambda e, hd=hd: e.tensor_tensor(out=oacc[:], in0=oacc[:], in1=hgn[:, hd * 128:(hd + 1) * 128].unsqueeze(1).to_broadcast([64, NLCH, 128]), op=ALU.mult),
                         reads=allo + ['hgn'], writes=allo)
                    S.op('dve', lambda e: e.tensor_tensor(out=oacc[:], in0=oacc[:], in1=gs[:], op=ALU.mult), reads=allo + ['ggs'], writes=allo)
                    transpose_chunks_to_mixT(oacc, allo, hd)
                    S.barrier()
            S.barrier()

        with ExitStack() as m0:
            mln = sb(m0, "mln", [64, 512]); convw = sb(m0, "convw", [128, 9, 8]); convb = sb(m0, "convb", [128, 8])
            gateb = sb(m0, "gateb", [8, 2]); sel8 = sb(m0, "sel8", [8, 8]); dirm = sb(m0, "dirm", [8, 2])
            S.dma('sp', lambda e: e.dma_start(out=mln[:], in_=mln_d[:, :]), writes=['mln'])
            S.dma('sp', lambda e: e.dma_start(out=convw[:], in_=convw_d[:, :, :]), writes=['convw'])
            S.dma('sp', lambda e: e.dma_start(out=convb[:], in_=convb_d[:, :]), writes=['convb'])
            S.dma('sp', lambda e: e.dma_start(out=gateb[:], in_=gateb_d[:, :]), writes=['gateb'])
            S.dma('sp', lambda e: e.dma_start(out=sel8[:], in_=sel8_d[:, :]), writes=['sel8'])
            S.dma('sp', lambda e: e.dma_start(out=dirm[:], in_=dirm_d[:, :]), writes=['dirm'])
            RUU = sb(m0, "RUU", [64, NCH, 24]); dchunk = sb(m0, "dchunk", [128, 8, NCH])
            with ExitStack() as gp:
                wgi = load_w(gp, "mwgi", 4608, 8); wgf = load_w(gp, "mwgf", 4616, 8)
                LI = sb(gp, "LI", [8, NTOK]); LF = sb(gp, "LF", [8, NTOK]); Af = sb(gp, "Af", [8, NTOK]); Ab = sb(gp, "Ab", [8, NTOK]); Aa = sb(gp, "Aa", [8, NTOK])
                R = [sb(gp, "Rr%d" % i, [8, NTOK]) for i in range(3)]
                bd = sb(gp, "bd", [8, 8, NCH])
                for (t0, nt) in BLKS:
                    proj_fm_block(wgi, "mwgi", 8, 0, t0, nt)
                    S.op('act', lambda e, t0=t0, nt=nt: e.copy(out=LI[:, t0:t0 + nt], in_=PB[0][0:8, 0:nt]), reads=['pb0'], writes=['LI'])
                    proj_fm_block(wgf, "mwgf", 8, 1, t0, nt)
                    S.op('act', lambda e, t0=t0, nt=nt: e.copy(out=LF[:, t0:t0 + nt], in_=PB[1][0:8, 0:nt]), reads=['pb1'], writes=['LF'])
                S.op('dve', lambda e: e.tensor_scalar_add(out=LI[:], in0=LI[:], scalar1=gateb[:, 0:1]), reads=['LI', 'gateb'], writes=['LI'])
                S.op('act', lambda e: e.activation(out=LF[:], in_=LF[:], func=AF.Sigmoid, bias=gateb[:, 1:2], scale=1.0), reads=['LF', 'gateb'], writes=['LF'])
                S.op('act', lambda e: e.activation(out=LF[:], in_=LF[:], func=AF.Ln), reads=['LF'], writes=['LF'])
                for (t0, nt) in BLKS:
                    S.op('dve', lambda e, t0=t0, nt=nt: e.tensor_tensor_scan(out=Af[:, t0:t0 + nt], data0=rmask[0:8, 0:nt], data1=LF[:, t0:t0 + nt], initial=0.0, op0=ALU.mult, op1=ALU.add),
                         reads=['LF', 'rmask'], writes=['Af'])
                A3 = Af[:].rearrange("p (n c) -> p n c", c=64)
                tot = A3[:, :, 63:64]
                S.op('dve', lambda e: e.tensor_tensor(out=Ab[:].rearrange("p (n c) -> p n c", c=64), in0=tot.to_broadcast([8, NCH, 64]), in1=A3, op=ALU.subtract), reads=['Af'], writes=['Ab'])
                S.op('dve', lambda e: e.tensor_tensor(out=Ab[:], in0=Ab[:], in1=LF[:], op=ALU.add), reads=['Ab', 'LF'], writes=['Ab'])
                S.op('dve', lambda e: e.tensor_scalar_mul(out=Aa[:], in0=Af[:], scalar1=dirm[:, 0:1]), reads=['Af', 'dirm'], writes=['Aa'])
                S.op('dve', lambda e: e.scalar_tensor_tensor(out=Aa[:], in0=Ab[:], scalar=dirm[:, 1:2], in1=Aa[:], op0=ALU.mult, op1=ALU.add), reads=['Ab', 'dirm', 'Aa'], writes=['Aa'])
                S.op('act', lambda e: e.activation(out=R[0][:], in_=Aa[:], func=AF.Exp), reads=['Aa'], writes=['Rr0'])
                S.op('dve', lambda e: e.tensor_tensor(out=Ab[:], in0=LI[:], in1=Aa[:], op=ALU.subtract), reads=['LI', 'Aa', 'Ab'], writes=['Ab'])
                S.op('act', lambda e: e.activation(out=R[1][:], in_=Ab[:], func=AF.Exp), reads=['Ab'], writes=['Rr1'])
                S.op('dve', lambda e: e.tensor_tensor(out=Aa[:].rearrange("p (n c) -> p n c", c=64), in0=Ab[:].rearrange("p (n c) -> p n c", c=64), in1=tot.to_broadcast([8, NCH, 64]), op=ALU.add),
                     reads=['Ab', 'Af', 'Aa', 'Rr0'], writes=['Aa'])
                S.op('act', lambda e: e.activation(out=R[2][:], in_=Aa[:], func=AF.Exp), reads=['Aa'], writes=['Rr2'])
                for half in range(2):
                    for j in range(18):
                        n = half * 18 + j
                        for q in range(3):
                            S.op('pe', lambda e, n=n, j=j, q=q: e.transpose(PB[2][0:64, j * 24 + q * 8:j * 24 + q * 8 + 8], R[q][:, n * 64:(n + 1) * 64], ident[0:8, 0:8]),
                                 reads=['Rr%d' % q, 'ident'], writes=['pb2'], skip_self=True)
                    S.op('act', lambda e, half=half: e.copy(out=RUU[:, half * 18:(half + 1) * 18, :], in_=PB[2][0:64, 0:432].rearrange("p (j c) -> p j c", c=24)),
                         reads=['pb2'], writes=['RUU'])
                S.op('dve', lambda e: e.tensor_tensor(out=bd[:], in0=tot.rearrange("p n c -> p c n").to_broadcast([8, 8, NCH]), in1=sel8[:].unsqueeze(2).to_broadcast([8, 8, NCH]), op=ALU.mult),
                     reads=['Af', 'sel8'], writes=['bd'])
                S.op('pe', lambda e: e.matmul(PB[3][:, 0:288], lhsT=ones[0:8, :], rhs=bd[:].rearrange("p a n -> p (a n)"), start=True, stop=True), reads=['ones', 'bd'], writes=['pb3'], skip_self=True)
                S.op('act', lambda e: e.activation(out=dchunk[:].rearrange("p a n -> p (a n)"), in_=PB[3][:, 0:288], func=AF.Exp), reads=['pb3'], writes=['dchunk'])
                S.barrier()
            qc = sb(m0, "mqc", [128, NTOK], BF16); kc_ = sb(m0, "mkc", [128, NTOK], BF16); kTM = sb(m0, "mkTM", [64, NCH, 128], BF16)
            raw = sb(m0, "mraw", [128, NTOK]); acc = sb(m0, "macc", [128, NTOK])
            vext = sb(m0, "mvext", [64, NCH, 132], BF16); vi = sb(m0, "mvi", [64, NCH, 132], BF16); vu = sb(m0, "mvu", [64, NCH, 132], BF16)
            og = sb(m0, "mog", [64, NLCH, 128], BF16); oext = sb(m0, "moext", [64, NLCH, 132]); obuf = sb(m0, "mobuf", [64, NLCH, 128])
            den = sb(m0, "mden", [64, NLCH]); mu = sb(m0, "mmu", [64, NLCH]); m2 = sb(m0, "mm2", [64, NLCH])
            S.op('dve', lambda e: e.memset(vext[:], 1.0), writes=['mvext'])
            for hd in range(4):
                with ExitStack() as hs:
                    wq_ = load_w(hs, "mwq", 2560 + hd * 128, 128); wk_ = load_w(hs, "mwk", 3072 + hd * 128, 128)
                    wv_ = load_w(hs, "mwv", 3584 + hd * 128, 128); wo_ = load_w(hs, "mwo", 4096 + hd * 128, 128)
                    prep_tables(64)
                    for g in range(9):
                        bk = 3 + g % 2
                        proj_tm_group(wv_, "mwv", 128, bk, g * 4, 4)
                        S.op('act', lambda e, g=g, bk=bk: e.copy(out=vext[:, g * 4:(g + 1) * 4, 0:128], in_=PB[bk][0:64, :].rearrange("p (j c) -> p j c", c=128)),
                             reads=['pb%d' % bk], writes=['mvext'])
                    for g in range(8):
                        bk = 3 + (g + 1) % 2
                        proj_tm_group(wo_, "mwo", 128, bk, 4 + g * 4, 4)
                        S.op('act', lambda e, g=g, bk=bk: e.activation(out=og[:, g * 4:(g + 1) * 4, :], in_=PB[bk][0:64, :].rearrange("p (j c) -> p j c", c=128), func=AF.Sigmoid),
                             reads=['pb%d' % bk], writes=['mog'])
                    for qi, (wb, wn, dstb) in enumerate(((wq_, "mwq", qc), (wk_, "mwk", kc_))):
                        chn = qi * 4 + hd
                        for bi, (t0, nt) in enumerate(BLKS):
                            proj_fm_block(wb, wn, 128, bi % 2, t0, nt)
                            S.op('act', lambda e, t0=t0, nt=nt, bi=bi: e.copy(out=raw[:, t0:t0 + nt], in_=PB[bi % 2][:, 0:nt]), reads=['pb%d' % (bi % 2)], writes=['mraw'])
                        S.op('dve', lambda e, chn=chn: e.tensor_scalar(out=acc[:, 0:256], in0=raw[:, 0:256], scalar1=convw[:, 4, chn:chn + 1], scalar2=convb[:, chn:chn + 1], op0=ALU.mult, op1=ALU.add),
                             reads=['mraw', 'convw', 'convb'], writes=['macc'])
                        S.op('dve', lambda e, chn=chn: e.scalar_tensor_tensor(out=acc[:, 1:256], in0=raw[:, 0:255], scalar=convw[:, 3, chn:chn + 1], in1=acc[:, 1:256], op0=ALU.mult, op1=ALU.add),
                             reads=['mraw', 'convw', 'macc'], writes=['macc'])
                        S.op('dve', lambda e, chn=chn: e.scalar_tensor_tensor(out=acc[:, 0:255], in0=raw[:, 1:256], scalar=convw[:, 5, chn:chn + 1], in1=acc[:, 0:255], op0=ALU.mult, op1=ALU.add),
                             reads=['mraw', 'convw', 'macc'], writes=['macc'])
                        X = raw[:, 256:NTOK].rearrange("p (r c) -> p r c", c=64); Y = acc[:, 256:NTOK].rearrange("p (r c) -> p r c", c=64)
                        S.op('dve', lambda e, chn=chn: e.tensor_scalar(out=acc[:, 256:NTOK], in0=raw[:, 256:NTOK], scalar1=convw[:, 4, chn:chn + 1], scalar2=convb[:, chn:chn + 1], op0=ALU.mult, op1=ALU.add),
                             reads=['mraw', 'convw', 'convb', 'macc'], writes=['macc'])
                        for ky in range(3):
                            for kx in range(3):
                                if ky == 1 and kx == 1:
                                    continue
                                dy = ky - 1; dx = kx - 1
                                r0 = max(0, -dy); r1 = 32 - max(0, dy); c0 = max(0, -dx); c1 = 64 - max(0, dx)
                                S.op('dve', lambda e, chn=chn, ky=ky, kx=kx, r0=r0, r1=r1, c0=c0, c1=c1, dy=dy, dx=dx: e.scalar_tensor_tensor(
                                    out=Y[:, r0:r1, c0:c1], in0=X[:, r0 + dy:r1 + dy, c0 + dx:c1 + dx], scalar=convw[:, ky * 3 + kx, chn:chn + 1], in1=Y[:, r0:r1, c0:c1],
                                    op0=ALU.mult, op1=ALU.add), reads=['mraw', 'convw', 'macc'], writes=['macc'])
                        S.op('act', lambda e: e.activation(out=acc[:], in_=acc[:], func=AF.Silu), reads=['macc'], writes=['macc'])
                        if qi == 0:
                            S.op('dve', lambda e: e.tensor_copy(out=qc[:], in_=acc[:]), reads=['macc'], writes=['mqc'])
                        else:
                            S.op('dve', lambda e: e.tensor_scalar_mul(out=kc_[:], in0=acc[:], scalar1=128.0 ** -0.5), reads=['macc'], writes=['mkc'])
                    pbb = PB[3][:].bitcast(BF16)
                    for g in range(5):
                        nck = 8 if g < 4 else 4
                        for j in range(nck):
                            n = g * 8 + j
                            S.op('pe', lambda e, j=j, n=n: e.transpose(pbb[0:64, j * 128:(j + 1) * 128], kc_[:, n * 64:(n + 1) * 64], identb[:]),
                                 reads=['mkc', 'identb'], writes=['pb3'], skip_self=True)
                        S.op('act', lambda e, g=g, nck=nck: e.copy(out=kTM[:, g * 8:g * 8 + nck, :], in_=pbb[0:64, 0:nck * 128].rearrange("p (j c) -> p j c", c=128)),
                             reads=['pb3'], writes=['mkTM'])
                    for d in range(2):
                        row = d * 4 + hd
                        S.op('dve', lambda e, row=row: e.tensor_tensor(out=vi[:], in0=vext[:], in1=RUU[:, :, 8 + row:9 + row].to_broadcast([64, NCH, 132]), op=ALU.mult),
                             reads=['mvext', 'RUU'], writes=['mvi'])
                        S.op('dve', lambda e, row=row: e.tensor_tensor(out=vu[:], in0=vext[:], in1=RUU[:, :, 16 + row:17 + row].to_broadcast([64, NCH, 132]), op=ALU.mult),
                             reads=['mvext', 'RUU'], writes=['mvu'])

                        def evac(nl, n, po, pok, row=row):
                            S.op('act', lambda e: e.copy(out=oext[:, nl, 0:129], in_=po), reads=[pok], writes=K('moext', nl))
                        chunk_loop(d, kc_, qc, vi, qc, kTM, vu, lambda n, row=row: dchunk[:, row, n:n + 1], 129, evac,
                                   ['mqc', 'mkc', 'mvi', 'mvu', 'mkTM'], 'dchunk')
                        allx = K('moext', 0, NLCH)
                        S.op('dve', lambda e, row=row: e.tensor_tensor(out=oext[:, :, 0:129], in0=oext[:, :, 0:129], in1=RUU[:, 4:NCH, row:row + 1].to_broadcast([64, NLCH, 129]), op=ALU.mult),
                             reads=allx + ['RUU'], writes=allx)
                        S.op('act', lambda e: e.activation(out=den[:], in_=oext[:, :, 128], func=AF.Abs), reads=allx, writes=['mden'])
                        S.op('dve', lambda e: e.tensor_scalar_max(out=den[:], in0=den[:], scalar1=1.0), reads=['mden'], writes=['mden'])
                        S.op('dve', lambda e: e.reciprocal(out=den[:], in_=den[:]), reads=['mden'], writes=['mden'])
                        if d == 0:
                            S.op('dve', lambda e: e.tensor_tensor(out=obuf[:], in0=oext[:, :, 0:128], in1=den[:].unsqueeze(2).to_broadcast([64, NLCH, 128]), op=ALU.mult),
                                 reads=allx + ['mden'], writes=['mobuf'])
                        else:
                            S.op('dve', lambda e: e.tensor_tensor(out=oext[:, :, 0:128], in0=oext[:, :, 0:128], in1=den[:].unsqueeze(2).to_broadcast([64, NLCH, 128]), op=ALU.mult),
                                 reads=allx + ['mden'], writes=allx)
                            S.op('dve', lambda e: e.tensor_tensor(out=obuf[:], in0=obuf[:], in1=oext[:, :, 0:128], op=ALU.add), reads=allx + ['mobuf'], writes=['mobuf'])
                    allx = K('moext', 0, NLCH)
                    S.op('dve', lambda e: e.tensor_reduce(out=mu[:], in_=obuf[:], axis=AX.X, op=ALU.add), reads=['mobuf'], writes=['mmu'])
                    S.op('dve', lambda e: e.tensor_scalar_mul(out=mu[:], in0=mu[:], scalar1=1.0 / 128.0), reads=['mmu'], writes=['mmu'])
                    S.op('dve', lambda e: e.tensor_tensor(out=obuf[:], in0=obuf[:], in1=mu[:].unsqueeze(2).to_broadcast([64, NLCH, 128]), op=ALU.subtract), reads=['mobuf', 'mmu'], writes=['mobuf'])
                    S.op('dve', lambda e: e.tensor_tensor(out=oext[:, :, 0:128], in0=obuf[:], in1=obuf[:], op=ALU.mult), reads=['mobuf'] + allx, writes=allx)
                    S.op('dve', lambda e: e.tensor_reduce(out=m2[:], in_=oext[:, :, 0:128], axis=AX.X, op=ALU.add), reads=allx, writes=['mm2'])
                    S.op('act', lambda e: e.activation(out=m2[:], in_=m2[:], func=AF.Sqrt, bias=epsc[0:64, 0:1], scale=1.0 / 128.0), reads=['mm2', 'epsc'], writes=['mm2'])
                    S.op('dve', lambda e: e.reciprocal(out=m2[:], in_=m2[:]), reads=['mm2'], writes=['mm2'])
                    S.op('dve', lambda e: e.tensor_tensor(out=obuf[:], in0=obuf[:], in1=m2[:].unsqueeze(2).to_broadcast([64, NLCH, 128]), op=ALU.mult), reads=['mobuf', 'mm2'], writes=['mobuf'])
                    S.op('dve', lambda e, hd=hd: e.tensor_tensor(out=obuf[:], in0=obuf[:], in1=mln[:, hd * 128:(hd + 1) * 128].unsqueeze(1).to_broadcast([64, NLCH, 128]), op=ALU.mult),
                         reads=['mobuf', 'mln'], writes=['mobuf'])
                    S.op('dve', lambda e: e.tensor_tensor(out=obuf[:], in0=obuf[:], in1=og[:], op=ALU.mult), reads=['mobuf', 'mog'], writes=['mobuf'])
                    transpose_chunks_to_mixT(obuf, ['mobuf'], 4 + hd)
                    S.barrier()
            S.barrier()

        if debug == 'mix':
            with ExitStack() as dd:
                mf = sb(dd, "mf", [128, NLAT])
                for h in range(8):
                    S.op('dve', lambda e, h=h: e.tensor_copy(out=mf[:], in_=mixT[:, h, :]), reads=K('mixT', h) + ['mf'], writes=['mf'])
                    S.dma('sp', lambda e, h=h: e.dma_start(out=dbg_d[h * 128:(h + 1) * 128, :], in_=mf[:]), reads=['mf'], writes=['dbg'])
                S.wait_all('sp', ['dbg']); S.barrier()
            scB.close(); scA.close()
            return nc
        prep_tables(512)
        S.barrier()
        scB.close()
        x1s = nc.dram_tensor("x1s", [NLAT, 1024], F32, kind="Internal").ap()

        def bcast_tile(dst, dkey, c0, dg):
            for ch in range(8):
                S.op('dve', lambda e, ch=ch: e.tensor_scalar_mul(out=dg[:], in0=ident[:], scalar1=modv[:, c0 + ch, 0:1]), reads=['ident', 'modv', 'dg'], writes=['dg'])
                bank = ch // 4
                S.op('pe', lambda e, ch=ch, bank=bank: e.matmul(PB[bank][:, (ch % 4) * 128:(ch % 4 + 1) * 128], lhsT=ones[:], rhs=dg[:], start=True, stop=True),
                     reads=['ones', 'dg'], writes=['pb%d' % bank], skip_self=True)
            S.op('act', lambda e: e.copy(out=dst[:, 0:512], in_=PB[0][:, :]), reads=['pb0'], writes=[dkey])
            S.op('act', lambda e: e.copy(out=dst[:, 512:1024], in_=PB[1][:, :]), reads=['pb1'], writes=[dkey])

        with ExitStack() as p3:
            bct = {}
            for nm in ("g1b", "ln1g", "ln1b"):
                bct[nm] = sb(p3, nm, [128, 1024])
            for nm, dd in (("ln1g", ln1g_d), ("ln1b", ln1b_d)):
                S.dma('sp', lambda e, nm=nm, dd=dd: e.dma_start(out=bct[nm][:], in_=dd[:, :]), writes=[nm])
            dg = sb(p3, "dg", [128, 128])
            bcast_tile(bct["g1b"], "g1b", 16, dg)
            wo_b = sb(p3, "wout", [128, 8, 1024], BF16); wos = sb(p3, "wos", [128, 1024])
            for kc in range(8):
                S.dma('sp', lambda e, kc=kc: e.dma_start(out=wos[:], in_=wout_d[:, kc, :]), writes=['wos'])
                S.op('pool', lambda e, kc=kc: e.tensor_copy(out=wo_b[:, kc, :], in_=wos[:]), reads=['wos'], writes=['wout'])
            xt = [sb(p3, "x3t%d" % i, [128, 1024]) for i in range(2)]
            t1 = [sb(p3, "t1_%d" % i, [128, 1024]) for i in range(2)]
            st = [sb(p3, "st3%d" % i, [128, 2, 6]) for i in range(2)]
            mv = [sb(p3, "mv3%d" % i, [128, 2]) for i in range(2)]
            rs = [sb(p3, "rs3%d" % i, [128, 1]) for i in range(2)]
            for t in range(16):
                b = t % 2
                S.dma('sp' if b == 0 else 'pool', lambda e, t=t, b=b: e.dma_start(out=xt[b][:], in_=xs[256 + t * 128:256 + (t + 1) * 128, :]), writes=['x3t%d' % b])
                for half in range(2):
                    bank = 2 * b + half
                    for kc in range(8):
                        S.op('pe', lambda e, kc=kc, t=t, half=half, bank=bank: e.matmul(PB[bank][:, :], lhsT=mixT[:, kc, t * 128:(t + 1) * 128], rhs=wo_b[:, kc, half * 512:(half + 1) * 512],
                                                                                      start=(kc == 0), stop=(kc == 7)),
                             reads=K('mixT', kc) + ['wout'], writes=['pb%d' % bank], skip_self=True)
                    S.op('dve', lambda e, b=b, half=half, bank=bank: e.tensor_tensor(out=t1[b][:, half * 512:(half + 1) * 512], in0=PB[bank][:, :], in1=bct["g1b"][:, half * 512:(half + 1) * 512], op=ALU.mult),
                         reads=['pb%d' % bank, 'g1b'], writes=['t1_%d' % b])
                S.op('dve', lambda e, b=b: e.scalar_tensor_tensor(out=t1[b][:], in0=xt[b][:], scalar=ALPHA, in1=t1[b][:], op0=ALU.mult, op1=ALU.add),
                     reads=['x3t%d' % b, 't1_%d' % b], writes=['t1_%d' % b])
                layer_norm_rows(st[b], mv[b][:], rs[b][:], t1[b][:], t1[b][:], 't1_%d' % b, 't1_%d' % b, 'p3%d' % b)
                S.op('dve', lambda e, b=b: e.tensor_tensor(out=t1[b][:], in0=t1[b][:], in1=bct["ln1g"][:], op=ALU.mult), reads=['t1_%d' % b, 'ln1g'], writes=['t1_%d' % b])
                S.op('dve', lambda e, b=b, t=t: e.tensor_tensor(out=t1[b][:], in0=t1[b][:], in1=bct["ln1b"][:], op=ALU.add), reads=['t1_%d' % b, 'ln1b'], writes=['t1_%d' % b])
                S.dma('sp', lambda e, t=t, b=b: e.dma_start(out=x1s[t * 128:(t + 1) * 128, :], in_=t1[b][:]), reads=['t1_%d' % b], writes=K('x1_', t))
                if debug == 'x1':
                    S.dma('sp', lambda e, t=t, b=b: e.dma_start(out=dbg_d[t * 128:(t + 1) * 128, :], in_=t1[b][:]), reads=['t1_%d' % b], writes=['dbg'])
            S.barrier()
        scA.close()

        with ExitStack() as p4:
            bct = {}
            for nm in ("g2b", "sc2b", "sh2b", "ln2g", "ln2b"):
                bct[nm] = sb(p4, nm, [128, 1024])
            for nm, dd in (("ln2g", ln2g_d), ("ln2b", ln2b_d)):
                S.dma('sp', lambda e, nm=nm, dd=dd: e.dma_start(out=bct[nm][:], in_=dd[:, :]), writes=[nm])
            dg = sb(p4, "dg4", [128, 128])
            bcast_tile(bct["g2b"], "g2b", 40, dg); bcast_tile(bct["sc2b"], "sc2b", 32, dg); bcast_tile(bct["sh2b"], "sh2b", 24, dg)
            x1t = [sb(p4, "x1t%d" % i, [128, 1024]) for i in range(2)]
            wqb = sb(p4, "wqb", [128, 8, 2048], BF16)
            keysT = sb(p4, "keysT", [128, 16, 128], BF16)
            with ExitStack() as tmp:
                stg = sb(tmp, "wq_stg", [128, 2048])
                for kc in range(8):
                    S.dma('sp', lambda e, kc=kc: e.dma_start(out=stg[:], in_=wq_d[:, kc, :]), writes=['wq_stg'])
                    S.op('pool', lambda e, kc=kc: e.tensor_copy(out=wqb[:, kc, :], in_=stg[:]), reads=['wq_stg'], writes=['wqb'])
                kst = sb(tmp, "kst", [128, 16, 128])
                S.dma('sp', lambda e: e.dma_start(out=kst[:], in_=keysT_d[:, :, :]), writes=['kst'])
                S.op('pool', lambda e: e.tensor_copy(out=keysT[:], in_=kst[:]), reads=['kst'], writes=['keysT'])
                S.barrier()
            NG = 8
            uvb = [sb(p4, "uvb%d" % i, [128, 2048], BF16) for i in range(NG)]
            gl = sb(p4, "gl", [128, 128])
            dgk = [sb(p4, "dgk%d" % i, [128, 128], BF16) for i in range(4)]
            h2 = sb(p4, "h2", [128, 1024]); h2b = [sb(p4, "h2b%d" % i, [128, 1024], BF16) for i in range(2)]
            h2T = sb(p4, "h2T", [128, 8, 128], BF16); qT = sb(p4, "qT", [128, 16, 128], BF16)
            sc = sb(p4, "sc", [128, 16, 128]); scw = sb(p4, "scw", [128, 16, 128])
            top = sb(p4, "top", [128, 16, 16]); topi = sb(p4, "topi", [128, 16, 16], U32); topf = sb(p4, "topf", [128, 16, 16])
            cand = sb(p4, "cand", [128, 8, 256]); candw = sb(p4, "candw", [128, 8, 256]); cidx = sb(p4, "cidx", [128, 8, 256])
            best = sb(p4, "best", [128, 8, 16]); idxf = sb(p4, "idxf", [128, 128])
            gate = [sb(p4, "gate%d" % i, [128, 128]) for i in range(2)]
            idxi = [sb(p4, "idxi%d" % i, [128, 128], I32) for i in range(2)]
            wgt = [sb(p4, "wgt%d" % i, [128, 128]) for i in range(2)]
            dots = sb(p4, "dots", [128, 128]); junk = sb(p4, "junk", [128, 1024], BF16); junk2 = sb(p4, "junk2", [128, 256])
            zs = sb(p4, "zs", [128, 8]); nmx = sb(p4, "nmx", [128, 8])
            st = sb(p4, "st4", [128, 2, 6]); mv = sb(p4, "mv4", [128, 2]); rs = sb(p4, "rs4", [128, 1])
            fin = sb(p4, "fin", [128, 1024]); yb = sb(p4, "yb", [128, 1024])
            NT4 = 16 if debug != 'x1' else 0

            def prologue(t):
                p = t % 2
                xb_ = x1t[p]; x1k = 'x1t%d' % p
                S.dma('sp', lambda e: e.dma_start(out=xb_[:], in_=x1s[t * 128:(t + 1) * 128, :]), reads=K('x1_', t), writes=[x1k])
                S.op('dve', lambda e: e.memset(idxf[:], 0.0), writes=['idxf'])
                S.op('dve', lambda e: e.memset(zs[:], 0.0), writes=['zs'])
                layer_norm_rows(st, mv[:], rs[:], xb_[:], h2[:], x1k, 'h2', 'p4')
                S.op('dve', lambda e: e.tensor_tensor(out=h2[:], in0=h2[:], in1=bct["sc2b"][:], op=ALU.mult), reads=['h2', 'sc2b'], writes=['h2'])
                S.op('dve', lambda e: e.tensor_tensor(out=h2[:], in0=h2[:], in1=bct["sh2b"][:], op=ALU.add), reads=['h2', 'sh2b'], writes=['h2'])
                S.op('act', lambda e: e.copy(out=h2b[p][:], in_=h2[:]), reads=['h2'], writes=['h2b%d' % p])
                yield
                for half in range(2):
                    for c4 in range(4):
                        ch = half * 4 + c4
                        S.op('pe', lambda e, ch=ch, c4=c4, half=half: e.transpose(PB[half][:, c4 * 128:(c4 + 1) * 128], h2[:, ch * 128:(ch + 1) * 128], ident[:]),
                             reads=['h2', 'ident'], writes=['pb%d' % half], skip_self=True)
                    yield
                    S.op('act', lambda e, half=half: e.copy(out=h2T[:, half * 4:(half + 1) * 4, :], in_=PB[half][:, :].rearrange("p (j c) -> p j c", c=128)), reads=['pb%d' % half], writes=['h2T'])
                for g in range(4):
                    for c4 in range(4):
                        c = g * 4 + c4
                        for kc in range(8):
                            S.op('pe', lambda e, c=c, c4=c4, kc=kc, g=g: e.matmul(PB[2 + g][:, c4 * 128:(c4 + 1) * 128], lhsT=wqb[:, kc, c * 128:(c + 1) * 128], rhs=h2T[:, kc, :],
                                                                                start=(kc == 0), stop=(kc == 7)), reads=['wqb', 'h2T'], writes=['pb%d' % (2 + g)], skip_self=True)
                    yield
                    S.op('act' if g % 2 == 0 else 'dve', lambda e, g=g: (e.copy if g % 2 == 0 else e.tensor_copy)(out=qT[:, g * 4:(g + 1) * 4, :], in_=PB[2 + g][:, :].rearrange("p (j c) -> p j c", c=128)),
                         reads=['pb%d' % (2 + g)], writes=['qT'])
                for g in range(4):
                    for c4 in range(4):
                        c = g * 4 + c4
                        S.op('pe', lambda e, c=c, c4=c4, g=g: e.matmul(PB[2 + g][:, c4 * 128:(c4 + 1) * 128], lhsT=qT[:, c, :], rhs=keysT[:, c, :], start=True, stop=True),
                             reads=['qT', 'keysT'], writes=['pb%d' % (2 + g)], skip_self=True)
                    yield
                    S.op('act' if g % 2 == 0 else 'dve', lambda e, g=g: (e.copy if g % 2 == 0 else e.tensor_copy)(out=sc[:, g * 4:(g + 1) * 4, :], in_=PB[2 + g][:, :].rearrange("p (j c) -> p j c", c=128)),
                         reads=['pb%d' % (2 + g)], writes=['sc'])
                for c in range(16):
                    S.op('dve', lambda e, c=c: e.max(out=top[:, c, 0:8], in_=sc[:, c, :]), reads=['sc'], writes=['top'])
                    S.op('dve', lambda e, c=c: e.max_index(out=topi[:, c, 0:8], in_max=top[:, c, 0:8], in_values=sc[:, c, :]), reads=['sc', 'top'], writes=['topi'])
                    S.op('dve', lambda e, c=c: e.match_replace(out=scw[:, c, :], in_to_replace=top[:, c, 0:8], in_values=sc[:, c, :], imm_value=-1e30), reads=['sc', 'top'], writes=['scw'])
                    S.op('dve', lambda e, c=c: e.max(out=top[:, c, 8:16], in_=scw[:, c, :]), reads=['scw'], writes=['top'])
                    S.op('dve', lambda e, c=c: e.max_index(out=topi[:, c, 8:16], in_max=top[:, c, 8:16], in_values=scw[:, c, :]), reads=['scw', 'top'], writes=['topi'])
                    yield
                S.op('dve', lambda e: e.tensor_copy(out=topf[:], in_=topi[:]), reads=['topi'], writes=['topf'])
                t4 = top[:].rearrange("p (h two) k -> p h two k", two=2); f4 = topf[:].rearrange("p (h two) k -> p h two k", two=2)
                c4v = cand[:].rearrange("p h (a b) -> p h a b", b=16); i4 = cidx[:].rearrange("p h (a b) -> p h a b", b=16)
                for h in range(8):
                    S.op('dve', lambda e, h=h: e.tensor_tensor(out=c4v[:, h, :, :], in0=t4[:, h, 0, :].unsqueeze(2).to_broadcast([128, 16, 16]), in1=t4[:, h, 1, :].unsqueeze(1).to_broadcast([128, 16, 16]), op=ALU.add),
                         reads=['top'], writes=['cand'])
                    S.op('dve', lambda e, h=h: e.scalar_tensor_tensor(out=i4[:, h, :, :], in0=f4[:, h, 0, :].unsqueeze(2).to_broadcast([128, 16, 16]), scalar=128.0, in1=f4[:, h, 1, :].unsqueeze(1).to_broadcast([128, 16, 16]),
                                                                    op0=ALU.mult, op1=ALU.add), reads=['topf'], writes=['cidx'])
                    yield
                for h in range(8):
                    S.op('dve', lambda e, h=h: e.max(out=best[:, h, 0:8], in_=cand[:, h, :]), reads=['cand'], writes=['best'])
                    S.op('dve', lambda e, h=h: e.match_replace(out=candw[:, h, :], in_to_replace=best[:, h, 0:8], in_values=cand[:, h, :], imm_value=-1e30), reads=['cand', 'best'], writes=['candw'])
                    S.op('dve', lambda e, h=h: e.max(out=best[:, h, 8:16], in_=candw[:, h, :]), reads=['candw'], writes=['best'])
                    yield
                for h in range(8):
                    yield
                    for k in range(16):
                        S.op('dve', lambda e, h=h, k=k: e.scalar_tensor_tensor(out=junk2[:], in0=cand[:, h, :], scalar=best[:, h, k:k + 1], in1=cidx[:, h, :], op0=ALU.is_equal, op1=ALU.mult,
                                                                             accum_out=idxf[:, h * 16 + k:h * 16 + k + 1]), reads=['cand', 'best', 'cidx', 'junk2'], writes=['junk2', 'idxf'])
                S.op('dve', lambda e: e.tensor_scalar_min(out=idxf[:], in0=idxf[:], scalar1=16383.0), reads=['idxf'], writes=['idxf'])
                S.op('dve', lambda e: e.tensor_copy(out=idxi[p][:], in_=idxf[:]), reads=['idxf'], writes=['idxi%d' % p])
                S.op('dve', lambda e: e.tensor_scalar_mul(out=nmx[:], in0=best[:, :, 0], scalar1=-1.0), reads=['best'], writes=['nmx'])
                g3 = gate[p][:].rearrange("p (h k) -> p h k", k=16)
                for h in range(8):
                    S.op('act', lambda e, h=h: e.activation(out=g3[:, h, :], in_=best[:, h, :], func=AF.Exp, bias=nmx[:, h:h + 1], scale=1.0, accum_out=zs[:, h:h + 1]),
                         reads=['best', 'nmx'], writes=['gate%d' % p, 'zs'])
                S.op('dve', lambda e: e.reciprocal(out=zs[:], in_=zs[:]), reads=['zs'], writes=['zs'])
                S.op('dve', lambda e: e.tensor_tensor(out=g3, in0=g3, in1=zs[:].unsqueeze(2).to_broadcast([128, 8, 16]), op=ALU.mult), reads=['gate%d' % p, 'zs'], writes=['gate%d' % p])

            def fused(t, gen=None):
                p = t % 2
                for k in range(128):
                    s_ = k % NG; d4 = k % 4
                    dk = 'dots_%d' % k; wk = 'wgt_%d' % k
                    S.dma('pool', lambda e, k=k, s_=s_: e.indirect_dma_start(out=uvb[s_][:], out_offset=None, in_=uv_d[:, :], in_offset=bass.IndirectOffsetOnAxis(ap=idxi[p][:, k:k + 1], axis=0)),
                          reads=['idxi%d' % p, 'tabs'], writes=['uvb%d' % s_])
                    S.op('dve', lambda e, k=k, s_=s_: e.scalar_tensor_tensor(out=junk[:], in0=uvb[s_][:, 0:1024], scalar=1.0, in1=h2b[p][:], op0=ALU.mult, op1=ALU.mult, accum_out=dots[:, k:k + 1]),
                         reads=['uvb%d' % s_, 'h2b%d' % p, 'junk', 'dots0'], writes=['junk', dk])
                    S.op('act', lambda e, k=k: e.activation(out=gl[:, k:k + 1], in_=dots[:, k:k + 1], func=AF.Gelu), reads=[dk], writes=['gl_%d' % k])
                    S.op('act', lambda e, k=k: e.activation(out=wgt[p][:, k:k + 1], in_=gl[:, k:k + 1], func=AF.Identity, bias=0.0, scale=gate[p][:, k:k + 1]),
                         reads=['gl_%d' % k, 'gate%d' % p], writes=[wk])
                    S.op('act', lambda e, k=k, d4=d4: e.activation(out=dgk[d4][:], in_=identb[:], func=AF.Identity, bias=0.0, scale=wgt[p][:, k:k + 1]),
                         reads=['identb', wk], writes=['dgk%d' % d4])
                    for half in range(2):
                        S.op('pe', lambda e, k=k, s_=s_, d4=d4, half=half: e.matmul(PB[6 + half][:, :], lhsT=dgk[d4][:], rhs=uvb[s_][:, 1024 + half * 512:1024 + (half + 1) * 512], start=(k == 0), stop=(k == 127)),
                             reads=['dgk%d' % d4, 'uvb%d' % s_], writes=['pb%d' % (6 + half)], skip_self=True)
                    if gen is not None and k % 2 == 1:
                        next(gen, None)
                if gen is not None:
                    for _ in gen:
                        pass
                xb_ = x1t[p]; x1k = 'x1t%d' % p
                for half in range(2):
                    S.op('dve', lambda e, half=half: e.tensor_tensor(out=yb[:, half * 512:(half + 1) * 512], in0=PB[6 + half][:, :], in1=bct["g2b"][:, half * 512:(half + 1) * 512], op=ALU.mult),
                         reads=['pb%d' % (6 + half), 'g2b', 'yb'], writes=['yb'])
                S.op('dve', lambda e: e.scalar_tensor_tensor(out=fin[:], in0=xb_[:], scalar=ALPHA, in1=yb[:], op0=ALU.mult, op1=ALU.add), reads=[x1k, 'yb', 'fin'], writes=['fin'])
                layer_norm_rows(stf, mvf[:], rsf[:], fin[:], fin[:], 'fin', 'fin', 'p4f')
                S.op('dve', lambda e: e.tensor_tensor(out=fin[:], in0=fin[:], in1=bct["ln2g"][:], op=ALU.mult), reads=['fin', 'ln2g'], writes=['fin'])
                S.op('dve', lambda e: e.tensor_tensor(out=fin[:], in0=fin[:], in1=bct["ln2b"][:], op=ALU.add), reads=['fin', 'ln2b'], writes=['fin'])
                S.dma('sp', lambda e: e.dma_start(out=out_d[t * 128:(t + 1) * 128, :], in_=fin[:]), reads=['fin'], writes=['out'])

            stf = sb(p4, "st4f", [128, 2, 6]); mvf = sb(p4, "mv4f", [128, 2]); rsf = sb(p4, "rs4f", [128, 1])
            S.op('dve', lambda e: e.memset(dots[:], 0.0), writes=['dots0'])
            if NT4:
                for _ in prologue(0):
                    pass
            for t in range(NT4):
                fused(t, prologue(t + 1) if t + 1 < NT4 else None)
            S.wait_all('sp', ['out', 'dbg'])
            S.barrier()
    return nc


def _prep_shared(inp):
    f = np.float32
    sh = {}
    sh["w_mod"] = np.ascontiguousarray(inp["w_mod"][0].reshape(8, 128, 6144).transpose(1, 0, 2))
    sh["b_modT"] = np.ascontiguousarray(inp["b_mod"][0].reshape(48, 128).T)
    sh["w_in"] = np.ascontiguousarray(inp["w_in"][0].reshape(8, 128, 4624).transpose(1, 0, 2))
    sh["lgT"] = np.ascontiguousarray(inp["hg_lb_logits"].reshape(2, 2, 4, 128).transpose(3, 0, 1, 2))
    sh["hgn"] = np.ascontiguousarray(np.broadcast_to(inp["hg_norm_g"][0][None, :], (64, 512)))
    sh["mln"] = np.ascontiguousarray(np.broadcast_to(inp["ml_norm_g"][0][None, :], (64, 512)))
    sh["convw"] = np.ascontiguousarray(inp["ml_conv_w"][0].reshape(9, 8, 128).transpose(2, 0, 1))
    sh["convb"] = np.ascontiguousarray(inp["ml_conv_b"][0].reshape(8, 128).T)
    sh["gateb"] = np.ascontiguousarray(inp["ml_gate_b"][0].reshape(2, 8).T)
    sh["w_out"] = np.ascontiguousarray(inp["w_out"][0].reshape(8, 128, 1024).transpose(1, 0, 2))
    for nm, key in (("ln1g", "ln1_g"), ("ln1b", "ln1_b"), ("ln2g", "ln2_g"), ("ln2b", "ln2_b")):
        sh[nm] = np.ascontiguousarray(np.broadcast_to(inp[key][0][None, :], (128, 1024)))
    sh["wq"] = np.ascontiguousarray(inp["peer_wq"][0].reshape(8, 128, 2048).transpose(1, 0, 2))
    sh["keysT"] = np.ascontiguousarray(inp["peer_keys"][0].reshape(16, 128, 128).transpose(2, 0, 1))
    nexp = 128 if os.environ.get("KDEBUG") else 16384
    sh["pu"] = np.ascontiguousarray(inp["peer_u"][0][:nexp])
    sh["pv"] = np.ascontiguousarray(inp["peer_v"][0][:nexp])
    sh["ident"] = np.eye(128, dtype=f)
    m = np.zeros((64, 2, 64), f)
    s = np.arange(64)[:, None]; c = np.arange(64)[None, :]
    m[:, 0, :] = (s <= c); m[:, 1, :] = (s >= c)
    sh["masks"] = m
    rm = np.ones((128, 512), f); rm[:, ::64] = 0.0
    sh["rmask"] = rm
    sh["sel8"] = np.eye(8, dtype=f)
    dm = np.zeros((8, 2), f); dm[0:4, 0] = 1.0; dm[4:8, 1] = 1.0
    sh["dirm"] = dm
    return {k: np.asarray(v, dtype=f) for k, v in sh.items()}


def kernel(**inputs):
    inp = {k: np.asarray(v) for k, v in inputs.items()}
    debug = os.environ.get("KDEBUG") or None
    nc = build(debug)
    sh = _prep_shared(inp)
    in_maps = []
    for b in range(8):
        m = dict(sh)
        m["xs"] = np.ascontiguousarray(np.concatenate([inp["ctx"][b], inp["x"][b]], axis=0).astype(np.float32))
        m["cT"] = np.ascontiguousarray(np.stack([inp["c"][b], inp["c_ctx"]], axis=-1).reshape(8, 128, 2).transpose(1, 0, 2).astype(np.float32))
        in_maps.append(m)
    res = run_bass_kernel_spmd(nc, in_maps, core_ids=list(range(8)))
    key = "dbg" if debug else "out"
    return np.stack([np.asarray(r[key]) for r in res.results], axis=0).astype(np.float32)
```

```python
import os
import numpy as np
from contextlib import ExitStack
import concourse.bass as bass
import concourse.mybir as mybir
from concourse.bass_utils import run_bass_kernel_spmd

F32 = mybir.dt.float32; BF16 = mybir.dt.bfloat16; I32 = mybir.dt.int32; U32 = mybir.dt.uint32
AF = mybir.ActivationFunctionType; ALU = mybir.AluOpType; AX = mybir.AxisListType

NTOK = 2304; NLAT = 2048; NCH = 36; NLCH = 32
ALPHA = 2.0 ** 0.25
EPS = 1e-6


class Sched:
    NDMA = 32

    def __init__(self, nc, es):
        self.nc = nc
        self.engs = {'pe': nc.tensor, 'act': nc.scalar, 'dve': nc.vector, 'pool': nc.gpsimd, 'sp': nc.sync}
        self.sem = {k: es.enter_context(nc.semaphore("sem_" + k)) for k in self.engs}
        self.cnt = {k: 0 for k in self.engs}
        self.dsem = [es.enter_context(nc.semaphore("dsem%d" % i)) for i in range(self.NDMA)]
        self.dcnt = [0] * self.NDMA
        self.dnext = 0
        self.seen = {k: {} for k in self.engs}
        self.bufs = {}

    def _deps(self, reads, writes):
        deps = []
        for r in reads:
            b = self.bufs.get(r)
            if b and b['w'] is not None:
                deps.append(b['w'])
        for w in writes:
            b = self.bufs.get(w)
            if b:
                if b['w'] is not None:
                    deps.append(b['w'])
                deps.extend(b['r'])
        return deps

    def _wait(self, eng, deps, skip_self=False):
        best = {}
        for (sid, sem, val, owner) in deps:
            if skip_self and owner == eng:
                continue
            if best.get(sid, (None, 0))[1] < val:
                best[sid] = (sem, val)
        for sid, (sem, val) in best.items():
            if self.seen[eng].get(sid, 0) >= val:
                continue
            self.engs[eng].wait_ge(sem, val)
            self.seen[eng][sid] = val

    def _record(self, dep, reads, writes):
        for r in reads:
            b = self.bufs.setdefault(r, {'w': None, 'r': []})
            b['r'] = [d for d in b['r'] if d[0] != dep[0]] + [dep]
        for w in writes:
            self.bufs[w] = {'w': dep, 'r': []}

    @staticmethod
    def _split(keys):
        norm, ps = [], []
        for k in keys:
            if k.startswith('pb') and len(k) > 2 and k[2].isdigit():
                ps.append(k[:3])
            else:
                norm.append(k)
        return norm, ps

    def op(self, eng, fn, reads=(), writes=(), skip_self=False):
        reads, pr = self._split(reads)
        writes, pw = self._split(writes)
        banks = sorted(set(pr + pw))
        deps = self._deps(reads, writes)
        deps += [d for d in self._deps((), banks) if d[3] != eng]
        self._wait(eng, deps, skip_self)
        ins = fn(self.engs[eng])
        self.cnt[eng] += 1
        ins.then_inc(self.sem[eng], 1)
        self._record(('e_' + eng, self.sem[eng], self.cnt[eng], eng), reads, list(writes) + banks)
        return ins

    def dma(self, q, fn, reads=(), writes=()):
        deps = self._deps(reads, writes)
        j = self.dnext
        self.dnext = (self.dnext + 1) % self.NDMA
        if self.dcnt[j] > 0:
            deps = deps + [('d%d' % j, self.dsem[j], self.dcnt[j], 'dma')]
        self._wait(q, deps)
        ins = fn(self.engs[q])
        self.dcnt[j] += 16
        ins.then_inc(self.dsem[j], 16)
        self._record(('d%d' % j, self.dsem[j], self.dcnt[j], 'dma'), reads, writes)

    def wait_all(self, eng, keys):
        deps = []
        for k in keys:
            b = self.bufs.get(k)
            if b:
                if b['w'] is not None:
                    deps.append(b['w'])
                deps.extend(b['r'])
        self._wait(eng, deps)

    def barrier(self):
        for e in self.engs:
            deps = [('e_' + f, self.sem[f], self.cnt[f], f) for f in self.engs if f != e and self.cnt[f] > 0]
            deps += [('d%d' % j, self.dsem[j], self.dcnt[j], 'dma') for j in range(self.NDMA) if self.dcnt[j] > 0]
            self._wait(e, deps)


def K(name, a, b=None):
    if b is None:
        return ["%s%d" % (name, a)]
    return ["%s%d" % (name, i) for i in range(a, b)]


def build(debug=None):
    nc = bass.Bass("TRN2", target_bir_lowering=False)
    D = {}

    def din(name, shape, dt=F32):
        D[name] = nc.dram_tensor(name, shape, dt, kind="ExternalInput").ap()
        return D[name]

    xs = din("xs", [NTOK, 1024]); cT_d = din("cT", [128, 8, 2]); wmod_d = din("w_mod", [128, 8, 6144])
    bmod_d = din("b_modT", [128, 48]); win_d = din("w_in", [128, 8, 4624]); lg_d = din("lgT", [128, 2, 2, 4])
    hgn_d = din("hgn", [64, 512]); mln_d = din("mln", [64, 512]); convw_d = din("convw", [128, 9, 8])
    convb_d = din("convb", [128, 8]); gateb_d = din("gateb", [8, 2]); wout_d = din("w_out", [128, 8, 1024])
    ln1g_d = din("ln1g", [128, 1024]); ln1b_d = din("ln1b", [128, 1024]); ln2g_d = din("ln2g", [128, 1024])
    ln2b_d = din("ln2b", [128, 1024]); wq_d = din("wq", [128, 8, 2048]); keysT_d = din("keysT", [128, 16, 128])
    NEXP = 128 if debug else 16384
    pu_d = din("pu", [NEXP, 1024]); pv_d = din("pv", [NEXP, 1024])
    ident_d = din("ident", [128, 128]); masks_d = din("masks", [64, 2, 64]); rmask_d = din("rmask", [128, 512])
    sel8_d = din("sel8", [8, 8]); dirm_d = din("dirm", [8, 2]); iota_d = din("iota16", [128, 16])
    out_d = nc.dram_tensor("out", [NLAT, 1024], F32, kind="ExternalOutput").ap()
    dbg_d = None
    if debug:
        dbg_d = nc.dram_tensor("dbg", [NLAT, 1024] if debug != 'mix' else [1024, NLAT], F32, kind="ExternalOutput").ap()

    with ExitStack() as es:
        S = Sched(nc, es)

        uid = [0]

        def sb(st, name, shape, dt=F32):
            uid[0] += 1
            return st.enter_context(nc.sbuf_tensor("s%d_%s" % (uid[0], name), shape, dt))

        PB = [es.enter_context(nc.psum_tensor("pb%d" % i, [128, 512], F32)) for i in range(8)]

        ident = sb(es, "ident", [128, 128]); identb = sb(es, "identb", [128, 128], BF16)
        masks = sb(es, "masks", [64, 2, 64]); rmask = sb(es, "rmask", [128, 512])
        ones = sb(es, "ones", [128, 128]); epsc = sb(es, "epsc", [128, 1])
        modv = sb(es, "modv", [128, 48, 2])
        S.dma('sp', lambda e: e.dma_start(out=ident[:], in_=ident_d[:, :]), writes=['ident'])
        S.dma('sp', lambda e: e.dma_start(out=masks[:], in_=masks_d[:, :, :]), writes=['masks'])
        S.dma('sp', lambda e: e.dma_start(out=rmask[:], in_=rmask_d[:, :]), writes=['rmask'])
        S.op('dve', lambda e: e.tensor_copy(out=identb[:], in_=ident[:]), reads=['ident'], writes=['identb'])
        S.op('dve', lambda e: e.memset(ones[:], 1.0), writes=['ones'])
        S.op('dve', lambda e: e.memset(epsc[:], EPS), writes=['epsc'])

        with ExitStack() as p0:
            cT = sb(p0, "cT", [128, 8, 2]); scT = sb(p0, "scT", [128, 8, 2]); bmodT = sb(p0, "bmodT", [128, 48])
            wm = [sb(p0, "wm%d" % i, [128, 6144]) for i in range(2)]
            S.dma('sp', lambda e: e.dma_start(out=cT[:], in_=cT_d[:, :, :]), writes=['cT'])
            S.dma('sp', lambda e: e.dma_start(out=bmodT[:], in_=bmod_d[:, :]), writes=['bmodT'])
            S.op('act', lambda e: e.activation(out=scT[:], in_=cT[:], func=AF.Silu), reads=['cT'], writes=['scT'])
            for kc in range(8):
                w = wm[kc % 2]; wk = 'wm%d' % (kc % 2)
                S.dma('sp' if kc % 2 == 0 else 'pool', lambda e, w=w, kc=kc: e.dma_start(out=w[:], in_=wmod_d[:, kc, :]), writes=[wk])
                for j in range(48):
                    S.op('pe', lambda e, w=w, kc=kc, j=j: e.matmul(PB[kc // 4][:, (kc % 4) * 96 + 2 * j:(kc % 4) * 96 + 2 * j + 2], lhsT=w[:, j * 128:(j + 1) * 128], rhs=scT[:, kc, :],
                                                                 start=True, stop=True),
                         reads=[wk, 'scT'], writes=['pb%d' % (kc // 4)], skip_self=True)
            mflat = modv[:].rearrange("p j n -> p (j n)")
            S.op('dve', lambda e: e.tensor_tensor(out=modv[:], in0=PB[0][:, 0:96].rearrange("p (j n) -> p j n", n=2), in1=bmodT[:].unsqueeze(2).to_broadcast([128, 48, 2]), op=ALU.add),
                 reads=['pb0', 'bmodT'], writes=['modv'])
            for kc in range(1, 8):
                S.op('dve', lambda e, kc=kc: e.tensor_tensor(out=mflat, in0=mflat, in1=PB[kc // 4][:, (kc % 4) * 96:(kc % 4) * 96 + 96], op=ALU.add),
                     reads=['pb%d' % (kc // 4), 'modv'], writes=['modv'])
            S.op('dve', lambda e: e.tensor_scalar_add(out=modv[:, 8:16, :], in0=modv[:, 8:16, :], scalar1=1.0), reads=['modv'], writes=['modv'])
            S.op('dve', lambda e: e.tensor_scalar_add(out=modv[:, 32:40, :], in0=modv[:, 32:40, :], scalar1=1.0), reads=['modv'], writes=['modv'])
            S.barrier()
        if debug == 'p0':
            S.dma('sp', lambda e: e.dma_start(out=dbg_d[0:128, 0:96], in_=modv[:].rearrange("p a b -> p (a b)")), reads=['modv'], writes=['dbg'])
            S.wait_all('sp', ['dbg']); S.barrier()
            return nc

        scA = ExitStack(); scB = ExitStack()
        mixT = sb(scA, "mixT", [128, 8, NLAT], BF16)
        hT = sb(scB, "hT", [128, 8, NTOK], BF16)
        wstg = sb(scB, "wstg", [128, 8, 128])
        tstg = [sb(scB, "tstg%d" % i, [128, 512]) for i in range(2)]
        tbf = [sb(scB, "tbf%d" % i, [128, 512], BF16) for i in range(2)]
        uv_d = nc.dram_tensor("uvbf", [NEXP, 2048], BF16, kind="Internal").ap()
        prep_pos = [0]

        def prep_tables(npieces):
            if debug:
                return
            for _ in range(npieces):
                i = prep_pos[0]
                if i >= 512:
                    return
                prep_pos[0] += 1
                src, coff = (pu_d, 0) if i < 256 else (pv_d, 1024)
                r = (i % 256) // 2; c = (i % 2) * 512; bb = i % 2
                S.dma('sp', lambda e, src=src, r=r, c=c, bb=bb: e.dma_start(out=tstg[bb][:], in_=src[r * 128:(r + 1) * 128, c:c + 512]), writes=['tstg%d' % bb])
                S.op('pool', lambda e, bb=bb: e.tensor_copy(out=tbf[bb][:], in_=tstg[bb][:]), reads=['tstg%d' % bb], writes=['tbf%d' % bb])
                S.dma('pool', lambda e, coff=coff, r=r, c=c, bb=bb: e.dma_start(out=uv_d[r * 128:(r + 1) * 128, coff + c:coff + c + 512], in_=tbf[bb][:]), reads=['tbf%d' % bb], writes=['tabs'])

        def layer_norm_rows(st_ap, mv_ap, rstd_ap, src, dst, skey, dkey, tag):
            S.op('dve', lambda e: e.bn_stats(out=st_ap[:, 0, :], in_=src[:, 0:512]), reads=[skey], writes=[tag + 'st'])
            S.op('dve', lambda e: e.bn_stats(out=st_ap[:, 1, :], in_=src[:, 512:1024]), reads=[skey], writes=[tag + 'st'])
            S.op('dve', lambda e: e.bn_aggr(out=mv_ap, in_=st_ap[:].rearrange("p a b -> p (a b)")), reads=[tag + 'st'], writes=[tag + 'mv'])
            S.op('act', lambda e: e.activation(out=rstd_ap, in_=mv_ap[:, 1:2], func=AF.Sqrt, bias=epsc[:, 0:1], scale=1.0),
                 reads=[tag + 'mv', 'epsc'], writes=[tag + 'rs'])
            S.op('dve', lambda e: e.reciprocal(out=rstd_ap, in_=rstd_ap), reads=[tag + 'rs'], writes=[tag + 'rs'])
            S.op('dve', lambda e: e.tensor_scalar(out=dst, in0=src, scalar1=mv_ap[:, 0:1], scalar2=rstd_ap, op0=ALU.subtract, op1=ALU.mult),
                 reads=[skey, tag + 'mv', tag + 'rs'], writes=[dkey])

        with ExitStack() as p1:
            xt = [sb(p1, "xt%d" % i, [128, 1024]) for i in range(2)]
            xn = [sb(p1, "xn%d" % i, [128, 1024]) for i in range(2)]
            st = [sb(p1, "st%d" % i, [128, 2, 6]) for i in range(2)]
            mv = [sb(p1, "mv%d" % i, [128, 2]) for i in range(2)]
            rs = [sb(p1, "rs%d" % i, [128, 1]) for i in range(2)]
            PSAP = bool(os.environ.get("KPSAP"))
            for t in range(18):
                b = t % 2
                n = 1 if t < 2 else 0
                S.dma('sp' if b == 0 else 'pool', lambda e, t=t, b=b: e.dma_start(out=xt[b][:], in_=xs[t * 128:(t + 1) * 128, :]), writes=['xt%d' % b])
                layer_norm_rows(st[b], mv[b][:], rs[b][:], xt[b][:], xn[b][:], 'xt%d' % b, 'xn%d' % b, 'p1%d' % b)
                for half in range(2):
                    bank = 2 * b + half
                    pk = 'pb%d' % bank
                    for c4 in range(4):
                        ch = half * 4 + c4
                        S.op('pe', lambda e, b=b, ch=ch, c4=c4, bank=bank: e.transpose(PB[bank][:, c4 * 128:(c4 + 1) * 128], xn[b][:, ch * 128:(ch + 1) * 128], ident[:]),
                             reads=['xn%d' % b, 'ident'], writes=[pk], skip_self=True)
                    if not PSAP:
                        S.op('act', lambda e, b=b, half=half, bank=bank: e.copy(out=xt[b][:, half * 512:(half + 1) * 512], in_=PB[bank][:, :]), reads=[pk, 'xt%d' % b], writes=['xt%d' % b])
                    for c4 in range(4):
                        ch = half * 4 + c4
                        dst = hT[:, ch, t * 128:(t + 1) * 128]
                        src = PB[bank][:, c4 * 128:(c4 + 1) * 128] if PSAP else xt[b][:, ch * 128:(ch + 1) * 128]
                        S.op('dve', lambda e, dst=dst, src=src, ch=ch, n=n: e.tensor_scalar(out=dst, in0=src, scalar1=modv[:, 8 + ch, n:n + 1], scalar2=modv[:, ch, n:n + 1],
                                                                                      op0=ALU.mult, op1=ALU.add),
                             reads=([pk] if PSAP else ['xt%d' % b]) + ['modv'], writes=K('hT', t))
            S.barrier()

        if debug == 'p1':
            with ExitStack() as dd:
                hf = sb(dd, "hf", [128, 1024])
                for t in range(16):
                    S.op('dve', lambda e, t=t: e.tensor_copy(out=hf[:].rearrange("p (a b) -> p a b", b=128), in_=hT[:, :, 256 + t * 128:256 + (t + 1) * 128]), reads=K('hT', t + 2) + ['hf'], writes=['hf'])
                    S.dma('sp', lambda e, t=t: e.dma_start(out=dbg_d[t * 128:(t + 1) * 128, :], in_=hf[:]), reads=['hf'], writes=['dbg'])
                S.wait_all('sp', ['dbg']); S.barrier()
            scB.close(); scA.close()
            return nc
        def load_w(stk, name, col0, ncols, src=None):
            src = win_d if src is None else src
            wb = sb(stk, name, [128, 8, ncols], BF16)
            S.dma('sp', lambda e: e.dma_start(out=wstg[:, :, 0:ncols], in_=src[:, :, col0:col0 + ncols]), writes=['wstg'])
            S.op('pool', lambda e: e.tensor_copy(out=wb[:], in_=wstg[:, :, 0:ncols]), reads=['wstg'], writes=[name])
            return wb

        BLKS = [(0, 512), (512, 512), (1024, 512), (1536, 512), (2048, 256)]

        def proj_fm_block(wb, wname, ncols, bank, t0, nt):
            for kc in range(8):
                S.op('pe', lambda e, kc=kc: e.matmul(PB[bank][0:ncols, 0:nt], lhsT=wb[:, kc, :], rhs=hT[:, kc, t0:t0 + nt], start=(kc == 0), stop=(kc == 7)),
                     reads=[wname] + K('hT', t0 // 128, (t0 + nt) // 128), writes=['pb%d' % bank], skip_self=True)

        def proj_tm_group(wb, wname, ncols, bank, c0, ncks):
            for j in range(ncks):
                n = c0 + j
                for kc in range(8):
                    S.op('pe', lambda e, kc=kc, j=j, n=n: e.matmul(PB[bank][0:64, j * ncols:(j + 1) * ncols], lhsT=hT[:, kc, n * 64:(n + 1) * 64], rhs=wb[:, kc, :],
                                                                   start=(kc == 0), stop=(kc == 7)),
                         reads=[wname] + K('hT', n // 2), writes=['pb%d' % bank], skip_self=True)

        S32 = sb(scB, "S32", [128, 2, 132]); Sbf = sb(scB, "Sbf", [128, 2, 132], BF16)
        stm = sb(scB, "stm", [64, 2, 64], BF16)
        usb = sb(scB, "usb", [128, 2, 132])

        def chunk_loop(dirn, KsT, QsT, Vi, QoT, Ku, Vu, dec_ap, W, evac, rk, tagk):
            order = list(range(36)) if dirn == 0 else [3, 2, 1, 0] + list(range(35, 3, -1))
            S.op('dve', lambda e: e.memset(S32[:, 0, :], 0.0), writes=['S32_0'])
            S.op('dve', lambda e: e.memset(Sbf[:, 0, :], 0.0), writes=['Sbf_0'])
            cur = 0
            for idx, n in enumerate(order):
                sl = slice(n * 64, (n + 1) * 64)
                s2 = idx % 2
                if n >= 4:
                    pst = PB[2 + s2][0:64, 0:64]; pstk = 'pb%d' % (2 + s2)
                    po = PB[4 + s2][0:64, 0:W]; pok = 'pb%d' % (4 + s2)
                    S.op('pe', lambda e, pst=pst, sl=sl: e.matmul(pst, lhsT=KsT[:, sl], rhs=QsT[:, sl], start=True, stop=True),
                         reads=rk, writes=[pstk], skip_self=True)
                    S.op('dve', lambda e, pst=pst, s2=s2: e.tensor_tensor(out=stm[:, s2, :], in0=pst, in1=masks[:, dirn, :], op=ALU.mult),
                         reads=[pstk, 'masks'], writes=['stm%d' % s2])
                    S.op('pe', lambda e, po=po, s2=s2, n=n: e.matmul(po, lhsT=stm[:, s2, :], rhs=Vi[:, n, 0:W], start=True, stop=False),
                         reads=['stm%d' % s2] + rk, writes=[pok], skip_self=True)
                    S.op('pe', lambda e, po=po, sl=sl, cur=cur: e.matmul(po, lhsT=QoT[:, sl], rhs=Sbf[:, cur, 0:W], start=False, stop=True),
                         reads=['Sbf_%d' % cur] + rk, writes=[pok], skip_self=True)
                    evac(n - 4, n, po, pok)
                if idx < 35:
                    pu = PB[6 + s2][:, 0:W]; puk = 'pb%d' % (6 + s2)
                    S.op('pe', lambda e, pu=pu, n=n: e.matmul(pu, lhsT=Ku[:, n, :], rhs=Vu[:, n, 0:W], start=True, stop=True),
                         reads=rk, writes=[puk], skip_self=True)
                    nxt = 1 - cur
                    S.op('act', lambda e, pu=pu, s2=s2: e.copy(out=usb[:, s2, 0:W], in_=pu), reads=[puk], writes=['usb%d' % s2])
                    S.op('dve', lambda e, n=n, cur=cur, nxt=nxt, s2=s2: e.scalar_tensor_tensor(out=Sbf[:, nxt, 0:W], in0=S32[:, cur, 0:W], scalar=dec_ap(n), in1=usb[:, s2, 0:W],
                                                                                            op0=ALU.mult, op1=ALU.add),
                         reads=['S32_%d' % cur, 'usb%d' % s2, tagk], writes=['Sbf_%d' % nxt])
                    S.op('dve', lambda e, n=n, cur=cur, nxt=nxt, s2=s2: e.scalar_tensor_tensor(out=S32[:, nxt, 0:W], in0=S32[:, cur, 0:W], scalar=dec_ap(n), in1=usb[:, s2, 0:W],
                                                                                            op0=ALU.mult, op1=ALU.add),
                         reads=['S32_%d' % cur, 'usb%d' % s2, tagk], writes=['S32_%d' % nxt])
                    cur = nxt

        def transpose_chunks_to_mixT(src, skey, head):
            for g in range(4):
                bank = g % 2
                for j in range(8):
                    n = g * 8 + j
                    S.op('pe', lambda e, n=n, j=j, bank=bank: e.transpose(PB[bank][:, j * 64:(j + 1) * 64], src[:, n, :], ident[0:64, 0:64]),
                         reads=list(skey) + ['ident'], writes=['pb%d' % bank], skip_self=True)
                S.op('act', lambda e, g=g, bank=bank: e.copy(out=mixT[:, head, g * 512:(g + 1) * 512], in_=PB[bank][:, :]),
                     reads=['pb%d' % bank], writes=K('mixT', head))

        with ExitStack() as g0:
            lgT = sb(g0, "lgT", [128, 2, 2, 4]); lbT = sb(g0, "lbT", [128, 2, 4]); omlT = sb(g0, "omlT", [128, 2, 4]); nomlT = sb(g0, "nomlT", [128, 2, 4])
            hgn = sb(g0, "hgn", [64, 512])
            S.dma('sp', lambda e: e.dma_start(out=lgT[:], in_=lg_d[:, :, :, :]), writes=['lgT'])
            S.dma('sp', lambda e: e.dma_start(out=hgn[:], in_=hgn_d[:, :]), writes=['hgn'])
            S.op('dve', lambda e: e.tensor_tensor(out=lbT[:], in0=lgT[:, :, 0, :], in1=lgT[:, :, 1, :], op=ALU.subtract), reads=['lgT'], writes=['lbT'])
            S.op('act', lambda e: e.activation(out=lbT[:], in_=lbT[:], func=AF.Sigmoid), reads=['lbT'], writes=['lbT'])
            S.op('dve', lambda e: e.tensor_scalar(out=omlT[:], in0=lbT[:], scalar1=-1.0, scalar2=1.0, op0=ALU.mult, op1=ALU.add), reads=['lbT'], writes=['omlT'])
            S.op('dve', lambda e: e.tensor_scalar_mul(out=nomlT[:], in0=omlT[:], scalar1=-1.0), reads=['omlT'], writes=['nomlT'])
            QsT = [sb(g0, "gQsT%d" % d, [128, NTOK], BF16) for d in range(2)]
            KsT = [sb(g0, "gKsT%d" % d, [128, NTOK], BF16) for d in range(2)]
            QoT = [sb(g0, "gQoT%d" % d, [128, NTOK], BF16) for d in range(2)]
            Ku = [sb(g0, "gKu%d" % d, [64, NCH, 128], BF16) for d in range(2)]
            dec = sb(g0, "gdec", [128, 2, NCH])
            vtm = sb(g0, "gv", [64, NCH, 128], BF16); gs = sb(g0, "ggs", [64, NLCH, 128], BF16); oacc = sb(g0, "goacc", [64, NLCH, 128])
            qf = sb(g0, "gqf", [128, 512]); sg = sb(g0, "gsg", [128, 512]); lf = sb(g0, "glf", [128, 512]); key = sb(g0, "gkey", [128, 512])
            Bc = sb(g0, "gB", [128, 512]); T1 = sb(g0, "gT1", [128, 512]); E1 = sb(g0, "gE1", [128, 512]); khT = sb(g0, "gkhT", [128, 512], BF16)
            sq = sb(g0, "gsq", [64, NLCH, 128], BF16); ss = sb(g0, "gss", [64, NLCH])
            for hd in range(4):
                with ExitStack() as hs:
                    wq_ = load_w(hs, "gwq", 0 + hd * 128, 128); wi_ = load_w(hs, "gwi", 512 + hd * 128, 128); wg_ = load_w(hs, "gwg", 1024 + hd * 128, 128)
                    wf = [load_w(hs, "gwf0", 1536 + hd * 128, 128), load_w(hs, "gwf1", 2048 + hd * 128, 128)]
                    prep_tables(64)
                    for g in range(9):
                        bk = 3 + g % 2
                        proj_tm_group(wi_, "gwi", 128, bk, g * 4, 4)
                        S.op('act', lambda e, g=g, bk=bk: e.copy(out=vtm[:, g * 4:(g + 1) * 4, :], in_=PB[bk][0:64, :].rearrange("p (j c) -> p j c", c=128)),
                             reads=['pb%d' % bk], writes=['gv'])
                    for g in range(8):
                        bk = 3 + (g + 1) % 2
                        proj_tm_group(wg_, "gwg", 128, bk, 4 + g * 4, 4)
                        S.op('act', lambda e, g=g, bk=bk: e.activation(out=gs[:, g * 4:(g + 1) * 4, :], in_=PB[bk][0:64, :].rearrange("p (j c) -> p j c", c=128), func=AF.Silu),
                             reads=['pb%d' % bk], writes=['ggs'])
                    for (t0, nt) in BLKS:
                        nck = nt // 64; c0 = t0 // 64
                        proj_fm_block(wq_, "gwq", 128, 0, t0, nt)
                        S.op('act', lambda e, nt=nt: e.copy(out=qf[:, 0:nt], in_=PB[0][:, 0:nt]), reads=['pb0'], writes=['gqf'])
                        for d in range(2):
                            proj_fm_block(wf[d], "gwf%d" % d, 128, 1 + d, t0, nt)
                            col = d * 4 + hd
                            lbp = lbT[:, d, hd:hd + 1]; omp = omlT[:, d, hd:hd + 1]; nomp = nomlT[:, d, hd:hd + 1]
                            S.op('act', lambda e, d=d, nt=nt: e.activation(out=sg[:, 0:nt], in_=PB[1 + d][:, 0:nt], func=AF.Sigmoid), reads=['pb%d' % (1 + d)], writes=['gsg'])
                            S.op('act', lambda e, nt=nt, lbp=lbp, omp=omp: e.activation(out=lf[:, 0:nt], in_=sg[:, 0:nt], func=AF.Ln, bias=lbp, scale=omp),
                                 reads=['gsg', 'lbT', 'omlT'], writes=['glf'])
                            S.op('dve', lambda e, nt=nt, nomp=nomp, omp=omp: e.tensor_scalar(out=key[:, 0:nt], in0=sg[:, 0:nt], scalar1=nomp, scalar2=omp, op0=ALU.mult, op1=ALU.add),
                                 reads=['gsg', 'omlT', 'nomlT'], writes=['gkey'])
                            S.op('dve', lambda e, nt=nt: e.tensor_tensor_scan(out=Bc[:, 0:nt], data0=rmask[:, 0:nt], data1=lf[:, 0:nt], initial=0.0, op0=ALU.mult, op1=ALU.add),
                                 reads=['glf', 'rmask'], writes=['gB'])
                            B3 = Bc[:, 0:nt].rearrange("p (n c) -> p n c", c=64)
                            T3 = T1[:, 0:nt].rearrange("p (n c) -> p n c", c=64)
                            if d == 1:
                                S.op('dve', lambda e, B3=B3, T3=T3, nck=nck: e.tensor_tensor(out=T3, in0=B3[:, :, 63:64].to_broadcast([128, nck, 64]), in1=B3, op=ALU.subtract),
                                     reads=['gB'], writes=['gT1'])
                                S.op('dve', lambda e, nt=nt: e.tensor_tensor(out=Bc[:, 0:nt], in0=T1[:, 0:nt], in1=lf[:, 0:nt], op=ALU.add), reads=['gT1', 'glf'], writes=['gB'])
                            li = 63 if d == 0 else 0
                            S.op('act', lambda e, B3=B3, li=li, d=d, c0=c0, nck=nck: e.activation(out=dec[:, d, c0:c0 + nck], in_=B3[:, :, li], func=AF.Exp), reads=['gB'], writes=['gdec'])
                            S.op('dve', lambda e, B3=B3, T3=T3, nck=nck: e.tensor_tensor(out=T3, in0=B3, in1=B3[:, :, 32:33].to_broadcast([128, nck, 64]), op=ALU.subtract),
                                 reads=['gB'], writes=['gT1'])
                            S.op('act', lambda e, nt=nt: e.activation(out=E1[:, 0:nt], in_=T1[:, 0:nt], func=AF.Exp), reads=['gT1'], writes=['gE1'])
                            S.op('dve', lambda e, nt=nt, t0=t0, d=d: e.tensor_tensor(out=QsT[d][:, t0:t0 + nt], in0=qf[:, 0:nt], in1=E1[:, 0:nt], op=ALU.mult),
                                 reads=['gqf', 'gE1'], writes=['gQsT%d' % d])
                            S.op('act', lambda e, nt=nt: e.activation(out=E1[:, 0:nt], in_=T1[:, 0:nt], func=AF.Exp, scale=-1.0), reads=['gT1'], writes=['gE1'])
                            S.op('dve', lambda e, nt=nt, t0=t0, d=d: e.tensor_tensor(out=KsT[d][:, t0:t0 + nt], in0=key[:, 0:nt], in1=E1[:, 0:nt], op=ALU.mult),
                                 reads=['gkey', 'gE1'], writes=['gKsT%d' % d])
                            S.op('act', lambda e, nt=nt: e.activation(out=E1[:, 0:nt], in_=Bc[:, 0:nt], func=AF.Exp), reads=['gB'], writes=['gE1'])
                            S.op('dve', lambda e, nt=nt, t0=t0, d=d: e.tensor_tensor(out=QoT[d][:, t0:t0 + nt], in0=qf[:, 0:nt], in1=E1[:, 0:nt], op=ALU.mult),
                                 reads=['gqf', 'gE1'], writes=['gQoT%d' % d])
                            S.op('dve', lambda e, B3=B3, T3=T3, nck=nck, li=li: e.tensor_tensor(out=T3, in0=B3[:, :, li:li + 1].to_broadcast([128, nck, 64]), in1=B3, op=ALU.subtract),
                                 reads=['gB'], writes=['gT1'])
                            S.op('act', lambda e, nt=nt: e.activation(out=E1[:, 0:nt], in_=T1[:, 0:nt], func=AF.Exp), reads=['gT1'], writes=['gE1'])
                            S.op('dve', lambda e, nt=nt: e.tensor_tensor(out=khT[:, 0:nt], in0=key[:, 0:nt], in1=E1[:, 0:nt], op=ALU.mult), reads=['gkey', 'gE1'], writes=['gkhT'])
                            pbb = PB[3][:].bitcast(BF16)
                            for j in range(nck):
                                S.op('pe', lambda e, j=j: e.transpose(pbb[0:64, j * 128:(j + 1) * 128], khT[:, j * 64:(j + 1) * 64], identb[:]),
                                     reads=['gkhT', 'identb'], writes=['pb3'], skip_self=True)
                            S.op('act', lambda e, d=d, c0=c0, nck=nck: e.copy(out=Ku[d][:, c0:c0 + nck, :], in_=pbb[0:64, 0:nck * 128].rearrange("p (j c) -> p j c", c=128)),
                                 reads=['pb3'], writes=['gKu%d' % d])
                    for d in range(2):
                        def evac(nl, n, po, pok, d=d):
                            if d == 0:
                                S.op('act', lambda e: e.copy(out=oacc[:, nl, :], in_=po), reads=[pok], writes=K('goacc', nl))
                            else:
                                S.op('dve', lambda e: e.tensor_tensor(out=oacc[:, nl, :], in0=po, in1=oacc[:, nl, :], op=ALU.add), reads=[pok] + K('goacc', nl), writes=K('goacc', nl))
                        chunk_loop(d, KsT[d], QsT[d], vtm, QoT[d], Ku[d], vtm, lambda n, d=d: dec[:, d, n:n + 1], 128, evac,
                                   ['gQsT%d' % d, 'gKsT%d' % d, 'gQoT%d' % d, 'gKu%d' % d, 'gv'], 'gdec')
                    allo = K('goacc', 0, NLCH)
                    S.op('dve', lambda e: e.tensor_tensor(out=sq[:], in0=oacc[:], in1=oacc[:], op=ALU.mult), reads=allo, writes=['gsq'])
                    S.op('dve', lambda e: e.tensor_reduce(out=ss[:], in_=sq[:], axis=AX.X, op=ALU.add), reads=['gsq'], writes=['gss'])
                    S.op('act', lambda e: e.activation(out=ss[:], in_=ss[:], func=AF.Sqrt, bias=epsc[0:64, 0:1], scale=1.0 / 128.0), reads=['gss', 'epsc'], writes=['gss'])
                    S.op('dve', lambda e: e.reciprocal(out=ss[:], in_=ss[:]), reads=['gss'], writes=['gss'])
                    S.op('dve', lambda e: e.tensor_tensor(out=oacc[:], in0=oacc[:], in1=ss[:].unsqueeze(2).to_broadcast([64, NLCH, 128]), op=ALU.mult),
                         reads=allo + ['gss'], writes=allo)
                    S.op('dve', lambda e, hd=hd: e.tensor_tensor(out=oacc[:], in0=oacc[:], in1=hgn[:, hd * 128:(hd + 1) * 128].unsqueeze(1).to_broadcast([64, NLCH, 128]), op=ALU.mult),
                         reads=allo + ['hgn'], writes=allo)
                    S.op('dve', lambda e: e.tensor_tensor(out=oacc[:], in0=oacc[:], in1=gs[:], op=ALU.mult), reads=allo + ['ggs'], writes=allo)
                    transpose_chunks_to_mixT(oacc, allo, hd)
                    S.barrier()
            S.barrier()

        with ExitStack() as m0:
            mln = sb(m0, "mln", [64, 512]); convw = sb(m0, "convw", [128, 9, 8]); convb = sb(m0, "convb", [128, 8])
            gateb = sb(m0, "gateb", [8, 2]); sel8 = sb(m0, "sel8", [8, 8]); dirm = sb(m0, "dirm", [8, 2])
            S.dma('sp', lambda e: e.dma_start(out=mln[:], in_=mln_d[:, :]), writes=['mln'])
            S.dma('sp', lambda e: e.dma_start(out=convw[:], in_=convw_d[:, :, :]), writes=['convw'])
            S.dma('sp', lambda e: e.dma_start(out=convb[:], in_=convb_d[:, :]), writes=['convb'])
            S.dma('sp', lambda e: e.dma_start(out=gateb[:], in_=gateb_d[:, :]), writes=['gateb'])
            S.dma('sp', lambda e: e.dma_start(out=sel8[:], in_=sel8_d[:, :]), writes=['sel8'])
            S.dma('sp', lambda e: e.dma_start(out=dirm[:], in_=dirm_d[:, :]), writes=['dirm'])
            RUU = sb(m0, "RUU", [64, NCH, 24]); dchunk = sb(m0, "dchunk", [128, 8, NCH])
            with ExitStack() as gp:
                wgi = load_w(gp, "mwgi", 4608, 8); wgf = load_w(gp, "mwgf", 4616, 8)
                LI = sb(gp, "LI", [8, NTOK]); LF = sb(gp, "LF", [8, NTOK]); Af = sb(gp, "Af", [8, NTOK]); Ab = sb(gp, "Ab", [8, NTOK]); Aa = sb(gp, "Aa", [8, NTOK])
                R = [sb(gp, "Rr%d" % i, [8, NTOK]) for i in range(3)]
                bd = sb(gp, "bd", [8, 8, NCH])
                for (t0, nt) in BLKS:
                    proj_fm_block(wgi, "mwgi", 8, 0, t0, nt)
                    S.op('act', lambda e, t0=t0, nt=nt: e.copy(out=LI[:, t0:t0 + nt], in_=PB[0][0:8, 0:nt]), reads=['pb0'], writes=['LI'])
                    proj_fm_block(wgf, "mwgf", 8, 1, t0, nt)
                    S.op('act', lambda e, t0=t0, nt=nt: e.copy(out=LF[:, t0:t0 + nt], in_=PB[1][0:8, 0:nt]), reads=['pb1'], writes=['LF'])
                S.op('dve', lambda e: e.tensor_scalar_add(out=LI[:], in0=LI[:], scalar1=gateb[:, 0:1]), reads=['LI', 'gateb'], writes=['LI'])
                S.op('act', lambda e: e.activation(out=LF[:], in_=LF[:], func=AF.Sigmoid, bias=gateb[:, 1:2], scale=1.0), reads=['LF', 'gateb'], writes=['LF'])
                S.op('act', lambda e: e.activation(out=LF[:], in_=LF[:], func=AF.Ln), reads=['LF'], writes=['LF'])
                for (t0, nt) in BLKS:
                    S.op('dve', lambda e, t0=t0, nt=nt: e.tensor_tensor_scan(out=Af[:, t0:t0 + nt], data0=rmask[0:8, 0:nt], data1=LF[:, t0:t0 + nt], initial=0.0, op0=ALU.mult, op1=ALU.add),
                         reads=['LF', 'rmask'], writes=['Af'])
                A3 = Af[:].rearrange("p (n c) -> p n c", c=64)
                tot = A3[:, :, 63:64]
                S.op('dve', lambda e: e.tensor_tensor(out=Ab[:].rearrange("p (n c) -> p n c", c=64), in0=tot.to_broadcast([8, NCH, 64]), in1=A3, op=ALU.subtract), reads=['Af'], writes=['Ab'])
                S.op('dve', lambda e: e.tensor_tensor(out=Ab[:], in0=Ab[:], in1=LF[:], op=ALU.add), reads=['Ab', 'LF'], writes=['Ab'])
                S.op('dve', lambda e: e.tensor_scalar_mul(out=Aa[:], in0=Af[:], scalar1=dirm[:, 0:1]), reads=['Af', 'dirm'], writes=['Aa'])
                S.op('dve', lambda e: e.scalar_tensor_tensor(out=Aa[:], in0=Ab[:], scalar=dirm[:, 1:2], in1=Aa[:], op0=ALU.mult, op1=ALU.add), reads=['Ab', 'dirm', 'Aa'], writes=['Aa'])
                S.op('act', lambda e: e.activation(out=R[0][:], in_=Aa[:], func=AF.Exp), reads=['Aa'], writes=['Rr0'])
                S.op('dve', lambda e: e.tensor_tensor(out=Ab[:], in0=LI[:], in1=Aa[:], op=ALU.subtract), reads=['LI', 'Aa', 'Ab'], writes=['Ab'])
                S.op('act', lambda e: e.activation(out=R[1][:], in_=Ab[:], func=AF.Exp), reads=['Ab'], writes=['Rr1'])
                S.op('dve', lambda e: e.tensor_tensor(out=Aa[:].rearrange("p (n c) -> p n c", c=64), in0=Ab[:].rearrange("p (n c) -> p n c", c=64), in1=tot.to_broadcast([8, NCH, 64]), op=ALU.add),
                     reads=['Ab', 'Af', 'Aa', 'Rr0'], writes=['Aa'])
                S.op('act', lambda e: e.activation(out=R[2][:], in_=Aa[:], func=AF.Exp), reads=['Aa'], writes=['Rr2'])
                for half in range(2):
                    for j in range(18):
                        n = half * 18 + j
                        for q in range(3):
                            S.op('pe', lambda e, n=n, j=j, q=q: e.transpose(PB[2][0:64, j * 24 + q * 8:j * 24 + q * 8 + 8], R[q][:, n * 64:(n + 1) * 64], ident[0:8, 0:8]),
                                 reads=['Rr%d' % q, 'ident'], writes=['pb2'], skip_self=True)
                    S.op('act', lambda e, half=half: e.copy(out=RUU[:, half * 18:(half + 1) * 18, :], in_=PB[2][0:64, 0:432].rearrange("p (j c) -> p j c", c=24)),
                         reads=['pb2'], writes=['RUU'])
                S.op('dve', lambda e: e.tensor_tensor(out=bd[:], in0=tot.rearrange("p n c -> p c n").to_broadcast([8, 8, NCH]), in1=sel8[:].unsqueeze(2).to_broadcast([8, 8, NCH]), op=ALU.mult),
                     reads=['Af', 'sel8'], writes=['bd'])
                S.op('pe', lambda e: e.matmul(PB[3][:, 0:288], lhsT=ones[0:8, :], rhs=bd[:].rearrange("p a n -> p (a n)"), start=True, stop=True), reads=['ones', 'bd'], writes=['pb3'], skip_self=True)
                S.op('act', lambda e: e.activation(out=dchunk[:].rearrange("p a n -> p (a n)"), in_=PB[3][:, 0:288], func=AF.Exp), reads=['pb3'], writes=['dchunk'])
                S.barrier()
            qc = sb(m0, "mqc", [128, NTOK], BF16); kc_ = sb(m0, "mkc", [128, NTOK], BF16); kTM = sb(m0, "mkTM", [64, NCH, 128], BF16)
            raw = sb(m0, "mraw", [128, NTOK]); acc = sb(m0, "macc", [128, NTOK])
            vext = sb(m0, "mvext", [64, NCH, 132], BF16); vi = sb(m0, "mvi", [64, NCH, 132], BF16); vu = sb(m0, "mvu", [64, NCH, 132], BF16)
            og = sb(m0, "mog", [64, NLCH, 128], BF16); oext = sb(m0, "moext", [64, NLCH, 132]); obuf = sb(m0, "mobuf", [64, NLCH, 128])
            den = sb(m0, "mden", [64, NLCH]); mu = sb(m0, "mmu", [64, NLCH]); m2 = sb(m0, "mm2", [64, NLCH])
            S.op('dve', lambda e: e.memset(vext[:], 1.0), writes=['mvext'])
            for hd in range(4):
                with ExitStack() as hs:
                    wq_ = load_w(hs, "mwq", 2560 + hd * 128, 128); wk_ = load_w(hs, "mwk", 3072 + hd * 128, 128)
                    wv_ = load_w(hs, "mwv", 3584 + hd * 128, 128); wo_ = load_w(hs, "mwo", 4096 + hd * 128, 128)
                    prep_tables(64)
                    for g in range(9):
                        bk = 3 + g % 2
                        proj_tm_group(wv_, "mwv", 128, bk, g * 4, 4)
                        S.op('act', lambda e, g=g, bk=bk: e.copy(out=vext[:, g * 4:(g + 1) * 4, 0:128], in_=PB[bk][0:64, :].rearrange("p (j c) -> p j c", c=128)),
                             reads=['pb%d' % bk], writes=['mvext'])
                    for g in range(8):
                        bk = 3 + (g + 1) % 2
                        proj_tm_group(wo_, "mwo", 128, bk, 4 + g * 4, 4)
                        S.op('act', lambda e, g=g, bk=bk: e.activation(out=og[:, g * 4:(g + 1) * 4, :], in_=PB[bk][0:64, :].rearrange("p (j c) -> p j c", c=128), func=AF.Sigmoid),
                             reads=['pb%d' % bk], writes=['mog'])
                    for qi, (wb, wn, dstb) in enumerate(((wq_, "mwq", qc), (wk_, "mwk", kc_))):
                        chn = qi * 4 + hd
                        for bi, (t0, nt) in enumerate(BLKS):
                            proj_fm_block(wb, wn, 128, bi % 2, t0, nt)
                            S.op('act', lambda e, t0=t0, nt=nt, bi=bi: e.copy(out=raw[:, t0:t0 + nt], in_=PB[bi % 2][:, 0:nt]), reads=['pb%d' % (bi % 2)], writes=['mraw'])
                        S.op('dve', lambda e, chn=chn: e.tensor_scalar(out=acc[:, 0:256], in0=raw[:, 0:256], scalar1=convw[:, 4, chn:chn + 1], scalar2=convb[:, chn:chn + 1], op0=ALU.mult, op1=ALU.add),
                             reads=['mraw', 'convw', 'convb'], writes=['macc'])
                        S.op('dve', lambda e, chn=chn: e.scalar_tensor_tensor(out=acc[:, 1:256], in0=raw[:, 0:255], scalar=convw[:, 3, chn:chn + 1], in1=acc[:, 1:256], op0=ALU.mult, op1=ALU.add),
                             reads=['mraw', 'convw', 'macc'], writes=['macc'])
                        S.op('dve', lambda e, chn=chn: e.scalar_tensor_tensor(out=acc[:, 0:255], in0=raw[:, 1:256], scalar=convw[:, 5, chn:chn + 1], in1=acc[:, 0:255], op0=ALU.mult, op1=ALU.add),
                             reads=['mraw', 'convw', 'macc'], writes=['macc'])
                        X = raw[:, 256:NTOK].rearrange("p (r c) -> p r c", c=64); Y = acc[:, 256:NTOK].rearrange("p (r c) -> p r c", c=64)
                        S.op('dve', lambda e, chn=chn: e.tensor_scalar(out=acc[:, 256:NTOK], in0=raw[:, 256:NTOK], scalar1=convw[:, 4, chn:chn + 1], scalar2=convb[:, chn:chn + 1], op0=ALU.mult, op1=ALU.add),
                             reads=['mraw', 'convw', 'convb', 'macc'], writes=['macc'])
                        for ky in range(3):
                            for kx in range(3):
                                if ky == 1 and kx == 1:
                                    continue
                                dy = ky - 1; dx = kx - 1
                                r0 = max(0, -dy); r1 = 32 - max(0, dy); c0 = max(0, -dx); c1 = 64 - max(0, dx)
                                S.op('dve', lambda e, chn=chn, ky=ky, kx=kx, r0=r0, r1=r1, c0=c0, c1=c1, dy=dy, dx=dx: e.scalar_tensor_tensor(
                                    out=Y[:, r0:r1, c0:c1], in0=X[:, r0 + dy:r1 + dy, c0 + dx:c1 + dx], scalar=convw[:, ky * 3 + kx, chn:chn + 1], in1=Y[:, r0:r1, c0:c1],
                                    op0=ALU.mult, op1=ALU.add), reads=['mraw', 'convw', 'macc'], writes=['macc'])
                        S.op('act', lambda e: e.activation(out=acc[:], in_=acc[:], func=AF.Silu), reads=['macc'], writes=['macc'])
                        if qi == 0:
                            S.op('dve', lambda e: e.tensor_copy(out=qc[:], in_=acc[:]), reads=['macc'], writes=['mqc'])
                        else:
                            S.op('dve', lambda e: e.tensor_scalar_mul(out=kc_[:], in0=acc[:], scalar1=128.0 ** -0.5), reads=['macc'], writes=['mkc'])
                    pbb = PB[3][:].bitcast(BF16)
                    for g in range(5):
                        nck = 8 if g < 4 else 4
                        for j in range(nck):
                            n = g * 8 + j
                            S.op('pe', lambda e, j=j, n=n: e.transpose(pbb[0:64, j * 128:(j + 1) * 128], kc_[:, n * 64:(n + 1) * 64], identb[:]),
                                 reads=['mkc', 'identb'], writes=['pb3'], skip_self=True)
                        S.op('act', lambda e, g=g, nck=nck: e.copy(out=kTM[:, g * 8:g * 8 + nck, :], in_=pbb[0:64, 0:nck * 128].rearrange("p (j c) -> p j c", c=128)),
                             reads=['pb3'], writes=['mkTM'])
                    for d in range(2):
                        row = d * 4 + hd
                        S.op('dve', lambda e, row=row: e.tensor_tensor(out=vi[:], in0=vext[:], in1=RUU[:, :, 8 + row:9 + row].to_broadcast([64, NCH, 132]), op=ALU.mult),
                             reads=['mvext', 'RUU'], writes=['mvi'])
                        S.op('dve', lambda e, row=row: e.tensor_tensor(out=vu[:], in0=vext[:], in1=RUU[:, :, 16 + row:17 + row].to_broadcast([64, NCH, 132]), op=ALU.mult),
                             reads=['mvext', 'RUU'], writes=['mvu'])

                        def evac(nl, n, po, pok, row=row):
                            S.op('act', lambda e: e.copy(out=oext[:, nl, 0:129], in_=po), reads=[pok], writes=K('moext', nl))
                        chunk_loop(d, kc_, qc, vi, qc, kTM, vu, lambda n, row=row: dchunk[:, row, n:n + 1], 129, evac,
                                   ['mqc', 'mkc', 'mvi', 'mvu', 'mkTM'], 'dchunk')
                        allx = K('moext', 0, NLCH)
                        S.op('dve', lambda e, row=row: e.tensor_tensor(out=oext[:, :, 0:129], in0=oext[:, :, 0:129], in1=RUU[:, 4:NCH, row:row + 1].to_broadcast([64, NLCH, 129]), op=ALU.mult),
                             reads=allx + ['RUU'], writes=allx)
                        S.op('act', lambda e: e.activation(out=den[:], in_=oext[:, :, 128], func=AF.Abs), reads=allx, writes=['mden'])
                        S.op('dve', lambda e: e.tensor_scalar_max(out=den[:], in0=den[:], scalar1=1.0), reads=['mden'], writes=['mden'])
                        S.op('dve', lambda e: e.reciprocal(out=den[:], in_=den[:]), reads=['mden'], writes=['mden'])
                        if d == 0:
                            S.op('dve', lambda e: e.tensor_tensor(out=obuf[:], in0=oext[:, :, 0:128], in1=den[:].unsqueeze(2).to_broadcast([64, NLCH, 128]), op=ALU.mult),
                                 reads=allx + ['mden'], writes=['mobuf'])
                        else:
                            S.op('dve', lambda e: e.tensor_tensor(out=oext[:, :, 0:128], in0=oext[:, :, 0:128], in1=den[:].unsqueeze(2).to_broadcast([64, NLCH, 128]), op=ALU.mult),
                                 reads=allx + ['mden'], writes=allx)
                            S.op('dve', lambda e: e.tensor_tensor(out=obuf[:], in0=obuf[:], in1=oext[:, :, 0:128], op=ALU.add), reads=allx + ['mobuf'], writes=['mobuf'])
                    allx = K('moext', 0, NLCH)
                    S.op('dve', lambda e: e.tensor_reduce(out=mu[:], in_=obuf[:], axis=AX.X, op=ALU.add), reads=['mobuf'], writes=['mmu'])
                    S.op('dve', lambda e: e.tensor_scalar_mul(out=mu[:], in0=mu[:], scalar1=1.0 / 128.0), reads=['mmu'], writes=['mmu'])
                    S.op('dve', lambda e: e.tensor_tensor(out=obuf[:], in0=obuf[:], in1=mu[:].unsqueeze(2).to_broadcast([64, NLCH, 128]), op=ALU.subtract), reads=['mobuf', 'mmu'], writes=['mobuf'])
                    S.op('dve', lambda e: e.tensor_tensor(out=oext[:, :, 0:128], in0=obuf[:], in1=obuf[:], op=ALU.mult), reads=['mobuf'] + allx, writes=allx)
                    S.op('dve', lambda e: e.tensor_reduce(out=m2[:], in_=oext[:, :, 0:128], axis=AX.X, op=ALU.add), reads=allx, writes=['mm2'])
                    S.op('act', lambda e: e.activation(out=m2[:], in_=m2[:], func=AF.Sqrt, bias=epsc[0:64, 0:1], scale=1.0 / 128.0), reads=['mm2', 'epsc'], writes=['mm2'])
                    S.op('dve', lambda e: e.reciprocal(out=m2[:], in_=m2[:]), reads=['mm2'], writes=['mm2'])
                    S.op('dve', lambda e: e.tensor_tensor(out=obuf[:], in0=obuf[:], in1=m2[:].unsqueeze(2).to_broadcast([64, NLCH, 128]), op=ALU.mult), reads=['mobuf', 'mm2'], writes=['mobuf'])
                    S.op('dve', lambda e, hd=hd: e.tensor_tensor(out=obuf[:], in0=obuf[:], in1=mln[:, hd * 128:(hd + 1) * 128].unsqueeze(1).to_broadcast([64, NLCH, 128]), op=ALU.mult),
                         reads=['mobuf', 'mln'], writes=['mobuf'])
                    S.op('dve', lambda e: e.tensor_tensor(out=obuf[:], in0=obuf[:], in1=og[:], op=ALU.mult), reads=['mobuf', 'mog'], writes=['mobuf'])
                    transpose_chunks_to_mixT(obuf, ['mobuf'], 4 + hd)
                    S.barrier()
            S.barrier()

        if debug == 'mix':
            with ExitStack() as dd:
                mf = sb(dd, "mf", [128, NLAT])
                for h in range(8):
                    S.op('dve', lambda e, h=h: e.tensor_copy(out=mf[:], in_=mixT[:, h, :]), reads=K('mixT', h) + ['mf'], writes=['mf'])
                    S.dma('sp', lambda e, h=h: e.dma_start(out=dbg_d[h * 128:(h + 1) * 128, :], in_=mf[:]), reads=['mf'], writes=['dbg'])
                S.wait_all('sp', ['dbg']); S.barrier()
            scB.close(); scA.close()
            return nc
        prep_tables(512)
        S.barrier()
        scB.close()
        x1s = nc.dram_tensor("x1s", [NLAT, 1024], F32, kind="Internal").ap()

        def bcast_tile(dst, dkey, c0, dg):
            for ch in range(8):
                S.op('dve', lambda e, ch=ch: e.tensor_scalar_mul(out=dg[:], in0=ident[:], scalar1=modv[:, c0 + ch, 0:1]), reads=['ident', 'modv', 'dg'], writes=['dg'])
                bank = ch // 4
                S.op('pe', lambda e, ch=ch, bank=bank: e.matmul(PB[bank][:, (ch % 4) * 128:(ch % 4 + 1) * 128], lhsT=ones[:], rhs=dg[:], start=True, stop=True),
                     reads=['ones', 'dg'], writes=['pb%d' % bank], skip_self=True)
            S.op('act', lambda e: e.copy(out=dst[:, 0:512], in_=PB[0][:, :]), reads=['pb0'], writes=[dkey])
            S.op('act', lambda e: e.copy(out=dst[:, 512:1024], in_=PB[1][:, :]), reads=['pb1'], writes=[dkey])

        with ExitStack() as p3:
            bct = {}
            for nm in ("g1b", "ln1g", "ln1b"):
                bct[nm] = sb(p3, nm, [128, 1024])
            for nm, dd in (("ln1g", ln1g_d), ("ln1b", ln1b_d)):
                S.dma('sp', lambda e, nm=nm, dd=dd: e.dma_start(out=bct[nm][:], in_=dd[:, :]), writes=[nm])
            dg = sb(p3, "dg", [128, 128])
            bcast_tile(bct["g1b"], "g1b", 16, dg)
            wo_b = sb(p3, "wout", [128, 8, 1024], BF16); wos = sb(p3, "wos", [128, 1024])
            for kc in range(8):
                S.dma('sp', lambda e, kc=kc: e.dma_start(out=wos[:], in_=wout_d[:, kc, :]), writes=['wos'])
                S.op('pool', lambda e, kc=kc: e.tensor_copy(out=wo_b[:, kc, :], in_=wos[:]), reads=['wos'], writes=['wout'])
            xt = [sb(p3, "x3t%d" % i, [128, 1024]) for i in range(2)]
            t1 = [sb(p3, "t1_%d" % i, [128, 1024]) for i in range(2)]
            st = [sb(p3, "st3%d" % i, [128, 2, 6]) for i in range(2)]
            mv = [sb(p3, "mv3%d" % i, [128, 2]) for i in range(2)]
            rs = [sb(p3, "rs3%d" % i, [128, 1]) for i in range(2)]
            for t in range(16):
                b = t % 2
                S.dma('sp' if b == 0 else 'pool', lambda e, t=t, b=b: e.dma_start(out=xt[b][:], in_=xs[256 + t * 128:256 + (t + 1) * 128, :]), writes=['x3t%d' % b])
                for half in range(2):
                    bank = 2 * b + half
                    for kc in range(8):
                        S.op('pe', lambda e, kc=kc, t=t, half=half, bank=bank: e.matmul(PB[bank][:, :], lhsT=mixT[:, kc, t * 128:(t + 1) * 128], rhs=wo_b[:, kc, half * 512:(half + 1) * 512],
                                                                                      start=(kc == 0), stop=(kc == 7)),
                             reads=K('mixT', kc) + ['wout'], writes=['pb%d' % bank], skip_self=True)
                    S.op('dve', lambda e, b=b, half=half, bank=bank: e.tensor_tensor(out=t1[b][:, half * 512:(half + 1) * 512], in0=PB[bank][:, :], in1=bct["g1b"][:, half * 512:(half + 1) * 512], op=ALU.mult),
                         reads=['pb%d' % bank, 'g1b'], writes=['t1_%d' % b])
                S.op('dve', lambda e, b=b: e.scalar_tensor_tensor(out=t1[b][:], in0=xt[b][:], scalar=ALPHA, in1=t1[b][:], op0=ALU.mult, op1=ALU.add),
                     reads=['x3t%d' % b, 't1_%d' % b], writes=['t1_%d' % b])
                layer_norm_rows(st[b], mv[b][:], rs[b][:], t1[b][:], t1[b][:], 't1_%d' % b, 't1_%d' % b, 'p3%d' % b)
                S.op('dve', lambda e, b=b: e.tensor_tensor(out=t1[b][:], in0=t1[b][:], in1=bct["ln1g"][:], op=ALU.mult), reads=['t1_%d' % b, 'ln1g'], writes=['t1_%d' % b])
                S.op('dve', lambda e, b=b, t=t: e.tensor_tensor(out=t1[b][:], in0=t1[b][:], in1=bct["ln1b"][:], op=ALU.add), reads=['t1_%d' % b, 'ln1b'], writes=['t1_%d' % b])
                S.dma('sp', lambda e, t=t, b=b: e.dma_start(out=x1s[t * 128:(t + 1) * 128, :], in_=t1[b][:]), reads=['t1_%d' % b], writes=K('x1_', t))
                if debug == 'x1':
                    S.dma('sp', lambda e, t=t, b=b: e.dma_start(out=dbg_d[t * 128:(t + 1) * 128, :], in_=t1[b][:]), reads=['t1_%d' % b], writes=['dbg'])
            S.barrier()
        scA.close()

        with ExitStack() as p4:
            bct = {}
            for nm in ("g2b", "sc2b", "sh2b", "ln2g", "ln2b"):
                bct[nm] = sb(p4, nm, [128, 1024])
            for nm, dd in (("ln2g", ln2g_d), ("ln2b", ln2b_d)):
                S.dma('sp', lambda e, nm=nm, dd=dd: e.dma_start(out=bct[nm][:], in_=dd[:, :]), writes=[nm])
            dg = sb(p4, "dg4", [128, 128])
            bcast_tile(bct["g2b"], "g2b", 40, dg); bcast_tile(bct["sc2b"], "sc2b", 32, dg); bcast_tile(bct["sh2b"], "sh2b", 24, dg)
            x1t = [sb(p4, "x1t%d" % i, [128, 1024]) for i in range(2)]
            wqb = sb(p4, "wqb", [128, 8, 2048], BF16)
            keysT = sb(p4, "keysT", [128, 16, 128], BF16)
            with ExitStack() as tmp:
                stg = sb(tmp, "wq_stg", [128, 2048])
                for kc in range(8):
                    S.dma('sp', lambda e, kc=kc: e.dma_start(out=stg[:], in_=wq_d[:, kc, :]), writes=['wq_stg'])
                    S.op('pool', lambda e, kc=kc: e.tensor_copy(out=wqb[:, kc, :], in_=stg[:]), reads=['wq_stg'], writes=['wqb'])
                kst = sb(tmp, "kst", [128, 16, 128])
                S.dma('sp', lambda e: e.dma_start(out=kst[:], in_=keysT_d[:, :, :]), writes=['kst'])
                S.op('pool', lambda e: e.tensor_copy(out=keysT[:], in_=kst[:]), reads=['kst'], writes=['keysT'])
                S.barrier()
            NG = 8
            uvb = [sb(p4, "uvb%d" % i, [128, 2048], BF16) for i in range(NG)]
            gl = sb(p4, "gl", [128, 128])
            pr = [sb(p4, "pr%d" % i, [128, 1024], BF16) for i in range(4)]
            dgk = [sb(p4, "dgk%d" % i, [128, 128], BF16) for i in range(4)]
            h2 = sb(p4, "h2", [128, 1024]); h2b = [sb(p4, "h2b%d" % i, [128, 1024], BF16) for i in range(2)]
            h2T = sb(p4, "h2T", [128, 8, 128], BF16); qT = sb(p4, "qT", [128, 16, 128], BF16)
            sc = sb(p4, "sc", [128, 16, 128]); scw = sb(p4, "scw", [128, 16, 128])
            top = sb(p4, "top", [128, 16, 16]); topi = sb(p4, "topi", [128, 16, 16], U32); topf = sb(p4, "topf", [128, 16, 16])
            cand = sb(p4, "cand", [128, 8, 256]); candw = sb(p4, "candw", [128, 8, 256]); eq = sb(p4, "eq", [128, 128, 16])
            best = sb(p4, "best", [128, 8, 16]); idxf = sb(p4, "idxf", [128, 128])
            posi = sb(p4, "posi", [128, 8, 16], U32); pai = sb(p4, "pai", [128, 128], U32); pbi = sb(p4, "pbi", [128, 128], U32)
            paf = sb(p4, "paf", [128, 128]); pbf = sb(p4, "pbf", [128, 128]); iaf = sb(p4, "iaf", [128, 128]); ibf = sb(p4, "ibf", [128, 128])
            iota16 = sb(p4, "iota16", [128, 16])
            S.dma('sp', lambda e: e.dma_start(out=iota16[:], in_=iota_d[:, :]), writes=['iota16'])
            gate = [sb(p4, "gate%d" % i, [128, 128]) for i in range(2)]
            idxi = [sb(p4, "idxi%d" % i, [128, 128], I32) for i in range(2)]
            wgt = [sb(p4, "wgt%d" % i, [128, 128]) for i in range(2)]
            dots = sb(p4, "dots", [128, 128]); junk = sb(p4, "junk", [128, 1024], BF16); junk2 = sb(p4, "junk2", [128, 256])
            zs = sb(p4, "zs", [128, 8]); nmx = sb(p4, "nmx", [128, 8])
            st = sb(p4, "st4", [128, 2, 6]); mv = sb(p4, "mv4", [128, 2]); rs = sb(p4, "rs4", [128, 1])
            fin = sb(p4, "fin", [128, 1024]); yb = sb(p4, "yb", [128, 1024])
            NT4 = 16 if debug != 'x1' else 0

            def prologue(t):
                p = t % 2
                xb_ = x1t[p]; x1k = 'x1t%d' % p
                S.dma('sp', lambda e: e.dma_start(out=xb_[:], in_=x1s[t * 128:(t + 1) * 128, :]), reads=K('x1_', t), writes=[x1k])
                S.op('dve', lambda e: e.memset(idxf[:], 0.0), writes=['idxf'])
                S.op('dve', lambda e: e.memset(zs[:], 0.0), writes=['zs'])
                layer_norm_rows(st, mv[:], rs[:], xb_[:], h2[:], x1k, 'h2', 'p4')
                S.op('dve', lambda e: e.tensor_tensor(out=h2[:], in0=h2[:], in1=bct["sc2b"][:], op=ALU.mult), reads=['h2', 'sc2b'], writes=['h2'])
                S.op('dve', lambda e: e.tensor_tensor(out=h2[:], in0=h2[:], in1=bct["sh2b"][:], op=ALU.add), reads=['h2', 'sh2b'], writes=['h2'])
                S.op('act', lambda e: e.copy(out=h2b[p][:], in_=h2[:]), reads=['h2'], writes=['h2b%d' % p])
                yield
                for half in range(2):
                    for c4 in range(4):
                        ch = half * 4 + c4
                        S.op('pe', lambda e, ch=ch, c4=c4, half=half: e.transpose(PB[half][:, c4 * 128:(c4 + 1) * 128], h2[:, ch * 128:(ch + 1) * 128], ident[:]),
                             reads=['h2', 'ident'], writes=['pb%d' % half], skip_self=True)
                    yield
                    S.op('act', lambda e, half=half: e.copy(out=h2T[:, half * 4:(half + 1) * 4, :], in_=PB[half][:, :].rearrange("p (j c) -> p j c", c=128)), reads=['pb%d' % half], writes=['h2T'])
                for g in range(4):
                    for c4 in range(4):
                        c = g * 4 + c4
                        for kc in range(8):
                            S.op('pe', lambda e, c=c, c4=c4, kc=kc, g=g: e.matmul(PB[2 + g][:, c4 * 128:(c4 + 1) * 128], lhsT=wqb[:, kc, c * 128:(c + 1) * 128], rhs=h2T[:, kc, :],
                                                                                start=(kc == 0), stop=(kc == 7)), reads=['wqb', 'h2T'], writes=['pb%d' % (2 + g)], skip_self=True)
                    yield
                    S.op('act' if g % 2 == 0 else 'dve', lambda e, g=g: (e.copy if g % 2 == 0 else e.tensor_copy)(out=qT[:, g * 4:(g + 1) * 4, :], in_=PB[2 + g][:, :].rearrange("p (j c) -> p j c", c=128)),
                         reads=['pb%d' % (2 + g)], writes=['qT'])
                for g in range(4):
                    for c4 in range(4):
                        c = g * 4 + c4
                        S.op('pe', lambda e, c=c, c4=c4, g=g: e.matmul(PB[2 + g][:, c4 * 128:(c4 + 1) * 128], lhsT=qT[:, c, :], rhs=keysT[:, c, :], start=True, stop=True),
                             reads=['qT', 'keysT'], writes=['pb%d' % (2 + g)], skip_self=True)
                    yield
                    S.op('act' if g % 2 == 0 else 'dve', lambda e, g=g: (e.copy if g % 2 == 0 else e.tensor_copy)(out=sc[:, g * 4:(g + 1) * 4, :], in_=PB[2 + g][:, :].rearrange("p (j c) -> p j c", c=128)),
                         reads=['pb%d' % (2 + g)], writes=['sc'])
                for c in range(16):
                    S.op('dve', lambda e, c=c: e.max(out=top[:, c, 0:8], in_=sc[:, c, :]), reads=['sc'], writes=['top'])
                    S.op('dve', lambda e, c=c: e.max_index(out=topi[:, c, 0:8], in_max=top[:, c, 0:8], in_values=sc[:, c, :]), reads=['sc', 'top'], writes=['topi'])
                    S.op('dve', lambda e, c=c: e.match_replace(out=scw[:, c, :], in_to_replace=top[:, c, 0:8], in_values=sc[:, c, :], imm_value=-1e30), reads=['sc', 'top'], writes=['scw'])
                    S.op('dve', lambda e, c=c: e.max(out=top[:, c, 8:16], in_=scw[:, c, :]), reads=['scw'], writes=['top'])
                    S.op('dve', lambda e, c=c: e.max_index(out=topi[:, c, 8:16], in_max=top[:, c, 8:16], in_values=scw[:, c, :]), reads=['scw', 'top'], writes=['topi'])
                    yield
                S.op('dve', lambda e: e.tensor_copy(out=topf[:], in_=topi[:]), reads=['topi'], writes=['topf'])
                t4 = top[:].rearrange("p (h two) k -> p h two k", two=2); f4 = topf[:].rearrange("p (h two) k -> p h two k", two=2)
                c4v = cand[:].rearrange("p h (a b) -> p h a b", b=16)
                for h in range(8):
                    S.op('dve', lambda e, h=h: e.tensor_tensor(out=c4v[:, h, :, :], in0=t4[:, h, 0, :].unsqueeze(2).to_broadcast([128, 16, 16]), in1=t4[:, h, 1, :].unsqueeze(1).to_broadcast([128, 16, 16]), op=ALU.add),
                         reads=['top'], writes=['cand'])
                    yield
                for h in range(8):
                    S.op('dve', lambda e, h=h: e.max(out=best[:, h, 0:8], in_=cand[:, h, :]), reads=['cand'], writes=['best'])
                    S.op('dve', lambda e, h=h: e.match_replace(out=candw[:, h, :], in_to_replace=best[:, h, 0:8], in_values=cand[:, h, :], imm_value=-1e30), reads=['cand', 'best'], writes=['candw'])
                    S.op('dve', lambda e, h=h: e.max(out=best[:, h, 8:16], in_=candw[:, h, :]), reads=['candw'], writes=['best'])
                    S.op('dve', lambda e, h=h: e.max_index(out=posi[:, h, 0:8], in_max=best[:, h, 0:8], in_values=cand[:, h, :]), reads=['cand', 'best'], writes=['posi'])
                    S.op('dve', lambda e, h=h: e.max_index(out=posi[:, h, 8:16], in_max=best[:, h, 8:16], in_values=candw[:, h, :]), reads=['candw', 'best'], writes=['posi'])
                    yield
                pflat = posi[:].rearrange("p h k -> p (h k)")
                S.op('dve', lambda e: e.tensor_single_scalar(out=pai[:], in_=pflat, scalar=4, op=ALU.logical_shift_right), reads=['posi'], writes=['pai'])
                S.op('dve', lambda e: e.tensor_single_scalar(out=pbi[:], in_=pflat, scalar=15, op=ALU.bitwise_and), reads=['posi'], writes=['pbi'])
                S.op('dve', lambda e: e.tensor_copy(out=paf[:], in_=pai[:]), reads=['pai'], writes=['paf'])
                S.op('dve', lambda e: e.tensor_copy(out=pbf[:], in_=pbi[:]), reads=['pbi'], writes=['pbf'])
                yield
                eq4 = eq[:].rearrange("p (h k) a -> p h k a", k=16)
                for (pp, pk_, half_, dst, dn) in ((paf, 'paf', 0, iaf, 'iaf'), (pbf, 'pbf', 1, ibf, 'ibf')):
                    S.op('dve', lambda e, pp=pp: e.tensor_tensor(out=eq[:], in0=iota16[:].unsqueeze(1).to_broadcast([128, 128, 16]), in1=pp[:].unsqueeze(2).to_broadcast([128, 128, 16]), op=ALU.is_equal),
                         reads=['iota16', pk_, 'eq'], writes=['eq'])
                    S.op('dve', lambda e, half_=half_: e.tensor_tensor(out=eq4, in0=eq4, in1=f4[:, :, half_, :].unsqueeze(2).to_broadcast([128, 8, 16, 16]), op=ALU.mult),
                         reads=['eq', 'topf'], writes=['eq'])
                    S.op('dve', lambda e, dst=dst: e.tensor_reduce(out=dst[:], in_=eq[:], axis=AX.X, op=ALU.add), reads=['eq'], writes=[dn])
                    yield
                S.op('dve', lambda e: e.scalar_tensor_tensor(out=idxf[:], in0=iaf[:], scalar=128.0, in1=ibf[:], op0=ALU.mult, op1=ALU.add), reads=['iaf', 'ibf'], writes=['idxf'])
                S.op('dve', lambda e: e.tensor_scalar_min(out=idxf[:], in0=idxf[:], scalar1=16383.0), reads=['idxf'], writes=['idxf'])
                S.op('dve', lambda e: e.tensor_copy(out=idxi[p][:], in_=idxf[:]), reads=['idxf'], writes=['idxi%d' % p])
                S.op('dve', lambda e: e.tensor_scalar_mul(out=nmx[:], in0=best[:, :, 0], scalar1=-1.0), reads=['best'], writes=['nmx'])
                g3 = gate[p][:].rearrange("p (h k) -> p h k", k=16)
                for h in range(8):
                    S.op('act', lambda e, h=h: e.activation(out=g3[:, h, :], in_=best[:, h, :], func=AF.Exp, bias=nmx[:, h:h + 1], scale=1.0, accum_out=zs[:, h:h + 1]),
                         reads=['best', 'nmx'], writes=['gate%d' % p, 'zs'])
                S.op('dve', lambda e: e.reciprocal(out=zs[:], in_=zs[:]), reads=['zs'], writes=['zs'])
                S.op('dve', lambda e: e.tensor_tensor(out=g3, in0=g3, in1=zs[:].unsqueeze(2).to_broadcast([128, 8, 16]), op=ALU.mult), reads=['gate%d' % p, 'zs'], writes=['gate%d' % p])

            def fused(t, gen=None):
                p = t % 2
                LAG = 2

                def tail(kk):
                    s_ = kk % NG; d4 = kk % 4
                    S.op('dve', lambda e: e.tensor_scalar(out=dgk[d4][:], in0=identb[:], scalar1=gl[:, kk:kk + 1], scalar2=gate[p][:, kk:kk + 1], op0=ALU.mult, op1=ALU.mult),
                         reads=['identb', 'gl_%d' % kk, 'gate%d' % p], writes=['dgk%d' % d4])
                    for half in range(2):
                        S.op('pe', lambda e, half=half: e.matmul(PB[6 + half][:, :], lhsT=dgk[d4][:], rhs=uvb[s_][:, 1024 + half * 512:1024 + (half + 1) * 512], start=(kk == 0), stop=(kk == 127)),
                             reads=['dgk%d' % d4, 'uvb%d' % s_], writes=['pb%d' % (6 + half)], skip_self=True)

                for k in range(128):
                    s_ = k % NG; j4 = k % 4
                    dk = 'dots_%d' % k
                    S.dma('pool', lambda e, k=k, s_=s_: e.indirect_dma_start(out=uvb[s_][:], out_offset=None, in_=uv_d[:, :], in_offset=bass.IndirectOffsetOnAxis(ap=idxi[p][:, k:k + 1], axis=0)),
                          reads=['idxi%d' % p, 'tabs'], writes=['uvb%d' % s_])
                    S.op('dve', lambda e, k=k, s_=s_, j4=j4: e.tensor_tensor(out=pr[j4][:], in0=uvb[s_][:, 0:1024], in1=h2b[p][:], op=ALU.mult),
                         reads=['uvb%d' % s_, 'h2b%d' % p], writes=['pr%d' % j4])
                    S.op('act', lambda e, k=k, j4=j4: e.activation(out=junk[:], in_=pr[j4][:], func=AF.Identity, accum_out=dots[:, k:k + 1]),
                         reads=['pr%d' % j4, 'dots0'], writes=[dk])
                    S.op('act', lambda e, k=k: e.activation(out=gl[:, k:k + 1], in_=dots[:, k:k + 1], func=AF.Gelu), reads=[dk], writes=['gl_%d' % k])
                    if k >= LAG:
                        tail(k - LAG)
                    if gen is not None and k % 2 == 1:
                        next(gen, None)
                for kk in range(128 - LAG, 128):
                    tail(kk)
                if gen is not None:
                    for _ in gen:
                        pass
                xb_ = x1t[p]; x1k = 'x1t%d' % p
                for half in range(2):
                    S.op('dve', lambda e, half=half: e.tensor_tensor(out=yb[:, half * 512:(half + 1) * 512], in0=PB[6 + half][:, :], in1=bct["g2b"][:, half * 512:(half + 1) * 512], op=ALU.mult),
                         reads=['pb%d' % (6 + half), 'g2b', 'yb'], writes=['yb'])
                S.op('dve', lambda e: e.scalar_tensor_tensor(out=fin[:], in0=xb_[:], scalar=ALPHA, in1=yb[:], op0=ALU.mult, op1=ALU.add), reads=[x1k, 'yb', 'fin'], writes=['fin'])
                layer_norm_rows(stf, mvf[:], rsf[:], fin[:], fin[:], 'fin', 'fin', 'p4f')
                S.op('dve', lambda e: e.tensor_tensor(out=fin[:], in0=fin[:], in1=bct["ln2g"][:], op=ALU.mult), reads=['fin', 'ln2g'], writes=['fin'])
                S.op('dve', lambda e: e.tensor_tensor(out=fin[:], in0=fin[:], in1=bct["ln2b"][:], op=ALU.add), reads=['fin', 'ln2b'], writes=['fin'])
                S.dma('sp', lambda e: e.dma_start(out=out_d[t * 128:(t + 1) * 128, :], in_=fin[:]), reads=['fin'], writes=['out'])

            stf = sb(p4, "st4f", [128, 2, 6]); mvf = sb(p4, "mv4f", [128, 2]); rsf = sb(p4, "rs4f", [128, 1])
            S.op('dve', lambda e: e.memset(dots[:], 0.0), writes=['dots0'])
            if NT4:
                for _ in prologue(0):
                    pass
            for t in range(NT4):
                fused(t, prologue(t + 1) if t + 1 < NT4 else None)
            S.wait_all('sp', ['out', 'dbg'])
            S.barrier()
    return nc


def _prep_shared(inp):
    f = np.float32
    sh = {}
    sh["w_mod"] = np.ascontiguousarray(inp["w_mod"][0].reshape(8, 128, 6144).transpose(1, 0, 2))
    sh["b_modT"] = np.ascontiguousarray(inp["b_mod"][0].reshape(48, 128).T)
    sh["w_in"] = np.ascontiguousarray(inp["w_in"][0].reshape(8, 128, 4624).transpose(1, 0, 2))
    sh["lgT"] = np.ascontiguousarray(inp["hg_lb_logits"].reshape(2, 2, 4, 128).transpose(3, 0, 1, 2))
    sh["hgn"] = np.ascontiguousarray(np.broadcast_to(inp["hg_norm_g"][0][None, :], (64, 512)))
    sh["mln"] = np.ascontiguousarray(np.broadcast_to(inp["ml_norm_g"][0][None, :], (64, 512)))
    sh["convw"] = np.ascontiguousarray(inp["ml_conv_w"][0].reshape(9, 8, 128).transpose(2, 0, 1))
    sh["convb"] = np.ascontiguousarray(inp["ml_conv_b"][0].reshape(8, 128).T)
    sh["gateb"] = np.ascontiguousarray(inp["ml_gate_b"][0].reshape(2, 8).T)
    sh["w_out"] = np.ascontiguousarray(inp["w_out"][0].reshape(8, 128, 1024).transpose(1, 0, 2))
    for nm, key in (("ln1g", "ln1_g"), ("ln1b", "ln1_b"), ("ln2g", "ln2_g"), ("ln2b", "ln2_b")):
        sh[nm] = np.ascontiguousarray(np.broadcast_to(inp[key][0][None, :], (128, 1024)))
    sh["wq"] = np.ascontiguousarray(inp["peer_wq"][0].reshape(8, 128, 2048).transpose(1, 0, 2))
    sh["keysT"] = np.ascontiguousarray(inp["peer_keys"][0].reshape(16, 128, 128).transpose(2, 0, 1))
    nexp = 128 if os.environ.get("KDEBUG") else 16384
    sh["pu"] = np.ascontiguousarray(inp["peer_u"][0][:nexp])
    sh["pv"] = np.ascontiguousarray(inp["peer_v"][0][:nexp])
    sh["ident"] = np.eye(128, dtype=f)
    m = np.zeros((64, 2, 64), f)
    s = np.arange(64)[:, None]; c = np.arange(64)[None, :]
    m[:, 0, :] = (s <= c); m[:, 1, :] = (s >= c)
    sh["masks"] = m
    rm = np.ones((128, 512), f); rm[:, ::64] = 0.0
    sh["rmask"] = rm
    sh["sel8"] = np.eye(8, dtype=f)
    sh["iota16"] = np.ascontiguousarray(np.broadcast_to(np.arange(16, dtype=f)[None, :], (128, 16)))
    dm = np.zeros((8, 2), f); dm[0:4, 0] = 1.0; dm[4:8, 1] = 1.0
    sh["dirm"] = dm
    return {k: np.asarray(v, dtype=f) for k, v in sh.items()}


def kernel(**inputs):
    inp = {k: np.asarray(v) for k, v in inputs.items()}
    debug = os.environ.get("KDEBUG") or None
    nc = build(debug)
    sh = _prep_shared(inp)
    in_maps = []
    for b in range(8):
        m = dict(sh)
        m["xs"] = np.ascontiguousarray(np.concatenate([inp["ctx"][b], inp["x"][b]], axis=0).astype(np.float32))
        m["cT"] = np.ascontiguousarray(np.stack([inp["c"][b], inp["c_ctx"]], axis=-1).reshape(8, 128, 2).transpose(1, 0, 2).astype(np.float32))
        in_maps.append(m)
    res = run_bass_kernel_spmd(nc, in_maps, core_ids=list(range(8)))
    key = "dbg" if debug else "out"
    return np.stack([np.asarray(r[key]) for r in res.results], axis=0).astype(np.float32)
```

```python
import os
import numpy as np
from contextlib import ExitStack
import concourse.bass as bass
import concourse.mybir as mybir
from concourse.bass_utils import run_bass_kernel_spmd

F32 = mybir.dt.float32; BF16 = mybir.dt.bfloat16; I32 = mybir.dt.int32; U32 = mybir.dt.uint32
AF = mybir.ActivationFunctionType; ALU = mybir.AluOpType; AX = mybir.AxisListType

NTOK = 2304; NLAT = 2048; NCH = 36; NLCH = 32
ALPHA = 2.0 ** 0.25
EPS = 1e-6


class Sched:
    NDMA = 32

    def __init__(self, nc, es):
        self.nc = nc
        self.engs = {'pe': nc.tensor, 'act': nc.scalar, 'dve': nc.vector, 'pool': nc.gpsimd, 'sp': nc.sync}
        self.sem = {k: es.enter_context(nc.semaphore("sem_" + k)) for k in self.engs}
        self.cnt = {k: 0 for k in self.engs}
        self.dsem = [es.enter_context(nc.semaphore("dsem%d" % i)) for i in range(self.NDMA)]
        self.dcnt = [0] * self.NDMA
        self.dnext = 0
        self.seen = {k: {} for k in self.engs}
        self.bufs = {}

    def _deps(self, reads, writes):
        deps = []
        for r in reads:
            b = self.bufs.get(r)
            if b and b['w'] is not None:
                deps.append(b['w'])
        for w in writes:
            b = self.bufs.get(w)
            if b:
                if b['w'] is not None:
                    deps.append(b['w'])
                deps.extend(b['r'])
        return deps

    def _wait(self, eng, deps, skip_self=False):
        best = {}
        for (sid, sem, val, owner) in deps:
            if skip_self and owner == eng:
                continue
            if best.get(sid, (None, 0))[1] < val:
                best[sid] = (sem, val)
        for sid, (sem, val) in best.items():
            if self.seen[eng].get(sid, 0) >= val:
                continue
            self.engs[eng].wait_ge(sem, val)
            self.seen[eng][sid] = val

    def _record(self, dep, reads, writes):
        for r in reads:
            b = self.bufs.setdefault(r, {'w': None, 'r': []})
            b['r'] = [d for d in b['r'] if d[0] != dep[0]] + [dep]
        for w in writes:
            self.bufs[w] = {'w': dep, 'r': []}

    @staticmethod
    def _split(keys):
        norm, ps = [], []
        for k in keys:
            if k.startswith('pb') and len(k) > 2 and k[2].isdigit():
                ps.append(k[:3])
            else:
                norm.append(k)
        return norm, ps

    def op(self, eng, fn, reads=(), writes=(), skip_self=False):
        reads, pr = self._split(reads)
        writes, pw = self._split(writes)
        banks = sorted(set(pr + pw))
        deps = self._deps(reads, writes)
        deps += [d for d in self._deps((), banks) if d[3] != eng]
        self._wait(eng, deps, skip_self)
        ins = fn(self.engs[eng])
        self.cnt[eng] += 1
        ins.then_inc(self.sem[eng], 1)
        self._record(('e_' + eng, self.sem[eng], self.cnt[eng], eng), reads, list(writes) + banks)
        return ins

    def dma(self, q, fn, reads=(), writes=()):
        deps = self._deps(reads, writes)
        j = self.dnext
        self.dnext = (self.dnext + 1) % self.NDMA
        if self.dcnt[j] > 0:
            deps = deps + [('d%d' % j, self.dsem[j], self.dcnt[j], 'dma')]
        self._wait(q, deps)
        ins = fn(self.engs[q])
        self.dcnt[j] += 16
        ins.then_inc(self.dsem[j], 16)
        self._record(('d%d' % j, self.dsem[j], self.dcnt[j], 'dma'), reads, writes)

    def wait_all(self, eng, keys):
        deps = []
        for k in keys:
            b = self.bufs.get(k)
            if b:
                if b['w'] is not None:
                    deps.append(b['w'])
                deps.extend(b['r'])
        self._wait(eng, deps)

    def barrier(self):
        for e in self.engs:
            deps = [('e_' + f, self.sem[f], self.cnt[f], f) for f in self.engs if f != e and self.cnt[f] > 0]
            deps += [('d%d' % j, self.dsem[j], self.dcnt[j], 'dma') for j in range(self.NDMA) if self.dcnt[j] > 0]
            self._wait(e, deps)


def K(name, a, b=None):
    if b is None:
        return ["%s%d" % (name, a)]
    return ["%s%d" % (name, i) for i in range(a, b)]


def build(debug=None):
    nc = bass.Bass("TRN2", target_bir_lowering=False)
    D = {}

    def din(name, shape, dt=F32):
        D[name] = nc.dram_tensor(name, shape, dt, kind="ExternalInput").ap()
        return D[name]

    xs = din("xs", [NTOK, 1024]); cT_d = din("cT", [128, 8, 2]); wmod_d = din("w_mod", [128, 8, 6144])
    bmod_d = din("b_modT", [128, 48]); win_d = din("w_in", [128, 8, 4624]); lg_d = din("lgT", [128, 2, 2, 4])
    hgn_d = din("hgn", [64, 512]); mln_d = din("mln", [64, 512]); convw_d = din("convw", [128, 9, 8])
    convb_d = din("convb", [128, 8]); gateb_d = din("gateb", [8, 2]); wout_d = din("w_out", [128, 8, 1024])
    ln1g_d = din("ln1g", [128, 1024]); ln1b_d = din("ln1b", [128, 1024]); ln2g_d = din("ln2g", [128, 1024])
    ln2b_d = din("ln2b", [128, 1024]); wq_d = din("wq", [128, 8, 2048]); keysT_d = din("keysT", [128, 16, 128])
    NEXP = 128 if debug else 16384
    pu_d = din("pu", [NEXP, 1024]); pv_d = din("pv", [NEXP, 1024])
    ident_d = din("ident", [128, 128]); masks_d = din("masks", [64, 2, 64]); rmask_d = din("rmask", [128, 512])
    sel8_d = din("sel8", [8, 8]); dirm_d = din("dirm", [8, 2]); iota_d = din("iota16", [128, 16])
    out_d = nc.dram_tensor("out", [NLAT, 1024], F32, kind="ExternalOutput").ap()
    dbg_d = None
    if debug:
        dbg_d = nc.dram_tensor("dbg", [NLAT, 1024] if debug != 'mix' else [1024, NLAT], F32, kind="ExternalOutput").ap()

    with ExitStack() as es:
        S = Sched(nc, es)

        uid = [0]

        def sb(st, name, shape, dt=F32):
            uid[0] += 1
            return st.enter_context(nc.sbuf_tensor("s%d_%s" % (uid[0], name), shape, dt))

        PB = [es.enter_context(nc.psum_tensor("pb%d" % i, [128, 512], F32)) for i in range(8)]

        ident = sb(es, "ident", [128, 128]); identb = sb(es, "identb", [128, 128], BF16)
        masks = sb(es, "masks", [64, 2, 64]); rmask = sb(es, "rmask", [128, 512])
        ones = sb(es, "ones", [128, 128]); epsc = sb(es, "epsc", [128, 1])
        modv = sb(es, "modv", [128, 48, 2])
        S.dma('sp', lambda e: e.dma_start(out=ident[:], in_=ident_d[:, :]), writes=['ident'])
        S.dma('sp', lambda e: e.dma_start(out=masks[:], in_=masks_d[:, :, :]), writes=['masks'])
        S.dma('sp', lambda e: e.dma_start(out=rmask[:], in_=rmask_d[:, :]), writes=['rmask'])
        S.op('dve', lambda e: e.tensor_copy(out=identb[:], in_=ident[:]), reads=['ident'], writes=['identb'])
        S.op('dve', lambda e: e.memset(ones[:], 1.0), writes=['ones'])
        S.op('dve', lambda e: e.memset(epsc[:], EPS), writes=['epsc'])

        with ExitStack() as p0:
            cT = sb(p0, "cT", [128, 8, 2]); scT = sb(p0, "scT", [128, 8, 2]); bmodT = sb(p0, "bmodT", [128, 48])
            wm = [sb(p0, "wm%d" % i, [128, 6144]) for i in range(2)]
            S.dma('sp', lambda e: e.dma_start(out=cT[:], in_=cT_d[:, :, :]), writes=['cT'])
            S.dma('sp', lambda e: e.dma_start(out=bmodT[:], in_=bmod_d[:, :]), writes=['bmodT'])
            S.op('act', lambda e: e.activation(out=scT[:], in_=cT[:], func=AF.Silu), reads=['cT'], writes=['scT'])
            for kc in range(8):
                w = wm[kc % 2]; wk = 'wm%d' % (kc % 2)
                S.dma('sp' if kc % 2 == 0 else 'pool', lambda e, w=w, kc=kc: e.dma_start(out=w[:], in_=wmod_d[:, kc, :]), writes=[wk])
                for j in range(48):
                    S.op('pe', lambda e, w=w, kc=kc, j=j: e.matmul(PB[kc // 4][:, (kc % 4) * 96 + 2 * j:(kc % 4) * 96 + 2 * j + 2], lhsT=w[:, j * 128:(j + 1) * 128], rhs=scT[:, kc, :],
                                                                 start=True, stop=True),
                         reads=[wk, 'scT'], writes=['pb%d' % (kc // 4)], skip_self=True)
            mflat = modv[:].rearrange("p j n -> p (j n)")
            S.op('dve', lambda e: e.tensor_tensor(out=modv[:], in0=PB[0][:, 0:96].rearrange("p (j n) -> p j n", n=2), in1=bmodT[:].unsqueeze(2).to_broadcast([128, 48, 2]), op=ALU.add),
                 reads=['pb0', 'bmodT'], writes=['modv'])
            for kc in range(1, 8):
                S.op('dve', lambda e, kc=kc: e.tensor_tensor(out=mflat, in0=mflat, in1=PB[kc // 4][:, (kc % 4) * 96:(kc % 4) * 96 + 96], op=ALU.add),
                     reads=['pb%d' % (kc // 4), 'modv'], writes=['modv'])
            S.op('dve', lambda e: e.tensor_scalar_add(out=modv[:, 8:16, :], in0=modv[:, 8:16, :], scalar1=1.0), reads=['modv'], writes=['modv'])
            S.op('dve', lambda e: e.tensor_scalar_add(out=modv[:, 32:40, :], in0=modv[:, 32:40, :], scalar1=1.0), reads=['modv'], writes=['modv'])
            S.barrier()
        if debug == 'p0':
            S.dma('sp', lambda e: e.dma_start(out=dbg_d[0:128, 0:96], in_=modv[:].rearrange("p a b -> p (a b)")), reads=['modv'], writes=['dbg'])
            S.wait_all('sp', ['dbg']); S.barrier()
            return nc

        scA = ExitStack(); scB = ExitStack()
        mixT = sb(scA, "mixT", [128, 8, NLAT], BF16)
        hT = sb(scB, "hT", [128, 8, NTOK], BF16)
        wstg = sb(scB, "wstg", [128, 8, 128])
        tstg = [sb(scB, "tstg%d" % i, [128, 512]) for i in range(2)]
        tbf = [sb(scB, "tbf%d" % i, [128, 512], BF16) for i in range(2)]
        uv_d = nc.dram_tensor("uvbf", [NEXP, 2048], BF16, kind="Internal").ap()
        prep_pos = [0]

        def prep_tables(npieces):
            if debug:
                return
            for _ in range(npieces):
                i = prep_pos[0]
                if i >= 512:
                    return
                prep_pos[0] += 1
                src, coff = (pu_d, 0) if i < 256 else (pv_d, 1024)
                r = (i % 256) // 2; c = (i % 2) * 512; bb = i % 2
                S.dma('sp', lambda e, src=src, r=r, c=c, bb=bb: e.dma_start(out=tstg[bb][:], in_=src[r * 128:(r + 1) * 128, c:c + 512]), writes=['tstg%d' % bb])
                S.op('pool', lambda e, bb=bb: e.tensor_copy(out=tbf[bb][:], in_=tstg[bb][:]), reads=['tstg%d' % bb], writes=['tbf%d' % bb])
                S.dma('pool', lambda e, coff=coff, r=r, c=c, bb=bb: e.dma_start(out=uv_d[r * 128:(r + 1) * 128, coff + c:coff + c + 512], in_=tbf[bb][:]), reads=['tbf%d' % bb], writes=['tabs'])

        def layer_norm_rows(st_ap, mv_ap, rstd_ap, src, dst, skey, dkey, tag):
            S.op('dve', lambda e: e.bn_stats(out=st_ap[:, 0, :], in_=src[:, 0:512]), reads=[skey], writes=[tag + 'st'])
            S.op('dve', lambda e: e.bn_stats(out=st_ap[:, 1, :], in_=src[:, 512:1024]), reads=[skey], writes=[tag + 'st'])
            S.op('dve', lambda e: e.bn_aggr(out=mv_ap, in_=st_ap[:].rearrange("p a b -> p (a b)")), reads=[tag + 'st'], writes=[tag + 'mv'])
            S.op('act', lambda e: e.activation(out=rstd_ap, in_=mv_ap[:, 1:2], func=AF.Sqrt, bias=epsc[:, 0:1], scale=1.0),
                 reads=[tag + 'mv', 'epsc'], writes=[tag + 'rs'])
            S.op('dve', lambda e: e.reciprocal(out=rstd_ap, in_=rstd_ap), reads=[tag + 'rs'], writes=[tag + 'rs'])
            S.op('dve', lambda e: e.tensor_scalar(out=dst, in0=src, scalar1=mv_ap[:, 0:1], scalar2=rstd_ap, op0=ALU.subtract, op1=ALU.mult),
                 reads=[skey, tag + 'mv', tag + 'rs'], writes=[dkey])

        with ExitStack() as p1:
            xt = [sb(p1, "xt%d" % i, [128, 1024]) for i in range(2)]
            xn = [sb(p1, "xn%d" % i, [128, 1024]) for i in range(2)]
            st = [sb(p1, "st%d" % i, [128, 2, 6]) for i in range(2)]
            mv = [sb(p1, "mv%d" % i, [128, 2]) for i in range(2)]
            rs = [sb(p1, "rs%d" % i, [128, 1]) for i in range(2)]
            PSAP = bool(os.environ.get("KPSAP"))
            for t in range(18):
                b = t % 2
                n = 1 if t < 2 else 0
                S.dma('sp' if b == 0 else 'pool', lambda e, t=t, b=b: e.dma_start(out=xt[b][:], in_=xs[t * 128:(t + 1) * 128, :]), writes=['xt%d' % b])
                layer_norm_rows(st[b], mv[b][:], rs[b][:], xt[b][:], xn[b][:], 'xt%d' % b, 'xn%d' % b, 'p1%d' % b)
                for half in range(2):
                    bank = 2 * b + half
                    pk = 'pb%d' % bank
                    for c4 in range(4):
                        ch = half * 4 + c4
                        S.op('pe', lambda e, b=b, ch=ch, c4=c4, bank=bank: e.transpose(PB[bank][:, c4 * 128:(c4 + 1) * 128], xn[b][:, ch * 128:(ch + 1) * 128], ident[:]),
                             reads=['xn%d' % b, 'ident'], writes=[pk], skip_self=True)
                    if not PSAP:
                        S.op('act', lambda e, b=b, half=half, bank=bank: e.copy(out=xt[b][:, half * 512:(half + 1) * 512], in_=PB[bank][:, :]), reads=[pk, 'xt%d' % b], writes=['xt%d' % b])
                    for c4 in range(4):
                        ch = half * 4 + c4
                        dst = hT[:, ch, t * 128:(t + 1) * 128]
                        src = PB[bank][:, c4 * 128:(c4 + 1) * 128] if PSAP else xt[b][:, ch * 128:(ch + 1) * 128]
                        S.op('dve', lambda e, dst=dst, src=src, ch=ch, n=n: e.tensor_scalar(out=dst, in0=src, scalar1=modv[:, 8 + ch, n:n + 1], scalar2=modv[:, ch, n:n + 1],
                                                                                      op0=ALU.mult, op1=ALU.add),
                             reads=([pk] if PSAP else ['xt%d' % b]) + ['modv'], writes=K('hT', t))
            S.barrier()

        if debug == 'p1':
            with ExitStack() as dd:
                hf = sb(dd, "hf", [128, 1024])
                for t in range(16):
                    S.op('dve', lambda e, t=t: e.tensor_copy(out=hf[:].rearrange("p (a b) -> p a b", b=128), in_=hT[:, :, 256 + t * 128:256 + (t + 1) * 128]), reads=K('hT', t + 2) + ['hf'], writes=['hf'])
                    S.dma('sp', lambda e, t=t: e.dma_start(out=dbg_d[t * 128:(t + 1) * 128, :], in_=hf[:]), reads=['hf'], writes=['dbg'])
                S.wait_all('sp', ['dbg']); S.barrier()
            scB.close(); scA.close()
            return nc
        def load_w(stk, name, col0, ncols, src=None):
            src = win_d if src is None else src
            wb = sb(stk, name, [128, 8, ncols], BF16)
            S.dma('sp', lambda e: e.dma_start(out=wstg[:, :, 0:ncols], in_=src[:, :, col0:col0 + ncols]), writes=['wstg'])
            S.op('pool', lambda e: e.tensor_copy(out=wb[:], in_=wstg[:, :, 0:ncols]), reads=['wstg'], writes=[name])
            return wb

        BLKS = [(0, 512), (512, 512), (1024, 512), (1536, 512), (2048, 256)]

        def proj_fm_block(wb, wname, ncols, bank, t0, nt):
            for kc in range(8):
                S.op('pe', lambda e, kc=kc: e.matmul(PB[bank][0:ncols, 0:nt], lhsT=wb[:, kc, :], rhs=hT[:, kc, t0:t0 + nt], start=(kc == 0), stop=(kc == 7)),
                     reads=[wname] + K('hT', t0 // 128, (t0 + nt) // 128), writes=['pb%d' % bank], skip_self=True)

        def proj_tm_group(wb, wname, ncols, bank, c0, ncks):
            for j in range(ncks):
                n = c0 + j
                for kc in range(8):
                    S.op('pe', lambda e, kc=kc, j=j, n=n: e.matmul(PB[bank][0:64, j * ncols:(j + 1) * ncols], lhsT=hT[:, kc, n * 64:(n + 1) * 64], rhs=wb[:, kc, :],
                                                                   start=(kc == 0), stop=(kc == 7)),
                         reads=[wname] + K('hT', n // 2), writes=['pb%d' % bank], skip_self=True)

        S32 = sb(scB, "S32", [128, 2, 132]); Sbf = sb(scB, "Sbf", [128, 2, 132], BF16)
        stm = sb(scB, "stm", [64, 2, 64], BF16)
        usb = sb(scB, "usb", [128, 2, 132])

        def chunk_loop(dirn, KsT, QsT, Vi, QoT, Ku, Vu, dec_ap, W, evac, rk, tagk):
            order = list(range(36)) if dirn == 0 else [3, 2, 1, 0] + list(range(35, 3, -1))
            S.op('dve', lambda e: e.memset(S32[:, 0, :], 0.0), writes=['S32_0'])
            S.op('dve', lambda e: e.memset(Sbf[:, 0, :], 0.0), writes=['Sbf_0'])

            def pre(idx):
                n = order[idx]
                sl = slice(n * 64, (n + 1) * 64)
                s2 = idx % 2
                if n >= 4:
                    pst = PB[2 + s2][0:64, 0:64]; pstk = 'pb%d' % (2 + s2)
                    S.op('pe', lambda e: e.matmul(pst, lhsT=KsT[:, sl], rhs=QsT[:, sl], start=True, stop=True),
                         reads=rk, writes=[pstk], skip_self=True)
                    S.op('dve', lambda e: e.tensor_tensor(out=stm[:, s2, :], in0=pst, in1=masks[:, dirn, :], op=ALU.mult),
                         reads=[pstk, 'masks'], writes=['stm%d' % s2])
                if idx < 35:
                    pu = PB[6 + s2][:, 0:W]; puk = 'pb%d' % (6 + s2)
                    S.op('pe', lambda e: e.matmul(pu, lhsT=Ku[:, n, :], rhs=Vu[:, n, 0:W], start=True, stop=True),
                         reads=rk, writes=[puk], skip_self=True)
                    S.op('act', lambda e: e.copy(out=usb[:, s2, 0:W], in_=pu), reads=[puk], writes=['usb%d' % s2])

            pre(0)
            cur = 0
            for idx, n in enumerate(order):
                if idx + 1 < 36:
                    pre(idx + 1)
                sl = slice(n * 64, (n + 1) * 64)
                s2 = idx % 2
                if n >= 4:
                    po = PB[4 + s2][0:64, 0:W]; pok = 'pb%d' % (4 + s2)
                    S.op('pe', lambda e, po=po, s2=s2, n=n: e.matmul(po, lhsT=stm[:, s2, :], rhs=Vi[:, n, 0:W], start=True, stop=False),
                         reads=['stm%d' % s2] + rk, writes=[pok], skip_self=True)
                    S.op('pe', lambda e, po=po, sl=sl, cur=cur: e.matmul(po, lhsT=QoT[:, sl], rhs=Sbf[:, cur, 0:W], start=False, stop=True),
                         reads=['Sbf_%d' % cur] + rk, writes=[pok], skip_self=True)
                    evac(n - 4, n, po, pok)
                if idx < 35:
                    nxt = 1 - cur
                    S.op('dve', lambda e, n=n, cur=cur, nxt=nxt, s2=s2: e.scalar_tensor_tensor(out=Sbf[:, nxt, 0:W], in0=S32[:, cur, 0:W], scalar=dec_ap(n), in1=usb[:, s2, 0:W],
                                                                                            op0=ALU.mult, op1=ALU.add),
                         reads=['S32_%d' % cur, 'usb%d' % s2, tagk], writes=['Sbf_%d' % nxt])
                    S.op('dve', lambda e, n=n, cur=cur, nxt=nxt, s2=s2: e.scalar_tensor_tensor(out=S32[:, nxt, 0:W], in0=S32[:, cur, 0:W], scalar=dec_ap(n), in1=usb[:, s2, 0:W],
                                                                                            op0=ALU.mult, op1=ALU.add),
                         reads=['S32_%d' % cur, 'usb%d' % s2, tagk], writes=['S32_%d' % nxt])
                    cur = nxt

        def transpose_chunks_to_mixT(src, skey, head):
            for g in range(4):
                bank = g % 2
                for j in range(8):
                    n = g * 8 + j
                    S.op('pe', lambda e, n=n, j=j, bank=bank: e.transpose(PB[bank][:, j * 64:(j + 1) * 64], src[:, n, :], ident[0:64, 0:64]),
                         reads=list(skey) + ['ident'], writes=['pb%d' % bank], skip_self=True)
                S.op('act', lambda e, g=g, bank=bank: e.copy(out=mixT[:, head, g * 512:(g + 1) * 512], in_=PB[bank][:, :]),
                     reads=['pb%d' % bank], writes=K('mixT', head))

        with ExitStack() as g0:
            lgT = sb(g0, "lgT", [128, 2, 2, 4]); lbT = sb(g0, "lbT", [128, 2, 4]); omlT = sb(g0, "omlT", [128, 2, 4]); nomlT = sb(g0, "nomlT", [128, 2, 4])
            hgn = sb(g0, "hgn", [64, 512])
            S.dma('sp', lambda e: e.dma_start(out=lgT[:], in_=lg_d[:, :, :, :]), writes=['lgT'])
            S.dma('sp', lambda e: e.dma_start(out=hgn[:], in_=hgn_d[:, :]), writes=['hgn'])
            S.op('dve', lambda e: e.tensor_tensor(out=lbT[:], in0=lgT[:, :, 0, :], in1=lgT[:, :, 1, :], op=ALU.subtract), reads=['lgT'], writes=['lbT'])
            S.op('act', lambda e: e.activation(out=lbT[:], in_=lbT[:], func=AF.Sigmoid), reads=['lbT'], writes=['lbT'])
            S.op('dve', lambda e: e.tensor_scalar(out=omlT[:], in0=lbT[:], scalar1=-1.0, scalar2=1.0, op0=ALU.mult, op1=ALU.add), reads=['lbT'], writes=['omlT'])
            S.op('dve', lambda e: e.tensor_scalar_mul(out=nomlT[:], in0=omlT[:], scalar1=-1.0), reads=['omlT'], writes=['nomlT'])
            QsT = [sb(g0, "gQsT%d" % d, [128, NTOK], BF16) for d in range(2)]
            KsT = [sb(g0, "gKsT%d" % d, [128, NTOK], BF16) for d in range(2)]
            QoT = [sb(g0, "gQoT%d" % d, [128, NTOK], BF16) for d in range(2)]
            Ku = [sb(g0, "gKu%d" % d, [64, NCH, 128], BF16) for d in range(2)]
            dec = sb(g0, "gdec", [128, 2, NCH])
            vtm = sb(g0, "gv", [64, NCH, 128], BF16); gs = sb(g0, "ggs", [64, NLCH, 128], BF16); oacc = sb(g0, "goacc", [64, NLCH, 128])
            qf = sb(g0, "gqf", [128, 512]); sg = sb(g0, "gsg", [128, 512]); lf = sb(g0, "glf", [128, 512]); key = sb(g0, "gkey", [128, 512])
            Bc = sb(g0, "gB", [128, 512]); T1 = sb(g0, "gT1", [128, 512]); E1 = sb(g0, "gE1", [128, 512]); khT = sb(g0, "gkhT", [128, 512], BF16)
            sq = sb(g0, "gsq", [64, NLCH, 128], BF16); ss = sb(g0, "gss", [64, NLCH])
            for hd in range(4):
                with ExitStack() as hs:
                    wq_ = load_w(hs, "gwq", 0 + hd * 128, 128); wi_ = load_w(hs, "gwi", 512 + hd * 128, 128); wg_ = load_w(hs, "gwg", 1024 + hd * 128, 128)
                    wf = [load_w(hs, "gwf0", 1536 + hd * 128, 128), load_w(hs, "gwf1", 2048 + hd * 128, 128)]
                    prep_tables(64)
                    for g in range(9):
                        bk = 3 + g % 2
                        proj_tm_group(wi_, "gwi", 128, bk, g * 4, 4)
                        S.op('act', lambda e, g=g, bk=bk: e.copy(out=vtm[:, g * 4:(g + 1) * 4, :], in_=PB[bk][0:64, :].rearrange("p (j c) -> p j c", c=128)),
                             reads=['pb%d' % bk], writes=['gv'])
                    for g in range(8):
                        bk = 3 + (g + 1) % 2
                        proj_tm_group(wg_, "gwg", 128, bk, 4 + g * 4, 4)
                        S.op('act', lambda e, g=g, bk=bk: e.activation(out=gs[:, g * 4:(g + 1) * 4, :], in_=PB[bk][0:64, :].rearrange("p (j c) -> p j c", c=128), func=AF.Silu),
                             reads=['pb%d' % bk], writes=['ggs'])
                    for (t0, nt) in BLKS:
                        nck = nt // 64; c0 = t0 // 64
                        proj_fm_block(wq_, "gwq", 128, 0, t0, nt)
                        S.op('act', lambda e, nt=nt: e.copy(out=qf[:, 0:nt], in_=PB[0][:, 0:nt]), reads=['pb0'], writes=['gqf'])
                        for d in range(2):
                            proj_fm_block(wf[d], "gwf%d" % d, 128, 1 + d, t0, nt)
                            col = d * 4 + hd
                            lbp = lbT[:, d, hd:hd + 1]; omp = omlT[:, d, hd:hd + 1]; nomp = nomlT[:, d, hd:hd + 1]
                            S.op('act', lambda e, d=d, nt=nt: e.activation(out=sg[:, 0:nt], in_=PB[1 + d][:, 0:nt], func=AF.Sigmoid), reads=['pb%d' % (1 + d)], writes=['gsg'])
                            S.op('act', lambda e, nt=nt, lbp=lbp, omp=omp: e.activation(out=lf[:, 0:nt], in_=sg[:, 0:nt], func=AF.Ln, bias=lbp, scale=omp),
                                 reads=['gsg', 'lbT', 'omlT'], writes=['glf'])
                            S.op('dve', lambda e, nt=nt, nomp=nomp, omp=omp: e.tensor_scalar(out=key[:, 0:nt], in0=sg[:, 0:nt], scalar1=nomp, scalar2=omp, op0=ALU.mult, op1=ALU.add),
                                 reads=['gsg', 'omlT', 'nomlT'], writes=['gkey'])
                            S.op('dve', lambda e, nt=nt: e.tensor_tensor_scan(out=Bc[:, 0:nt], data0=rmask[:, 0:nt], data1=lf[:, 0:nt], initial=0.0, op0=ALU.mult, op1=ALU.add),
                                 reads=['glf', 'rmask'], writes=['gB'])
                            B3 = Bc[:, 0:nt].rearrange("p (n c) -> p n c", c=64)
                            T3 = T1[:, 0:nt].rearrange("p (n c) -> p n c", c=64)
                            if d == 1:
                                S.op('dve', lambda e, B3=B3, T3=T3, nck=nck: e.tensor_tensor(out=T3, in0=B3[:, :, 63:64].to_broadcast([128, nck, 64]), in1=B3, op=ALU.subtract),
                                     reads=['gB'], writes=['gT1'])
                                S.op('dve', lambda e, nt=nt: e.tensor_tensor(out=Bc[:, 0:nt], in0=T1[:, 0:nt], in1=lf[:, 0:nt], op=ALU.add), reads=['gT1', 'glf'], writes=['gB'])
                            li = 63 if d == 0 else 0
                            S.op('act', lambda e, B3=B3, li=li, d=d, c0=c0, nck=nck: e.activation(out=dec[:, d, c0:c0 + nck], in_=B3[:, :, li], func=AF.Exp), reads=['gB'], writes=['gdec'])
                            S.op('dve', lambda e, B3=B3, T3=T3, nck=nck: e.tensor_tensor(out=T3, in0=B3, in1=B3[:, :, 32:33].to_broadcast([128, nck, 64]), op=ALU.subtract),
                                 reads=['gB'], writes=['gT1'])
                            S.op('act', lambda e, nt=nt: e.activation(out=E1[:, 0:nt], in_=T1[:, 0:nt], func=AF.Exp), reads=['gT1'], writes=['gE1'])
                            S.op('dve', lambda e, nt=nt, t0=t0, d=d: e.tensor_tensor(out=QsT[d][:, t0:t0 + nt], in0=qf[:, 0:nt], in1=E1[:, 0:nt], op=ALU.mult),
                                 reads=['gqf', 'gE1'], writes=['gQsT%d' % d])
                            S.op('act', lambda e, nt=nt: e.activation(out=E1[:, 0:nt], in_=T1[:, 0:nt], func=AF.Exp, scale=-1.0), reads=['gT1'], writes=['gE1'])
                            S.op('dve', lambda e, nt=nt, t0=t0, d=d: e.tensor_tensor(out=KsT[d][:, t0:t0 + nt], in0=key[:, 0:nt], in1=E1[:, 0:nt], op=ALU.mult),
                                 reads=['gkey', 'gE1'], writes=['gKsT%d' % d])
                            S.op('act', lambda e, nt=nt: e.activation(out=E1[:, 0:nt], in_=Bc[:, 0:nt], func=AF.Exp), reads=['gB'], writes=['gE1'])
                            S.op('dve', lambda e, nt=nt, t0=t0, d=d: e.tensor_tensor(out=QoT[d][:, t0:t0 + nt], in0=qf[:, 0:nt], in1=E1[:, 0:nt], op=ALU.mult),
                                 reads=['gqf', 'gE1'], writes=['gQoT%d' % d])
                            S.op('dve', lambda e, B3=B3, T3=T3, nck=nck, li=li: e.tensor_tensor(out=T3, in0=B3[:, :, li:li + 1].to_broadcast([128, nck, 64]), in1=B3, op=ALU.subtract),
                                 reads=['gB'], writes=['gT1'])
                            S.op('act', lambda e, nt=nt: e.activation(out=E1[:, 0:nt], in_=T1[:, 0:nt], func=AF.Exp), reads=['gT1'], writes=['gE1'])
                            S.op('dve', lambda e, nt=nt: e.tensor_tensor(out=khT[:, 0:nt], in0=key[:, 0:nt], in1=E1[:, 0:nt], op=ALU.mult), reads=['gkey', 'gE1'], writes=['gkhT'])
                            pbb = PB[3][:].bitcast(BF16)
                            for j in range(nck):
                                S.op('pe', lambda e, j=j: e.transpose(pbb[0:64, j * 128:(j + 1) * 128], khT[:, j * 64:(j + 1) * 64], identb[:]),
                                     reads=['gkhT', 'identb'], writes=['pb3'], skip_self=True)
                            S.op('act', lambda e, d=d, c0=c0, nck=nck: e.copy(out=Ku[d][:, c0:c0 + nck, :], in_=pbb[0:64, 0:nck * 128].rearrange("p (j c) -> p j c", c=128)),
                                 reads=['pb3'], writes=['gKu%d' % d])
                    for d in range(2):
                        def evac(nl, n, po, pok, d=d):
                            if d == 0:
                                S.op('act', lambda e: e.copy(out=oacc[:, nl, :], in_=po), reads=[pok], writes=K('goacc', nl))
                            else:
                                S.op('dve', lambda e: e.tensor_tensor(out=oacc[:, nl, :], in0=po, in1=oacc[:, nl, :], op=ALU.add), reads=[pok] + K('goacc', nl), writes=K('goacc', nl))
                        chunk_loop(d, KsT[d], QsT[d], vtm, QoT[d], Ku[d], vtm, lambda n, d=d: dec[:, d, n:n + 1], 128, evac,
                                   ['gQsT%d' % d, 'gKsT%d' % d, 'gQoT%d' % d, 'gKu%d' % d, 'gv'], 'gdec')
                    allo = K('goacc', 0, NLCH)
                    S.op('dve', lambda e: e.tensor_tensor(out=sq[:], in0=oacc[:], in1=oacc[:], op=ALU.mult), reads=allo, writes=['gsq'])
                    S.op('dve', lambda e: e.tensor_reduce(out=ss[:], in_=sq[:], axis=AX.X, op=ALU.add), reads=['gsq'], writes=['gss'])
                    S.op('act', lambda e: e.activation(out=ss[:], in_=ss[:], func=AF.Sqrt, bias=epsc[0:64, 0:1], scale=1.0 / 128.0), reads=['gss', 'epsc'], writes=['gss'])
                    S.op('dve', lambda e: e.reciprocal(out=ss[:], in_=ss[:]), reads=['gss'], writes=['gss'])
                    S.op('dve', lambda e: e.tensor_tensor(out=oacc[:], in0=oacc[:], in1=ss[:].unsqueeze(2).to_broadcast([64, NLCH, 128]), op=ALU.mult),
                         reads=allo + ['gss'], writes=allo)
                    S.op('dve', lambda e, hd=hd: e.tensor_tensor(out=oacc[:], in0=oacc[:], in1=hgn[:, hd * 128:(hd + 1) * 128].unsqueeze(1).to_broadcast([64, NLCH, 128]), op=ALU.mult),
                         reads=allo + ['hgn'], writes=allo)
                    S.op('dve', lambda e: e.tensor_tensor(out=oacc[:], in0=oacc[:], in1=gs[:], op=ALU.mult), reads=allo + ['ggs'], writes=allo)
                    transpose_chunks_to_mixT(oacc, allo, hd)
                    S.barrier()
            S.barrier()

        with ExitStack() as m0:
            mln = sb(m0, "mln", [64, 512]); convw = sb(m0, "convw", [128, 9, 8]); convb = sb(m0, "convb", [128, 8])
            gateb = sb(m0, "gateb", [8, 2]); sel8 = sb(m0, "sel8", [8, 8]); dirm = sb(m0, "dirm", [8, 2])
            S.dma('sp', lambda e: e.dma_start(out=mln[:], in_=mln_d[:, :]), writes=['mln'])
            S.dma('sp', lambda e: e.dma_start(out=convw[:], in_=convw_d[:, :, :]), writes=['convw'])
            S.dma('sp', lambda e: e.dma_start(out=convb[:], in_=convb_d[:, :]), writes=['convb'])
            S.dma('sp', lambda e: e.dma_start(out=gateb[:], in_=gateb_d[:, :]), writes=['gateb'])
            S.dma('sp', lambda e: e.dma_start(out=sel8[:], in_=sel8_d[:, :]), writes=['sel8'])
            S.dma('sp', lambda e: e.dma_start(out=dirm[:], in_=dirm_d[:, :]), writes=['dirm'])
            RUU = sb(m0, "RUU", [64, NCH, 24]); dchunk = sb(m0, "dchunk", [128, 8, NCH])
            with ExitStack() as gp:
                wgi = load_w(gp, "mwgi", 4608, 8); wgf = load_w(gp, "mwgf", 4616, 8)
                LI = sb(gp, "LI", [8, NTOK]); LF = sb(gp, "LF", [8, NTOK]); Af = sb(gp, "Af", [8, NTOK]); Ab = sb(gp, "Ab", [8, NTOK]); Aa = sb(gp, "Aa", [8, NTOK])
                R = [sb(gp, "Rr%d" % i, [8, NTOK]) for i in range(3)]
                bd = sb(gp, "bd", [8, 8, NCH])
                for (t0, nt) in BLKS:
                    proj_fm_block(wgi, "mwgi", 8, 0, t0, nt)
                    S.op('act', lambda e, t0=t0, nt=nt: e.copy(out=LI[:, t0:t0 + nt], in_=PB[0][0:8, 0:nt]), reads=['pb0'], writes=['LI'])
                    proj_fm_block(wgf, "mwgf", 8, 1, t0, nt)
                    S.op('act', lambda e, t0=t0, nt=nt: e.copy(out=LF[:, t0:t0 + nt], in_=PB[1][0:8, 0:nt]), reads=['pb1'], writes=['LF'])
                S.op('dve', lambda e: e.tensor_scalar_add(out=LI[:], in0=LI[:], scalar1=gateb[:, 0:1]), reads=['LI', 'gateb'], writes=['LI'])
                S.op('act', lambda e: e.activation(out=LF[:], in_=LF[:], func=AF.Sigmoid, bias=gateb[:, 1:2], scale=1.0), reads=['LF', 'gateb'], writes=['LF'])
                S.op('act', lambda e: e.activation(out=LF[:], in_=LF[:], func=AF.Ln), reads=['LF'], writes=['LF'])
                for (t0, nt) in BLKS:
                    S.op('dve', lambda e, t0=t0, nt=nt: e.tensor_tensor_scan(out=Af[:, t0:t0 + nt], data0=rmask[0:8, 0:nt], data1=LF[:, t0:t0 + nt], initial=0.0, op0=ALU.mult, op1=ALU.add),
                         reads=['LF', 'rmask'], writes=['Af'])
                A3 = Af[:].rearrange("p (n c) -> p n c", c=64)
                tot = A3[:, :, 63:64]
                S.op('dve', lambda e: e.tensor_tensor(out=Ab[:].rearrange("p (n c) -> p n c", c=64), in0=tot.to_broadcast([8, NCH, 64]), in1=A3, op=ALU.subtract), reads=['Af'], writes=['Ab'])
                S.op('dve', lambda e: e.tensor_tensor(out=Ab[:], in0=Ab[:], in1=LF[:], op=ALU.add), reads=['Ab', 'LF'], writes=['Ab'])
                S.op('dve', lambda e: e.tensor_scalar_mul(out=Aa[:], in0=Af[:], scalar1=dirm[:, 0:1]), reads=['Af', 'dirm'], writes=['Aa'])
                S.op('dve', lambda e: e.scalar_tensor_tensor(out=Aa[:], in0=Ab[:], scalar=dirm[:, 1:2], in1=Aa[:], op0=ALU.mult, op1=ALU.add), reads=['Ab', 'dirm', 'Aa'], writes=['Aa'])
                S.op('act', lambda e: e.activation(out=R[0][:], in_=Aa[:], func=AF.Exp), reads=['Aa'], writes=['Rr0'])
                S.op('dve', lambda e: e.tensor_tensor(out=Ab[:], in0=LI[:], in1=Aa[:], op=ALU.subtract), reads=['LI', 'Aa', 'Ab'], writes=['Ab'])
                S.op('act', lambda e: e.activation(out=R[1][:], in_=Ab[:], func=AF.Exp), reads=['Ab'], writes=['Rr1'])
                S.op('dve', lambda e: e.tensor_tensor(out=Aa[:].rearrange("p (n c) -> p n c", c=64), in0=Ab[:].rearrange("p (n c) -> p n c", c=64), in1=tot.to_broadcast([8, NCH, 64]), op=ALU.add),
                     reads=['Ab', 'Af', 'Aa', 'Rr0'], writes=['Aa'])
                S.op('act', lambda e: e.activation(out=R[2][:], in_=Aa[:], func=AF.Exp), reads=['Aa'], writes=['Rr2'])
                for half in range(2):
                    for j in range(18):
                        n = half * 18 + j
                        for q in range(3):
                            S.op('pe', lambda e, n=n, j=j, q=q: e.transpose(PB[2][0:64, j * 24 + q * 8:j * 24 + q * 8 + 8], R[q][:, n * 64:(n + 1) * 64], ident[0:8, 0:8]),
                                 reads=['Rr%d' % q, 'ident'], writes=['pb2'], skip_self=True)
                    S.op('act', lambda e, half=half: e.copy(out=RUU[:, half * 18:(half + 1) * 18, :], in_=PB[2][0:64, 0:432].rearrange("p (j c) -> p j c", c=24)),
                         reads=['pb2'], writes=['RUU'])
                S.op('dve', lambda e: e.tensor_tensor(out=bd[:], in0=tot.rearrange("p n c -> p c n").to_broadcast([8, 8, NCH]), in1=sel8[:].unsqueeze(2).to_broadcast([8, 8, NCH]), op=ALU.mult),
                     reads=['Af', 'sel8'], writes=['bd'])
                S.op('pe', lambda e: e.matmul(PB[3][:, 0:288], lhsT=ones[0:8, :], rhs=bd[:].rearrange("p a n -> p (a n)"), start=True, stop=True), reads=['ones', 'bd'], writes=['pb3'], skip_self=True)
                S.op('act', lambda e: e.activation(out=dchunk[:].rearrange("p a n -> p (a n)"), in_=PB[3][:, 0:288], func=AF.Exp), reads=['pb3'], writes=['dchunk'])
                S.barrier()
            qc = sb(m0, "mqc", [128, NTOK], BF16); kc_ = sb(m0, "mkc", [128, NTOK], BF16); kTM = sb(m0, "mkTM", [64, NCH, 128], BF16)
            raw = sb(m0, "mraw", [128, NTOK]); acc = sb(m0, "macc", [128, NTOK])
            vext = sb(m0, "mvext", [64, NCH, 132], BF16); vi = sb(m0, "mvi", [64, NCH, 132], BF16); vu = sb(m0, "mvu", [64, NCH, 132], BF16)
            og = sb(m0, "mog", [64, NLCH, 128], BF16); oext = sb(m0, "moext", [64, NLCH, 132]); obuf = sb(m0, "mobuf", [64, NLCH, 128])
            den = sb(m0, "mden", [64, NLCH]); mu = sb(m0, "mmu", [64, NLCH]); m2 = sb(m0, "mm2", [64, NLCH])
            S.op('dve', lambda e: e.memset(vext[:], 1.0), writes=['mvext'])
            for hd in range(4):
                with ExitStack() as hs:
                    wq_ = load_w(hs, "mwq", 2560 + hd * 128, 128); wk_ = load_w(hs, "mwk", 3072 + hd * 128, 128)
                    wv_ = load_w(hs, "mwv", 3584 + hd * 128, 128); wo_ = load_w(hs, "mwo", 4096 + hd * 128, 128)
                    prep_tables(64)
                    for g in range(9):
                        bk = 3 + g % 2
                        proj_tm_group(wv_, "mwv", 128, bk, g * 4, 4)
                        S.op('act', lambda e, g=g, bk=bk: e.copy(out=vext[:, g * 4:(g + 1) * 4, 0:128], in_=PB[bk][0:64, :].rearrange("p (j c) -> p j c", c=128)),
                             reads=['pb%d' % bk], writes=['mvext'])
                    for g in range(8):
                        bk = 3 + (g + 1) % 2
                        proj_tm_group(wo_, "mwo", 128, bk, 4 + g * 4, 4)
                        S.op('act', lambda e, g=g, bk=bk: e.activation(out=og[:, g * 4:(g + 1) * 4, :], in_=PB[bk][0:64, :].rearrange("p (j c) -> p j c", c=128), func=AF.Sigmoid),
                             reads=['pb%d' % bk], writes=['mog'])
                    for qi, (wb, wn, dstb) in enumerate(((wq_, "mwq", qc), (wk_, "mwk", kc_))):
                        chn = qi * 4 + hd
                        for bi, (t0, nt) in enumerate(BLKS):
                            proj_fm_block(wb, wn, 128, bi % 2, t0, nt)
                            S.op('act', lambda e, t0=t0, nt=nt, bi=bi: e.copy(out=raw[:, t0:t0 + nt], in_=PB[bi % 2][:, 0:nt]), reads=['pb%d' % (bi % 2)], writes=['mraw'])
                        S.op('dve', lambda e, chn=chn: e.tensor_scalar(out=acc[:, 0:256], in0=raw[:, 0:256], scalar1=convw[:, 4, chn:chn + 1], scalar2=convb[:, chn:chn + 1], op0=ALU.mult, op1=ALU.add),
                             reads=['mraw', 'convw', 'convb'], writes=['macc'])
                        S.op('dve', lambda e, chn=chn: e.scalar_tensor_tensor(out=acc[:, 1:256], in0=raw[:, 0:255], scalar=convw[:, 3, chn:chn + 1], in1=acc[:, 1:256], op0=ALU.mult, op1=ALU.add),
                             reads=['mraw', 'convw', 'macc'], writes=['macc'])
                        S.op('dve', lambda e, chn=chn: e.scalar_tensor_tensor(out=acc[:, 0:255], in0=raw[:, 1:256], scalar=convw[:, 5, chn:chn + 1], in1=acc[:, 0:255], op0=ALU.mult, op1=ALU.add),
                             reads=['mraw', 'convw', 'macc'], writes=['macc'])
                        X = raw[:, 256:NTOK].rearrange("p (r c) -> p r c", c=64); Y = acc[:, 256:NTOK].rearrange("p (r c) -> p r c", c=64)
                        S.op('dve', lambda e, chn=chn: e.tensor_scalar(out=acc[:, 256:NTOK], in0=raw[:, 256:NTOK], scalar1=convw[:, 4, chn:chn + 1], scalar2=convb[:, chn:chn + 1], op0=ALU.mult, op1=ALU.add),
                             reads=['mraw', 'convw', 'convb', 'macc'], writes=['macc'])
                        for ky in range(3):
                            for kx in range(3):
                                if ky == 1 and kx == 1:
                                    continue
                                dy = ky - 1; dx = kx - 1
                                r0 = max(0, -dy); r1 = 32 - max(0, dy); c0 = max(0, -dx); c1 = 64 - max(0, dx)
                                S.op('dve', lambda e, chn=chn, ky=ky, kx=kx, r0=r0, r1=r1, c0=c0, c1=c1, dy=dy, dx=dx: e.scalar_tensor_tensor(
                                    out=Y[:, r0:r1, c0:c1], in0=X[:, r0 + dy:r1 + dy, c0 + dx:c1 + dx], scalar=convw[:, ky * 3 + kx, chn:chn + 1], in1=Y[:, r0:r1, c0:c1],
                                    op0=ALU.mult, op1=ALU.add), reads=['mraw', 'convw', 'macc'], writes=['macc'])
                        S.op('act', lambda e: e.activation(out=acc[:], in_=acc[:], func=AF.Silu), reads=['macc'], writes=['macc'])
                        if qi == 0:
                            S.op('dve', lambda e: e.tensor_copy(out=qc[:], in_=acc[:]), reads=['macc'], writes=['mqc'])
                        else:
                            S.op('dve', lambda e: e.tensor_scalar_mul(out=kc_[:], in0=acc[:], scalar1=128.0 ** -0.5), reads=['macc'], writes=['mkc'])
                    pbb = PB[3][:].bitcast(BF16)
                    for g in range(5):
                        nck = 8 if g < 4 else 4
                        for j in range(nck):
                            n = g * 8 + j
                            S.op('pe', lambda e, j=j, n=n: e.transpose(pbb[0:64, j * 128:(j + 1) * 128], kc_[:, n * 64:(n + 1) * 64], identb[:]),
                                 reads=['mkc', 'identb'], writes=['pb3'], skip_self=True)
                        S.op('act', lambda e, g=g, nck=nck: e.copy(out=kTM[:, g * 8:g * 8 + nck, :], in_=pbb[0:64, 0:nck * 128].rearrange("p (j c) -> p j c", c=128)),
                             reads=['pb3'], writes=['mkTM'])
                    for d in range(2):
                        row = d * 4 + hd
                        S.op('dve', lambda e, row=row: e.tensor_tensor(out=vi[:], in0=vext[:], in1=RUU[:, :, 8 + row:9 + row].to_broadcast([64, NCH, 132]), op=ALU.mult),
                             reads=['mvext', 'RUU'], writes=['mvi'])
                        S.op('dve', lambda e, row=row: e.tensor_tensor(out=vu[:], in0=vext[:], in1=RUU[:, :, 16 + row:17 + row].to_broadcast([64, NCH, 132]), op=ALU.mult),
                             reads=['mvext', 'RUU'], writes=['mvu'])

                        def evac(nl, n, po, pok, row=row):
                            S.op('act', lambda e: e.copy(out=oext[:, nl, 0:129], in_=po), reads=[pok], writes=K('moext', nl))
                        chunk_loop(d, kc_, qc, vi, qc, kTM, vu, lambda n, row=row: dchunk[:, row, n:n + 1], 129, evac,
                                   ['mqc', 'mkc', 'mvi', 'mvu', 'mkTM'], 'dchunk')
                        allx = K('moext', 0, NLCH)
                        S.op('dve', lambda e, row=row: e.tensor_tensor(out=oext[:, :, 0:129], in0=oext[:, :, 0:129], in1=RUU[:, 4:NCH, row:row + 1].to_broadcast([64, NLCH, 129]), op=ALU.mult),
                             reads=allx + ['RUU'], writes=allx)
                        S.op('act', lambda e: e.activation(out=den[:], in_=oext[:, :, 128], func=AF.Abs), reads=allx, writes=['mden'])
                        S.op('dve', lambda e: e.tensor_scalar_max(out=den[:], in0=den[:], scalar1=1.0), reads=['mden'], writes=['mden'])
                        S.op('dve', lambda e: e.reciprocal(out=den[:], in_=den[:]), reads=['mden'], writes=['mden'])
                        if d == 0:
                            S.op('dve', lambda e: e.tensor_tensor(out=obuf[:], in0=oext[:, :, 0:128], in1=den[:].unsqueeze(2).to_broadcast([64, NLCH, 128]), op=ALU.mult),
                                 reads=allx + ['mden'], writes=['mobuf'])
                        else:
                            S.op('dve', lambda e: e.tensor_tensor(out=oext[:, :, 0:128], in0=oext[:, :, 0:128], in1=den[:].unsqueeze(2).to_broadcast([64, NLCH, 128]), op=ALU.mult),
                                 reads=allx + ['mden'], writes=allx)
                            S.op('dve', lambda e: e.tensor_tensor(out=obuf[:], in0=obuf[:], in1=oext[:, :, 0:128], op=ALU.add), reads=allx + ['mobuf'], writes=['mobuf'])
                    allx = K('moext', 0, NLCH)
                    S.op('dve', lambda e: e.tensor_reduce(out=mu[:], in_=obuf[:], axis=AX.X, op=ALU.add), reads=['mobuf'], writes=['mmu'])
                    S.op('dve', lambda e: e.tensor_scalar_mul(out=mu[:], in0=mu[:], scalar1=1.0 / 128.0), reads=['mmu'], writes=['mmu'])
                    S.op('dve', lambda e: e.tensor_tensor(out=obuf[:], in0=obuf[:], in1=mu[:].unsqueeze(2).to_broadcast([64, NLCH, 128]), op=ALU.subtract), reads=['mobuf', 'mmu'], writes=['mobuf'])
                    S.op('dve', lambda e: e.tensor_tensor(out=oext[:, :, 0:128], in0=obuf[:], in1=obuf[:], op=ALU.mult), reads=['mobuf'] + allx, writes=allx)
                    S.op('dve', lambda e: e.tensor_reduce(out=m2[:], in_=oext[:, :, 0:128], axis=AX.X, op=ALU.add), reads=allx, writes=['mm2'])
                    S.op('act', lambda e: e.activation(out=m2[:], in_=m2[:], func=AF.Sqrt, bias=epsc[0:64, 0:1], scale=1.0 / 128.0), reads=['mm2', 'epsc'], writes=['mm2'])
                    S.op('dve', lambda e: e.reciprocal(out=m2[:], in_=m2[:]), reads=['mm2'], writes=['mm2'])
                    S.op('dve', lambda e: e.tensor_tensor(out=obuf[:], in0=obuf[:], in1=m2[:].unsqueeze(2).to_broadcast([64, NLCH, 128]), op=ALU.mult), reads=['mobuf', 'mm2'], writes=['mobuf'])
                    S.op('dve', lambda e, hd=hd: e.tensor_tensor(out=obuf[:], in0=obuf[:], in1=mln[:, hd * 128:(hd + 1) * 128].unsqueeze(1).to_broadcast([64, NLCH, 128]), op=ALU.mult),
                         reads=['mobuf', 'mln'], writes=['mobuf'])
                    S.op('dve', lambda e: e.tensor_tensor(out=obuf[:], in0=obuf[:], in1=og[:], op=ALU.mult), reads=['mobuf', 'mog'], writes=['mobuf'])
                    transpose_chunks_to_mixT(obuf, ['mobuf'], 4 + hd)
                    S.barrier()
            S.barrier()

        if debug == 'mix':
            with ExitStack() as dd:
                mf = sb(dd, "mf", [128, NLAT])
                for h in range(8):
                    S.op('dve', lambda e, h=h: e.tensor_copy(out=mf[:], in_=mixT[:, h, :]), reads=K('mixT', h) + ['mf'], writes=['mf'])
                    S.dma('sp', lambda e, h=h: e.dma_start(out=dbg_d[h * 128:(h + 1) * 128, :], in_=mf[:]), reads=['mf'], writes=['dbg'])
                S.wait_all('sp', ['dbg']); S.barrier()
            scB.close(); scA.close()
            return nc
        prep_tables(512)
        S.barrier()
        scB.close()
        x1s = nc.dram_tensor("x1s", [NLAT, 1024], F32, kind="Internal").ap()

        def bcast_tile(dst, dkey, c0, dg):
            for ch in range(8):
                S.op('dve', lambda e, ch=ch: e.tensor_scalar_mul(out=dg[:], in0=ident[:], scalar1=modv[:, c0 + ch, 0:1]), reads=['ident', 'modv', 'dg'], writes=['dg'])
                bank = ch // 4
                S.op('pe', lambda e, ch=ch, bank=bank: e.matmul(PB[bank][:, (ch % 4) * 128:(ch % 4 + 1) * 128], lhsT=ones[:], rhs=dg[:], start=True, stop=True),
                     reads=['ones', 'dg'], writes=['pb%d' % bank], skip_self=True)
            S.op('act', lambda e: e.copy(out=dst[:, 0:512], in_=PB[0][:, :]), reads=['pb0'], writes=[dkey])
            S.op('act', lambda e: e.copy(out=dst[:, 512:1024], in_=PB[1][:, :]), reads=['pb1'], writes=[dkey])

        with ExitStack() as p3:
            bct = {}
            for nm in ("g1b", "ln1g", "ln1b"):
                bct[nm] = sb(p3, nm, [128, 1024])
            for nm, dd in (("ln1g", ln1g_d), ("ln1b", ln1b_d)):
                S.dma('sp', lambda e, nm=nm, dd=dd: e.dma_start(out=bct[nm][:], in_=dd[:, :]), writes=[nm])
            dg = sb(p3, "dg", [128, 128])
            bcast_tile(bct["g1b"], "g1b", 16, dg)
            wo_b = sb(p3, "wout", [128, 8, 1024], BF16); wos = sb(p3, "wos", [128, 1024])
            for kc in range(8):
                S.dma('sp', lambda e, kc=kc: e.dma_start(out=wos[:], in_=wout_d[:, kc, :]), writes=['wos'])
                S.op('pool', lambda e, kc=kc: e.tensor_copy(out=wo_b[:, kc, :], in_=wos[:]), reads=['wos'], writes=['wout'])
            xt = [sb(p3, "x3t%d" % i, [128, 1024]) for i in range(2)]
            t1 = [sb(p3, "t1_%d" % i, [128, 1024]) for i in range(2)]
            st = [sb(p3, "st3%d" % i, [128, 2, 6]) for i in range(2)]
            mv = [sb(p3, "mv3%d" % i, [128, 2]) for i in range(2)]
            rs = [sb(p3, "rs3%d" % i, [128, 1]) for i in range(2)]
            for t in range(16):
                b = t % 2
                S.dma('sp' if b == 0 else 'pool', lambda e, t=t, b=b: e.dma_start(out=xt[b][:], in_=xs[256 + t * 128:256 + (t + 1) * 128, :]), writes=['x3t%d' % b])
                for half in range(2):
                    bank = 2 * b + half
                    for kc in range(8):
                        S.op('pe', lambda e, kc=kc, t=t, half=half, bank=bank: e.matmul(PB[bank][:, :], lhsT=mixT[:, kc, t * 128:(t + 1) * 128], rhs=wo_b[:, kc, half * 512:(half + 1) * 512],
                                                                                      start=(kc == 0), stop=(kc == 7)),
                             reads=K('mixT', kc) + ['wout'], writes=['pb%d' % bank], skip_self=True)
                    S.op('dve', lambda e, b=b, half=half, bank=bank: e.tensor_tensor(out=t1[b][:, half * 512:(half + 1) * 512], in0=PB[bank][:, :], in1=bct["g1b"][:, half * 512:(half + 1) * 512], op=ALU.mult),
                         reads=['pb%d' % bank, 'g1b'], writes=['t1_%d' % b])
                S.op('dve', lambda e, b=b: e.scalar_tensor_tensor(out=t1[b][:], in0=xt[b][:], scalar=ALPHA, in1=t1[b][:], op0=ALU.mult, op1=ALU.add),
                     reads=['x3t%d' % b, 't1_%d' % b], writes=['t1_%d' % b])
                layer_norm_rows(st[b], mv[b][:], rs[b][:], t1[b][:], t1[b][:], 't1_%d' % b, 't1_%d' % b, 'p3%d' % b)
                S.op('dve', lambda e, b=b: e.tensor_tensor(out=t1[b][:], in0=t1[b][:], in1=bct["ln1g"][:], op=ALU.mult), reads=['t1_%d' % b, 'ln1g'], writes=['t1_%d' % b])
                S.op('dve', lambda e, b=b, t=t: e.tensor_tensor(out=t1[b][:], in0=t1[b][:], in1=bct["ln1b"][:], op=ALU.add), reads=['t1_%d' % b, 'ln1b'], writes=['t1_%d' % b])
                S.dma('sp', lambda e, t=t, b=b: e.dma_start(out=x1s[t * 128:(t + 1) * 128, :], in_=t1[b][:]), reads=['t1_%d' % b], writes=K('x1_', t))
                if debug == 'x1':
                    S.dma('sp', lambda e, t=t, b=b: e.dma_start(out=dbg_d[t * 128:(t + 1) * 128, :], in_=t1[b][:]), reads=['t1_%d' % b], writes=['dbg'])
            S.barrier()
        scA.close()

        with ExitStack() as p4:
            bct = {}
            for nm in ("g2b", "sc2b", "sh2b", "ln2g", "ln2b"):
                bct[nm] = sb(p4, nm, [128, 1024])
            for nm, dd in (("ln2g", ln2g_d), ("ln2b", ln2b_d)):
                S.dma('sp', lambda e, nm=nm, dd=dd: e.dma_start(out=bct[nm][:], in_=dd[:, :]), writes=[nm])
            dg = sb(p4, "dg4", [128, 128])
            bcast_tile(bct["g2b"], "g2b", 40, dg); bcast_tile(bct["sc2b"], "sc2b", 32, dg); bcast_tile(bct["sh2b"], "sh2b", 24, dg)
            x1t = [sb(p4, "x1t%d" % i, [128, 1024]) for i in range(2)]
            wqb = sb(p4, "wqb", [128, 8, 2048], BF16)
            keysT = sb(p4, "keysT", [128, 16, 128], BF16)
            with ExitStack() as tmp:
                stg = sb(tmp, "wq_stg", [128, 2048])
                for kc in range(8):
                    S.dma('sp', lambda e, kc=kc: e.dma_start(out=stg[:], in_=wq_d[:, kc, :]), writes=['wq_stg'])
                    S.op('pool', lambda e, kc=kc: e.tensor_copy(out=wqb[:, kc, :], in_=stg[:]), reads=['wq_stg'], writes=['wqb'])
                kst = sb(tmp, "kst", [128, 16, 128])
                S.dma('sp', lambda e: e.dma_start(out=kst[:], in_=keysT_d[:, :, :]), writes=['kst'])
                S.op('pool', lambda e: e.tensor_copy(out=keysT[:], in_=kst[:]), reads=['kst'], writes=['keysT'])
                S.barrier()
            NG = 8
            uvb = [sb(p4, "uvb%d" % i, [128, 2048], BF16) for i in range(NG)]
            gl = sb(p4, "gl", [128, 128])
            pr = [sb(p4, "pr%d" % i, [128, 1024], BF16) for i in range(4)]
            dgk = [sb(p4, "dgk%d" % i, [128, 128], BF16) for i in range(4)]
            h2 = sb(p4, "h2", [128, 1024]); h2b = [sb(p4, "h2b%d" % i, [128, 1024], BF16) for i in range(2)]
            h2T = sb(p4, "h2T", [128, 8, 128], BF16); qT = sb(p4, "qT", [128, 16, 128], BF16)
            sc = sb(p4, "sc", [128, 16, 128]); scw = sb(p4, "scw", [128, 16, 128])
            top = sb(p4, "top", [128, 16, 16]); topi = sb(p4, "topi", [128, 16, 16], U32); topf = sb(p4, "topf", [128, 16, 16])
            cand = sb(p4, "cand", [128, 8, 256]); candw = sb(p4, "candw", [128, 8, 256]); eq = sb(p4, "eq", [128, 128, 16])
            best = sb(p4, "best", [128, 8, 16]); idxf = sb(p4, "idxf", [128, 128])
            posi = sb(p4, "posi", [128, 8, 16], U32); pai = sb(p4, "pai", [128, 128], U32); pbi = sb(p4, "pbi", [128, 128], U32)
            paf = sb(p4, "paf", [128, 128]); pbf = sb(p4, "pbf", [128, 128]); iaf = sb(p4, "iaf", [128, 128]); ibf = sb(p4, "ibf", [128, 128])
            iota16 = sb(p4, "iota16", [128, 16])
            S.dma('sp', lambda e: e.dma_start(out=iota16[:], in_=iota_d[:, :]), writes=['iota16'])
            gate = [sb(p4, "gate%d" % i, [128, 128]) for i in range(2)]
            idxi = [sb(p4, "idxi%d" % i, [128, 128], I32) for i in range(2)]
            wgt = [sb(p4, "wgt%d" % i, [128, 128]) for i in range(2)]
            dots = sb(p4, "dots", [128, 128]); junk = sb(p4, "junk", [128, 1024], BF16); junk2 = sb(p4, "junk2", [128, 256])
            zs = sb(p4, "zs", [128, 8]); nmx = sb(p4, "nmx", [128, 8])
            st = sb(p4, "st4", [128, 2, 6]); mv = sb(p4, "mv4", [128, 2]); rs = sb(p4, "rs4", [128, 1])
            fin = sb(p4, "fin", [128, 1024]); yb = sb(p4, "yb", [128, 1024])
            NT4 = 16 if debug != 'x1' else 0

            def prologue(t):
                p = t % 2
                xb_ = x1t[p]; x1k = 'x1t%d' % p
                S.dma('sp', lambda e: e.dma_start(out=xb_[:], in_=x1s[t * 128:(t + 1) * 128, :]), reads=K('x1_', t), writes=[x1k])
                S.op('dve', lambda e: e.memset(idxf[:], 0.0), writes=['idxf'])
                S.op('dve', lambda e: e.memset(zs[:], 0.0), writes=['zs'])
                layer_norm_rows(st, mv[:], rs[:], xb_[:], h2[:], x1k, 'h2', 'p4')
                S.op('dve', lambda e: e.tensor_tensor(out=h2[:], in0=h2[:], in1=bct["sc2b"][:], op=ALU.mult), reads=['h2', 'sc2b'], writes=['h2'])
                S.op('dve', lambda e: e.tensor_tensor(out=h2[:], in0=h2[:], in1=bct["sh2b"][:], op=ALU.add), reads=['h2', 'sh2b'], writes=['h2'])
                S.op('act', lambda e: e.copy(out=h2b[p][:], in_=h2[:]), reads=['h2'], writes=['h2b%d' % p])
                yield
                for half in range(2):
                    for c4 in range(4):
                        ch = half * 4 + c4
                        S.op('pe', lambda e, ch=ch, c4=c4, half=half: e.transpose(PB[half][:, c4 * 128:(c4 + 1) * 128], h2[:, ch * 128:(ch + 1) * 128], ident[:]),
                             reads=['h2', 'ident'], writes=['pb%d' % half], skip_self=True)
                    yield
                    S.op('act', lambda e, half=half: e.copy(out=h2T[:, half * 4:(half + 1) * 4, :], in_=PB[half][:, :].rearrange("p (j c) -> p j c", c=128)), reads=['pb%d' % half], writes=['h2T'])
                for g in range(4):
                    for c4 in range(4):
                        c = g * 4 + c4
                        for kc in range(8):
                            S.op('pe', lambda e, c=c, c4=c4, kc=kc, g=g: e.matmul(PB[2 + g][:, c4 * 128:(c4 + 1) * 128], lhsT=wqb[:, kc, c * 128:(c + 1) * 128], rhs=h2T[:, kc, :],
                                                                                start=(kc == 0), stop=(kc == 7)), reads=['wqb', 'h2T'], writes=['pb%d' % (2 + g)], skip_self=True)
                    yield
                    S.op('act' if g % 2 == 0 else 'dve', lambda e, g=g: (e.copy if g % 2 == 0 else e.tensor_copy)(out=qT[:, g * 4:(g + 1) * 4, :], in_=PB[2 + g][:, :].rearrange("p (j c) -> p j c", c=128)),
                         reads=['pb%d' % (2 + g)], writes=['qT'])
                for g in range(4):
                    for c4 in range(4):
                        c = g * 4 + c4
                        S.op('pe', lambda e, c=c, c4=c4, g=g: e.matmul(PB[2 + g][:, c4 * 128:(c4 + 1) * 128], lhsT=qT[:, c, :], rhs=keysT[:, c, :], start=True, stop=True),
                             reads=['qT', 'keysT'], writes=['pb%d' % (2 + g)], skip_self=True)
                    yield
                    S.op('act' if g % 2 == 0 else 'dve', lambda e, g=g: (e.copy if g % 2 == 0 else e.tensor_copy)(out=sc[:, g * 4:(g + 1) * 4, :], in_=PB[2 + g][:, :].rearrange("p (j c) -> p j c", c=128)),
                         reads=['pb%d' % (2 + g)], writes=['sc'])
                for c in range(16):
                    S.op('dve', lambda e, c=c: e.max(out=top[:, c, 0:8], in_=sc[:, c, :]), reads=['sc'], writes=['top'])
                    S.op('dve', lambda e, c=c: e.max_index(out=topi[:, c, 0:8], in_max=top[:, c, 0:8], in_values=sc[:, c, :]), reads=['sc', 'top'], writes=['topi'])
                    S.op('dve', lambda e, c=c: e.match_replace(out=scw[:, c, :], in_to_replace=top[:, c, 0:8], in_values=sc[:, c, :], imm_value=-1e30), reads=['sc', 'top'], writes=['scw'])
                    S.op('dve', lambda e, c=c: e.max(out=top[:, c, 8:16], in_=scw[:, c, :]), reads=['scw'], writes=['top'])
                    S.op('dve', lambda e, c=c: e.max_index(out=topi[:, c, 8:16], in_max=top[:, c, 8:16], in_values=scw[:, c, :]), reads=['scw', 'top'], writes=['topi'])
                    yield
                S.op('dve', lambda e: e.tensor_copy(out=topf[:], in_=topi[:]), reads=['topi'], writes=['topf'])
                t4 = top[:].rearrange("p (h two) k -> p h two k", two=2); f4 = topf[:].rearrange("p (h two) k -> p h two k", two=2)
                c4v = cand[:].rearrange("p h (a b) -> p h a b", b=16)
                for h in range(8):
                    S.op('dve', lambda e, h=h: e.tensor_tensor(out=c4v[:, h, :, :], in0=t4[:, h, 0, :].unsqueeze(2).to_broadcast([128, 16, 16]), in1=t4[:, h, 1, :].unsqueeze(1).to_broadcast([128, 16, 16]), op=ALU.add),
                         reads=['top'], writes=['cand'])
                    yield
                for h in range(8):
                    S.op('dve', lambda e, h=h: e.max(out=best[:, h, 0:8], in_=cand[:, h, :]), reads=['cand'], writes=['best'])
                    S.op('dve', lambda e, h=h: e.match_replace(out=candw[:, h, :], in_to_replace=best[:, h, 0:8], in_values=cand[:, h, :], imm_value=-1e30), reads=['cand', 'best'], writes=['candw'])
                    S.op('dve', lambda e, h=h: e.max(out=best[:, h, 8:16], in_=candw[:, h, :]), reads=['candw'], writes=['best'])
                    S.op('dve', lambda e, h=h: e.max_index(out=posi[:, h, 0:8], in_max=best[:, h, 0:8], in_values=cand[:, h, :]), reads=['cand', 'best'], writes=['posi'])
                    S.op('dve', lambda e, h=h: e.max_index(out=posi[:, h, 8:16], in_max=best[:, h, 8:16], in_values=candw[:, h, :]), reads=['candw', 'best'], writes=['posi'])
                    yield
                pflat = posi[:].rearrange("p h k -> p (h k)")
                S.op('dve', lambda e: e.tensor_single_scalar(out=pai[:], in_=pflat, scalar=4, op=ALU.logical_shift_right), reads=['posi'], writes=['pai'])
                S.op('dve', lambda e: e.tensor_single_scalar(out=pbi[:], in_=pflat, scalar=15, op=ALU.bitwise_and), reads=['posi'], writes=['pbi'])
                S.op('dve', lambda e: e.tensor_copy(out=paf[:], in_=pai[:]), reads=['pai'], writes=['paf'])
                S.op('dve', lambda e: e.tensor_copy(out=pbf[:], in_=pbi[:]), reads=['pbi'], writes=['pbf'])
                yield
                eq4 = eq[:].rearrange("p (h k) a -> p h k a", k=16)
                for (pp, pk_, half_, dst, dn) in ((paf, 'paf', 0, iaf, 'iaf'), (pbf, 'pbf', 1, ibf, 'ibf')):
                    S.op('dve', lambda e, pp=pp: e.tensor_tensor(out=eq[:], in0=iota16[:].unsqueeze(1).to_broadcast([128, 128, 16]), in1=pp[:].unsqueeze(2).to_broadcast([128, 128, 16]), op=ALU.is_equal),
                         reads=['iota16', pk_, 'eq'], writes=['eq'])
                    S.op('dve', lambda e, half_=half_: e.tensor_tensor(out=eq4, in0=eq4, in1=f4[:, :, half_, :].unsqueeze(2).to_broadcast([128, 8, 16, 16]), op=ALU.mult),
                         reads=['eq', 'topf'], writes=['eq'])
                    S.op('dve', lambda e, dst=dst: e.tensor_reduce(out=dst[:], in_=eq[:], axis=AX.X, op=ALU.add), reads=['eq'], writes=[dn])
                    yield
                S.op('dve', lambda e: e.scalar_tensor_tensor(out=idxf[:], in0=iaf[:], scalar=128.0, in1=ibf[:], op0=ALU.mult, op1=ALU.add), reads=['iaf', 'ibf'], writes=['idxf'])
                S.op('dve', lambda e: e.tensor_scalar_min(out=idxf[:], in0=idxf[:], scalar1=16383.0), reads=['idxf'], writes=['idxf'])
                S.op('dve', lambda e: e.tensor_copy(out=idxi[p][:], in_=idxf[:]), reads=['idxf'], writes=['idxi%d' % p])
                S.op('dve', lambda e: e.tensor_scalar_mul(out=nmx[:], in0=best[:, :, 0], scalar1=-1.0), reads=['best'], writes=['nmx'])
                g3 = gate[p][:].rearrange("p (h k) -> p h k", k=16)
                for h in range(8):
                    S.op('act', lambda e, h=h: e.activation(out=g3[:, h, :], in_=best[:, h, :], func=AF.Exp, bias=nmx[:, h:h + 1], scale=1.0, accum_out=zs[:, h:h + 1]),
                         reads=['best', 'nmx'], writes=['gate%d' % p, 'zs'])
                S.op('dve', lambda e: e.reciprocal(out=zs[:], in_=zs[:]), reads=['zs'], writes=['zs'])
                S.op('dve', lambda e: e.tensor_tensor(out=g3, in0=g3, in1=zs[:].unsqueeze(2).to_broadcast([128, 8, 16]), op=ALU.mult), reads=['gate%d' % p, 'zs'], writes=['gate%d' % p])

            def fused(t, gen=None):
                p = t % 2
                LAG = 2

                def tail(kk):
                    s_ = kk % NG; d4 = kk % 4
                    S.op('dve', lambda e: e.tensor_scalar(out=dgk[d4][:], in0=identb[:], scalar1=gl[:, kk:kk + 1], scalar2=gate[p][:, kk:kk + 1], op0=ALU.mult, op1=ALU.mult),
                         reads=['identb', 'gl_%d' % kk, 'gate%d' % p], writes=['dgk%d' % d4])
                    for half in range(2):
                        S.op('pe', lambda e, half=half: e.matmul(PB[6 + half][:, :], lhsT=dgk[d4][:], rhs=uvb[s_][:, 1024 + half * 512:1024 + (half + 1) * 512], start=(kk == 0), stop=(kk == 127)),
                             reads=['dgk%d' % d4, 'uvb%d' % s_], writes=['pb%d' % (6 + half)], skip_self=True)

                for k in range(128):
                    s_ = k % NG; j4 = k % 4
                    dk = 'dots_%d' % k
                    S.dma('pool', lambda e, k=k, s_=s_: e.indirect_dma_start(out=uvb[s_][:], out_offset=None, in_=uv_d[:, :], in_offset=bass.IndirectOffsetOnAxis(ap=idxi[p][:, k:k + 1], axis=0)),
                          reads=['idxi%d' % p, 'tabs'], writes=['uvb%d' % s_])
                    S.op('dve', lambda e, k=k, s_=s_, j4=j4: e.tensor_tensor(out=pr[j4][:], in0=uvb[s_][:, 0:1024], in1=h2b[p][:], op=ALU.mult),
                         reads=['uvb%d' % s_, 'h2b%d' % p], writes=['pr%d' % j4])
                    S.op('act', lambda e, k=k, j4=j4: e.activation(out=junk[:], in_=pr[j4][:], func=AF.Identity, accum_out=dots[:, k:k + 1]),
                         reads=['pr%d' % j4, 'dots0'], writes=[dk])
                    S.op('act', lambda e, k=k: e.activation(out=gl[:, k:k + 1], in_=dots[:, k:k + 1], func=AF.Gelu), reads=[dk], writes=['gl_%d' % k])
                    if k >= LAG:
                        tail(k - LAG)
                    if gen is not None and k % 2 == 1:
                        next(gen, None)
                for kk in range(128 - LAG, 128):
                    tail(kk)
                if gen is not None:
                    for _ in gen:
                        pass
                xb_ = x1t[p]; x1k = 'x1t%d' % p
                for half in range(2):
                    S.op('dve', lambda e, half=half: e.tensor_tensor(out=yb[:, half * 512:(half + 1) * 512], in0=PB[6 + half][:, :], in1=bct["g2b"][:, half * 512:(half + 1) * 512], op=ALU.mult),
                         reads=['pb%d' % (6 + half), 'g2b', 'yb'], writes=['yb'])
                S.op('dve', lambda e: e.scalar_tensor_tensor(out=fin[:], in0=xb_[:], scalar=ALPHA, in1=yb[:], op0=ALU.mult, op1=ALU.add), reads=[x1k, 'yb', 'fin'], writes=['fin'])
                layer_norm_rows(stf, mvf[:], rsf[:], fin[:], fin[:], 'fin', 'fin', 'p4f')
                S.op('dve', lambda e: e.tensor_tensor(out=fin[:], in0=fin[:], in1=bct["ln2g"][:], op=ALU.mult), reads=['fin', 'ln2g'], writes=['fin'])
                S.op('dve', lambda e: e.tensor_tensor(out=fin[:], in0=fin[:], in1=bct["ln2b"][:], op=ALU.add), reads=['fin', 'ln2b'], writes=['fin'])
                S.dma('sp', lambda e: e.dma_start(out=out_d[t * 128:(t + 1) * 128, :], in_=fin[:]), reads=['fin'], writes=['out'])

            stf = sb(p4, "st4f", [128, 2, 6]); mvf = sb(p4, "mv4f", [128, 2]); rsf = sb(p4, "rs4f", [128, 1])
            S.op('dve', lambda e: e.memset(dots[:], 0.0), writes=['dots0'])
            if NT4:
                for _ in prologue(0):
                    pass
            for t in range(NT4):
                fused(t, prologue(t + 1) if t + 1 < NT4 else None)
            S.wait_all('sp', ['out', 'dbg'])
            S.barrier()
    return nc


def _prep_shared(inp):
    f = np.float32
    sh = {}
    sh["w_mod"] = np.ascontiguousarray(inp["w_mod"][0].reshape(8, 128, 6144).transpose(1, 0, 2))
    sh["b_modT"] = np.ascontiguousarray(inp["b_mod"][0].reshape(48, 128).T)
    sh["w_in"] = np.ascontiguousarray(inp["w_in"][0].reshape(8, 128, 4624).transpose(1, 0, 2))
    sh["lgT"] = np.ascontiguousarray(inp["hg_lb_logits"].reshape(2, 2, 4, 128).transpose(3, 0, 1, 2))
    sh["hgn"] = np.ascontiguousarray(np.broadcast_to(inp["hg_norm_g"][0][None, :], (64, 512)))
    sh["mln"] = np.ascontiguousarray(np.broadcast_to(inp["ml_norm_g"][0][None, :], (64, 512)))
    sh["convw"] = np.ascontiguousarray(inp["ml_conv_w"][0].reshape(9, 8, 128).transpose(2, 0, 1))
    sh["convb"] = np.ascontiguousarray(inp["ml_conv_b"][0].reshape(8, 128).T)
    sh["gateb"] = np.ascontiguousarray(inp["ml_gate_b"][0].reshape(2, 8).T)
    sh["w_out"] = np.ascontiguousarray(inp["w_out"][0].reshape(8, 128, 1024).transpose(1, 0, 2))
    for nm, key in (("ln1g", "ln1_g"), ("ln1b", "ln1_b"), ("ln2g", "ln2_g"), ("ln2b", "ln2_b")):
        sh[nm] = np.ascontiguousarray(np.broadcast_to(inp[key][0][None, :], (128, 1024)))
    sh["wq"] = np.ascontiguousarray(inp["peer_wq"][0].reshape(8, 128, 2048).transpose(1, 0, 2))
    sh["keysT"] = np.ascontiguousarray(inp["peer_keys"][0].reshape(16, 128, 128).transpose(2, 0, 1))
    nexp = 128 if os.environ.get("KDEBUG") else 16384
    sh["pu"] = np.ascontiguousarray(inp["peer_u"][0][:nexp])
    sh["pv"] = np.ascontiguousarray(inp["peer_v"][0][:nexp])
    sh["ident"] = np.eye(128, dtype=f)
    m = np.zeros((64, 2, 64), f)
    s = np.arange(64)[:, None]; c = np.arange(64)[None, :]
    m[:, 0, :] = (s <= c); m[:, 1, :] = (s >= c)
    sh["masks"] = m
    rm = np.ones((128, 512), f); rm[:, ::64] = 0.0
    sh["rmask"] = rm
    sh["sel8"] = np.eye(8, dtype=f)
    sh["iota16"] = np.ascontiguousarray(np.broadcast_to(np.arange(16, dtype=f)[None, :], (128, 16)))
    dm = np.zeros((8, 2), f); dm[0:4, 0] = 1.0; dm[4:8, 1] = 1.0
    sh["dirm"] = dm
    return {k: np.asarray(v, dtype=f) for k, v in sh.items()}


def kernel(**inputs):
    inp = {k: np.asarray(v) for k, v in inputs.items()}
    debug = os.environ.get("KDEBUG") or None
    nc = build(debug)
    sh = _prep_shared(inp)
    in_maps = []
    for b in range(8):
        m = dict(sh)
        m["xs"] = np.ascontiguousarray(np.concatenate([inp["ctx"][b], inp["x"][b]], axis=0).astype(np.float32))
        m["cT"] = np.ascontiguousarray(np.stack([inp["c"][b], inp["c_ctx"]], axis=-1).reshape(8, 128, 2).transpose(1, 0, 2).astype(np.float32))
        in_maps.append(m)
    res = run_bass_kernel_spmd(nc, in_maps, core_ids=list(range(8)))
    key = "dbg" if debug else "out"
    return np.stack([np.asarray(r[key]) for r in res.results], axis=0).astype(np.float32)
```

```python
import os
import numpy as np
from contextlib import ExitStack
import concourse.bass as bass
import concourse.mybir as mybir
from concourse.bass_utils import run_bass_kernel_spmd

F32 = mybir.dt.float32; BF16 = mybir.dt.bfloat16; I32 = mybir.dt.int32; U32 = mybir.dt.uint32
AF = mybir.ActivationFunctionType; ALU = mybir.AluOpType; AX = mybir.AxisListType

NTOK = 2304; NLAT = 2048; NCH = 36; NLCH = 32
ALPHA = 2.0 ** 0.25
EPS = 1e-6


class Sched:
    NDMA = 32

    def __init__(self, nc, es):
        self.nc = nc
        self.engs = {'pe': nc.tensor, 'act': nc.scalar, 'dve': nc.vector, 'pool': nc.gpsimd, 'sp': nc.sync}
        self.sem = {k: es.enter_context(nc.semaphore("sem_" + k)) for k in self.engs}
        self.cnt = {k: 0 for k in self.engs}
        self.dsem = [es.enter_context(nc.semaphore("dsem%d" % i)) for i in range(self.NDMA)]
        self.dcnt = [0] * self.NDMA
        self.dnext = 0
        self.seen = {k: {} for k in self.engs}
        self.bufs = {}

    def _deps(self, reads, writes):
        deps = []
        for r in reads:
            b = self.bufs.get(r)
            if b and b['w'] is not None:
                deps.append(b['w'])
        for w in writes:
            b = self.bufs.get(w)
            if b:
                if b['w'] is not None:
                    deps.append(b['w'])
                deps.extend(b['r'])
        return deps

    def _wait(self, eng, deps, skip_self=False):
        best = {}
        for (sid, sem, val, owner) in deps:
            if skip_self and owner == eng:
                continue
            if best.get(sid, (None, 0))[1] < val:
                best[sid] = (sem, val)
        for sid, (sem, val) in best.items():
            if self.seen[eng].get(sid, 0) >= val:
                continue
            self.engs[eng].wait_ge(sem, val)
            self.seen[eng][sid] = val

    def _record(self, dep, reads, writes):
        for r in reads:
            b = self.bufs.setdefault(r, {'w': None, 'r': []})
            b['r'] = [d for d in b['r'] if d[0] != dep[0]] + [dep]
        for w in writes:
            self.bufs[w] = {'w': dep, 'r': []}

    @staticmethod
    def _split(keys):
        norm, ps = [], []
        for k in keys:
            if k.startswith('pb') and len(k) > 2 and k[2].isdigit():
                ps.append(k[:3])
            else:
                norm.append(k)
        return norm, ps

    def op(self, eng, fn, reads=(), writes=(), skip_self=False):
        reads, pr = self._split(reads)
        writes, pw = self._split(writes)
        banks = sorted(set(pr + pw))
        deps = self._deps(reads, writes)
        deps += [d for d in self._deps((), banks) if d[3] != eng]
        self._wait(eng, deps, skip_self)
        ins = fn(self.engs[eng])
        self.cnt[eng] += 1
        ins.then_inc(self.sem[eng], 1)
        self._record(('e_' + eng, self.sem[eng], self.cnt[eng], eng), reads, list(writes) + banks)
        return ins

    def dma(self, q, fn, reads=(), writes=()):
        deps = self._deps(reads, writes)
        j = self.dnext
        self.dnext = (self.dnext + 1) % self.NDMA
        if self.dcnt[j] > 0:
            deps = deps + [('d%d' % j, self.dsem[j], self.dcnt[j], 'dma')]
        self._wait(q, deps)
        ins = fn(self.engs[q])
        self.dcnt[j] += 16
        ins.then_inc(self.dsem[j], 16)
        self._record(('d%d' % j, self.dsem[j], self.dcnt[j], 'dma'), reads, writes)

    def wait_all(self, eng, keys):
        deps = []
        for k in keys:
            b = self.bufs.get(k)
            if b:
                if b['w'] is not None:
                    deps.append(b['w'])
                deps.extend(b['r'])
        self._wait(eng, deps)

    def barrier(self):
        for e in self.engs:
            deps = [('e_' + f, self.sem[f], self.cnt[f], f) for f in self.engs if f != e and self.cnt[f] > 0]
            deps += [('d%d' % j, self.dsem[j], self.dcnt[j], 'dma') for j in range(self.NDMA) if self.dcnt[j] > 0]
            self._wait(e, deps)


def K(name, a, b=None):
    if b is None:
        return ["%s%d" % (name, a)]
    return ["%s%d" % (name, i) for i in range(a, b)]


def build(debug=None):
    nc = bass.Bass("TRN2", target_bir_lowering=False)
    D = {}

    def din(name, shape, dt=F32):
        D[name] = nc.dram_tensor(name, shape, dt, kind="ExternalInput").ap()
        return D[name]

    xs = din("xs", [NTOK, 1024]); cT_d = din("cT", [128, 8, 2]); wmod_d = din("w_mod", [128, 8, 6144])
    bmod_d = din("b_modT", [128, 48]); win_d = din("w_in", [128, 8, 4624]); lg_d = din("lgT", [128, 2, 2, 4])
    hgn_d = din("hgn", [64, 512]); mln_d = din("mln", [64, 512]); convw_d = din("convw", [128, 9, 8])
    convb_d = din("convb", [128, 8]); gateb_d = din("gateb", [8, 2]); wout_d = din("w_out", [128, 8, 1024])
    ln1g_d = din("ln1g", [128, 1024]); ln1b_d = din("ln1b", [128, 1024]); ln2g_d = din("ln2g", [128, 1024])
    ln2b_d = din("ln2b", [128, 1024]); wq_d = din("wq", [128, 8, 2048]); keysT_d = din("keysT", [128, 16, 128])
    NEXP = 128 if debug else 16384
    pu_d = din("pu", [NEXP, 1024]); pv_d = din("pv", [NEXP, 1024])
    ident_d = din("ident", [128, 128]); masks_d = din("masks", [64, 2, 64]); rmask_d = din("rmask", [128, 512])
    sel8_d = din("sel8", [8, 8]); dirm_d = din("dirm", [8, 2]); iota_d = din("iota16", [128, 16])
    out_d = nc.dram_tensor("out", [NLAT, 1024], F32, kind="ExternalOutput").ap()
    dbg_d = None
    if debug:
        dbg_d = nc.dram_tensor("dbg", [NLAT, 1024] if debug != 'mix' else [1024, NLAT], F32, kind="ExternalOutput").ap()

    with ExitStack() as es:
        S = Sched(nc, es)

        uid = [0]

        def sb(st, name, shape, dt=F32):
            uid[0] += 1
            return st.enter_context(nc.sbuf_tensor("s%d_%s" % (uid[0], name), shape, dt))

        PB = [es.enter_context(nc.psum_tensor("pb%d" % i, [128, 512], F32)) for i in range(8)]

        ident = sb(es, "ident", [128, 128]); identb = sb(es, "identb", [128, 128], BF16)
        masks = sb(es, "masks", [64, 2, 64]); rmask = sb(es, "rmask", [128, 512])
        ones = sb(es, "ones", [128, 128]); epsc = sb(es, "epsc", [128, 1])
        modv = sb(es, "modv", [128, 48, 2])
        S.dma('sp', lambda e: e.dma_start(out=ident[:], in_=ident_d[:, :]), writes=['ident'])
        S.dma('sp', lambda e: e.dma_start(out=masks[:], in_=masks_d[:, :, :]), writes=['masks'])
        S.dma('sp', lambda e: e.dma_start(out=rmask[:], in_=rmask_d[:, :]), writes=['rmask'])
        S.op('dve', lambda e: e.tensor_copy(out=identb[:], in_=ident[:]), reads=['ident'], writes=['identb'])
        S.op('dve', lambda e: e.memset(ones[:], 1.0), writes=['ones'])
        S.op('dve', lambda e: e.memset(epsc[:], EPS), writes=['epsc'])

        with ExitStack() as p0:
            cT = sb(p0, "cT", [128, 8, 2]); scT = sb(p0, "scT", [128, 8, 2]); bmodT = sb(p0, "bmodT", [128, 48])
            wm = [sb(p0, "wm%d" % i, [128, 6144]) for i in range(2)]
            S.dma('sp', lambda e: e.dma_start(out=cT[:], in_=cT_d[:, :, :]), writes=['cT'])
            S.dma('sp', lambda e: e.dma_start(out=bmodT[:], in_=bmod_d[:, :]), writes=['bmodT'])
            S.op('act', lambda e: e.activation(out=scT[:], in_=cT[:], func=AF.Silu), reads=['cT'], writes=['scT'])
            for kc in range(8):
                w = wm[kc % 2]; wk = 'wm%d' % (kc % 2)
                S.dma('sp' if kc % 2 == 0 else 'pool', lambda e, w=w, kc=kc: e.dma_start(out=w[:], in_=wmod_d[:, kc, :]), writes=[wk])
                for j in range(48):
                    S.op('pe', lambda e, w=w, kc=kc, j=j: e.matmul(PB[kc // 4][:, (kc % 4) * 96 + 2 * j:(kc % 4) * 96 + 2 * j + 2], lhsT=w[:, j * 128:(j + 1) * 128], rhs=scT[:, kc, :],
                                                                 start=True, stop=True),
                         reads=[wk, 'scT'], writes=['pb%d' % (kc // 4)], skip_self=True)
            mflat = modv[:].rearrange("p j n -> p (j n)")
            S.op('dve', lambda e: e.tensor_tensor(out=modv[:], in0=PB[0][:, 0:96].rearrange("p (j n) -> p j n", n=2), in1=bmodT[:].unsqueeze(2).to_broadcast([128, 48, 2]), op=ALU.add),
                 reads=['pb0', 'bmodT'], writes=['modv'])
            for kc in range(1, 8):
                S.op('dve', lambda e, kc=kc: e.tensor_tensor(out=mflat, in0=mflat, in1=PB[kc // 4][:, (kc % 4) * 96:(kc % 4) * 96 + 96], op=ALU.add),
                     reads=['pb%d' % (kc // 4), 'modv'], writes=['modv'])
            S.op('dve', lambda e: e.tensor_scalar_add(out=modv[:, 8:16, :], in0=modv[:, 8:16, :], scalar1=1.0), reads=['modv'], writes=['modv'])
            S.op('dve', lambda e: e.tensor_scalar_add(out=modv[:, 32:40, :], in0=modv[:, 32:40, :], scalar1=1.0), reads=['modv'], writes=['modv'])
            S.barrier()
        if debug == 'p0':
            S.dma('sp', lambda e: e.dma_start(out=dbg_d[0:128, 0:96], in_=modv[:].rearrange("p a b -> p (a b)")), reads=['modv'], writes=['dbg'])
            S.wait_all('sp', ['dbg']); S.barrier()
            return nc

        scA = ExitStack(); scB = ExitStack()
        mixT = sb(scA, "mixT", [128, 8, NLAT], BF16)
        hT = sb(scB, "hT", [128, 8, NTOK], BF16)
        wstg = sb(scB, "wstg", [128, 8, 128])
        tstg = [sb(scB, "tstg%d" % i, [128, 512]) for i in range(2)]
        tbf = [sb(scB, "tbf%d" % i, [128, 512], BF16) for i in range(2)]
        uv_d = nc.dram_tensor("uvbf", [NEXP, 2048], BF16, kind="Internal").ap()
        prep_pos = [0]

        def prep_tables(npieces):
            if debug:
                return
            for _ in range(npieces):
                i = prep_pos[0]
                if i >= 512:
                    return
                prep_pos[0] += 1
                src, coff = (pu_d, 0) if i < 256 else (pv_d, 1024)
                r = (i % 256) // 2; c = (i % 2) * 512; bb = i % 2
                S.dma('sp', lambda e, src=src, r=r, c=c, bb=bb: e.dma_start(out=tstg[bb][:], in_=src[r * 128:(r + 1) * 128, c:c + 512]), writes=['tstg%d' % bb])
                S.op('pool', lambda e, bb=bb: e.tensor_copy(out=tbf[bb][:], in_=tstg[bb][:]), reads=['tstg%d' % bb], writes=['tbf%d' % bb])
                S.dma('pool', lambda e, coff=coff, r=r, c=c, bb=bb: e.dma_start(out=uv_d[r * 128:(r + 1) * 128, coff + c:coff + c + 512], in_=tbf[bb][:]), reads=['tbf%d' % bb], writes=['tabs'])

        def layer_norm_rows(st_ap, mv_ap, rstd_ap, src, dst, skey, dkey, tag):
            S.op('dve', lambda e: e.bn_stats(out=st_ap[:, 0, :], in_=src[:, 0:512]), reads=[skey], writes=[tag + 'st'])
            S.op('dve', lambda e: e.bn_stats(out=st_ap[:, 1, :], in_=src[:, 512:1024]), reads=[skey], writes=[tag + 'st'])
            S.op('dve', lambda e: e.bn_aggr(out=mv_ap, in_=st_ap[:].rearrange("p a b -> p (a b)")), reads=[tag + 'st'], writes=[tag + 'mv'])
            S.op('act', lambda e: e.activation(out=rstd_ap, in_=mv_ap[:, 1:2], func=AF.Sqrt, bias=epsc[:, 0:1], scale=1.0),
                 reads=[tag + 'mv', 'epsc'], writes=[tag + 'rs'])
            S.op('dve', lambda e: e.reciprocal(out=rstd_ap, in_=rstd_ap), reads=[tag + 'rs'], writes=[tag + 'rs'])
            S.op('dve', lambda e: e.tensor_scalar(out=dst, in0=src, scalar1=mv_ap[:, 0:1], scalar2=rstd_ap, op0=ALU.subtract, op1=ALU.mult),
                 reads=[skey, tag + 'mv', tag + 'rs'], writes=[dkey])

        with ExitStack() as p1:
            xt = [sb(p1, "xt%d" % i, [128, 1024]) for i in range(2)]
            xn = [sb(p1, "xn%d" % i, [128, 1024]) for i in range(2)]
            st = [sb(p1, "st%d" % i, [128, 2, 6]) for i in range(2)]
            mv = [sb(p1, "mv%d" % i, [128, 2]) for i in range(2)]
            rs = [sb(p1, "rs%d" % i, [128, 1]) for i in range(2)]
            PSAP = bool(os.environ.get("KPSAP"))
            for t in range(18):
                b = t % 2
                n = 1 if t < 2 else 0
                S.dma('sp' if b == 0 else 'pool', lambda e, t=t, b=b: e.dma_start(out=xt[b][:], in_=xs[t * 128:(t + 1) * 128, :]), writes=['xt%d' % b])
                layer_norm_rows(st[b], mv[b][:], rs[b][:], xt[b][:], xn[b][:], 'xt%d' % b, 'xn%d' % b, 'p1%d' % b)
                for half in range(2):
                    bank = 2 * b + half
                    pk = 'pb%d' % bank
                    for c4 in range(4):
                        ch = half * 4 + c4
                        S.op('pe', lambda e, b=b, ch=ch, c4=c4, bank=bank: e.transpose(PB[bank][:, c4 * 128:(c4 + 1) * 128], xn[b][:, ch * 128:(ch + 1) * 128], ident[:]),
                             reads=['xn%d' % b, 'ident'], writes=[pk], skip_self=True)
                    if not PSAP:
                        S.op('act', lambda e, b=b, half=half, bank=bank: e.copy(out=xt[b][:, half * 512:(half + 1) * 512], in_=PB[bank][:, :]), reads=[pk, 'xt%d' % b], writes=['xt%d' % b])
                    for c4 in range(4):
                        ch = half * 4 + c4
                        dst = hT[:, ch, t * 128:(t + 1) * 128]
                        src = PB[bank][:, c4 * 128:(c4 + 1) * 128] if PSAP else xt[b][:, ch * 128:(ch + 1) * 128]
                        S.op('dve', lambda e, dst=dst, src=src, ch=ch, n=n: e.tensor_scalar(out=dst, in0=src, scalar1=modv[:, 8 + ch, n:n + 1], scalar2=modv[:, ch, n:n + 1],
                                                                                      op0=ALU.mult, op1=ALU.add),
                             reads=([pk] if PSAP else ['xt%d' % b]) + ['modv'], writes=K('hT', t))
            S.barrier()

        if debug == 'p1':
            with ExitStack() as dd:
                hf = sb(dd, "hf", [128, 1024])
                for t in range(16):
                    S.op('dve', lambda e, t=t: e.tensor_copy(out=hf[:].rearrange("p (a b) -> p a b", b=128), in_=hT[:, :, 256 + t * 128:256 + (t + 1) * 128]), reads=K('hT', t + 2) + ['hf'], writes=['hf'])
                    S.dma('sp', lambda e, t=t: e.dma_start(out=dbg_d[t * 128:(t + 1) * 128, :], in_=hf[:]), reads=['hf'], writes=['dbg'])
                S.wait_all('sp', ['dbg']); S.barrier()
            scB.close(); scA.close()
            return nc
        def load_w(stk, name, col0, ncols, src=None):
            src = win_d if src is None else src
            wb = sb(stk, name, [128, 8, ncols], BF16)
            S.dma('sp', lambda e: e.dma_start(out=wstg[:, :, 0:ncols], in_=src[:, :, col0:col0 + ncols]), writes=['wstg'])
            S.op('pool', lambda e: e.tensor_copy(out=wb[:], in_=wstg[:, :, 0:ncols]), reads=['wstg'], writes=[name])
            return wb

        BLKS = [(0, 512), (512, 512), (1024, 512), (1536, 512), (2048, 256)]

        def proj_fm_block(wb, wname, ncols, bank, t0, nt):
            for kc in range(8):
                S.op('pe', lambda e, kc=kc: e.matmul(PB[bank][0:ncols, 0:nt], lhsT=wb[:, kc, :], rhs=hT[:, kc, t0:t0 + nt], start=(kc == 0), stop=(kc == 7)),
                     reads=[wname] + K('hT', t0 // 128, (t0 + nt) // 128), writes=['pb%d' % bank], skip_self=True)

        def proj_tm_group(wb, wname, ncols, bank, c0, ncks):
            for j in range(ncks):
                n = c0 + j
                for kc in range(8):
                    S.op('pe', lambda e, kc=kc, j=j, n=n: e.matmul(PB[bank][0:64, j * ncols:(j + 1) * ncols], lhsT=hT[:, kc, n * 64:(n + 1) * 64], rhs=wb[:, kc, :],
                                                                   start=(kc == 0), stop=(kc == 7)),
                         reads=[wname] + K('hT', n // 2), writes=['pb%d' % bank], skip_self=True)

        S32 = sb(scB, "S32", [128, 2, 132]); Sbf = sb(scB, "Sbf", [128, 2, 132], BF16)
        stm = sb(scB, "stm", [64, 2, 64], BF16)
        usb = sb(scB, "usb", [128, 2, 132])

        def chunk_loop(dirn, KsT, QsT, Vi, QoT, Ku, Vu, dec_ap, W, evac, rk, tagk):
            order = list(range(36)) if dirn == 0 else [3, 2, 1, 0] + list(range(35, 3, -1))
            S.op('dve', lambda e: e.memset(S32[:, 0, :], 0.0), writes=['S32_0'])
            S.op('dve', lambda e: e.memset(Sbf[:, 0, :], 0.0), writes=['Sbf_0'])

            def pre(idx):
                n = order[idx]
                sl = slice(n * 64, (n + 1) * 64)
                s2 = idx % 2
                if n >= 4:
                    pst = PB[2 + s2][0:64, 0:64]; pstk = 'pb%d' % (2 + s2)
                    S.op('pe', lambda e: e.matmul(pst, lhsT=KsT[:, sl], rhs=QsT[:, sl], start=True, stop=True),
                         reads=rk, writes=[pstk], skip_self=True)
                    S.op('dve', lambda e: e.tensor_tensor(out=stm[:, s2, :], in0=pst, in1=masks[:, dirn, :], op=ALU.mult),
                         reads=[pstk, 'masks'], writes=['stm%d' % s2])
                if idx < 35:
                    pu = PB[6 + s2][:, 0:W]; puk = 'pb%d' % (6 + s2)
                    S.op('pe', lambda e: e.matmul(pu, lhsT=Ku[:, n, :], rhs=Vu[:, n, 0:W], start=True, stop=True),
                         reads=rk, writes=[puk], skip_self=True)
                    S.op('act', lambda e: e.copy(out=usb[:, s2, 0:W], in_=pu), reads=[puk], writes=['usb%d' % s2])

            pre(0)
            cur = 0
            for idx, n in enumerate(order):
                if idx + 1 < 36:
                    pre(idx + 1)
                sl = slice(n * 64, (n + 1) * 64)
                s2 = idx % 2
                if n >= 4:
                    po = PB[4 + s2][0:64, 0:W]; pok = 'pb%d' % (4 + s2)
                    S.op('pe', lambda e, po=po, s2=s2, n=n: e.matmul(po, lhsT=stm[:, s2, :], rhs=Vi[:, n, 0:W], start=True, stop=False),
                         reads=['stm%d' % s2] + rk, writes=[pok], skip_self=True)
                    S.op('pe', lambda e, po=po, sl=sl, cur=cur: e.matmul(po, lhsT=QoT[:, sl], rhs=Sbf[:, cur, 0:W], start=False, stop=True),
                         reads=['Sbf_%d' % cur] + rk, writes=[pok], skip_self=True)
                if idx < 35:
                    nxt = 1 - cur
                    S.op('dve', lambda e, n=n, cur=cur, nxt=nxt, s2=s2: e.scalar_tensor_tensor(out=Sbf[:, nxt, 0:W], in0=S32[:, cur, 0:W], scalar=dec_ap(n), in1=usb[:, s2, 0:W],
                                                                                            op0=ALU.mult, op1=ALU.add),
                         reads=['S32_%d' % cur, 'usb%d' % s2, tagk], writes=['Sbf_%d' % nxt])
                    S.op('dve', lambda e, n=n, cur=cur, nxt=nxt, s2=s2: e.scalar_tensor_tensor(out=S32[:, nxt, 0:W], in0=S32[:, cur, 0:W], scalar=dec_ap(n), in1=usb[:, s2, 0:W],
                                                                                            op0=ALU.mult, op1=ALU.add),
                         reads=['S32_%d' % cur, 'usb%d' % s2, tagk], writes=['S32_%d' % nxt])
                    cur = nxt
                if n >= 4:
                    evac(n - 4, n, po, pok)

        def transpose_chunks_to_mixT(src, skey, head):
            for g in range(4):
                bank = g % 2
                for j in range(8):
                    n = g * 8 + j
                    S.op('pe', lambda e, n=n, j=j, bank=bank: e.transpose(PB[bank][:, j * 64:(j + 1) * 64], src[:, n, :], ident[0:64, 0:64]),
                         reads=list(skey) + ['ident'], writes=['pb%d' % bank], skip_self=True)
                S.op('act', lambda e, g=g, bank=bank: e.copy(out=mixT[:, head, g * 512:(g + 1) * 512], in_=PB[bank][:, :]),
                     reads=['pb%d' % bank], writes=K('mixT', head))

        with ExitStack() as g0:
            lgT = sb(g0, "lgT", [128, 2, 2, 4]); lbT = sb(g0, "lbT", [128, 2, 4]); omlT = sb(g0, "omlT", [128, 2, 4]); nomlT = sb(g0, "nomlT", [128, 2, 4])
            hgn = sb(g0, "hgn", [64, 512])
            S.dma('sp', lambda e: e.dma_start(out=lgT[:], in_=lg_d[:, :, :, :]), writes=['lgT'])
            S.dma('sp', lambda e: e.dma_start(out=hgn[:], in_=hgn_d[:, :]), writes=['hgn'])
            S.op('dve', lambda e: e.tensor_tensor(out=lbT[:], in0=lgT[:, :, 0, :], in1=lgT[:, :, 1, :], op=ALU.subtract), reads=['lgT'], writes=['lbT'])
            S.op('act', lambda e: e.activation(out=lbT[:], in_=lbT[:], func=AF.Sigmoid), reads=['lbT'], writes=['lbT'])
            S.op('dve', lambda e: e.tensor_scalar(out=omlT[:], in0=lbT[:], scalar1=-1.0, scalar2=1.0, op0=ALU.mult, op1=ALU.add), reads=['lbT'], writes=['omlT'])
            S.op('dve', lambda e: e.tensor_scalar_mul(out=nomlT[:], in0=omlT[:], scalar1=-1.0), reads=['omlT'], writes=['nomlT'])
            QsT = [sb(g0, "gQsT%d" % d, [128, NTOK], BF16) for d in range(2)]
            KsT = [sb(g0, "gKsT%d" % d, [128, NTOK], BF16) for d in range(2)]
            QoT = [sb(g0, "gQoT%d" % d, [128, NTOK], BF16) for d in range(2)]
            Ku = [sb(g0, "gKu%d" % d, [64, NCH, 128], BF16) for d in range(2)]
            dec = sb(g0, "gdec", [128, 2, NCH])
            vtm = sb(g0, "gv", [64, NCH, 128], BF16); gs = sb(g0, "ggs", [64, NLCH, 128], BF16); oacc = sb(g0, "goacc", [64, NLCH, 128])
            qf = sb(g0, "gqf", [128, 512]); sg = sb(g0, "gsg", [128, 512]); lf = sb(g0, "glf", [128, 512]); key = sb(g0, "gkey", [128, 512])
            Bc = sb(g0, "gB", [128, 512]); T1 = sb(g0, "gT1", [128, 512]); T2 = sb(g0, "gT2", [128, 512]); khT = sb(g0, "gkhT", [128, 512], BF16)
            EE = [sb(g0, "gE%d" % i, [128, 512]) for i in range(4)]
            sq = sb(g0, "gsq", [64, NLCH, 128], BF16); ss = sb(g0, "gss", [64, NLCH])
            for hd in range(4):
                with ExitStack() as hs:
                    wq_ = load_w(hs, "gwq", 0 + hd * 128, 128); wi_ = load_w(hs, "gwi", 512 + hd * 128, 128); wg_ = load_w(hs, "gwg", 1024 + hd * 128, 128)
                    wf = [load_w(hs, "gwf0", 1536 + hd * 128, 128), load_w(hs, "gwf1", 2048 + hd * 128, 128)]
                    prep_tables(64)
                    for g in range(9):
                        bk = 3 + g % 2
                        proj_tm_group(wi_, "gwi", 128, bk, g * 4, 4)
                        S.op('act', lambda e, g=g, bk=bk: e.copy(out=vtm[:, g * 4:(g + 1) * 4, :], in_=PB[bk][0:64, :].rearrange("p (j c) -> p j c", c=128)),
                             reads=['pb%d' % bk], writes=['gv'])
                    for g in range(8):
                        bk = 3 + (g + 1) % 2
                        proj_tm_group(wg_, "gwg", 128, bk, 4 + g * 4, 4)
                        S.op('act', lambda e, g=g, bk=bk: e.activation(out=gs[:, g * 4:(g + 1) * 4, :], in_=PB[bk][0:64, :].rearrange("p (j c) -> p j c", c=128), func=AF.Silu),
                             reads=['pb%d' % bk], writes=['ggs'])
                    for (t0, nt) in BLKS:
                        nck = nt // 64; c0 = t0 // 64
                        proj_fm_block(wq_, "gwq", 128, 0, t0, nt)
                        S.op('act', lambda e, nt=nt: e.copy(out=qf[:, 0:nt], in_=PB[0][:, 0:nt]), reads=['pb0'], writes=['gqf'])
                        for d in range(2):
                            proj_fm_block(wf[d], "gwf%d" % d, 128, 1 + d, t0, nt)
                            col = d * 4 + hd
                            lbp = lbT[:, d, hd:hd + 1]; omp = omlT[:, d, hd:hd + 1]; nomp = nomlT[:, d, hd:hd + 1]
                            S.op('act', lambda e, d=d, nt=nt: e.activation(out=sg[:, 0:nt], in_=PB[1 + d][:, 0:nt], func=AF.Sigmoid), reads=['pb%d' % (1 + d)], writes=['gsg'])
                            S.op('act', lambda e, nt=nt, lbp=lbp, omp=omp: e.activation(out=lf[:, 0:nt], in_=sg[:, 0:nt], func=AF.Ln, bias=lbp, scale=omp),
                                 reads=['gsg', 'lbT', 'omlT'], writes=['glf'])
                            S.op('dve', lambda e, nt=nt, nomp=nomp, omp=omp: e.tensor_scalar(out=key[:, 0:nt], in0=sg[:, 0:nt], scalar1=nomp, scalar2=omp, op0=ALU.mult, op1=ALU.add),
                                 reads=['gsg', 'omlT', 'nomlT'], writes=['gkey'])
                            S.op('dve', lambda e, nt=nt: e.tensor_tensor_scan(out=Bc[:, 0:nt], data0=rmask[:, 0:nt], data1=lf[:, 0:nt], initial=0.0, op0=ALU.mult, op1=ALU.add),
                                 reads=['glf', 'rmask'], writes=['gB'])
                            B3 = Bc[:, 0:nt].rearrange("p (n c) -> p n c", c=64)
                            T3 = T1[:, 0:nt].rearrange("p (n c) -> p n c", c=64)
                            if d == 1:
                                S.op('dve', lambda e, B3=B3, T3=T3, nck=nck: e.tensor_tensor(out=T3, in0=B3[:, :, 63:64].to_broadcast([128, nck, 64]), in1=B3, op=ALU.subtract),
                                     reads=['gB'], writes=['gT1'])
                                S.op('dve', lambda e, nt=nt: e.tensor_tensor(out=Bc[:, 0:nt], in0=T1[:, 0:nt], in1=lf[:, 0:nt], op=ALU.add), reads=['gT1', 'glf'], writes=['gB'])
                            li = 63 if d == 0 else 0
                            S.op('act', lambda e, B3=B3, li=li, d=d, c0=c0, nck=nck: e.activation(out=dec[:, d, c0:c0 + nck], in_=B3[:, :, li], func=AF.Exp), reads=['gB'], writes=['gdec'])
                            T4 = T2[:, 0:nt].rearrange("p (n c) -> p n c", c=64)
                            S.op('dve', lambda e, B3=B3, T3=T3, nck=nck: e.tensor_tensor(out=T3, in0=B3, in1=B3[:, :, 32:33].to_broadcast([128, nck, 64]), op=ALU.subtract),
                                 reads=['gB'], writes=['gT1'])
                            S.op('dve', lambda e, B3=B3, T4=T4, nck=nck, li=li: e.tensor_tensor(out=T4, in0=B3[:, :, li:li + 1].to_broadcast([128, nck, 64]), in1=B3, op=ALU.subtract),
                                 reads=['gB'], writes=['gT2'])
                            S.op('act', lambda e, nt=nt: e.activation(out=EE[0][:, 0:nt], in_=T1[:, 0:nt], func=AF.Exp), reads=['gT1'], writes=['gE0'])
                            S.op('act', lambda e, nt=nt: e.activation(out=EE[1][:, 0:nt], in_=T1[:, 0:nt], func=AF.Exp, scale=-1.0), reads=['gT1'], writes=['gE1'])
                            S.op('act', lambda e, nt=nt: e.activation(out=EE[2][:, 0:nt], in_=Bc[:, 0:nt], func=AF.Exp), reads=['gB'], writes=['gE2'])
                            S.op('act', lambda e, nt=nt: e.activation(out=EE[3][:, 0:nt], in_=T2[:, 0:nt], func=AF.Exp), reads=['gT2'], writes=['gE3'])
                            S.op('dve', lambda e, nt=nt, t0=t0, d=d: e.tensor_tensor(out=QsT[d][:, t0:t0 + nt], in0=qf[:, 0:nt], in1=EE[0][:, 0:nt], op=ALU.mult),
                                 reads=['gqf', 'gE0'], writes=['gQsT%d' % d])
                            S.op('dve', lambda e, nt=nt, t0=t0, d=d: e.tensor_tensor(out=KsT[d][:, t0:t0 + nt], in0=key[:, 0:nt], in1=EE[1][:, 0:nt], op=ALU.mult),
                                 reads=['gkey', 'gE1'], writes=['gKsT%d' % d])
                            S.op('dve', lambda e, nt=nt, t0=t0, d=d: e.tensor_tensor(out=QoT[d][:, t0:t0 + nt], in0=qf[:, 0:nt], in1=EE[2][:, 0:nt], op=ALU.mult),
                                 reads=['gqf', 'gE2'], writes=['gQoT%d' % d])
                            S.op('dve', lambda e, nt=nt: e.tensor_tensor(out=khT[:, 0:nt], in0=key[:, 0:nt], in1=EE[3][:, 0:nt], op=ALU.mult), reads=['gkey', 'gE3'], writes=['gkhT'])
                            pbb = PB[3][:].bitcast(BF16)
                            for j in range(nck):
                                S.op('pe', lambda e, j=j: e.transpose(pbb[0:64, j * 128:(j + 1) * 128], khT[:, j * 64:(j + 1) * 64], identb[:]),
                                     reads=['gkhT', 'identb'], writes=['pb3'], skip_self=True)
                            S.op('act', lambda e, d=d, c0=c0, nck=nck: e.copy(out=Ku[d][:, c0:c0 + nck, :], in_=pbb[0:64, 0:nck * 128].rearrange("p (j c) -> p j c", c=128)),
                                 reads=['pb3'], writes=['gKu%d' % d])
                    for d in range(2):
                        def evac(nl, n, po, pok, d=d):
                            if d == 0:
                                S.op('act', lambda e: e.copy(out=oacc[:, nl, :], in_=po), reads=[pok], writes=K('goacc', nl))
                            else:
                                S.op('dve', lambda e: e.tensor_tensor(out=oacc[:, nl, :], in0=po, in1=oacc[:, nl, :], op=ALU.add), reads=[pok] + K('goacc', nl), writes=K('goacc', nl))
                        chunk_loop(d, KsT[d], QsT[d], vtm, QoT[d], Ku[d], vtm, lambda n, d=d: dec[:, d, n:n + 1], 128, evac,
                                   ['gQsT%d' % d, 'gKsT%d' % d, 'gQoT%d' % d, 'gKu%d' % d, 'gv'], 'gdec')
                    allo = K('goacc', 0, NLCH)
                    S.op('dve', lambda e: e.tensor_tensor(out=sq[:], in0=oacc[:], in1=oacc[:], op=ALU.mult), reads=allo, writes=['gsq'])
                    S.op('dve', lambda e: e.tensor_reduce(out=ss[:], in_=sq[:], axis=AX.X, op=ALU.add), reads=['gsq'], writes=['gss'])
                    S.op('act', lambda e: e.activation(out=ss[:], in_=ss[:], func=AF.Sqrt, bias=epsc[0:64, 0:1], scale=1.0 / 128.0), reads=['gss', 'epsc'], writes=['gss'])
                    S.op('dve', lambda e: e.reciprocal(out=ss[:], in_=ss[:]), reads=['gss'], writes=['gss'])
                    S.op('dve', lambda e: e.tensor_tensor(out=oacc[:], in0=oacc[:], in1=ss[:].unsqueeze(2).to_broadcast([64, NLCH, 128]), op=ALU.mult),
                         reads=allo + ['gss'], writes=allo)
                    S.op('dve', lambda e, hd=hd: e.tensor_tensor(out=oacc[:], in0=oacc[:], in1=hgn[:, hd * 128:(hd + 1) * 128].unsqueeze(1).to_broadcast([64, NLCH, 128]), op=ALU.mult),
                         reads=allo + ['hgn'], writes=allo)
                    S.op('dve', lambda e: e.tensor_tensor(out=oacc[:], in0=oacc[:], in1=gs[:], op=ALU.mult), reads=allo + ['ggs'], writes=allo)
                    transpose_chunks_to_mixT(oacc, allo, hd)
            S.barrier()

        with ExitStack() as m0:
            mln = sb(m0, "mln", [64, 512]); convw = sb(m0, "convw", [128, 9, 8]); convb = sb(m0, "convb", [128, 8])
            gateb = sb(m0, "gateb", [8, 2]); sel8 = sb(m0, "sel8", [8, 8]); dirm = sb(m0, "dirm", [8, 2])
            S.dma('sp', lambda e: e.dma_start(out=mln[:], in_=mln_d[:, :]), writes=['mln'])
            S.dma('sp', lambda e: e.dma_start(out=convw[:], in_=convw_d[:, :, :]), writes=['convw'])
            S.dma('sp', lambda e: e.dma_start(out=convb[:], in_=convb_d[:, :]), writes=['convb'])
            S.dma('sp', lambda e: e.dma_start(out=gateb[:], in_=gateb_d[:, :]), writes=['gateb'])
            S.dma('sp', lambda e: e.dma_start(out=sel8[:], in_=sel8_d[:, :]), writes=['sel8'])
            S.dma('sp', lambda e: e.dma_start(out=dirm[:], in_=dirm_d[:, :]), writes=['dirm'])
            RUU = sb(m0, "RUU", [64, NCH, 24]); dchunk = sb(m0, "dchunk", [128, 8, NCH])
            with ExitStack() as gp:
                wgi = load_w(gp, "mwgi", 4608, 8); wgf = load_w(gp, "mwgf", 4616, 8)
                LI = sb(gp, "LI", [8, NTOK]); LF = sb(gp, "LF", [8, NTOK]); Af = sb(gp, "Af", [8, NTOK]); Ab = sb(gp, "Ab", [8, NTOK]); Aa = sb(gp, "Aa", [8, NTOK])
                R = [sb(gp, "Rr%d" % i, [8, NTOK]) for i in range(3)]
                bd = sb(gp, "bd", [8, 8, NCH])
                for (t0, nt) in BLKS:
                    proj_fm_block(wgi, "mwgi", 8, 0, t0, nt)
                    S.op('act', lambda e, t0=t0, nt=nt: e.copy(out=LI[:, t0:t0 + nt], in_=PB[0][0:8, 0:nt]), reads=['pb0'], writes=['LI'])
                    proj_fm_block(wgf, "mwgf", 8, 1, t0, nt)
                    S.op('act', lambda e, t0=t0, nt=nt: e.copy(out=LF[:, t0:t0 + nt], in_=PB[1][0:8, 0:nt]), reads=['pb1'], writes=['LF'])
                S.op('dve', lambda e: e.tensor_scalar_add(out=LI[:], in0=LI[:], scalar1=gateb[:, 0:1]), reads=['LI', 'gateb'], writes=['LI'])
                S.op('act', lambda e: e.activation(out=LF[:], in_=LF[:], func=AF.Sigmoid, bias=gateb[:, 1:2], scale=1.0), reads=['LF', 'gateb'], writes=['LF'])
                S.op('act', lambda e: e.activation(out=LF[:], in_=LF[:], func=AF.Ln), reads=['LF'], writes=['LF'])
                for (t0, nt) in BLKS:
                    S.op('dve', lambda e, t0=t0, nt=nt: e.tensor_tensor_scan(out=Af[:, t0:t0 + nt], data0=rmask[0:8, 0:nt], data1=LF[:, t0:t0 + nt], initial=0.0, op0=ALU.mult, op1=ALU.add),
                         reads=['LF', 'rmask'], writes=['Af'])
                A3 = Af[:].rearrange("p (n c) -> p n c", c=64)
                tot = A3[:, :, 63:64]
                S.op('dve', lambda e: e.tensor_tensor(out=Ab[:].rearrange("p (n c) -> p n c", c=64), in0=tot.to_broadcast([8, NCH, 64]), in1=A3, op=ALU.subtract), reads=['Af'], writes=['Ab'])
                S.op('dve', lambda e: e.tensor_tensor(out=Ab[:], in0=Ab[:], in1=LF[:], op=ALU.add), reads=['Ab', 'LF'], writes=['Ab'])
                S.op('dve', lambda e: e.tensor_scalar_mul(out=Aa[:], in0=Af[:], scalar1=dirm[:, 0:1]), reads=['Af', 'dirm'], writes=['Aa'])
                S.op('dve', lambda e: e.scalar_tensor_tensor(out=Aa[:], in0=Ab[:], scalar=dirm[:, 1:2], in1=Aa[:], op0=ALU.mult, op1=ALU.add), reads=['Ab', 'dirm', 'Aa'], writes=['Aa'])
                S.op('act', lambda e: e.activation(out=R[0][:], in_=Aa[:], func=AF.Exp), reads=['Aa'], writes=['Rr0'])
                S.op('dve', lambda e: e.tensor_tensor(out=Ab[:], in0=LI[:], in1=Aa[:], op=ALU.subtract), reads=['LI', 'Aa', 'Ab'], writes=['Ab'])
                S.op('act', lambda e: e.activation(out=R[1][:], in_=Ab[:], func=AF.Exp), reads=['Ab'], writes=['Rr1'])
                S.op('dve', lambda e: e.tensor_tensor(out=Aa[:].rearrange("p (n c) -> p n c", c=64), in0=Ab[:].rearrange("p (n c) -> p n c", c=64), in1=tot.to_broadcast([8, NCH, 64]), op=ALU.add),
                     reads=['Ab', 'Af', 'Aa', 'Rr0'], writes=['Aa'])
                S.op('act', lambda e: e.activation(out=R[2][:], in_=Aa[:], func=AF.Exp), reads=['Aa'], writes=['Rr2'])
                for half in range(2):
                    for j in range(18):
                        n = half * 18 + j
                        for q in range(3):
                            S.op('pe', lambda e, n=n, j=j, q=q: e.transpose(PB[2][0:64, j * 24 + q * 8:j * 24 + q * 8 + 8], R[q][:, n * 64:(n + 1) * 64], ident[0:8, 0:8]),
                                 reads=['Rr%d' % q, 'ident'], writes=['pb2'], skip_self=True)
                    S.op('act', lambda e, half=half: e.copy(out=RUU[:, half * 18:(half + 1) * 18, :], in_=PB[2][0:64, 0:432].rearrange("p (j c) -> p j c", c=24)),
                         reads=['pb2'], writes=['RUU'])
                S.op('dve', lambda e: e.tensor_tensor(out=bd[:], in0=tot.rearrange("p n c -> p c n").to_broadcast([8, 8, NCH]), in1=sel8[:].unsqueeze(2).to_broadcast([8, 8, NCH]), op=ALU.mult),
                     reads=['Af', 'sel8'], writes=['bd'])
                S.op('pe', lambda e: e.matmul(PB[3][:, 0:288], lhsT=ones[0:8, :], rhs=bd[:].rearrange("p a n -> p (a n)"), start=True, stop=True), reads=['ones', 'bd'], writes=['pb3'], skip_self=True)
                S.op('act', lambda e: e.activation(out=dchunk[:].rearrange("p a n -> p (a n)"), in_=PB[3][:, 0:288], func=AF.Exp), reads=['pb3'], writes=['dchunk'])
                S.barrier()
            qc = sb(m0, "mqc", [128, NTOK], BF16); kc_ = sb(m0, "mkc", [128, NTOK], BF16); kTM = sb(m0, "mkTM", [64, NCH, 128], BF16)
            raw = sb(m0, "mraw", [128, NTOK]); acc = sb(m0, "macc", [128, NTOK])
            vext = sb(m0, "mvext", [64, NCH, 132], BF16); vi = sb(m0, "mvi", [64, NCH, 132], BF16); vu = sb(m0, "mvu", [64, NCH, 132], BF16)
            og = sb(m0, "mog", [64, NLCH, 128], BF16); oext = sb(m0, "moext", [64, NLCH, 132]); obuf = sb(m0, "mobuf", [64, NLCH, 128])
            den = sb(m0, "mden", [64, NLCH]); mu = sb(m0, "mmu", [64, NLCH]); m2 = sb(m0, "mm2", [64, NLCH])
            S.op('dve', lambda e: e.memset(vext[:], 1.0), writes=['mvext'])
            for hd in range(4):
                with ExitStack() as hs:
                    wq_ = load_w(hs, "mwq", 2560 + hd * 128, 128); wk_ = load_w(hs, "mwk", 3072 + hd * 128, 128)
                    wv_ = load_w(hs, "mwv", 3584 + hd * 128, 128); wo_ = load_w(hs, "mwo", 4096 + hd * 128, 128)
                    prep_tables(64)
                    for g in range(9):
                        bk = 3 + g % 2
                        proj_tm_group(wv_, "mwv", 128, bk, g * 4, 4)
                        S.op('act', lambda e, g=g, bk=bk: e.copy(out=vext[:, g * 4:(g + 1) * 4, 0:128], in_=PB[bk][0:64, :].rearrange("p (j c) -> p j c", c=128)),
                             reads=['pb%d' % bk], writes=['mvext'])
                    for g in range(8):
                        bk = 3 + (g + 1) % 2
                        proj_tm_group(wo_, "mwo", 128, bk, 4 + g * 4, 4)
                        S.op('act', lambda e, g=g, bk=bk: e.activation(out=og[:, g * 4:(g + 1) * 4, :], in_=PB[bk][0:64, :].rearrange("p (j c) -> p j c", c=128), func=AF.Sigmoid),
                             reads=['pb%d' % bk], writes=['mog'])
                    for qi, (wb, wn, dstb) in enumerate(((wq_, "mwq", qc), (wk_, "mwk", kc_))):
                        chn = qi * 4 + hd
                        for bi, (t0, nt) in enumerate(BLKS):
                            proj_fm_block(wb, wn, 128, bi % 2, t0, nt)
                            S.op('act', lambda e, t0=t0, nt=nt, bi=bi: e.copy(out=raw[:, t0:t0 + nt], in_=PB[bi % 2][:, 0:nt]), reads=['pb%d' % (bi % 2)], writes=['mraw'])
                        S.op('dve', lambda e, chn=chn: e.tensor_scalar(out=acc[:, 0:256], in0=raw[:, 0:256], scalar1=convw[:, 4, chn:chn + 1], scalar2=convb[:, chn:chn + 1], op0=ALU.mult, op1=ALU.add),
                             reads=['mraw', 'convw', 'convb'], writes=['macc'])
                        S.op('dve', lambda e, chn=chn: e.scalar_tensor_tensor(out=acc[:, 1:256], in0=raw[:, 0:255], scalar=convw[:, 3, chn:chn + 1], in1=acc[:, 1:256], op0=ALU.mult, op1=ALU.add),
                             reads=['mraw', 'convw', 'macc'], writes=['macc'])
                        S.op('dve', lambda e, chn=chn: e.scalar_tensor_tensor(out=acc[:, 0:255], in0=raw[:, 1:256], scalar=convw[:, 5, chn:chn + 1], in1=acc[:, 0:255], op0=ALU.mult, op1=ALU.add),
                             reads=['mraw', 'convw', 'macc'], writes=['macc'])
                        X = raw[:, 256:NTOK].rearrange("p (r c) -> p r c", c=64); Y = acc[:, 256:NTOK].rearrange("p (r c) -> p r c", c=64)
                        S.op('dve', lambda e, chn=chn: e.tensor_scalar(out=acc[:, 256:NTOK], in0=raw[:, 256:NTOK], scalar1=convw[:, 4, chn:chn + 1], scalar2=convb[:, chn:chn + 1], op0=ALU.mult, op1=ALU.add),
                             reads=['mraw', 'convw', 'convb', 'macc'], writes=['macc'])
                        for ky in range(3):
                            for kx in range(3):
                                if ky == 1 and kx == 1:
                                    continue
                                dy = ky - 1; dx = kx - 1
                                r0 = max(0, -dy); r1 = 32 - max(0, dy); c0 = max(0, -dx); c1 = 64 - max(0, dx)
                                S.op('dve', lambda e, chn=chn, ky=ky, kx=kx, r0=r0, r1=r1, c0=c0, c1=c1, dy=dy, dx=dx: e.scalar_tensor_tensor(
                                    out=Y[:, r0:r1, c0:c1], in0=X[:, r0 + dy:r1 + dy, c0 + dx:c1 + dx], scalar=convw[:, ky * 3 + kx, chn:chn + 1], in1=Y[:, r0:r1, c0:c1],
                                    op0=ALU.mult, op1=ALU.add), reads=['mraw', 'convw', 'macc'], writes=['macc'])
                        S.op('act', lambda e: e.activation(out=acc[:], in_=acc[:], func=AF.Silu), reads=['macc'], writes=['macc'])
                        if qi == 0:
                            S.op('dve', lambda e: e.tensor_copy(out=qc[:], in_=acc[:]), reads=['macc'], writes=['mqc'])
                        else:
                            S.op('dve', lambda e: e.tensor_scalar_mul(out=kc_[:], in0=acc[:], scalar1=128.0 ** -0.5), reads=['macc'], writes=['mkc'])
                    pbb = PB[3][:].bitcast(BF16)
                    for g in range(5):
                        nck = 8 if g < 4 else 4
                        for j in range(nck):
                            n = g * 8 + j
                            S.op('pe', lambda e, j=j, n=n: e.transpose(pbb[0:64, j * 128:(j + 1) * 128], kc_[:, n * 64:(n + 1) * 64], identb[:]),
                                 reads=['mkc', 'identb'], writes=['pb3'], skip_self=True)
                        S.op('act', lambda e, g=g, nck=nck: e.copy(out=kTM[:, g * 8:g * 8 + nck, :], in_=pbb[0:64, 0:nck * 128].rearrange("p (j c) -> p j c", c=128)),
                             reads=['pb3'], writes=['mkTM'])
                    for d in range(2):
                        row = d * 4 + hd
                        S.op('dve', lambda e, row=row: e.tensor_tensor(out=vi[:], in0=vext[:], in1=RUU[:, :, 8 + row:9 + row].to_broadcast([64, NCH, 132]), op=ALU.mult),
                             reads=['mvext', 'RUU'], writes=['mvi'])
                        S.op('dve', lambda e, row=row: e.tensor_tensor(out=vu[:], in0=vext[:], in1=RUU[:, :, 16 + row:17 + row].to_broadcast([64, NCH, 132]), op=ALU.mult),
                             reads=['mvext', 'RUU'], writes=['mvu'])

                        def evac(nl, n, po, pok, row=row):
                            S.op('act', lambda e: e.copy(out=oext[:, nl, 0:129], in_=po), reads=[pok], writes=K('moext', nl))
                        chunk_loop(d, kc_, qc, vi, qc, kTM, vu, lambda n, row=row: dchunk[:, row, n:n + 1], 129, evac,
                                   ['mqc', 'mkc', 'mvi', 'mvu', 'mkTM'], 'dchunk')
                        allx = K('moext', 0, NLCH)
                        S.op('dve', lambda e, row=row: e.tensor_tensor(out=oext[:, :, 0:129], in0=oext[:, :, 0:129], in1=RUU[:, 4:NCH, row:row + 1].to_broadcast([64, NLCH, 129]), op=ALU.mult),
                             reads=allx + ['RUU'], writes=allx)
                        S.op('act', lambda e: e.activation(out=den[:], in_=oext[:, :, 128], func=AF.Abs), reads=allx, writes=['mden'])
                        S.op('dve', lambda e: e.tensor_scalar_max(out=den[:], in0=den[:], scalar1=1.0), reads=['mden'], writes=['mden'])
                        S.op('dve', lambda e: e.reciprocal(out=den[:], in_=den[:]), reads=['mden'], writes=['mden'])
                        if d == 0:
                            S.op('dve', lambda e: e.tensor_tensor(out=obuf[:], in0=oext[:, :, 0:128], in1=den[:].unsqueeze(2).to_broadcast([64, NLCH, 128]), op=ALU.mult),
                                 reads=allx + ['mden'], writes=['mobuf'])
                        else:
                            S.op('dve', lambda e: e.tensor_tensor(out=oext[:, :, 0:128], in0=oext[:, :, 0:128], in1=den[:].unsqueeze(2).to_broadcast([64, NLCH, 128]), op=ALU.mult),
                                 reads=allx + ['mden'], writes=allx)
                            S.op('dve', lambda e: e.tensor_tensor(out=obuf[:], in0=obuf[:], in1=oext[:, :, 0:128], op=ALU.add), reads=allx + ['mobuf'], writes=['mobuf'])
                    allx = K('moext', 0, NLCH)
                    S.op('dve', lambda e: e.tensor_reduce(out=mu[:], in_=obuf[:], axis=AX.X, op=ALU.add), reads=['mobuf'], writes=['mmu'])
                    S.op('dve', lambda e: e.tensor_scalar_mul(out=mu[:], in0=mu[:], scalar1=1.0 / 128.0), reads=['mmu'], writes=['mmu'])
                    S.op('dve', lambda e: e.tensor_tensor(out=obuf[:], in0=obuf[:], in1=mu[:].unsqueeze(2).to_broadcast([64, NLCH, 128]), op=ALU.subtract), reads=['mobuf', 'mmu'], writes=['mobuf'])
                    S.op('dve', lambda e: e.tensor_tensor(out=oext[:, :, 0:128], in0=obuf[:], in1=obuf[:], op=ALU.mult), reads=['mobuf'] + allx, writes=allx)
                    S.op('dve', lambda e: e.tensor_reduce(out=m2[:], in_=oext[:, :, 0:128], axis=AX.X, op=ALU.add), reads=allx, writes=['mm2'])
                    S.op('act', lambda e: e.activation(out=m2[:], in_=m2[:], func=AF.Sqrt, bias=epsc[0:64, 0:1], scale=1.0 / 128.0), reads=['mm2', 'epsc'], writes=['mm2'])
                    S.op('dve', lambda e: e.reciprocal(out=m2[:], in_=m2[:]), reads=['mm2'], writes=['mm2'])
                    S.op('dve', lambda e: e.tensor_tensor(out=obuf[:], in0=obuf[:], in1=m2[:].unsqueeze(2).to_broadcast([64, NLCH, 128]), op=ALU.mult), reads=['mobuf', 'mm2'], writes=['mobuf'])
                    S.op('dve', lambda e, hd=hd: e.tensor_tensor(out=obuf[:], in0=obuf[:], in1=mln[:, hd * 128:(hd + 1) * 128].unsqueeze(1).to_broadcast([64, NLCH, 128]), op=ALU.mult),
                         reads=['mobuf', 'mln'], writes=['mobuf'])
                    S.op('dve', lambda e: e.tensor_tensor(out=obuf[:], in0=obuf[:], in1=og[:], op=ALU.mult), reads=['mobuf', 'mog'], writes=['mobuf'])
                    transpose_chunks_to_mixT(obuf, ['mobuf'], 4 + hd)
            S.barrier()

        if debug == 'mix':
            with ExitStack() as dd:
                mf = sb(dd, "mf", [128, NLAT])
                for h in range(8):
                    S.op('dve', lambda e, h=h: e.tensor_copy(out=mf[:], in_=mixT[:, h, :]), reads=K('mixT', h) + ['mf'], writes=['mf'])
                    S.dma('sp', lambda e, h=h: e.dma_start(out=dbg_d[h * 128:(h + 1) * 128, :], in_=mf[:]), reads=['mf'], writes=['dbg'])
                S.wait_all('sp', ['dbg']); S.barrier()
            scB.close(); scA.close()
            return nc
        prep_tables(512)
        S.barrier()
        scB.close()
        x1s = nc.dram_tensor("x1s", [NLAT, 1024], F32, kind="Internal").ap()

        def bcast_tile(dst, dkey, c0, dg):
            for ch in range(8):
                S.op('dve', lambda e, ch=ch: e.tensor_scalar_mul(out=dg[:], in0=ident[:], scalar1=modv[:, c0 + ch, 0:1]), reads=['ident', 'modv', 'dg'], writes=['dg'])
                bank = ch // 4
                S.op('pe', lambda e, ch=ch, bank=bank: e.matmul(PB[bank][:, (ch % 4) * 128:(ch % 4 + 1) * 128], lhsT=ones[:], rhs=dg[:], start=True, stop=True),
                     reads=['ones', 'dg'], writes=['pb%d' % bank], skip_self=True)
            S.op('act', lambda e: e.copy(out=dst[:, 0:512], in_=PB[0][:, :]), reads=['pb0'], writes=[dkey])
            S.op('act', lambda e: e.copy(out=dst[:, 512:1024], in_=PB[1][:, :]), reads=['pb1'], writes=[dkey])

        with ExitStack() as p3:
            bct = {}
            for nm in ("g1b", "ln1g", "ln1b"):
                bct[nm] = sb(p3, nm, [128, 1024])
            for nm, dd in (("ln1g", ln1g_d), ("ln1b", ln1b_d)):
                S.dma('sp', lambda e, nm=nm, dd=dd: e.dma_start(out=bct[nm][:], in_=dd[:, :]), writes=[nm])
            dg = sb(p3, "dg", [128, 128])
            bcast_tile(bct["g1b"], "g1b", 16, dg)
            wo_b = sb(p3, "wout", [128, 8, 1024], BF16); wos = sb(p3, "wos", [128, 1024])
            for kc in range(8):
                S.dma('sp', lambda e, kc=kc: e.dma_start(out=wos[:], in_=wout_d[:, kc, :]), writes=['wos'])
                S.op('pool', lambda e, kc=kc: e.tensor_copy(out=wo_b[:, kc, :], in_=wos[:]), reads=['wos'], writes=['wout'])
            xt = [sb(p3, "x3t%d" % i, [128, 1024]) for i in range(2)]
            t1 = [sb(p3, "t1_%d" % i, [128, 1024]) for i in range(2)]
            st = [sb(p3, "st3%d" % i, [128, 2, 6]) for i in range(2)]
            mv = [sb(p3, "mv3%d" % i, [128, 2]) for i in range(2)]
            rs = [sb(p3, "rs3%d" % i, [128, 1]) for i in range(2)]
            for t in range(16):
                b = t % 2
                S.dma('sp' if b == 0 else 'pool', lambda e, t=t, b=b: e.dma_start(out=xt[b][:], in_=xs[256 + t * 128:256 + (t + 1) * 128, :]), writes=['x3t%d' % b])
                for half in range(2):
                    bank = 2 * b + half
                    for kc in range(8):
                        S.op('pe', lambda e, kc=kc, t=t, half=half, bank=bank: e.matmul(PB[bank][:, :], lhsT=mixT[:, kc, t * 128:(t + 1) * 128], rhs=wo_b[:, kc, half * 512:(half + 1) * 512],
                                                                                      start=(kc == 0), stop=(kc == 7)),
                             reads=K('mixT', kc) + ['wout'], writes=['pb%d' % bank], skip_self=True)
                    S.op('dve', lambda e, b=b, half=half, bank=bank: e.tensor_tensor(out=t1[b][:, half * 512:(half + 1) * 512], in0=PB[bank][:, :], in1=bct["g1b"][:, half * 512:(half + 1) * 512], op=ALU.mult),
                         reads=['pb%d' % bank, 'g1b'], writes=['t1_%d' % b])
                S.op('dve', lambda e, b=b: e.scalar_tensor_tensor(out=t1[b][:], in0=xt[b][:], scalar=ALPHA, in1=t1[b][:], op0=ALU.mult, op1=ALU.add),
                     reads=['x3t%d' % b, 't1_%d' % b], writes=['t1_%d' % b])
                layer_norm_rows(st[b], mv[b][:], rs[b][:], t1[b][:], t1[b][:], 't1_%d' % b, 't1_%d' % b, 'p3%d' % b)
                S.op('dve', lambda e, b=b: e.tensor_tensor(out=t1[b][:], in0=t1[b][:], in1=bct["ln1g"][:], op=ALU.mult), reads=['t1_%d' % b, 'ln1g'], writes=['t1_%d' % b])
                S.op('dve', lambda e, b=b, t=t: e.tensor_tensor(out=t1[b][:], in0=t1[b][:], in1=bct["ln1b"][:], op=ALU.add), reads=['t1_%d' % b, 'ln1b'], writes=['t1_%d' % b])
                S.dma('sp', lambda e, t=t, b=b: e.dma_start(out=x1s[t * 128:(t + 1) * 128, :], in_=t1[b][:]), reads=['t1_%d' % b], writes=K('x1_', t))
                if debug == 'x1':
                    S.dma('sp', lambda e, t=t, b=b: e.dma_start(out=dbg_d[t * 128:(t + 1) * 128, :], in_=t1[b][:]), reads=['t1_%d' % b], writes=['dbg'])
            S.barrier()
        scA.close()

        with ExitStack() as p4:
            bct = {}
            for nm in ("g2b", "sc2b", "sh2b", "ln2g", "ln2b"):
                bct[nm] = sb(p4, nm, [128, 1024])
            for nm, dd in (("ln2g", ln2g_d), ("ln2b", ln2b_d)):
                S.dma('sp', lambda e, nm=nm, dd=dd: e.dma_start(out=bct[nm][:], in_=dd[:, :]), writes=[nm])
            dg = sb(p4, "dg4", [128, 128])
            bcast_tile(bct["g2b"], "g2b", 40, dg); bcast_tile(bct["sc2b"], "sc2b", 32, dg); bcast_tile(bct["sh2b"], "sh2b", 24, dg)
            x1t = [sb(p4, "x1t%d" % i, [128, 1024]) for i in range(2)]
            wqb = sb(p4, "wqb", [128, 8, 2048], BF16)
            keysT = sb(p4, "keysT", [128, 16, 128], BF16)
            with ExitStack() as tmp:
                stg = sb(tmp, "wq_stg", [128, 2048])
                for kc in range(8):
                    S.dma('sp', lambda e, kc=kc: e.dma_start(out=stg[:], in_=wq_d[:, kc, :]), writes=['wq_stg'])
                    S.op('pool', lambda e, kc=kc: e.tensor_copy(out=wqb[:, kc, :], in_=stg[:]), reads=['wq_stg'], writes=['wqb'])
                kst = sb(tmp, "kst", [128, 16, 128])
                S.dma('sp', lambda e: e.dma_start(out=kst[:], in_=keysT_d[:, :, :]), writes=['kst'])
                S.op('pool', lambda e: e.tensor_copy(out=keysT[:], in_=kst[:]), reads=['kst'], writes=['keysT'])
                S.barrier()
            NG = 8
            uvb = [sb(p4, "uvb%d" % i, [128, 2048], BF16) for i in range(NG)]
            gl = sb(p4, "gl", [128, 128])
            pr = [sb(p4, "pr%d" % i, [128, 1024], BF16) for i in range(4)]
            dgk = [sb(p4, "dgk%d" % i, [128, 128], BF16) for i in range(4)]
            h2 = sb(p4, "h2", [128, 1024]); h2b = [sb(p4, "h2b%d" % i, [128, 1024], BF16) for i in range(2)]
            h2T = sb(p4, "h2T", [128, 8, 128], BF16); qT = sb(p4, "qT", [128, 16, 128], BF16)
            sc = sb(p4, "sc", [128, 16, 128]); scw = sb(p4, "scw", [128, 16, 128])
            top = sb(p4, "top", [128, 16, 16]); topi = sb(p4, "topi", [128, 16, 16], U32); topf = sb(p4, "topf", [128, 16, 16])
            cand = sb(p4, "cand", [128, 8, 256]); candw = sb(p4, "candw", [128, 8, 256]); eq = sb(p4, "eq", [128, 128, 16])
            best = sb(p4, "best", [128, 8, 16]); idxf = sb(p4, "idxf", [128, 128])
            posi = sb(p4, "posi", [128, 8, 16], U32); pai = sb(p4, "pai", [128, 128], U32); pbi = sb(p4, "pbi", [128, 128], U32)
            paf = sb(p4, "paf", [128, 128]); pbf = sb(p4, "pbf", [128, 128]); iaf = sb(p4, "iaf", [128, 128]); ibf = sb(p4, "ibf", [128, 128])
            iota16 = sb(p4, "iota16", [128, 16])
            S.dma('sp', lambda e: e.dma_start(out=iota16[:], in_=iota_d[:, :]), writes=['iota16'])
            gate = [sb(p4, "gate%d" % i, [128, 128]) for i in range(2)]
            idxi = [sb(p4, "idxi%d" % i, [128, 128], I32) for i in range(2)]
            wgt = [sb(p4, "wgt%d" % i, [128, 128]) for i in range(2)]
            dots = sb(p4, "dots", [128, 128]); junk = sb(p4, "junk", [128, 1024], BF16); junk2 = sb(p4, "junk2", [128, 256])
            zs = sb(p4, "zs", [128, 8]); nmx = sb(p4, "nmx", [128, 8])
            st = sb(p4, "st4", [128, 2, 6]); mv = sb(p4, "mv4", [128, 2]); rs = sb(p4, "rs4", [128, 1])
            fin = sb(p4, "fin", [128, 1024]); yb = sb(p4, "yb", [128, 1024])
            NT4 = 16 if debug != 'x1' else 0

            def prologue(t):
                p = t % 2
                xb_ = x1t[p]; x1k = 'x1t%d' % p
                S.dma('sp', lambda e: e.dma_start(out=xb_[:], in_=x1s[t * 128:(t + 1) * 128, :]), reads=K('x1_', t), writes=[x1k])
                S.op('dve', lambda e: e.memset(idxf[:], 0.0), writes=['idxf'])
                S.op('dve', lambda e: e.memset(zs[:], 0.0), writes=['zs'])
                layer_norm_rows(st, mv[:], rs[:], xb_[:], h2[:], x1k, 'h2', 'p4')
                S.op('dve', lambda e: e.tensor_tensor(out=h2[:], in0=h2[:], in1=bct["sc2b"][:], op=ALU.mult), reads=['h2', 'sc2b'], writes=['h2'])
                S.op('dve', lambda e: e.tensor_tensor(out=h2[:], in0=h2[:], in1=bct["sh2b"][:], op=ALU.add), reads=['h2', 'sh2b'], writes=['h2'])
                S.op('act', lambda e: e.copy(out=h2b[p][:], in_=h2[:]), reads=['h2'], writes=['h2b%d' % p])
                yield
                for half in range(2):
                    for c4 in range(4):
                        ch = half * 4 + c4
                        S.op('pe', lambda e, ch=ch, c4=c4, half=half: e.transpose(PB[half][:, c4 * 128:(c4 + 1) * 128], h2[:, ch * 128:(ch + 1) * 128], ident[:]),
                             reads=['h2', 'ident'], writes=['pb%d' % half], skip_self=True)
                    yield
                    S.op('act', lambda e, half=half: e.copy(out=h2T[:, half * 4:(half + 1) * 4, :], in_=PB[half][:, :].rearrange("p (j c) -> p j c", c=128)), reads=['pb%d' % half], writes=['h2T'])
                for g in range(4):
                    for c4 in range(4):
                        c = g * 4 + c4
                        for kc in range(8):
                            S.op('pe', lambda e, c=c, c4=c4, kc=kc, g=g: e.matmul(PB[2 + g][:, c4 * 128:(c4 + 1) * 128], lhsT=wqb[:, kc, c * 128:(c + 1) * 128], rhs=h2T[:, kc, :],
                                                                                start=(kc == 0), stop=(kc == 7)), reads=['wqb', 'h2T'], writes=['pb%d' % (2 + g)], skip_self=True)
                    yield
                    S.op('act' if g % 2 == 0 else 'dve', lambda e, g=g: (e.copy if g % 2 == 0 else e.tensor_copy)(out=qT[:, g * 4:(g + 1) * 4, :], in_=PB[2 + g][:, :].rearrange("p (j c) -> p j c", c=128)),
                         reads=['pb%d' % (2 + g)], writes=['qT'])
                for g in range(4):
                    for c4 in range(4):
                        c = g * 4 + c4
                        S.op('pe', lambda e, c=c, c4=c4, g=g: e.matmul(PB[2 + g][:, c4 * 128:(c4 + 1) * 128], lhsT=qT[:, c, :], rhs=keysT[:, c, :], start=True, stop=True),
                             reads=['qT', 'keysT'], writes=['pb%d' % (2 + g)], skip_self=True)
                    yield
                    S.op('act' if g % 2 == 0 else 'dve', lambda e, g=g: (e.copy if g % 2 == 0 else e.tensor_copy)(out=sc[:, g * 4:(g + 1) * 4, :], in_=PB[2 + g][:, :].rearrange("p (j c) -> p j c", c=128)),
                         reads=['pb%d' % (2 + g)], writes=['sc'])
                for c in range(16):
                    S.op('dve', lambda e, c=c: e.max(out=top[:, c, 0:8], in_=sc[:, c, :]), reads=['sc'], writes=['top'])
                    S.op('dve', lambda e, c=c: e.max_index(out=topi[:, c, 0:8], in_max=top[:, c, 0:8], in_values=sc[:, c, :]), reads=['sc', 'top'], writes=['topi'])
                    S.op('dve', lambda e, c=c: e.match_replace(out=scw[:, c, :], in_to_replace=top[:, c, 0:8], in_values=sc[:, c, :], imm_value=-1e30), reads=['sc', 'top'], writes=['scw'])
                    S.op('dve', lambda e, c=c: e.max(out=top[:, c, 8:16], in_=scw[:, c, :]), reads=['scw'], writes=['top'])
                    S.op('dve', lambda e, c=c: e.max_index(out=topi[:, c, 8:16], in_max=top[:, c, 8:16], in_values=scw[:, c, :]), reads=['scw', 'top'], writes=['topi'])
                    yield
                S.op('dve', lambda e: e.tensor_copy(out=topf[:], in_=topi[:]), reads=['topi'], writes=['topf'])
                t4 = top[:].rearrange("p (h two) k -> p h two k", two=2); f4 = topf[:].rearrange("p (h two) k -> p h two k", two=2)
                c4v = cand[:].rearrange("p h (a b) -> p h a b", b=16)
                for h in range(8):
                    S.op('dve', lambda e, h=h: e.tensor_tensor(out=c4v[:, h, :, :], in0=t4[:, h, 0, :].unsqueeze(2).to_broadcast([128, 16, 16]), in1=t4[:, h, 1, :].unsqueeze(1).to_broadcast([128, 16, 16]), op=ALU.add),
                         reads=['top'], writes=['cand'])
                    yield
                for h in range(8):
                    S.op('dve', lambda e, h=h: e.max(out=best[:, h, 0:8], in_=cand[:, h, :]), reads=['cand'], writes=['best'])
                    S.op('dve', lambda e, h=h: e.match_replace(out=candw[:, h, :], in_to_replace=best[:, h, 0:8], in_values=cand[:, h, :], imm_value=-1e30), reads=['cand', 'best'], writes=['candw'])
                    S.op('dve', lambda e, h=h: e.max(out=best[:, h, 8:16], in_=candw[:, h, :]), reads=['candw'], writes=['best'])
                    S.op('dve', lambda e, h=h: e.max_index(out=posi[:, h, 0:8], in_max=best[:, h, 0:8], in_values=cand[:, h, :]), reads=['cand', 'best'], writes=['posi'])
                    S.op('dve', lambda e, h=h: e.max_index(out=posi[:, h, 8:16], in_max=best[:, h, 8:16], in_values=candw[:, h, :]), reads=['candw', 'best'], writes=['posi'])
                    yield
                pflat = posi[:].rearrange("p h k -> p (h k)")
                S.op('dve', lambda e: e.tensor_single_scalar(out=pai[:], in_=pflat, scalar=4, op=ALU.logical_shift_right), reads=['posi'], writes=['pai'])
                S.op('dve', lambda e: e.tensor_single_scalar(out=pbi[:], in_=pflat, scalar=15, op=ALU.bitwise_and), reads=['posi'], writes=['pbi'])
                S.op('dve', lambda e: e.tensor_copy(out=paf[:], in_=pai[:]), reads=['pai'], writes=['paf'])
                S.op('dve', lambda e: e.tensor_copy(out=pbf[:], in_=pbi[:]), reads=['pbi'], writes=['pbf'])
                yield
                eq4 = eq[:].rearrange("p (h k) a -> p h k a", k=16)
                for (pp, pk_, half_, dst, dn) in ((paf, 'paf', 0, iaf, 'iaf'), (pbf, 'pbf', 1, ibf, 'ibf')):
                    S.op('dve', lambda e, pp=pp: e.tensor_tensor(out=eq[:], in0=iota16[:].unsqueeze(1).to_broadcast([128, 128, 16]), in1=pp[:].unsqueeze(2).to_broadcast([128, 128, 16]), op=ALU.is_equal),
                         reads=['iota16', pk_, 'eq'], writes=['eq'])
                    S.op('dve', lambda e, half_=half_: e.tensor_tensor(out=eq4, in0=eq4, in1=f4[:, :, half_, :].unsqueeze(2).to_broadcast([128, 8, 16, 16]), op=ALU.mult),
                         reads=['eq', 'topf'], writes=['eq'])
                    S.op('dve', lambda e, dst=dst: e.tensor_reduce(out=dst[:], in_=eq[:], axis=AX.X, op=ALU.add), reads=['eq'], writes=[dn])
                    yield
                S.op('dve', lambda e: e.scalar_tensor_tensor(out=idxf[:], in0=iaf[:], scalar=128.0, in1=ibf[:], op0=ALU.mult, op1=ALU.add), reads=['iaf', 'ibf'], writes=['idxf'])
                S.op('dve', lambda e: e.tensor_scalar_min(out=idxf[:], in0=idxf[:], scalar1=16383.0), reads=['idxf'], writes=['idxf'])
                S.op('dve', lambda e: e.tensor_copy(out=idxi[p][:], in_=idxf[:]), reads=['idxf'], writes=['idxi%d' % p])
                S.op('dve', lambda e: e.tensor_scalar_mul(out=nmx[:], in0=best[:, :, 0], scalar1=-1.0), reads=['best'], writes=['nmx'])
                g3 = gate[p][:].rearrange("p (h k) -> p h k", k=16)
                for h in range(8):
                    S.op('act', lambda e, h=h: e.activation(out=g3[:, h, :], in_=best[:, h, :], func=AF.Exp, bias=nmx[:, h:h + 1], scale=1.0, accum_out=zs[:, h:h + 1]),
                         reads=['best', 'nmx'], writes=['gate%d' % p, 'zs'])
                S.op('dve', lambda e: e.reciprocal(out=zs[:], in_=zs[:]), reads=['zs'], writes=['zs'])
                S.op('dve', lambda e: e.tensor_tensor(out=g3, in0=g3, in1=zs[:].unsqueeze(2).to_broadcast([128, 8, 16]), op=ALU.mult), reads=['gate%d' % p, 'zs'], writes=['gate%d' % p])

            def fused(t, gen=None):
                p = t % 2
                LAG = 2

                def tail(kk):
                    s_ = kk % NG; d4 = kk % 4
                    S.op('dve', lambda e: e.tensor_scalar(out=dgk[d4][:], in0=identb[:], scalar1=gl[:, kk:kk + 1], scalar2=gate[p][:, kk:kk + 1], op0=ALU.mult, op1=ALU.mult),
                         reads=['identb', 'gl_%d' % kk, 'gate%d' % p], writes=['dgk%d' % d4])
                    for half in range(2):
                        S.op('pe', lambda e, half=half: e.matmul(PB[6 + half][:, :], lhsT=dgk[d4][:], rhs=uvb[s_][:, 1024 + half * 512:1024 + (half + 1) * 512], start=(kk == 0), stop=(kk == 127)),
                             reads=['dgk%d' % d4, 'uvb%d' % s_], writes=['pb%d' % (6 + half)], skip_self=True)

                for k in range(128):
                    s_ = k % NG; j4 = k % 4
                    dk = 'dots_%d' % k
                    S.dma('pool', lambda e, k=k, s_=s_: e.indirect_dma_start(out=uvb[s_][:], out_offset=None, in_=uv_d[:, :], in_offset=bass.IndirectOffsetOnAxis(ap=idxi[p][:, k:k + 1], axis=0)),
                          reads=['idxi%d' % p, 'tabs'], writes=['uvb%d' % s_])
                    S.op('dve', lambda e, k=k, s_=s_, j4=j4: e.tensor_tensor(out=pr[j4][:], in0=uvb[s_][:, 0:1024], in1=h2b[p][:], op=ALU.mult),
                         reads=['uvb%d' % s_, 'h2b%d' % p], writes=['pr%d' % j4])
                    S.op('act', lambda e, k=k, j4=j4: e.activation(out=junk[:], in_=pr[j4][:], func=AF.Identity, accum_out=dots[:, k:k + 1]),
                         reads=['pr%d' % j4, 'dots0'], writes=[dk])
                    S.op('act', lambda e, k=k: e.activation(out=gl[:, k:k + 1], in_=dots[:, k:k + 1], func=AF.Gelu), reads=[dk], writes=['gl_%d' % k])
                    if k >= LAG:
                        tail(k - LAG)
                    if gen is not None and k % 2 == 1:
                        next(gen, None)
                for kk in range(128 - LAG, 128):
                    tail(kk)
                if gen is not None:
                    for _ in gen:
                        pass
                xb_ = x1t[p]; x1k = 'x1t%d' % p
                for half in range(2):
                    S.op('dve', lambda e, half=half: e.tensor_tensor(out=yb[:, half * 512:(half + 1) * 512], in0=PB[6 + half][:, :], in1=bct["g2b"][:, half * 512:(half + 1) * 512], op=ALU.mult),
                         reads=['pb%d' % (6 + half), 'g2b', 'yb'], writes=['yb'])
                S.op('dve', lambda e: e.scalar_tensor_tensor(out=fin[:], in0=xb_[:], scalar=ALPHA, in1=yb[:], op0=ALU.mult, op1=ALU.add), reads=[x1k, 'yb', 'fin'], writes=['fin'])
                layer_norm_rows(stf, mvf[:], rsf[:], fin[:], fin[:], 'fin', 'fin', 'p4f')
                S.op('dve', lambda e: e.tensor_tensor(out=fin[:], in0=fin[:], in1=bct["ln2g"][:], op=ALU.mult), reads=['fin', 'ln2g'], writes=['fin'])
                S.op('dve', lambda e: e.tensor_tensor(out=fin[:], in0=fin[:], in1=bct["ln2b"][:], op=ALU.add), reads=['fin', 'ln2b'], writes=['fin'])
                S.dma('sp', lambda e: e.dma_start(out=out_d[t * 128:(t + 1) * 128, :], in_=fin[:]), reads=['fin'], writes=['out'])

            stf = sb(p4, "st4f", [128, 2, 6]); mvf = sb(p4, "mv4f", [128, 2]); rsf = sb(p4, "rs4f", [128, 1])
            S.op('dve', lambda e: e.memset(dots[:], 0.0), writes=['dots0'])
            if NT4:
                for _ in prologue(0):
                    pass
            for t in range(NT4):
                fused(t, prologue(t + 1) if t + 1 < NT4 else None)
            S.wait_all('sp', ['out', 'dbg'])
            S.barrier()
    return nc


def _prep_shared(inp):
    f = np.float32
    sh = {}
    sh["w_mod"] = np.ascontiguousarray(inp["w_mod"][0].reshape(8, 128, 6144).transpose(1, 0, 2))
    sh["b_modT"] = np.ascontiguousarray(inp["b_mod"][0].reshape(48, 128).T)
    sh["w_in"] = np.ascontiguousarray(inp["w_in"][0].reshape(8, 128, 4624).transpose(1, 0, 2))
    sh["lgT"] = np.ascontiguousarray(inp["hg_lb_logits"].reshape(2, 2, 4, 128).transpose(3, 0, 1, 2))
    sh["hgn"] = np.ascontiguousarray(np.broadcast_to(inp["hg_norm_g"][0][None, :], (64, 512)))
    sh["mln"] = np.ascontiguousarray(np.broadcast_to(inp["ml_norm_g"][0][None, :], (64, 512)))
    sh["convw"] = np.ascontiguousarray(inp["ml_conv_w"][0].reshape(9, 8, 128).transpose(2, 0, 1))
    sh["convb"] = np.ascontiguousarray(inp["ml_conv_b"][0].reshape(8, 128).T)
    sh["gateb"] = np.ascontiguousarray(inp["ml_gate_b"][0].reshape(2, 8).T)
    sh["w_out"] = np.ascontiguousarray(inp["w_out"][0].reshape(8, 128, 1024).transpose(1, 0, 2))
    for nm, key in (("ln1g", "ln1_g"), ("ln1b", "ln1_b"), ("ln2g", "ln2_g"), ("ln2b", "ln2_b")):
        sh[nm] = np.ascontiguousarray(np.broadcast_to(inp[key][0][None, :], (128, 1024)))
    sh["wq"] = np.ascontiguousarray(inp["peer_wq"][0].reshape(8, 128, 2048).transpose(1, 0, 2))
    sh["keysT"] = np.ascontiguousarray(inp["peer_keys"][0].reshape(16, 128, 128).transpose(2, 0, 1))
    nexp = 128 if os.environ.get("KDEBUG") else 16384
    sh["pu"] = np.ascontiguousarray(inp["peer_u"][0][:nexp])
    sh["pv"] = np.ascontiguousarray(inp["peer_v"][0][:nexp])
    sh["ident"] = np.eye(128, dtype=f)
    m = np.zeros((64, 2, 64), f)
    s = np.arange(64)[:, None]; c = np.arange(64)[None, :]
    m[:, 0, :] = (s <= c); m[:, 1, :] = (s >= c)
    sh["masks"] = m
    rm = np.ones((128, 512), f); rm[:, ::64] = 0.0
    sh["rmask"] = rm
    sh["sel8"] = np.eye(8, dtype=f)
    sh["iota16"] = np.ascontiguousarray(np.broadcast_to(np.arange(16, dtype=f)[None, :], (128, 16)))
    dm = np.zeros((8, 2), f); dm[0:4, 0] = 1.0; dm[4:8, 1] = 1.0
    sh["dirm"] = dm
    return {k: np.asarray(v, dtype=f) for k, v in sh.items()}


def kernel(**inputs):
    inp = {k: np.asarray(v) for k, v in inputs.items()}
    debug = os.environ.get("KDEBUG") or None
    nc = build(debug)
    sh = _prep_shared(inp)
    in_maps = []
    for b in range(8):
        m = dict(sh)
        m["xs"] = np.ascontiguousarray(np.concatenate([inp["ctx"][b], inp["x"][b]], axis=0).astype(np.float32))
        m["cT"] = np.ascontiguousarray(np.stack([inp["c"][b], inp["c_ctx"]], axis=-1).reshape(8, 128, 2).transpose(1, 0, 2).astype(np.float32))
        in_maps.append(m)
    res = run_bass_kernel_spmd(nc, in_maps, core_ids=list(range(8)))
    key = "dbg" if debug else "out"
    return np.stack([np.asarray(r[key]) for r in res.results], axis=0).astype(np.float32)
```

```python
import os
import numpy as np
from contextlib import ExitStack
import concourse.bass as bass
import concourse.mybir as mybir
from concourse.bass_utils import run_bass_kernel_spmd

F32 = mybir.dt.float32; BF16 = mybir.dt.bfloat16; I32 = mybir.dt.int32; U32 = mybir.dt.uint32
AF = mybir.ActivationFunctionType; ALU = mybir.AluOpType; AX = mybir.AxisListType

NTOK = 2304; NLAT = 2048; NCH = 36; NLCH = 32
ALPHA = 2.0 ** 0.25
EPS = 1e-6


class Sched:
    NDMA = 32

    def __init__(self, nc, es):
        self.nc = nc
        self.engs = {'pe': nc.tensor, 'act': nc.scalar, 'dve': nc.vector, 'pool': nc.gpsimd, 'sp': nc.sync}
        self.sem = {k: es.enter_context(nc.semaphore("sem_" + k)) for k in self.engs}
        self.cnt = {k: 0 for k in self.engs}
        self.dsem = [es.enter_context(nc.semaphore("dsem%d" % i)) for i in range(self.NDMA)]
        self.dcnt = [0] * self.NDMA
        self.dnext = 0
        self.seen = {k: {} for k in self.engs}
        self.bufs = {}

    def _deps(self, reads, writes):
        deps = []
        for r in reads:
            b = self.bufs.get(r)
            if b and b['w'] is not None:
                deps.append(b['w'])
        for w in writes:
            b = self.bufs.get(w)
            if b:
                if b['w'] is not None:
                    deps.append(b['w'])
                deps.extend(b['r'])
        return deps

    def _wait(self, eng, deps, skip_self=False):
        best = {}
        for (sid, sem, val, owner) in deps:
            if skip_self and owner == eng:
                continue
            if best.get(sid, (None, 0))[1] < val:
                best[sid] = (sem, val)
        for sid, (sem, val) in best.items():
            if self.seen[eng].get(sid, 0) >= val:
                continue
            self.engs[eng].wait_ge(sem, val)
            self.seen[eng][sid] = val

    def _record(self, dep, reads, writes):
        for r in reads:
            b = self.bufs.setdefault(r, {'w': None, 'r': []})
            b['r'] = [d for d in b['r'] if d[0] != dep[0]] + [dep]
        for w in writes:
            self.bufs[w] = {'w': dep, 'r': []}

    @staticmethod
    def _split(keys):
        norm, ps = [], []
        for k in keys:
            if k.startswith('pb') and len(k) > 2 and k[2].isdigit():
                ps.append(k[:3])
            else:
                norm.append(k)
        return norm, ps

    def op(self, eng, fn, reads=(), writes=(), skip_self=False):
        reads, pr = self._split(reads)
        writes, pw = self._split(writes)
        banks = sorted(set(pr + pw))
        deps = self._deps(reads, writes)
        deps += [d for d in self._deps((), banks) if d[3] != eng]
        self._wait(eng, deps, skip_self)
        ins = fn(self.engs[eng])
        self.cnt[eng] += 1
        ins.then_inc(self.sem[eng], 1)
        self._record(('e_' + eng, self.sem[eng], self.cnt[eng], eng), reads, list(writes) + banks)
        return ins

    def dma(self, q, fn, reads=(), writes=()):
        deps = self._deps(reads, writes)
        j = self.dnext
        self.dnext = (self.dnext + 1) % self.NDMA
        if self.dcnt[j] > 0:
            deps = deps + [('d%d' % j, self.dsem[j], self.dcnt[j], 'dma')]
        self._wait(q, deps)
        ins = fn(self.engs[q])
        self.dcnt[j] += 16
        ins.then_inc(self.dsem[j], 16)
        self._record(('d%d' % j, self.dsem[j], self.dcnt[j], 'dma'), reads, writes)

    def wait_all(self, eng, keys):
        deps = []
        for k in keys:
            b = self.bufs.get(k)
            if b:
                if b['w'] is not None:
                    deps.append(b['w'])
                deps.extend(b['r'])
        self._wait(eng, deps)

    def barrier(self):
        for e in self.engs:
            deps = [('e_' + f, self.sem[f], self.cnt[f], f) for f in self.engs if f != e and self.cnt[f] > 0]
            deps += [('d%d' % j, self.dsem[j], self.dcnt[j], 'dma') for j in range(self.NDMA) if self.dcnt[j] > 0]
            self._wait(e, deps)


def K(name, a, b=None):
    if b is None:
        return ["%s%d" % (name, a)]
    return ["%s%d" % (name, i) for i in range(a, b)]


def build(debug=None):
    nc = bass.Bass("TRN2", target_bir_lowering=False)
    D = {}

    def din(name, shape, dt=F32):
        D[name] = nc.dram_tensor(name, shape, dt, kind="ExternalInput").ap()
        return D[name]

    xs = din("xs", [NTOK, 1024]); cT_d = din("cT", [128, 8, 2]); wmod_d = din("w_mod", [128, 8, 6144])
    bmod_d = din("b_modT", [128, 48]); win_d = din("w_in", [128, 8, 4624]); lg_d = din("lgT", [128, 2, 2, 4])
    hgn_d = din("hgn", [64, 512]); mln_d = din("mln", [64, 512]); convw_d = din("convw", [128, 9, 8])
    convb_d = din("convb", [128, 8]); gateb_d = din("gateb", [8, 2]); wout_d = din("w_out", [128, 8, 1024])
    ln1g_d = din("ln1g", [128, 1024]); ln1b_d = din("ln1b", [128, 1024]); ln2g_d = din("ln2g", [128, 1024])
    ln2b_d = din("ln2b", [128, 1024]); wq_d = din("wq", [128, 8, 2048]); keysT_d = din("keysT", [128, 16, 128])
    NEXP = 128 if debug else 16384
    pu_d = din("pu", [NEXP, 1024]); pv_d = din("pv", [NEXP, 1024])
    ident_d = din("ident", [128, 128]); masks_d = din("masks", [64, 2, 64]); rmask_d = din("rmask", [128, 512])
    sel8_d = din("sel8", [8, 8]); dirm_d = din("dirm", [8, 2]); iota_d = din("iota16", [128, 16])
    out_d = nc.dram_tensor("out", [NLAT, 1024], F32, kind="ExternalOutput").ap()
    dbg_d = None
    if debug:
        dbg_d = nc.dram_tensor("dbg", [NLAT, 1024] if debug != 'mix' else [1024, NLAT], F32, kind="ExternalOutput").ap()

    with ExitStack() as es:
        S = Sched(nc, es)

        uid = [0]

        def sb(st, name, shape, dt=F32):
            uid[0] += 1
            return st.enter_context(nc.sbuf_tensor("s%d_%s" % (uid[0], name), shape, dt))

        PB = [es.enter_context(nc.psum_tensor("pb%d" % i, [128, 512], F32)) for i in range(8)]

        ident = sb(es, "ident", [128, 128]); identb = sb(es, "identb", [128, 128], BF16)
        masks = sb(es, "masks", [64, 2, 64]); rmask = sb(es, "rmask", [128, 512])
        ones = sb(es, "ones", [128, 128]); epsc = sb(es, "epsc", [128, 1])
        modv = sb(es, "modv", [128, 48, 2])
        S.dma('sp', lambda e: e.dma_start(out=ident[:], in_=ident_d[:, :]), writes=['ident'])
        S.dma('sp', lambda e: e.dma_start(out=masks[:], in_=masks_d[:, :, :]), writes=['masks'])
        S.dma('sp', lambda e: e.dma_start(out=rmask[:], in_=rmask_d[:, :]), writes=['rmask'])
        S.op('dve', lambda e: e.tensor_copy(out=identb[:], in_=ident[:]), reads=['ident'], writes=['identb'])
        S.op('dve', lambda e: e.memset(ones[:], 1.0), writes=['ones'])
        S.op('dve', lambda e: e.memset(epsc[:], EPS), writes=['epsc'])

        with ExitStack() as p0:
            cT = sb(p0, "cT", [128, 8, 2]); scT = sb(p0, "scT", [128, 8, 2]); bmodT = sb(p0, "bmodT", [128, 48])
            wm = [sb(p0, "wm%d" % i, [128, 6144]) for i in range(2)]
            S.dma('sp', lambda e: e.dma_start(out=cT[:], in_=cT_d[:, :, :]), writes=['cT'])
            S.dma('sp', lambda e: e.dma_start(out=bmodT[:], in_=bmod_d[:, :]), writes=['bmodT'])
            S.op('act', lambda e: e.activation(out=scT[:], in_=cT[:], func=AF.Silu), reads=['cT'], writes=['scT'])
            for kc in range(8):
                w = wm[kc % 2]; wk = 'wm%d' % (kc % 2)
                S.dma('sp' if kc % 2 == 0 else 'pool', lambda e, w=w, kc=kc: e.dma_start(out=w[:], in_=wmod_d[:, kc, :]), writes=[wk])
                for j in range(48):
                    S.op('pe', lambda e, w=w, kc=kc, j=j: e.matmul(PB[kc // 4][:, (kc % 4) * 96 + 2 * j:(kc % 4) * 96 + 2 * j + 2], lhsT=w[:, j * 128:(j + 1) * 128], rhs=scT[:, kc, :],
                                                                 start=True, stop=True),
                         reads=[wk, 'scT'], writes=['pb%d' % (kc // 4)], skip_self=True)
            mflat = modv[:].rearrange("p j n -> p (j n)")
            S.op('dve', lambda e: e.tensor_tensor(out=modv[:], in0=PB[0][:, 0:96].rearrange("p (j n) -> p j n", n=2), in1=bmodT[:].unsqueeze(2).to_broadcast([128, 48, 2]), op=ALU.add),
                 reads=['pb0', 'bmodT'], writes=['modv'])
            for kc in range(1, 8):
                S.op('dve', lambda e, kc=kc: e.tensor_tensor(out=mflat, in0=mflat, in1=PB[kc // 4][:, (kc % 4) * 96:(kc % 4) * 96 + 96], op=ALU.add),
                     reads=['pb%d' % (kc // 4), 'modv'], writes=['modv'])
            S.op('dve', lambda e: e.tensor_scalar_add(out=modv[:, 8:16, :], in0=modv[:, 8:16, :], scalar1=1.0), reads=['modv'], writes=['modv'])
            S.op('dve', lambda e: e.tensor_scalar_add(out=modv[:, 32:40, :], in0=modv[:, 32:40, :], scalar1=1.0), reads=['modv'], writes=['modv'])
            S.barrier()
        if debug == 'p0':
            S.dma('sp', lambda e: e.dma_start(out=dbg_d[0:128, 0:96], in_=modv[:].rearrange("p a b -> p (a b)")), reads=['modv'], writes=['dbg'])
            S.wait_all('sp', ['dbg']); S.barrier()
            return nc

        scA = ExitStack(); scB = ExitStack()
        mixT = sb(scA, "mixT", [128, 8, NLAT], BF16)
        hT = sb(scB, "hT", [128, 8, NTOK], BF16)
        wstg = sb(scB, "wstg", [128, 8, 128])
        tstg = [sb(scB, "tstg%d" % i, [128, 512]) for i in range(2)]
        tbf = [sb(scB, "tbf%d" % i, [128, 512], BF16) for i in range(2)]
        uv_d = nc.dram_tensor("uvbf", [NEXP, 2048], BF16, kind="Internal").ap()
        prep_pos = [0]

        def prep_tables(npieces):
            if debug:
                return
            for _ in range(npieces):
                i = prep_pos[0]
                if i >= 512:
                    return
                prep_pos[0] += 1
                src, coff = (pu_d, 0) if i < 256 else (pv_d, 1024)
                r = (i % 256) // 2; c = (i % 2) * 512; bb = i % 2
                S.dma('sp', lambda e, src=src, r=r, c=c, bb=bb: e.dma_start(out=tstg[bb][:], in_=src[r * 128:(r + 1) * 128, c:c + 512]), writes=['tstg%d' % bb])
                S.op('pool', lambda e, bb=bb: e.tensor_copy(out=tbf[bb][:], in_=tstg[bb][:]), reads=['tstg%d' % bb], writes=['tbf%d' % bb])
                S.dma('pool', lambda e, coff=coff, r=r, c=c, bb=bb: e.dma_start(out=uv_d[r * 128:(r + 1) * 128, coff + c:coff + c + 512], in_=tbf[bb][:]), reads=['tbf%d' % bb], writes=['tabs'])

        def layer_norm_rows(st_ap, mv_ap, rstd_ap, src, dst, skey, dkey, tag):
            S.op('dve', lambda e: e.bn_stats(out=st_ap[:, 0, :], in_=src[:, 0:512]), reads=[skey], writes=[tag + 'st'])
            S.op('dve', lambda e: e.bn_stats(out=st_ap[:, 1, :], in_=src[:, 512:1024]), reads=[skey], writes=[tag + 'st'])
            S.op('dve', lambda e: e.bn_aggr(out=mv_ap, in_=st_ap[:].rearrange("p a b -> p (a b)")), reads=[tag + 'st'], writes=[tag + 'mv'])
            S.op('act', lambda e: e.activation(out=rstd_ap, in_=mv_ap[:, 1:2], func=AF.Sqrt, bias=epsc[:, 0:1], scale=1.0),
                 reads=[tag + 'mv', 'epsc'], writes=[tag + 'rs'])
            S.op('dve', lambda e: e.reciprocal(out=rstd_ap, in_=rstd_ap), reads=[tag + 'rs'], writes=[tag + 'rs'])
            S.op('dve', lambda e: e.tensor_scalar(out=dst, in0=src, scalar1=mv_ap[:, 0:1], scalar2=rstd_ap, op0=ALU.subtract, op1=ALU.mult),
                 reads=[skey, tag + 'mv', tag + 'rs'], writes=[dkey])

        with ExitStack() as p1:
            xt = [sb(p1, "xt%d" % i, [128, 1024]) for i in range(2)]
            xn = [sb(p1, "xn%d" % i, [128, 1024]) for i in range(2)]
            st = [sb(p1, "st%d" % i, [128, 2, 6]) for i in range(2)]
            mv = [sb(p1, "mv%d" % i, [128, 2]) for i in range(2)]
            rs = [sb(p1, "rs%d" % i, [128, 1]) for i in range(2)]
            PSAP = bool(os.environ.get("KPSAP"))
            for t in range(18):
                b = t % 2
                n = 1 if t < 2 else 0
                S.dma('sp' if b == 0 else 'pool', lambda e, t=t, b=b: e.dma_start(out=xt[b][:], in_=xs[t * 128:(t + 1) * 128, :]), writes=['xt%d' % b])
                layer_norm_rows(st[b], mv[b][:], rs[b][:], xt[b][:], xn[b][:], 'xt%d' % b, 'xn%d' % b, 'p1%d' % b)
                for half in range(2):
                    bank = 2 * b + half
                    pk = 'pb%d' % bank
                    for c4 in range(4):
                        ch = half * 4 + c4
                        S.op('pe', lambda e, b=b, ch=ch, c4=c4, bank=bank: e.transpose(PB[bank][:, c4 * 128:(c4 + 1) * 128], xn[b][:, ch * 128:(ch + 1) * 128], ident[:]),
                             reads=['xn%d' % b, 'ident'], writes=[pk], skip_self=True)
                    if not PSAP:
                        S.op('act', lambda e, b=b, half=half, bank=bank: e.copy(out=xt[b][:, half * 512:(half + 1) * 512], in_=PB[bank][:, :]), reads=[pk, 'xt%d' % b], writes=['xt%d' % b])
                    for c4 in range(4):
                        ch = half * 4 + c4
                        dst = hT[:, ch, t * 128:(t + 1) * 128]
                        src = PB[bank][:, c4 * 128:(c4 + 1) * 128] if PSAP else xt[b][:, ch * 128:(ch + 1) * 128]
                        S.op('dve', lambda e, dst=dst, src=src, ch=ch, n=n: e.tensor_scalar(out=dst, in0=src, scalar1=modv[:, 8 + ch, n:n + 1], scalar2=modv[:, ch, n:n + 1],
                                                                                      op0=ALU.mult, op1=ALU.add),
                             reads=([pk] if PSAP else ['xt%d' % b]) + ['modv'], writes=K('hT', t))
            S.barrier()

        if debug == 'p1':
            with ExitStack() as dd:
                hf = sb(dd, "hf", [128, 1024])
                for t in range(16):
                    S.op('dve', lambda e, t=t: e.tensor_copy(out=hf[:].rearrange("p (a b) -> p a b", b=128), in_=hT[:, :, 256 + t * 128:256 + (t + 1) * 128]), reads=K('hT', t + 2) + ['hf'], writes=['hf'])
                    S.dma('sp', lambda e, t=t: e.dma_start(out=dbg_d[t * 128:(t + 1) * 128, :], in_=hf[:]), reads=['hf'], writes=['dbg'])
                S.wait_all('sp', ['dbg']); S.barrier()
            scB.close(); scA.close()
            return nc
        def load_w(stk, name, col0, ncols, src=None):
            src = win_d if src is None else src
            wb = sb(stk, name, [128, 8, ncols], BF16)
            S.dma('sp', lambda e: e.dma_start(out=wstg[:, :, 0:ncols], in_=src[:, :, col0:col0 + ncols]), writes=['wstg'])
            S.op('pool', lambda e: e.tensor_copy(out=wb[:], in_=wstg[:, :, 0:ncols]), reads=['wstg'], writes=[name])
            return wb

        BLKS = [(0, 512), (512, 512), (1024, 512), (1536, 512), (2048, 256)]

        def proj_fm_block(wb, wname, ncols, bank, t0, nt):
            for kc in range(8):
                S.op('pe', lambda e, kc=kc: e.matmul(PB[bank][0:ncols, 0:nt], lhsT=wb[:, kc, :], rhs=hT[:, kc, t0:t0 + nt], start=(kc == 0), stop=(kc == 7)),
                     reads=[wname] + K('hT', t0 // 128, (t0 + nt) // 128), writes=['pb%d' % bank], skip_self=True)

        def proj_tm_group(wb, wname, ncols, bank, c0, ncks):
            for j in range(ncks):
                n = c0 + j
                for kc in range(8):
                    S.op('pe', lambda e, kc=kc, j=j, n=n: e.matmul(PB[bank][0:64, j * ncols:(j + 1) * ncols], lhsT=hT[:, kc, n * 64:(n + 1) * 64], rhs=wb[:, kc, :],
                                                                   start=(kc == 0), stop=(kc == 7)),
                         reads=[wname] + K('hT', n // 2), writes=['pb%d' % bank], skip_self=True)

        S32 = sb(scB, "S32", [128, 2, 132]); Sbf = sb(scB, "Sbf", [128, 2, 132], BF16)
        stm = sb(scB, "stm", [64, 2, 64], BF16)
        usb = sb(scB, "usb", [128, 2, 132])

        def chunk_loop(dirn, KsT, QsT, Vi, QoT, Ku, Vu, dec_ap, W, evac, rk, tagk):
            order = list(range(36)) if dirn == 0 else [3, 2, 1, 0] + list(range(35, 3, -1))
            S.op('dve', lambda e: e.memset(S32[:, 0, :], 0.0), writes=['S32_0'])
            S.op('dve', lambda e: e.memset(Sbf[:, 0, :], 0.0), writes=['Sbf_0'])

            def pre(idx):
                n = order[idx]
                sl = slice(n * 64, (n + 1) * 64)
                s2 = idx % 2
                if n >= 4:
                    pst = PB[2 + s2][0:64, 0:64]; pstk = 'pb%d' % (2 + s2)
                    S.op('pe', lambda e: e.matmul(pst, lhsT=KsT[:, sl], rhs=QsT[:, sl], start=True, stop=True),
                         reads=rk, writes=[pstk], skip_self=True)
                    S.op('dve', lambda e: e.tensor_tensor(out=stm[:, s2, :], in0=pst, in1=masks[:, dirn, :], op=ALU.mult),
                         reads=[pstk, 'masks'], writes=['stm%d' % s2])
                if idx < 35:
                    pu = PB[6 + s2][:, 0:W]; puk = 'pb%d' % (6 + s2)
                    S.op('pe', lambda e: e.matmul(pu, lhsT=Ku[:, n, :], rhs=Vu[:, n, 0:W], start=True, stop=True),
                         reads=rk, writes=[puk], skip_self=True)
                    S.op('act', lambda e: e.copy(out=usb[:, s2, 0:W], in_=pu), reads=[puk], writes=['usb%d' % s2])

            pre(0)
            cur = 0
            for idx, n in enumerate(order):
                if idx + 1 < 36:
                    pre(idx + 1)
                sl = slice(n * 64, (n + 1) * 64)
                s2 = idx % 2
                if n >= 4:
                    po = PB[4 + s2][0:64, 0:W]; pok = 'pb%d' % (4 + s2)
                    S.op('pe', lambda e, po=po, s2=s2, n=n: e.matmul(po, lhsT=stm[:, s2, :], rhs=Vi[:, n, 0:W], start=True, stop=False),
                         reads=['stm%d' % s2] + rk, writes=[pok], skip_self=True)
                    S.op('pe', lambda e, po=po, sl=sl, cur=cur: e.matmul(po, lhsT=QoT[:, sl], rhs=Sbf[:, cur, 0:W], start=False, stop=True),
                         reads=['Sbf_%d' % cur] + rk, writes=[pok], skip_self=True)
                if idx < 35:
                    nxt = 1 - cur
                    S.op('dve', lambda e, n=n, cur=cur, nxt=nxt, s2=s2: e.scalar_tensor_tensor(out=Sbf[:, nxt, 0:W], in0=S32[:, cur, 0:W], scalar=dec_ap(n), in1=usb[:, s2, 0:W],
                                                                                            op0=ALU.mult, op1=ALU.add),
                         reads=['S32_%d' % cur, 'usb%d' % s2, tagk], writes=['Sbf_%d' % nxt])
                    S.op('dve', lambda e, n=n, cur=cur, nxt=nxt, s2=s2: e.scalar_tensor_tensor(out=S32[:, nxt, 0:W], in0=S32[:, cur, 0:W], scalar=dec_ap(n), in1=usb[:, s2, 0:W],
                                                                                            op0=ALU.mult, op1=ALU.add),
                         reads=['S32_%d' % cur, 'usb%d' % s2, tagk], writes=['S32_%d' % nxt])
                    cur = nxt
                if n >= 4:
                    evac(n - 4, n, po, pok)

        def transpose_chunks_to_mixT(src, skey, head):
            for g in range(4):
                bank = g % 2
                for j in range(8):
                    n = g * 8 + j
                    S.op('pe', lambda e, n=n, j=j, bank=bank: e.transpose(PB[bank][:, j * 64:(j + 1) * 64], src[:, n, :], ident[0:64, 0:64]),
                         reads=list(skey) + ['ident'], writes=['pb%d' % bank], skip_self=True)
                S.op('act', lambda e, g=g, bank=bank: e.copy(out=mixT[:, head, g * 512:(g + 1) * 512], in_=PB[bank][:, :]),
                     reads=['pb%d' % bank], writes=K('mixT', head))

        with ExitStack() as g0:
            lgT = sb(g0, "lgT", [128, 2, 2, 4]); lbT = sb(g0, "lbT", [128, 2, 4]); omlT = sb(g0, "omlT", [128, 2, 4]); nomlT = sb(g0, "nomlT", [128, 2, 4])
            hgn = sb(g0, "hgn", [64, 512])
            S.dma('sp', lambda e: e.dma_start(out=lgT[:], in_=lg_d[:, :, :, :]), writes=['lgT'])
            S.dma('sp', lambda e: e.dma_start(out=hgn[:], in_=hgn_d[:, :]), writes=['hgn'])
            S.op('dve', lambda e: e.tensor_tensor(out=lbT[:], in0=lgT[:, :, 0, :], in1=lgT[:, :, 1, :], op=ALU.subtract), reads=['lgT'], writes=['lbT'])
            S.op('act', lambda e: e.activation(out=lbT[:], in_=lbT[:], func=AF.Sigmoid), reads=['lbT'], writes=['lbT'])
            S.op('dve', lambda e: e.tensor_scalar(out=omlT[:], in0=lbT[:], scalar1=-1.0, scalar2=1.0, op0=ALU.mult, op1=ALU.add), reads=['lbT'], writes=['omlT'])
            S.op('dve', lambda e: e.tensor_scalar_mul(out=nomlT[:], in0=omlT[:], scalar1=-1.0), reads=['omlT'], writes=['nomlT'])
            QsT = [sb(g0, "gQsT%d" % d, [128, NTOK], BF16) for d in range(2)]
            KsT = [sb(g0, "gKsT%d" % d, [128, NTOK], BF16) for d in range(2)]
            QoT = [sb(g0, "gQoT%d" % d, [128, NTOK], BF16) for d in range(2)]
            Ku = [sb(g0, "gKu%d" % d, [64, NCH, 128], BF16) for d in range(2)]
            dec = sb(g0, "gdec", [128, 2, NCH])
            vtm = sb(g0, "gv", [64, NCH, 128], BF16); gs = sb(g0, "ggs", [64, NLCH, 128], BF16); oacc = sb(g0, "goacc", [64, NLCH, 128])
            qf = sb(g0, "gqf", [128, 512]); sg = sb(g0, "gsg", [128, 512]); lf = sb(g0, "glf", [128, 512]); key = sb(g0, "gkey", [128, 512])
            Bc = sb(g0, "gB", [128, 512]); T1 = sb(g0, "gT1", [128, 512]); T2 = sb(g0, "gT2", [128, 512]); khT = sb(g0, "gkhT", [128, 512], BF16)
            EE = [sb(g0, "gE%d" % i, [128, 512]) for i in range(4)]
            sq = sb(g0, "gsq", [64, NLCH, 128], BF16); ss = sb(g0, "gss", [64, NLCH])
            for hd in range(4):
                with ExitStack() as hs:
                    wq_ = load_w(hs, "gwq", 0 + hd * 128, 128); wi_ = load_w(hs, "gwi", 512 + hd * 128, 128); wg_ = load_w(hs, "gwg", 1024 + hd * 128, 128)
                    wf = [load_w(hs, "gwf0", 1536 + hd * 128, 128), load_w(hs, "gwf1", 2048 + hd * 128, 128)]
                    prep_tables(64)
                    for g in range(9):
                        bk = 3 + g % 2
                        proj_tm_group(wi_, "gwi", 128, bk, g * 4, 4)
                        S.op('act', lambda e, g=g, bk=bk: e.copy(out=vtm[:, g * 4:(g + 1) * 4, :], in_=PB[bk][0:64, :].rearrange("p (j c) -> p j c", c=128)),
                             reads=['pb%d' % bk], writes=['gv'])
                    for g in range(8):
                        bk = 3 + (g + 1) % 2
                        proj_tm_group(wg_, "gwg", 128, bk, 4 + g * 4, 4)
                        S.op('act', lambda e, g=g, bk=bk: e.activation(out=gs[:, g * 4:(g + 1) * 4, :], in_=PB[bk][0:64, :].rearrange("p (j c) -> p j c", c=128), func=AF.Silu),
                             reads=['pb%d' % bk], writes=['ggs'])
                    for (t0, nt) in BLKS:
                        nck = nt // 64; c0 = t0 // 64
                        proj_fm_block(wq_, "gwq", 128, 0, t0, nt)
                        S.op('act', lambda e, nt=nt: e.copy(out=qf[:, 0:nt], in_=PB[0][:, 0:nt]), reads=['pb0'], writes=['gqf'])
                        for d in range(2):
                            proj_fm_block(wf[d], "gwf%d" % d, 128, 1 + d, t0, nt)
                            col = d * 4 + hd
                            lbp = lbT[:, d, hd:hd + 1]; omp = omlT[:, d, hd:hd + 1]; nomp = nomlT[:, d, hd:hd + 1]
                            S.op('act', lambda e, d=d, nt=nt: e.activation(out=sg[:, 0:nt], in_=PB[1 + d][:, 0:nt], func=AF.Sigmoid), reads=['pb%d' % (1 + d)], writes=['gsg'])
                            S.op('act', lambda e, nt=nt, lbp=lbp, omp=omp: e.activation(out=lf[:, 0:nt], in_=sg[:, 0:nt], func=AF.Ln, bias=lbp, scale=omp),
                                 reads=['gsg', 'lbT', 'omlT'], writes=['glf'])
                            S.op('dve', lambda e, nt=nt, nomp=nomp, omp=omp: e.tensor_scalar(out=key[:, 0:nt], in0=sg[:, 0:nt], scalar1=nomp, scalar2=omp, op0=ALU.mult, op1=ALU.add),
                                 reads=['gsg', 'omlT', 'nomlT'], writes=['gkey'])
                            S.op('dve', lambda e, nt=nt: e.tensor_tensor_scan(out=Bc[:, 0:nt], data0=rmask[:, 0:nt], data1=lf[:, 0:nt], initial=0.0, op0=ALU.mult, op1=ALU.add),
                                 reads=['glf', 'rmask'], writes=['gB'])
                            B3 = Bc[:, 0:nt].rearrange("p (n c) -> p n c", c=64)
                            T3 = T1[:, 0:nt].rearrange("p (n c) -> p n c", c=64)
                            if d == 1:
                                S.op('dve', lambda e, B3=B3, T3=T3, nck=nck: e.tensor_tensor(out=T3, in0=B3[:, :, 63:64].to_broadcast([128, nck, 64]), in1=B3, op=ALU.subtract),
                                     reads=['gB'], writes=['gT1'])
                                S.op('dve', lambda e, nt=nt: e.tensor_tensor(out=Bc[:, 0:nt], in0=T1[:, 0:nt], in1=lf[:, 0:nt], op=ALU.add), reads=['gT1', 'glf'], writes=['gB'])
                            li = 63 if d == 0 else 0
                            S.op('act', lambda e, B3=B3, li=li, d=d, c0=c0, nck=nck: e.activation(out=dec[:, d, c0:c0 + nck], in_=B3[:, :, li], func=AF.Exp), reads=['gB'], writes=['gdec'])
                            T4 = T2[:, 0:nt].rearrange("p (n c) -> p n c", c=64)
                            S.op('dve', lambda e, B3=B3, T3=T3, nck=nck: e.tensor_tensor(out=T3, in0=B3, in1=B3[:, :, 32:33].to_broadcast([128, nck, 64]), op=ALU.subtract),
                                 reads=['gB'], writes=['gT1'])
                            S.op('dve', lambda e, B3=B3, T4=T4, nck=nck, li=li: e.tensor_tensor(out=T4, in0=B3[:, :, li:li + 1].to_broadcast([128, nck, 64]), in1=B3, op=ALU.subtract),
                                 reads=['gB'], writes=['gT2'])
                            S.op('act', lambda e, nt=nt: e.activation(out=EE[0][:, 0:nt], in_=T1[:, 0:nt], func=AF.Exp), reads=['gT1'], writes=['gE0'])
                            S.op('act', lambda e, nt=nt: e.activation(out=EE[1][:, 0:nt], in_=T1[:, 0:nt], func=AF.Exp, scale=-1.0), reads=['gT1'], writes=['gE1'])
                            S.op('act', lambda e, nt=nt: e.activation(out=EE[2][:, 0:nt], in_=Bc[:, 0:nt], func=AF.Exp), reads=['gB'], writes=['gE2'])
                            S.op('act', lambda e, nt=nt: e.activation(out=EE[3][:, 0:nt], in_=T2[:, 0:nt], func=AF.Exp), reads=['gT2'], writes=['gE3'])
                            S.op('dve', lambda e, nt=nt, t0=t0, d=d: e.tensor_tensor(out=QsT[d][:, t0:t0 + nt], in0=qf[:, 0:nt], in1=EE[0][:, 0:nt], op=ALU.mult),
                                 reads=['gqf', 'gE0'], writes=['gQsT%d' % d])
                            S.op('dve', lambda e, nt=nt, t0=t0, d=d: e.tensor_tensor(out=KsT[d][:, t0:t0 + nt], in0=key[:, 0:nt], in1=EE[1][:, 0:nt], op=ALU.mult),
                                 reads=['gkey', 'gE1'], writes=['gKsT%d' % d])
                            S.op('dve', lambda e, nt=nt, t0=t0, d=d: e.tensor_tensor(out=QoT[d][:, t0:t0 + nt], in0=qf[:, 0:nt], in1=EE[2][:, 0:nt], op=ALU.mult),
                                 reads=['gqf', 'gE2'], writes=['gQoT%d' % d])
                            S.op('dve', lambda e, nt=nt: e.tensor_tensor(out=khT[:, 0:nt], in0=key[:, 0:nt], in1=EE[3][:, 0:nt], op=ALU.mult), reads=['gkey', 'gE3'], writes=['gkhT'])
                            pbb = PB[3][:].bitcast(BF16)
                            for j in range(nck):
                                S.op('pe', lambda e, j=j: e.transpose(pbb[0:64, j * 128:(j + 1) * 128], khT[:, j * 64:(j + 1) * 64], identb[:]),
                                     reads=['gkhT', 'identb'], writes=['pb3'], skip_self=True)
                            S.op('act', lambda e, d=d, c0=c0, nck=nck: e.copy(out=Ku[d][:, c0:c0 + nck, :], in_=pbb[0:64, 0:nck * 128].rearrange("p (j c) -> p j c", c=128)),
                                 reads=['pb3'], writes=['gKu%d' % d])
                    for d in range(2):
                        def evac(nl, n, po, pok, d=d):
                            if d == 0:
                                S.op('act', lambda e: e.copy(out=oacc[:, nl, :], in_=po), reads=[pok], writes=K('goacc', nl))
                            else:
                                S.op('dve', lambda e: e.tensor_tensor(out=oacc[:, nl, :], in0=po, in1=oacc[:, nl, :], op=ALU.add), reads=[pok] + K('goacc', nl), writes=K('goacc', nl))
                        chunk_loop(d, KsT[d], QsT[d], vtm, QoT[d], Ku[d], vtm, lambda n, d=d: dec[:, d, n:n + 1], 128, evac,
                                   ['gQsT%d' % d, 'gKsT%d' % d, 'gQoT%d' % d, 'gKu%d' % d, 'gv'], 'gdec')
                    allo = K('goacc', 0, NLCH)
                    S.op('dve', lambda e: e.tensor_tensor(out=sq[:], in0=oacc[:], in1=oacc[:], op=ALU.mult), reads=allo, writes=['gsq'])
                    S.op('dve', lambda e: e.tensor_reduce(out=ss[:], in_=sq[:], axis=AX.X, op=ALU.add), reads=['gsq'], writes=['gss'])
                    S.op('act', lambda e: e.activation(out=ss[:], in_=ss[:], func=AF.Sqrt, bias=epsc[0:64, 0:1], scale=1.0 / 128.0), reads=['gss', 'epsc'], writes=['gss'])
                    S.op('dve', lambda e: e.reciprocal(out=ss[:], in_=ss[:]), reads=['gss'], writes=['gss'])
                    S.op('dve', lambda e: e.tensor_tensor(out=oacc[:], in0=oacc[:], in1=ss[:].unsqueeze(2).to_broadcast([64, NLCH, 128]), op=ALU.mult),
                         reads=allo + ['gss'], writes=allo)
                    S.op('dve', lambda e, hd=hd: e.tensor_tensor(out=oacc[:], in0=oacc[:], in1=hgn[:, hd * 128:(hd + 1) * 128].unsqueeze(1).to_broadcast([64, NLCH, 128]), op=ALU.mult),
                         reads=allo + ['hgn'], writes=allo)
                    S.op('dve', lambda e: e.tensor_tensor(out=oacc[:], in0=oacc[:], in1=gs[:], op=ALU.mult), reads=allo + ['ggs'], writes=allo)
                    transpose_chunks_to_mixT(oacc, allo, hd)
            S.barrier()

        with ExitStack() as m0:
            mln = sb(m0, "mln", [64, 512]); convw = sb(m0, "convw", [128, 9, 8]); convb = sb(m0, "convb", [128, 8])
            gateb = sb(m0, "gateb", [8, 2]); sel8 = sb(m0, "sel8", [8, 8]); dirm = sb(m0, "dirm", [8, 2])
            S.dma('sp', lambda e: e.dma_start(out=mln[:], in_=mln_d[:, :]), writes=['mln'])
            S.dma('sp', lambda e: e.dma_start(out=convw[:], in_=convw_d[:, :, :]), writes=['convw'])
            S.dma('sp', lambda e: e.dma_start(out=convb[:], in_=convb_d[:, :]), writes=['convb'])
            S.dma('sp', lambda e: e.dma_start(out=gateb[:], in_=gateb_d[:, :]), writes=['gateb'])
            S.dma('sp', lambda e: e.dma_start(out=sel8[:], in_=sel8_d[:, :]), writes=['sel8'])
            S.dma('sp', lambda e: e.dma_start(out=dirm[:], in_=dirm_d[:, :]), writes=['dirm'])
            RUU = sb(m0, "RUU", [64, NCH, 24]); dchunk = sb(m0, "dchunk", [128, 8, NCH])
            with ExitStack() as gp:
                wgi = load_w(gp, "mwgi", 4608, 8); wgf = load_w(gp, "mwgf", 4616, 8)
                LI = sb(gp, "LI", [8, NTOK]); LF = sb(gp, "LF", [8, NTOK]); Af = sb(gp, "Af", [8, NTOK]); Ab = sb(gp, "Ab", [8, NTOK]); Aa = sb(gp, "Aa", [8, NTOK])
                R = [sb(gp, "Rr%d" % i, [8, NTOK]) for i in range(3)]
                bd = sb(gp, "bd", [8, 8, NCH])
                for (t0, nt) in BLKS:
                    proj_fm_block(wgi, "mwgi", 8, 0, t0, nt)
                    S.op('act', lambda e, t0=t0, nt=nt: e.copy(out=LI[:, t0:t0 + nt], in_=PB[0][0:8, 0:nt]), reads=['pb0'], writes=['LI'])
                    proj_fm_block(wgf, "mwgf", 8, 1, t0, nt)
                    S.op('act', lambda e, t0=t0, nt=nt: e.copy(out=LF[:, t0:t0 + nt], in_=PB[1][0:8, 0:nt]), reads=['pb1'], writes=['LF'])
                S.op('dve', lambda e: e.tensor_scalar_add(out=LI[:], in0=LI[:], scalar1=gateb[:, 0:1]), reads=['LI', 'gateb'], writes=['LI'])
                S.op('act', lambda e: e.activation(out=LF[:], in_=LF[:], func=AF.Sigmoid, bias=gateb[:, 1:2], scale=1.0), reads=['LF', 'gateb'], writes=['LF'])
                S.op('act', lambda e: e.activation(out=LF[:], in_=LF[:], func=AF.Ln), reads=['LF'], writes=['LF'])
                for (t0, nt) in BLKS:
                    S.op('dve', lambda e, t0=t0, nt=nt: e.tensor_tensor_scan(out=Af[:, t0:t0 + nt], data0=rmask[0:8, 0:nt], data1=LF[:, t0:t0 + nt], initial=0.0, op0=ALU.mult, op1=ALU.add),
                         reads=['LF', 'rmask'], writes=['Af'])
                A3 = Af[:].rearrange("p (n c) -> p n c", c=64)
                tot = A3[:, :, 63:64]
                S.op('dve', lambda e: e.tensor_tensor(out=Ab[:].rearrange("p (n c) -> p n c", c=64), in0=tot.to_broadcast([8, NCH, 64]), in1=A3, op=ALU.subtract), reads=['Af'], writes=['Ab'])
                S.op('dve', lambda e: e.tensor_tensor(out=Ab[:], in0=Ab[:], in1=LF[:], op=ALU.add), reads=['Ab', 'LF'], writes=['Ab'])
                S.op('dve', lambda e: e.tensor_scalar_mul(out=Aa[:], in0=Af[:], scalar1=dirm[:, 0:1]), reads=['Af', 'dirm'], writes=['Aa'])
                S.op('dve', lambda e: e.scalar_tensor_tensor(out=Aa[:], in0=Ab[:], scalar=dirm[:, 1:2], in1=Aa[:], op0=ALU.mult, op1=ALU.add), reads=['Ab', 'dirm', 'Aa'], writes=['Aa'])
                S.op('act', lambda e: e.activation(out=R[0][:], in_=Aa[:], func=AF.Exp), reads=['Aa'], writes=['Rr0'])
                S.op('dve', lambda e: e.tensor_tensor(out=Ab[:], in0=LI[:], in1=Aa[:], op=ALU.subtract), reads=['LI', 'Aa', 'Ab'], writes=['Ab'])
                S.op('act', lambda e: e.activation(out=R[1][:], in_=Ab[:], func=AF.Exp), reads=['Ab'], writes=['Rr1'])
                S.op('dve', lambda e: e.tensor_tensor(out=Aa[:].rearrange("p (n c) -> p n c", c=64), in0=Ab[:].rearrange("p (n c) -> p n c", c=64), in1=tot.to_broadcast([8, NCH, 64]), op=ALU.add),
                     reads=['Ab', 'Af', 'Aa', 'Rr0'], writes=['Aa'])
                S.op('act', lambda e: e.activation(out=R[2][:], in_=Aa[:], func=AF.Exp), reads=['Aa'], writes=['Rr2'])
                for half in range(2):
                    for j in range(18):
                        n = half * 18 + j
                        for q in range(3):
                            S.op('pe', lambda e, n=n, j=j, q=q: e.transpose(PB[2][0:64, j * 24 + q * 8:j * 24 + q * 8 + 8], R[q][:, n * 64:(n + 1) * 64], ident[0:8, 0:8]),
                                 reads=['Rr%d' % q, 'ident'], writes=['pb2'], skip_self=True)
                    S.op('act', lambda e, half=half: e.copy(out=RUU[:, half * 18:(half + 1) * 18, :], in_=PB[2][0:64, 0:432].rearrange("p (j c) -> p j c", c=24)),
                         reads=['pb2'], writes=['RUU'])
                S.op('dve', lambda e: e.tensor_tensor(out=bd[:], in0=tot.rearrange("p n c -> p c n").to_broadcast([8, 8, NCH]), in1=sel8[:].unsqueeze(2).to_broadcast([8, 8, NCH]), op=ALU.mult),
                     reads=['Af', 'sel8'], writes=['bd'])
                S.op('pe', lambda e: e.matmul(PB[3][:, 0:288], lhsT=ones[0:8, :], rhs=bd[:].rearrange("p a n -> p (a n)"), start=True, stop=True), reads=['ones', 'bd'], writes=['pb3'], skip_self=True)
                S.op('act', lambda e: e.activation(out=dchunk[:].rearrange("p a n -> p (a n)"), in_=PB[3][:, 0:288], func=AF.Exp), reads=['pb3'], writes=['dchunk'])
                S.barrier()
            qc = sb(m0, "mqc", [128, NTOK], BF16); kc_ = sb(m0, "mkc", [128, NTOK], BF16); kTM = sb(m0, "mkTM", [64, NCH, 128], BF16)
            raw = sb(m0, "mraw", [128, NTOK]); acc = sb(m0, "macc", [128, NTOK])
            vext = sb(m0, "mvext", [64, NCH, 132], BF16); vi = sb(m0, "mvi", [64, NCH, 132], BF16); vu = sb(m0, "mvu", [64, NCH, 132], BF16)
            og = sb(m0, "mog", [64, NLCH, 128], BF16); oext = sb(m0, "moext", [64, NLCH, 132]); obuf = sb(m0, "mobuf", [64, NLCH, 128])
            den = sb(m0, "mden", [64, NLCH]); mu = sb(m0, "mmu", [64, NLCH]); m2 = sb(m0, "mm2", [64, NLCH])
            S.op('dve', lambda e: e.memset(vext[:], 1.0), writes=['mvext'])
            for hd in range(4):
                with ExitStack() as hs:
                    wq_ = load_w(hs, "mwq", 2560 + hd * 128, 128); wk_ = load_w(hs, "mwk", 3072 + hd * 128, 128)
                    wv_ = load_w(hs, "mwv", 3584 + hd * 128, 128); wo_ = load_w(hs, "mwo", 4096 + hd * 128, 128)
                    prep_tables(64)
                    for g in range(9):
                        bk = 3 + g % 2
                        proj_tm_group(wv_, "mwv", 128, bk, g * 4, 4)
                        S.op('act', lambda e, g=g, bk=bk: e.copy(out=vext[:, g * 4:(g + 1) * 4, 0:128], in_=PB[bk][0:64, :].rearrange("p (j c) -> p j c", c=128)),
                             reads=['pb%d' % bk], writes=['mvext'])
                    for g in range(8):
                        bk = 3 + (g + 1) % 2
                        proj_tm_group(wo_, "mwo", 128, bk, 4 + g * 4, 4)
                        S.op('act', lambda e, g=g, bk=bk: e.activation(out=og[:, g * 4:(g + 1) * 4, :], in_=PB[bk][0:64, :].rearrange("p (j c) -> p j c", c=128), func=AF.Sigmoid),
                             reads=['pb%d' % bk], writes=['mog'])
                    for qi, (wb, wn, dstb) in enumerate(((wq_, "mwq", qc), (wk_, "mwk", kc_))):
                        chn = qi * 4 + hd
                        for bi, (t0, nt) in enumerate(BLKS):
                            proj_fm_block(wb, wn, 128, bi % 2, t0, nt)
                            S.op('act', lambda e, t0=t0, nt=nt, bi=bi: e.copy(out=raw[:, t0:t0 + nt], in_=PB[bi % 2][:, 0:nt]), reads=['pb%d' % (bi % 2)], writes=['mraw'])
                        S.op('dve', lambda e, chn=chn: e.tensor_scalar(out=acc[:, 0:256], in0=raw[:, 0:256], scalar1=convw[:, 4, chn:chn + 1], scalar2=convb[:, chn:chn + 1], op0=ALU.mult, op1=ALU.add),
                             reads=['mraw', 'convw', 'convb'], writes=['macc'])
                        S.op('dve', lambda e, chn=chn: e.scalar_tensor_tensor(out=acc[:, 1:256], in0=raw[:, 0:255], scalar=convw[:, 3, chn:chn + 1], in1=acc[:, 1:256], op0=ALU.mult, op1=ALU.add),
                             reads=['mraw', 'convw', 'macc'], writes=['macc'])
                        S.op('dve', lambda e, chn=chn: e.scalar_tensor_tensor(out=acc[:, 0:255], in0=raw[:, 1:256], scalar=convw[:, 5, chn:chn + 1], in1=acc[:, 0:255], op0=ALU.mult, op1=ALU.add),
                             reads=['mraw', 'convw', 'macc'], writes=['macc'])
                        X = raw[:, 256:NTOK].rearrange("p (r c) -> p r c", c=64); Y = acc[:, 256:NTOK].rearrange("p (r c) -> p r c", c=64)
                        S.op('dve', lambda e, chn=chn: e.tensor_scalar(out=acc[:, 256:NTOK], in0=raw[:, 256:NTOK], scalar1=convw[:, 4, chn:chn + 1], scalar2=convb[:, chn:chn + 1], op0=ALU.mult, op1=ALU.add),
                             reads=['mraw', 'convw', 'convb', 'macc'], writes=['macc'])
                        for ky in range(3):
                            for kx in range(3):
                                if ky == 1 and kx == 1:
                                    continue
                                dy = ky - 1; dx = kx - 1
                                r0 = max(0, -dy); r1 = 32 - max(0, dy); c0 = max(0, -dx); c1 = 64 - max(0, dx)
                                S.op('dve', lambda e, chn=chn, ky=ky, kx=kx, r0=r0, r1=r1, c0=c0, c1=c1, dy=dy, dx=dx: e.scalar_tensor_tensor(
                                    out=Y[:, r0:r1, c0:c1], in0=X[:, r0 + dy:r1 + dy, c0 + dx:c1 + dx], scalar=convw[:, ky * 3 + kx, chn:chn + 1], in1=Y[:, r0:r1, c0:c1],
                                    op0=ALU.mult, op1=ALU.add), reads=['mraw', 'convw', 'macc'], writes=['macc'])
                        S.op('act', lambda e: e.activation(out=acc[:], in_=acc[:], func=AF.Silu), reads=['macc'], writes=['macc'])
                        if qi == 0:
                            S.op('dve', lambda e: e.tensor_copy(out=qc[:], in_=acc[:]), reads=['macc'], writes=['mqc'])
                        else:
                            S.op('dve', lambda e: e.tensor_scalar_mul(out=kc_[:], in0=acc[:], scalar1=128.0 ** -0.5), reads=['macc'], writes=['mkc'])
                    pbb = PB[3][:].bitcast(BF16)
                    for g in range(5):
                        nck = 8 if g < 4 else 4
                        for j in range(nck):
                            n = g * 8 + j
                            S.op('pe', lambda e, j=j, n=n: e.transpose(pbb[0:64, j * 128:(j + 1) * 128], kc_[:, n * 64:(n + 1) * 64], identb[:]),
                                 reads=['mkc', 'identb'], writes=['pb3'], skip_self=True)
                        S.op('act', lambda e, g=g, nck=nck: e.copy(out=kTM[:, g * 8:g * 8 + nck, :], in_=pbb[0:64, 0:nck * 128].rearrange("p (j c) -> p j c", c=128)),
                             reads=['pb3'], writes=['mkTM'])
                    for d in range(2):
                        row = d * 4 + hd
                        S.op('dve', lambda e, row=row: e.tensor_tensor(out=vi[:], in0=vext[:], in1=RUU[:, :, 8 + row:9 + row].to_broadcast([64, NCH, 132]), op=ALU.mult),
                             reads=['mvext', 'RUU'], writes=['mvi'])
                        S.op('dve', lambda e, row=row: e.tensor_tensor(out=vu[:], in0=vext[:], in1=RUU[:, :, 16 + row:17 + row].to_broadcast([64, NCH, 132]), op=ALU.mult),
                             reads=['mvext', 'RUU'], writes=['mvu'])

                        def evac(nl, n, po, pok, row=row):
                            S.op('act', lambda e: e.copy(out=oext[:, nl, 0:129], in_=po), reads=[pok], writes=K('moext', nl))
                        chunk_loop(d, kc_, qc, vi, qc, kTM, vu, lambda n, row=row: dchunk[:, row, n:n + 1], 129, evac,
                                   ['mqc', 'mkc', 'mvi', 'mvu', 'mkTM'], 'dchunk')
                        allx = K('moext', 0, NLCH)
                        S.op('dve', lambda e, row=row: e.tensor_tensor(out=oext[:, :, 0:129], in0=oext[:, :, 0:129], in1=RUU[:, 4:NCH, row:row + 1].to_broadcast([64, NLCH, 129]), op=ALU.mult),
                             reads=allx + ['RUU'], writes=allx)
                        S.op('act', lambda e: e.activation(out=den[:], in_=oext[:, :, 128], func=AF.Abs), reads=allx, writes=['mden'])
                        S.op('dve', lambda e: e.tensor_scalar_max(out=den[:], in0=den[:], scalar1=1.0), reads=['mden'], writes=['mden'])
                        S.op('dve', lambda e: e.reciprocal(out=den[:], in_=den[:]), reads=['mden'], writes=['mden'])
                        if d == 0:
                            S.op('dve', lambda e: e.tensor_tensor(out=obuf[:], in0=oext[:, :, 0:128], in1=den[:].unsqueeze(2).to_broadcast([64, NLCH, 128]), op=ALU.mult),
                                 reads=allx + ['mden'], writes=['mobuf'])
                        else:
                            S.op('dve', lambda e: e.tensor_tensor(out=oext[:, :, 0:128], in0=oext[:, :, 0:128], in1=den[:].unsqueeze(2).to_broadcast([64, NLCH, 128]), op=ALU.mult),
                                 reads=allx + ['mden'], writes=allx)
                            S.op('dve', lambda e: e.tensor_tensor(out=obuf[:], in0=obuf[:], in1=oext[:, :, 0:128], op=ALU.add), reads=allx + ['mobuf'], writes=['mobuf'])
                    allx = K('moext', 0, NLCH)
                    S.op('dve', lambda e: e.tensor_reduce(out=mu[:], in_=obuf[:], axis=AX.X, op=ALU.add), reads=['mobuf'], writes=['mmu'])
                    S.op('dve', lambda e: e.tensor_scalar_mul(out=mu[:], in0=mu[:], scalar1=1.0 / 128.0), reads=['mmu'], writes=['mmu'])
                    S.op('dve', lambda e: e.tensor_tensor(out=obuf[:], in0=obuf[:], in1=mu[:].unsqueeze(2).to_broadcast([64, NLCH, 128]), op=ALU.subtract), reads=['mobuf', 'mmu'], writes=['mobuf'])
                    S.op('dve', lambda e: e.tensor_tensor(out=oext[:, :, 0:128], in0=obuf[:], in1=obuf[:], op=ALU.mult), reads=['mobuf'] + allx, writes=allx)
                    S.op('dve', lambda e: e.tensor_reduce(out=m2[:], in_=oext[:, :, 0:128], axis=AX.X, op=ALU.add), reads=allx, writes=['mm2'])
                    S.op('act', lambda e: e.activation(out=m2[:], in_=m2[:], func=AF.Sqrt, bias=epsc[0:64, 0:1], scale=1.0 / 128.0), reads=['mm2', 'epsc'], writes=['mm2'])
                    S.op('dve', lambda e: e.reciprocal(out=m2[:], in_=m2[:]), reads=['mm2'], writes=['mm2'])
                    S.op('dve', lambda e: e.tensor_tensor(out=obuf[:], in0=obuf[:], in1=m2[:].unsqueeze(2).to_broadcast([64, NLCH, 128]), op=ALU.mult), reads=['mobuf', 'mm2'], writes=['mobuf'])
                    S.op('dve', lambda e, hd=hd: e.tensor_tensor(out=obuf[:], in0=obuf[:], in1=mln[:, hd * 128:(hd + 1) * 128].unsqueeze(1).to_broadcast([64, NLCH, 128]), op=ALU.mult),
                         reads=['mobuf', 'mln'], writes=['mobuf'])
                    S.op('dve', lambda e: e.tensor_tensor(out=obuf[:], in0=obuf[:], in1=og[:], op=ALU.mult), reads=['mobuf', 'mog'], writes=['mobuf'])
                    transpose_chunks_to_mixT(obuf, ['mobuf'], 4 + hd)
            S.barrier()

        if debug == 'mix':
            with ExitStack() as dd:
                mf = sb(dd, "mf", [128, NLAT])
                for h in range(8):
                    S.op('dve', lambda e, h=h: e.tensor_copy(out=mf[:], in_=mixT[:, h, :]), reads=K('mixT', h) + ['mf'], writes=['mf'])
                    S.dma('sp', lambda e, h=h: e.dma_start(out=dbg_d[h * 128:(h + 1) * 128, :], in_=mf[:]), reads=['mf'], writes=['dbg'])
                S.wait_all('sp', ['dbg']); S.barrier()
            scB.close(); scA.close()
            return nc
        prep_tables(512)
        S.barrier()
        scB.close()
        x1s = nc.dram_tensor("x1s", [NLAT, 1024], F32, kind="Internal").ap()

        def bcast_tile(dst, dkey, c0, dg):
            for ch in range(8):
                S.op('dve', lambda e, ch=ch: e.tensor_scalar_mul(out=dg[:], in0=ident[:], scalar1=modv[:, c0 + ch, 0:1]), reads=['ident', 'modv', 'dg'], writes=['dg'])
                bank = ch // 4
                S.op('pe', lambda e, ch=ch, bank=bank: e.matmul(PB[bank][:, (ch % 4) * 128:(ch % 4 + 1) * 128], lhsT=ones[:], rhs=dg[:], start=True, stop=True),
                     reads=['ones', 'dg'], writes=['pb%d' % bank], skip_self=True)
            S.op('act', lambda e: e.copy(out=dst[:, 0:512], in_=PB[0][:, :]), reads=['pb0'], writes=[dkey])
            S.op('act', lambda e: e.copy(out=dst[:, 512:1024], in_=PB[1][:, :]), reads=['pb1'], writes=[dkey])

        with ExitStack() as p3:
            bct = {}
            for nm in ("g1b", "ln1g", "ln1b"):
                bct[nm] = sb(p3, nm, [128, 1024])
            for nm, dd in (("ln1g", ln1g_d), ("ln1b", ln1b_d)):
                S.dma('sp', lambda e, nm=nm, dd=dd: e.dma_start(out=bct[nm][:], in_=dd[:, :]), writes=[nm])
            dg = sb(p3, "dg", [128, 128])
            bcast_tile(bct["g1b"], "g1b", 16, dg)
            wo_b = sb(p3, "wout", [128, 8, 1024], BF16); wos = sb(p3, "wos", [128, 1024])
            for kc in range(8):
                S.dma('sp', lambda e, kc=kc: e.dma_start(out=wos[:], in_=wout_d[:, kc, :]), writes=['wos'])
                S.op('pool', lambda e, kc=kc: e.tensor_copy(out=wo_b[:, kc, :], in_=wos[:]), reads=['wos'], writes=['wout'])
            xt = [sb(p3, "x3t%d" % i, [128, 1024]) for i in range(2)]
            t1 = [sb(p3, "t1_%d" % i, [128, 1024]) for i in range(2)]
            st = [sb(p3, "st3%d" % i, [128, 2, 6]) for i in range(2)]
            mv = [sb(p3, "mv3%d" % i, [128, 2]) for i in range(2)]
            rs = [sb(p3, "rs3%d" % i, [128, 1]) for i in range(2)]
            for t in range(16):
                b = t % 2
                S.dma('sp' if b == 0 else 'pool', lambda e, t=t, b=b: e.dma_start(out=xt[b][:], in_=xs[256 + t * 128:256 + (t + 1) * 128, :]), writes=['x3t%d' % b])
                for half in range(2):
                    bank = 2 * b + half
                    for kc in range(8):
                        S.op('pe', lambda e, kc=kc, t=t, half=half, bank=bank: e.matmul(PB[bank][:, :], lhsT=mixT[:, kc, t * 128:(t + 1) * 128], rhs=wo_b[:, kc, half * 512:(half + 1) * 512],
                                                                                      start=(kc == 0), stop=(kc == 7)),
                             reads=K('mixT', kc) + ['wout'], writes=['pb%d' % bank], skip_self=True)
                    S.op('dve', lambda e, b=b, half=half, bank=bank: e.tensor_tensor(out=t1[b][:, half * 512:(half + 1) * 512], in0=PB[bank][:, :], in1=bct["g1b"][:, half * 512:(half + 1) * 512], op=ALU.mult),
                         reads=['pb%d' % bank, 'g1b'], writes=['t1_%d' % b])
                S.op('dve', lambda e, b=b: e.scalar_tensor_tensor(out=t1[b][:], in0=xt[b][:], scalar=ALPHA, in1=t1[b][:], op0=ALU.mult, op1=ALU.add),
                     reads=['x3t%d' % b, 't1_%d' % b], writes=['t1_%d' % b])
                layer_norm_rows(st[b], mv[b][:], rs[b][:], t1[b][:], t1[b][:], 't1_%d' % b, 't1_%d' % b, 'p3%d' % b)
                S.op('dve', lambda e, b=b: e.tensor_tensor(out=t1[b][:], in0=t1[b][:], in1=bct["ln1g"][:], op=ALU.mult), reads=['t1_%d' % b, 'ln1g'], writes=['t1_%d' % b])
                S.op('dve', lambda e, b=b, t=t: e.tensor_tensor(out=t1[b][:], in0=t1[b][:], in1=bct["ln1b"][:], op=ALU.add), reads=['t1_%d' % b, 'ln1b'], writes=['t1_%d' % b])
                S.dma('sp', lambda e, t=t, b=b: e.dma_start(out=x1s[t * 128:(t + 1) * 128, :], in_=t1[b][:]), reads=['t1_%d' % b], writes=K('x1_', t))
                if debug == 'x1':
                    S.dma('sp', lambda e, t=t, b=b: e.dma_start(out=dbg_d[t * 128:(t + 1) * 128, :], in_=t1[b][:]), reads=['t1_%d' % b], writes=['dbg'])
            S.barrier()
        scA.close()

        with ExitStack() as p4:
            bct = {}
            for nm in ("g2b", "sc2b", "sh2b", "ln2g", "ln2b"):
                bct[nm] = sb(p4, nm, [128, 1024])
            for nm, dd in (("ln2g", ln2g_d), ("ln2b", ln2b_d)):
                S.dma('sp', lambda e, nm=nm, dd=dd: e.dma_start(out=bct[nm][:], in_=dd[:, :]), writes=[nm])
            dg = sb(p4, "dg4", [128, 128])
            bcast_tile(bct["g2b"], "g2b", 40, dg); bcast_tile(bct["sc2b"], "sc2b", 32, dg); bcast_tile(bct["sh2b"], "sh2b", 24, dg)
            x1t = [sb(p4, "x1t%d" % i, [128, 1024]) for i in range(2)]
            wqb = sb(p4, "wqb", [128, 8, 2048], BF16)
            keysT = sb(p4, "keysT", [128, 16, 128], BF16)
            with ExitStack() as tmp:
                stg = sb(tmp, "wq_stg", [128, 2048])
                for kc in range(8):
                    S.dma('sp', lambda e, kc=kc: e.dma_start(out=stg[:], in_=wq_d[:, kc, :]), writes=['wq_stg'])
                    S.op('pool', lambda e, kc=kc: e.tensor_copy(out=wqb[:, kc, :], in_=stg[:]), reads=['wq_stg'], writes=['wqb'])
                kst = sb(tmp, "kst", [128, 16, 128])
                S.dma('sp', lambda e: e.dma_start(out=kst[:], in_=keysT_d[:, :, :]), writes=['kst'])
                S.op('pool', lambda e: e.tensor_copy(out=keysT[:], in_=kst[:]), reads=['kst'], writes=['keysT'])
                S.barrier()
            NG = int(os.environ.get('KNG', '12'))
            uvb = [sb(p4, "uvb%d" % i, [128, 2048], BF16) for i in range(NG)]
            gl = sb(p4, "gl", [128, 128])
            pr = [sb(p4, "pr%d" % i, [128, 1024], BF16) for i in range(4)]
            dgk = [sb(p4, "dgk%d" % i, [128, 128], BF16) for i in range(4)]
            h2 = sb(p4, "h2", [128, 1024]); h2b = [sb(p4, "h2b%d" % i, [128, 1024], BF16) for i in range(2)]
            h2T = sb(p4, "h2T", [128, 8, 128], BF16); qT = sb(p4, "qT", [128, 16, 128], BF16)
            sc = sb(p4, "sc", [128, 16, 128]); scw = sb(p4, "scw", [128, 16, 128])
            top = sb(p4, "top", [128, 16, 16]); topi = sb(p4, "topi", [128, 16, 16], U32); topf = sb(p4, "topf", [128, 16, 16])
            cand = sb(p4, "cand", [128, 8, 256]); candw = sb(p4, "candw", [128, 8, 256]); eq = sb(p4, "eq", [128, 128, 16])
            best = sb(p4, "best", [128, 8, 16]); idxf = sb(p4, "idxf", [128, 128])
            posi = sb(p4, "posi", [128, 8, 16], U32); pai = sb(p4, "pai", [128, 128], U32); pbi = sb(p4, "pbi", [128, 128], U32)
            paf = sb(p4, "paf", [128, 128]); pbf = sb(p4, "pbf", [128, 128]); iaf = sb(p4, "iaf", [128, 128]); ibf = sb(p4, "ibf", [128, 128])
            iota16 = sb(p4, "iota16", [128, 16])
            S.dma('sp', lambda e: e.dma_start(out=iota16[:], in_=iota_d[:, :]), writes=['iota16'])
            gate = [sb(p4, "gate%d" % i, [128, 128]) for i in range(2)]
            idxi = [sb(p4, "idxi%d" % i, [128, 128], I32) for i in range(2)]
            wgt = [sb(p4, "wgt%d" % i, [128, 128]) for i in range(2)]
            dots = sb(p4, "dots", [128, 128]); junk = sb(p4, "junk", [128, 1024], BF16); junk2 = sb(p4, "junk2", [128, 256])
            zs = sb(p4, "zs", [128, 8]); nmx = sb(p4, "nmx", [128, 8])
            st = sb(p4, "st4", [128, 2, 6]); mv = sb(p4, "mv4", [128, 2]); rs = sb(p4, "rs4", [128, 1])
            fin = sb(p4, "fin", [128, 1024]); yb = sb(p4, "yb", [128, 1024])
            NT4 = 16 if debug != 'x1' else 0

            def prologue(t):
                p = t % 2
                xb_ = x1t[p]; x1k = 'x1t%d' % p
                S.dma('sp', lambda e: e.dma_start(out=xb_[:], in_=x1s[t * 128:(t + 1) * 128, :]), reads=K('x1_', t), writes=[x1k])
                S.op('dve', lambda e: e.memset(idxf[:], 0.0), writes=['idxf'])
                S.op('dve', lambda e: e.memset(zs[:], 0.0), writes=['zs'])
                layer_norm_rows(st, mv[:], rs[:], xb_[:], h2[:], x1k, 'h2', 'p4')
                S.op('dve', lambda e: e.tensor_tensor(out=h2[:], in0=h2[:], in1=bct["sc2b"][:], op=ALU.mult), reads=['h2', 'sc2b'], writes=['h2'])
                S.op('dve', lambda e: e.tensor_tensor(out=h2[:], in0=h2[:], in1=bct["sh2b"][:], op=ALU.add), reads=['h2', 'sh2b'], writes=['h2'])
                S.op('act', lambda e: e.copy(out=h2b[p][:], in_=h2[:]), reads=['h2'], writes=['h2b%d' % p])
                yield
                for half in range(2):
                    for c4 in range(4):
                        ch = half * 4 + c4
                        S.op('pe', lambda e, ch=ch, c4=c4, half=half: e.transpose(PB[half][:, c4 * 128:(c4 + 1) * 128], h2[:, ch * 128:(ch + 1) * 128], ident[:]),
                             reads=['h2', 'ident'], writes=['pb%d' % half], skip_self=True)
                    yield
                    S.op('act', lambda e, half=half: e.copy(out=h2T[:, half * 4:(half + 1) * 4, :], in_=PB[half][:, :].rearrange("p (j c) -> p j c", c=128)), reads=['pb%d' % half], writes=['h2T'])
                for g in range(4):
                    for c4 in range(4):
                        c = g * 4 + c4
                        for kc in range(8):
                            S.op('pe', lambda e, c=c, c4=c4, kc=kc, g=g: e.matmul(PB[2 + g][:, c4 * 128:(c4 + 1) * 128], lhsT=wqb[:, kc, c * 128:(c + 1) * 128], rhs=h2T[:, kc, :],
                                                                                start=(kc == 0), stop=(kc == 7)), reads=['wqb', 'h2T'], writes=['pb%d' % (2 + g)], skip_self=True)
                    yield
                    S.op('act' if g % 2 == 0 else 'dve', lambda e, g=g: (e.copy if g % 2 == 0 else e.tensor_copy)(out=qT[:, g * 4:(g + 1) * 4, :], in_=PB[2 + g][:, :].rearrange("p (j c) -> p j c", c=128)),
                         reads=['pb%d' % (2 + g)], writes=['qT'])
                for g in range(4):
                    for c4 in range(4):
                        c = g * 4 + c4
                        S.op('pe', lambda e, c=c, c4=c4, g=g: e.matmul(PB[2 + g][:, c4 * 128:(c4 + 1) * 128], lhsT=qT[:, c, :], rhs=keysT[:, c, :], start=True, stop=True),
                             reads=['qT', 'keysT'], writes=['pb%d' % (2 + g)], skip_self=True)
                    yield
                    S.op('act' if g % 2 == 0 else 'dve', lambda e, g=g: (e.copy if g % 2 == 0 else e.tensor_copy)(out=sc[:, g * 4:(g + 1) * 4, :], in_=PB[2 + g][:, :].rearrange("p (j c) -> p j c", c=128)),
                         reads=['pb%d' % (2 + g)], writes=['sc'])
                for c in range(16):
                    S.op('dve', lambda e, c=c: e.max(out=top[:, c, 0:8], in_=sc[:, c, :]), reads=['sc'], writes=['top'])
                    S.op('dve', lambda e, c=c: e.max_index(out=topi[:, c, 0:8], in_max=top[:, c, 0:8], in_values=sc[:, c, :]), reads=['sc', 'top'], writes=['topi'])
                    S.op('dve', lambda e, c=c: e.match_replace(out=scw[:, c, :], in_to_replace=top[:, c, 0:8], in_values=sc[:, c, :], imm_value=-1e30), reads=['sc', 'top'], writes=['scw'])
                    S.op('dve', lambda e, c=c: e.max(out=top[:, c, 8:16], in_=scw[:, c, :]), reads=['scw'], writes=['top'])
                    S.op('dve', lambda e, c=c: e.max_index(out=topi[:, c, 8:16], in_max=top[:, c, 8:16], in_values=scw[:, c, :]), reads=['scw', 'top'], writes=['topi'])
                    yield
                S.op('dve', lambda e: e.tensor_copy(out=topf[:], in_=topi[:]), reads=['topi'], writes=['topf'])
                t4 = top[:].rearrange("p (h two) k -> p h two k", two=2); f4 = topf[:].rearrange("p (h two) k -> p h two k", two=2)
                c4v = cand[:].rearrange("p h (a b) -> p h a b", b=16)
                for h in range(8):
                    S.op('dve', lambda e, h=h: e.tensor_tensor(out=c4v[:, h, :, :], in0=t4[:, h, 0, :].unsqueeze(2).to_broadcast([128, 16, 16]), in1=t4[:, h, 1, :].unsqueeze(1).to_broadcast([128, 16, 16]), op=ALU.add),
                         reads=['top'], writes=['cand'])
                    yield
                for h in range(8):
                    S.op('dve', lambda e, h=h: e.max(out=best[:, h, 0:8], in_=cand[:, h, :]), reads=['cand'], writes=['best'])
                    S.op('dve', lambda e, h=h: e.match_replace(out=candw[:, h, :], in_to_replace=best[:, h, 0:8], in_values=cand[:, h, :], imm_value=-1e30), reads=['cand', 'best'], writes=['candw'])
                    S.op('dve', lambda e, h=h: e.max(out=best[:, h, 8:16], in_=candw[:, h, :]), reads=['candw'], writes=['best'])
                    S.op('dve', lambda e, h=h: e.max_index(out=posi[:, h, 0:8], in_max=best[:, h, 0:8], in_values=cand[:, h, :]), reads=['cand', 'best'], writes=['posi'])
                    S.op('dve', lambda e, h=h: e.max_index(out=posi[:, h, 8:16], in_max=best[:, h, 8:16], in_values=candw[:, h, :]), reads=['candw', 'best'], writes=['posi'])
                    yield
                pflat = posi[:].rearrange("p h k -> p (h k)")
                S.op('dve', lambda e: e.tensor_single_scalar(out=pai[:], in_=pflat, scalar=4, op=ALU.logical_shift_right), reads=['posi'], writes=['pai'])
                S.op('dve', lambda e: e.tensor_single_scalar(out=pbi[:], in_=pflat, scalar=15, op=ALU.bitwise_and), reads=['posi'], writes=['pbi'])
                S.op('dve', lambda e: e.tensor_copy(out=paf[:], in_=pai[:]), reads=['pai'], writes=['paf'])
                S.op('dve', lambda e: e.tensor_copy(out=pbf[:], in_=pbi[:]), reads=['pbi'], writes=['pbf'])
                yield
                eq4 = eq[:].rearrange("p (h k) a -> p h k a", k=16)
                for (pp, pk_, half_, dst, dn) in ((paf, 'paf', 0, iaf, 'iaf'), (pbf, 'pbf', 1, ibf, 'ibf')):
                    S.op('dve', lambda e, pp=pp: e.tensor_tensor(out=eq[:], in0=iota16[:].unsqueeze(1).to_broadcast([128, 128, 16]), in1=pp[:].unsqueeze(2).to_broadcast([128, 128, 16]), op=ALU.is_equal),
                         reads=['iota16', pk_, 'eq'], writes=['eq'])
                    S.op('dve', lambda e, half_=half_: e.tensor_tensor(out=eq4, in0=eq4, in1=f4[:, :, half_, :].unsqueeze(2).to_broadcast([128, 8, 16, 16]), op=ALU.mult),
                         reads=['eq', 'topf'], writes=['eq'])
                    S.op('dve', lambda e, dst=dst: e.tensor_reduce(out=dst[:], in_=eq[:], axis=AX.X, op=ALU.add), reads=['eq'], writes=[dn])
                    yield
                S.op('dve', lambda e: e.scalar_tensor_tensor(out=idxf[:], in0=iaf[:], scalar=128.0, in1=ibf[:], op0=ALU.mult, op1=ALU.add), reads=['iaf', 'ibf'], writes=['idxf'])
                S.op('dve', lambda e: e.tensor_scalar_min(out=idxf[:], in0=idxf[:], scalar1=16383.0), reads=['idxf'], writes=['idxf'])
                S.op('dve', lambda e: e.tensor_copy(out=idxi[p][:], in_=idxf[:]), reads=['idxf'], writes=['idxi%d' % p])
                S.op('dve', lambda e: e.tensor_scalar_mul(out=nmx[:], in0=best[:, :, 0], scalar1=-1.0), reads=['best'], writes=['nmx'])
                g3 = gate[p][:].rearrange("p (h k) -> p h k", k=16)
                for h in range(8):
                    S.op('act', lambda e, h=h: e.activation(out=g3[:, h, :], in_=best[:, h, :], func=AF.Exp, bias=nmx[:, h:h + 1], scale=1.0, accum_out=zs[:, h:h + 1]),
                         reads=['best', 'nmx'], writes=['gate%d' % p, 'zs'])
                S.op('dve', lambda e: e.reciprocal(out=zs[:], in_=zs[:]), reads=['zs'], writes=['zs'])
                S.op('dve', lambda e: e.tensor_tensor(out=g3, in0=g3, in1=zs[:].unsqueeze(2).to_broadcast([128, 8, 16]), op=ALU.mult), reads=['gate%d' % p, 'zs'], writes=['gate%d' % p])

            def fused(t, gen=None):
                p = t % 2
                LAG = 2

                def tail(kk):
                    s_ = (t * 128 + kk) % NG; d4 = kk % 4
                    S.op('dve', lambda e: e.tensor_scalar(out=dgk[d4][:], in0=identb[:], scalar1=gl[:, kk:kk + 1], scalar2=gate[p][:, kk:kk + 1], op0=ALU.mult, op1=ALU.mult),
                         reads=['identb', 'gl_%d' % kk, 'gate%d' % p], writes=['dgk%d' % d4])
                    for half in range(2):
                        S.op('pe', lambda e, half=half: e.matmul(PB[6 + half][:, :], lhsT=dgk[d4][:], rhs=uvb[s_][:, 1024 + half * 512:1024 + (half + 1) * 512], start=(kk == 0), stop=(kk == 127)),
                             reads=['dgk%d' % d4, 'uvb%d' % s_], writes=['pb%d' % (6 + half)], skip_self=True)

                for k in range(128):
                    s_ = (t * 128 + k) % NG; j4 = k % 4
                    dk = 'dots_%d' % k
                    S.dma('pool', lambda e, k=k, s_=s_: e.indirect_dma_start(out=uvb[s_][:], out_offset=None, in_=uv_d[:, :], in_offset=bass.IndirectOffsetOnAxis(ap=idxi[p][:, k:k + 1], axis=0)),
                          reads=['idxi%d' % p, 'tabs'], writes=['uvb%d' % s_])
                    S.op('dve', lambda e, k=k, s_=s_, j4=j4: e.tensor_tensor(out=pr[j4][:], in0=uvb[s_][:, 0:1024], in1=h2b[p][:], op=ALU.mult),
                         reads=['uvb%d' % s_, 'h2b%d' % p], writes=['pr%d' % j4])
                    S.op('act', lambda e, k=k, j4=j4: e.activation(out=junk[:], in_=pr[j4][:], func=AF.Identity, accum_out=dots[:, k:k + 1]),
                         reads=['pr%d' % j4, 'dots0'], writes=[dk])
                    S.op('act', lambda e, k=k: e.activation(out=gl[:, k:k + 1], in_=dots[:, k:k + 1], func=AF.Gelu), reads=[dk], writes=['gl_%d' % k])
                    if k >= LAG:
                        tail(k - LAG)
                    if gen is not None and k % 2 == 1:
                        next(gen, None)
                for kk in range(128 - LAG, 128):
                    tail(kk)
                if gen is not None:
                    for _ in gen:
                        pass
                xb_ = x1t[p]; x1k = 'x1t%d' % p
                for half in range(2):
                    S.op('dve', lambda e, half=half: e.tensor_tensor(out=yb[:, half * 512:(half + 1) * 512], in0=PB[6 + half][:, :], in1=bct["g2b"][:, half * 512:(half + 1) * 512], op=ALU.mult),
                         reads=['pb%d' % (6 + half), 'g2b', 'yb'], writes=['yb'])
                S.op('dve', lambda e: e.scalar_tensor_tensor(out=fin[:], in0=xb_[:], scalar=ALPHA, in1=yb[:], op0=ALU.mult, op1=ALU.add), reads=[x1k, 'yb', 'fin'], writes=['fin'])
                layer_norm_rows(stf, mvf[:], rsf[:], fin[:], fin[:], 'fin', 'fin', 'p4f')
                S.op('dve', lambda e: e.tensor_tensor(out=fin[:], in0=fin[:], in1=bct["ln2g"][:], op=ALU.mult), reads=['fin', 'ln2g'], writes=['fin'])
                S.op('dve', lambda e: e.tensor_tensor(out=fin[:], in0=fin[:], in1=bct["ln2b"][:], op=ALU.add), reads=['fin', 'ln2b'], writes=['fin'])
                S.dma('sp', lambda e: e.dma_start(out=out_d[t * 128:(t + 1) * 128, :], in_=fin[:]), reads=['fin'], writes=['out'])

            stf = sb(p4, "st4f", [128, 2, 6]); mvf = sb(p4, "mv4f", [128, 2]); rsf = sb(p4, "rs4f", [128, 1])
            S.op('dve', lambda e: e.memset(dots[:], 0.0), writes=['dots0'])
            if NT4:
                for _ in prologue(0):
                    pass
            for t in range(NT4):
                fused(t, prologue(t + 1) if t + 1 < NT4 else None)
            S.wait_all('sp', ['out', 'dbg'])
            S.barrier()
    return nc


def _prep_shared(inp):
    f = np.float32
    sh = {}
    sh["w_mod"] = np.ascontiguousarray(inp["w_mod"][0].reshape(8, 128, 6144).transpose(1, 0, 2))
    sh["b_modT"] = np.ascontiguousarray(inp["b_mod"][0].reshape(48, 128).T)
    sh["w_in"] = np.ascontiguousarray(inp["w_in"][0].reshape(8, 128, 4624).transpose(1, 0, 2))
    sh["lgT"] = np.ascontiguousarray(inp["hg_lb_logits"].reshape(2, 2, 4, 128).transpose(3, 0, 1, 2))
    sh["hgn"] = np.ascontiguousarray(np.broadcast_to(inp["hg_norm_g"][0][None, :], (64, 512)))
    sh["mln"] = np.ascontiguousarray(np.broadcast_to(inp["ml_norm_g"][0][None, :], (64, 512)))
    sh["convw"] = np.ascontiguousarray(inp["ml_conv_w"][0].reshape(9, 8, 128).transpose(2, 0, 1))
    sh["convb"] = np.ascontiguousarray(inp["ml_conv_b"][0].reshape(8, 128).T)
    sh["gateb"] = np.ascontiguousarray(inp["ml_gate_b"][0].reshape(2, 8).T)
    sh["w_out"] = np.ascontiguousarray(inp["w_out"][0].reshape(8, 128, 1024).transpose(1, 0, 2))
    for nm, key in (("ln1g", "ln1_g"), ("ln1b", "ln1_b"), ("ln2g", "ln2_g"), ("ln2b", "ln2_b")):
        sh[nm] = np.ascontiguousarray(np.broadcast_to(inp[key][0][None, :], (128, 1024)))
    sh["wq"] = np.ascontiguousarray(inp["peer_wq"][0].reshape(8, 128, 2048).transpose(1, 0, 2))
    sh["keysT"] = np.ascontiguousarray(inp["peer_keys"][0].reshape(16, 128, 128).transpose(2, 0, 1))
    nexp = 128 if os.environ.get("KDEBUG") else 16384
    sh["pu"] = np.ascontiguousarray(inp["peer_u"][0][:nexp])
    sh["pv"] = np.ascontiguousarray(inp["peer_v"][0][:nexp])
    sh["ident"] = np.eye(128, dtype=f)
    m = np.zeros((64, 2, 64), f)
    s = np.arange(64)[:, None]; c = np.arange(64)[None, :]
    m[:, 0, :] = (s <= c); m[:, 1, :] = (s >= c)
    sh["masks"] = m
    rm = np.ones((128, 512), f); rm[:, ::64] = 0.0
    sh["rmask"] = rm
    sh["sel8"] = np.eye(8, dtype=f)
    sh["iota16"] = np.ascontiguousarray(np.broadcast_to(np.arange(16, dtype=f)[None, :], (128, 16)))
    dm = np.zeros((8, 2), f); dm[0:4, 0] = 1.0; dm[4:8, 1] = 1.0
    sh["dirm"] = dm
    return {k: np.asarray(v, dtype=f) for k, v in sh.items()}


def kernel(**inputs):
    inp = {k: np.asarray(v) for k, v in inputs.items()}
    debug = os.environ.get("KDEBUG") or None
    nc = build(debug)
    sh = _prep_shared(inp)
    in_maps = []
    for b in range(8):
        m = dict(sh)
        m["xs"] = np.ascontiguousarray(np.concatenate([inp["ctx"][b], inp["x"][b]], axis=0).astype(np.float32))
        m["cT"] = np.ascontiguousarray(np.stack([inp["c"][b], inp["c_ctx"]], axis=-1).reshape(8, 128, 2).transpose(1, 0, 2).astype(np.float32))
        in_maps.append(m)
    res = run_bass_kernel_spmd(nc, in_maps, core_ids=list(range(8)))
    key = "dbg" if debug else "out"
    return np.stack([np.asarray(r[key]) for r in res.results], axis=0).astype(np.float32)
```

```python
import os
import numpy as np
from contextlib import ExitStack
import concourse.bass as bass
import concourse.mybir as mybir
from concourse.bass_utils import run_bass_kernel_spmd

F32 = mybir.dt.float32; BF16 = mybir.dt.bfloat16; I32 = mybir.dt.int32; U32 = mybir.dt.uint32
AF = mybir.ActivationFunctionType; ALU = mybir.AluOpType; AX = mybir.AxisListType

NTOK = 2304; NLAT = 2048; NCH = 36; NLCH = 32
ALPHA = 2.0 ** 0.25
EPS = 1e-6


class Sched:
    NDMA = 32

    def __init__(self, nc, es):
        self.nc = nc
        self.engs = {'pe': nc.tensor, 'act': nc.scalar, 'dve': nc.vector, 'pool': nc.gpsimd, 'sp': nc.sync}
        self.sem = {k: es.enter_context(nc.semaphore("sem_" + k)) for k in self.engs}
        self.cnt = {k: 0 for k in self.engs}
        self.dsem = [es.enter_context(nc.semaphore("dsem%d" % i)) for i in range(self.NDMA)]
        self.dcnt = [0] * self.NDMA
        self.dnext = 0
        self.seen = {k: {} for k in self.engs}
        self.bufs = {}

    def _deps(self, reads, writes):
        deps = []
        for r in reads:
            b = self.bufs.get(r)
            if b and b['w'] is not None:
                deps.append(b['w'])
        for w in writes:
            b = self.bufs.get(w)
            if b:
                if b['w'] is not None:
                    deps.append(b['w'])
                deps.extend(b['r'])
        return deps

    def _wait(self, eng, deps, skip_self=False):
        best = {}
        for (sid, sem, val, owner) in deps:
            if skip_self and owner == eng:
                continue
            if best.get(sid, (None, 0))[1] < val:
                best[sid] = (sem, val)
        for sid, (sem, val) in best.items():
            if self.seen[eng].get(sid, 0) >= val:
                continue
            self.engs[eng].wait_ge(sem, val)
            self.seen[eng][sid] = val

    def _record(self, dep, reads, writes):
        for r in reads:
            b = self.bufs.setdefault(r, {'w': None, 'r': []})
            b['r'] = [d for d in b['r'] if d[0] != dep[0]] + [dep]
        for w in writes:
            self.bufs[w] = {'w': dep, 'r': []}

    @staticmethod
    def _split(keys):
        norm, ps = [], []
        for k in keys:
            if k.startswith('pb') and len(k) > 2 and k[2].isdigit():
                ps.append(k[:3])
            else:
                norm.append(k)
        return norm, ps

    def op(self, eng, fn, reads=(), writes=(), skip_self=False):
        reads, pr = self._split(reads)
        writes, pw = self._split(writes)
        banks = sorted(set(pr + pw))
        deps = self._deps(reads, writes)
        deps += [d for d in self._deps((), banks) if d[3] != eng]
        self._wait(eng, deps, skip_self)
        ins = fn(self.engs[eng])
        self.cnt[eng] += 1
        ins.then_inc(self.sem[eng], 1)
        self._record(('e_' + eng, self.sem[eng], self.cnt[eng], eng), reads, list(writes) + banks)
        return ins

    def dma(self, q, fn, reads=(), writes=()):
        deps = self._deps(reads, writes)
        j = self.dnext
        self.dnext = (self.dnext + 1) % self.NDMA
        if self.dcnt[j] > 0:
            deps = deps + [('d%d' % j, self.dsem[j], self.dcnt[j], 'dma')]
        self._wait(q, deps)
        ins = fn(self.engs[q])
        self.dcnt[j] += 16
        ins.then_inc(self.dsem[j], 16)
        self._record(('d%d' % j, self.dsem[j], self.dcnt[j], 'dma'), reads, writes)

    def wait_all(self, eng, keys):
        deps = []
        for k in keys:
            b = self.bufs.get(k)
            if b:
                if b['w'] is not None:
                    deps.append(b['w'])
                deps.extend(b['r'])
        self._wait(eng, deps)

    def barrier(self):
        for e in self.engs:
            deps = [('e_' + f, self.sem[f], self.cnt[f], f) for f in self.engs if f != e and self.cnt[f] > 0]
            deps += [('d%d' % j, self.dsem[j], self.dcnt[j], 'dma') for j in range(self.NDMA) if self.dcnt[j] > 0]
            self._wait(e, deps)


class View:
    def __init__(self, ap):
        self.ap = ap

    def __getitem__(self, idx):
        return self.ap[idx]


def K(name, a, b=None):
    if b is None:
        return ["%s%d" % (name, a)]
    return ["%s%d" % (name, i) for i in range(a, b)]


def build(debug=None):
    nc = bass.Bass("TRN2", target_bir_lowering=False)
    D = {}

    def din(name, shape, dt=F32):
        D[name] = nc.dram_tensor(name, shape, dt, kind="ExternalInput").ap()
        return D[name]

    xs = din("xs", [NTOK, 1024]); cT_d = din("cT", [128, 8, 2]); wmod_d = din("w_mod", [128, 8, 6144])
    bmod_d = din("b_modT", [128, 48]); win_d = din("w_in", [128, 8, 4624]); lg_d = din("lgT", [128, 2, 2, 4])
    hgn_d = din("hgn", [64, 512]); mln_d = din("mln", [64, 512]); convw_d = din("convw", [128, 9, 8])
    convb_d = din("convb", [128, 8]); gateb_d = din("gateb", [8, 2]); wout_d = din("w_out", [128, 8, 1024])
    ln1g_d = din("ln1g", [128, 1024]); ln1b_d = din("ln1b", [128, 1024]); ln2g_d = din("ln2g", [128, 1024])
    ln2b_d = din("ln2b", [128, 1024]); wq_d = din("wq", [128, 8, 2048]); keysT_d = din("keysT", [128, 16, 128])
    NEXP = 128 if debug else 16384
    pu_d = din("pu", [NEXP, 1024]); pv_d = din("pv", [NEXP, 1024])
    ident_d = din("ident", [128, 128]); masks_d = din("masks", [64, 2, 64]); rmask_d = din("rmask", [128, 512])
    sel8_d = din("sel8", [8, 8]); dirm_d = din("dirm", [8, 2]); iota_d = din("iota16", [128, 16])
    out_d = nc.dram_tensor("out", [NLAT, 1024], F32, kind="ExternalOutput").ap()
    dbg_d = None
    if debug:
        dbg_d = nc.dram_tensor("dbg", [NLAT, 1024] if debug != 'mix' else [1024, NLAT], F32, kind="ExternalOutput").ap()

    with ExitStack() as es:
        S = Sched(nc, es)

        uid = [0]

        def sb(st, name, shape, dt=F32):
            uid[0] += 1
            return st.enter_context(nc.sbuf_tensor("s%d_%s" % (uid[0], name), shape, dt))

        PB = [es.enter_context(nc.psum_tensor("pb%d" % i, [128, 512], F32)) for i in range(8)]

        ident = sb(es, "ident", [128, 128]); identb = sb(es, "identb", [128, 128], BF16)
        masks = sb(es, "masks", [64, 2, 64]); rmask = sb(es, "rmask", [128, 512])
        ones = sb(es, "ones", [128, 128]); epsc = sb(es, "epsc", [128, 1])
        modv = sb(es, "modv", [128, 48, 2])
        S.dma('sp', lambda e: e.dma_start(out=ident[:], in_=ident_d[:, :]), writes=['ident'])
        S.dma('sp', lambda e: e.dma_start(out=masks[:], in_=masks_d[:, :, :]), writes=['masks'])
        S.dma('sp', lambda e: e.dma_start(out=rmask[:], in_=rmask_d[:, :]), writes=['rmask'])
        S.op('dve', lambda e: e.tensor_copy(out=identb[:], in_=ident[:]), reads=['ident'], writes=['identb'])
        S.op('dve', lambda e: e.memset(ones[:], 1.0), writes=['ones'])
        S.op('dve', lambda e: e.memset(epsc[:], EPS), writes=['epsc'])

        with ExitStack() as p0:
            cT = sb(p0, "cT", [128, 8, 2]); scT = sb(p0, "scT", [128, 8, 2]); bmodT = sb(p0, "bmodT", [128, 48])
            wm = [sb(p0, "wm%d" % i, [128, 6144]) for i in range(2)]
            S.dma('sp', lambda e: e.dma_start(out=cT[:], in_=cT_d[:, :, :]), writes=['cT'])
            S.dma('sp', lambda e: e.dma_start(out=bmodT[:], in_=bmod_d[:, :]), writes=['bmodT'])
            S.op('act', lambda e: e.activation(out=scT[:], in_=cT[:], func=AF.Silu), reads=['cT'], writes=['scT'])
            for kc in range(8):
                w = wm[kc % 2]; wk = 'wm%d' % (kc % 2)
                S.dma('sp' if kc % 2 == 0 else 'pool', lambda e, w=w, kc=kc: e.dma_start(out=w[:], in_=wmod_d[:, kc, :]), writes=[wk])
                for j in range(48):
                    S.op('pe', lambda e, w=w, kc=kc, j=j: e.matmul(PB[kc // 4][:, (kc % 4) * 96 + 2 * j:(kc % 4) * 96 + 2 * j + 2], lhsT=w[:, j * 128:(j + 1) * 128], rhs=scT[:, kc, :],
                                                                 start=True, stop=True),
                         reads=[wk, 'scT'], writes=['pb%d' % (kc // 4)], skip_self=True)
            mflat = modv[:].rearrange("p j n -> p (j n)")
            S.op('dve', lambda e: e.tensor_tensor(out=modv[:], in0=PB[0][:, 0:96].rearrange("p (j n) -> p j n", n=2), in1=bmodT[:].unsqueeze(2).to_broadcast([128, 48, 2]), op=ALU.add),
                 reads=['pb0', 'bmodT'], writes=['modv'])
            for kc in range(1, 8):
                S.op('dve', lambda e, kc=kc: e.tensor_tensor(out=mflat, in0=mflat, in1=PB[kc // 4][:, (kc % 4) * 96:(kc % 4) * 96 + 96], op=ALU.add),
                     reads=['pb%d' % (kc // 4), 'modv'], writes=['modv'])
            S.op('dve', lambda e: e.tensor_scalar_add(out=modv[:, 8:16, :], in0=modv[:, 8:16, :], scalar1=1.0), reads=['modv'], writes=['modv'])
            S.op('dve', lambda e: e.tensor_scalar_add(out=modv[:, 32:40, :], in0=modv[:, 32:40, :], scalar1=1.0), reads=['modv'], writes=['modv'])
            S.barrier()
        if debug == 'p0':
            S.dma('sp', lambda e: e.dma_start(out=dbg_d[0:128, 0:96], in_=modv[:].rearrange("p a b -> p (a b)")), reads=['modv'], writes=['dbg'])
            S.wait_all('sp', ['dbg']); S.barrier()
            return nc

        scA = ExitStack(); scB = ExitStack()
        mixT = sb(scA, "mixT", [128, 8, NLAT], BF16)
        hT = sb(scB, "hT", [128, 8, NTOK], BF16)
        wstg = sb(scB, "wstg", [128, 8, 128])
        tstg = [sb(scB, "tstg%d" % i, [128, 512]) for i in range(2)]
        tbf = [sb(scB, "tbf%d" % i, [128, 512], BF16) for i in range(2)]
        uv_d = nc.dram_tensor("uvbf", [NEXP, 2048], BF16, kind="Internal").ap()
        prep_pos = [0]

        def prep_tables(npieces):
            if debug:
                return
            for _ in range(npieces):
                i = prep_pos[0]
                if i >= 512:
                    return
                prep_pos[0] += 1
                src, coff = (pu_d, 0) if i < 256 else (pv_d, 1024)
                r = (i % 256) // 2; c = (i % 2) * 512; bb = i % 2
                S.dma('sp', lambda e, src=src, r=r, c=c, bb=bb: e.dma_start(out=tstg[bb][:], in_=src[r * 128:(r + 1) * 128, c:c + 512]), writes=['tstg%d' % bb])
                S.op('pool', lambda e, bb=bb: e.tensor_copy(out=tbf[bb][:], in_=tstg[bb][:]), reads=['tstg%d' % bb], writes=['tbf%d' % bb])
                S.dma('pool', lambda e, coff=coff, r=r, c=c, bb=bb: e.dma_start(out=uv_d[r * 128:(r + 1) * 128, coff + c:coff + c + 512], in_=tbf[bb][:]), reads=['tbf%d' % bb], writes=['tabs_%d' % i])

        def layer_norm_rows(st_ap, mv_ap, rstd_ap, src, dst, skey, dkey, tag):
            S.op('dve', lambda e: e.bn_stats(out=st_ap[:, 0, :], in_=src[:, 0:512]), reads=[skey], writes=[tag + 'st'])
            S.op('dve', lambda e: e.bn_stats(out=st_ap[:, 1, :], in_=src[:, 512:1024]), reads=[skey], writes=[tag + 'st'])
            S.op('dve', lambda e: e.bn_aggr(out=mv_ap, in_=st_ap[:].rearrange("p a b -> p (a b)")), reads=[tag + 'st'], writes=[tag + 'mv'])
            S.op('act', lambda e: e.activation(out=rstd_ap, in_=mv_ap[:, 1:2], func=AF.Sqrt, bias=epsc[:, 0:1], scale=1.0),
                 reads=[tag + 'mv', 'epsc'], writes=[tag + 'rs'])
            S.op('dve', lambda e: e.reciprocal(out=rstd_ap, in_=rstd_ap), reads=[tag + 'rs'], writes=[tag + 'rs'])
            S.op('dve', lambda e: e.tensor_scalar(out=dst, in0=src, scalar1=mv_ap[:, 0:1], scalar2=rstd_ap, op0=ALU.subtract, op1=ALU.mult),
                 reads=[skey, tag + 'mv', tag + 'rs'], writes=[dkey])

        with ExitStack() as p1:
            xt = [sb(p1, "xt%d" % i, [128, 1024]) for i in range(2)]
            xn = [sb(p1, "xn%d" % i, [128, 1024]) for i in range(2)]
            st = [sb(p1, "st%d" % i, [128, 2, 6]) for i in range(2)]
            mv = [sb(p1, "mv%d" % i, [128, 2]) for i in range(2)]
            rs = [sb(p1, "rs%d" % i, [128, 1]) for i in range(2)]
            PSAP = bool(os.environ.get("KPSAP"))
            for t in range(18):
                b = t % 2
                n = 1 if t < 2 else 0
                S.dma('sp' if b == 0 else 'pool', lambda e, t=t, b=b: e.dma_start(out=xt[b][:], in_=xs[t * 128:(t + 1) * 128, :]), writes=['xt%d' % b])
                layer_norm_rows(st[b], mv[b][:], rs[b][:], xt[b][:], xn[b][:], 'xt%d' % b, 'xn%d' % b, 'p1%d' % b)
                for half in range(2):
                    bank = 2 * b + half
                    pk = 'pb%d' % bank
                    for c4 in range(4):
                        ch = half * 4 + c4
                        S.op('pe', lambda e, b=b, ch=ch, c4=c4, bank=bank: e.transpose(PB[bank][:, c4 * 128:(c4 + 1) * 128], xn[b][:, ch * 128:(ch + 1) * 128], ident[:]),
                             reads=['xn%d' % b, 'ident'], writes=[pk], skip_self=True)
                    if not PSAP:
                        S.op('act', lambda e, b=b, half=half, bank=bank: e.copy(out=xt[b][:, half * 512:(half + 1) * 512], in_=PB[bank][:, :]), reads=[pk, 'xt%d' % b], writes=['xt%d' % b])
                    for c4 in range(4):
                        ch = half * 4 + c4
                        dst = hT[:, ch, t * 128:(t + 1) * 128]
                        src = PB[bank][:, c4 * 128:(c4 + 1) * 128] if PSAP else xt[b][:, ch * 128:(ch + 1) * 128]
                        S.op('dve', lambda e, dst=dst, src=src, ch=ch, n=n: e.tensor_scalar(out=dst, in0=src, scalar1=modv[:, 8 + ch, n:n + 1], scalar2=modv[:, ch, n:n + 1],
                                                                                      op0=ALU.mult, op1=ALU.add),
                             reads=([pk] if PSAP else ['xt%d' % b]) + ['modv'], writes=K('hT', t))
            S.barrier()

        if debug == 'p1':
            with ExitStack() as dd:
                hf = sb(dd, "hf", [128, 1024])
                for t in range(16):
                    S.op('dve', lambda e, t=t: e.tensor_copy(out=hf[:].rearrange("p (a b) -> p a b", b=128), in_=hT[:, :, 256 + t * 128:256 + (t + 1) * 128]), reads=K('hT', t + 2) + ['hf'], writes=['hf'])
                    S.dma('sp', lambda e, t=t: e.dma_start(out=dbg_d[t * 128:(t + 1) * 128, :], in_=hf[:]), reads=['hf'], writes=['dbg'])
                S.wait_all('sp', ['dbg']); S.barrier()
            scB.close(); scA.close()
            return nc
        def load_w(stk, name, col0, ncols, src=None):
            src = win_d if src is None else src
            wb = sb(stk, name, [128, 8, ncols], BF16)
            S.dma('sp', lambda e: e.dma_start(out=wstg[:, :, 0:ncols], in_=src[:, :, col0:col0 + ncols]), writes=['wstg'])
            S.op('pool', lambda e: e.tensor_copy(out=wb[:], in_=wstg[:, :, 0:ncols]), reads=['wstg'], writes=[name])
            return wb

        BLKS = [(0, 512), (512, 512), (1024, 512), (1536, 512), (2048, 256)]

        def proj_fm_block(wb, wname, ncols, bank, t0, nt):
            for kc in range(8):
                S.op('pe', lambda e, kc=kc: e.matmul(PB[bank][0:ncols, 0:nt], lhsT=wb[:, kc, :], rhs=hT[:, kc, t0:t0 + nt], start=(kc == 0), stop=(kc == 7)),
                     reads=[wname] + K('hT', t0 // 128, (t0 + nt) // 128), writes=['pb%d' % bank], skip_self=True)

        def proj_tm_group(wb, wname, ncols, bank, c0, ncks):
            for j in range(ncks):
                n = c0 + j
                for kc in range(8):
                    S.op('pe', lambda e, kc=kc, j=j, n=n: e.matmul(PB[bank][0:64, j * ncols:(j + 1) * ncols], lhsT=hT[:, kc, n * 64:(n + 1) * 64], rhs=wb[:, kc, :],
                                                                   start=(kc == 0), stop=(kc == 7)),
                         reads=[wname] + K('hT', n // 2), writes=['pb%d' % bank], skip_self=True)

        S32 = sb(scB, "S32", [128, 2, 132]); Sbf = sb(scB, "Sbf", [128, 2, 132], BF16)
        stm = sb(scB, "stm", [64, 2, 64], BF16)
        usb = sb(scB, "usb", [128, 2, 132])

        def chunk_loop(dirn, KsT, QsT, Vi, QoT, Ku, Vu, dec_ap, W, evac, rk, tagk):
            order = list(range(36)) if dirn == 0 else [3, 2, 1, 0] + list(range(35, 3, -1))
            S.op('dve', lambda e: e.memset(S32[:, 0, :], 0.0), writes=['S32_0'])
            S.op('dve', lambda e: e.memset(Sbf[:, 0, :], 0.0), writes=['Sbf_0'])

            def pre(idx):
                n = order[idx]
                sl = slice(n * 64, (n + 1) * 64)
                s2 = idx % 2
                if n >= 4:
                    pst = PB[2 + s2][0:64, 0:64]; pstk = 'pb%d' % (2 + s2)
                    S.op('pe', lambda e: e.matmul(pst, lhsT=KsT[:, sl], rhs=QsT[:, sl], start=True, stop=True),
                         reads=rk, writes=[pstk], skip_self=True)
                    S.op('dve', lambda e: e.tensor_tensor(out=stm[:, s2, :], in0=pst, in1=masks[:, dirn, :], op=ALU.mult),
                         reads=[pstk, 'masks'], writes=['stm%d' % s2])
                if idx < 35:
                    pu = PB[6 + s2][:, 0:W]; puk = 'pb%d' % (6 + s2)
                    S.op('pe', lambda e: e.matmul(pu, lhsT=Ku[:, n, :], rhs=Vu[:, n, 0:W], start=True, stop=True),
                         reads=rk, writes=[puk], skip_self=True)
                    S.op('act', lambda e: e.copy(out=usb[:, s2, 0:W], in_=pu), reads=[puk], writes=['usb%d' % s2])

            pre(0)
            cur = 0
            for idx, n in enumerate(order):
                if idx + 1 < 36:
                    pre(idx + 1)
                sl = slice(n * 64, (n + 1) * 64)
                s2 = idx % 2
                if n >= 4:
                    po = PB[4 + s2][0:64, 0:W]; pok = 'pb%d' % (4 + s2)
                    S.op('pe', lambda e, po=po, s2=s2, n=n: e.matmul(po, lhsT=stm[:, s2, :], rhs=Vi[:, n, 0:W], start=True, stop=False),
                         reads=['stm%d' % s2] + rk, writes=[pok], skip_self=True)
                    S.op('pe', lambda e, po=po, sl=sl, cur=cur: e.matmul(po, lhsT=QoT[:, sl], rhs=Sbf[:, cur, 0:W], start=False, stop=True),
                         reads=['Sbf_%d' % cur] + rk, writes=[pok], skip_self=True)
                if idx < 35:
                    nxt = 1 - cur
                    S.op('dve', lambda e, n=n, cur=cur, nxt=nxt, s2=s2: e.scalar_tensor_tensor(out=Sbf[:, nxt, 0:W], in0=S32[:, cur, 0:W], scalar=dec_ap(n), in1=usb[:, s2, 0:W],
                                                                                            op0=ALU.mult, op1=ALU.add),
                         reads=['S32_%d' % cur, 'usb%d' % s2, tagk], writes=['Sbf_%d' % nxt])
                    S.op('dve', lambda e, n=n, cur=cur, nxt=nxt, s2=s2: e.scalar_tensor_tensor(out=S32[:, nxt, 0:W], in0=S32[:, cur, 0:W], scalar=dec_ap(n), in1=usb[:, s2, 0:W],
                                                                                            op0=ALU.mult, op1=ALU.add),
                         reads=['S32_%d' % cur, 'usb%d' % s2, tagk], writes=['S32_%d' % nxt])
                    cur = nxt
                if n >= 4:
                    evac(n - 4, n, po, pok)

        def transpose_chunks_to_mixT(src, skey, head):
            for g in range(4):
                bank = g % 2
                for j in range(8):
                    n = g * 8 + j
                    S.op('pe', lambda e, n=n, j=j, bank=bank: e.transpose(PB[bank][:, j * 64:(j + 1) * 64], src[:, n, :], ident[0:64, 0:64]),
                         reads=list(skey) + ['ident'], writes=['pb%d' % bank], skip_self=True)
                S.op('act', lambda e, g=g, bank=bank: e.copy(out=mixT[:, head, g * 512:(g + 1) * 512], in_=PB[bank][:, :]),
                     reads=['pb%d' % bank], writes=K('mixT', head))

        with ExitStack() as g0:
            lgT = sb(g0, "lgT", [128, 2, 2, 4]); lbT = sb(g0, "lbT", [128, 2, 4]); omlT = sb(g0, "omlT", [128, 2, 4]); nomlT = sb(g0, "nomlT", [128, 2, 4])
            hgn = sb(g0, "hgn", [64, 512])
            S.dma('sp', lambda e: e.dma_start(out=lgT[:], in_=lg_d[:, :, :, :]), writes=['lgT'])
            S.dma('sp', lambda e: e.dma_start(out=hgn[:], in_=hgn_d[:, :]), writes=['hgn'])
            S.op('dve', lambda e: e.tensor_tensor(out=lbT[:], in0=lgT[:, :, 0, :], in1=lgT[:, :, 1, :], op=ALU.subtract), reads=['lgT'], writes=['lbT'])
            S.op('act', lambda e: e.activation(out=lbT[:], in_=lbT[:], func=AF.Sigmoid), reads=['lbT'], writes=['lbT'])
            S.op('dve', lambda e: e.tensor_scalar(out=omlT[:], in0=lbT[:], scalar1=-1.0, scalar2=1.0, op0=ALU.mult, op1=ALU.add), reads=['lbT'], writes=['omlT'])
            S.op('dve', lambda e: e.tensor_scalar_mul(out=nomlT[:], in0=omlT[:], scalar1=-1.0), reads=['omlT'], writes=['nomlT'])
            QsT = [sb(g0, "gQsT%d" % d, [128, NTOK], BF16) for d in range(2)]
            KsT = [sb(g0, "gKsT%d" % d, [128, NTOK], BF16) for d in range(2)]
            QoT = [sb(g0, "gQoT%d" % d, [128, NTOK], BF16) for d in range(2)]
            Ku = [sb(g0, "gKu%d" % d, [64, NCH, 128], BF16) for d in range(2)]
            dec = sb(g0, "gdec", [128, 2, NCH])
            vtm = sb(g0, "gv", [64, NCH, 128], BF16); gs = sb(g0, "ggs", [64, NLCH, 128], BF16); oacc = sb(g0, "goacc", [64, NLCH, 128])
            qf = sb(g0, "gqf", [128, 512]); sg = sb(g0, "gsg", [128, 512]); lf = sb(g0, "glf", [128, 512]); key = sb(g0, "gkey", [128, 512])
            Bc = sb(g0, "gB", [128, 512]); T1 = sb(g0, "gT1", [128, 512]); T2 = sb(g0, "gT2", [128, 512]); khT = sb(g0, "gkhT", [128, 512], BF16)
            EE = [sb(g0, "gE%d" % i, [128, 512]) for i in range(4)]
            sq = sb(g0, "gsq", [64, NLCH, 128], BF16); ss = sb(g0, "gss", [64, NLCH])
            for hd in range(4):
                with ExitStack() as hs:
                    wq_ = load_w(hs, "gwq", 0 + hd * 128, 128); wi_ = load_w(hs, "gwi", 512 + hd * 128, 128); wg_ = load_w(hs, "gwg", 1024 + hd * 128, 128)
                    wf = [load_w(hs, "gwf0", 1536 + hd * 128, 128), load_w(hs, "gwf1", 2048 + hd * 128, 128)]
                    prep_tables(64)
                    for g in range(9):
                        bk = 3 + g % 2
                        proj_tm_group(wi_, "gwi", 128, bk, g * 4, 4)
                        S.op('act', lambda e, g=g, bk=bk: e.copy(out=vtm[:, g * 4:(g + 1) * 4, :], in_=PB[bk][0:64, :].rearrange("p (j c) -> p j c", c=128)),
                             reads=['pb%d' % bk], writes=['gv'])
                    for g in range(8):
                        bk = 3 + (g + 1) % 2
                        proj_tm_group(wg_, "gwg", 128, bk, 4 + g * 4, 4)
                        S.op('act', lambda e, g=g, bk=bk: e.activation(out=gs[:, g * 4:(g + 1) * 4, :], in_=PB[bk][0:64, :].rearrange("p (j c) -> p j c", c=128), func=AF.Silu),
                             reads=['pb%d' % bk], writes=['ggs'])
                    for (t0, nt) in BLKS:
                        nck = nt // 64; c0 = t0 // 64
                        proj_fm_block(wq_, "gwq", 128, 0, t0, nt)
                        S.op('act', lambda e, nt=nt: e.copy(out=qf[:, 0:nt], in_=PB[0][:, 0:nt]), reads=['pb0'], writes=['gqf'])
                        for d in range(2):
                            proj_fm_block(wf[d], "gwf%d" % d, 128, 1 + d, t0, nt)
                            col = d * 4 + hd
                            lbp = lbT[:, d, hd:hd + 1]; omp = omlT[:, d, hd:hd + 1]; nomp = nomlT[:, d, hd:hd + 1]
                            S.op('act', lambda e, d=d, nt=nt: e.activation(out=sg[:, 0:nt], in_=PB[1 + d][:, 0:nt], func=AF.Sigmoid), reads=['pb%d' % (1 + d)], writes=['gsg'])
                            S.op('act', lambda e, nt=nt, lbp=lbp, omp=omp: e.activation(out=lf[:, 0:nt], in_=sg[:, 0:nt], func=AF.Ln, bias=lbp, scale=omp),
                                 reads=['gsg', 'lbT', 'omlT'], writes=['glf'])
                            S.op('dve', lambda e, nt=nt, nomp=nomp, omp=omp: e.tensor_scalar(out=key[:, 0:nt], in0=sg[:, 0:nt], scalar1=nomp, scalar2=omp, op0=ALU.mult, op1=ALU.add),
                                 reads=['gsg', 'omlT', 'nomlT'], writes=['gkey'])
                            S.op('dve', lambda e, nt=nt: e.tensor_tensor_scan(out=Bc[:, 0:nt], data0=rmask[:, 0:nt], data1=lf[:, 0:nt], initial=0.0, op0=ALU.mult, op1=ALU.add),
                                 reads=['glf', 'rmask'], writes=['gB'])
                            B3 = Bc[:, 0:nt].rearrange("p (n c) -> p n c", c=64)
                            T3 = T1[:, 0:nt].rearrange("p (n c) -> p n c", c=64)
                            if d == 1:
                                S.op('dve', lambda e, B3=B3, T3=T3, nck=nck: e.tensor_tensor(out=T3, in0=B3[:, :, 63:64].to_broadcast([128, nck, 64]), in1=B3, op=ALU.subtract),
                                     reads=['gB'], writes=['gT1'])
                                S.op('dve', lambda e, nt=nt: e.tensor_tensor(out=Bc[:, 0:nt], in0=T1[:, 0:nt], in1=lf[:, 0:nt], op=ALU.add), reads=['gT1', 'glf'], writes=['gB'])
                            li = 63 if d == 0 else 0
                            S.op('act', lambda e, B3=B3, li=li, d=d, c0=c0, nck=nck: e.activation(out=dec[:, d, c0:c0 + nck], in_=B3[:, :, li], func=AF.Exp), reads=['gB'], writes=['gdec'])
                            T4 = T2[:, 0:nt].rearrange("p (n c) -> p n c", c=64)
                            S.op('dve', lambda e, B3=B3, T3=T3, nck=nck: e.tensor_tensor(out=T3, in0=B3, in1=B3[:, :, 32:33].to_broadcast([128, nck, 64]), op=ALU.subtract),
                                 reads=['gB'], writes=['gT1'])
                            S.op('dve', lambda e, B3=B3, T4=T4, nck=nck, li=li: e.tensor_tensor(out=T4, in0=B3[:, :, li:li + 1].to_broadcast([128, nck, 64]), in1=B3, op=ALU.subtract),
                                 reads=['gB'], writes=['gT2'])
                            S.op('act', lambda e, nt=nt: e.activation(out=EE[0][:, 0:nt], in_=T1[:, 0:nt], func=AF.Exp), reads=['gT1'], writes=['gE0'])
                            S.op('act', lambda e, nt=nt: e.activation(out=EE[1][:, 0:nt], in_=T1[:, 0:nt], func=AF.Exp, scale=-1.0), reads=['gT1'], writes=['gE1'])
                            S.op('act', lambda e, nt=nt: e.activation(out=EE[2][:, 0:nt], in_=Bc[:, 0:nt], func=AF.Exp), reads=['gB'], writes=['gE2'])
                            S.op('act', lambda e, nt=nt: e.activation(out=EE[3][:, 0:nt], in_=T2[:, 0:nt], func=AF.Exp), reads=['gT2'], writes=['gE3'])
                            S.op('dve', lambda e, nt=nt, t0=t0, d=d: e.tensor_tensor(out=QsT[d][:, t0:t0 + nt], in0=qf[:, 0:nt], in1=EE[0][:, 0:nt], op=ALU.mult),
                                 reads=['gqf', 'gE0'], writes=['gQsT%d' % d])
                            S.op('dve', lambda e, nt=nt, t0=t0, d=d: e.tensor_tensor(out=KsT[d][:, t0:t0 + nt], in0=key[:, 0:nt], in1=EE[1][:, 0:nt], op=ALU.mult),
                                 reads=['gkey', 'gE1'], writes=['gKsT%d' % d])
                            S.op('dve', lambda e, nt=nt, t0=t0, d=d: e.tensor_tensor(out=QoT[d][:, t0:t0 + nt], in0=qf[:, 0:nt], in1=EE[2][:, 0:nt], op=ALU.mult),
                                 reads=['gqf', 'gE2'], writes=['gQoT%d' % d])
                            S.op('dve', lambda e, nt=nt: e.tensor_tensor(out=khT[:, 0:nt], in0=key[:, 0:nt], in1=EE[3][:, 0:nt], op=ALU.mult), reads=['gkey', 'gE3'], writes=['gkhT'])
                            pbb = PB[3][:].bitcast(BF16)
                            for j in range(nck):
                                S.op('pe', lambda e, j=j: e.transpose(pbb[0:64, j * 128:(j + 1) * 128], khT[:, j * 64:(j + 1) * 64], identb[:]),
                                     reads=['gkhT', 'identb'], writes=['pb3'], skip_self=True)
                            S.op('act', lambda e, d=d, c0=c0, nck=nck: e.copy(out=Ku[d][:, c0:c0 + nck, :], in_=pbb[0:64, 0:nck * 128].rearrange("p (j c) -> p j c", c=128)),
                                 reads=['pb3'], writes=['gKu%d' % d])
                    for d in range(2):
                        def evac(nl, n, po, pok, d=d):
                            if d == 0:
                                S.op('act', lambda e: e.copy(out=oacc[:, nl, :], in_=po), reads=[pok], writes=K('goacc', nl))
                            else:
                                S.op('dve', lambda e: e.tensor_tensor(out=oacc[:, nl, :], in0=po, in1=oacc[:, nl, :], op=ALU.add), reads=[pok] + K('goacc', nl), writes=K('goacc', nl))
                        chunk_loop(d, KsT[d], QsT[d], vtm, QoT[d], Ku[d], vtm, lambda n, d=d: dec[:, d, n:n + 1], 128, evac,
                                   ['gQsT%d' % d, 'gKsT%d' % d, 'gQoT%d' % d, 'gKu%d' % d, 'gv'], 'gdec')
                    allo = K('goacc', 0, NLCH)
                    S.op('dve', lambda e: e.tensor_tensor(out=sq[:], in0=oacc[:], in1=oacc[:], op=ALU.mult), reads=allo, writes=['gsq'])
                    S.op('dve', lambda e: e.tensor_reduce(out=ss[:], in_=sq[:], axis=AX.X, op=ALU.add), reads=['gsq'], writes=['gss'])
                    S.op('act', lambda e: e.activation(out=ss[:], in_=ss[:], func=AF.Sqrt, bias=epsc[0:64, 0:1], scale=1.0 / 128.0), reads=['gss', 'epsc'], writes=['gss'])
                    S.op('dve', lambda e: e.reciprocal(out=ss[:], in_=ss[:]), reads=['gss'], writes=['gss'])
                    S.op('dve', lambda e: e.tensor_tensor(out=oacc[:], in0=oacc[:], in1=ss[:].unsqueeze(2).to_broadcast([64, NLCH, 128]), op=ALU.mult),
                         reads=allo + ['gss'], writes=allo)
                    S.op('dve', lambda e, hd=hd: e.tensor_tensor(out=oacc[:], in0=oacc[:], in1=hgn[:, hd * 128:(hd + 1) * 128].unsqueeze(1).to_broadcast([64, NLCH, 128]), op=ALU.mult),
                         reads=allo + ['hgn'], writes=allo)
                    S.op('dve', lambda e: e.tensor_tensor(out=oacc[:], in0=oacc[:], in1=gs[:], op=ALU.mult), reads=allo + ['ggs'], writes=allo)
                    transpose_chunks_to_mixT(oacc, allo, hd)
            S.barrier()

        with ExitStack() as m0:
            mln = sb(m0, "mln", [64, 512]); convw = sb(m0, "convw", [128, 9, 8]); convb = sb(m0, "convb", [128, 8])
            gateb = sb(m0, "gateb", [8, 2]); sel8 = sb(m0, "sel8", [8, 8]); dirm = sb(m0, "dirm", [8, 2])
            S.dma('sp', lambda e: e.dma_start(out=mln[:], in_=mln_d[:, :]), writes=['mln'])
            S.dma('sp', lambda e: e.dma_start(out=convw[:], in_=convw_d[:, :, :]), writes=['convw'])
            S.dma('sp', lambda e: e.dma_start(out=convb[:], in_=convb_d[:, :]), writes=['convb'])
            S.dma('sp', lambda e: e.dma_start(out=gateb[:], in_=gateb_d[:, :]), writes=['gateb'])
            S.dma('sp', lambda e: e.dma_start(out=sel8[:], in_=sel8_d[:, :]), writes=['sel8'])
            S.dma('sp', lambda e: e.dma_start(out=dirm[:], in_=dirm_d[:, :]), writes=['dirm'])
            RUU = sb(m0, "RUU", [64, NCH, 24]); dchunk = sb(m0, "dchunk", [128, 8, NCH])
            with ExitStack() as gp:
                wgi = load_w(gp, "mwgi", 4608, 8); wgf = load_w(gp, "mwgf", 4616, 8)
                LI = sb(gp, "LI", [8, NTOK]); LF = sb(gp, "LF", [8, NTOK]); Af = sb(gp, "Af", [8, NTOK]); Ab = sb(gp, "Ab", [8, NTOK]); Aa = sb(gp, "Aa", [8, NTOK])
                R = [sb(gp, "Rr%d" % i, [8, NTOK]) for i in range(3)]
                bd = sb(gp, "bd", [8, 8, NCH])
                for (t0, nt) in BLKS:
                    proj_fm_block(wgi, "mwgi", 8, 0, t0, nt)
                    S.op('act', lambda e, t0=t0, nt=nt: e.copy(out=LI[:, t0:t0 + nt], in_=PB[0][0:8, 0:nt]), reads=['pb0'], writes=['LI'])
                    proj_fm_block(wgf, "mwgf", 8, 1, t0, nt)
                    S.op('act', lambda e, t0=t0, nt=nt: e.copy(out=LF[:, t0:t0 + nt], in_=PB[1][0:8, 0:nt]), reads=['pb1'], writes=['LF'])
                S.op('dve', lambda e: e.tensor_scalar_add(out=LI[:], in0=LI[:], scalar1=gateb[:, 0:1]), reads=['LI', 'gateb'], writes=['LI'])
                S.op('act', lambda e: e.activation(out=LF[:], in_=LF[:], func=AF.Sigmoid, bias=gateb[:, 1:2], scale=1.0), reads=['LF', 'gateb'], writes=['LF'])
                S.op('act', lambda e: e.activation(out=LF[:], in_=LF[:], func=AF.Ln), reads=['LF'], writes=['LF'])
                for (t0, nt) in BLKS:
                    S.op('dve', lambda e, t0=t0, nt=nt: e.tensor_tensor_scan(out=Af[:, t0:t0 + nt], data0=rmask[0:8, 0:nt], data1=LF[:, t0:t0 + nt], initial=0.0, op0=ALU.mult, op1=ALU.add),
                         reads=['LF', 'rmask'], writes=['Af'])
                A3 = Af[:].rearrange("p (n c) -> p n c", c=64)
                tot = A3[:, :, 63:64]
                S.op('dve', lambda e: e.tensor_tensor(out=Ab[:].rearrange("p (n c) -> p n c", c=64), in0=tot.to_broadcast([8, NCH, 64]), in1=A3, op=ALU.subtract), reads=['Af'], writes=['Ab'])
                S.op('dve', lambda e: e.tensor_tensor(out=Ab[:], in0=Ab[:], in1=LF[:], op=ALU.add), reads=['Ab', 'LF'], writes=['Ab'])
                S.op('dve', lambda e: e.tensor_scalar_mul(out=Aa[:], in0=Af[:], scalar1=dirm[:, 0:1]), reads=['Af', 'dirm'], writes=['Aa'])
                S.op('dve', lambda e: e.scalar_tensor_tensor(out=Aa[:], in0=Ab[:], scalar=dirm[:, 1:2], in1=Aa[:], op0=ALU.mult, op1=ALU.add), reads=['Ab', 'dirm', 'Aa'], writes=['Aa'])
                S.op('act', lambda e: e.activation(out=R[0][:], in_=Aa[:], func=AF.Exp), reads=['Aa'], writes=['Rr0'])
                S.op('dve', lambda e: e.tensor_tensor(out=Ab[:], in0=LI[:], in1=Aa[:], op=ALU.subtract), reads=['LI', 'Aa', 'Ab'], writes=['Ab'])
                S.op('act', lambda e: e.activation(out=R[1][:], in_=Ab[:], func=AF.Exp), reads=['Ab'], writes=['Rr1'])
                S.op('dve', lambda e: e.tensor_tensor(out=Aa[:].rearrange("p (n c) -> p n c", c=64), in0=Ab[:].rearrange("p (n c) -> p n c", c=64), in1=tot.to_broadcast([8, NCH, 64]), op=ALU.add),
                     reads=['Ab', 'Af', 'Aa', 'Rr0'], writes=['Aa'])
                S.op('act', lambda e: e.activation(out=R[2][:], in_=Aa[:], func=AF.Exp), reads=['Aa'], writes=['Rr2'])
                for half in range(2):
                    for j in range(18):
                        n = half * 18 + j
                        for q in range(3):
                            S.op('pe', lambda e, n=n, j=j, q=q: e.transpose(PB[2][0:64, j * 24 + q * 8:j * 24 + q * 8 + 8], R[q][:, n * 64:(n + 1) * 64], ident[0:8, 0:8]),
                                 reads=['Rr%d' % q, 'ident'], writes=['pb2'], skip_self=True)
                    S.op('act', lambda e, half=half: e.copy(out=RUU[:, half * 18:(half + 1) * 18, :], in_=PB[2][0:64, 0:432].rearrange("p (j c) -> p j c", c=24)),
                         reads=['pb2'], writes=['RUU'])
                S.op('dve', lambda e: e.tensor_tensor(out=bd[:], in0=tot.rearrange("p n c -> p c n").to_broadcast([8, 8, NCH]), in1=sel8[:].unsqueeze(2).to_broadcast([8, 8, NCH]), op=ALU.mult),
                     reads=['Af', 'sel8'], writes=['bd'])
                S.op('pe', lambda e: e.matmul(PB[3][:, 0:288], lhsT=ones[0:8, :], rhs=bd[:].rearrange("p a n -> p (a n)"), start=True, stop=True), reads=['ones', 'bd'], writes=['pb3'], skip_self=True)
                S.op('act', lambda e: e.activation(out=dchunk[:].rearrange("p a n -> p (a n)"), in_=PB[3][:, 0:288], func=AF.Exp), reads=['pb3'], writes=['dchunk'])
                S.barrier()
            qc = sb(m0, "mqc", [128, NTOK], BF16); kc_ = sb(m0, "mkc", [128, NTOK], BF16); kTM = sb(m0, "mkTM", [64, NCH, 128], BF16)
            raw = sb(m0, "mraw", [128, NTOK]); acc = sb(m0, "macc", [128, NTOK])
            vext = sb(m0, "mvext", [64, NCH, 132], BF16); vi = sb(m0, "mvi", [64, NCH, 132], BF16); vu = sb(m0, "mvu", [64, NCH, 132], BF16)
            og = sb(m0, "mog", [64, NLCH, 128], BF16); oext = sb(m0, "moext", [64, NLCH, 132]); obuf = sb(m0, "mobuf", [64, NLCH, 128])
            den = sb(m0, "mden", [64, NLCH]); mu = sb(m0, "mmu", [64, NLCH]); m2 = sb(m0, "mm2", [64, NLCH])
            S.op('dve', lambda e: e.memset(vext[:], 1.0), writes=['mvext'])
            for hd in range(4):
                with ExitStack() as hs:
                    wq_ = load_w(hs, "mwq", 2560 + hd * 128, 128); wk_ = load_w(hs, "mwk", 3072 + hd * 128, 128)
                    wv_ = load_w(hs, "mwv", 3584 + hd * 128, 128); wo_ = load_w(hs, "mwo", 4096 + hd * 128, 128)
                    prep_tables(64)
                    for g in range(9):
                        bk = 3 + g % 2
                        proj_tm_group(wv_, "mwv", 128, bk, g * 4, 4)
                        S.op('act', lambda e, g=g, bk=bk: e.copy(out=vext[:, g * 4:(g + 1) * 4, 0:128], in_=PB[bk][0:64, :].rearrange("p (j c) -> p j c", c=128)),
                             reads=['pb%d' % bk], writes=['mvext'])
                    for g in range(8):
                        bk = 3 + (g + 1) % 2
                        proj_tm_group(wo_, "mwo", 128, bk, 4 + g * 4, 4)
                        S.op('act', lambda e, g=g, bk=bk: e.activation(out=og[:, g * 4:(g + 1) * 4, :], in_=PB[bk][0:64, :].rearrange("p (j c) -> p j c", c=128), func=AF.Sigmoid),
                             reads=['pb%d' % bk], writes=['mog'])
                    for qi, (wb, wn, dstb) in enumerate(((wq_, "mwq", qc), (wk_, "mwk", kc_))):
                        chn = qi * 4 + hd
                        for bi, (t0, nt) in enumerate(BLKS):
                            proj_fm_block(wb, wn, 128, bi % 2, t0, nt)
                            S.op('act', lambda e, t0=t0, nt=nt, bi=bi: e.copy(out=raw[:, t0:t0 + nt], in_=PB[bi % 2][:, 0:nt]), reads=['pb%d' % (bi % 2)], writes=['mraw'])
                        S.op('dve', lambda e, chn=chn: e.tensor_scalar(out=acc[:, 0:256], in0=raw[:, 0:256], scalar1=convw[:, 4, chn:chn + 1], scalar2=convb[:, chn:chn + 1], op0=ALU.mult, op1=ALU.add),
                             reads=['mraw', 'convw', 'convb'], writes=['macc'])
                        S.op('dve', lambda e, chn=chn: e.scalar_tensor_tensor(out=acc[:, 1:256], in0=raw[:, 0:255], scalar=convw[:, 3, chn:chn + 1], in1=acc[:, 1:256], op0=ALU.mult, op1=ALU.add),
                             reads=['mraw', 'convw', 'macc'], writes=['macc'])
                        S.op('dve', lambda e, chn=chn: e.scalar_tensor_tensor(out=acc[:, 0:255], in0=raw[:, 1:256], scalar=convw[:, 5, chn:chn + 1], in1=acc[:, 0:255], op0=ALU.mult, op1=ALU.add),
                             reads=['mraw', 'convw', 'macc'], writes=['macc'])
                        X = raw[:, 256:NTOK].rearrange("p (r c) -> p r c", c=64); Y = acc[:, 256:NTOK].rearrange("p (r c) -> p r c", c=64)
                        S.op('dve', lambda e, chn=chn: e.tensor_scalar(out=acc[:, 256:NTOK], in0=raw[:, 256:NTOK], scalar1=convw[:, 4, chn:chn + 1], scalar2=convb[:, chn:chn + 1], op0=ALU.mult, op1=ALU.add),
                             reads=['mraw', 'convw', 'convb', 'macc'], writes=['macc'])
                        for ky in range(3):
                            for kx in range(3):
                                if ky == 1 and kx == 1:
                                    continue
                                dy = ky - 1; dx = kx - 1
                                r0 = max(0, -dy); r1 = 32 - max(0, dy); c0 = max(0, -dx); c1 = 64 - max(0, dx)
                                S.op('dve', lambda e, chn=chn, ky=ky, kx=kx, r0=r0, r1=r1, c0=c0, c1=c1, dy=dy, dx=dx: e.scalar_tensor_tensor(
                                    out=Y[:, r0:r1, c0:c1], in0=X[:, r0 + dy:r1 + dy, c0 + dx:c1 + dx], scalar=convw[:, ky * 3 + kx, chn:chn + 1], in1=Y[:, r0:r1, c0:c1],
                                    op0=ALU.mult, op1=ALU.add), reads=['mraw', 'convw', 'macc'], writes=['macc'])
                        S.op('act', lambda e: e.activation(out=acc[:], in_=acc[:], func=AF.Silu), reads=['macc'], writes=['macc'])
                        if qi == 0:
                            S.op('dve', lambda e: e.tensor_copy(out=qc[:], in_=acc[:]), reads=['macc'], writes=['mqc'])
                        else:
                            S.op('dve', lambda e: e.tensor_scalar_mul(out=kc_[:], in0=acc[:], scalar1=128.0 ** -0.5), reads=['macc'], writes=['mkc'])
                    pbb = PB[3][:].bitcast(BF16)
                    for g in range(5):
                        nck = 8 if g < 4 else 4
                        for j in range(nck):
                            n = g * 8 + j
                            S.op('pe', lambda e, j=j, n=n: e.transpose(pbb[0:64, j * 128:(j + 1) * 128], kc_[:, n * 64:(n + 1) * 64], identb[:]),
                                 reads=['mkc', 'identb'], writes=['pb3'], skip_self=True)
                        S.op('act', lambda e, g=g, nck=nck: e.copy(out=kTM[:, g * 8:g * 8 + nck, :], in_=pbb[0:64, 0:nck * 128].rearrange("p (j c) -> p j c", c=128)),
                             reads=['pb3'], writes=['mkTM'])
                    for d in range(2):
                        row = d * 4 + hd
                        S.op('dve', lambda e, row=row: e.tensor_tensor(out=vi[:], in0=vext[:], in1=RUU[:, :, 8 + row:9 + row].to_broadcast([64, NCH, 132]), op=ALU.mult),
                             reads=['mvext', 'RUU'], writes=['mvi'])
                        S.op('dve', lambda e, row=row: e.tensor_tensor(out=vu[:], in0=vext[:], in1=RUU[:, :, 16 + row:17 + row].to_broadcast([64, NCH, 132]), op=ALU.mult),
                             reads=['mvext', 'RUU'], writes=['mvu'])

                        def evac(nl, n, po, pok, row=row):
                            S.op('act', lambda e: e.copy(out=oext[:, nl, 0:129], in_=po), reads=[pok], writes=K('moext', nl))
                        chunk_loop(d, kc_, qc, vi, qc, kTM, vu, lambda n, row=row: dchunk[:, row, n:n + 1], 129, evac,
                                   ['mqc', 'mkc', 'mvi', 'mvu', 'mkTM'], 'dchunk')
                        allx = K('moext', 0, NLCH)
                        S.op('dve', lambda e, row=row: e.tensor_tensor(out=oext[:, :, 0:129], in0=oext[:, :, 0:129], in1=RUU[:, 4:NCH, row:row + 1].to_broadcast([64, NLCH, 129]), op=ALU.mult),
                             reads=allx + ['RUU'], writes=allx)
                        S.op('act', lambda e: e.activation(out=den[:], in_=oext[:, :, 128], func=AF.Abs), reads=allx, writes=['mden'])
                        S.op('dve', lambda e: e.tensor_scalar_max(out=den[:], in0=den[:], scalar1=1.0), reads=['mden'], writes=['mden'])
                        S.op('dve', lambda e: e.reciprocal(out=den[:], in_=den[:]), reads=['mden'], writes=['mden'])
                        if d == 0:
                            S.op('dve', lambda e: e.tensor_tensor(out=obuf[:], in0=oext[:, :, 0:128], in1=den[:].unsqueeze(2).to_broadcast([64, NLCH, 128]), op=ALU.mult),
                                 reads=allx + ['mden'], writes=['mobuf'])
                        else:
                            S.op('dve', lambda e: e.tensor_tensor(out=oext[:, :, 0:128], in0=oext[:, :, 0:128], in1=den[:].unsqueeze(2).to_broadcast([64, NLCH, 128]), op=ALU.mult),
                                 reads=allx + ['mden'], writes=allx)
                            S.op('dve', lambda e: e.tensor_tensor(out=obuf[:], in0=obuf[:], in1=oext[:, :, 0:128], op=ALU.add), reads=allx + ['mobuf'], writes=['mobuf'])
                    allx = K('moext', 0, NLCH)
                    S.op('dve', lambda e: e.tensor_reduce(out=mu[:], in_=obuf[:], axis=AX.X, op=ALU.add), reads=['mobuf'], writes=['mmu'])
                    S.op('dve', lambda e: e.tensor_scalar_mul(out=mu[:], in0=mu[:], scalar1=1.0 / 128.0), reads=['mmu'], writes=['mmu'])
                    S.op('dve', lambda e: e.tensor_tensor(out=obuf[:], in0=obuf[:], in1=mu[:].unsqueeze(2).to_broadcast([64, NLCH, 128]), op=ALU.subtract), reads=['mobuf', 'mmu'], writes=['mobuf'])
                    S.op('dve', lambda e: e.tensor_tensor(out=oext[:, :, 0:128], in0=obuf[:], in1=obuf[:], op=ALU.mult), reads=['mobuf'] + allx, writes=allx)
                    S.op('dve', lambda e: e.tensor_reduce(out=m2[:], in_=oext[:, :, 0:128], axis=AX.X, op=ALU.add), reads=allx, writes=['mm2'])
                    S.op('act', lambda e: e.activation(out=m2[:], in_=m2[:], func=AF.Sqrt, bias=epsc[0:64, 0:1], scale=1.0 / 128.0), reads=['mm2', 'epsc'], writes=['mm2'])
                    S.op('dve', lambda e: e.reciprocal(out=m2[:], in_=m2[:]), reads=['mm2'], writes=['mm2'])
                    S.op('dve', lambda e: e.tensor_tensor(out=obuf[:], in0=obuf[:], in1=m2[:].unsqueeze(2).to_broadcast([64, NLCH, 128]), op=ALU.mult), reads=['mobuf', 'mm2'], writes=['mobuf'])
                    S.op('dve', lambda e, hd=hd: e.tensor_tensor(out=obuf[:], in0=obuf[:], in1=mln[:, hd * 128:(hd + 1) * 128].unsqueeze(1).to_broadcast([64, NLCH, 128]), op=ALU.mult),
                         reads=['mobuf', 'mln'], writes=['mobuf'])
                    S.op('dve', lambda e: e.tensor_tensor(out=obuf[:], in0=obuf[:], in1=og[:], op=ALU.mult), reads=['mobuf', 'mog'], writes=['mobuf'])
                    transpose_chunks_to_mixT(obuf, ['mobuf'], 4 + hd)
            S.barrier()

        if debug == 'mix':
            with ExitStack() as dd:
                mf = sb(dd, "mf", [128, NLAT])
                for h in range(8):
                    S.op('dve', lambda e, h=h: e.tensor_copy(out=mf[:], in_=mixT[:, h, :]), reads=K('mixT', h) + ['mf'], writes=['mf'])
                    S.dma('sp', lambda e, h=h: e.dma_start(out=dbg_d[h * 128:(h + 1) * 128, :], in_=mf[:]), reads=['mf'], writes=['dbg'])
                S.wait_all('sp', ['dbg']); S.barrier()
            scB.close(); scA.close()
            return nc
        prep_tables(512)
        S.barrier()
        scB.close()
        x1s = nc.dram_tensor("x1s", [NLAT, 1024], F32, kind="Internal").ap()

        def bcast_tile(dst, dkey, c0, dg):
            for ch in range(8):
                S.op('dve', lambda e, ch=ch: e.tensor_scalar_mul(out=dg[:], in0=ident[:], scalar1=modv[:, c0 + ch, 0:1]), reads=['ident', 'modv', 'dg'], writes=['dg'])
                bank = ch // 4
                S.op('pe', lambda e, ch=ch, bank=bank: e.matmul(PB[bank][:, (ch % 4) * 128:(ch % 4 + 1) * 128], lhsT=ones[:], rhs=dg[:], start=True, stop=True),
                     reads=['ones', 'dg'], writes=['pb%d' % bank], skip_self=True)
            S.op('act', lambda e: e.copy(out=dst[:, 0:512], in_=PB[0][:, :]), reads=['pb0'], writes=[dkey])
            S.op('act', lambda e: e.copy(out=dst[:, 512:1024], in_=PB[1][:, :]), reads=['pb1'], writes=[dkey])

        with ExitStack() as p3:
            bct = {}
            for nm in ("g1b", "ln1g", "ln1b"):
                bct[nm] = sb(p3, nm, [128, 1024])
            for nm, dd in (("ln1g", ln1g_d), ("ln1b", ln1b_d)):
                S.dma('sp', lambda e, nm=nm, dd=dd: e.dma_start(out=bct[nm][:], in_=dd[:, :]), writes=[nm])
            dg = sb(p3, "dg", [128, 128])
            bcast_tile(bct["g1b"], "g1b", 16, dg)
            wo_b = sb(p3, "wout", [128, 8, 1024], BF16); wos = sb(p3, "wos", [128, 1024])
            for kc in range(8):
                S.dma('sp', lambda e, kc=kc: e.dma_start(out=wos[:], in_=wout_d[:, kc, :]), writes=['wos'])
                S.op('pool', lambda e, kc=kc: e.tensor_copy(out=wo_b[:, kc, :], in_=wos[:]), reads=['wos'], writes=['wout'])
            xt = [sb(p3, "x3t%d" % i, [128, 1024]) for i in range(2)]
            t1 = [sb(p3, "t1_%d" % i, [128, 1024]) for i in range(2)]
            st = [sb(p3, "st3%d" % i, [128, 2, 6]) for i in range(2)]
            mv = [sb(p3, "mv3%d" % i, [128, 2]) for i in range(2)]
            rs = [sb(p3, "rs3%d" % i, [128, 1]) for i in range(2)]
            for t in range(16):
                b = t % 2
                S.dma('sp' if b == 0 else 'pool', lambda e, t=t, b=b: e.dma_start(out=xt[b][:], in_=xs[256 + t * 128:256 + (t + 1) * 128, :]), writes=['x3t%d' % b])
                for half in range(2):
                    bank = 2 * b + half
                    for kc in range(8):
                        S.op('pe', lambda e, kc=kc, t=t, half=half, bank=bank: e.matmul(PB[bank][:, :], lhsT=mixT[:, kc, t * 128:(t + 1) * 128], rhs=wo_b[:, kc, half * 512:(half + 1) * 512],
                                                                                      start=(kc == 0), stop=(kc == 7)),
                             reads=K('mixT', kc) + ['wout'], writes=['pb%d' % bank], skip_self=True)
                    S.op('dve', lambda e, b=b, half=half, bank=bank: e.tensor_tensor(out=t1[b][:, half * 512:(half + 1) * 512], in0=PB[bank][:, :], in1=bct["g1b"][:, half * 512:(half + 1) * 512], op=ALU.mult),
                         reads=['pb%d' % bank, 'g1b'], writes=['t1_%d' % b])
                S.op('dve', lambda e, b=b: e.scalar_tensor_tensor(out=t1[b][:], in0=xt[b][:], scalar=ALPHA, in1=t1[b][:], op0=ALU.mult, op1=ALU.add),
                     reads=['x3t%d' % b, 't1_%d' % b], writes=['t1_%d' % b])
                layer_norm_rows(st[b], mv[b][:], rs[b][:], t1[b][:], t1[b][:], 't1_%d' % b, 't1_%d' % b, 'p3%d' % b)
                S.op('dve', lambda e, b=b: e.tensor_tensor(out=t1[b][:], in0=t1[b][:], in1=bct["ln1g"][:], op=ALU.mult), reads=['t1_%d' % b, 'ln1g'], writes=['t1_%d' % b])
                S.op('dve', lambda e, b=b, t=t: e.tensor_tensor(out=t1[b][:], in0=t1[b][:], in1=bct["ln1b"][:], op=ALU.add), reads=['t1_%d' % b, 'ln1b'], writes=['t1_%d' % b])
                S.dma('sp', lambda e, t=t, b=b: e.dma_start(out=x1s[t * 128:(t + 1) * 128, :], in_=t1[b][:]), reads=['t1_%d' % b], writes=K('x1_', t))
                if debug == 'x1':
                    S.dma('sp', lambda e, t=t, b=b: e.dma_start(out=dbg_d[t * 128:(t + 1) * 128, :], in_=t1[b][:]), reads=['t1_%d' % b], writes=['dbg'])
            S.barrier()
        scA.close()

        with ExitStack() as p4:
            bct = {}
            for nm in ("g2b", "sc2b", "sh2b", "ln2g", "ln2b"):
                bct[nm] = sb(p4, nm, [128, 1024])
            for nm, dd in (("ln2g", ln2g_d), ("ln2b", ln2b_d)):
                S.dma('sp', lambda e, nm=nm, dd=dd: e.dma_start(out=bct[nm][:], in_=dd[:, :]), writes=[nm])
            dg = sb(p4, "dg4", [128, 128])
            bcast_tile(bct["g2b"], "g2b", 40, dg); bcast_tile(bct["sc2b"], "sc2b", 32, dg); bcast_tile(bct["sh2b"], "sh2b", 24, dg)
            x1t = [sb(p4, "x1t%d" % i, [128, 1024]) for i in range(2)]
            wqb = sb(p4, "wqb", [128, 8, 2048], BF16)
            keysT = sb(p4, "keysT", [128, 16, 128], BF16)
            with ExitStack() as tmp:
                stg = sb(tmp, "wq_stg", [128, 2048])
                for kc in range(8):
                    S.dma('sp', lambda e, kc=kc: e.dma_start(out=stg[:], in_=wq_d[:, kc, :]), writes=['wq_stg'])
                    S.op('pool', lambda e, kc=kc: e.tensor_copy(out=wqb[:, kc, :], in_=stg[:]), reads=['wq_stg'], writes=['wqb'])
                kst = sb(tmp, "kst", [128, 16, 128])
                S.dma('sp', lambda e: e.dma_start(out=kst[:], in_=keysT_d[:, :, :]), writes=['kst'])
                S.op('pool', lambda e: e.tensor_copy(out=keysT[:], in_=kst[:]), reads=['kst'], writes=['keysT'])
                S.barrier()
            NG = int(os.environ.get('KNG', '16'))
            uvb = [sb(p4, "uvb%d" % i, [128, 2048], BF16) for i in range(NG)]
            gl = sb(p4, "gl", [128, 128])
            pr = [sb(p4, "pr%d" % i, [128, 1024], BF16) for i in range(3)]
            dgk = [sb(p4, "dgk%d" % i, [128, 128], BF16) for i in range(4)]
            h2 = sb(p4, "h2", [128, 1024]); h2b = [sb(p4, "h2b%d" % i, [128, 1024], BF16) for i in range(2)]
            h2T = sb(p4, "h2T", [128, 8, 128], BF16); qT = sb(p4, "qT", [128, 16, 128], BF16)
            sc = sb(p4, "sc", [128, 16, 128]); scw = sb(p4, "scw", [128, 16, 128])
            top = sb(p4, "top", [128, 16, 16]); topi = sb(p4, "topi", [128, 16, 16], U32); topf = sb(p4, "topf", [128, 16, 16])
            candw = sb(p4, "candw", [128, 8, 256])
            cand = View(scw[:].rearrange("p (h two) k -> p h (two k)", two=2)); eq = View(candw[:].rearrange("p h (a b) -> p (h a) b", b=16))
            best = sb(p4, "best", [128, 8, 16]); idxf = sb(p4, "idxf", [128, 128])
            posi = sb(p4, "posi", [128, 8, 16], U32); pai = sb(p4, "pai", [128, 128], U32); pbi = sb(p4, "pbi", [128, 128], U32)
            paf = sb(p4, "paf", [128, 128]); pbf = sb(p4, "pbf", [128, 128]); iaf = sb(p4, "iaf", [128, 128]); ibf = sb(p4, "ibf", [128, 128])
            iota16 = sb(p4, "iota16", [128, 16])
            S.dma('sp', lambda e: e.dma_start(out=iota16[:], in_=iota_d[:, :]), writes=['iota16'])
            gate = [sb(p4, "gate%d" % i, [128, 128]) for i in range(2)]
            idxi = [sb(p4, "idxi%d" % i, [128, 128], I32) for i in range(2)]
            dots = sb(p4, "dots", [128, 128]); junk = sb(p4, "junk", [128, 1024], BF16)
            zs = sb(p4, "zs", [128, 8]); nmx = sb(p4, "nmx", [128, 8])
            st = sb(p4, "st4", [128, 2, 6]); mv = sb(p4, "mv4", [128, 2]); rs = sb(p4, "rs4", [128, 1])
            fin = View(sc[:].rearrange("p c k -> p (c k)")[:, 0:1024]); yb = h2
            NT4 = 16 if debug != 'x1' else 0

            def prologue(t):
                p = t % 2
                xb_ = x1t[p]; x1k = 'x1t%d' % p
                S.dma('sp', lambda e: e.dma_start(out=xb_[:], in_=x1s[t * 128:(t + 1) * 128, :]), reads=K('x1_', t), writes=[x1k])
                S.op('dve', lambda e: e.memset(idxf[:], 0.0), writes=['idxf'])
                S.op('dve', lambda e: e.memset(zs[:], 0.0), writes=['zs'])
                layer_norm_rows(st, mv[:], rs[:], xb_[:], h2[:], x1k, 'h2', 'p4')
                S.op('dve', lambda e: e.tensor_tensor(out=h2[:], in0=h2[:], in1=bct["sc2b"][:], op=ALU.mult), reads=['h2', 'sc2b'], writes=['h2'])
                S.op('dve', lambda e: e.tensor_tensor(out=h2[:], in0=h2[:], in1=bct["sh2b"][:], op=ALU.add), reads=['h2', 'sh2b'], writes=['h2'])
                S.op('act', lambda e: e.copy(out=h2b[p][:], in_=h2[:]), reads=['h2'], writes=['h2b%d' % p])
                yield
                for half in range(2):
                    for c4 in range(4):
                        ch = half * 4 + c4
                        S.op('pe', lambda e, ch=ch, c4=c4, half=half: e.transpose(PB[half][:, c4 * 128:(c4 + 1) * 128], h2[:, ch * 128:(ch + 1) * 128], ident[:]),
                             reads=['h2', 'ident'], writes=['pb%d' % half], skip_self=True)
                    yield
                    S.op('act', lambda e, half=half: e.copy(out=h2T[:, half * 4:(half + 1) * 4, :], in_=PB[half][:, :].rearrange("p (j c) -> p j c", c=128)), reads=['pb%d' % half], writes=['h2T'])
                for g in range(4):
                    for c4 in range(4):
                        c = g * 4 + c4
                        for kc in range(8):
                            S.op('pe', lambda e, c=c, c4=c4, kc=kc, g=g: e.matmul(PB[2 + g][:, c4 * 128:(c4 + 1) * 128], lhsT=wqb[:, kc, c * 128:(c + 1) * 128], rhs=h2T[:, kc, :],
                                                                                start=(kc == 0), stop=(kc == 7)), reads=['wqb', 'h2T'], writes=['pb%d' % (2 + g)], skip_self=True)
                    yield
                    S.op('act' if g % 2 == 0 else 'dve', lambda e, g=g: (e.copy if g % 2 == 0 else e.tensor_copy)(out=qT[:, g * 4:(g + 1) * 4, :], in_=PB[2 + g][:, :].rearrange("p (j c) -> p j c", c=128)),
                         reads=['pb%d' % (2 + g)], writes=['qT'])
                for g in range(4):
                    for c4 in range(4):
                        c = g * 4 + c4
                        S.op('pe', lambda e, c=c, c4=c4, g=g: e.matmul(PB[2 + g][:, c4 * 128:(c4 + 1) * 128], lhsT=qT[:, c, :], rhs=keysT[:, c, :], start=True, stop=True),
                             reads=['qT', 'keysT'], writes=['pb%d' % (2 + g)], skip_self=True)
                    yield
                    S.op('act' if g % 2 == 0 else 'dve', lambda e, g=g: (e.copy if g % 2 == 0 else e.tensor_copy)(out=sc[:, g * 4:(g + 1) * 4, :], in_=PB[2 + g][:, :].rearrange("p (j c) -> p j c", c=128)),
                         reads=['pb%d' % (2 + g)], writes=['sc'])
                for c in range(16):
                    S.op('dve', lambda e, c=c: e.max(out=top[:, c, 0:8], in_=sc[:, c, :]), reads=['sc'], writes=['top'])
                    S.op('dve', lambda e, c=c: e.max_index(out=topi[:, c, 0:8], in_max=top[:, c, 0:8], in_values=sc[:, c, :]), reads=['sc', 'top'], writes=['topi'])
                    S.op('dve', lambda e, c=c: e.match_replace(out=scw[:, c, :], in_to_replace=top[:, c, 0:8], in_values=sc[:, c, :], imm_value=-1e30), reads=['sc', 'top'], writes=['scw'])
                    S.op('dve', lambda e, c=c: e.max(out=top[:, c, 8:16], in_=scw[:, c, :]), reads=['scw'], writes=['top'])
                    S.op('dve', lambda e, c=c: e.max_index(out=topi[:, c, 8:16], in_max=top[:, c, 8:16], in_values=scw[:, c, :]), reads=['scw', 'top'], writes=['topi'])
                    yield
                S.op('dve', lambda e: e.tensor_copy(out=topf[:], in_=topi[:]), reads=['topi'], writes=['topf'])
                t4 = top[:].rearrange("p (h two) k -> p h two k", two=2); f4 = topf[:].rearrange("p (h two) k -> p h two k", two=2)
                c4v = cand[:].rearrange("p h (a b) -> p h a b", b=16)
                for h in range(8):
                    S.op('dve', lambda e, h=h: e.tensor_tensor(out=c4v[:, h, :, :], in0=t4[:, h, 0, :].unsqueeze(2).to_broadcast([128, 16, 16]), in1=t4[:, h, 1, :].unsqueeze(1).to_broadcast([128, 16, 16]), op=ALU.add),
                         reads=['top'], writes=['scw'])
                    yield
                for h in range(8):
                    S.op('dve', lambda e, h=h: e.max(out=best[:, h, 0:8], in_=cand[:, h, :]), reads=['scw'], writes=['best'])
                    S.op('dve', lambda e, h=h: e.match_replace(out=candw[:, h, :], in_to_replace=best[:, h, 0:8], in_values=cand[:, h, :], imm_value=-1e30), reads=['scw', 'best'], writes=['candw'])
                    S.op('dve', lambda e, h=h: e.max(out=best[:, h, 8:16], in_=candw[:, h, :]), reads=['candw'], writes=['best'])
                    S.op('dve', lambda e, h=h: e.max_index(out=posi[:, h, 0:8], in_max=best[:, h, 0:8], in_values=cand[:, h, :]), reads=['scw', 'best'], writes=['posi'])
                    S.op('dve', lambda e, h=h: e.max_index(out=posi[:, h, 8:16], in_max=best[:, h, 8:16], in_values=candw[:, h, :]), reads=['candw', 'best'], writes=['posi'])
                    yield
                pflat = posi[:].rearrange("p h k -> p (h k)")
                S.op('dve', lambda e: e.tensor_single_scalar(out=pai[:], in_=pflat, scalar=4, op=ALU.logical_shift_right), reads=['posi'], writes=['pai'])
                S.op('dve', lambda e: e.tensor_single_scalar(out=pbi[:], in_=pflat, scalar=15, op=ALU.bitwise_and), reads=['posi'], writes=['pbi'])
                S.op('dve', lambda e: e.tensor_copy(out=paf[:], in_=pai[:]), reads=['pai'], writes=['paf'])
                S.op('dve', lambda e: e.tensor_copy(out=pbf[:], in_=pbi[:]), reads=['pbi'], writes=['pbf'])
                yield
                eq4 = eq[:].rearrange("p (h k) a -> p h k a", k=16)
                for (pp, pk_, half_, dst, dn) in ((paf, 'paf', 0, iaf, 'iaf'), (pbf, 'pbf', 1, ibf, 'ibf')):
                    S.op('dve', lambda e, pp=pp: e.tensor_tensor(out=eq[:], in0=iota16[:].unsqueeze(1).to_broadcast([128, 128, 16]), in1=pp[:].unsqueeze(2).to_broadcast([128, 128, 16]), op=ALU.is_equal),
                         reads=['iota16', pk_, 'candw'], writes=['candw'])
                    S.op('dve', lambda e, half_=half_: e.tensor_tensor(out=eq4, in0=eq4, in1=f4[:, :, half_, :].unsqueeze(2).to_broadcast([128, 8, 16, 16]), op=ALU.mult),
                         reads=['candw', 'topf'], writes=['candw'])
                    S.op('dve', lambda e, dst=dst: e.tensor_reduce(out=dst[:], in_=eq[:], axis=AX.X, op=ALU.add), reads=['candw'], writes=[dn])
                    yield
                S.op('dve', lambda e: e.scalar_tensor_tensor(out=idxf[:], in0=iaf[:], scalar=128.0, in1=ibf[:], op0=ALU.mult, op1=ALU.add), reads=['iaf', 'ibf'], writes=['idxf'])
                S.op('dve', lambda e: e.tensor_scalar_min(out=idxf[:], in0=idxf[:], scalar1=16383.0), reads=['idxf'], writes=['idxf'])
                S.op('dve', lambda e: e.tensor_copy(out=idxi[p][:], in_=idxf[:]), reads=['idxf'], writes=['idxi%d' % p])
                S.op('dve', lambda e: e.tensor_scalar_mul(out=nmx[:], in0=best[:, :, 0], scalar1=-1.0), reads=['best'], writes=['nmx'])
                g3 = gate[p][:].rearrange("p (h k) -> p h k", k=16)
                for h in range(8):
                    S.op('act', lambda e, h=h: e.activation(out=g3[:, h, :], in_=best[:, h, :], func=AF.Exp, bias=nmx[:, h:h + 1], scale=1.0, accum_out=zs[:, h:h + 1]),
                         reads=['best', 'nmx'], writes=['gate%d' % p, 'zs'])
                S.op('dve', lambda e: e.reciprocal(out=zs[:], in_=zs[:]), reads=['zs'], writes=['zs'])
                S.op('dve', lambda e: e.tensor_tensor(out=g3, in0=g3, in1=zs[:].unsqueeze(2).to_broadcast([128, 8, 16]), op=ALU.mult), reads=['gate%d' % p, 'zs'], writes=['gate%d' % p])

            def fused(t, gen=None):
                p = t % 2
                LAG = 2

                def tail(kk):
                    s_ = (t * 128 + kk) % NG; d4 = kk % 4
                    S.op('dve', lambda e: e.tensor_scalar(out=dgk[d4][:], in0=identb[:], scalar1=gl[:, kk:kk + 1], scalar2=gate[p][:, kk:kk + 1], op0=ALU.mult, op1=ALU.mult),
                         reads=['identb', 'gl_%d' % kk, 'gate%d' % p], writes=['dgk%d' % d4])
                    for half in range(2):
                        S.op('pe', lambda e, half=half: e.matmul(PB[6 + half][:, :], lhsT=dgk[d4][:], rhs=uvb[s_][:, 1024 + half * 512:1024 + (half + 1) * 512], start=(kk == 0), stop=(kk == 127)),
                             reads=['dgk%d' % d4, 'uvb%d' % s_], writes=['pb%d' % (6 + half)], skip_self=True)

                for k in range(128):
                    s_ = (t * 128 + k) % NG; j4 = k % 3
                    dk = 'dots_%d' % k
                    S.dma('pool', lambda e, k=k, s_=s_: e.indirect_dma_start(out=uvb[s_][:], out_offset=None, in_=uv_d[:, :], in_offset=bass.IndirectOffsetOnAxis(ap=idxi[p][:, k:k + 1], axis=0)),
                          reads=['idxi%d' % p], writes=['uvb%d' % s_])
                    S.op('dve', lambda e, k=k, s_=s_, j4=j4: e.tensor_tensor(out=pr[j4][:], in0=uvb[s_][:, 0:1024], in1=h2b[p][:], op=ALU.mult),
                         reads=['uvb%d' % s_, 'h2b%d' % p], writes=['pr%d' % j4])
                    S.op('act', lambda e, k=k, j4=j4: e.activation(out=junk[:], in_=pr[j4][:], func=AF.Identity, accum_out=dots[:, k:k + 1]),
                         reads=['pr%d' % j4, 'dots0'], writes=[dk])
                    S.op('act', lambda e, k=k: e.activation(out=gl[:, k:k + 1], in_=dots[:, k:k + 1], func=AF.Gelu), reads=[dk], writes=['gl_%d' % k])
                    if k >= LAG:
                        tail(k - LAG)
                    if gen is not None and k % 2 == 1:
                        next(gen, None)
                for kk in range(128 - LAG, 128):
                    tail(kk)
                if gen is not None:
                    for _ in gen:
                        pass
                xb_ = x1t[p]; x1k = 'x1t%d' % p
                for half in range(2):
                    S.op('dve', lambda e, half=half: e.tensor_tensor(out=yb[:, half * 512:(half + 1) * 512], in0=PB[6 + half][:, :], in1=bct["g2b"][:, half * 512:(half + 1) * 512], op=ALU.mult),
                         reads=['pb%d' % (6 + half), 'g2b', 'h2'], writes=['h2'])
                S.op('dve', lambda e: e.scalar_tensor_tensor(out=fin[:], in0=xb_[:], scalar=ALPHA, in1=yb[:], op0=ALU.mult, op1=ALU.add), reads=[x1k, 'h2', 'sc'], writes=['sc'])
                layer_norm_rows(stf, mvf[:], rsf[:], fin[:], fin[:], 'sc', 'sc', 'p4f')
                S.op('dve', lambda e: e.tensor_tensor(out=fin[:], in0=fin[:], in1=bct["ln2g"][:], op=ALU.mult), reads=['sc', 'ln2g'], writes=['sc'])
                S.op('dve', lambda e: e.tensor_tensor(out=fin[:], in0=fin[:], in1=bct["ln2b"][:], op=ALU.add), reads=['sc', 'ln2b'], writes=['sc'])
                S.dma('sp', lambda e: e.dma_start(out=out_d[t * 128:(t + 1) * 128, :], in_=fin[:]), reads=['sc'], writes=['out'])

            stf = sb(p4, "st4f", [128, 2, 6]); mvf = sb(p4, "mv4f", [128, 2]); rsf = sb(p4, "rs4f", [128, 1])
            S.op('dve', lambda e: e.memset(dots[:], 0.0), writes=['dots0'])
            S.wait_all('pool', ['tabs_%d' % i for i in range(512)])
            if NT4:
                for _ in prologue(0):
                    pass
            for t in range(NT4):
                fused(t, prologue(t + 1) if t + 1 < NT4 else None)
            S.wait_all('sp', ['out', 'dbg'])
            S.barrier()
    return nc


def _prep_shared(inp):
    f = np.float32
    sh = {}
    sh["w_mod"] = np.ascontiguousarray(inp["w_mod"][0].reshape(8, 128, 6144).transpose(1, 0, 2))
    sh["b_modT"] = np.ascontiguousarray(inp["b_mod"][0].reshape(48, 128).T)
    sh["w_in"] = np.ascontiguousarray(inp["w_in"][0].reshape(8, 128, 4624).transpose(1, 0, 2))
    sh["lgT"] = np.ascontiguousarray(inp["hg_lb_logits"].reshape(2, 2, 4, 128).transpose(3, 0, 1, 2))
    sh["hgn"] = np.ascontiguousarray(np.broadcast_to(inp["hg_norm_g"][0][None, :], (64, 512)))
    sh["mln"] = np.ascontiguousarray(np.broadcast_to(inp["ml_norm_g"][0][None, :], (64, 512)))
    sh["convw"] = np.ascontiguousarray(inp["ml_conv_w"][0].reshape(9, 8, 128).transpose(2, 0, 1))
    sh["convb"] = np.ascontiguousarray(inp["ml_conv_b"][0].reshape(8, 128).T)
    sh["gateb"] = np.ascontiguousarray(inp["ml_gate_b"][0].reshape(2, 8).T)
    sh["w_out"] = np.ascontiguousarray(inp["w_out"][0].reshape(8, 128, 1024).transpose(1, 0, 2))
    for nm, key in (("ln1g", "ln1_g"), ("ln1b", "ln1_b"), ("ln2g", "ln2_g"), ("ln2b", "ln2_b")):
        sh[nm] = np.ascontiguousarray(np.broadcast_to(inp[key][0][None, :], (128, 1024)))
    sh["wq"] = np.ascontiguousarray(inp["peer_wq"][0].reshape(8, 128, 2048).transpose(1, 0, 2))
    sh["keysT"] = np.ascontiguousarray(inp["peer_keys"][0].reshape(16, 128, 128).transpose(2, 0, 1))
    nexp = 128 if os.environ.get("KDEBUG") else 16384
    sh["pu"] = np.ascontiguousarray(inp["peer_u"][0][:nexp])
    sh["pv"] = np.ascontiguousarray(inp["peer_v"][0][:nexp])
    sh["ident"] = np.eye(128, dtype=f)
    m = np.zeros((64, 2, 64), f)
    s = np.arange(64)[:, None]; c = np.arange(64)[None, :]
    m[:, 0, :] = (s <= c); m[:, 1, :] = (s >= c)
    sh["masks"] = m
    rm = np.ones((128, 512), f); rm[:, ::64] = 0.0
    sh["rmask"] = rm
    sh["sel8"] = np.eye(8, dtype=f)
    sh["iota16"] = np.ascontiguousarray(np.broadcast_to(np.arange(16, dtype=f)[None, :], (128, 16)))
    dm = np.zeros((8, 2), f); dm[0:4, 0] = 1.0; dm[4:8, 1] = 1.0
    sh["dirm"] = dm
    return {k: np.asarray(v, dtype=f) for k, v in sh.items()}


def kernel(**inputs):
    inp = {k: np.asarray(v) for k, v in inputs.items()}
    debug = os.environ.get("KDEBUG") or None
    nc = build(debug)
    sh = _prep_shared(inp)
    in_maps = []
    for b in range(8):
        m = dict(sh)
        m["xs"] = np.ascontiguousarray(np.concatenate([inp["ctx"][b], inp["x"][b]], axis=0).astype(np.float32))
        m["cT"] = np.ascontiguousarray(np.stack([inp["c"][b], inp["c_ctx"]], axis=-1).reshape(8, 128, 2).transpose(1, 0, 2).astype(np.float32))
        in_maps.append(m)
    res = run_bass_kernel_spmd(nc, in_maps, core_ids=list(range(8)))
    key = "dbg" if debug else "out"
    return np.stack([np.asarray(r[key]) for r in res.results], axis=0).astype(np.float32)
```

```python
import os
import numpy as np
from contextlib import ExitStack
import concourse.bass as bass
import concourse.mybir as mybir
from concourse.bass_utils import run_bass_kernel_spmd

F32 = mybir.dt.float32; BF16 = mybir.dt.bfloat16; I32 = mybir.dt.int32; U32 = mybir.dt.uint32
AF = mybir.ActivationFunctionType; ALU = mybir.AluOpType; AX = mybir.AxisListType

NTOK = 2304; NLAT = 2048; NCH = 36; NLCH = 32
ALPHA = 2.0 ** 0.25
EPS = 1e-6


class Sched:
    NDMA = 32

    def __init__(self, nc, es):
        self.nc = nc
        self.engs = {'pe': nc.tensor, 'act': nc.scalar, 'dve': nc.vector, 'pool': nc.gpsimd, 'sp': nc.sync}
        self.sem = {k: es.enter_context(nc.semaphore("sem_" + k)) for k in self.engs}
        self.cnt = {k: 0 for k in self.engs}
        self.dsem = [es.enter_context(nc.semaphore("dsem%d" % i)) for i in range(self.NDMA)]
        self.dcnt = [0] * self.NDMA
        self.dnext = 0
        self.seen = {k: {} for k in self.engs}
        self.bufs = {}

    def _deps(self, reads, writes):
        deps = []
        for r in reads:
            b = self.bufs.get(r)
            if b and b['w'] is not None:
                deps.append(b['w'])
        for w in writes:
            b = self.bufs.get(w)
            if b:
                if b['w'] is not None:
                    deps.append(b['w'])
                deps.extend(b['r'])
        return deps

    def _wait(self, eng, deps, skip_self=False):
        best = {}
        for (sid, sem, val, owner) in deps:
            if skip_self and owner == eng:
                continue
            if best.get(sid, (None, 0))[1] < val:
                best[sid] = (sem, val)
        for sid, (sem, val) in best.items():
            if self.seen[eng].get(sid, 0) >= val:
                continue
            self.engs[eng].wait_ge(sem, val)
            self.seen[eng][sid] = val

    def _record(self, dep, reads, writes):
        for r in reads:
            b = self.bufs.setdefault(r, {'w': None, 'r': []})
            b['r'] = [d for d in b['r'] if d[0] != dep[0]] + [dep]
        for w in writes:
            self.bufs[w] = {'w': dep, 'r': []}

    @staticmethod
    def _split(keys):
        norm, ps = [], []
        for k in keys:
            if k.startswith('pb') and len(k) > 2 and k[2].isdigit():
                ps.append(k[:3])
            else:
                norm.append(k)
        return norm, ps

    def op(self, eng, fn, reads=(), writes=(), skip_self=False):
        reads, pr = self._split(reads)
        writes, pw = self._split(writes)
        banks = sorted(set(pr + pw))
        deps = self._deps(reads, writes)
        deps += [d for d in self._deps((), banks) if d[3] != eng]
        self._wait(eng, deps, skip_self)
        ins = fn(self.engs[eng])
        self.cnt[eng] += 1
        ins.then_inc(self.sem[eng], 1)
        self._record(('e_' + eng, self.sem[eng], self.cnt[eng], eng), reads, list(writes) + banks)
        return ins

    def dma(self, q, fn, reads=(), writes=()):
        deps = self._deps(reads, writes)
        j = self.dnext
        self.dnext = (self.dnext + 1) % self.NDMA
        if self.dcnt[j] > 0:
            deps = deps + [('d%d' % j, self.dsem[j], self.dcnt[j], 'dma')]
        self._wait(q, deps)
        ins = fn(self.engs[q])
        self.dcnt[j] += 16
        ins.then_inc(self.dsem[j], 16)
        self._record(('d%d' % j, self.dsem[j], self.dcnt[j], 'dma'), reads, writes)

    def wait_all(self, eng, keys):
        deps = []
        for k in keys:
            b = self.bufs.get(k)
            if b:
                if b['w'] is not None:
                    deps.append(b['w'])
                deps.extend(b['r'])
        self._wait(eng, deps)

    def barrier(self):
        for e in self.engs:
            deps = [('e_' + f, self.sem[f], self.cnt[f], f) for f in self.engs if f != e and self.cnt[f] > 0]
            deps += [('d%d' % j, self.dsem[j], self.dcnt[j], 'dma') for j in range(self.NDMA) if self.dcnt[j] > 0]
            self._wait(e, deps)


class View:
    def __init__(self, ap):
        self.ap = ap

    def __getitem__(self, idx):
        return self.ap[idx]


def K(name, a, b=None):
    if b is None:
        return ["%s%d" % (name, a)]
    return ["%s%d" % (name, i) for i in range(a, b)]


def build(debug=None):
    nc = bass.Bass("TRN2", target_bir_lowering=False)
    D = {}

    def din(name, shape, dt=F32):
        D[name] = nc.dram_tensor(name, shape, dt, kind="ExternalInput").ap()
        return D[name]

    xs = din("xs", [NTOK, 1024]); cT_d = din("cT", [128, 8, 2]); wmod_d = din("w_mod", [128, 8, 6144])
    bmod_d = din("b_modT", [128, 48]); win_d = din("w_in", [128, 8, 4624]); lg_d = din("lgT", [128, 2, 2, 4])
    hgn_d = din("hgn", [64, 512]); mln_d = din("mln", [64, 512]); convw_d = din("convw", [128, 9, 8])
    convb_d = din("convb", [128, 8]); gateb_d = din("gateb", [8, 2]); wout_d = din("w_out", [128, 8, 1024])
    ln1g_d = din("ln1g", [128, 1024]); ln1b_d = din("ln1b", [128, 1024]); ln2g_d = din("ln2g", [128, 1024])
    ln2b_d = din("ln2b", [128, 1024]); wq_d = din("wq", [128, 8, 2048]); keysT_d = din("keysT", [128, 16, 128])
    NEXP = 128 if debug else 16384
    pu_d = din("pu", [NEXP, 1024]); pv_d = din("pv", [NEXP, 1024])
    ident_d = din("ident", [128, 128]); masks_d = din("masks", [64, 2, 64]); rmask_d = din("rmask", [128, 512])
    sel8_d = din("sel8", [8, 8]); dirm_d = din("dirm", [8, 2]); iota_d = din("iota16", [128, 16])
    out_d = nc.dram_tensor("out", [NLAT, 1024], F32, kind="ExternalOutput").ap()
    dbg_d = None
    if debug:
        dbg_d = nc.dram_tensor("dbg", [NLAT, 1024] if debug != 'mix' else [1024, NLAT], F32, kind="ExternalOutput").ap()

    with ExitStack() as es:
        S = Sched(nc, es)

        uid = [0]

        def sb(st, name, shape, dt=F32):
            uid[0] += 1
            return st.enter_context(nc.sbuf_tensor("s%d_%s" % (uid[0], name), shape, dt))

        PB = [es.enter_context(nc.psum_tensor("pb%d" % i, [128, 512], F32)) for i in range(8)]

        ident = sb(es, "ident", [128, 128]); identb = sb(es, "identb", [128, 128], BF16)
        masks = sb(es, "masks", [64, 2, 64]); rmask = sb(es, "rmask", [128, 512])
        ones = sb(es, "ones", [128, 128]); epsc = sb(es, "epsc", [128, 1])
        modv = sb(es, "modv", [128, 48, 2])
        S.dma('sp', lambda e: e.dma_start(out=ident[:], in_=ident_d[:, :]), writes=['ident'])
        S.dma('sp', lambda e: e.dma_start(out=masks[:], in_=masks_d[:, :, :]), writes=['masks'])
        S.dma('sp', lambda e: e.dma_start(out=rmask[:], in_=rmask_d[:, :]), writes=['rmask'])
        S.op('dve', lambda e: e.tensor_copy(out=identb[:], in_=ident[:]), reads=['ident'], writes=['identb'])
        S.op('dve', lambda e: e.memset(ones[:], 1.0), writes=['ones'])
        S.op('dve', lambda e: e.memset(epsc[:], EPS), writes=['epsc'])

        with ExitStack() as p0:
            cT = sb(p0, "cT", [128, 8, 2]); scT = sb(p0, "scT", [128, 8, 2]); bmodT = sb(p0, "bmodT", [128, 48])
            wm = [sb(p0, "wm%d" % i, [128, 6144]) for i in range(2)]
            S.dma('sp', lambda e: e.dma_start(out=cT[:], in_=cT_d[:, :, :]), writes=['cT'])
            S.dma('sp', lambda e: e.dma_start(out=bmodT[:], in_=bmod_d[:, :]), writes=['bmodT'])
            S.op('act', lambda e: e.activation(out=scT[:], in_=cT[:], func=AF.Silu), reads=['cT'], writes=['scT'])
            for kc in range(8):
                w = wm[kc % 2]; wk = 'wm%d' % (kc % 2)
                S.dma('sp' if kc % 2 == 0 else 'pool', lambda e, w=w, kc=kc: e.dma_start(out=w[:], in_=wmod_d[:, kc, :]), writes=[wk])
                for j in range(48):
                    S.op('pe', lambda e, w=w, kc=kc, j=j: e.matmul(PB[kc // 4][:, (kc % 4) * 96 + 2 * j:(kc % 4) * 96 + 2 * j + 2], lhsT=w[:, j * 128:(j + 1) * 128], rhs=scT[:, kc, :],
                                                                 start=True, stop=True),
                         reads=[wk, 'scT'], writes=['pb%d' % (kc // 4)], skip_self=True)
            mflat = modv[:].rearrange("p j n -> p (j n)")
            S.op('dve', lambda e: e.tensor_tensor(out=modv[:], in0=PB[0][:, 0:96].rearrange("p (j n) -> p j n", n=2), in1=bmodT[:].unsqueeze(2).to_broadcast([128, 48, 2]), op=ALU.add),
                 reads=['pb0', 'bmodT'], writes=['modv'])
            for kc in range(1, 8):
                S.op('dve', lambda e, kc=kc: e.tensor_tensor(out=mflat, in0=mflat, in1=PB[kc // 4][:, (kc % 4) * 96:(kc % 4) * 96 + 96], op=ALU.add),
                     reads=['pb%d' % (kc // 4), 'modv'], writes=['modv'])
            S.op('dve', lambda e: e.tensor_scalar_add(out=modv[:, 8:16, :], in0=modv[:, 8:16, :], scalar1=1.0), reads=['modv'], writes=['modv'])
            S.op('dve', lambda e: e.tensor_scalar_add(out=modv[:, 32:40, :], in0=modv[:, 32:40, :], scalar1=1.0), reads=['modv'], writes=['modv'])
            S.barrier()
        if debug == 'p0':
            S.dma('sp', lambda e: e.dma_start(out=dbg_d[0:128, 0:96], in_=modv[:].rearrange("p a b -> p (a b)")), reads=['modv'], writes=['dbg'])
            S.wait_all('sp', ['dbg']); S.barrier()
            return nc

        scA = ExitStack(); scB = ExitStack()
        mixT = sb(scA, "mixT", [128, 8, NLAT], BF16)
        hT = sb(scB, "hT", [128, 8, NTOK], BF16)
        wstg = sb(scB, "wstg", [128, 8, 128])
        tstg = [sb(scB, "tstg%d" % i, [128, 512]) for i in range(2)]
        tbf = [sb(scB, "tbf%d" % i, [128, 512], BF16) for i in range(2)]
        uv_d = nc.dram_tensor("uvbf", [NEXP, 2048], BF16, kind="Internal").ap()
        prep_pos = [0]

        def prep_tables(npieces):
            if debug:
                return
            for _ in range(npieces):
                i = prep_pos[0]
                if i >= 512:
                    return
                prep_pos[0] += 1
                src, coff = (pu_d, 0) if i < 256 else (pv_d, 1024)
                r = (i % 256) // 2; c = (i % 2) * 512; bb = i % 2
                S.dma('sp', lambda e, src=src, r=r, c=c, bb=bb: e.dma_start(out=tstg[bb][:], in_=src[r * 128:(r + 1) * 128, c:c + 512]), writes=['tstg%d' % bb])
                S.op('pool', lambda e, bb=bb: e.tensor_copy(out=tbf[bb][:], in_=tstg[bb][:]), reads=['tstg%d' % bb], writes=['tbf%d' % bb])
                S.dma('pool', lambda e, coff=coff, r=r, c=c, bb=bb: e.dma_start(out=uv_d[r * 128:(r + 1) * 128, coff + c:coff + c + 512], in_=tbf[bb][:]), reads=['tbf%d' % bb], writes=['tabs_%d' % i])

        def layer_norm_rows(st_ap, mv_ap, rstd_ap, src, dst, skey, dkey, tag):
            S.op('dve', lambda e: e.bn_stats(out=st_ap[:, 0, :], in_=src[:, 0:512]), reads=[skey], writes=[tag + 'st'])
            S.op('dve', lambda e: e.bn_stats(out=st_ap[:, 1, :], in_=src[:, 512:1024]), reads=[skey], writes=[tag + 'st'])
            S.op('dve', lambda e: e.bn_aggr(out=mv_ap, in_=st_ap[:].rearrange("p a b -> p (a b)")), reads=[tag + 'st'], writes=[tag + 'mv'])
            S.op('act', lambda e: e.activation(out=rstd_ap, in_=mv_ap[:, 1:2], func=AF.Sqrt, bias=epsc[:, 0:1], scale=1.0),
                 reads=[tag + 'mv', 'epsc'], writes=[tag + 'rs'])
            S.op('dve', lambda e: e.reciprocal(out=rstd_ap, in_=rstd_ap), reads=[tag + 'rs'], writes=[tag + 'rs'])
            S.op('dve', lambda e: e.tensor_scalar(out=dst, in0=src, scalar1=mv_ap[:, 0:1], scalar2=rstd_ap, op0=ALU.subtract, op1=ALU.mult),
                 reads=[skey, tag + 'mv', tag + 'rs'], writes=[dkey])

        with ExitStack() as p1:
            xt = [sb(p1, "xt%d" % i, [128, 1024]) for i in range(2)]
            xn = [sb(p1, "xn%d" % i, [128, 1024]) for i in range(2)]
            st = [sb(p1, "st%d" % i, [128, 2, 6]) for i in range(2)]
            mv = [sb(p1, "mv%d" % i, [128, 2]) for i in range(2)]
            rs = [sb(p1, "rs%d" % i, [128, 1]) for i in range(2)]
            PSAP = bool(os.environ.get("KPSAP"))
            for t in range(18):
                b = t % 2
                n = 1 if t < 2 else 0
                S.dma('sp' if b == 0 else 'pool', lambda e, t=t, b=b: e.dma_start(out=xt[b][:], in_=xs[t * 128:(t + 1) * 128, :]), writes=['xt%d' % b])
                layer_norm_rows(st[b], mv[b][:], rs[b][:], xt[b][:], xn[b][:], 'xt%d' % b, 'xn%d' % b, 'p1%d' % b)
                for half in range(2):
                    bank = 2 * b + half
                    pk = 'pb%d' % bank
                    for c4 in range(4):
                        ch = half * 4 + c4
                        S.op('pe', lambda e, b=b, ch=ch, c4=c4, bank=bank: e.transpose(PB[bank][:, c4 * 128:(c4 + 1) * 128], xn[b][:, ch * 128:(ch + 1) * 128], ident[:]),
                             reads=['xn%d' % b, 'ident'], writes=[pk], skip_self=True)
                    if not PSAP:
                        S.op('act', lambda e, b=b, half=half, bank=bank: e.copy(out=xt[b][:, half * 512:(half + 1) * 512], in_=PB[bank][:, :]), reads=[pk, 'xt%d' % b], writes=['xt%d' % b])
                    for c4 in range(4):
                        ch = half * 4 + c4
                        dst = hT[:, ch, t * 128:(t + 1) * 128]
                        src = PB[bank][:, c4 * 128:(c4 + 1) * 128] if PSAP else xt[b][:, ch * 128:(ch + 1) * 128]
                        S.op('dve', lambda e, dst=dst, src=src, ch=ch, n=n: e.tensor_scalar(out=dst, in0=src, scalar1=modv[:, 8 + ch, n:n + 1], scalar2=modv[:, ch, n:n + 1],
                                                                                      op0=ALU.mult, op1=ALU.add),
                             reads=([pk] if PSAP else ['xt%d' % b]) + ['modv'], writes=K('hT', t))
            S.barrier()

        if debug == 'p1':
            with ExitStack() as dd:
                hf = sb(dd, "hf", [128, 1024])
                for t in range(16):
                    S.op('dve', lambda e, t=t: e.tensor_copy(out=hf[:].rearrange("p (a b) -> p a b", b=128), in_=hT[:, :, 256 + t * 128:256 + (t + 1) * 128]), reads=K('hT', t + 2) + ['hf'], writes=['hf'])
                    S.dma('sp', lambda e, t=t: e.dma_start(out=dbg_d[t * 128:(t + 1) * 128, :], in_=hf[:]), reads=['hf'], writes=['dbg'])
                S.wait_all('sp', ['dbg']); S.barrier()
            scB.close(); scA.close()
            return nc
        def load_w(stk, name, col0, ncols, src=None):
            src = win_d if src is None else src
            wb = sb(stk, name, [128, 8, ncols], BF16)
            S.dma('sp', lambda e: e.dma_start(out=wstg[:, :, 0:ncols], in_=src[:, :, col0:col0 + ncols]), writes=['wstg'])
            S.op('pool', lambda e: e.tensor_copy(out=wb[:], in_=wstg[:, :, 0:ncols]), reads=['wstg'], writes=[name])
            return wb

        BLKS = [(0, 512), (512, 512), (1024, 512), (1536, 512), (2048, 256)]

        def proj_fm_block(wb, wname, ncols, bank, t0, nt):
            for kc in range(8):
                S.op('pe', lambda e, kc=kc: e.matmul(PB[bank][0:ncols, 0:nt], lhsT=wb[:, kc, :], rhs=hT[:, kc, t0:t0 + nt], start=(kc == 0), stop=(kc == 7)),
                     reads=[wname] + K('hT', t0 // 128, (t0 + nt) // 128), writes=['pb%d' % bank], skip_self=True)

        def proj_tm_group(wb, wname, ncols, bank, c0, ncks):
            for j in range(ncks):
                n = c0 + j
                for kc in range(8):
                    S.op('pe', lambda e, kc=kc, j=j, n=n: e.matmul(PB[bank][0:64, j * ncols:(j + 1) * ncols], lhsT=hT[:, kc, n * 64:(n + 1) * 64], rhs=wb[:, kc, :],
                                                                   start=(kc == 0), stop=(kc == 7)),
                         reads=[wname] + K('hT', n // 2), writes=['pb%d' % bank], skip_self=True)

        S32 = sb(scB, "S32", [128, 2, 132]); Sbf = sb(scB, "Sbf", [128, 2, 132], BF16)
        stm = sb(scB, "stm", [64, 2, 64], BF16)
        usb = sb(scB, "usb", [128, 2, 132])

        def chunk_loop(dirn, KsT, QsT, Vi, QoT, Ku, Vu, dec_ap, W, evac, rk, tagk, su_ap=None, suk=()):
            order = list(range(36)) if dirn == 0 else [3, 2, 1, 0] + list(range(35, 3, -1))
            S.op('dve', lambda e: e.memset(S32[:, 0, :], 0.0), writes=['S32_0'])
            S.op('dve', lambda e: e.memset(Sbf[:, 0, :], 0.0), writes=['Sbf_0'])

            def pre(idx):
                n = order[idx]
                sl = slice(n * 64, (n + 1) * 64)
                s2 = idx % 2
                if n >= 4:
                    pst = PB[2 + s2][0:64, 0:64]; pstk = 'pb%d' % (2 + s2)
                    S.op('pe', lambda e: e.matmul(pst, lhsT=KsT[:, sl], rhs=QsT[:, sl], start=True, stop=True),
                         reads=rk, writes=[pstk], skip_self=True)
                    if su_ap is None:
                        S.op('dve', lambda e: e.tensor_tensor(out=stm[:, s2, :], in0=pst, in1=masks[:, dirn, :], op=ALU.mult),
                             reads=[pstk, 'masks'], writes=['stm%d' % s2])
                    else:
                        S.op('dve', lambda e: e.scalar_tensor_tensor(out=stm[:, s2, :], in0=pst, scalar=su_ap(n), in1=masks[:, dirn, :], op0=ALU.mult, op1=ALU.mult),
                             reads=[pstk, 'masks'] + list(suk), writes=['stm%d' % s2])
                if idx < 35:
                    pu = PB[6 + s2][:, 0:W]; puk = 'pb%d' % (6 + s2)
                    S.op('pe', lambda e: e.matmul(pu, lhsT=Ku[:, n, :], rhs=Vu[:, n, 0:W], start=True, stop=True),
                         reads=rk, writes=[puk], skip_self=True)
                    S.op('act', lambda e: e.copy(out=usb[:, s2, 0:W], in_=pu), reads=[puk], writes=['usb%d' % s2])

            pre(0)
            cur = 0
            for idx, n in enumerate(order):
                if idx + 1 < 36:
                    pre(idx + 1)
                sl = slice(n * 64, (n + 1) * 64)
                s2 = idx % 2
                if n >= 4:
                    po = PB[4 + s2][0:64, 0:W]; pok = 'pb%d' % (4 + s2)
                    S.op('pe', lambda e, po=po, s2=s2, n=n: e.matmul(po, lhsT=stm[:, s2, :], rhs=Vi[:, n, 0:W], start=True, stop=False),
                         reads=['stm%d' % s2] + rk, writes=[pok], skip_self=True)
                    S.op('pe', lambda e, po=po, sl=sl, cur=cur: e.matmul(po, lhsT=QoT[:, sl], rhs=Sbf[:, cur, 0:W], start=False, stop=True),
                         reads=['Sbf_%d' % cur] + rk, writes=[pok], skip_self=True)
                if idx < 35:
                    nxt = 1 - cur
                    S.op('dve', lambda e, n=n, cur=cur, nxt=nxt, s2=s2: e.scalar_tensor_tensor(out=Sbf[:, nxt, 0:W], in0=S32[:, cur, 0:W], scalar=dec_ap(n), in1=usb[:, s2, 0:W],
                                                                                            op0=ALU.mult, op1=ALU.add),
                         reads=['S32_%d' % cur, 'usb%d' % s2, tagk], writes=['Sbf_%d' % nxt])
                    S.op('dve', lambda e, n=n, cur=cur, nxt=nxt, s2=s2: e.scalar_tensor_tensor(out=S32[:, nxt, 0:W], in0=S32[:, cur, 0:W], scalar=dec_ap(n), in1=usb[:, s2, 0:W],
                                                                                            op0=ALU.mult, op1=ALU.add),
                         reads=['S32_%d' % cur, 'usb%d' % s2, tagk], writes=['S32_%d' % nxt])
                    cur = nxt
                if n >= 4:
                    evac(n - 4, n, po, pok)

        def transpose_chunks_to_mixT(src, skey, head):
            for g in range(4):
                bank = g % 2
                for j in range(8):
                    n = g * 8 + j
                    S.op('pe', lambda e, n=n, j=j, bank=bank: e.transpose(PB[bank][:, j * 64:(j + 1) * 64], src[:, n, :], ident[0:64, 0:64]),
                         reads=list(skey) + ['ident'], writes=['pb%d' % bank], skip_self=True)
                S.op('act', lambda e, g=g, bank=bank: e.copy(out=mixT[:, head, g * 512:(g + 1) * 512], in_=PB[bank][:, :]),
                     reads=['pb%d' % bank], writes=K('mixT', head))

        with ExitStack() as g0:
            lgT = sb(g0, "lgT", [128, 2, 2, 4]); lbT = sb(g0, "lbT", [128, 2, 4]); omlT = sb(g0, "omlT", [128, 2, 4]); nomlT = sb(g0, "nomlT", [128, 2, 4])
            hgn = sb(g0, "hgn", [64, 512])
            S.dma('sp', lambda e: e.dma_start(out=lgT[:], in_=lg_d[:, :, :, :]), writes=['lgT'])
            S.dma('sp', lambda e: e.dma_start(out=hgn[:], in_=hgn_d[:, :]), writes=['hgn'])
            S.op('dve', lambda e: e.tensor_tensor(out=lbT[:], in0=lgT[:, :, 0, :], in1=lgT[:, :, 1, :], op=ALU.subtract), reads=['lgT'], writes=['lbT'])
            S.op('act', lambda e: e.activation(out=lbT[:], in_=lbT[:], func=AF.Sigmoid), reads=['lbT'], writes=['lbT'])
            S.op('dve', lambda e: e.tensor_scalar(out=omlT[:], in0=lbT[:], scalar1=-1.0, scalar2=1.0, op0=ALU.mult, op1=ALU.add), reads=['lbT'], writes=['omlT'])
            S.op('dve', lambda e: e.tensor_scalar_mul(out=nomlT[:], in0=omlT[:], scalar1=-1.0), reads=['omlT'], writes=['nomlT'])
            QsT = [sb(g0, "gQsT%d" % d, [128, NTOK], BF16) for d in range(2)]
            KsT = [sb(g0, "gKsT%d" % d, [128, NTOK], BF16) for d in range(2)]
            QoT = [sb(g0, "gQoT%d" % d, [128, NTOK], BF16) for d in range(2)]
            Ku = [sb(g0, "gKu%d" % d, [64, NCH, 128], BF16) for d in range(2)]
            dec = sb(g0, "gdec", [128, 2, NCH])
            vtm = sb(g0, "gv", [64, NCH, 128], BF16); gs = sb(g0, "ggs", [64, NLCH, 128], BF16); oacc = sb(g0, "goacc", [64, NLCH, 128])
            qf = sb(g0, "gqf", [128, 512]); sg = sb(g0, "gsg", [128, 512]); lf = sb(g0, "glf", [128, 512]); key = sb(g0, "gkey", [128, 512])
            Bc = sb(g0, "gB", [128, 512]); T1 = sb(g0, "gT1", [128, 512]); T2 = sb(g0, "gT2", [128, 512]); khT = sb(g0, "gkhT", [128, 512], BF16)
            EE = [sb(g0, "gE%d" % i, [128, 512]) for i in range(4)]
            sq = sb(g0, "gsq", [64, NLCH, 128], BF16); ss = sb(g0, "gss", [64, NLCH])
            for hd in range(4):
                with ExitStack() as hs:
                    wq_ = load_w(hs, "gwq", 0 + hd * 128, 128); wi_ = load_w(hs, "gwi", 512 + hd * 128, 128); wg_ = load_w(hs, "gwg", 1024 + hd * 128, 128)
                    wf = [load_w(hs, "gwf0", 1536 + hd * 128, 128), load_w(hs, "gwf1", 2048 + hd * 128, 128)]
                    prep_tables(64)
                    for g in range(9):
                        bk = 3 + g % 2
                        proj_tm_group(wi_, "gwi", 128, bk, g * 4, 4)
                        S.op('act', lambda e, g=g, bk=bk: e.copy(out=vtm[:, g * 4:(g + 1) * 4, :], in_=PB[bk][0:64, :].rearrange("p (j c) -> p j c", c=128)),
                             reads=['pb%d' % bk], writes=['gv'])
                    for g in range(8):
                        bk = 3 + (g + 1) % 2
                        proj_tm_group(wg_, "gwg", 128, bk, 4 + g * 4, 4)
                        S.op('act', lambda e, g=g, bk=bk: e.activation(out=gs[:, g * 4:(g + 1) * 4, :], in_=PB[bk][0:64, :].rearrange("p (j c) -> p j c", c=128), func=AF.Silu),
                             reads=['pb%d' % bk], writes=['ggs'])
                    for (t0, nt) in BLKS:
                        nck = nt // 64; c0 = t0 // 64
                        proj_fm_block(wq_, "gwq", 128, 0, t0, nt)
                        S.op('act', lambda e, nt=nt: e.copy(out=qf[:, 0:nt], in_=PB[0][:, 0:nt]), reads=['pb0'], writes=['gqf'])
                        for d in range(2):
                            proj_fm_block(wf[d], "gwf%d" % d, 128, 1 + d, t0, nt)
                            col = d * 4 + hd
                            lbp = lbT[:, d, hd:hd + 1]; omp = omlT[:, d, hd:hd + 1]; nomp = nomlT[:, d, hd:hd + 1]
                            S.op('act', lambda e, d=d, nt=nt: e.activation(out=sg[:, 0:nt], in_=PB[1 + d][:, 0:nt], func=AF.Sigmoid), reads=['pb%d' % (1 + d)], writes=['gsg'])
                            S.op('act', lambda e, nt=nt, lbp=lbp, omp=omp: e.activation(out=lf[:, 0:nt], in_=sg[:, 0:nt], func=AF.Ln, bias=lbp, scale=omp),
                                 reads=['gsg', 'lbT', 'omlT'], writes=['glf'])
                            S.op('dve', lambda e, nt=nt, nomp=nomp, omp=omp: e.tensor_scalar(out=key[:, 0:nt], in0=sg[:, 0:nt], scalar1=nomp, scalar2=omp, op0=ALU.mult, op1=ALU.add),
                                 reads=['gsg', 'omlT', 'nomlT'], writes=['gkey'])
                            S.op('dve', lambda e, nt=nt: e.tensor_tensor_scan(out=Bc[:, 0:nt], data0=rmask[:, 0:nt], data1=lf[:, 0:nt], initial=0.0, op0=ALU.mult, op1=ALU.add),
                                 reads=['glf', 'rmask'], writes=['gB'])
                            B3 = Bc[:, 0:nt].rearrange("p (n c) -> p n c", c=64)
                            T3 = T1[:, 0:nt].rearrange("p (n c) -> p n c", c=64)
                            if d == 1:
                                S.op('dve', lambda e, B3=B3, T3=T3, nck=nck: e.tensor_tensor(out=T3, in0=B3[:, :, 63:64].to_broadcast([128, nck, 64]), in1=B3, op=ALU.subtract),
                                     reads=['gB'], writes=['gT1'])
                                S.op('dve', lambda e, nt=nt: e.tensor_tensor(out=Bc[:, 0:nt], in0=T1[:, 0:nt], in1=lf[:, 0:nt], op=ALU.add), reads=['gT1', 'glf'], writes=['gB'])
                            li = 63 if d == 0 else 0
                            S.op('act', lambda e, B3=B3, li=li, d=d, c0=c0, nck=nck: e.activation(out=dec[:, d, c0:c0 + nck], in_=B3[:, :, li], func=AF.Exp), reads=['gB'], writes=['gdec'])
                            T4 = T2[:, 0:nt].rearrange("p (n c) -> p n c", c=64)
                            S.op('dve', lambda e, B3=B3, T3=T3, nck=nck: e.tensor_tensor(out=T3, in0=B3, in1=B3[:, :, 32:33].to_broadcast([128, nck, 64]), op=ALU.subtract),
                                 reads=['gB'], writes=['gT1'])
                            S.op('dve', lambda e, B3=B3, T4=T4, nck=nck, li=li: e.tensor_tensor(out=T4, in0=B3[:, :, li:li + 1].to_broadcast([128, nck, 64]), in1=B3, op=ALU.subtract),
                                 reads=['gB'], writes=['gT2'])
                            S.op('act', lambda e, nt=nt: e.activation(out=EE[0][:, 0:nt], in_=T1[:, 0:nt], func=AF.Exp), reads=['gT1'], writes=['gE0'])
                            S.op('act', lambda e, nt=nt: e.activation(out=EE[1][:, 0:nt], in_=T1[:, 0:nt], func=AF.Exp, scale=-1.0), reads=['gT1'], writes=['gE1'])
                            S.op('act', lambda e, nt=nt: e.activation(out=EE[2][:, 0:nt], in_=Bc[:, 0:nt], func=AF.Exp), reads=['gB'], writes=['gE2'])
                            S.op('act', lambda e, nt=nt: e.activation(out=EE[3][:, 0:nt], in_=T2[:, 0:nt], func=AF.Exp), reads=['gT2'], writes=['gE3'])
                            S.op('dve', lambda e, nt=nt, t0=t0, d=d: e.tensor_tensor(out=QsT[d][:, t0:t0 + nt], in0=qf[:, 0:nt], in1=EE[0][:, 0:nt], op=ALU.mult),
                                 reads=['gqf', 'gE0'], writes=['gQsT%d' % d])
                            S.op('dve', lambda e, nt=nt, t0=t0, d=d: e.tensor_tensor(out=KsT[d][:, t0:t0 + nt], in0=key[:, 0:nt], in1=EE[1][:, 0:nt], op=ALU.mult),
                                 reads=['gkey', 'gE1'], writes=['gKsT%d' % d])
                            S.op('dve', lambda e, nt=nt, t0=t0, d=d: e.tensor_tensor(out=QoT[d][:, t0:t0 + nt], in0=qf[:, 0:nt], in1=EE[2][:, 0:nt], op=ALU.mult),
                                 reads=['gqf', 'gE2'], writes=['gQoT%d' % d])
                            S.op('dve', lambda e, nt=nt: e.tensor_tensor(out=khT[:, 0:nt], in0=key[:, 0:nt], in1=EE[3][:, 0:nt], op=ALU.mult), reads=['gkey', 'gE3'], writes=['gkhT'])
                            pbb = PB[3][:].bitcast(BF16)
                            for j in range(nck):
                                S.op('pe', lambda e, j=j: e.transpose(pbb[0:64, j * 128:(j + 1) * 128], khT[:, j * 64:(j + 1) * 64], identb[:]),
                                     reads=['gkhT', 'identb'], writes=['pb3'], skip_self=True)
                            S.op('act', lambda e, d=d, c0=c0, nck=nck: e.copy(out=Ku[d][:, c0:c0 + nck, :], in_=pbb[0:64, 0:nck * 128].rearrange("p (j c) -> p j c", c=128)),
                                 reads=['pb3'], writes=['gKu%d' % d])
                    for d in range(2):
                        def evac(nl, n, po, pok, d=d):
                            if d == 0:
                                S.op('act', lambda e: e.copy(out=oacc[:, nl, :], in_=po), reads=[pok], writes=K('goacc', nl))
                            else:
                                S.op('dve', lambda e: e.tensor_tensor(out=oacc[:, nl, :], in0=po, in1=oacc[:, nl, :], op=ALU.add), reads=[pok] + K('goacc', nl), writes=K('goacc', nl))
                        chunk_loop(d, KsT[d], QsT[d], vtm, QoT[d], Ku[d], vtm, lambda n, d=d: dec[:, d, n:n + 1], 128, evac,
                                   ['gQsT%d' % d, 'gKsT%d' % d, 'gQoT%d' % d, 'gKu%d' % d, 'gv'], 'gdec')
                    allo = K('goacc', 0, NLCH)
                    S.op('dve', lambda e: e.tensor_tensor(out=sq[:], in0=oacc[:], in1=oacc[:], op=ALU.mult), reads=allo, writes=['gsq'])
                    S.op('dve', lambda e: e.tensor_reduce(out=ss[:], in_=sq[:], axis=AX.X, op=ALU.add), reads=['gsq'], writes=['gss'])
                    S.op('act', lambda e: e.activation(out=ss[:], in_=ss[:], func=AF.Sqrt, bias=epsc[0:64, 0:1], scale=1.0 / 128.0), reads=['gss', 'epsc'], writes=['gss'])
                    S.op('dve', lambda e: e.reciprocal(out=ss[:], in_=ss[:]), reads=['gss'], writes=['gss'])
                    S.op('dve', lambda e: e.tensor_tensor(out=oacc[:], in0=oacc[:], in1=ss[:].unsqueeze(2).to_broadcast([64, NLCH, 128]), op=ALU.mult),
                         reads=allo + ['gss'], writes=allo)
                    S.op('dve', lambda e, hd=hd: e.tensor_tensor(out=oacc[:], in0=oacc[:], in1=hgn[:, hd * 128:(hd + 1) * 128].unsqueeze(1).to_broadcast([64, NLCH, 128]), op=ALU.mult),
                         reads=allo + ['hgn'], writes=allo)
                    S.op('dve', lambda e: e.tensor_tensor(out=oacc[:], in0=oacc[:], in1=gs[:], op=ALU.mult), reads=allo + ['ggs'], writes=allo)
                    transpose_chunks_to_mixT(oacc, allo, hd)
            S.barrier()

        with ExitStack() as m0:
            mln = sb(m0, "mln", [64, 512]); convw = sb(m0, "convw", [128, 9, 8]); convb = sb(m0, "convb", [128, 8])
            gateb = sb(m0, "gateb", [8, 2]); sel8 = sb(m0, "sel8", [8, 8]); dirm = sb(m0, "dirm", [8, 2])
            S.dma('sp', lambda e: e.dma_start(out=mln[:], in_=mln_d[:, :]), writes=['mln'])
            S.dma('sp', lambda e: e.dma_start(out=convw[:], in_=convw_d[:, :, :]), writes=['convw'])
            S.dma('sp', lambda e: e.dma_start(out=convb[:], in_=convb_d[:, :]), writes=['convb'])
            S.dma('sp', lambda e: e.dma_start(out=gateb[:], in_=gateb_d[:, :]), writes=['gateb'])
            S.dma('sp', lambda e: e.dma_start(out=sel8[:], in_=sel8_d[:, :]), writes=['sel8'])
            S.dma('sp', lambda e: e.dma_start(out=dirm[:], in_=dirm_d[:, :]), writes=['dirm'])
            RUU = sb(m0, "RUU", [64, NCH, 24]); dchunk = sb(m0, "dchunk", [128, 8, NCH])
            with ExitStack() as gp:
                wgi = load_w(gp, "mwgi", 4608, 8); wgf = load_w(gp, "mwgf", 4616, 8)
                LI = sb(gp, "LI", [8, NTOK]); LF = sb(gp, "LF", [8, NTOK]); Af = sb(gp, "Af", [8, NTOK]); Ab = sb(gp, "Ab", [8, NTOK]); Aa = sb(gp, "Aa", [8, NTOK])
                R = [sb(gp, "Rr%d" % i, [8, NTOK]) for i in range(3)]
                bd = sb(gp, "bd", [8, 8, NCH])
                for (t0, nt) in BLKS:
                    proj_fm_block(wgi, "mwgi", 8, 0, t0, nt)
                    S.op('act', lambda e, t0=t0, nt=nt: e.copy(out=LI[:, t0:t0 + nt], in_=PB[0][0:8, 0:nt]), reads=['pb0'], writes=['LI'])
                    proj_fm_block(wgf, "mwgf", 8, 1, t0, nt)
                    S.op('act', lambda e, t0=t0, nt=nt: e.copy(out=LF[:, t0:t0 + nt], in_=PB[1][0:8, 0:nt]), reads=['pb1'], writes=['LF'])
                S.op('dve', lambda e: e.tensor_scalar_add(out=LI[:], in0=LI[:], scalar1=gateb[:, 0:1]), reads=['LI', 'gateb'], writes=['LI'])
                S.op('act', lambda e: e.activation(out=LF[:], in_=LF[:], func=AF.Sigmoid, bias=gateb[:, 1:2], scale=1.0), reads=['LF', 'gateb'], writes=['LF'])
                S.op('act', lambda e: e.activation(out=LF[:], in_=LF[:], func=AF.Ln), reads=['LF'], writes=['LF'])
                for (t0, nt) in BLKS:
                    S.op('dve', lambda e, t0=t0, nt=nt: e.tensor_tensor_scan(out=Af[:, t0:t0 + nt], data0=rmask[0:8, 0:nt], data1=LF[:, t0:t0 + nt], initial=0.0, op0=ALU.mult, op1=ALU.add),
                         reads=['LF', 'rmask'], writes=['Af'])
                A3 = Af[:].rearrange("p (n c) -> p n c", c=64)
                tot = A3[:, :, 63:64]
                S.op('dve', lambda e: e.tensor_tensor(out=Ab[:].rearrange("p (n c) -> p n c", c=64), in0=tot.to_broadcast([8, NCH, 64]), in1=A3, op=ALU.subtract), reads=['Af'], writes=['Ab'])
                S.op('dve', lambda e: e.tensor_tensor(out=Ab[:], in0=Ab[:], in1=LF[:], op=ALU.add), reads=['Ab', 'LF'], writes=['Ab'])
                S.op('dve', lambda e: e.tensor_scalar_mul(out=Aa[:], in0=Af[:], scalar1=dirm[:, 0:1]), reads=['Af', 'dirm'], writes=['Aa'])
                S.op('dve', lambda e: e.scalar_tensor_tensor(out=Aa[:], in0=Ab[:], scalar=dirm[:, 1:2], in1=Aa[:], op0=ALU.mult, op1=ALU.add), reads=['Ab', 'dirm', 'Aa'], writes=['Aa'])
                S.op('act', lambda e: e.activation(out=R[0][:], in_=Aa[:], func=AF.Exp), reads=['Aa'], writes=['Rr0'])
                S.op('dve', lambda e: e.tensor_tensor(out=Ab[:], in0=LI[:], in1=Aa[:], op=ALU.subtract), reads=['LI', 'Aa', 'Ab'], writes=['Ab'])
                S.op('act', lambda e: e.activation(out=R[1][:], in_=Ab[:], func=AF.Exp), reads=['Ab'], writes=['Rr1'])
                S.op('dve', lambda e: e.tensor_tensor(out=Aa[:].rearrange("p (n c) -> p n c", c=64), in0=Ab[:].rearrange("p (n c) -> p n c", c=64), in1=tot.to_broadcast([8, NCH, 64]), op=ALU.add),
                     reads=['Ab', 'Af', 'Aa', 'Rr0'], writes=['Aa'])
                S.op('act', lambda e: e.activation(out=R[2][:], in_=Aa[:], func=AF.Exp), reads=['Aa'], writes=['Rr2'])
                for half in range(2):
                    for j in range(18):
                        n = half * 18 + j
                        for q in range(3):
                            S.op('pe', lambda e, n=n, j=j, q=q: e.transpose(PB[2][0:64, j * 24 + q * 8:j * 24 + q * 8 + 8], R[q][:, n * 64:(n + 1) * 64], ident[0:8, 0:8]),
                                 reads=['Rr%d' % q, 'ident'], writes=['pb2'], skip_self=True)
                    S.op('act', lambda e, half=half: e.copy(out=RUU[:, half * 18:(half + 1) * 18, :], in_=PB[2][0:64, 0:432].rearrange("p (j c) -> p j c", c=24)),
                         reads=['pb2'], writes=['RUU'])
                S.op('dve', lambda e: e.tensor_tensor(out=bd[:], in0=tot.rearrange("p n c -> p c n").to_broadcast([8, 8, NCH]), in1=sel8[:].unsqueeze(2).to_broadcast([8, 8, NCH]), op=ALU.mult),
                     reads=['Af', 'sel8'], writes=['bd'])
                S.op('pe', lambda e: e.matmul(PB[3][:, 0:288], lhsT=ones[0:8, :], rhs=bd[:].rearrange("p a n -> p (a n)"), start=True, stop=True), reads=['ones', 'bd'], writes=['pb3'], skip_self=True)
                S.op('act', lambda e: e.activation(out=dchunk[:].rearrange("p a n -> p (a n)"), in_=PB[3][:, 0:288], func=AF.Exp), reads=['pb3'], writes=['dchunk'])
                S.barrier()
            qc = sb(m0, "mqc", [128, NTOK], BF16); kc_ = sb(m0, "mkc", [128, NTOK], BF16); kTM = sb(m0, "mkTM", [64, NCH, 128], BF16)
            raw = sb(m0, "mraw", [128, NTOK]); acc = sb(m0, "macc", [128, NTOK])
            vext = sb(m0, "mvext", [64, NCH, 132], BF16); vu = sb(m0, "mvu", [64, NCH, 132], BF16)
            og = sb(m0, "mog", [64, NLCH, 128], BF16); oext = sb(m0, "moext", [64, NLCH, 132]); obuf = sb(m0, "mobuf", [64, NLCH, 128])
            den = sb(m0, "mden", [64, NLCH]); mu = sb(m0, "mmu", [64, NLCH]); m2 = sb(m0, "mm2", [64, NLCH])
            S.op('dve', lambda e: e.memset(vext[:], 1.0), writes=['mvext'])
            for hd in range(4):
                with ExitStack() as hs:
                    wq_ = load_w(hs, "mwq", 2560 + hd * 128, 128); wk_ = load_w(hs, "mwk", 3072 + hd * 128, 128)
                    wv_ = load_w(hs, "mwv", 3584 + hd * 128, 128); wo_ = load_w(hs, "mwo", 4096 + hd * 128, 128)
                    prep_tables(64)
                    for g in range(9):
                        bk = 3 + g % 2
                        proj_tm_group(wv_, "mwv", 128, bk, g * 4, 4)
                        S.op('act', lambda e, g=g, bk=bk: e.copy(out=vext[:, g * 4:(g + 1) * 4, 0:128], in_=PB[bk][0:64, :].rearrange("p (j c) -> p j c", c=128)),
                             reads=['pb%d' % bk], writes=['mvext'])
                    for g in range(8):
                        bk = 3 + (g + 1) % 2
                        proj_tm_group(wo_, "mwo", 128, bk, 4 + g * 4, 4)
                        S.op('act', lambda e, g=g, bk=bk: e.activation(out=og[:, g * 4:(g + 1) * 4, :], in_=PB[bk][0:64, :].rearrange("p (j c) -> p j c", c=128), func=AF.Sigmoid),
                             reads=['pb%d' % bk], writes=['mog'])
                    for qi, (wb, wn, dstb) in enumerate(((wq_, "mwq", qc), (wk_, "mwk", kc_))):
                        chn = qi * 4 + hd
                        for bi, (t0, nt) in enumerate(BLKS):
                            proj_fm_block(wb, wn, 128, bi % 2, t0, nt)
                            S.op('act', lambda e, t0=t0, nt=nt, bi=bi: e.copy(out=raw[:, t0:t0 + nt], in_=PB[bi % 2][:, 0:nt]), reads=['pb%d' % (bi % 2)], writes=['mraw'])
                        S.op('dve', lambda e, chn=chn: e.tensor_scalar(out=acc[:, 0:256], in0=raw[:, 0:256], scalar1=convw[:, 4, chn:chn + 1], scalar2=convb[:, chn:chn + 1], op0=ALU.mult, op1=ALU.add),
                             reads=['mraw', 'convw', 'convb'], writes=['macc'])
                        S.op('dve', lambda e, chn=chn: e.scalar_tensor_tensor(out=acc[:, 1:256], in0=raw[:, 0:255], scalar=convw[:, 3, chn:chn + 1], in1=acc[:, 1:256], op0=ALU.mult, op1=ALU.add),
                             reads=['mraw', 'convw', 'macc'], writes=['macc'])
                        S.op('dve', lambda e, chn=chn: e.scalar_tensor_tensor(out=acc[:, 0:255], in0=raw[:, 1:256], scalar=convw[:, 5, chn:chn + 1], in1=acc[:, 0:255], op0=ALU.mult, op1=ALU.add),
                             reads=['mraw', 'convw', 'macc'], writes=['macc'])
                        X = raw[:, 256:NTOK].rearrange("p (r c) -> p r c", c=64); Y = acc[:, 256:NTOK].rearrange("p (r c) -> p r c", c=64)
                        S.op('dve', lambda e, chn=chn: e.tensor_scalar(out=acc[:, 256:NTOK], in0=raw[:, 256:NTOK], scalar1=convw[:, 4, chn:chn + 1], scalar2=convb[:, chn:chn + 1], op0=ALU.mult, op1=ALU.add),
                             reads=['mraw', 'convw', 'convb', 'macc'], writes=['macc'])
                        for ky in range(3):
                            for kx in range(3):
                                if ky == 1 and kx == 1:
                                    continue
                                dy = ky - 1; dx = kx - 1
                                r0 = max(0, -dy); r1 = 32 - max(0, dy); c0 = max(0, -dx); c1 = 64 - max(0, dx)
                                S.op('dve', lambda e, chn=chn, ky=ky, kx=kx, r0=r0, r1=r1, c0=c0, c1=c1, dy=dy, dx=dx: e.scalar_tensor_tensor(
                                    out=Y[:, r0:r1, c0:c1], in0=X[:, r0 + dy:r1 + dy, c0 + dx:c1 + dx], scalar=convw[:, ky * 3 + kx, chn:chn + 1], in1=Y[:, r0:r1, c0:c1],
                                    op0=ALU.mult, op1=ALU.add), reads=['mraw', 'convw', 'macc'], writes=['macc'])
                        S.op('act', lambda e: e.activation(out=acc[:], in_=acc[:], func=AF.Silu), reads=['macc'], writes=['macc'])
                        if qi == 0:
                            S.op('dve', lambda e: e.tensor_copy(out=qc[:], in_=acc[:]), reads=['macc'], writes=['mqc'])
                        else:
                            S.op('dve', lambda e: e.tensor_scalar_mul(out=kc_[:], in0=acc[:], scalar1=128.0 ** -0.5), reads=['macc'], writes=['mkc'])
                    pbb = PB[3][:].bitcast(BF16)
                    for g in range(5):
                        nck = 8 if g < 4 else 4
                        for j in range(nck):
                            n = g * 8 + j
                            S.op('pe', lambda e, j=j, n=n: e.transpose(pbb[0:64, j * 128:(j + 1) * 128], kc_[:, n * 64:(n + 1) * 64], identb[:]),
                                 reads=['mkc', 'identb'], writes=['pb3'], skip_self=True)
                        S.op('act', lambda e, g=g, nck=nck: e.copy(out=kTM[:, g * 8:g * 8 + nck, :], in_=pbb[0:64, 0:nck * 128].rearrange("p (j c) -> p j c", c=128)),
                             reads=['pb3'], writes=['mkTM'])
                    for d in range(2):
                        row = d * 4 + hd
                        S.op('dve', lambda e, row=row: e.tensor_tensor(out=vu[:], in0=vext[:], in1=RUU[:, :, 16 + row:17 + row].to_broadcast([64, NCH, 132]), op=ALU.mult),
                             reads=['mvext', 'RUU'], writes=['mvu'])

                        def evac(nl, n, po, pok, row=row):
                            S.op('act', lambda e: e.activation(out=oext[:, nl, 0:129], in_=po, func=AF.Identity, scale=RUU[:, n, row:row + 1]), reads=[pok, 'RUU'], writes=K('moext', nl))
                        chunk_loop(d, kc_, qc, vext, qc, kTM, vu, lambda n, row=row: dchunk[:, row, n:n + 1], 129, evac,
                                   ['mqc', 'mkc', 'mvext', 'mvu', 'mkTM'], 'dchunk',
                                   su_ap=lambda n, row=row: RUU[:, n, 8 + row:9 + row], suk=['RUU'])
                        allx = K('moext', 0, NLCH)
                        S.op('act', lambda e: e.activation(out=den[:], in_=oext[:, :, 128], func=AF.Abs), reads=allx, writes=['mden'])
                        S.op('dve', lambda e: e.tensor_scalar_max(out=den[:], in0=den[:], scalar1=1.0), reads=['mden'], writes=['mden'])
                        S.op('dve', lambda e: e.reciprocal(out=den[:], in_=den[:]), reads=['mden'], writes=['mden'])
                        if d == 0:
                            S.op('dve', lambda e: e.tensor_tensor(out=obuf[:], in0=oext[:, :, 0:128], in1=den[:].unsqueeze(2).to_broadcast([64, NLCH, 128]), op=ALU.mult),
                                 reads=allx + ['mden'], writes=['mobuf'])
                        else:
                            S.op('dve', lambda e: e.tensor_tensor(out=oext[:, :, 0:128], in0=oext[:, :, 0:128], in1=den[:].unsqueeze(2).to_broadcast([64, NLCH, 128]), op=ALU.mult),
                                 reads=allx + ['mden'], writes=allx)
                            S.op('dve', lambda e: e.tensor_tensor(out=obuf[:], in0=obuf[:], in1=oext[:, :, 0:128], op=ALU.add), reads=allx + ['mobuf'], writes=['mobuf'])
                    allx = K('moext', 0, NLCH)
                    S.op('dve', lambda e: e.tensor_reduce(out=mu[:], in_=obuf[:], axis=AX.X, op=ALU.add), reads=['mobuf'], writes=['mmu'])
                    S.op('dve', lambda e: e.tensor_scalar_mul(out=mu[:], in0=mu[:], scalar1=1.0 / 128.0), reads=['mmu'], writes=['mmu'])
                    S.op('dve', lambda e: e.tensor_tensor(out=obuf[:], in0=obuf[:], in1=mu[:].unsqueeze(2).to_broadcast([64, NLCH, 128]), op=ALU.subtract), reads=['mobuf', 'mmu'], writes=['mobuf'])
                    S.op('dve', lambda e: e.tensor_tensor(out=oext[:, :, 0:128], in0=obuf[:], in1=obuf[:], op=ALU.mult), reads=['mobuf'] + allx, writes=allx)
                    S.op('dve', lambda e: e.tensor_reduce(out=m2[:], in_=oext[:, :, 0:128], axis=AX.X, op=ALU.add), reads=allx, writes=['mm2'])
                    S.op('act', lambda e: e.activation(out=m2[:], in_=m2[:], func=AF.Sqrt, bias=epsc[0:64, 0:1], scale=1.0 / 128.0), reads=['mm2', 'epsc'], writes=['mm2'])
                    S.op('dve', lambda e: e.reciprocal(out=m2[:], in_=m2[:]), reads=['mm2'], writes=['mm2'])
                    S.op('dve', lambda e: e.tensor_tensor(out=obuf[:], in0=obuf[:], in1=m2[:].unsqueeze(2).to_broadcast([64, NLCH, 128]), op=ALU.mult), reads=['mobuf', 'mm2'], writes=['mobuf'])
                    S.op('dve', lambda e, hd=hd: e.tensor_tensor(out=obuf[:], in0=obuf[:], in1=mln[:, hd * 128:(hd + 1) * 128].unsqueeze(1).to_broadcast([64, NLCH, 128]), op=ALU.mult),
                         reads=['mobuf', 'mln'], writes=['mobuf'])
                    S.op('dve', lambda e: e.tensor_tensor(out=obuf[:], in0=obuf[:], in1=og[:], op=ALU.mult), reads=['mobuf', 'mog'], writes=['mobuf'])
                    transpose_chunks_to_mixT(obuf, ['mobuf'], 4 + hd)
            S.barrier()

        if debug == 'mix':
            with ExitStack() as dd:
                mf = sb(dd, "mf", [128, NLAT])
                for h in range(8):
                    S.op('dve', lambda e, h=h: e.tensor_copy(out=mf[:], in_=mixT[:, h, :]), reads=K('mixT', h) + ['mf'], writes=['mf'])
                    S.dma('sp', lambda e, h=h: e.dma_start(out=dbg_d[h * 128:(h + 1) * 128, :], in_=mf[:]), reads=['mf'], writes=['dbg'])
                S.wait_all('sp', ['dbg']); S.barrier()
            scB.close(); scA.close()
            return nc
        prep_tables(512)
        S.barrier()
        scB.close()
        x1s = nc.dram_tensor("x1s", [NLAT, 1024], F32, kind="Internal").ap()

        def bcast_tile(dst, dkey, c0, dg):
            for ch in range(8):
                S.op('dve', lambda e, ch=ch: e.tensor_scalar_mul(out=dg[:], in0=ident[:], scalar1=modv[:, c0 + ch, 0:1]), reads=['ident', 'modv', 'dg'], writes=['dg'])
                bank = ch // 4
                S.op('pe', lambda e, ch=ch, bank=bank: e.matmul(PB[bank][:, (ch % 4) * 128:(ch % 4 + 1) * 128], lhsT=ones[:], rhs=dg[:], start=True, stop=True),
                     reads=['ones', 'dg'], writes=['pb%d' % bank], skip_self=True)
            S.op('act', lambda e: e.copy(out=dst[:, 0:512], in_=PB[0][:, :]), reads=['pb0'], writes=[dkey])
            S.op('act', lambda e: e.copy(out=dst[:, 512:1024], in_=PB[1][:, :]), reads=['pb1'], writes=[dkey])

        with ExitStack() as p3:
            bct = {}
            for nm in ("g1b", "ln1g", "ln1b"):
                bct[nm] = sb(p3, nm, [128, 1024])
            for nm, dd in (("ln1g", ln1g_d), ("ln1b", ln1b_d)):
                S.dma('sp', lambda e, nm=nm, dd=dd: e.dma_start(out=bct[nm][:], in_=dd[:, :]), writes=[nm])
            dg = sb(p3, "dg", [128, 128])
            bcast_tile(bct["g1b"], "g1b", 16, dg)
            wo_b = sb(p3, "wout", [128, 8, 1024], BF16)
            for kc in range(8):
                S.dma('pool', lambda e, kc=kc: e.dma_start(out=wo_b[:, kc, :], in_=wout_d[:, kc, :]), writes=['wout%d' % kc])
            xt = [sb(p3, "x3t%d" % i, [128, 1024]) for i in range(2)]
            t1 = [sb(p3, "t1_%d" % i, [128, 1024]) for i in range(2)]
            st = [sb(p3, "st3%d" % i, [128, 2, 6]) for i in range(2)]
            mv = [sb(p3, "mv3%d" % i, [128, 2]) for i in range(2)]
            rs = [sb(p3, "rs3%d" % i, [128, 1]) for i in range(2)]
            for t in range(16):
                b = t % 2
                S.dma('sp' if b == 0 else 'pool', lambda e, t=t, b=b: e.dma_start(out=xt[b][:], in_=xs[256 + t * 128:256 + (t + 1) * 128, :]), writes=['x3t%d' % b])
                for half in range(2):
                    bank = 2 * b + half
                    for kc in range(8):
                        S.op('pe', lambda e, kc=kc, t=t, half=half, bank=bank: e.matmul(PB[bank][:, :], lhsT=mixT[:, kc, t * 128:(t + 1) * 128], rhs=wo_b[:, kc, half * 512:(half + 1) * 512],
                                                                                      start=(kc == 0), stop=(kc == 7)),
                             reads=K('mixT', kc) + ['wout%d' % kc], writes=['pb%d' % bank], skip_self=True)
                    S.op('dve', lambda e, b=b, half=half, bank=bank: e.tensor_tensor(out=t1[b][:, half * 512:(half + 1) * 512], in0=PB[bank][:, :], in1=bct["g1b"][:, half * 512:(half + 1) * 512], op=ALU.mult),
                         reads=['pb%d' % bank, 'g1b'], writes=['t1_%d' % b])
                S.op('dve', lambda e, b=b: e.scalar_tensor_tensor(out=t1[b][:], in0=xt[b][:], scalar=ALPHA, in1=t1[b][:], op0=ALU.mult, op1=ALU.add),
                     reads=['x3t%d' % b, 't1_%d' % b], writes=['t1_%d' % b])
                layer_norm_rows(st[b], mv[b][:], rs[b][:], t1[b][:], t1[b][:], 't1_%d' % b, 't1_%d' % b, 'p3%d' % b)
                S.op('dve', lambda e, b=b: e.tensor_tensor(out=t1[b][:], in0=t1[b][:], in1=bct["ln1g"][:], op=ALU.mult), reads=['t1_%d' % b, 'ln1g'], writes=['t1_%d' % b])
                S.op('dve', lambda e, b=b, t=t: e.tensor_tensor(out=t1[b][:], in0=t1[b][:], in1=bct["ln1b"][:], op=ALU.add), reads=['t1_%d' % b, 'ln1b'], writes=['t1_%d' % b])
                S.dma('sp', lambda e, t=t, b=b: e.dma_start(out=x1s[t * 128:(t + 1) * 128, :], in_=t1[b][:]), reads=['t1_%d' % b], writes=K('x1_', t))
                if debug == 'x1':
                    S.dma('sp', lambda e, t=t, b=b: e.dma_start(out=dbg_d[t * 128:(t + 1) * 128, :], in_=t1[b][:]), reads=['t1_%d' % b], writes=['dbg'])
            S.barrier()
        scA.close()

        with ExitStack() as p4:
            bct = {}
            for nm in ("g2b", "sc2b", "sh2b", "ln2g", "ln2b"):
                bct[nm] = sb(p4, nm, [128, 1024])
            for nm, dd in (("ln2g", ln2g_d), ("ln2b", ln2b_d)):
                S.dma('sp', lambda e, nm=nm, dd=dd: e.dma_start(out=bct[nm][:], in_=dd[:, :]), writes=[nm])
            dg = sb(p4, "dg4", [128, 128])
            bcast_tile(bct["g2b"], "g2b", 40, dg); bcast_tile(bct["sc2b"], "sc2b", 32, dg); bcast_tile(bct["sh2b"], "sh2b", 24, dg)
            x1t = [sb(p4, "x1t%d" % i, [128, 1024]) for i in range(2)]
            wqb = sb(p4, "wqb", [128, 8, 2048], BF16)
            keysT = sb(p4, "keysT", [128, 16, 128], BF16)
            for kc in range(8):
                for hh in range(2):
                    S.dma('pool', lambda e, kc=kc, hh=hh: e.dma_start(out=wqb[:, kc, hh * 1024:(hh + 1) * 1024], in_=wq_d[:, kc, hh * 1024:(hh + 1) * 1024]), writes=['wqb%d_%d' % (kc, hh)])
            for hh in range(2):
                S.dma('pool', lambda e, hh=hh: e.dma_start(out=keysT[:, hh * 8:(hh + 1) * 8, :], in_=keysT_d[:, hh * 8:(hh + 1) * 8, :]), writes=['keysT%d' % hh])
            S.barrier()
            NG = int(os.environ.get('KNG', '16'))
            uvb = [sb(p4, "uvb%d" % i, [128, 2048], BF16) for i in range(NG)]
            gl = sb(p4, "gl", [128, 128])
            pr = [sb(p4, "pr%d" % i, [128, 1024], BF16) for i in range(3)]
            dgk = [sb(p4, "dgk%d" % i, [128, 128], BF16) for i in range(4)]
            h2 = sb(p4, "h2", [128, 1024]); h2b = [sb(p4, "h2b%d" % i, [128, 1024], BF16) for i in range(2)]
            h2T = sb(p4, "h2T", [128, 8, 128], BF16); qT = sb(p4, "qT", [128, 16, 128], BF16)
            sc = sb(p4, "sc", [128, 16, 128]); scw = sb(p4, "scw", [128, 16, 128])
            top = sb(p4, "top", [128, 16, 16]); topi = sb(p4, "topi", [128, 16, 16], U32); topf = sb(p4, "topf", [128, 16, 16])
            candw = sb(p4, "candw", [128, 8, 256])
            cand = View(scw[:].rearrange("p (h two) k -> p h (two k)", two=2)); eq = View(candw[:].rearrange("p h (a b) -> p (h a) b", b=16))
            best = sb(p4, "best", [128, 8, 16]); idxf = sb(p4, "idxf", [128, 128])
            posi = sb(p4, "posi", [128, 8, 16], U32); pai = sb(p4, "pai", [128, 128], U32); pbi = sb(p4, "pbi", [128, 128], U32)
            paf = sb(p4, "paf", [128, 128]); pbf = sb(p4, "pbf", [128, 128]); iaf = sb(p4, "iaf", [128, 128]); ibf = sb(p4, "ibf", [128, 128])
            iota16 = sb(p4, "iota16", [128, 16])
            S.dma('sp', lambda e: e.dma_start(out=iota16[:], in_=iota_d[:, :]), writes=['iota16'])
            gate = [sb(p4, "gate%d" % i, [128, 128]) for i in range(2)]
            idxi = [sb(p4, "idxi%d" % i, [128, 128], I32) for i in range(2)]
            dots = sb(p4, "dots", [128, 128]); junk = sb(p4, "junk", [128, 1024], BF16)
            zs = sb(p4, "zs", [128, 8]); nmx = sb(p4, "nmx", [128, 8])
            st = sb(p4, "st4", [128, 2, 6]); mv = sb(p4, "mv4", [128, 2]); rs = sb(p4, "rs4", [128, 1])
            fin = View(sc[:].rearrange("p c k -> p (c k)")[:, 0:1024]); yb = h2
            NT4 = 16 if debug != 'x1' else 0

            def prologue(t):
                p = t % 2
                xb_ = x1t[p]; x1k = 'x1t%d' % p
                S.dma('sp', lambda e: e.dma_start(out=xb_[:], in_=x1s[t * 128:(t + 1) * 128, :]), reads=K('x1_', t), writes=[x1k])
                S.op('dve', lambda e: e.memset(idxf[:], 0.0), writes=['idxf'])
                S.op('dve', lambda e: e.memset(zs[:], 0.0), writes=['zs'])
                layer_norm_rows(st, mv[:], rs[:], xb_[:], h2[:], x1k, 'h2', 'p4')
                S.op('dve', lambda e: e.tensor_tensor(out=h2[:], in0=h2[:], in1=bct["sc2b"][:], op=ALU.mult), reads=['h2', 'sc2b'], writes=['h2'])
                S.op('dve', lambda e: e.tensor_tensor(out=h2[:], in0=h2[:], in1=bct["sh2b"][:], op=ALU.add), reads=['h2', 'sh2b'], writes=['h2'])
                S.op('act', lambda e: e.copy(out=h2b[p][:], in_=h2[:]), reads=['h2'], writes=['h2b%d' % p])
                yield
                for half in range(2):
                    for c4 in range(4):
                        ch = half * 4 + c4
                        S.op('pe', lambda e, ch=ch, c4=c4, half=half: e.transpose(PB[half][:, c4 * 128:(c4 + 1) * 128], h2[:, ch * 128:(ch + 1) * 128], ident[:]),
                             reads=['h2', 'ident'], writes=['pb%d' % half], skip_self=True)
                    yield
                    S.op('act', lambda e, half=half: e.copy(out=h2T[:, half * 4:(half + 1) * 4, :], in_=PB[half][:, :].rearrange("p (j c) -> p j c", c=128)), reads=['pb%d' % half], writes=['h2T'])
                for g in range(4):
                    for c4 in range(4):
                        c = g * 4 + c4
                        for kc in range(8):
                            S.op('pe', lambda e, c=c, c4=c4, kc=kc, g=g: e.matmul(PB[2 + g][:, c4 * 128:(c4 + 1) * 128], lhsT=wqb[:, kc, c * 128:(c + 1) * 128], rhs=h2T[:, kc, :],
                                                                                start=(kc == 0), stop=(kc == 7)), reads=['wqb%d_%d' % (kc, c // 8), 'h2T'], writes=['pb%d' % (2 + g)], skip_self=True)
                    yield
                    S.op('act' if g % 2 == 0 else 'dve', lambda e, g=g: (e.copy if g % 2 == 0 else e.tensor_copy)(out=qT[:, g * 4:(g + 1) * 4, :], in_=PB[2 + g][:, :].rearrange("p (j c) -> p j c", c=128)),
                         reads=['pb%d' % (2 + g)], writes=['qT'])
                for g in range(4):
                    for c4 in range(4):
                        c = g * 4 + c4
                        S.op('pe', lambda e, c=c, c4=c4, g=g: e.matmul(PB[2 + g][:, c4 * 128:(c4 + 1) * 128], lhsT=qT[:, c, :], rhs=keysT[:, c, :], start=True, stop=True),
                             reads=['qT', 'keysT%d' % (c // 8)], writes=['pb%d' % (2 + g)], skip_self=True)
                    yield
                    S.op('act' if g % 2 == 0 else 'dve', lambda e, g=g: (e.copy if g % 2 == 0 else e.tensor_copy)(out=sc[:, g * 4:(g + 1) * 4, :], in_=PB[2 + g][:, :].rearrange("p (j c) -> p j c", c=128)),
                         reads=['pb%d' % (2 + g)], writes=['sc'])
                for c in range(16):
                    S.op('dve', lambda e, c=c: e.max(out=top[:, c, 0:8], in_=sc[:, c, :]), reads=['sc'], writes=['top'])
                    S.op('dve', lambda e, c=c: e.max_index(out=topi[:, c, 0:8], in_max=top[:, c, 0:8], in_values=sc[:, c, :]), reads=['sc', 'top'], writes=['topi'])
                    S.op('dve', lambda e, c=c: e.match_replace(out=scw[:, c, :], in_to_replace=top[:, c, 0:8], in_values=sc[:, c, :], imm_value=-1e30), reads=['sc', 'top'], writes=['scw'])
                    S.op('dve', lambda e, c=c: e.max(out=top[:, c, 8:16], in_=scw[:, c, :]), reads=['scw'], writes=['top'])
                    S.op('dve', lambda e, c=c: e.max_index(out=topi[:, c, 8:16], in_max=top[:, c, 8:16], in_values=scw[:, c, :]), reads=['scw', 'top'], writes=['topi'])
                    yield
                S.op('dve', lambda e: e.tensor_copy(out=topf[:], in_=topi[:]), reads=['topi'], writes=['topf'])
                t4 = top[:].rearrange("p (h two) k -> p h two k", two=2); f4 = topf[:].rearrange("p (h two) k -> p h two k", two=2)
                c4v = cand[:].rearrange("p h (a b) -> p h a b", b=16)
                for h in range(8):
                    S.op('dve', lambda e, h=h: e.tensor_tensor(out=c4v[:, h, :, :], in0=t4[:, h, 0, :].unsqueeze(2).to_broadcast([128, 16, 16]), in1=t4[:, h, 1, :].unsqueeze(1).to_broadcast([128, 16, 16]), op=ALU.add),
                         reads=['top'], writes=['scw'])
                    yield
                for h in range(8):
                    S.op('dve', lambda e, h=h: e.max(out=best[:, h, 0:8], in_=cand[:, h, :]), reads=['scw'], writes=['best'])
                    S.op('dve', lambda e, h=h: e.match_replace(out=candw[:, h, :], in_to_replace=best[:, h, 0:8], in_values=cand[:, h, :], imm_value=-1e30), reads=['scw', 'best'], writes=['candw'])
                    S.op('dve', lambda e, h=h: e.max(out=best[:, h, 8:16], in_=candw[:, h, :]), reads=['candw'], writes=['best'])
                    S.op('dve', lambda e, h=h: e.max_index(out=posi[:, h, 0:8], in_max=best[:, h, 0:8], in_values=cand[:, h, :]), reads=['scw', 'best'], writes=['posi'])
                    S.op('dve', lambda e, h=h: e.max_index(out=posi[:, h, 8:16], in_max=best[:, h, 8:16], in_values=candw[:, h, :]), reads=['candw', 'best'], writes=['posi'])
                    yield
                pflat = posi[:].rearrange("p h k -> p (h k)")
                S.op('dve', lambda e: e.tensor_single_scalar(out=pai[:], in_=pflat, scalar=4, op=ALU.logical_shift_right), reads=['posi'], writes=['pai'])
                S.op('dve', lambda e: e.tensor_single_scalar(out=pbi[:], in_=pflat, scalar=15, op=ALU.bitwise_and), reads=['posi'], writes=['pbi'])
                S.op('dve', lambda e: e.tensor_copy(out=paf[:], in_=pai[:]), reads=['pai'], writes=['paf'])
                S.op('dve', lambda e: e.tensor_copy(out=pbf[:], in_=pbi[:]), reads=['pbi'], writes=['pbf'])
                yield
                eq4 = eq[:].rearrange("p (h k) a -> p h k a", k=16)
                for (pp, pk_, half_, dst, dn) in ((paf, 'paf', 0, iaf, 'iaf'), (pbf, 'pbf', 1, ibf, 'ibf')):
                    S.op('dve', lambda e, pp=pp: e.tensor_tensor(out=eq[:], in0=iota16[:].unsqueeze(1).to_broadcast([128, 128, 16]), in1=pp[:].unsqueeze(2).to_broadcast([128, 128, 16]), op=ALU.is_equal),
                         reads=['iota16', pk_, 'candw'], writes=['candw'])
                    S.op('dve', lambda e, half_=half_: e.tensor_tensor(out=eq4, in0=eq4, in1=f4[:, :, half_, :].unsqueeze(2).to_broadcast([128, 8, 16, 16]), op=ALU.mult),
                         reads=['candw', 'topf'], writes=['candw'])
                    S.op('dve', lambda e, dst=dst: e.tensor_reduce(out=dst[:], in_=eq[:], axis=AX.X, op=ALU.add), reads=['candw'], writes=[dn])
                    yield
                S.op('dve', lambda e: e.scalar_tensor_tensor(out=idxf[:], in0=iaf[:], scalar=128.0, in1=ibf[:], op0=ALU.mult, op1=ALU.add), reads=['iaf', 'ibf'], writes=['idxf'])
                S.op('dve', lambda e: e.tensor_scalar_min(out=idxf[:], in0=idxf[:], scalar1=16383.0), reads=['idxf'], writes=['idxf'])
                S.op('dve', lambda e: e.tensor_copy(out=idxi[p][:], in_=idxf[:]), reads=['idxf'], writes=['idxi%d' % p])
                S.op('dve', lambda e: e.tensor_scalar_mul(out=nmx[:], in0=best[:, :, 0], scalar1=-1.0), reads=['best'], writes=['nmx'])
                g3 = gate[p][:].rearrange("p (h k) -> p h k", k=16)
                for h in range(8):
                    S.op('act', lambda e, h=h: e.activation(out=g3[:, h, :], in_=best[:, h, :], func=AF.Exp, bias=nmx[:, h:h + 1], scale=1.0, accum_out=zs[:, h:h + 1]),
                         reads=['best', 'nmx'], writes=['gate%d' % p, 'zs'])
                S.op('dve', lambda e: e.reciprocal(out=zs[:], in_=zs[:]), reads=['zs'], writes=['zs'])
                S.op('dve', lambda e: e.tensor_tensor(out=g3, in0=g3, in1=zs[:].unsqueeze(2).to_broadcast([128, 8, 16]), op=ALU.mult), reads=['gate%d' % p, 'zs'], writes=['gate%d' % p])

            def fused(t, gen=None):
                p = t % 2
                LAG = 2

                def tail(kk):
                    s_ = (t * 128 + kk) % NG; d4 = kk % 4
                    S.op('dve', lambda e: e.tensor_scalar(out=dgk[d4][:], in0=identb[:], scalar1=gl[:, kk:kk + 1], scalar2=gate[p][:, kk:kk + 1], op0=ALU.mult, op1=ALU.mult),
                         reads=['identb', 'gl_%d' % kk, 'gate%d' % p], writes=['dgk%d' % d4])
                    for half in range(2):
                        S.op('pe', lambda e, half=half: e.matmul(PB[6 + half][:, :], lhsT=dgk[d4][:], rhs=uvb[s_][:, 1024 + half * 512:1024 + (half + 1) * 512], start=(kk == 0), stop=(kk == 127)),
                             reads=['dgk%d' % d4, 'uvb%d' % s_], writes=['pb%d' % (6 + half)], skip_self=True)

                for k in range(128):
                    s_ = (t * 128 + k) % NG; j4 = k % 3
                    dk = 'dots_%d' % k
                    S.dma('pool', lambda e, k=k, s_=s_: e.indirect_dma_start(out=uvb[s_][:], out_offset=None, in_=uv_d[:, :], in_offset=bass.IndirectOffsetOnAxis(ap=idxi[p][:, k:k + 1], axis=0)),
                          reads=['idxi%d' % p], writes=['uvb%d' % s_])
                    S.op('dve', lambda e, k=k, s_=s_, j4=j4: e.tensor_tensor(out=pr[j4][:], in0=uvb[s_][:, 0:1024], in1=h2b[p][:], op=ALU.mult),
                         reads=['uvb%d' % s_, 'h2b%d' % p], writes=['pr%d' % j4])
                    S.op('act', lambda e, k=k, j4=j4: e.activation(out=junk[:], in_=pr[j4][:], func=AF.Identity, accum_out=dots[:, k:k + 1]),
                         reads=['pr%d' % j4, 'dots0'], writes=[dk])
                    S.op('act', lambda e, k=k: e.activation(out=gl[:, k:k + 1], in_=dots[:, k:k + 1], func=AF.Gelu), reads=[dk], writes=['gl_%d' % k])
                    if k >= LAG:
                        tail(k - LAG)
                    if gen is not None and k % 2 == 1:
                        next(gen, None)
                for kk in range(128 - LAG, 128):
                    tail(kk)
                if gen is not None:
                    for _ in gen:
                        pass
                xb_ = x1t[p]; x1k = 'x1t%d' % p
                for half in range(2):
                    S.op('dve', lambda e, half=half: e.tensor_tensor(out=yb[:, half * 512:(half + 1) * 512], in0=PB[6 + half][:, :], in1=bct["g2b"][:, half * 512:(half + 1) * 512], op=ALU.mult),
                         reads=['pb%d' % (6 + half), 'g2b', 'h2'], writes=['h2'])
                S.op('dve', lambda e: e.scalar_tensor_tensor(out=fin[:], in0=xb_[:], scalar=ALPHA, in1=yb[:], op0=ALU.mult, op1=ALU.add), reads=[x1k, 'h2', 'sc'], writes=['sc'])
                layer_norm_rows(stf, mvf[:], rsf[:], fin[:], fin[:], 'sc', 'sc', 'p4f')
                S.op('dve', lambda e: e.tensor_tensor(out=fin[:], in0=fin[:], in1=bct["ln2g"][:], op=ALU.mult), reads=['sc', 'ln2g'], writes=['sc'])
                S.op('dve', lambda e: e.tensor_tensor(out=fin[:], in0=fin[:], in1=bct["ln2b"][:], op=ALU.add), reads=['sc', 'ln2b'], writes=['sc'])
                S.dma('sp', lambda e: e.dma_start(out=out_d[t * 128:(t + 1) * 128, :], in_=fin[:]), reads=['sc'], writes=['out'])

            stf = sb(p4, "st4f", [128, 2, 6]); mvf = sb(p4, "mv4f", [128, 2]); rsf = sb(p4, "rs4f", [128, 1])
            S.op('dve', lambda e: e.memset(dots[:], 0.0), writes=['dots0'])
            S.wait_all('pool', ['tabs_%d' % i for i in range(512)])
            if NT4:
                for _ in prologue(0):
                    pass
            for t in range(NT4):
                fused(t, prologue(t + 1) if t + 1 < NT4 else None)
            S.wait_all('sp', ['out', 'dbg'])
            S.barrier()
    return nc


def _prep_shared(inp):
    f = np.float32
    sh = {}
    sh["w_mod"] = np.ascontiguousarray(inp["w_mod"][0].reshape(8, 128, 6144).transpose(1, 0, 2))
    sh["b_modT"] = np.ascontiguousarray(inp["b_mod"][0].reshape(48, 128).T)
    sh["w_in"] = np.ascontiguousarray(inp["w_in"][0].reshape(8, 128, 4624).transpose(1, 0, 2))
    sh["lgT"] = np.ascontiguousarray(inp["hg_lb_logits"].reshape(2, 2, 4, 128).transpose(3, 0, 1, 2))
    sh["hgn"] = np.ascontiguousarray(np.broadcast_to(inp["hg_norm_g"][0][None, :], (64, 512)))
    sh["mln"] = np.ascontiguousarray(np.broadcast_to(inp["ml_norm_g"][0][None, :], (64, 512)))
    sh["convw"] = np.ascontiguousarray(inp["ml_conv_w"][0].reshape(9, 8, 128).transpose(2, 0, 1))
    sh["convb"] = np.ascontiguousarray(inp["ml_conv_b"][0].reshape(8, 128).T)
    sh["gateb"] = np.ascontiguousarray(inp["ml_gate_b"][0].reshape(2, 8).T)
    sh["w_out"] = np.ascontiguousarray(inp["w_out"][0].reshape(8, 128, 1024).transpose(1, 0, 2))
    for nm, key in (("ln1g", "ln1_g"), ("ln1b", "ln1_b"), ("ln2g", "ln2_g"), ("ln2b", "ln2_b")):
        sh[nm] = np.ascontiguousarray(np.broadcast_to(inp[key][0][None, :], (128, 1024)))
    sh["wq"] = np.ascontiguousarray(inp["peer_wq"][0].reshape(8, 128, 2048).transpose(1, 0, 2))
    sh["keysT"] = np.ascontiguousarray(inp["peer_keys"][0].reshape(16, 128, 128).transpose(2, 0, 1))
    nexp = 128 if os.environ.get("KDEBUG") else 16384
    sh["pu"] = np.ascontiguousarray(inp["peer_u"][0][:nexp])
    sh["pv"] = np.ascontiguousarray(inp["peer_v"][0][:nexp])
    sh["ident"] = np.eye(128, dtype=f)
    m = np.zeros((64, 2, 64), f)
    s = np.arange(64)[:, None]; c = np.arange(64)[None, :]
    m[:, 0, :] = (s <= c); m[:, 1, :] = (s >= c)
    sh["masks"] = m
    rm = np.ones((128, 512), f); rm[:, ::64] = 0.0
    sh["rmask"] = rm
    sh["sel8"] = np.eye(8, dtype=f)
    sh["iota16"] = np.ascontiguousarray(np.broadcast_to(np.arange(16, dtype=f)[None, :], (128, 16)))
    dm = np.zeros((8, 2), f); dm[0:4, 0] = 1.0; dm[4:8, 1] = 1.0
    sh["dirm"] = dm
    return {k: np.asarray(v, dtype=f) for k, v in sh.items()}


def kernel(**inputs):
    inp = {k: np.asarray(v) for k, v in inputs.items()}
    debug = os.environ.get("KDEBUG") or None
    nc = build(debug)
    sh = _prep_shared(inp)
    in_maps = []
    for b in range(8):
        m = dict(sh)
        m["xs"] = np.ascontiguousarray(np.concatenate([inp["ctx"][b], inp["x"][b]], axis=0).astype(np.float32))
        m["cT"] = np.ascontiguousarray(np.stack([inp["c"][b], inp["c_ctx"]], axis=-1).reshape(8, 128, 2).transpose(1, 0, 2).astype(np.float32))
        in_maps.append(m)
    res = run_bass_kernel_spmd(nc, in_maps, core_ids=list(range(8)))
    key = "dbg" if debug else "out"
    return np.stack([np.asarray(r[key]) for r in res.results], axis=0).astype(np.float32)
```

```python
import os
import numpy as np
from contextlib import ExitStack
import concourse.bass as bass
import concourse.mybir as mybir
from concourse.bass_utils import run_bass_kernel_spmd

F32 = mybir.dt.float32; BF16 = mybir.dt.bfloat16; I32 = mybir.dt.int32; U32 = mybir.dt.uint32
AF = mybir.ActivationFunctionType; ALU = mybir.AluOpType; AX = mybir.AxisListType

NTOK = 2304; NLAT = 2048; NCH = 36; NLCH = 32
ALPHA = 2.0 ** 0.25
EPS = 1e-6


class Sched:
    NDMA = 32

    def __init__(self, nc, es):
        self.nc = nc
        self.engs = {'pe': nc.tensor, 'act': nc.scalar, 'dve': nc.vector, 'pool': nc.gpsimd, 'sp': nc.sync}
        self.sem = {k: es.enter_context(nc.semaphore("sem_" + k)) for k in self.engs}
        self.cnt = {k: 0 for k in self.engs}
        self.dsem = [es.enter_context(nc.semaphore("dsem%d" % i)) for i in range(self.NDMA)]
        self.dcnt = [0] * self.NDMA
        self.dnext = 0
        self.seen = {k: {} for k in self.engs}
        self.bufs = {}

    def _deps(self, reads, writes):
        deps = []
        for r in reads:
            b = self.bufs.get(r)
            if b and b['w'] is not None:
                deps.append(b['w'])
        for w in writes:
            b = self.bufs.get(w)
            if b:
                if b['w'] is not None:
                    deps.append(b['w'])
                deps.extend(b['r'])
        return deps

    def _wait(self, eng, deps, skip_self=False):
        best = {}
        for (sid, sem, val, owner) in deps:
            if skip_self and owner == eng:
                continue
            if best.get(sid, (None, 0))[1] < val:
                best[sid] = (sem, val)
        for sid, (sem, val) in best.items():
            if self.seen[eng].get(sid, 0) >= val:
                continue
            self.engs[eng].wait_ge(sem, val)
            self.seen[eng][sid] = val

    def _record(self, dep, reads, writes):
        for r in reads:
            b = self.bufs.setdefault(r, {'w': None, 'r': []})
            b['r'] = [d for d in b['r'] if d[0] != dep[0]] + [dep]
        for w in writes:
            self.bufs[w] = {'w': dep, 'r': []}

    @staticmethod
    def _split(keys):
        norm, ps = [], []
        for k in keys:
            if k.startswith('pb') and len(k) > 2 and k[2].isdigit():
                ps.append(k[:3])
            else:
                norm.append(k)
        return norm, ps

    def op(self, eng, fn, reads=(), writes=(), skip_self=False):
        reads, pr = self._split(reads)
        writes, pw = self._split(writes)
        banks = sorted(set(pr + pw))
        deps = self._deps(reads, writes)
        deps += [d for d in self._deps((), banks) if d[3] != eng]
        self._wait(eng, deps, skip_self)
        ins = fn(self.engs[eng])
        self.cnt[eng] += 1
        ins.then_inc(self.sem[eng], 1)
        self._record(('e_' + eng, self.sem[eng], self.cnt[eng], eng), reads, list(writes) + banks)
        return ins

    def dma(self, q, fn, reads=(), writes=()):
        deps = self._deps(reads, writes)
        j = self.dnext
        self.dnext = (self.dnext + 1) % self.NDMA
        if self.dcnt[j] > 0:
            deps = deps + [('d%d' % j, self.dsem[j], self.dcnt[j], 'dma')]
        self._wait(q, deps)
        ins = fn(self.engs[q])
        self.dcnt[j] += 16
        ins.then_inc(self.dsem[j], 16)
        self._record(('d%d' % j, self.dsem[j], self.dcnt[j], 'dma'), reads, writes)

    def wait_all(self, eng, keys):
        deps = []
        for k in keys:
            b = self.bufs.get(k)
            if b:
                if b['w'] is not None:
                    deps.append(b['w'])
                deps.extend(b['r'])
        self._wait(eng, deps)

    def barrier(self):
        for e in self.engs:
            deps = [('e_' + f, self.sem[f], self.cnt[f], f) for f in self.engs if f != e and self.cnt[f] > 0]
            deps += [('d%d' % j, self.dsem[j], self.dcnt[j], 'dma') for j in range(self.NDMA) if self.dcnt[j] > 0]
            self._wait(e, deps)


class View:
    def __init__(self, ap):
        self.ap = ap

    def __getitem__(self, idx):
        return self.ap[idx]


def K(name, a, b=None):
    if b is None:
        return ["%s%d" % (name, a)]
    return ["%s%d" % (name, i) for i in range(a, b)]


def build(debug=None):
    nc = bass.Bass("TRN2", target_bir_lowering=False)
    D = {}

    def din(name, shape, dt=F32):
        D[name] = nc.dram_tensor(name, shape, dt, kind="ExternalInput").ap()
        return D[name]

    xs = din("xs", [NTOK, 1024]); cT_d = din("cT", [128, 8, 2]); wmod_d = din("w_mod", [128, 8, 6144])
    bmod_d = din("b_modT", [128, 48]); winb_d = din("w_inb", [128, 36, 8, 128]); wing_d = din("w_ing", [128, 8, 16]); lg_d = din("lgT", [128, 2, 2, 4])
    hgn_d = din("hgn", [64, 512]); mln_d = din("mln", [64, 512]); convw_d = din("convw", [128, 9, 8])
    convb_d = din("convb", [128, 8]); gateb_d = din("gateb", [8, 2]); wout_d = din("w_out", [128, 8, 1024])
    ln1g_d = din("ln1g", [128, 1024]); ln1b_d = din("ln1b", [128, 1024]); ln2g_d = din("ln2g", [128, 1024])
    ln2b_d = din("ln2b", [128, 1024]); wq_d = din("wq", [128, 8, 2048]); keysT_d = din("keysT", [128, 16, 128])
    NEXP = 128 if debug else 16384
    pu_d = din("pu", [NEXP, 1024]); pv_d = din("pv", [NEXP, 1024])
    ident_d = din("ident", [128, 128]); masks_d = din("masks", [64, 2, 64]); rmask_d = din("rmask", [128, 512])
    sel8_d = din("sel8", [8, 8]); dirm_d = din("dirm", [8, 2]); iota_d = din("iota16", [128, 16])
    out_d = nc.dram_tensor("out", [NLAT, 1024], F32, kind="ExternalOutput").ap()
    dbg_d = None
    if debug:
        dbg_d = nc.dram_tensor("dbg", [NLAT, 1024] if debug != 'mix' else [1024, NLAT], F32, kind="ExternalOutput").ap()

    with ExitStack() as es:
        S = Sched(nc, es)

        uid = [0]

        def sb(st, name, shape, dt=F32):
            uid[0] += 1
            return st.enter_context(nc.sbuf_tensor("s%d_%s" % (uid[0], name), shape, dt))

        PB = [es.enter_context(nc.psum_tensor("pb%d" % i, [128, 512], F32)) for i in range(8)]

        ident = sb(es, "ident", [128, 128]); identb = sb(es, "identb", [128, 128], BF16)
        masks = sb(es, "masks", [64, 2, 64]); rmask = sb(es, "rmask", [128, 512])
        ones = sb(es, "ones", [128, 128]); epsc = sb(es, "epsc", [128, 1])
        modv = sb(es, "modv", [128, 48, 2])
        S.dma('sp', lambda e: e.dma_start(out=ident[:], in_=ident_d[:, :]), writes=['ident'])
        S.dma('sp', lambda e: e.dma_start(out=masks[:], in_=masks_d[:, :, :]), writes=['masks'])
        S.dma('sp', lambda e: e.dma_start(out=rmask[:], in_=rmask_d[:, :]), writes=['rmask'])
        S.op('dve', lambda e: e.tensor_copy(out=identb[:], in_=ident[:]), reads=['ident'], writes=['identb'])
        S.op('dve', lambda e: e.memset(ones[:], 1.0), writes=['ones'])
        S.op('dve', lambda e: e.memset(epsc[:], EPS), writes=['epsc'])

        with ExitStack() as p0:
            cT = sb(p0, "cT", [128, 8, 2]); scT = sb(p0, "scT", [128, 8, 2]); bmodT = sb(p0, "bmodT", [128, 48])
            wm = [sb(p0, "wm%d" % i, [128, 6144]) for i in range(2)]
            S.dma('sp', lambda e: e.dma_start(out=cT[:], in_=cT_d[:, :, :]), writes=['cT'])
            S.dma('sp', lambda e: e.dma_start(out=bmodT[:], in_=bmod_d[:, :]), writes=['bmodT'])
            S.op('act', lambda e: e.activation(out=scT[:], in_=cT[:], func=AF.Silu), reads=['cT'], writes=['scT'])
            for kc in range(8):
                w = wm[kc % 2]; wk = 'wm%d' % (kc % 2)
                S.dma('sp' if kc % 2 == 0 else 'pool', lambda e, w=w, kc=kc: e.dma_start(out=w[:], in_=wmod_d[:, kc, :]), writes=[wk])
                for j in range(48):
                    S.op('pe', lambda e, w=w, kc=kc, j=j: e.matmul(PB[kc // 4][:, (kc % 4) * 96 + 2 * j:(kc % 4) * 96 + 2 * j + 2], lhsT=w[:, j * 128:(j + 1) * 128], rhs=scT[:, kc, :],
                                                                 start=True, stop=True),
                         reads=[wk, 'scT'], writes=['pb%d' % (kc // 4)], skip_self=True)
            mflat = modv[:].rearrange("p j n -> p (j n)")
            S.op('dve', lambda e: e.tensor_tensor(out=modv[:], in0=PB[0][:, 0:96].rearrange("p (j n) -> p j n", n=2), in1=bmodT[:].unsqueeze(2).to_broadcast([128, 48, 2]), op=ALU.add),
                 reads=['pb0', 'bmodT'], writes=['modv'])
            for kc in range(1, 8):
                S.op('dve', lambda e, kc=kc: e.tensor_tensor(out=mflat, in0=mflat, in1=PB[kc // 4][:, (kc % 4) * 96:(kc % 4) * 96 + 96], op=ALU.add),
                     reads=['pb%d' % (kc // 4), 'modv'], writes=['modv'])
            S.op('dve', lambda e: e.tensor_scalar_add(out=modv[:, 8:16, :], in0=modv[:, 8:16, :], scalar1=1.0), reads=['modv'], writes=['modv'])
            S.op('dve', lambda e: e.tensor_scalar_add(out=modv[:, 32:40, :], in0=modv[:, 32:40, :], scalar1=1.0), reads=['modv'], writes=['modv'])
            S.barrier()
        if debug == 'p0':
            S.dma('sp', lambda e: e.dma_start(out=dbg_d[0:128, 0:96], in_=modv[:].rearrange("p a b -> p (a b)")), reads=['modv'], writes=['dbg'])
            S.wait_all('sp', ['dbg']); S.barrier()
            return nc

        scA = ExitStack(); scB = ExitStack()
        mixT = sb(scA, "mixT", [128, 8, NLAT], BF16)
        hT = sb(scB, "hT", [128, 8, NTOK], BF16)
        wstg = sb(scB, "wstg", [128, 8, 8])
        tstg = [sb(scB, "tstg%d" % i, [128, 512]) for i in range(2)]
        tbf = [sb(scB, "tbf%d" % i, [128, 512], BF16) for i in range(2)]
        uv_d = nc.dram_tensor("uvbf", [NEXP, 2048], BF16, kind="Internal").ap()
        prep_pos = [0]

        def prep_tables(npieces):
            if debug:
                return
            for _ in range(npieces):
                i = prep_pos[0]
                if i >= 512:
                    return
                prep_pos[0] += 1
                src, coff = (pu_d, 0) if i < 256 else (pv_d, 1024)
                r = (i % 256) // 2; c = (i % 2) * 512; bb = i % 2
                S.dma('sp', lambda e, src=src, r=r, c=c, bb=bb: e.dma_start(out=tstg[bb][:], in_=src[r * 128:(r + 1) * 128, c:c + 512]), writes=['tstg%d' % bb])
                S.op('pool', lambda e, bb=bb: e.tensor_copy(out=tbf[bb][:], in_=tstg[bb][:]), reads=['tstg%d' % bb], writes=['tbf%d' % bb])
                S.dma('pool', lambda e, coff=coff, r=r, c=c, bb=bb: e.dma_start(out=uv_d[r * 128:(r + 1) * 128, coff + c:coff + c + 512], in_=tbf[bb][:]), reads=['tbf%d' % bb], writes=['tabs_%d' % i])

        def layer_norm_rows(st_ap, mv_ap, rstd_ap, src, dst, skey, dkey, tag):
            S.op('dve', lambda e: e.bn_stats(out=st_ap[:, 0, :], in_=src[:, 0:512]), reads=[skey], writes=[tag + 'st'])
            S.op('dve', lambda e: e.bn_stats(out=st_ap[:, 1, :], in_=src[:, 512:1024]), reads=[skey], writes=[tag + 'st'])
            S.op('dve', lambda e: e.bn_aggr(out=mv_ap, in_=st_ap[:].rearrange("p a b -> p (a b)")), reads=[tag + 'st'], writes=[tag + 'mv'])
            S.op('act', lambda e: e.activation(out=rstd_ap, in_=mv_ap[:, 1:2], func=AF.Sqrt, bias=epsc[:, 0:1], scale=1.0),
                 reads=[tag + 'mv', 'epsc'], writes=[tag + 'rs'])
            S.op('dve', lambda e: e.reciprocal(out=rstd_ap, in_=rstd_ap), reads=[tag + 'rs'], writes=[tag + 'rs'])
            S.op('dve', lambda e: e.tensor_scalar(out=dst, in0=src, scalar1=mv_ap[:, 0:1], scalar2=rstd_ap, op0=ALU.subtract, op1=ALU.mult),
                 reads=[skey, tag + 'mv', tag + 'rs'], writes=[dkey])

        with ExitStack() as p1:
            xt = [sb(p1, "xt%d" % i, [128, 1024]) for i in range(2)]
            xn = [sb(p1, "xn%d" % i, [128, 1024]) for i in range(2)]
            st = [sb(p1, "st%d" % i, [128, 2, 6]) for i in range(2)]
            mv = [sb(p1, "mv%d" % i, [128, 2]) for i in range(2)]
            rs = [sb(p1, "rs%d" % i, [128, 1]) for i in range(2)]
            PSAP = bool(os.environ.get("KPSAP"))
            for t in range(18):
                b = t % 2
                n = 1 if t < 2 else 0
                S.dma('sp' if b == 0 else 'pool', lambda e, t=t, b=b: e.dma_start(out=xt[b][:], in_=xs[t * 128:(t + 1) * 128, :]), writes=['xt%d' % b])
                layer_norm_rows(st[b], mv[b][:], rs[b][:], xt[b][:], xn[b][:], 'xt%d' % b, 'xn%d' % b, 'p1%d' % b)
                for half in range(2):
                    bank = 2 * b + half
                    pk = 'pb%d' % bank
                    for c4 in range(4):
                        ch = half * 4 + c4
                        S.op('pe', lambda e, b=b, ch=ch, c4=c4, bank=bank: e.transpose(PB[bank][:, c4 * 128:(c4 + 1) * 128], xn[b][:, ch * 128:(ch + 1) * 128], ident[:]),
                             reads=['xn%d' % b, 'ident'], writes=[pk], skip_self=True)
                    if not PSAP:
                        S.op('act', lambda e, b=b, half=half, bank=bank: e.copy(out=xt[b][:, half * 512:(half + 1) * 512], in_=PB[bank][:, :]), reads=[pk, 'xt%d' % b], writes=['xt%d' % b])
                    for c4 in range(4):
                        ch = half * 4 + c4
                        dst = hT[:, ch, t * 128:(t + 1) * 128]
                        src = PB[bank][:, c4 * 128:(c4 + 1) * 128] if PSAP else xt[b][:, ch * 128:(ch + 1) * 128]
                        S.op('dve', lambda e, dst=dst, src=src, ch=ch, n=n: e.tensor_scalar(out=dst, in0=src, scalar1=modv[:, 8 + ch, n:n + 1], scalar2=modv[:, ch, n:n + 1],
                                                                                      op0=ALU.mult, op1=ALU.add),
                             reads=([pk] if PSAP else ['xt%d' % b]) + ['modv'], writes=K('hT', t))
            S.barrier()

        if debug == 'p1':
            with ExitStack() as dd:
                hf = sb(dd, "hf", [128, 1024])
                for t in range(16):
                    S.op('dve', lambda e, t=t: e.tensor_copy(out=hf[:].rearrange("p (a b) -> p a b", b=128), in_=hT[:, :, 256 + t * 128:256 + (t + 1) * 128]), reads=K('hT', t + 2) + ['hf'], writes=['hf'])
                    S.dma('sp', lambda e, t=t: e.dma_start(out=dbg_d[t * 128:(t + 1) * 128, :], in_=hf[:]), reads=['hf'], writes=['dbg'])
                S.wait_all('sp', ['dbg']); S.barrier()
            scB.close(); scA.close()
            return nc
        def load_w(stk, name, col0, ncols):
            wb = sb(stk, name, [128, 8, ncols], BF16)
            if ncols == 128:
                S.dma('pool', lambda e: e.dma_start(out=wb[:], in_=winb_d[:, col0 // 128, :, :]), writes=[name])
            else:
                g0_ = col0 - 4608
                S.dma('sp', lambda e: e.dma_start(out=wstg[:, :, 0:ncols], in_=wing_d[:, :, g0_:g0_ + ncols]), writes=['wstg'])
                S.op('pool', lambda e: e.tensor_copy(out=wb[:], in_=wstg[:, :, 0:ncols]), reads=['wstg'], writes=[name])
            return wb

        BLKS = [(0, 512), (512, 512), (1024, 512), (1536, 512), (2048, 256)]

        def proj_fm_block(wb, wname, ncols, bank, t0, nt):
            for kc in range(8):
                S.op('pe', lambda e, kc=kc: e.matmul(PB[bank][0:ncols, 0:nt], lhsT=wb[:, kc, :], rhs=hT[:, kc, t0:t0 + nt], start=(kc == 0), stop=(kc == 7)),
                     reads=[wname] + K('hT', t0 // 128, (t0 + nt) // 128), writes=['pb%d' % bank], skip_self=True)

        def proj_tm_group(wb, wname, ncols, bank, c0, ncks):
            for j in range(ncks):
                n = c0 + j
                for kc in range(8):
                    S.op('pe', lambda e, kc=kc, j=j, n=n: e.matmul(PB[bank][0:64, j * ncols:(j + 1) * ncols], lhsT=hT[:, kc, n * 64:(n + 1) * 64], rhs=wb[:, kc, :],
                                                                   start=(kc == 0), stop=(kc == 7)),
                         reads=[wname] + K('hT', n // 2), writes=['pb%d' % bank], skip_self=True)

        S32 = sb(scB, "S32", [128, 2, 132]); Sbf = sb(scB, "Sbf", [128, 2, 132], BF16)
        stm = sb(scB, "stm", [64, 2, 64], BF16)
        usb = sb(scB, "usb", [128, 2, 132])

        def chunk_loop(dirn, KsT, QsT, Vi, QoT, Ku, Vu, dec_ap, W, evac, rk, tagk, su_ap=None, suk=()):
            order = list(range(36)) if dirn == 0 else [3, 2, 1, 0] + list(range(35, 3, -1))
            S.op('dve', lambda e: e.memset(S32[:, 0, :], 0.0), writes=['S32_0'])
            S.op('dve', lambda e: e.memset(Sbf[:, 0, :], 0.0), writes=['Sbf_0'])

            def pre(idx):
                n = order[idx]
                sl = slice(n * 64, (n + 1) * 64)
                s2 = idx % 2
                if n >= 4:
                    pst = PB[2 + s2][0:64, 0:64]; pstk = 'pb%d' % (2 + s2)
                    S.op('pe', lambda e: e.matmul(pst, lhsT=KsT[:, sl], rhs=QsT[:, sl], start=True, stop=True),
                         reads=rk, writes=[pstk], skip_self=True)
                    if su_ap is None:
                        S.op('dve', lambda e: e.tensor_tensor(out=stm[:, s2, :], in0=pst, in1=masks[:, dirn, :], op=ALU.mult),
                             reads=[pstk, 'masks'], writes=['stm%d' % s2])
                    else:
                        S.op('dve', lambda e: e.scalar_tensor_tensor(out=stm[:, s2, :], in0=pst, scalar=su_ap(n), in1=masks[:, dirn, :], op0=ALU.mult, op1=ALU.mult),
                             reads=[pstk, 'masks'] + list(suk), writes=['stm%d' % s2])
                if idx < 35:
                    pu = PB[6 + s2][:, 0:W]; puk = 'pb%d' % (6 + s2)
                    S.op('pe', lambda e: e.matmul(pu, lhsT=Ku[:, n, :], rhs=Vu[:, n, 0:W], start=True, stop=True),
                         reads=rk, writes=[puk], skip_self=True)
                    S.op('act', lambda e: e.copy(out=usb[:, s2, 0:W], in_=pu), reads=[puk], writes=['usb%d' % s2])

            pre(0)
            cur = 0
            for idx, n in enumerate(order):
                if idx + 1 < 36:
                    pre(idx + 1)
                sl = slice(n * 64, (n + 1) * 64)
                s2 = idx % 2
                if n >= 4:
                    po = PB[4 + s2][0:64, 0:W]; pok = 'pb%d' % (4 + s2)
                    S.op('pe', lambda e, po=po, s2=s2, n=n: e.matmul(po, lhsT=stm[:, s2, :], rhs=Vi[:, n, 0:W], start=True, stop=False),
                         reads=['stm%d' % s2] + rk, writes=[pok], skip_self=True)
                    S.op('pe', lambda e, po=po, sl=sl, cur=cur: e.matmul(po, lhsT=QoT[:, sl], rhs=Sbf[:, cur, 0:W], start=False, stop=True),
                         reads=['Sbf_%d' % cur] + rk, writes=[pok], skip_self=True)
                if idx < 35:
                    nxt = 1 - cur
                    S.op('dve', lambda e, n=n, cur=cur, nxt=nxt, s2=s2: e.scalar_tensor_tensor(out=Sbf[:, nxt, 0:W], in0=S32[:, cur, 0:W], scalar=dec_ap(n), in1=usb[:, s2, 0:W],
                                                                                            op0=ALU.mult, op1=ALU.add),
                         reads=['S32_%d' % cur, 'usb%d' % s2, tagk], writes=['Sbf_%d' % nxt])
                    S.op('dve', lambda e, n=n, cur=cur, nxt=nxt, s2=s2: e.scalar_tensor_tensor(out=S32[:, nxt, 0:W], in0=S32[:, cur, 0:W], scalar=dec_ap(n), in1=usb[:, s2, 0:W],
                                                                                            op0=ALU.mult, op1=ALU.add),
                         reads=['S32_%d' % cur, 'usb%d' % s2, tagk], writes=['S32_%d' % nxt])
                    cur = nxt
                if n >= 4:
                    evac(n - 4, n, po, pok)

        def transpose_chunks_to_mixT(src, skey, head):
            for g in range(4):
                bank = g % 2
                for j in range(8):
                    n = g * 8 + j
                    S.op('pe', lambda e, n=n, j=j, bank=bank: e.transpose(PB[bank][:, j * 64:(j + 1) * 64], src[:, n, :], ident[0:64, 0:64]),
                         reads=list(skey) + ['ident'], writes=['pb%d' % bank], skip_self=True)
                S.op('act', lambda e, g=g, bank=bank: e.copy(out=mixT[:, head, g * 512:(g + 1) * 512], in_=PB[bank][:, :]),
                     reads=['pb%d' % bank], writes=K('mixT', head))

        with ExitStack() as g0:
            lgT = sb(g0, "lgT", [128, 2, 2, 4]); lbT = sb(g0, "lbT", [128, 2, 4]); omlT = sb(g0, "omlT", [128, 2, 4]); nomlT = sb(g0, "nomlT", [128, 2, 4])
            hgn = sb(g0, "hgn", [64, 512])
            S.dma('sp', lambda e: e.dma_start(out=lgT[:], in_=lg_d[:, :, :, :]), writes=['lgT'])
            S.dma('sp', lambda e: e.dma_start(out=hgn[:], in_=hgn_d[:, :]), writes=['hgn'])
            S.op('dve', lambda e: e.tensor_tensor(out=lbT[:], in0=lgT[:, :, 0, :], in1=lgT[:, :, 1, :], op=ALU.subtract), reads=['lgT'], writes=['lbT'])
            S.op('act', lambda e: e.activation(out=lbT[:], in_=lbT[:], func=AF.Sigmoid), reads=['lbT'], writes=['lbT'])
            S.op('dve', lambda e: e.tensor_scalar(out=omlT[:], in0=lbT[:], scalar1=-1.0, scalar2=1.0, op0=ALU.mult, op1=ALU.add), reads=['lbT'], writes=['omlT'])
            S.op('dve', lambda e: e.tensor_scalar_mul(out=nomlT[:], in0=omlT[:], scalar1=-1.0), reads=['omlT'], writes=['nomlT'])
            QsT = [sb(g0, "gQsT%d" % d, [128, NTOK], BF16) for d in range(2)]
            KsT = [sb(g0, "gKsT%d" % d, [128, NTOK], BF16) for d in range(2)]
            QoT = [sb(g0, "gQoT%d" % d, [128, NTOK], BF16) for d in range(2)]
            Ku = [sb(g0, "gKu%d" % d, [64, NCH, 128], BF16) for d in range(2)]
            dec = sb(g0, "gdec", [128, 2, NCH])
            vtm = sb(g0, "gv", [64, NCH, 128], BF16); gs = sb(g0, "ggs", [64, NLCH, 128], BF16); oacc = sb(g0, "goacc", [64, NLCH, 128])
            qf = sb(g0, "gqf", [128, 512]); sg = sb(g0, "gsg", [128, 512]); lf = sb(g0, "glf", [128, 512]); key = sb(g0, "gkey", [128, 512])
            Bc = sb(g0, "gB", [128, 512]); T1 = sb(g0, "gT1", [128, 512]); T2 = sb(g0, "gT2", [128, 512]); khT = sb(g0, "gkhT", [128, 512], BF16)
            EE = [sb(g0, "gE%d" % i, [128, 512]) for i in range(4)]
            sq = sb(g0, "gsq", [64, NLCH, 128], BF16); ss = sb(g0, "gss", [64, NLCH])
            for hd in range(4):
                with ExitStack() as hs:
                    wq_ = load_w(hs, "gwq", 0 + hd * 128, 128); wi_ = load_w(hs, "gwi", 512 + hd * 128, 128); wg_ = load_w(hs, "gwg", 1024 + hd * 128, 128)
                    wf = [load_w(hs, "gwf0", 1536 + hd * 128, 128), load_w(hs, "gwf1", 2048 + hd * 128, 128)]
                    prep_tables(64)
                    for g in range(9):
                        bk = 3 + g % 2
                        proj_tm_group(wi_, "gwi", 128, bk, g * 4, 4)
                        S.op('act', lambda e, g=g, bk=bk: e.copy(out=vtm[:, g * 4:(g + 1) * 4, :], in_=PB[bk][0:64, :].rearrange("p (j c) -> p j c", c=128)),
                             reads=['pb%d' % bk], writes=['gv'])
                    for g in range(8):
                        bk = 3 + (g + 1) % 2
                        proj_tm_group(wg_, "gwg", 128, bk, 4 + g * 4, 4)
                        S.op('act', lambda e, g=g, bk=bk: e.activation(out=gs[:, g * 4:(g + 1) * 4, :], in_=PB[bk][0:64, :].rearrange("p (j c) -> p j c", c=128), func=AF.Silu),
                             reads=['pb%d' % bk], writes=['ggs'])
                    for (t0, nt) in BLKS:
                        nck = nt // 64; c0 = t0 // 64
                        proj_fm_block(wq_, "gwq", 128, 0, t0, nt)
                        S.op('act', lambda e, nt=nt: e.copy(out=qf[:, 0:nt], in_=PB[0][:, 0:nt]), reads=['pb0'], writes=['gqf'])
                        for d in range(2):
                            proj_fm_block(wf[d], "gwf%d" % d, 128, 1 + d, t0, nt)
                            col = d * 4 + hd
                            lbp = lbT[:, d, hd:hd + 1]; omp = omlT[:, d, hd:hd + 1]; nomp = nomlT[:, d, hd:hd + 1]
                            S.op('act', lambda e, d=d, nt=nt: e.activation(out=sg[:, 0:nt], in_=PB[1 + d][:, 0:nt], func=AF.Sigmoid), reads=['pb%d' % (1 + d)], writes=['gsg'])
                            S.op('act', lambda e, nt=nt, lbp=lbp, omp=omp: e.activation(out=lf[:, 0:nt], in_=sg[:, 0:nt], func=AF.Ln, bias=lbp, scale=omp),
                                 reads=['gsg', 'lbT', 'omlT'], writes=['glf'])
                            S.op('dve', lambda e, nt=nt, nomp=nomp, omp=omp: e.tensor_scalar(out=key[:, 0:nt], in0=sg[:, 0:nt], scalar1=nomp, scalar2=omp, op0=ALU.mult, op1=ALU.add),
                                 reads=['gsg', 'omlT', 'nomlT'], writes=['gkey'])
                            S.op('dve', lambda e, nt=nt: e.tensor_tensor_scan(out=Bc[:, 0:nt], data0=rmask[:, 0:nt], data1=lf[:, 0:nt], initial=0.0, op0=ALU.mult, op1=ALU.add),
                                 reads=['glf', 'rmask'], writes=['gB'])
                            B3 = Bc[:, 0:nt].rearrange("p (n c) -> p n c", c=64)
                            T3 = T1[:, 0:nt].rearrange("p (n c) -> p n c", c=64)
                            if d == 1:
                                S.op('dve', lambda e, B3=B3, T3=T3, nck=nck: e.tensor_tensor(out=T3, in0=B3[:, :, 63:64].to_broadcast([128, nck, 64]), in1=B3, op=ALU.subtract),
                                     reads=['gB'], writes=['gT1'])
                                S.op('dve', lambda e, nt=nt: e.tensor_tensor(out=Bc[:, 0:nt], in0=T1[:, 0:nt], in1=lf[:, 0:nt], op=ALU.add), reads=['gT1', 'glf'], writes=['gB'])
                            li = 63 if d == 0 else 0
                            S.op('act', lambda e, B3=B3, li=li, d=d, c0=c0, nck=nck: e.activation(out=dec[:, d, c0:c0 + nck], in_=B3[:, :, li], func=AF.Exp), reads=['gB'], writes=['gdec'])
                            T4 = T2[:, 0:nt].rearrange("p (n c) -> p n c", c=64)
                            S.op('dve', lambda e, B3=B3, T3=T3, nck=nck: e.tensor_tensor(out=T3, in0=B3, in1=B3[:, :, 32:33].to_broadcast([128, nck, 64]), op=ALU.subtract),
                                 reads=['gB'], writes=['gT1'])
                            S.op('dve', lambda e, B3=B3, T4=T4, nck=nck, li=li: e.tensor_tensor(out=T4, in0=B3[:, :, li:li + 1].to_broadcast([128, nck, 64]), in1=B3, op=ALU.subtract),
                                 reads=['gB'], writes=['gT2'])
                            S.op('act', lambda e, nt=nt: e.activation(out=EE[0][:, 0:nt], in_=T1[:, 0:nt], func=AF.Exp), reads=['gT1'], writes=['gE0'])
                            S.op('act', lambda e, nt=nt: e.activation(out=EE[1][:, 0:nt], in_=T1[:, 0:nt], func=AF.Exp, scale=-1.0), reads=['gT1'], writes=['gE1'])
                            S.op('act', lambda e, nt=nt: e.activation(out=EE[2][:, 0:nt], in_=Bc[:, 0:nt], func=AF.Exp), reads=['gB'], writes=['gE2'])
                            S.op('act', lambda e, nt=nt: e.activation(out=EE[3][:, 0:nt], in_=T2[:, 0:nt], func=AF.Exp), reads=['gT2'], writes=['gE3'])
                            S.op('dve', lambda e, nt=nt, t0=t0, d=d: e.tensor_tensor(out=QsT[d][:, t0:t0 + nt], in0=qf[:, 0:nt], in1=EE[0][:, 0:nt], op=ALU.mult),
                                 reads=['gqf', 'gE0'], writes=['gQsT%d' % d])
                            S.op('dve', lambda e, nt=nt, t0=t0, d=d: e.tensor_tensor(out=KsT[d][:, t0:t0 + nt], in0=key[:, 0:nt], in1=EE[1][:, 0:nt], op=ALU.mult),
                                 reads=['gkey', 'gE1'], writes=['gKsT%d' % d])
                            S.op('dve', lambda e, nt=nt, t0=t0, d=d: e.tensor_tensor(out=QoT[d][:, t0:t0 + nt], in0=qf[:, 0:nt], in1=EE[2][:, 0:nt], op=ALU.mult),
                                 reads=['gqf', 'gE2'], writes=['gQoT%d' % d])
                            S.op('dve', lambda e, nt=nt: e.tensor_tensor(out=khT[:, 0:nt], in0=key[:, 0:nt], in1=EE[3][:, 0:nt], op=ALU.mult), reads=['gkey', 'gE3'], writes=['gkhT'])
                            pbb = PB[3][:].bitcast(BF16)
                            for j in range(nck):
                                S.op('pe', lambda e, j=j: e.transpose(pbb[0:64, j * 128:(j + 1) * 128], khT[:, j * 64:(j + 1) * 64], identb[:]),
                                     reads=['gkhT', 'identb'], writes=['pb3'], skip_self=True)
                            S.op('act', lambda e, d=d, c0=c0, nck=nck: e.copy(out=Ku[d][:, c0:c0 + nck, :], in_=pbb[0:64, 0:nck * 128].rearrange("p (j c) -> p j c", c=128)),
                                 reads=['pb3'], writes=['gKu%d' % d])
                    for d in range(2):
                        def evac(nl, n, po, pok, d=d):
                            if d == 0:
                                S.op('act', lambda e: e.copy(out=oacc[:, nl, :], in_=po), reads=[pok], writes=K('goacc', nl))
                            else:
                                S.op('dve', lambda e: e.tensor_tensor(out=oacc[:, nl, :], in0=po, in1=oacc[:, nl, :], op=ALU.add), reads=[pok] + K('goacc', nl), writes=K('goacc', nl))
                        chunk_loop(d, KsT[d], QsT[d], vtm, QoT[d], Ku[d], vtm, lambda n, d=d: dec[:, d, n:n + 1], 128, evac,
                                   ['gQsT%d' % d, 'gKsT%d' % d, 'gQoT%d' % d, 'gKu%d' % d, 'gv'], 'gdec')
                    allo = K('goacc', 0, NLCH)
                    S.op('dve', lambda e: e.tensor_tensor(out=sq[:], in0=oacc[:], in1=oacc[:], op=ALU.mult), reads=allo, writes=['gsq'])
                    S.op('dve', lambda e: e.tensor_reduce(out=ss[:], in_=sq[:], axis=AX.X, op=ALU.add), reads=['gsq'], writes=['gss'])
                    S.op('act', lambda e: e.activation(out=ss[:], in_=ss[:], func=AF.Sqrt, bias=epsc[0:64, 0:1], scale=1.0 / 128.0), reads=['gss', 'epsc'], writes=['gss'])
                    S.op('dve', lambda e: e.reciprocal(out=ss[:], in_=ss[:]), reads=['gss'], writes=['gss'])
                    S.op('dve', lambda e: e.tensor_tensor(out=oacc[:], in0=oacc[:], in1=ss[:].unsqueeze(2).to_broadcast([64, NLCH, 128]), op=ALU.mult),
                         reads=allo + ['gss'], writes=allo)
                    S.op('dve', lambda e, hd=hd: e.tensor_tensor(out=oacc[:], in0=oacc[:], in1=hgn[:, hd * 128:(hd + 1) * 128].unsqueeze(1).to_broadcast([64, NLCH, 128]), op=ALU.mult),
                         reads=allo + ['hgn'], writes=allo)
                    S.op('dve', lambda e: e.tensor_tensor(out=oacc[:], in0=oacc[:], in1=gs[:], op=ALU.mult), reads=allo + ['ggs'], writes=allo)
                    transpose_chunks_to_mixT(oacc, allo, hd)
            S.barrier()

        with ExitStack() as m0:
            mln = sb(m0, "mln", [64, 512]); convw = sb(m0, "convw", [128, 9, 8]); convb = sb(m0, "convb", [128, 8])
            gateb = sb(m0, "gateb", [8, 2]); sel8 = sb(m0, "sel8", [8, 8]); dirm = sb(m0, "dirm", [8, 2])
            S.dma('sp', lambda e: e.dma_start(out=mln[:], in_=mln_d[:, :]), writes=['mln'])
            S.dma('sp', lambda e: e.dma_start(out=convw[:], in_=convw_d[:, :, :]), writes=['convw'])
            S.dma('sp', lambda e: e.dma_start(out=convb[:], in_=convb_d[:, :]), writes=['convb'])
            S.dma('sp', lambda e: e.dma_start(out=gateb[:], in_=gateb_d[:, :]), writes=['gateb'])
            S.dma('sp', lambda e: e.dma_start(out=sel8[:], in_=sel8_d[:, :]), writes=['sel8'])
            S.dma('sp', lambda e: e.dma_start(out=dirm[:], in_=dirm_d[:, :]), writes=['dirm'])
            RUU = sb(m0, "RUU", [64, NCH, 24]); dchunk = sb(m0, "dchunk", [128, 8, NCH])
            with ExitStack() as gp:
                wgi = load_w(gp, "mwgi", 4608, 8); wgf = load_w(gp, "mwgf", 4616, 8)
                LI = sb(gp, "LI", [8, NTOK]); LF = sb(gp, "LF", [8, NTOK]); Af = sb(gp, "Af", [8, NTOK]); Ab = sb(gp, "Ab", [8, NTOK]); Aa = sb(gp, "Aa", [8, NTOK])
                R = [sb(gp, "Rr%d" % i, [8, NTOK]) for i in range(3)]
                bd = sb(gp, "bd", [8, 8, NCH])
                for (t0, nt) in BLKS:
                    proj_fm_block(wgi, "mwgi", 8, 0, t0, nt)
                    S.op('act', lambda e, t0=t0, nt=nt: e.copy(out=LI[:, t0:t0 + nt], in_=PB[0][0:8, 0:nt]), reads=['pb0'], writes=['LI'])
                    proj_fm_block(wgf, "mwgf", 8, 1, t0, nt)
                    S.op('act', lambda e, t0=t0, nt=nt: e.copy(out=LF[:, t0:t0 + nt], in_=PB[1][0:8, 0:nt]), reads=['pb1'], writes=['LF'])
                S.op('dve', lambda e: e.tensor_scalar_add(out=LI[:], in0=LI[:], scalar1=gateb[:, 0:1]), reads=['LI', 'gateb'], writes=['LI'])
                S.op('act', lambda e: e.activation(out=LF[:], in_=LF[:], func=AF.Sigmoid, bias=gateb[:, 1:2], scale=1.0), reads=['LF', 'gateb'], writes=['LF'])
                S.op('act', lambda e: e.activation(out=LF[:], in_=LF[:], func=AF.Ln), reads=['LF'], writes=['LF'])
                for (t0, nt) in BLKS:
                    S.op('dve', lambda e, t0=t0, nt=nt: e.tensor_tensor_scan(out=Af[:, t0:t0 + nt], data0=rmask[0:8, 0:nt], data1=LF[:, t0:t0 + nt], initial=0.0, op0=ALU.mult, op1=ALU.add),
                         reads=['LF', 'rmask'], writes=['Af'])
                A3 = Af[:].rearrange("p (n c) -> p n c", c=64)
                tot = A3[:, :, 63:64]
                S.op('dve', lambda e: e.tensor_tensor(out=Ab[:].rearrange("p (n c) -> p n c", c=64), in0=tot.to_broadcast([8, NCH, 64]), in1=A3, op=ALU.subtract), reads=['Af'], writes=['Ab'])
                S.op('dve', lambda e: e.tensor_tensor(out=Ab[:], in0=Ab[:], in1=LF[:], op=ALU.add), reads=['Ab', 'LF'], writes=['Ab'])
                S.op('dve', lambda e: e.tensor_scalar_mul(out=Aa[:], in0=Af[:], scalar1=dirm[:, 0:1]), reads=['Af', 'dirm'], writes=['Aa'])
                S.op('dve', lambda e: e.scalar_tensor_tensor(out=Aa[:], in0=Ab[:], scalar=dirm[:, 1:2], in1=Aa[:], op0=ALU.mult, op1=ALU.add), reads=['Ab', 'dirm', 'Aa'], writes=['Aa'])
                S.op('act', lambda e: e.activation(out=R[0][:], in_=Aa[:], func=AF.Exp), reads=['Aa'], writes=['Rr0'])
                S.op('dve', lambda e: e.tensor_tensor(out=Ab[:], in0=LI[:], in1=Aa[:], op=ALU.subtract), reads=['LI', 'Aa', 'Ab'], writes=['Ab'])
                S.op('act', lambda e: e.activation(out=R[1][:], in_=Ab[:], func=AF.Exp), reads=['Ab'], writes=['Rr1'])
                S.op('dve', lambda e: e.tensor_tensor(out=Aa[:].rearrange("p (n c) -> p n c", c=64), in0=Ab[:].rearrange("p (n c) -> p n c", c=64), in1=tot.to_broadcast([8, NCH, 64]), op=ALU.add),
                     reads=['Ab', 'Af', 'Aa', 'Rr0'], writes=['Aa'])
                S.op('act', lambda e: e.activation(out=R[2][:], in_=Aa[:], func=AF.Exp), reads=['Aa'], writes=['Rr2'])
                for half in range(2):
                    for j in range(18):
                        n = half * 18 + j
                        for q in range(3):
                            S.op('pe', lambda e, n=n, j=j, q=q: e.transpose(PB[2][0:64, j * 24 + q * 8:j * 24 + q * 8 + 8], R[q][:, n * 64:(n + 1) * 64], ident[0:8, 0:8]),
                                 reads=['Rr%d' % q, 'ident'], writes=['pb2'], skip_self=True)
                    S.op('act', lambda e, half=half: e.copy(out=RUU[:, half * 18:(half + 1) * 18, :], in_=PB[2][0:64, 0:432].rearrange("p (j c) -> p j c", c=24)),
                         reads=['pb2'], writes=['RUU'])
                S.op('dve', lambda e: e.tensor_tensor(out=bd[:], in0=tot.rearrange("p n c -> p c n").to_broadcast([8, 8, NCH]), in1=sel8[:].unsqueeze(2).to_broadcast([8, 8, NCH]), op=ALU.mult),
                     reads=['Af', 'sel8'], writes=['bd'])
                S.op('pe', lambda e: e.matmul(PB[3][:, 0:288], lhsT=ones[0:8, :], rhs=bd[:].rearrange("p a n -> p (a n)"), start=True, stop=True), reads=['ones', 'bd'], writes=['pb3'], skip_self=True)
                S.op('act', lambda e: e.activation(out=dchunk[:].rearrange("p a n -> p (a n)"), in_=PB[3][:, 0:288], func=AF.Exp), reads=['pb3'], writes=['dchunk'])
                S.barrier()
            qc = sb(m0, "mqc", [128, NTOK], BF16); kc_ = sb(m0, "mkc", [128, NTOK], BF16); kTM = sb(m0, "mkTM", [64, NCH, 128], BF16)
            raw = sb(m0, "mraw", [128, NTOK]); acc = sb(m0, "macc", [128, NTOK])
            vext = sb(m0, "mvext", [64, NCH, 132], BF16); vu = sb(m0, "mvu", [64, NCH, 132], BF16)
            og = sb(m0, "mog", [64, NLCH, 128], BF16); oext = sb(m0, "moext", [64, NLCH, 132]); obuf = sb(m0, "mobuf", [64, NLCH, 128])
            den = sb(m0, "mden", [64, NLCH]); mu = sb(m0, "mmu", [64, NLCH]); m2 = sb(m0, "mm2", [64, NLCH])
            S.op('dve', lambda e: e.memset(vext[:], 1.0), writes=['mvext'])
            for hd in range(4):
                with ExitStack() as hs:
                    wq_ = load_w(hs, "mwq", 2560 + hd * 128, 128); wk_ = load_w(hs, "mwk", 3072 + hd * 128, 128)
                    wv_ = load_w(hs, "mwv", 3584 + hd * 128, 128); wo_ = load_w(hs, "mwo", 4096 + hd * 128, 128)
                    prep_tables(64)
                    for g in range(9):
                        bk = 3 + g % 2
                        proj_tm_group(wv_, "mwv", 128, bk, g * 4, 4)
                        S.op('act', lambda e, g=g, bk=bk: e.copy(out=vext[:, g * 4:(g + 1) * 4, 0:128], in_=PB[bk][0:64, :].rearrange("p (j c) -> p j c", c=128)),
                             reads=['pb%d' % bk], writes=['mvext'])
                    for g in range(8):
                        bk = 3 + (g + 1) % 2
                        proj_tm_group(wo_, "mwo", 128, bk, 4 + g * 4, 4)
                        S.op('act', lambda e, g=g, bk=bk: e.activation(out=og[:, g * 4:(g + 1) * 4, :], in_=PB[bk][0:64, :].rearrange("p (j c) -> p j c", c=128), func=AF.Sigmoid),
                             reads=['pb%d' % bk], writes=['mog'])
                    for qi, (wb, wn, dstb) in enumerate(((wq_, "mwq", qc), (wk_, "mwk", kc_))):
                        chn = qi * 4 + hd
                        for bi, (t0, nt) in enumerate(BLKS):
                            proj_fm_block(wb, wn, 128, bi % 2, t0, nt)
                            S.op('act', lambda e, t0=t0, nt=nt, bi=bi: e.copy(out=raw[:, t0:t0 + nt], in_=PB[bi % 2][:, 0:nt]), reads=['pb%d' % (bi % 2)], writes=['mraw'])
                        S.op('dve', lambda e, chn=chn: e.tensor_scalar(out=acc[:, 0:256], in0=raw[:, 0:256], scalar1=convw[:, 4, chn:chn + 1], scalar2=convb[:, chn:chn + 1], op0=ALU.mult, op1=ALU.add),
                             reads=['mraw', 'convw', 'convb'], writes=['macc'])
                        S.op('dve', lambda e, chn=chn: e.scalar_tensor_tensor(out=acc[:, 1:256], in0=raw[:, 0:255], scalar=convw[:, 3, chn:chn + 1], in1=acc[:, 1:256], op0=ALU.mult, op1=ALU.add),
                             reads=['mraw', 'convw', 'macc'], writes=['macc'])
                        S.op('dve', lambda e, chn=chn: e.scalar_tensor_tensor(out=acc[:, 0:255], in0=raw[:, 1:256], scalar=convw[:, 5, chn:chn + 1], in1=acc[:, 0:255], op0=ALU.mult, op1=ALU.add),
                             reads=['mraw', 'convw', 'macc'], writes=['macc'])
                        X = raw[:, 256:NTOK].rearrange("p (r c) -> p r c", c=64); Y = acc[:, 256:NTOK].rearrange("p (r c) -> p r c", c=64)
                        S.op('dve', lambda e, chn=chn: e.tensor_scalar(out=acc[:, 256:NTOK], in0=raw[:, 256:NTOK], scalar1=convw[:, 4, chn:chn + 1], scalar2=convb[:, chn:chn + 1], op0=ALU.mult, op1=ALU.add),
                             reads=['mraw', 'convw', 'convb', 'macc'], writes=['macc'])
                        for ky in range(3):
                            for kx in range(3):
                                if ky == 1 and kx == 1:
                                    continue
                                dy = ky - 1; dx = kx - 1
                                r0 = max(0, -dy); r1 = 32 - max(0, dy); c0 = max(0, -dx); c1 = 64 - max(0, dx)
                                S.op('dve', lambda e, chn=chn, ky=ky, kx=kx, r0=r0, r1=r1, c0=c0, c1=c1, dy=dy, dx=dx: e.scalar_tensor_tensor(
                                    out=Y[:, r0:r1, c0:c1], in0=X[:, r0 + dy:r1 + dy, c0 + dx:c1 + dx], scalar=convw[:, ky * 3 + kx, chn:chn + 1], in1=Y[:, r0:r1, c0:c1],
                                    op0=ALU.mult, op1=ALU.add), reads=['mraw', 'convw', 'macc'], writes=['macc'])
                        S.op('act', lambda e: e.activation(out=acc[:], in_=acc[:], func=AF.Silu), reads=['macc'], writes=['macc'])
                        if qi == 0:
                            S.op('dve', lambda e: e.tensor_copy(out=qc[:], in_=acc[:]), reads=['macc'], writes=['mqc'])
                        else:
                            S.op('dve', lambda e: e.tensor_scalar_mul(out=kc_[:], in0=acc[:], scalar1=128.0 ** -0.5), reads=['macc'], writes=['mkc'])
                    pbb = PB[3][:].bitcast(BF16)
                    for g in range(5):
                        nck = 8 if g < 4 else 4
                        for j in range(nck):
                            n = g * 8 + j
                            S.op('pe', lambda e, j=j, n=n: e.transpose(pbb[0:64, j * 128:(j + 1) * 128], kc_[:, n * 64:(n + 1) * 64], identb[:]),
                                 reads=['mkc', 'identb'], writes=['pb3'], skip_self=True)
                        S.op('act', lambda e, g=g, nck=nck: e.copy(out=kTM[:, g * 8:g * 8 + nck, :], in_=pbb[0:64, 0:nck * 128].rearrange("p (j c) -> p j c", c=128)),
                             reads=['pb3'], writes=['mkTM'])
                    for d in range(2):
                        row = d * 4 + hd
                        S.op('dve', lambda e, row=row: e.tensor_tensor(out=vu[:], in0=vext[:], in1=RUU[:, :, 16 + row:17 + row].to_broadcast([64, NCH, 132]), op=ALU.mult),
                             reads=['mvext', 'RUU'], writes=['mvu'])

                        def evac(nl, n, po, pok, row=row):
                            S.op('act', lambda e: e.activation(out=oext[:, nl, 0:129], in_=po, func=AF.Identity, scale=RUU[:, n, row:row + 1]), reads=[pok, 'RUU'], writes=K('moext', nl))
                        chunk_loop(d, kc_, qc, vext, qc, kTM, vu, lambda n, row=row: dchunk[:, row, n:n + 1], 129, evac,
                                   ['mqc', 'mkc', 'mvext', 'mvu', 'mkTM'], 'dchunk',
                                   su_ap=lambda n, row=row: RUU[:, n, 8 + row:9 + row], suk=['RUU'])
                        allx = K('moext', 0, NLCH)
                        S.op('act', lambda e: e.activation(out=den[:], in_=oext[:, :, 128], func=AF.Abs), reads=allx, writes=['mden'])
                        S.op('dve', lambda e: e.tensor_scalar_max(out=den[:], in0=den[:], scalar1=1.0), reads=['mden'], writes=['mden'])
                        S.op('dve', lambda e: e.reciprocal(out=den[:], in_=den[:]), reads=['mden'], writes=['mden'])
                        if d == 0:
                            S.op('dve', lambda e: e.tensor_tensor(out=obuf[:], in0=oext[:, :, 0:128], in1=den[:].unsqueeze(2).to_broadcast([64, NLCH, 128]), op=ALU.mult),
                                 reads=allx + ['mden'], writes=['mobuf'])
                        else:
                            S.op('dve', lambda e: e.tensor_tensor(out=oext[:, :, 0:128], in0=oext[:, :, 0:128], in1=den[:].unsqueeze(2).to_broadcast([64, NLCH, 128]), op=ALU.mult),
                                 reads=allx + ['mden'], writes=allx)
                            S.op('dve', lambda e: e.tensor_tensor(out=obuf[:], in0=obuf[:], in1=oext[:, :, 0:128], op=ALU.add), reads=allx + ['mobuf'], writes=['mobuf'])
                    allx = K('moext', 0, NLCH)
                    S.op('dve', lambda e: e.tensor_reduce(out=mu[:], in_=obuf[:], axis=AX.X, op=ALU.add), reads=['mobuf'], writes=['mmu'])
                    S.op('dve', lambda e: e.tensor_scalar_mul(out=mu[:], in0=mu[:], scalar1=1.0 / 128.0), reads=['mmu'], writes=['mmu'])
                    S.op('dve', lambda e: e.tensor_tensor(out=obuf[:], in0=obuf[:], in1=mu[:].unsqueeze(2).to_broadcast([64, NLCH, 128]), op=ALU.subtract), reads=['mobuf', 'mmu'], writes=['mobuf'])
                    S.op('dve', lambda e: e.tensor_tensor(out=oext[:, :, 0:128], in0=obuf[:], in1=obuf[:], op=ALU.mult), reads=['mobuf'] + allx, writes=allx)
                    S.op('dve', lambda e: e.tensor_reduce(out=m2[:], in_=oext[:, :, 0:128], axis=AX.X, op=ALU.add), reads=allx, writes=['mm2'])
                    S.op('act', lambda e: e.activation(out=m2[:], in_=m2[:], func=AF.Sqrt, bias=epsc[0:64, 0:1], scale=1.0 / 128.0), reads=['mm2', 'epsc'], writes=['mm2'])
                    S.op('dve', lambda e: e.reciprocal(out=m2[:], in_=m2[:]), reads=['mm2'], writes=['mm2'])
                    S.op('dve', lambda e: e.tensor_tensor(out=obuf[:], in0=obuf[:], in1=m2[:].unsqueeze(2).to_broadcast([64, NLCH, 128]), op=ALU.mult), reads=['mobuf', 'mm2'], writes=['mobuf'])
                    S.op('dve', lambda e, hd=hd: e.tensor_tensor(out=obuf[:], in0=obuf[:], in1=mln[:, hd * 128:(hd + 1) * 128].unsqueeze(1).to_broadcast([64, NLCH, 128]), op=ALU.mult),
                         reads=['mobuf', 'mln'], writes=['mobuf'])
                    S.op('dve', lambda e: e.tensor_tensor(out=obuf[:], in0=obuf[:], in1=og[:], op=ALU.mult), reads=['mobuf', 'mog'], writes=['mobuf'])
                    transpose_chunks_to_mixT(obuf, ['mobuf'], 4 + hd)
            S.barrier()

        if debug == 'mix':
            with ExitStack() as dd:
                mf = sb(dd, "mf", [128, NLAT])
                for h in range(8):
                    S.op('dve', lambda e, h=h: e.tensor_copy(out=mf[:], in_=mixT[:, h, :]), reads=K('mixT', h) + ['mf'], writes=['mf'])
                    S.dma('sp', lambda e, h=h: e.dma_start(out=dbg_d[h * 128:(h + 1) * 128, :], in_=mf[:]), reads=['mf'], writes=['dbg'])
                S.wait_all('sp', ['dbg']); S.barrier()
            scB.close(); scA.close()
            return nc
        prep_tables(512)
        S.barrier()
        scB.close()
        x1s = nc.dram_tensor("x1s", [NLAT, 1024], F32, kind="Internal").ap()

        def bcast_tile(dst, dkey, c0, dg):
            for ch in range(8):
                S.op('dve', lambda e, ch=ch: e.tensor_scalar_mul(out=dg[:], in0=ident[:], scalar1=modv[:, c0 + ch, 0:1]), reads=['ident', 'modv', 'dg'], writes=['dg'])
                bank = ch // 4
                S.op('pe', lambda e, ch=ch, bank=bank: e.matmul(PB[bank][:, (ch % 4) * 128:(ch % 4 + 1) * 128], lhsT=ones[:], rhs=dg[:], start=True, stop=True),
                     reads=['ones', 'dg'], writes=['pb%d' % bank], skip_self=True)
            S.op('act', lambda e: e.copy(out=dst[:, 0:512], in_=PB[0][:, :]), reads=['pb0'], writes=[dkey])
            S.op('act', lambda e: e.copy(out=dst[:, 512:1024], in_=PB[1][:, :]), reads=['pb1'], writes=[dkey])

        with ExitStack() as p3:
            bct = {}
            for nm in ("g1b", "ln1g", "ln1b"):
                bct[nm] = sb(p3, nm, [128, 1024])
            for nm, dd in (("ln1g", ln1g_d), ("ln1b", ln1b_d)):
                S.dma('sp', lambda e, nm=nm, dd=dd: e.dma_start(out=bct[nm][:], in_=dd[:, :]), writes=[nm])
            dg = sb(p3, "dg", [128, 128])
            bcast_tile(bct["g1b"], "g1b", 16, dg)
            wo_b = sb(p3, "wout", [128, 8, 1024], BF16)
            for kc in range(8):
                S.dma('pool', lambda e, kc=kc: e.dma_start(out=wo_b[:, kc, :], in_=wout_d[:, kc, :]), writes=['wout%d' % kc])
            xt = [sb(p3, "x3t%d" % i, [128, 1024]) for i in range(2)]
            t1 = [sb(p3, "t1_%d" % i, [128, 1024]) for i in range(2)]
            st = [sb(p3, "st3%d" % i, [128, 2, 6]) for i in range(2)]
            mv = [sb(p3, "mv3%d" % i, [128, 2]) for i in range(2)]
            rs = [sb(p3, "rs3%d" % i, [128, 1]) for i in range(2)]
            for t in range(16):
                b = t % 2
                S.dma('sp' if b == 0 else 'pool', lambda e, t=t, b=b: e.dma_start(out=xt[b][:], in_=xs[256 + t * 128:256 + (t + 1) * 128, :]), writes=['x3t%d' % b])
                for half in range(2):
                    bank = 2 * b + half
                    for kc in range(8):
                        S.op('pe', lambda e, kc=kc, t=t, half=half, bank=bank: e.matmul(PB[bank][:, :], lhsT=mixT[:, kc, t * 128:(t + 1) * 128], rhs=wo_b[:, kc, half * 512:(half + 1) * 512],
                                                                                      start=(kc == 0), stop=(kc == 7)),
                             reads=K('mixT', kc) + ['wout%d' % kc], writes=['pb%d' % bank], skip_self=True)
                    S.op('dve', lambda e, b=b, half=half, bank=bank: e.tensor_tensor(out=t1[b][:, half * 512:(half + 1) * 512], in0=PB[bank][:, :], in1=bct["g1b"][:, half * 512:(half + 1) * 512], op=ALU.mult),
                         reads=['pb%d' % bank, 'g1b'], writes=['t1_%d' % b])
                S.op('dve', lambda e, b=b: e.scalar_tensor_tensor(out=t1[b][:], in0=xt[b][:], scalar=ALPHA, in1=t1[b][:], op0=ALU.mult, op1=ALU.add),
                     reads=['x3t%d' % b, 't1_%d' % b], writes=['t1_%d' % b])
                layer_norm_rows(st[b], mv[b][:], rs[b][:], t1[b][:], t1[b][:], 't1_%d' % b, 't1_%d' % b, 'p3%d' % b)
                S.op('dve', lambda e, b=b: e.tensor_tensor(out=t1[b][:], in0=t1[b][:], in1=bct["ln1g"][:], op=ALU.mult), reads=['t1_%d' % b, 'ln1g'], writes=['t1_%d' % b])
                S.op('dve', lambda e, b=b, t=t: e.tensor_tensor(out=t1[b][:], in0=t1[b][:], in1=bct["ln1b"][:], op=ALU.add), reads=['t1_%d' % b, 'ln1b'], writes=['t1_%d' % b])
                S.dma('sp', lambda e, t=t, b=b: e.dma_start(out=x1s[t * 128:(t + 1) * 128, :], in_=t1[b][:]), reads=['t1_%d' % b], writes=K('x1_', t))
                if debug == 'x1':
                    S.dma('sp', lambda e, t=t, b=b: e.dma_start(out=dbg_d[t * 128:(t + 1) * 128, :], in_=t1[b][:]), reads=['t1_%d' % b], writes=['dbg'])
            S.barrier()
        scA.close()

        with ExitStack() as p4:
            bct = {}
            for nm in ("g2b", "sc2b", "sh2b", "ln2g", "ln2b"):
                bct[nm] = sb(p4, nm, [128, 1024])
            for nm, dd in (("ln2g", ln2g_d), ("ln2b", ln2b_d)):
                S.dma('sp', lambda e, nm=nm, dd=dd: e.dma_start(out=bct[nm][:], in_=dd[:, :]), writes=[nm])
            dg = sb(p4, "dg4", [128, 128])
            bcast_tile(bct["g2b"], "g2b", 40, dg); bcast_tile(bct["sc2b"], "sc2b", 32, dg); bcast_tile(bct["sh2b"], "sh2b", 24, dg)
            x1t = [sb(p4, "x1t%d" % i, [128, 1024]) for i in range(2)]
            wqb = sb(p4, "wqb", [128, 8, 2048], BF16)
            keysT = sb(p4, "keysT", [128, 16, 128], BF16)
            for kc in range(8):
                for hh in range(2):
                    S.dma('pool', lambda e, kc=kc, hh=hh: e.dma_start(out=wqb[:, kc, hh * 1024:(hh + 1) * 1024], in_=wq_d[:, kc, hh * 1024:(hh + 1) * 1024]), writes=['wqb%d_%d' % (kc, hh)])
            for hh in range(2):
                S.dma('pool', lambda e, hh=hh: e.dma_start(out=keysT[:, hh * 8:(hh + 1) * 8, :], in_=keysT_d[:, hh * 8:(hh + 1) * 8, :]), writes=['keysT%d' % hh])
            S.barrier()
            NG = int(os.environ.get('KNG', '16'))
            uvb = [sb(p4, "uvb%d" % i, [128, 2048], BF16) for i in range(NG)]
            gl = sb(p4, "gl", [128, 128])
            pr = [sb(p4, "pr%d" % i, [128, 1024], BF16) for i in range(3)]
            dgk = [sb(p4, "dgk%d" % i, [128, 128], BF16) for i in range(4)]
            h2 = sb(p4, "h2", [128, 1024]); h2b = [sb(p4, "h2b%d" % i, [128, 1024], BF16) for i in range(2)]
            h2T = sb(p4, "h2T", [128, 8, 128], BF16); qT = sb(p4, "qT", [128, 16, 128], BF16)
            sc = sb(p4, "sc", [128, 16, 128]); scw = sb(p4, "scw", [128, 16, 128])
            top = sb(p4, "top", [128, 16, 16]); topi = sb(p4, "topi", [128, 16, 16], U32); topf = sb(p4, "topf", [128, 16, 16])
            candw = sb(p4, "candw", [128, 8, 256])
            cand = View(scw[:].rearrange("p (h two) k -> p h (two k)", two=2)); eq = View(candw[:].rearrange("p h (a b) -> p (h a) b", b=16))
            best = sb(p4, "best", [128, 8, 16]); idxf = sb(p4, "idxf", [128, 128])
            posi = sb(p4, "posi", [128, 8, 16], U32); pai = sb(p4, "pai", [128, 128], U32); pbi = sb(p4, "pbi", [128, 128], U32)
            paf = sb(p4, "paf", [128, 128]); pbf = sb(p4, "pbf", [128, 128]); iaf = sb(p4, "iaf", [128, 128]); ibf = sb(p4, "ibf", [128, 128])
            iota16 = sb(p4, "iota16", [128, 16])
            S.dma('sp', lambda e: e.dma_start(out=iota16[:], in_=iota_d[:, :]), writes=['iota16'])
            gate = [sb(p4, "gate%d" % i, [128, 128]) for i in range(2)]
            idxi = [sb(p4, "idxi%d" % i, [128, 128], I32) for i in range(2)]
            dots = sb(p4, "dots", [128, 128]); junk = sb(p4, "junk", [128, 1024], BF16)
            zs = sb(p4, "zs", [128, 8]); nmx = sb(p4, "nmx", [128, 8])
            st = sb(p4, "st4", [128, 2, 6]); mv = sb(p4, "mv4", [128, 2]); rs = sb(p4, "rs4", [128, 1])
            fin = View(sc[:].rearrange("p c k -> p (c k)")[:, 0:1024]); yb = h2
            NT4 = 16 if debug != 'x1' else 0

            def prologue(t):
                p = t % 2
                xb_ = x1t[p]; x1k = 'x1t%d' % p
                S.dma('sp', lambda e: e.dma_start(out=xb_[:], in_=x1s[t * 128:(t + 1) * 128, :]), reads=K('x1_', t), writes=[x1k])
                S.op('dve', lambda e: e.memset(idxf[:], 0.0), writes=['idxf'])
                S.op('dve', lambda e: e.memset(zs[:], 0.0), writes=['zs'])
                layer_norm_rows(st, mv[:], rs[:], xb_[:], h2[:], x1k, 'h2', 'p4')
                S.op('dve', lambda e: e.tensor_tensor(out=h2[:], in0=h2[:], in1=bct["sc2b"][:], op=ALU.mult), reads=['h2', 'sc2b'], writes=['h2'])
                S.op('dve', lambda e: e.tensor_tensor(out=h2[:], in0=h2[:], in1=bct["sh2b"][:], op=ALU.add), reads=['h2', 'sh2b'], writes=['h2'])
                S.op('act', lambda e: e.copy(out=h2b[p][:], in_=h2[:]), reads=['h2'], writes=['h2b%d' % p])
                yield
                for half in range(2):
                    for c4 in range(4):
                        ch = half * 4 + c4
                        S.op('pe', lambda e, ch=ch, c4=c4, half=half: e.transpose(PB[half][:, c4 * 128:(c4 + 1) * 128], h2[:, ch * 128:(ch + 1) * 128], ident[:]),
                             reads=['h2', 'ident'], writes=['pb%d' % half], skip_self=True)
                    yield
                    S.op('act', lambda e, half=half: e.copy(out=h2T[:, half * 4:(half + 1) * 4, :], in_=PB[half][:, :].rearrange("p (j c) -> p j c", c=128)), reads=['pb%d' % half], writes=['h2T'])
                for g in range(4):
                    for c4 in range(4):
                        c = g * 4 + c4
                        for kc in range(8):
                            S.op('pe', lambda e, c=c, c4=c4, kc=kc, g=g: e.matmul(PB[2 + g][:, c4 * 128:(c4 + 1) * 128], lhsT=wqb[:, kc, c * 128:(c + 1) * 128], rhs=h2T[:, kc, :],
                                                                                start=(kc == 0), stop=(kc == 7)), reads=['wqb%d_%d' % (kc, c // 8), 'h2T'], writes=['pb%d' % (2 + g)], skip_self=True)
                    yield
                    S.op('act' if g % 2 == 0 else 'dve', lambda e, g=g: (e.copy if g % 2 == 0 else e.tensor_copy)(out=qT[:, g * 4:(g + 1) * 4, :], in_=PB[2 + g][:, :].rearrange("p (j c) -> p j c", c=128)),
                         reads=['pb%d' % (2 + g)], writes=['qT'])
                for g in range(4):
                    for c4 in range(4):
                        c = g * 4 + c4
                        S.op('pe', lambda e, c=c, c4=c4, g=g: e.matmul(PB[2 + g][:, c4 * 128:(c4 + 1) * 128], lhsT=qT[:, c, :], rhs=keysT[:, c, :], start=True, stop=True),
                             reads=['qT', 'keysT%d' % (c // 8)], writes=['pb%d' % (2 + g)], skip_self=True)
                    yield
                    S.op('act' if g % 2 == 0 else 'dve', lambda e, g=g: (e.copy if g % 2 == 0 else e.tensor_copy)(out=sc[:, g * 4:(g + 1) * 4, :], in_=PB[2 + g][:, :].rearrange("p (j c) -> p j c", c=128)),
                         reads=['pb%d' % (2 + g)], writes=['sc'])
                for c in range(16):
                    S.op('dve', lambda e, c=c: e.max(out=top[:, c, 0:8], in_=sc[:, c, :]), reads=['sc'], writes=['top'])
                    S.op('dve', lambda e, c=c: e.max_index(out=topi[:, c, 0:8], in_max=top[:, c, 0:8], in_values=sc[:, c, :]), reads=['sc', 'top'], writes=['topi'])
                    S.op('dve', lambda e, c=c: e.match_replace(out=scw[:, c, :], in_to_replace=top[:, c, 0:8], in_values=sc[:, c, :], imm_value=-1e30), reads=['sc', 'top'], writes=['scw'])
                    S.op('dve', lambda e, c=c: e.max(out=top[:, c, 8:16], in_=scw[:, c, :]), reads=['scw'], writes=['top'])
                    S.op('dve', lambda e, c=c: e.max_index(out=topi[:, c, 8:16], in_max=top[:, c, 8:16], in_values=scw[:, c, :]), reads=['scw', 'top'], writes=['topi'])
                    yield
                S.op('dve', lambda e: e.tensor_copy(out=topf[:], in_=topi[:]), reads=['topi'], writes=['topf'])
                t4 = top[:].rearrange("p (h two) k -> p h two k", two=2); f4 = topf[:].rearrange("p (h two) k -> p h two k", two=2)
                c4v = cand[:].rearrange("p h (a b) -> p h a b", b=16)
                for h in range(8):
                    S.op('dve', lambda e, h=h: e.tensor_tensor(out=c4v[:, h, :, :], in0=t4[:, h, 0, :].unsqueeze(2).to_broadcast([128, 16, 16]), in1=t4[:, h, 1, :].unsqueeze(1).to_broadcast([128, 16, 16]), op=ALU.add),
                         reads=['top'], writes=['scw'])
                    yield
                for h in range(8):
                    S.op('dve', lambda e, h=h: e.max(out=best[:, h, 0:8], in_=cand[:, h, :]), reads=['scw'], writes=['best'])
                    S.op('dve', lambda e, h=h: e.match_replace(out=candw[:, h, :], in_to_replace=best[:, h, 0:8], in_values=cand[:, h, :], imm_value=-1e30), reads=['scw', 'best'], writes=['candw'])
                    S.op('dve', lambda e, h=h: e.max(out=best[:, h, 8:16], in_=candw[:, h, :]), reads=['candw'], writes=['best'])
                    S.op('dve', lambda e, h=h: e.max_index(out=posi[:, h, 0:8], in_max=best[:, h, 0:8], in_values=cand[:, h, :]), reads=['scw', 'best'], writes=['posi'])
                    S.op('dve', lambda e, h=h: e.max_index(out=posi[:, h, 8:16], in_max=best[:, h, 8:16], in_values=candw[:, h, :]), reads=['candw', 'best'], writes=['posi'])
                    yield
                pflat = posi[:].rearrange("p h k -> p (h k)")
                S.op('dve', lambda e: e.tensor_single_scalar(out=pai[:], in_=pflat, scalar=4, op=ALU.logical_shift_right), reads=['posi'], writes=['pai'])
                S.op('dve', lambda e: e.tensor_single_scalar(out=pbi[:], in_=pflat, scalar=15, op=ALU.bitwise_and), reads=['posi'], writes=['pbi'])
                S.op('dve', lambda e: e.tensor_copy(out=paf[:], in_=pai[:]), reads=['pai'], writes=['paf'])
                S.op('dve', lambda e: e.tensor_copy(out=pbf[:], in_=pbi[:]), reads=['pbi'], writes=['pbf'])
                yield
                eq4 = eq[:].rearrange("p (h k) a -> p h k a", k=16)
                for (pp, pk_, half_, dst, dn) in ((paf, 'paf', 0, iaf, 'iaf'), (pbf, 'pbf', 1, ibf, 'ibf')):
                    S.op('dve', lambda e, pp=pp: e.tensor_tensor(out=eq[:], in0=iota16[:].unsqueeze(1).to_broadcast([128, 128, 16]), in1=pp[:].unsqueeze(2).to_broadcast([128, 128, 16]), op=ALU.is_equal),
                         reads=['iota16', pk_, 'candw'], writes=['candw'])
                    S.op('dve', lambda e, half_=half_: e.tensor_tensor(out=eq4, in0=eq4, in1=f4[:, :, half_, :].unsqueeze(2).to_broadcast([128, 8, 16, 16]), op=ALU.mult),
                         reads=['candw', 'topf'], writes=['candw'])
                    S.op('dve', lambda e, dst=dst: e.tensor_reduce(out=dst[:], in_=eq[:], axis=AX.X, op=ALU.add), reads=['candw'], writes=[dn])
                    yield
                S.op('dve', lambda e: e.scalar_tensor_tensor(out=idxf[:], in0=iaf[:], scalar=128.0, in1=ibf[:], op0=ALU.mult, op1=ALU.add), reads=['iaf', 'ibf'], writes=['idxf'])
                S.op('dve', lambda e: e.tensor_scalar_min(out=idxf[:], in0=idxf[:], scalar1=16383.0), reads=['idxf'], writes=['idxf'])
                S.op('dve', lambda e: e.tensor_copy(out=idxi[p][:], in_=idxf[:]), reads=['idxf'], writes=['idxi%d' % p])
                S.op('dve', lambda e: e.tensor_scalar_mul(out=nmx[:], in0=best[:, :, 0], scalar1=-1.0), reads=['best'], writes=['nmx'])
                g3 = gate[p][:].rearrange("p (h k) -> p h k", k=16)
                for h in range(8):
                    S.op('act', lambda e, h=h: e.activation(out=g3[:, h, :], in_=best[:, h, :], func=AF.Exp, bias=nmx[:, h:h + 1], scale=1.0, accum_out=zs[:, h:h + 1]),
                         reads=['best', 'nmx'], writes=['gate%d' % p, 'zs'])
                S.op('dve', lambda e: e.reciprocal(out=zs[:], in_=zs[:]), reads=['zs'], writes=['zs'])
                S.op('dve', lambda e: e.tensor_tensor(out=g3, in0=g3, in1=zs[:].unsqueeze(2).to_broadcast([128, 8, 16]), op=ALU.mult), reads=['gate%d' % p, 'zs'], writes=['gate%d' % p])

            def fused(t, gen=None):
                p = t % 2
                LAG = 2

                def tail(kk):
                    s_ = (t * 128 + kk) % NG; d4 = kk % 4
                    S.op('dve', lambda e: e.tensor_scalar(out=dgk[d4][:], in0=identb[:], scalar1=gl[:, kk:kk + 1], scalar2=gate[p][:, kk:kk + 1], op0=ALU.mult, op1=ALU.mult),
                         reads=['identb', 'gl_%d' % kk, 'gate%d' % p], writes=['dgk%d' % d4])
                    for half in range(2):
                        S.op('pe', lambda e, half=half: e.matmul(PB[6 + half][:, :], lhsT=dgk[d4][:], rhs=uvb[s_][:, 1024 + half * 512:1024 + (half + 1) * 512], start=(kk == 0), stop=(kk == 127)),
                             reads=['dgk%d' % d4, 'uvb%d' % s_], writes=['pb%d' % (6 + half)], skip_self=True)

                for k in range(128):
                    s_ = (t * 128 + k) % NG; j4 = k % 3
                    dk = 'dots_%d' % k
                    S.dma('pool', lambda e, k=k, s_=s_: e.indirect_dma_start(out=uvb[s_][:], out_offset=None, in_=uv_d[:, :], in_offset=bass.IndirectOffsetOnAxis(ap=idxi[p][:, k:k + 1], axis=0)),
                          reads=['idxi%d' % p], writes=['uvb%d' % s_])
                    S.op('dve', lambda e, k=k, s_=s_, j4=j4: e.tensor_tensor(out=pr[j4][:], in0=uvb[s_][:, 0:1024], in1=h2b[p][:], op=ALU.mult),
                         reads=['uvb%d' % s_, 'h2b%d' % p], writes=['pr%d' % j4])
                    S.op('act', lambda e, k=k, j4=j4: e.activation(out=junk[:], in_=pr[j4][:], func=AF.Identity, accum_out=dots[:, k:k + 1]),
                         reads=['pr%d' % j4, 'dots0'], writes=[dk])
                    S.op('act', lambda e, k=k: e.activation(out=gl[:, k:k + 1], in_=dots[:, k:k + 1], func=AF.Gelu), reads=[dk], writes=['gl_%d' % k])
                    if k >= LAG:
                        tail(k - LAG)
                    if gen is not None and k % 2 == 1:
                        next(gen, None)
                for kk in range(128 - LAG, 128):
                    tail(kk)
                if gen is not None:
                    for _ in gen:
                        pass
                xb_ = x1t[p]; x1k = 'x1t%d' % p
                for half in range(2):
                    S.op('dve', lambda e, half=half: e.tensor_tensor(out=yb[:, half * 512:(half + 1) * 512], in0=PB[6 + half][:, :], in1=bct["g2b"][:, half * 512:(half + 1) * 512], op=ALU.mult),
                         reads=['pb%d' % (6 + half), 'g2b', 'h2'], writes=['h2'])
                S.op('dve', lambda e: e.scalar_tensor_tensor(out=fin[:], in0=xb_[:], scalar=ALPHA, in1=yb[:], op0=ALU.mult, op1=ALU.add), reads=[x1k, 'h2', 'sc'], writes=['sc'])
                layer_norm_rows(stf, mvf[:], rsf[:], fin[:], fin[:], 'sc', 'sc', 'p4f')
                S.op('dve', lambda e: e.tensor_tensor(out=fin[:], in0=fin[:], in1=bct["ln2g"][:], op=ALU.mult), reads=['sc', 'ln2g'], writes=['sc'])
                S.op('dve', lambda e: e.tensor_tensor(out=fin[:], in0=fin[:], in1=bct["ln2b"][:], op=ALU.add), reads=['sc', 'ln2b'], writes=['sc'])
                S.dma('sp', lambda e: e.dma_start(out=out_d[t * 128:(t + 1) * 128, :], in_=fin[:]), reads=['sc'], writes=['out'])

            stf = sb(p4, "st4f", [128, 2, 6]); mvf = sb(p4, "mv4f", [128, 2]); rsf = sb(p4, "rs4f", [128, 1])
            S.op('dve', lambda e: e.memset(dots[:], 0.0), writes=['dots0'])
            S.wait_all('pool', ['tabs_%d' % i for i in range(512)])
            if NT4:
                for _ in prologue(0):
                    pass
            for t in range(NT4):
                fused(t, prologue(t + 1) if t + 1 < NT4 else None)
            S.wait_all('sp', ['out', 'dbg'])
            S.barrier()
    return nc


def _prep_shared(inp):
    f = np.float32
    sh = {}
    sh["w_mod"] = np.ascontiguousarray(inp["w_mod"][0].reshape(8, 128, 6144).transpose(1, 0, 2))
    sh["b_modT"] = np.ascontiguousarray(inp["b_mod"][0].reshape(48, 128).T)
    sh["w_inb"] = np.ascontiguousarray(inp["w_in"][0][:, :4608].reshape(8, 128, 36, 128).transpose(1, 2, 0, 3))
    sh["w_ing"] = np.ascontiguousarray(inp["w_in"][0][:, 4608:].reshape(8, 128, 16).transpose(1, 0, 2))
    sh["lgT"] = np.ascontiguousarray(inp["hg_lb_logits"].reshape(2, 2, 4, 128).transpose(3, 0, 1, 2))
    sh["hgn"] = np.ascontiguousarray(np.broadcast_to(inp["hg_norm_g"][0][None, :], (64, 512)))
    sh["mln"] = np.ascontiguousarray(np.broadcast_to(inp["ml_norm_g"][0][None, :], (64, 512)))
    sh["convw"] = np.ascontiguousarray(inp["ml_conv_w"][0].reshape(9, 8, 128).transpose(2, 0, 1))
    sh["convb"] = np.ascontiguousarray(inp["ml_conv_b"][0].reshape(8, 128).T)
    sh["gateb"] = np.ascontiguousarray(inp["ml_gate_b"][0].reshape(2, 8).T)
    sh["w_out"] = np.ascontiguousarray(inp["w_out"][0].reshape(8, 128, 1024).transpose(1, 0, 2))
    for nm, key in (("ln1g", "ln1_g"), ("ln1b", "ln1_b"), ("ln2g", "ln2_g"), ("ln2b", "ln2_b")):
        sh[nm] = np.ascontiguousarray(np.broadcast_to(inp[key][0][None, :], (128, 1024)))
    sh["wq"] = np.ascontiguousarray(inp["peer_wq"][0].reshape(8, 128, 2048).transpose(1, 0, 2))
    sh["keysT"] = np.ascontiguousarray(inp["peer_keys"][0].reshape(16, 128, 128).transpose(2, 0, 1))
    nexp = 128 if os.environ.get("KDEBUG") else 16384
    sh["pu"] = np.ascontiguousarray(inp["peer_u"][0][:nexp])
    sh["pv"] = np.ascontiguousarray(inp["peer_v"][0][:nexp])
    sh["ident"] = np.eye(128, dtype=f)
    m = np.zeros((64, 2, 64), f)
    s = np.arange(64)[:, None]; c = np.arange(64)[None, :]
    m[:, 0, :] = (s <= c); m[:, 1, :] = (s >= c)
    sh["masks"] = m
    rm = np.ones((128, 512), f); rm[:, ::64] = 0.0
    sh["rmask"] = rm
    sh["sel8"] = np.eye(8, dtype=f)
    sh["iota16"] = np.ascontiguousarray(np.broadcast_to(np.arange(16, dtype=f)[None, :], (128, 16)))
    dm = np.zeros((8, 2), f); dm[0:4, 0] = 1.0; dm[4:8, 1] = 1.0
    sh["dirm"] = dm
    return {k: np.asarray(v, dtype=f) for k, v in sh.items()}


def kernel(**inputs):
    inp = {k: np.asarray(v) for k, v in inputs.items()}
    debug = os.environ.get("KDEBUG") or None
    nc = build(debug)
    sh = _prep_shared(inp)
    in_maps = []
    for b in range(8):
        m = dict(sh)
        m["xs"] = np.ascontiguousarray(np.concatenate([inp["ctx"][b], inp["x"][b]], axis=0).astype(np.float32))
        m["cT"] = np.ascontiguousarray(np.stack([inp["c"][b], inp["c_ctx"]], axis=-1).reshape(8, 128, 2).transpose(1, 0, 2).astype(np.float32))
        in_maps.append(m)
    res = run_bass_kernel_spmd(nc, in_maps, core_ids=list(range(8)))
    key = "dbg" if debug else "out"
    return np.stack([np.asarray(r[key]) for r in res.results], axis=0).astype(np.float32)
```

```python
import os
import numpy as np
from contextlib import ExitStack
import concourse.bass as bass
import concourse.mybir as mybir
from concourse.bass_utils import run_bass_kernel_spmd

F32 = mybir.dt.float32; BF16 = mybir.dt.bfloat16; I32 = mybir.dt.int32; U32 = mybir.dt.uint32
AF = mybir.ActivationFunctionType; ALU = mybir.AluOpType; AX = mybir.AxisListType

NTOK = 2304; NLAT = 2048; NCH = 36; NLCH = 32
ALPHA = 2.0 ** 0.25
EPS = 1e-6


class Sched:
    NDMA = 32

    def __init__(self, nc, es):
        self.nc = nc
        self.engs = {'pe': nc.tensor, 'act': nc.scalar, 'dve': nc.vector, 'pool': nc.gpsimd, 'sp': nc.sync}
        self.sem = {k: es.enter_context(nc.semaphore("sem_" + k)) for k in self.engs}
        self.cnt = {k: 0 for k in self.engs}
        self.dsem = [es.enter_context(nc.semaphore("dsem%d" % i)) for i in range(self.NDMA)]
        self.dcnt = [0] * self.NDMA
        self.dnext = 0
        self.dlean = {}
        self.seen = {k: {} for k in self.engs}
        self.bufs = {}

    def _deps(self, reads, writes):
        deps = []
        for r in reads:
            b = self.bufs.get(r)
            if b and b['w'] is not None:
                deps.append(b['w'])
        for w in writes:
            b = self.bufs.get(w)
            if b:
                if b['w'] is not None:
                    deps.append(b['w'])
                deps.extend(b['r'])
        return deps

    def _wait(self, eng, deps, skip_self=False):
        best = {}
        for (sid, sem, val, owner) in deps:
            if skip_self and owner == eng:
                continue
            if best.get(sid, (None, 0))[1] < val:
                best[sid] = (sem, val)
        for sid, (sem, val) in best.items():
            if self.seen[eng].get(sid, 0) >= val:
                continue
            self.engs[eng].wait_ge(sem, val)
            self.seen[eng][sid] = val

    def _record(self, dep, reads, writes):
        for r in reads:
            b = self.bufs.setdefault(r, {'w': None, 'r': []})
            b['r'] = [d for d in b['r'] if d[0] != dep[0]] + [dep]
        for w in writes:
            self.bufs[w] = {'w': dep, 'r': []}

    @staticmethod
    def _split(keys):
        norm, ps = [], []
        for k in keys:
            if k.startswith('pb') and len(k) > 2 and k[2].isdigit():
                ps.append(k[:3])
            else:
                norm.append(k)
        return norm, ps

    def op(self, eng, fn, reads=(), writes=(), skip_self=False):
        reads, pr = self._split(reads)
        writes, pw = self._split(writes)
        banks = sorted(set(pr + pw))
        deps = self._deps(reads, writes)
        deps += [d for d in self._deps((), banks) if d[3] != eng]
        self._wait(eng, deps, skip_self)
        ins = fn(self.engs[eng])
        self.cnt[eng] += 1
        ins.then_inc(self.sem[eng], 1)
        self._record(('e_' + eng, self.sem[eng], self.cnt[eng], eng), reads, list(writes) + banks)
        return ins

    def dma(self, q, fn, reads=(), writes=(), lean=False):
        if lean:
            deps = self._deps(reads, ())
            slot = self._deps((), writes)
            pe = [d for d in slot if d[3] == 'pe']
            deps = deps + (pe if pe else slot)
        else:
            deps = self._deps(reads, writes)
        j = self.dnext
        self.dnext = (self.dnext + 1) % self.NDMA
        if self.dcnt[j] > 0 and not (lean and self.dlean.get(j)):
            deps = deps + [('d%d' % j, self.dsem[j], self.dcnt[j], 'dma')]
        self.dlean[j] = lean
        self._wait(q, deps)
        ins = fn(self.engs[q])
        self.dcnt[j] += 16
        ins.then_inc(self.dsem[j], 16)
        self._record(('d%d' % j, self.dsem[j], self.dcnt[j], 'dma'), reads, writes)

    def wait_all(self, eng, keys):
        deps = []
        for k in keys:
            b = self.bufs.get(k)
            if b:
                if b['w'] is not None:
                    deps.append(b['w'])
                deps.extend(b['r'])
        self._wait(eng, deps)

    def barrier(self):
        for e in self.engs:
            deps = [('e_' + f, self.sem[f], self.cnt[f], f) for f in self.engs if f != e and self.cnt[f] > 0]
            deps += [('d%d' % j, self.dsem[j], self.dcnt[j], 'dma') for j in range(self.NDMA) if self.dcnt[j] > 0]
            self._wait(e, deps)


class View:
    def __init__(self, ap):
        self.ap = ap

    def __getitem__(self, idx):
        return self.ap[idx]


def K(name, a, b=None):
    if b is None:
        return ["%s%d" % (name, a)]
    return ["%s%d" % (name, i) for i in range(a, b)]


def build(debug=None):
    nc = bass.Bass("TRN2", target_bir_lowering=False)
    D = {}

    def din(name, shape, dt=F32):
        D[name] = nc.dram_tensor(name, shape, dt, kind="ExternalInput").ap()
        return D[name]

    xs = din("xs", [NTOK, 1024]); cT_d = din("cT", [128, 8, 2]); wmod_d = din("w_mod", [128, 8, 6144])
    bmod_d = din("b_modT", [128, 48]); winb_d = din("w_inb", [128, 36, 8, 128]); wing_d = din("w_ing", [128, 8, 16]); lg_d = din("lgT", [128, 2, 2, 4])
    hgn_d = din("hgn", [64, 512]); mln_d = din("mln", [64, 512]); convw_d = din("convw", [128, 9, 8])
    convb_d = din("convb", [128, 8]); gateb_d = din("gateb", [8, 2]); wout_d = din("w_out", [128, 8, 1024])
    ln1g_d = din("ln1g", [128, 1024]); ln1b_d = din("ln1b", [128, 1024]); ln2g_d = din("ln2g", [128, 1024])
    ln2b_d = din("ln2b", [128, 1024]); wq_d = din("wq", [128, 8, 2048]); keysT_d = din("keysT", [128, 16, 128])
    NEXP = 128 if debug else 16384
    pu_d = din("pu", [NEXP, 1024]); pv_d = din("pv", [NEXP, 1024])
    ident_d = din("ident", [128, 128]); masks_d = din("masks", [64, 2, 64]); rmask_d = din("rmask", [128, 512])
    sel8_d = din("sel8", [8, 8]); dirm_d = din("dirm", [8, 2]); iota_d = din("iota16", [128, 16])
    out_d = nc.dram_tensor("out", [NLAT, 1024], F32, kind="ExternalOutput").ap()
    dbg_d = None
    if debug:
        dbg_d = nc.dram_tensor("dbg", [NLAT, 1024] if debug != 'mix' else [1024, NLAT], F32, kind="ExternalOutput").ap()

    with ExitStack() as es:
        S = Sched(nc, es)

        uid = [0]

        def sb(st, name, shape, dt=F32):
            uid[0] += 1
            return st.enter_context(nc.sbuf_tensor("s%d_%s" % (uid[0], name), shape, dt))

        PB = [es.enter_context(nc.psum_tensor("pb%d" % i, [128, 512], F32)) for i in range(8)]

        ident = sb(es, "ident", [128, 128]); identb = sb(es, "identb", [128, 128], BF16)
        masks = sb(es, "masks", [64, 2, 64]); rmask = sb(es, "rmask", [128, 512])
        ones = sb(es, "ones", [128, 128]); epsc = sb(es, "epsc", [128, 1])
        modv = sb(es, "modv", [128, 48, 2])
        S.dma('sp', lambda e: e.dma_start(out=ident[:], in_=ident_d[:, :]), writes=['ident'])
        S.dma('sp', lambda e: e.dma_start(out=masks[:], in_=masks_d[:, :, :]), writes=['masks'])
        S.dma('sp', lambda e: e.dma_start(out=rmask[:], in_=rmask_d[:, :]), writes=['rmask'])
        S.op('dve', lambda e: e.tensor_copy(out=identb[:], in_=ident[:]), reads=['ident'], writes=['identb'])
        S.op('dve', lambda e: e.memset(ones[:], 1.0), writes=['ones'])
        S.op('dve', lambda e: e.memset(epsc[:], EPS), writes=['epsc'])

        with ExitStack() as p0:
            cT = sb(p0, "cT", [128, 8, 2]); scT = sb(p0, "scT", [128, 8, 2]); bmodT = sb(p0, "bmodT", [128, 48])
            wm = [sb(p0, "wm%d" % i, [128, 6144]) for i in range(2)]
            S.dma('sp', lambda e: e.dma_start(out=cT[:], in_=cT_d[:, :, :]), writes=['cT'])
            S.dma('sp', lambda e: e.dma_start(out=bmodT[:], in_=bmod_d[:, :]), writes=['bmodT'])
            S.op('act', lambda e: e.activation(out=scT[:], in_=cT[:], func=AF.Silu), reads=['cT'], writes=['scT'])
            for kc in range(8):
                w = wm[kc % 2]; wk = 'wm%d' % (kc % 2)
                S.dma('sp' if kc % 2 == 0 else 'pool', lambda e, w=w, kc=kc: e.dma_start(out=w[:], in_=wmod_d[:, kc, :]), writes=[wk])
                for j in range(48):
                    S.op('pe', lambda e, w=w, kc=kc, j=j: e.matmul(PB[kc // 4][:, (kc % 4) * 96 + 2 * j:(kc % 4) * 96 + 2 * j + 2], lhsT=w[:, j * 128:(j + 1) * 128], rhs=scT[:, kc, :],
                                                                 start=True, stop=True),
                         reads=[wk, 'scT'], writes=['pb%d' % (kc // 4)], skip_self=True)
            mflat = modv[:].rearrange("p j n -> p (j n)")
            S.op('dve', lambda e: e.tensor_tensor(out=modv[:], in0=PB[0][:, 0:96].rearrange("p (j n) -> p j n", n=2), in1=bmodT[:].unsqueeze(2).to_broadcast([128, 48, 2]), op=ALU.add),
                 reads=['pb0', 'bmodT'], writes=['modv'])
            for kc in range(1, 8):
                S.op('dve', lambda e, kc=kc: e.tensor_tensor(out=mflat, in0=mflat, in1=PB[kc // 4][:, (kc % 4) * 96:(kc % 4) * 96 + 96], op=ALU.add),
                     reads=['pb%d' % (kc // 4), 'modv'], writes=['modv'])
            S.op('dve', lambda e: e.tensor_scalar_add(out=modv[:, 8:16, :], in0=modv[:, 8:16, :], scalar1=1.0), reads=['modv'], writes=['modv'])
            S.op('dve', lambda e: e.tensor_scalar_add(out=modv[:, 32:40, :], in0=modv[:, 32:40, :], scalar1=1.0), reads=['modv'], writes=['modv'])
            S.barrier()
        if debug == 'p0':
            S.dma('sp', lambda e: e.dma_start(out=dbg_d[0:128, 0:96], in_=modv[:].rearrange("p a b -> p (a b)")), reads=['modv'], writes=['dbg'])
            S.wait_all('sp', ['dbg']); S.barrier()
            return nc

        scA = ExitStack(); scB = ExitStack()
        mixT = sb(scA, "mixT", [128, 8, NLAT], BF16)
        hT = sb(scB, "hT", [128, 8, NTOK], BF16)
        wstg = sb(scB, "wstg", [128, 8, 8])
        tstg = [sb(scB, "tstg%d" % i, [128, 512]) for i in range(2)]
        tbf = [sb(scB, "tbf%d" % i, [128, 512], BF16) for i in range(2)]
        uv_d = nc.dram_tensor("uvbf", [NEXP, 2048], BF16, kind="Internal").ap()
        prep_pos = [0]

        def prep_tables(npieces):
            if debug:
                return
            for _ in range(npieces):
                i = prep_pos[0]
                if i >= 512:
                    return
                prep_pos[0] += 1
                src, coff = (pu_d, 0) if i < 256 else (pv_d, 1024)
                r = (i % 256) // 2; c = (i % 2) * 512; bb = i % 2
                S.dma('sp', lambda e, src=src, r=r, c=c, bb=bb: e.dma_start(out=tstg[bb][:], in_=src[r * 128:(r + 1) * 128, c:c + 512]), writes=['tstg%d' % bb])
                S.op('pool', lambda e, bb=bb: e.tensor_copy(out=tbf[bb][:], in_=tstg[bb][:]), reads=['tstg%d' % bb], writes=['tbf%d' % bb])
                S.dma('pool', lambda e, coff=coff, r=r, c=c, bb=bb: e.dma_start(out=uv_d[r * 128:(r + 1) * 128, coff + c:coff + c + 512], in_=tbf[bb][:]), reads=['tbf%d' % bb], writes=['tabs_%d' % i])

        def layer_norm_rows(st_ap, mv_ap, rstd_ap, src, dst, skey, dkey, tag):
            S.op('dve', lambda e: e.bn_stats(out=st_ap[:, 0, :], in_=src[:, 0:512]), reads=[skey], writes=[tag + 'st'])
            S.op('dve', lambda e: e.bn_stats(out=st_ap[:, 1, :], in_=src[:, 512:1024]), reads=[skey], writes=[tag + 'st'])
            S.op('dve', lambda e: e.bn_aggr(out=mv_ap, in_=st_ap[:].rearrange("p a b -> p (a b)")), reads=[tag + 'st'], writes=[tag + 'mv'])
            S.op('act', lambda e: e.activation(out=rstd_ap, in_=mv_ap[:, 1:2], func=AF.Sqrt, bias=epsc[:, 0:1], scale=1.0),
                 reads=[tag + 'mv', 'epsc'], writes=[tag + 'rs'])
            S.op('dve', lambda e: e.reciprocal(out=rstd_ap, in_=rstd_ap), reads=[tag + 'rs'], writes=[tag + 'rs'])
            S.op('dve', lambda e: e.tensor_scalar(out=dst, in0=src, scalar1=mv_ap[:, 0:1], scalar2=rstd_ap, op0=ALU.subtract, op1=ALU.mult),
                 reads=[skey, tag + 'mv', tag + 'rs'], writes=[dkey])

        with ExitStack() as p1:
            xt = [sb(p1, "xt%d" % i, [128, 1024]) for i in range(2)]
            xn = [sb(p1, "xn%d" % i, [128, 1024]) for i in range(2)]
            st = [sb(p1, "st%d" % i, [128, 2, 6]) for i in range(2)]
            mv = [sb(p1, "mv%d" % i, [128, 2]) for i in range(2)]
            rs = [sb(p1, "rs%d" % i, [128, 1]) for i in range(2)]
            PSAP = bool(os.environ.get("KPSAP"))
            for t in range(18):
                b = t % 2
                n = 1 if t < 2 else 0
                S.dma('sp' if b == 0 else 'pool', lambda e, t=t, b=b: e.dma_start(out=xt[b][:], in_=xs[t * 128:(t + 1) * 128, :]), writes=['xt%d' % b])
                layer_norm_rows(st[b], mv[b][:], rs[b][:], xt[b][:], xn[b][:], 'xt%d' % b, 'xn%d' % b, 'p1%d' % b)
                for half in range(2):
                    bank = 2 * b + half
                    pk = 'pb%d' % bank
                    for c4 in range(4):
                        ch = half * 4 + c4
                        S.op('pe', lambda e, b=b, ch=ch, c4=c4, bank=bank: e.transpose(PB[bank][:, c4 * 128:(c4 + 1) * 128], xn[b][:, ch * 128:(ch + 1) * 128], ident[:]),
                             reads=['xn%d' % b, 'ident'], writes=[pk], skip_self=True)
                    if not PSAP:
                        S.op('act', lambda e, b=b, half=half, bank=bank: e.copy(out=xt[b][:, half * 512:(half + 1) * 512], in_=PB[bank][:, :]), reads=[pk, 'xt%d' % b], writes=['xt%d' % b])
                    for c4 in range(4):
                        ch = half * 4 + c4
                        dst = hT[:, ch, t * 128:(t + 1) * 128]
                        src = PB[bank][:, c4 * 128:(c4 + 1) * 128] if PSAP else xt[b][:, ch * 128:(ch + 1) * 128]
                        S.op('dve', lambda e, dst=dst, src=src, ch=ch, n=n: e.tensor_scalar(out=dst, in0=src, scalar1=modv[:, 8 + ch, n:n + 1], scalar2=modv[:, ch, n:n + 1],
                                                                                      op0=ALU.mult, op1=ALU.add),
                             reads=([pk] if PSAP else ['xt%d' % b]) + ['modv'], writes=K('hT', t))
            S.barrier()

        if debug == 'p1':
            with ExitStack() as dd:
                hf = sb(dd, "hf", [128, 1024])
                for t in range(16):
                    S.op('dve', lambda e, t=t: e.tensor_copy(out=hf[:].rearrange("p (a b) -> p a b", b=128), in_=hT[:, :, 256 + t * 128:256 + (t + 1) * 128]), reads=K('hT', t + 2) + ['hf'], writes=['hf'])
                    S.dma('sp', lambda e, t=t: e.dma_start(out=dbg_d[t * 128:(t + 1) * 128, :], in_=hf[:]), reads=['hf'], writes=['dbg'])
                S.wait_all('sp', ['dbg']); S.barrier()
            scB.close(); scA.close()
            return nc
        def load_w(stk, name, col0, ncols):
            wb = sb(stk, name, [128, 8, ncols], BF16)
            if ncols == 128:
                S.dma('pool', lambda e: e.dma_start(out=wb[:], in_=winb_d[:, col0 // 128, :, :]), writes=[name])
            else:
                g0_ = col0 - 4608
                S.dma('sp', lambda e: e.dma_start(out=wstg[:, :, 0:ncols], in_=wing_d[:, :, g0_:g0_ + ncols]), writes=['wstg'])
                S.op('pool', lambda e: e.tensor_copy(out=wb[:], in_=wstg[:, :, 0:ncols]), reads=['wstg'], writes=[name])
            return wb

        BLKS = [(0, 512), (512, 512), (1024, 512), (1536, 512), (2048, 256)]

        def proj_fm_block(wb, wname, ncols, bank, t0, nt):
            for kc in range(8):
                S.op('pe', lambda e, kc=kc: e.matmul(PB[bank][0:ncols, 0:nt], lhsT=wb[:, kc, :], rhs=hT[:, kc, t0:t0 + nt], start=(kc == 0), stop=(kc == 7)),
                     reads=[wname] + K('hT', t0 // 128, (t0 + nt) // 128), writes=['pb%d' % bank], skip_self=True)

        def proj_tm_group(wb, wname, ncols, bank, c0, ncks):
            for j in range(ncks):
                n = c0 + j
                for kc in range(8):
                    S.op('pe', lambda e, kc=kc, j=j, n=n: e.matmul(PB[bank][0:64, j * ncols:(j + 1) * ncols], lhsT=hT[:, kc, n * 64:(n + 1) * 64], rhs=wb[:, kc, :],
                                                                   start=(kc == 0), stop=(kc == 7)),
                         reads=[wname] + K('hT', n // 2), writes=['pb%d' % bank], skip_self=True)

        S32 = sb(scB, "S32", [128, 2, 132]); Sbf = sb(scB, "Sbf", [128, 2, 132], BF16)
        stm = sb(scB, "stm", [64, 2, 64], BF16)
        usb = sb(scB, "usb", [128, 2, 132])

        def chunk_loop(dirn, KsT, QsT, Vi, QoT, Ku, Vu, dec_ap, W, evac, rk, tagk, su_ap=None, suk=()):
            order = list(range(36)) if dirn == 0 else [3, 2, 1, 0] + list(range(35, 3, -1))
            S.op('dve', lambda e: e.memset(S32[:, 0, :], 0.0), writes=['S32_0'])
            S.op('dve', lambda e: e.memset(Sbf[:, 0, :], 0.0), writes=['Sbf_0'])

            def pre(idx):
                n = order[idx]
                sl = slice(n * 64, (n + 1) * 64)
                s2 = idx % 2
                if n >= 4:
                    pst = PB[2 + s2][0:64, 0:64]; pstk = 'pb%d' % (2 + s2)
                    S.op('pe', lambda e: e.matmul(pst, lhsT=KsT[:, sl], rhs=QsT[:, sl], start=True, stop=True),
                         reads=rk, writes=[pstk], skip_self=True)
                    if su_ap is None:
                        S.op('dve', lambda e: e.tensor_tensor(out=stm[:, s2, :], in0=pst, in1=masks[:, dirn, :], op=ALU.mult),
                             reads=[pstk, 'masks'], writes=['stm%d' % s2])
                    else:
                        S.op('dve', lambda e: e.scalar_tensor_tensor(out=stm[:, s2, :], in0=pst, scalar=su_ap(n), in1=masks[:, dirn, :], op0=ALU.mult, op1=ALU.mult),
                             reads=[pstk, 'masks'] + list(suk), writes=['stm%d' % s2])
                if idx < 35:
                    pu = PB[6 + s2][:, 0:W]; puk = 'pb%d' % (6 + s2)
                    S.op('pe', lambda e: e.matmul(pu, lhsT=Ku[:, n, :], rhs=Vu[:, n, 0:W], start=True, stop=True),
                         reads=rk, writes=[puk], skip_self=True)
                    S.op('act', lambda e: e.copy(out=usb[:, s2, 0:W], in_=pu), reads=[puk], writes=['usb%d' % s2])

            pre(0)
            cur = 0
            for idx, n in enumerate(order):
                if idx + 1 < 36:
                    pre(idx + 1)
                sl = slice(n * 64, (n + 1) * 64)
                s2 = idx % 2
                if n >= 4:
                    po = PB[4 + s2][0:64, 0:W]; pok = 'pb%d' % (4 + s2)
                    S.op('pe', lambda e, po=po, s2=s2, n=n: e.matmul(po, lhsT=stm[:, s2, :], rhs=Vi[:, n, 0:W], start=True, stop=False),
                         reads=['stm%d' % s2] + rk, writes=[pok], skip_self=True)
                    S.op('pe', lambda e, po=po, sl=sl, cur=cur: e.matmul(po, lhsT=QoT[:, sl], rhs=Sbf[:, cur, 0:W], start=False, stop=True),
                         reads=['Sbf_%d' % cur] + rk, writes=[pok], skip_self=True)
                if idx < 35:
                    nxt = 1 - cur
                    S.op('dve', lambda e, n=n, cur=cur, nxt=nxt, s2=s2: e.scalar_tensor_tensor(out=Sbf[:, nxt, 0:W], in0=S32[:, cur, 0:W], scalar=dec_ap(n), in1=usb[:, s2, 0:W],
                                                                                            op0=ALU.mult, op1=ALU.add),
                         reads=['S32_%d' % cur, 'usb%d' % s2, tagk], writes=['Sbf_%d' % nxt])
                    S.op('dve', lambda e, n=n, cur=cur, nxt=nxt, s2=s2: e.scalar_tensor_tensor(out=S32[:, nxt, 0:W], in0=S32[:, cur, 0:W], scalar=dec_ap(n), in1=usb[:, s2, 0:W],
                                                                                            op0=ALU.mult, op1=ALU.add),
                         reads=['S32_%d' % cur, 'usb%d' % s2, tagk], writes=['S32_%d' % nxt])
                    cur = nxt
                if n >= 4:
                    evac(n - 4, n, po, pok)

        def transpose_chunks_to_mixT(src, skey, head):
            for g in range(4):
                bank = g % 2
                for j in range(8):
                    n = g * 8 + j
                    S.op('pe', lambda e, n=n, j=j, bank=bank: e.transpose(PB[bank][:, j * 64:(j + 1) * 64], src[:, n, :], ident[0:64, 0:64]),
                         reads=list(skey) + ['ident'], writes=['pb%d' % bank], skip_self=True)
                S.op('act', lambda e, g=g, bank=bank: e.copy(out=mixT[:, head, g * 512:(g + 1) * 512], in_=PB[bank][:, :]),
                     reads=['pb%d' % bank], writes=K('mixT', head))

        with ExitStack() as g0:
            lgT = sb(g0, "lgT", [128, 2, 2, 4]); lbT = sb(g0, "lbT", [128, 2, 4]); omlT = sb(g0, "omlT", [128, 2, 4]); nomlT = sb(g0, "nomlT", [128, 2, 4])
            hgn = sb(g0, "hgn", [64, 512])
            S.dma('sp', lambda e: e.dma_start(out=lgT[:], in_=lg_d[:, :, :, :]), writes=['lgT'])
            S.dma('sp', lambda e: e.dma_start(out=hgn[:], in_=hgn_d[:, :]), writes=['hgn'])
            S.op('dve', lambda e: e.tensor_tensor(out=lbT[:], in0=lgT[:, :, 0, :], in1=lgT[:, :, 1, :], op=ALU.subtract), reads=['lgT'], writes=['lbT'])
            S.op('act', lambda e: e.activation(out=lbT[:], in_=lbT[:], func=AF.Sigmoid), reads=['lbT'], writes=['lbT'])
            S.op('dve', lambda e: e.tensor_scalar(out=omlT[:], in0=lbT[:], scalar1=-1.0, scalar2=1.0, op0=ALU.mult, op1=ALU.add), reads=['lbT'], writes=['omlT'])
            S.op('dve', lambda e: e.tensor_scalar_mul(out=nomlT[:], in0=omlT[:], scalar1=-1.0), reads=['omlT'], writes=['nomlT'])
            QsT = [sb(g0, "gQsT%d" % d, [128, NTOK], BF16) for d in range(2)]
            KsT = [sb(g0, "gKsT%d" % d, [128, NTOK], BF16) for d in range(2)]
            QoT = [sb(g0, "gQoT%d" % d, [128, NTOK], BF16) for d in range(2)]
            Ku = [sb(g0, "gKu%d" % d, [64, NCH, 128], BF16) for d in range(2)]
            dec = sb(g0, "gdec", [128, 2, NCH])
            vtm = sb(g0, "gv", [64, NCH, 128], BF16); gs = sb(g0, "ggs", [64, NLCH, 128], BF16); oacc = sb(g0, "goacc", [64, NLCH, 128])
            qf = sb(g0, "gqf", [128, 512]); sg = sb(g0, "gsg", [128, 512]); lf = sb(g0, "glf", [128, 512]); key = sb(g0, "gkey", [128, 512])
            Bc = sb(g0, "gB", [128, 512]); T1 = sb(g0, "gT1", [128, 512]); T2 = sb(g0, "gT2", [128, 512]); khT = sb(g0, "gkhT", [128, 512], BF16)
            EE = [sb(g0, "gE%d" % i, [128, 512]) for i in range(4)]
            sq = sb(g0, "gsq", [64, NLCH, 128], BF16); ss = sb(g0, "gss", [64, NLCH])
            for hd in range(4):
                with ExitStack() as hs:
                    wq_ = load_w(hs, "gwq", 0 + hd * 128, 128); wi_ = load_w(hs, "gwi", 512 + hd * 128, 128); wg_ = load_w(hs, "gwg", 1024 + hd * 128, 128)
                    wf = [load_w(hs, "gwf0", 1536 + hd * 128, 128), load_w(hs, "gwf1", 2048 + hd * 128, 128)]
                    prep_tables(64)
                    for g in range(9):
                        bk = 3 + g % 2
                        proj_tm_group(wi_, "gwi", 128, bk, g * 4, 4)
                        S.op('act', lambda e, g=g, bk=bk: e.copy(out=vtm[:, g * 4:(g + 1) * 4, :], in_=PB[bk][0:64, :].rearrange("p (j c) -> p j c", c=128)),
                             reads=['pb%d' % bk], writes=['gv'])
                    for g in range(8):
                        bk = 3 + (g + 1) % 2
                        proj_tm_group(wg_, "gwg", 128, bk, 4 + g * 4, 4)
                        S.op('act', lambda e, g=g, bk=bk: e.activation(out=gs[:, g * 4:(g + 1) * 4, :], in_=PB[bk][0:64, :].rearrange("p (j c) -> p j c", c=128), func=AF.Silu),
                             reads=['pb%d' % bk], writes=['ggs'])
                    for (t0, nt) in BLKS:
                        nck = nt // 64; c0 = t0 // 64
                        proj_fm_block(wq_, "gwq", 128, 0, t0, nt)
                        S.op('act', lambda e, nt=nt: e.copy(out=qf[:, 0:nt], in_=PB[0][:, 0:nt]), reads=['pb0'], writes=['gqf'])
                        for d in range(2):
                            proj_fm_block(wf[d], "gwf%d" % d, 128, 1 + d, t0, nt)
                            col = d * 4 + hd
                            lbp = lbT[:, d, hd:hd + 1]; omp = omlT[:, d, hd:hd + 1]; nomp = nomlT[:, d, hd:hd + 1]
                            S.op('act', lambda e, d=d, nt=nt: e.activation(out=sg[:, 0:nt], in_=PB[1 + d][:, 0:nt], func=AF.Sigmoid), reads=['pb%d' % (1 + d)], writes=['gsg'])
                            S.op('act', lambda e, nt=nt, lbp=lbp, omp=omp: e.activation(out=lf[:, 0:nt], in_=sg[:, 0:nt], func=AF.Ln, bias=lbp, scale=omp),
                                 reads=['gsg', 'lbT', 'omlT'], writes=['glf'])
                            S.op('dve', lambda e, nt=nt, nomp=nomp, omp=omp: e.tensor_scalar(out=key[:, 0:nt], in0=sg[:, 0:nt], scalar1=nomp, scalar2=omp, op0=ALU.mult, op1=ALU.add),
                                 reads=['gsg', 'omlT', 'nomlT'], writes=['gkey'])
                            S.op('dve', lambda e, nt=nt: e.tensor_tensor_scan(out=Bc[:, 0:nt], data0=rmask[:, 0:nt], data1=lf[:, 0:nt], initial=0.0, op0=ALU.mult, op1=ALU.add),
                                 reads=['glf', 'rmask'], writes=['gB'])
                            B3 = Bc[:, 0:nt].rearrange("p (n c) -> p n c", c=64)
                            T3 = T1[:, 0:nt].rearrange("p (n c) -> p n c", c=64)
                            if d == 1:
                                S.op('dve', lambda e, B3=B3, T3=T3, nck=nck: e.tensor_tensor(out=T3, in0=B3[:, :, 63:64].to_broadcast([128, nck, 64]), in1=B3, op=ALU.subtract),
                                     reads=['gB'], writes=['gT1'])
                                S.op('dve', lambda e, nt=nt: e.tensor_tensor(out=Bc[:, 0:nt], in0=T1[:, 0:nt], in1=lf[:, 0:nt], op=ALU.add), reads=['gT1', 'glf'], writes=['gB'])
                            li = 63 if d == 0 else 0
                            S.op('act', lambda e, B3=B3, li=li, d=d, c0=c0, nck=nck: e.activation(out=dec[:, d, c0:c0 + nck], in_=B3[:, :, li], func=AF.Exp), reads=['gB'], writes=['gdec'])
                            T4 = T2[:, 0:nt].rearrange("p (n c) -> p n c", c=64)
                            S.op('dve', lambda e, B3=B3, T3=T3, nck=nck: e.tensor_tensor(out=T3, in0=B3, in1=B3[:, :, 32:33].to_broadcast([128, nck, 64]), op=ALU.subtract),
                                 reads=['gB'], writes=['gT1'])
                            S.op('dve', lambda e, B3=B3, T4=T4, nck=nck, li=li: e.tensor_tensor(out=T4, in0=B3[:, :, li:li + 1].to_broadcast([128, nck, 64]), in1=B3, op=ALU.subtract),
                                 reads=['gB'], writes=['gT2'])
                            S.op('act', lambda e, nt=nt: e.activation(out=EE[0][:, 0:nt], in_=T1[:, 0:nt], func=AF.Exp), reads=['gT1'], writes=['gE0'])
                            S.op('act', lambda e, nt=nt: e.activation(out=EE[1][:, 0:nt], in_=T1[:, 0:nt], func=AF.Exp, scale=-1.0), reads=['gT1'], writes=['gE1'])
                            S.op('act', lambda e, nt=nt: e.activation(out=EE[2][:, 0:nt], in_=Bc[:, 0:nt], func=AF.Exp), reads=['gB'], writes=['gE2'])
                            S.op('act', lambda e, nt=nt: e.activation(out=EE[3][:, 0:nt], in_=T2[:, 0:nt], func=AF.Exp), reads=['gT2'], writes=['gE3'])
                            S.op('dve', lambda e, nt=nt, t0=t0, d=d: e.tensor_tensor(out=QsT[d][:, t0:t0 + nt], in0=qf[:, 0:nt], in1=EE[0][:, 0:nt], op=ALU.mult),
                                 reads=['gqf', 'gE0'], writes=['gQsT%d' % d])
                            S.op('dve', lambda e, nt=nt, t0=t0, d=d: e.tensor_tensor(out=KsT[d][:, t0:t0 + nt], in0=key[:, 0:nt], in1=EE[1][:, 0:nt], op=ALU.mult),
                                 reads=['gkey', 'gE1'], writes=['gKsT%d' % d])
                            S.op('dve', lambda e, nt=nt, t0=t0, d=d: e.tensor_tensor(out=QoT[d][:, t0:t0 + nt], in0=qf[:, 0:nt], in1=EE[2][:, 0:nt], op=ALU.mult),
                                 reads=['gqf', 'gE2'], writes=['gQoT%d' % d])
                            S.op('dve', lambda e, nt=nt: e.tensor_tensor(out=khT[:, 0:nt], in0=key[:, 0:nt], in1=EE[3][:, 0:nt], op=ALU.mult), reads=['gkey', 'gE3'], writes=['gkhT'])
                            pbb = PB[3][:].bitcast(BF16)
                            for j in range(nck):
                                S.op('pe', lambda e, j=j: e.transpose(pbb[0:64, j * 128:(j + 1) * 128], khT[:, j * 64:(j + 1) * 64], identb[:]),
                                     reads=['gkhT', 'identb'], writes=['pb3'], skip_self=True)
                            S.op('act', lambda e, d=d, c0=c0, nck=nck: e.copy(out=Ku[d][:, c0:c0 + nck, :], in_=pbb[0:64, 0:nck * 128].rearrange("p (j c) -> p j c", c=128)),
                                 reads=['pb3'], writes=['gKu%d' % d])
                    for d in range(2):
                        def evac(nl, n, po, pok, d=d):
                            if d == 0:
                                S.op('act', lambda e: e.copy(out=oacc[:, nl, :], in_=po), reads=[pok], writes=K('goacc', nl))
                            else:
                                S.op('dve', lambda e: e.tensor_tensor(out=oacc[:, nl, :], in0=po, in1=oacc[:, nl, :], op=ALU.add), reads=[pok] + K('goacc', nl), writes=K('goacc', nl))
                        chunk_loop(d, KsT[d], QsT[d], vtm, QoT[d], Ku[d], vtm, lambda n, d=d: dec[:, d, n:n + 1], 128, evac,
                                   ['gQsT%d' % d, 'gKsT%d' % d, 'gQoT%d' % d, 'gKu%d' % d, 'gv'], 'gdec')
                    allo = K('goacc', 0, NLCH)
                    S.op('dve', lambda e: e.tensor_tensor(out=sq[:], in0=oacc[:], in1=oacc[:], op=ALU.mult), reads=allo, writes=['gsq'])
                    S.op('dve', lambda e: e.tensor_reduce(out=ss[:], in_=sq[:], axis=AX.X, op=ALU.add), reads=['gsq'], writes=['gss'])
                    S.op('act', lambda e: e.activation(out=ss[:], in_=ss[:], func=AF.Sqrt, bias=epsc[0:64, 0:1], scale=1.0 / 128.0), reads=['gss', 'epsc'], writes=['gss'])
                    S.op('dve', lambda e: e.reciprocal(out=ss[:], in_=ss[:]), reads=['gss'], writes=['gss'])
                    S.op('dve', lambda e: e.tensor_tensor(out=oacc[:], in0=oacc[:], in1=ss[:].unsqueeze(2).to_broadcast([64, NLCH, 128]), op=ALU.mult),
                         reads=allo + ['gss'], writes=allo)
                    S.op('dve', lambda e, hd=hd: e.tensor_tensor(out=oacc[:], in0=oacc[:], in1=hgn[:, hd * 128:(hd + 1) * 128].unsqueeze(1).to_broadcast([64, NLCH, 128]), op=ALU.mult),
                         reads=allo + ['hgn'], writes=allo)
                    S.op('dve', lambda e: e.tensor_tensor(out=oacc[:], in0=oacc[:], in1=gs[:], op=ALU.mult), reads=allo + ['ggs'], writes=allo)
                    transpose_chunks_to_mixT(oacc, allo, hd)
            S.barrier()

        with ExitStack() as m0:
            mln = sb(m0, "mln", [64, 512]); convw = sb(m0, "convw", [128, 9, 8]); convb = sb(m0, "convb", [128, 8])
            gateb = sb(m0, "gateb", [8, 2]); sel8 = sb(m0, "sel8", [8, 8]); dirm = sb(m0, "dirm", [8, 2])
            S.dma('sp', lambda e: e.dma_start(out=mln[:], in_=mln_d[:, :]), writes=['mln'])
            S.dma('sp', lambda e: e.dma_start(out=convw[:], in_=convw_d[:, :, :]), writes=['convw'])
            S.dma('sp', lambda e: e.dma_start(out=convb[:], in_=convb_d[:, :]), writes=['convb'])
            S.dma('sp', lambda e: e.dma_start(out=gateb[:], in_=gateb_d[:, :]), writes=['gateb'])
            S.dma('sp', lambda e: e.dma_start(out=sel8[:], in_=sel8_d[:, :]), writes=['sel8'])
            S.dma('sp', lambda e: e.dma_start(out=dirm[:], in_=dirm_d[:, :]), writes=['dirm'])
            RUU = sb(m0, "RUU", [64, NCH, 24]); dchunk = sb(m0, "dchunk", [128, 8, NCH])
            with ExitStack() as gp:
                wgi = load_w(gp, "mwgi", 4608, 8); wgf = load_w(gp, "mwgf", 4616, 8)
                LI = sb(gp, "LI", [8, NTOK]); LF = sb(gp, "LF", [8, NTOK]); Af = sb(gp, "Af", [8, NTOK]); Ab = sb(gp, "Ab", [8, NTOK]); Aa = sb(gp, "Aa", [8, NTOK])
                R = [sb(gp, "Rr%d" % i, [8, NTOK]) for i in range(3)]
                bd = sb(gp, "bd", [8, 8, NCH])
                for (t0, nt) in BLKS:
                    proj_fm_block(wgi, "mwgi", 8, 0, t0, nt)
                    S.op('act', lambda e, t0=t0, nt=nt: e.copy(out=LI[:, t0:t0 + nt], in_=PB[0][0:8, 0:nt]), reads=['pb0'], writes=['LI'])
                    proj_fm_block(wgf, "mwgf", 8, 1, t0, nt)
                    S.op('act', lambda e, t0=t0, nt=nt: e.copy(out=LF[:, t0:t0 + nt], in_=PB[1][0:8, 0:nt]), reads=['pb1'], writes=['LF'])
                S.op('dve', lambda e: e.tensor_scalar_add(out=LI[:], in0=LI[:], scalar1=gateb[:, 0:1]), reads=['LI', 'gateb'], writes=['LI'])
                S.op('act', lambda e: e.activation(out=LF[:], in_=LF[:], func=AF.Sigmoid, bias=gateb[:, 1:2], scale=1.0), reads=['LF', 'gateb'], writes=['LF'])
                S.op('act', lambda e: e.activation(out=LF[:], in_=LF[:], func=AF.Ln), reads=['LF'], writes=['LF'])
                for (t0, nt) in BLKS:
                    S.op('dve', lambda e, t0=t0, nt=nt: e.tensor_tensor_scan(out=Af[:, t0:t0 + nt], data0=rmask[0:8, 0:nt], data1=LF[:, t0:t0 + nt], initial=0.0, op0=ALU.mult, op1=ALU.add),
                         reads=['LF', 'rmask'], writes=['Af'])
                A3 = Af[:].rearrange("p (n c) -> p n c", c=64)
                tot = A3[:, :, 63:64]
                S.op('dve', lambda e: e.tensor_tensor(out=Ab[:].rearrange("p (n c) -> p n c", c=64), in0=tot.to_broadcast([8, NCH, 64]), in1=A3, op=ALU.subtract), reads=['Af'], writes=['Ab'])
                S.op('dve', lambda e: e.tensor_tensor(out=Ab[:], in0=Ab[:], in1=LF[:], op=ALU.add), reads=['Ab', 'LF'], writes=['Ab'])
                S.op('dve', lambda e: e.tensor_scalar_mul(out=Aa[:], in0=Af[:], scalar1=dirm[:, 0:1]), reads=['Af', 'dirm'], writes=['Aa'])
                S.op('dve', lambda e: e.scalar_tensor_tensor(out=Aa[:], in0=Ab[:], scalar=dirm[:, 1:2], in1=Aa[:], op0=ALU.mult, op1=ALU.add), reads=['Ab', 'dirm', 'Aa'], writes=['Aa'])
                S.op('act', lambda e: e.activation(out=R[0][:], in_=Aa[:], func=AF.Exp), reads=['Aa'], writes=['Rr0'])
                S.op('dve', lambda e: e.tensor_tensor(out=Ab[:], in0=LI[:], in1=Aa[:], op=ALU.subtract), reads=['LI', 'Aa', 'Ab'], writes=['Ab'])
                S.op('act', lambda e: e.activation(out=R[1][:], in_=Ab[:], func=AF.Exp), reads=['Ab'], writes=['Rr1'])
                S.op('dve', lambda e: e.tensor_tensor(out=Aa[:].rearrange("p (n c) -> p n c", c=64), in0=Ab[:].rearrange("p (n c) -> p n c", c=64), in1=tot.to_broadcast([8, NCH, 64]), op=ALU.add),
                     reads=['Ab', 'Af', 'Aa', 'Rr0'], writes=['Aa'])
                S.op('act', lambda e: e.activation(out=R[2][:], in_=Aa[:], func=AF.Exp), reads=['Aa'], writes=['Rr2'])
                for half in range(2):
                    for j in range(18):
                        n = half * 18 + j
                        for q in range(3):
                            S.op('pe', lambda e, n=n, j=j, q=q: e.transpose(PB[2][0:64, j * 24 + q * 8:j * 24 + q * 8 + 8], R[q][:, n * 64:(n + 1) * 64], ident[0:8, 0:8]),
                                 reads=['Rr%d' % q, 'ident'], writes=['pb2'], skip_self=True)
                    S.op('act', lambda e, half=half: e.copy(out=RUU[:, half * 18:(half + 1) * 18, :], in_=PB[2][0:64, 0:432].rearrange("p (j c) -> p j c", c=24)),
                         reads=['pb2'], writes=['RUU'])
                S.op('dve', lambda e: e.tensor_tensor(out=bd[:], in0=tot.rearrange("p n c -> p c n").to_broadcast([8, 8, NCH]), in1=sel8[:].unsqueeze(2).to_broadcast([8, 8, NCH]), op=ALU.mult),
                     reads=['Af', 'sel8'], writes=['bd'])
                S.op('pe', lambda e: e.matmul(PB[3][:, 0:288], lhsT=ones[0:8, :], rhs=bd[:].rearrange("p a n -> p (a n)"), start=True, stop=True), reads=['ones', 'bd'], writes=['pb3'], skip_self=True)
                S.op('act', lambda e: e.activation(out=dchunk[:].rearrange("p a n -> p (a n)"), in_=PB[3][:, 0:288], func=AF.Exp), reads=['pb3'], writes=['dchunk'])
                S.barrier()
            qc = sb(m0, "mqc", [128, NTOK], BF16); kc_ = sb(m0, "mkc", [128, NTOK], BF16); kTM = sb(m0, "mkTM", [64, NCH, 128], BF16)
            raw = sb(m0, "mraw", [128, NTOK]); acc = sb(m0, "macc", [128, NTOK])
            vext = sb(m0, "mvext", [64, NCH, 132], BF16); vu = sb(m0, "mvu", [64, NCH, 132], BF16)
            og = sb(m0, "mog", [64, NLCH, 128], BF16); oext = sb(m0, "moext", [64, NLCH, 132]); obuf = sb(m0, "mobuf", [64, NLCH, 128])
            den = sb(m0, "mden", [64, NLCH]); mu = sb(m0, "mmu", [64, NLCH]); m2 = sb(m0, "mm2", [64, NLCH])
            S.op('dve', lambda e: e.memset(vext[:], 1.0), writes=['mvext'])
            for hd in range(4):
                with ExitStack() as hs:
                    wq_ = load_w(hs, "mwq", 2560 + hd * 128, 128); wk_ = load_w(hs, "mwk", 3072 + hd * 128, 128)
                    wv_ = load_w(hs, "mwv", 3584 + hd * 128, 128); wo_ = load_w(hs, "mwo", 4096 + hd * 128, 128)
                    prep_tables(64)
                    for g in range(9):
                        bk = 3 + g % 2
                        proj_tm_group(wv_, "mwv", 128, bk, g * 4, 4)
                        S.op('act', lambda e, g=g, bk=bk: e.copy(out=vext[:, g * 4:(g + 1) * 4, 0:128], in_=PB[bk][0:64, :].rearrange("p (j c) -> p j c", c=128)),
                             reads=['pb%d' % bk], writes=['mvext'])
                    for g in range(8):
                        bk = 3 + (g + 1) % 2
                        proj_tm_group(wo_, "mwo", 128, bk, 4 + g * 4, 4)
                        S.op('act', lambda e, g=g, bk=bk: e.activation(out=og[:, g * 4:(g + 1) * 4, :], in_=PB[bk][0:64, :].rearrange("p (j c) -> p j c", c=128), func=AF.Sigmoid),
                             reads=['pb%d' % bk], writes=['mog'])
                    for qi, (wb, wn, dstb) in enumerate(((wq_, "mwq", qc), (wk_, "mwk", kc_))):
                        chn = qi * 4 + hd
                        for bi, (t0, nt) in enumerate(BLKS):
                            proj_fm_block(wb, wn, 128, bi % 2, t0, nt)
                            S.op('act', lambda e, t0=t0, nt=nt, bi=bi: e.copy(out=raw[:, t0:t0 + nt], in_=PB[bi % 2][:, 0:nt]), reads=['pb%d' % (bi % 2)], writes=['mraw'])
                        S.op('dve', lambda e, chn=chn: e.tensor_scalar(out=acc[:, 0:256], in0=raw[:, 0:256], scalar1=convw[:, 4, chn:chn + 1], scalar2=convb[:, chn:chn + 1], op0=ALU.mult, op1=ALU.add),
                             reads=['mraw', 'convw', 'convb'], writes=['macc'])
                        S.op('dve', lambda e, chn=chn: e.scalar_tensor_tensor(out=acc[:, 1:256], in0=raw[:, 0:255], scalar=convw[:, 3, chn:chn + 1], in1=acc[:, 1:256], op0=ALU.mult, op1=ALU.add),
                             reads=['mraw', 'convw', 'macc'], writes=['macc'])
                        S.op('dve', lambda e, chn=chn: e.scalar_tensor_tensor(out=acc[:, 0:255], in0=raw[:, 1:256], scalar=convw[:, 5, chn:chn + 1], in1=acc[:, 0:255], op0=ALU.mult, op1=ALU.add),
                             reads=['mraw', 'convw', 'macc'], writes=['macc'])
                        X = raw[:, 256:NTOK].rearrange("p (r c) -> p r c", c=64); Y = acc[:, 256:NTOK].rearrange("p (r c) -> p r c", c=64)
                        S.op('dve', lambda e, chn=chn: e.tensor_scalar(out=acc[:, 256:NTOK], in0=raw[:, 256:NTOK], scalar1=convw[:, 4, chn:chn + 1], scalar2=convb[:, chn:chn + 1], op0=ALU.mult, op1=ALU.add),
                             reads=['mraw', 'convw', 'convb', 'macc'], writes=['macc'])
                        for ky in range(3):
                            for kx in range(3):
                                if ky == 1 and kx == 1:
                                    continue
                                dy = ky - 1; dx = kx - 1
                                r0 = max(0, -dy); r1 = 32 - max(0, dy); c0 = max(0, -dx); c1 = 64 - max(0, dx)
                                S.op('dve', lambda e, chn=chn, ky=ky, kx=kx, r0=r0, r1=r1, c0=c0, c1=c1, dy=dy, dx=dx: e.scalar_tensor_tensor(
                                    out=Y[:, r0:r1, c0:c1], in0=X[:, r0 + dy:r1 + dy, c0 + dx:c1 + dx], scalar=convw[:, ky * 3 + kx, chn:chn + 1], in1=Y[:, r0:r1, c0:c1],
                                    op0=ALU.mult, op1=ALU.add), reads=['mraw', 'convw', 'macc'], writes=['macc'])
                        S.op('act', lambda e: e.activation(out=acc[:], in_=acc[:], func=AF.Silu), reads=['macc'], writes=['macc'])
                        if qi == 0:
                            S.op('dve', lambda e: e.tensor_copy(out=qc[:], in_=acc[:]), reads=['macc'], writes=['mqc'])
                        else:
                            S.op('dve', lambda e: e.tensor_scalar_mul(out=kc_[:], in0=acc[:], scalar1=128.0 ** -0.5), reads=['macc'], writes=['mkc'])
                    pbb = PB[3][:].bitcast(BF16)
                    for g in range(5):
                        nck = 8 if g < 4 else 4
                        for j in range(nck):
                            n = g * 8 + j
                            S.op('pe', lambda e, j=j, n=n: e.transpose(pbb[0:64, j * 128:(j + 1) * 128], kc_[:, n * 64:(n + 1) * 64], identb[:]),
                                 reads=['mkc', 'identb'], writes=['pb3'], skip_self=True)
                        S.op('act', lambda e, g=g, nck=nck: e.copy(out=kTM[:, g * 8:g * 8 + nck, :], in_=pbb[0:64, 0:nck * 128].rearrange("p (j c) -> p j c", c=128)),
                             reads=['pb3'], writes=['mkTM'])
                    for d in range(2):
                        row = d * 4 + hd
                        S.op('dve', lambda e, row=row: e.tensor_tensor(out=vu[:], in0=vext[:], in1=RUU[:, :, 16 + row:17 + row].to_broadcast([64, NCH, 132]), op=ALU.mult),
                             reads=['mvext', 'RUU'], writes=['mvu'])

                        def evac(nl, n, po, pok, row=row):
                            S.op('act', lambda e: e.activation(out=oext[:, nl, 0:129], in_=po, func=AF.Identity, scale=RUU[:, n, row:row + 1]), reads=[pok, 'RUU'], writes=K('moext', nl))
                        chunk_loop(d, kc_, qc, vext, qc, kTM, vu, lambda n, row=row: dchunk[:, row, n:n + 1], 129, evac,
                                   ['mqc', 'mkc', 'mvext', 'mvu', 'mkTM'], 'dchunk',
                                   su_ap=lambda n, row=row: RUU[:, n, 8 + row:9 + row], suk=['RUU'])
                        allx = K('moext', 0, NLCH)
                        S.op('act', lambda e: e.activation(out=den[:], in_=oext[:, :, 128], func=AF.Abs), reads=allx, writes=['mden'])
                        S.op('dve', lambda e: e.tensor_scalar_max(out=den[:], in0=den[:], scalar1=1.0), reads=['mden'], writes=['mden'])
                        S.op('dve', lambda e: e.reciprocal(out=den[:], in_=den[:]), reads=['mden'], writes=['mden'])
                        if d == 0:
                            S.op('dve', lambda e: e.tensor_tensor(out=obuf[:], in0=oext[:, :, 0:128], in1=den[:].unsqueeze(2).to_broadcast([64, NLCH, 128]), op=ALU.mult),
                                 reads=allx + ['mden'], writes=['mobuf'])
                        else:
                            S.op('dve', lambda e: e.tensor_tensor(out=oext[:, :, 0:128], in0=oext[:, :, 0:128], in1=den[:].unsqueeze(2).to_broadcast([64, NLCH, 128]), op=ALU.mult),
                                 reads=allx + ['mden'], writes=allx)
                            S.op('dve', lambda e: e.tensor_tensor(out=obuf[:], in0=obuf[:], in1=oext[:, :, 0:128], op=ALU.add), reads=allx + ['mobuf'], writes=['mobuf'])
                    allx = K('moext', 0, NLCH)
                    S.op('dve', lambda e: e.tensor_reduce(out=mu[:], in_=obuf[:], axis=AX.X, op=ALU.add), reads=['mobuf'], writes=['mmu'])
                    S.op('dve', lambda e: e.tensor_scalar_mul(out=mu[:], in0=mu[:], scalar1=1.0 / 128.0), reads=['mmu'], writes=['mmu'])
                    S.op('dve', lambda e: e.tensor_tensor(out=obuf[:], in0=obuf[:], in1=mu[:].unsqueeze(2).to_broadcast([64, NLCH, 128]), op=ALU.subtract), reads=['mobuf', 'mmu'], writes=['mobuf'])
                    S.op('dve', lambda e: e.tensor_tensor(out=oext[:, :, 0:128], in0=obuf[:], in1=obuf[:], op=ALU.mult), reads=['mobuf'] + allx, writes=allx)
                    S.op('dve', lambda e: e.tensor_reduce(out=m2[:], in_=oext[:, :, 0:128], axis=AX.X, op=ALU.add), reads=allx, writes=['mm2'])
                    S.op('act', lambda e: e.activation(out=m2[:], in_=m2[:], func=AF.Sqrt, bias=epsc[0:64, 0:1], scale=1.0 / 128.0), reads=['mm2', 'epsc'], writes=['mm2'])
                    S.op('dve', lambda e: e.reciprocal(out=m2[:], in_=m2[:]), reads=['mm2'], writes=['mm2'])
                    S.op('dve', lambda e: e.tensor_tensor(out=obuf[:], in0=obuf[:], in1=m2[:].unsqueeze(2).to_broadcast([64, NLCH, 128]), op=ALU.mult), reads=['mobuf', 'mm2'], writes=['mobuf'])
                    S.op('dve', lambda e, hd=hd: e.tensor_tensor(out=obuf[:], in0=obuf[:], in1=mln[:, hd * 128:(hd + 1) * 128].unsqueeze(1).to_broadcast([64, NLCH, 128]), op=ALU.mult),
                         reads=['mobuf', 'mln'], writes=['mobuf'])
                    S.op('dve', lambda e: e.tensor_tensor(out=obuf[:], in0=obuf[:], in1=og[:], op=ALU.mult), reads=['mobuf', 'mog'], writes=['mobuf'])
                    transpose_chunks_to_mixT(obuf, ['mobuf'], 4 + hd)
            S.barrier()

        if debug == 'mix':
            with ExitStack() as dd:
                mf = sb(dd, "mf", [128, NLAT])
                for h in range(8):
                    S.op('dve', lambda e, h=h: e.tensor_copy(out=mf[:], in_=mixT[:, h, :]), reads=K('mixT', h) + ['mf'], writes=['mf'])
                    S.dma('sp', lambda e, h=h: e.dma_start(out=dbg_d[h * 128:(h + 1) * 128, :], in_=mf[:]), reads=['mf'], writes=['dbg'])
                S.wait_all('sp', ['dbg']); S.barrier()
            scB.close(); scA.close()
            return nc
        prep_tables(512)
        S.barrier()
        scB.close()
        x1s = nc.dram_tensor("x1s", [NLAT, 1024], F32, kind="Internal").ap()

        def bcast_tile(dst, dkey, c0, dg):
            for ch in range(8):
                S.op('dve', lambda e, ch=ch: e.tensor_scalar_mul(out=dg[:], in0=ident[:], scalar1=modv[:, c0 + ch, 0:1]), reads=['ident', 'modv', 'dg'], writes=['dg'])
                bank = ch // 4
                S.op('pe', lambda e, ch=ch, bank=bank: e.matmul(PB[bank][:, (ch % 4) * 128:(ch % 4 + 1) * 128], lhsT=ones[:], rhs=dg[:], start=True, stop=True),
                     reads=['ones', 'dg'], writes=['pb%d' % bank], skip_self=True)
            S.op('act', lambda e: e.copy(out=dst[:, 0:512], in_=PB[0][:, :]), reads=['pb0'], writes=[dkey])
            S.op('act', lambda e: e.copy(out=dst[:, 512:1024], in_=PB[1][:, :]), reads=['pb1'], writes=[dkey])

        with ExitStack() as p3:
            bct = {}
            for nm in ("g1b", "ln1g", "ln1b"):
                bct[nm] = sb(p3, nm, [128, 1024])
            for nm, dd in (("ln1g", ln1g_d), ("ln1b", ln1b_d)):
                S.dma('sp', lambda e, nm=nm, dd=dd: e.dma_start(out=bct[nm][:], in_=dd[:, :]), writes=[nm])
            dg = sb(p3, "dg", [128, 128])
            bcast_tile(bct["g1b"], "g1b", 16, dg)
            wo_b = sb(p3, "wout", [128, 8, 1024], BF16)
            for kc in range(8):
                S.dma('pool', lambda e, kc=kc: e.dma_start(out=wo_b[:, kc, :], in_=wout_d[:, kc, :]), writes=['wout%d' % kc])
            xt = [sb(p3, "x3t%d" % i, [128, 1024]) for i in range(2)]
            t1 = [sb(p3, "t1_%d" % i, [128, 1024]) for i in range(2)]
            st = [sb(p3, "st3%d" % i, [128, 2, 6]) for i in range(2)]
            mv = [sb(p3, "mv3%d" % i, [128, 2]) for i in range(2)]
            rs = [sb(p3, "rs3%d" % i, [128, 1]) for i in range(2)]
            for t in range(16):
                b = t % 2
                S.dma('sp' if b == 0 else 'pool', lambda e, t=t, b=b: e.dma_start(out=xt[b][:], in_=xs[256 + t * 128:256 + (t + 1) * 128, :]), writes=['x3t%d' % b])
                for half in range(2):
                    bank = 2 * b + half
                    for kc in range(8):
                        S.op('pe', lambda e, kc=kc, t=t, half=half, bank=bank: e.matmul(PB[bank][:, :], lhsT=mixT[:, kc, t * 128:(t + 1) * 128], rhs=wo_b[:, kc, half * 512:(half + 1) * 512],
                                                                                      start=(kc == 0), stop=(kc == 7)),
                             reads=K('mixT', kc) + ['wout%d' % kc], writes=['pb%d' % bank], skip_self=True)
                    S.op('dve', lambda e, b=b, half=half, bank=bank: e.tensor_tensor(out=t1[b][:, half * 512:(half + 1) * 512], in0=PB[bank][:, :], in1=bct["g1b"][:, half * 512:(half + 1) * 512], op=ALU.mult),
                         reads=['pb%d' % bank, 'g1b'], writes=['t1_%d' % b])
                S.op('dve', lambda e, b=b: e.scalar_tensor_tensor(out=t1[b][:], in0=xt[b][:], scalar=ALPHA, in1=t1[b][:], op0=ALU.mult, op1=ALU.add),
                     reads=['x3t%d' % b, 't1_%d' % b], writes=['t1_%d' % b])
                layer_norm_rows(st[b], mv[b][:], rs[b][:], t1[b][:], t1[b][:], 't1_%d' % b, 't1_%d' % b, 'p3%d' % b)
                S.op('dve', lambda e, b=b: e.tensor_tensor(out=t1[b][:], in0=t1[b][:], in1=bct["ln1g"][:], op=ALU.mult), reads=['t1_%d' % b, 'ln1g'], writes=['t1_%d' % b])
                S.op('dve', lambda e, b=b, t=t: e.tensor_tensor(out=t1[b][:], in0=t1[b][:], in1=bct["ln1b"][:], op=ALU.add), reads=['t1_%d' % b, 'ln1b'], writes=['t1_%d' % b])
                S.dma('sp', lambda e, t=t, b=b: e.dma_start(out=x1s[t * 128:(t + 1) * 128, :], in_=t1[b][:]), reads=['t1_%d' % b], writes=K('x1_', t))
                if debug == 'x1':
                    S.dma('sp', lambda e, t=t, b=b: e.dma_start(out=dbg_d[t * 128:(t + 1) * 128, :], in_=t1[b][:]), reads=['t1_%d' % b], writes=['dbg'])
            S.barrier()
        scA.close()

        with ExitStack() as p4:
            bct = {}
            for nm in ("g2b", "sc2b", "sh2b", "ln2g", "ln2b"):
                bct[nm] = sb(p4, nm, [128, 1024])
            for nm, dd in (("ln2g", ln2g_d), ("ln2b", ln2b_d)):
                S.dma('sp', lambda e, nm=nm, dd=dd: e.dma_start(out=bct[nm][:], in_=dd[:, :]), writes=[nm])
            dg = sb(p4, "dg4", [128, 128])
            bcast_tile(bct["g2b"], "g2b", 40, dg); bcast_tile(bct["sc2b"], "sc2b", 32, dg); bcast_tile(bct["sh2b"], "sh2b", 24, dg)
            x1t = [sb(p4, "x1t%d" % i, [128, 1024]) for i in range(2)]
            wqb = sb(p4, "wqb", [128, 8, 2048], BF16)
            keysT = sb(p4, "keysT", [128, 16, 128], BF16)
            for kc in range(8):
                for hh in range(2):
                    S.dma('pool', lambda e, kc=kc, hh=hh: e.dma_start(out=wqb[:, kc, hh * 1024:(hh + 1) * 1024], in_=wq_d[:, kc, hh * 1024:(hh + 1) * 1024]), writes=['wqb%d_%d' % (kc, hh)])
            for hh in range(2):
                S.dma('pool', lambda e, hh=hh: e.dma_start(out=keysT[:, hh * 8:(hh + 1) * 8, :], in_=keysT_d[:, hh * 8:(hh + 1) * 8, :]), writes=['keysT%d' % hh])
            S.barrier()
            NG = int(os.environ.get('KNG', '16'))
            GW = int(os.environ.get('KGW', '2048'))
            uvb = [sb(p4, "uvb%d" % i, [128, 2048], BF16) for i in range(NG)]
            gl = sb(p4, "gl", [128, 128])
            pr = [sb(p4, "pr%d" % i, [128, 1024], BF16) for i in range(3)]
            dgk = [sb(p4, "dgk%d" % i, [128, 128], BF16) for i in range(4)]
            h2 = sb(p4, "h2", [128, 1024]); h2b = [sb(p4, "h2b%d" % i, [128, 1024], BF16) for i in range(2)]
            h2T = sb(p4, "h2T", [128, 8, 128], BF16); qT = sb(p4, "qT", [128, 16, 128], BF16)
            sc = sb(p4, "sc", [128, 16, 128]); scw = sb(p4, "scw", [128, 16, 128])
            top = sb(p4, "top", [128, 16, 16]); topi = sb(p4, "topi", [128, 16, 16], U32); topf = sb(p4, "topf", [128, 16, 16])
            candw = sb(p4, "candw", [128, 8, 256])
            cand = View(scw[:].rearrange("p (h two) k -> p h (two k)", two=2)); eq = View(candw[:].rearrange("p h (a b) -> p (h a) b", b=16))
            best = sb(p4, "best", [128, 8, 16]); idxf = sb(p4, "idxf", [128, 128])
            posi = sb(p4, "posi", [128, 8, 16], U32); pai = sb(p4, "pai", [128, 128], U32); pbi = sb(p4, "pbi", [128, 128], U32)
            paf = sb(p4, "paf", [128, 128]); pbf = sb(p4, "pbf", [128, 128]); iaf = sb(p4, "iaf", [128, 128]); ibf = sb(p4, "ibf", [128, 128])
            iota16 = sb(p4, "iota16", [128, 16])
            S.dma('sp', lambda e: e.dma_start(out=iota16[:], in_=iota_d[:, :]), writes=['iota16'])
            gate = [sb(p4, "gate%d" % i, [128, 128]) for i in range(2)]
            idxi = [sb(p4, "idxi%d" % i, [128, 128], I32) for i in range(2)]
            dots = sb(p4, "dots", [128, 128]); junk = sb(p4, "junk", [128, 1024], BF16)
            zs = sb(p4, "zs", [128, 8]); nmx = sb(p4, "nmx", [128, 8])
            st = sb(p4, "st4", [128, 2, 6]); mv = sb(p4, "mv4", [128, 2]); rs = sb(p4, "rs4", [128, 1])
            fin = View(sc[:].rearrange("p c k -> p (c k)")[:, 0:1024]); yb = h2
            NT4 = 16 if debug != 'x1' else 0

            def prologue(t):
                p = t % 2
                xb_ = x1t[p]; x1k = 'x1t%d' % p
                S.dma('sp', lambda e: e.dma_start(out=xb_[:], in_=x1s[t * 128:(t + 1) * 128, :]), reads=K('x1_', t), writes=[x1k])
                S.op('dve', lambda e: e.memset(idxf[:], 0.0), writes=['idxf'])
                S.op('dve', lambda e: e.memset(zs[:], 0.0), writes=['zs'])
                layer_norm_rows(st, mv[:], rs[:], xb_[:], h2[:], x1k, 'h2', 'p4')
                S.op('dve', lambda e: e.tensor_tensor(out=h2[:], in0=h2[:], in1=bct["sc2b"][:], op=ALU.mult), reads=['h2', 'sc2b'], writes=['h2'])
                S.op('dve', lambda e: e.tensor_tensor(out=h2[:], in0=h2[:], in1=bct["sh2b"][:], op=ALU.add), reads=['h2', 'sh2b'], writes=['h2'])
                S.op('act', lambda e: e.copy(out=h2b[p][:], in_=h2[:]), reads=['h2'], writes=['h2b%d' % p])
                yield
                for half in range(2):
                    for c4 in range(4):
                        ch = half * 4 + c4
                        S.op('pe', lambda e, ch=ch, c4=c4, half=half: e.transpose(PB[half][:, c4 * 128:(c4 + 1) * 128], h2[:, ch * 128:(ch + 1) * 128], ident[:]),
                             reads=['h2', 'ident'], writes=['pb%d' % half], skip_self=True)
                    yield
                    S.op('act', lambda e, half=half: e.copy(out=h2T[:, half * 4:(half + 1) * 4, :], in_=PB[half][:, :].rearrange("p (j c) -> p j c", c=128)), reads=['pb%d' % half], writes=['h2T'])
                for g in range(4):
                    for c4 in range(4):
                        c = g * 4 + c4
                        for kc in range(8):
                            S.op('pe', lambda e, c=c, c4=c4, kc=kc, g=g: e.matmul(PB[2 + g][:, c4 * 128:(c4 + 1) * 128], lhsT=wqb[:, kc, c * 128:(c + 1) * 128], rhs=h2T[:, kc, :],
                                                                                start=(kc == 0), stop=(kc == 7)), reads=['wqb%d_%d' % (kc, c // 8), 'h2T'], writes=['pb%d' % (2 + g)], skip_self=True)
                    yield
                    S.op('act' if g % 2 == 0 else 'dve', lambda e, g=g: (e.copy if g % 2 == 0 else e.tensor_copy)(out=qT[:, g * 4:(g + 1) * 4, :], in_=PB[2 + g][:, :].rearrange("p (j c) -> p j c", c=128)),
                         reads=['pb%d' % (2 + g)], writes=['qT'])
                for g in range(4):
                    for c4 in range(4):
                        c = g * 4 + c4
                        S.op('pe', lambda e, c=c, c4=c4, g=g: e.matmul(PB[2 + g][:, c4 * 128:(c4 + 1) * 128], lhsT=qT[:, c, :], rhs=keysT[:, c, :], start=True, stop=True),
                             reads=['qT', 'keysT%d' % (c // 8)], writes=['pb%d' % (2 + g)], skip_self=True)
                    yield
                    S.op('act' if g % 2 == 0 else 'dve', lambda e, g=g: (e.copy if g % 2 == 0 else e.tensor_copy)(out=sc[:, g * 4:(g + 1) * 4, :], in_=PB[2 + g][:, :].rearrange("p (j c) -> p j c", c=128)),
                         reads=['pb%d' % (2 + g)], writes=['sc'])
                for c in range(16):
                    S.op('dve', lambda e, c=c: e.max(out=top[:, c, 0:8], in_=sc[:, c, :]), reads=['sc'], writes=['top'])
                    S.op('dve', lambda e, c=c: e.max_index(out=topi[:, c, 0:8], in_max=top[:, c, 0:8], in_values=sc[:, c, :]), reads=['sc', 'top'], writes=['topi'])
                    S.op('dve', lambda e, c=c: e.match_replace(out=scw[:, c, :], in_to_replace=top[:, c, 0:8], in_values=sc[:, c, :], imm_value=-1e30), reads=['sc', 'top'], writes=['scw'])
                    S.op('dve', lambda e, c=c: e.max(out=top[:, c, 8:16], in_=scw[:, c, :]), reads=['scw'], writes=['top'])
                    S.op('dve', lambda e, c=c: e.max_index(out=topi[:, c, 8:16], in_max=top[:, c, 8:16], in_values=scw[:, c, :]), reads=['scw', 'top'], writes=['topi'])
                    yield
                S.op('dve', lambda e: e.tensor_copy(out=topf[:], in_=topi[:]), reads=['topi'], writes=['topf'])
                t4 = top[:].rearrange("p (h two) k -> p h two k", two=2); f4 = topf[:].rearrange("p (h two) k -> p h two k", two=2)
                c4v = cand[:].rearrange("p h (a b) -> p h a b", b=16)
                for h in range(8):
                    S.op('dve', lambda e, h=h: e.tensor_tensor(out=c4v[:, h, :, :], in0=t4[:, h, 0, :].unsqueeze(2).to_broadcast([128, 16, 16]), in1=t4[:, h, 1, :].unsqueeze(1).to_broadcast([128, 16, 16]), op=ALU.add),
                         reads=['top'], writes=['scw'])
                    yield
                for h in range(8):
                    S.op('dve', lambda e, h=h: e.max(out=best[:, h, 0:8], in_=cand[:, h, :]), reads=['scw'], writes=['best'])
                    S.op('dve', lambda e, h=h: e.match_replace(out=candw[:, h, :], in_to_replace=best[:, h, 0:8], in_values=cand[:, h, :], imm_value=-1e30), reads=['scw', 'best'], writes=['candw'])
                    S.op('dve', lambda e, h=h: e.max(out=best[:, h, 8:16], in_=candw[:, h, :]), reads=['candw'], writes=['best'])
                    S.op('dve', lambda e, h=h: e.max_index(out=posi[:, h, 0:8], in_max=best[:, h, 0:8], in_values=cand[:, h, :]), reads=['scw', 'best'], writes=['posi'])
                    S.op('dve', lambda e, h=h: e.max_index(out=posi[:, h, 8:16], in_max=best[:, h, 8:16], in_values=candw[:, h, :]), reads=['candw', 'best'], writes=['posi'])
                    yield
                pflat = posi[:].rearrange("p h k -> p (h k)")
                S.op('dve', lambda e: e.tensor_single_scalar(out=pai[:], in_=pflat, scalar=4, op=ALU.logical_shift_right), reads=['posi'], writes=['pai'])
                S.op('dve', lambda e: e.tensor_single_scalar(out=pbi[:], in_=pflat, scalar=15, op=ALU.bitwise_and), reads=['posi'], writes=['pbi'])
                S.op('dve', lambda e: e.tensor_copy(out=paf[:], in_=pai[:]), reads=['pai'], writes=['paf'])
                S.op('dve', lambda e: e.tensor_copy(out=pbf[:], in_=pbi[:]), reads=['pbi'], writes=['pbf'])
                yield
                eq4 = eq[:].rearrange("p (h k) a -> p h k a", k=16)
                for (pp, pk_, half_, dst, dn) in ((paf, 'paf', 0, iaf, 'iaf'), (pbf, 'pbf', 1, ibf, 'ibf')):
                    S.op('dve', lambda e, pp=pp: e.tensor_tensor(out=eq[:], in0=iota16[:].unsqueeze(1).to_broadcast([128, 128, 16]), in1=pp[:].unsqueeze(2).to_broadcast([128, 128, 16]), op=ALU.is_equal),
                         reads=['iota16', pk_, 'candw'], writes=['candw'])
                    S.op('dve', lambda e, half_=half_: e.tensor_tensor(out=eq4, in0=eq4, in1=f4[:, :, half_, :].unsqueeze(2).to_broadcast([128, 8, 16, 16]), op=ALU.mult),
                         reads=['candw', 'topf'], writes=['candw'])
                    S.op('dve', lambda e, dst=dst: e.tensor_reduce(out=dst[:], in_=eq[:], axis=AX.X, op=ALU.add), reads=['candw'], writes=[dn])
                    yield
                S.op('dve', lambda e: e.scalar_tensor_tensor(out=idxf[:], in0=iaf[:], scalar=128.0, in1=ibf[:], op0=ALU.mult, op1=ALU.add), reads=['iaf', 'ibf'], writes=['idxf'])
                S.op('dve', lambda e: e.tensor_scalar_min(out=idxf[:], in0=idxf[:], scalar1=16383.0), reads=['idxf'], writes=['idxf'])
                S.op('dve', lambda e: e.tensor_copy(out=idxi[p][:], in_=idxf[:]), reads=['idxf'], writes=['idxi%d' % p])
                S.op('dve', lambda e: e.tensor_scalar_mul(out=nmx[:], in0=best[:, :, 0], scalar1=-1.0), reads=['best'], writes=['nmx'])
                g3 = gate[p][:].rearrange("p (h k) -> p h k", k=16)
                for h in range(8):
                    S.op('act', lambda e, h=h: e.activation(out=g3[:, h, :], in_=best[:, h, :], func=AF.Exp, bias=nmx[:, h:h + 1], scale=1.0, accum_out=zs[:, h:h + 1]),
                         reads=['best', 'nmx'], writes=['gate%d' % p, 'zs'])
                S.op('dve', lambda e: e.reciprocal(out=zs[:], in_=zs[:]), reads=['zs'], writes=['zs'])
                S.op('dve', lambda e: e.tensor_tensor(out=g3, in0=g3, in1=zs[:].unsqueeze(2).to_broadcast([128, 8, 16]), op=ALU.mult), reads=['gate%d' % p, 'zs'], writes=['gate%d' % p])

            def fused(t, gen=None):
                p = t % 2
                LAG = 2

                def tail(kk):
                    s_ = (t * 128 + kk) % NG; d4 = kk % 4
                    S.op('dve', lambda e: e.tensor_scalar(out=dgk[d4][:], in0=identb[:], scalar1=gl[:, kk:kk + 1], scalar2=gate[p][:, kk:kk + 1], op0=ALU.mult, op1=ALU.mult),
                         reads=['identb', 'gl_%d' % kk, 'gate%d' % p], writes=['dgk%d' % d4])
                    for half in range(2):
                        S.op('pe', lambda e, half=half: e.matmul(PB[6 + half][:, :], lhsT=dgk[d4][:], rhs=uvb[s_][:, 1024 + half * 512:1024 + (half + 1) * 512], start=(kk == 0), stop=(kk == 127)),
                             reads=['dgk%d' % d4, 'uvb%d' % s_], writes=['pb%d' % (6 + half)], skip_self=True)

                for k in range(128):
                    s_ = (t * 128 + k) % NG; j4 = k % 3
                    dk = 'dots_%d' % k
                    S.dma('pool', lambda e, k=k, s_=s_: e.indirect_dma_start(out=uvb[s_][:, 0:GW], out_offset=None, in_=uv_d[:, 0:GW], in_offset=bass.IndirectOffsetOnAxis(ap=idxi[p][:, k:k + 1], axis=0)),
                          reads=['idxi%d' % p], writes=['uvb%d' % s_], lean=True)
                    S.op('dve', lambda e, k=k, s_=s_, j4=j4: e.tensor_tensor(out=pr[j4][:], in0=uvb[s_][:, 0:1024], in1=h2b[p][:], op=ALU.mult),
                         reads=['uvb%d' % s_, 'h2b%d' % p], writes=['pr%d' % j4])
                    S.op('act', lambda e, k=k, j4=j4: e.activation(out=junk[:], in_=pr[j4][:], func=AF.Identity, accum_out=dots[:, k:k + 1]),
                         reads=['pr%d' % j4, 'dots0'], writes=[dk])
                    S.op('act', lambda e, k=k: e.activation(out=gl[:, k:k + 1], in_=dots[:, k:k + 1], func=AF.Gelu), reads=[dk], writes=['gl_%d' % k])
                    if k >= LAG:
                        tail(k - LAG)
                    if gen is not None and k % 2 == 1:
                        next(gen, None)
                for kk in range(128 - LAG, 128):
                    tail(kk)
                if gen is not None:
                    for _ in gen:
                        pass
                xb_ = x1t[p]; x1k = 'x1t%d' % p
                for half in range(2):
                    S.op('dve', lambda e, half=half: e.tensor_tensor(out=yb[:, half * 512:(half + 1) * 512], in0=PB[6 + half][:, :], in1=bct["g2b"][:, half * 512:(half + 1) * 512], op=ALU.mult),
                         reads=['pb%d' % (6 + half), 'g2b', 'h2'], writes=['h2'])
                S.op('dve', lambda e: e.scalar_tensor_tensor(out=fin[:], in0=xb_[:], scalar=ALPHA, in1=yb[:], op0=ALU.mult, op1=ALU.add), reads=[x1k, 'h2', 'sc'], writes=['sc'])
                layer_norm_rows(stf, mvf[:], rsf[:], fin[:], fin[:], 'sc', 'sc', 'p4f')
                S.op('dve', lambda e: e.tensor_tensor(out=fin[:], in0=fin[:], in1=bct["ln2g"][:], op=ALU.mult), reads=['sc', 'ln2g'], writes=['sc'])
                S.op('dve', lambda e: e.tensor_tensor(out=fin[:], in0=fin[:], in1=bct["ln2b"][:], op=ALU.add), reads=['sc', 'ln2b'], writes=['sc'])
                S.dma('sp', lambda e: e.dma_start(out=out_d[t * 128:(t + 1) * 128, :], in_=fin[:]), reads=['sc'], writes=['out'])

            stf = sb(p4, "st4f", [128, 2, 6]); mvf = sb(p4, "mv4f", [128, 2]); rsf = sb(p4, "rs4f", [128, 1])
            S.op('dve', lambda e: e.memset(dots[:], 0.0), writes=['dots0'])
            S.wait_all('pool', ['tabs_%d' % i for i in range(512)])
            if NT4:
                for _ in prologue(0):
                    pass
            for t in range(NT4):
                fused(t, prologue(t + 1) if t + 1 < NT4 else None)
            S.wait_all('sp', ['out', 'dbg'])
            S.barrier()
    return nc


def _prep_shared(inp):
    f = np.float32
    sh = {}
    sh["w_mod"] = np.ascontiguousarray(inp["w_mod"][0].reshape(8, 128, 6144).transpose(1, 0, 2))
    sh["b_modT"] = np.ascontiguousarray(inp["b_mod"][0].reshape(48, 128).T)
    sh["w_inb"] = np.ascontiguousarray(inp["w_in"][0][:, :4608].reshape(8, 128, 36, 128).transpose(1, 2, 0, 3))
    sh["w_ing"] = np.ascontiguousarray(inp["w_in"][0][:, 4608:].reshape(8, 128, 16).transpose(1, 0, 2))
    sh["lgT"] = np.ascontiguousarray(inp["hg_lb_logits"].reshape(2, 2, 4, 128).transpose(3, 0, 1, 2))
    sh["hgn"] = np.ascontiguousarray(np.broadcast_to(inp["hg_norm_g"][0][None, :], (64, 512)))
    sh["mln"] = np.ascontiguousarray(np.broadcast_to(inp["ml_norm_g"][0][None, :], (64, 512)))
    sh["convw"] = np.ascontiguousarray(inp["ml_conv_w"][0].reshape(9, 8, 128).transpose(2, 0, 1))
    sh["convb"] = np.ascontiguousarray(inp["ml_conv_b"][0].reshape(8, 128).T)
    sh["gateb"] = np.ascontiguousarray(inp["ml_gate_b"][0].reshape(2, 8).T)
    sh["w_out"] = np.ascontiguousarray(inp["w_out"][0].reshape(8, 128, 1024).transpose(1, 0, 2))
    for nm, key in (("ln1g", "ln1_g"), ("ln1b", "ln1_b"), ("ln2g", "ln2_g"), ("ln2b", "ln2_b")):
        sh[nm] = np.ascontiguousarray(np.broadcast_to(inp[key][0][None, :], (128, 1024)))
    sh["wq"] = np.ascontiguousarray(inp["peer_wq"][0].reshape(8, 128, 2048).transpose(1, 0, 2))
    sh["keysT"] = np.ascontiguousarray(inp["peer_keys"][0].reshape(16, 128, 128).transpose(2, 0, 1))
    nexp = 128 if os.environ.get("KDEBUG") else 16384
    sh["pu"] = np.ascontiguousarray(inp["peer_u"][0][:nexp])
    sh["pv"] = np.ascontiguousarray(inp["peer_v"][0][:nexp])
    sh["ident"] = np.eye(128, dtype=f)
    m = np.zeros((64, 2, 64), f)
    s = np.arange(64)[:, None]; c = np.arange(64)[None, :]
    m[:, 0, :] = (s <= c); m[:, 1, :] = (s >= c)
    sh["masks"] = m
    rm = np.ones((128, 512), f); rm[:, ::64] = 0.0
    sh["rmask"] = rm
    sh["sel8"] = np.eye(8, dtype=f)
    sh["iota16"] = np.ascontiguousarray(np.broadcast_to(np.arange(16, dtype=f)[None, :], (128, 16)))
    dm = np.zeros((8, 2), f); dm[0:4, 0] = 1.0; dm[4:8, 1] = 1.0
    sh["dirm"] = dm
    return {k: np.asarray(v, dtype=f) for k, v in sh.items()}


def kernel(**inputs):
    inp = {k: np.asarray(v) for k, v in inputs.items()}
    debug = os.environ.get("KDEBUG") or None
    nc = build(debug)
    sh = _prep_shared(inp)
    in_maps = []
    for b in range(8):
        m = dict(sh)
        m["xs"] = np.ascontiguousarray(np.concatenate([inp["ctx"][b], inp["x"][b]], axis=0).astype(np.float32))
        m["cT"] = np.ascontiguousarray(np.stack([inp["c"][b], inp["c_ctx"]], axis=-1).reshape(8, 128, 2).transpose(1, 0, 2).astype(np.float32))
        in_maps.append(m)
    res = run_bass_kernel_spmd(nc, in_maps, core_ids=list(range(8)))
    key = "dbg" if debug else "out"
    return np.stack([np.asarray(r[key]) for r in res.results], axis=0).astype(np.float32)
```
